# Optimizing a Trainium2 kernel written in Bass

```python
import jax, jax.numpy as jnp
from jax import lax
import numpy as np

D_MODEL = 1024
BATCH = 2
SEQ = 8192
DEPTH = 1

NSA_HEADS = 8
NSA_KV_GROUPS = 2
NSA_HEAD_DIM = 64
NSA_HEADS_PER_GROUP = NSA_HEADS // NSA_KV_GROUPS
NSA_WIDTH = NSA_HEADS * NSA_HEAD_DIM
NSA_KV_WIDTH = NSA_KV_GROUPS * NSA_HEAD_DIM
CMP_LEN = 32
CMP_STRIDE = 16
SLC_LEN = 64
SLC_TOPN = 16
WINDOW = 512
Q_BLOCK = 128
ROPE_THETA = 500000.0
ROPE_DIM = NSA_HEAD_DIM // 4
HG_HEADS = 4
HG_KEY_DIM = 128
HG_VAL_DIM = 128
HG_WIDTH = HG_HEADS * HG_VAL_DIM
HG_CHUNK = 64
N_GROUPS = 4
EXPERTS_PER_GROUP = 8
N_EXPERTS = N_GROUPS * EXPERTS_PER_GROUP
TOP_K_IN_GROUP = 2
D_FF_EXPERT = 512
RMS_EPS = 1e-6
NEG_INF = -1e30
FORCE_SCORE = 1e4

IN_SPLITS = [NSA_WIDTH, 6 * NSA_KV_WIDTH, 3 * NSA_HEADS,
             HG_HEADS * HG_KEY_DIM, HG_HEADS * HG_KEY_DIM, HG_WIDTH, HG_WIDTH, 2 * D_MODEL]
IN_COLS = sum(IN_SPLITS)

kernel_name = 'hybrid_nsa_hgrn2_hmoe'


def rmsnorm(x, g):
    x32 = x.astype(jnp.float32)
    y = x32 * lax.rsqrt(jnp.mean(x32 * x32, axis=-1, keepdims=True) + RMS_EPS)
    return (y * g.astype(jnp.float32)).astype(x.dtype)


def partial_rope(t, pos):
    half = ROPE_DIM // 2
    inv_freq = ROPE_THETA ** (-jnp.arange(half, dtype=jnp.float32) / half)
    ang = pos.astype(jnp.float32)[:, None] * inv_freq[None, :]
    cos = jnp.cos(ang)[:, None, :]
    sin = jnp.sin(ang)[:, None, :]
    t1 = t[..., :half]
    t2 = t[..., half:ROPE_DIM]
    return jnp.concatenate([t1 * cos - t2 * sin, t2 * cos + t1 * sin, t[..., ROPE_DIM:]], axis=-1)


def masked_softmax(s, mask):
    s = jnp.where(mask, s, NEG_INF)
    return jax.nn.softmax(s, axis=-1) * mask


def nsa_mixer(q, k_cmp_in, v_cmp_in, k_slc, v_slc, k_win, v_win, gate_logits, w_cmp_k, w_cmp_v, cmp_pos):
    f32 = jnp.float32
    b, s = q.shape[0], q.shape[1]
    G, R, dk = NSA_KV_GROUPS, NSA_HEADS_PER_GROUP, NSA_HEAD_DIM
    pos = jnp.arange(s)
    scale = dk ** -0.5
    q = partial_rope(q.astype(f32), pos).reshape(b, s, G, R, dk)
    k_slc = partial_rope(k_slc.astype(f32), pos)
    k_win = partial_rope(k_win.astype(f32), pos)
    v_slc = v_slc.astype(f32)
    v_win = v_win.astype(f32)
    gates = jax.nn.sigmoid(gate_logits.astype(f32)).reshape(b, s, G, R, 3)

    n_cmp = (s - CMP_LEN) // CMP_STRIDE + 1
    cmp_start = jnp.arange(n_cmp) * CMP_STRIDE
    cmp_end = cmp_start + CMP_LEN - 1
    cmp_idx = cmp_start[:, None] + jnp.arange(CMP_LEN)[None, :]

    def compress(t, w):
        blk = t.astype(f32)[:, cmp_idx] + cmp_pos.astype(f32)[None, None, :, None, :]
        blk = blk.transpose(0, 1, 3, 2, 4).reshape(b, n_cmp, G, CMP_LEN * dk)
        return jnp.einsum('bngc,cd->bngd', blk, w.astype(f32))

    k_cmp = partial_rope(compress(k_cmp_in, w_cmp_k), cmp_start)
    v_cmp = compress(v_cmp_in, w_cmp_v)

    n_sel = s // SLC_LEN
    top_n = min(SLC_TOPN, n_sel)
    sel_start = jnp.arange(n_sel) * SLC_LEN
    blk_ids = jnp.arange(n_sel)
    overlap = ((cmp_start[:, None] < sel_start[None, :] + SLC_LEN) &
               (cmp_end[:, None] >= sel_start[None, :])).astype(f32)
    k_sel_blocks = k_slc.reshape(b, n_sel, SLC_LEN, G, dk).transpose(0, 3, 1, 2, 4)
    v_sel_blocks = v_slc.reshape(b, n_sel, SLC_LEN, G, dk).transpose(0, 3, 1, 2, 4)
    gather_blocks = jax.vmap(jax.vmap(lambda blocks, ids: blocks[ids]))
    slc_offsets = jnp.arange(SLC_LEN)

    k_win_pad = jnp.pad(k_win, ((0, 0), (WINDOW, 0), (0, 0), (0, 0)))
    v_win_pad = jnp.pad(v_win, ((0, 0), (WINDOW, 0), (0, 0), (0, 0)))
    win_offsets = jnp.arange(WINDOW + Q_BLOCK) - WINDOW
    q_offsets = jnp.arange(Q_BLOCK)

    def query_block(c):
        t0 = c * Q_BLOCK
        tpos = t0 + q_offsets
        qb = lax.dynamic_slice_in_dim(q, t0, Q_BLOCK, axis=1)
        gb = lax.dynamic_slice_in_dim(gates, t0, Q_BLOCK, axis=1)
        cmask = cmp_end[None, :] <= tpos[:, None]
        p_cmp = masked_softmax(jnp.einsum('bqgrd,bngd->bgrqn', qb, k_cmp) * scale, cmask)
        o_cmp = jnp.einsum('bgrqn,bngd->bqgrd', p_cmp, v_cmp)
        imp = jnp.einsum('bgrqn,nj->bgqj', p_cmp, overlap)
        force = (blk_ids[None, :] == (tpos // SLC_LEN)[:, None]) | (blk_ids[None, :] == 0)
        valid = sel_start[None, :] <= tpos[:, None]
        score = jnp.where(force, FORCE_SCORE, jnp.where(valid, imp, -1.0))
        _, sel_idx = lax.top_k(score, top_n)
        k_sel = gather_blocks(k_sel_blocks, sel_idx).reshape(b, G, Q_BLOCK, top_n * SLC_LEN, dk)
        v_sel = gather_blocks(v_sel_blocks, sel_idx).reshape(b, G, Q_BLOCK, top_n * SLC_LEN, dk)
        sel_pos = (sel_idx[..., None] * SLC_LEN + slc_offsets).reshape(b, G, Q_BLOCK, top_n * SLC_LEN)
        smask = (sel_pos <= tpos[None, None, :, None])[:, :, None]
        p_slc = masked_softmax(jnp.einsum('bqgrd,bgqmd->bgrqm', qb, k_sel) * scale, smask)
        o_slc = jnp.einsum('bgrqm,bgqmd->bqgrd', p_slc, v_sel)
        kw = lax.dynamic_slice_in_dim(k_win_pad, t0, WINDOW + Q_BLOCK, axis=1)
        vw = lax.dynamic_slice_in_dim(v_win_pad, t0, WINDOW + Q_BLOCK, axis=1)
        kpos = t0 + win_offsets
        rel = tpos[:, None] - kpos[None, :]
        wmask = (rel >= 0) & (rel < WINDOW) & (kpos[None, :] >= 0)
        p_win = masked_softmax(jnp.einsum('bqgrd,bmgd->bgrqm', qb, kw) * scale, wmask)
        o_win = jnp.einsum('bgrqm,bmgd->bqgrd', p_win, vw)
        return gb[..., 0:1] * o_cmp + gb[..., 1:2] * o_slc + gb[..., 2:3] * o_win

    out = lax.map(query_block, jnp.arange(s // Q_BLOCK))
    return out.transpose(1, 0, 2, 3, 4, 5).reshape(b, s, NSA_WIDTH)


def hgrn2_mixer(q, f_logit, i_in, lb):
    f32 = jnp.float32
    b, s = q.shape[0], q.shape[1]
    q = q.astype(f32).reshape(b, s, HG_HEADS, HG_KEY_DIM)
    f = lb + (1.0 - lb) * jax.nn.sigmoid(f_logit.astype(f32).reshape(b, s, HG_HEADS, HG_KEY_DIM))
    log_f = jnp.log(f)
    k = 1.0 - f
    v = i_in.astype(f32).reshape(b, s, HG_HEADS, HG_VAL_DIM)
    n_ch = s // HG_CHUNK

    def to_chunks(t):
        return t.reshape(b, n_ch, HG_CHUNK, HG_HEADS, t.shape[-1]).transpose(1, 0, 3, 2, 4)

    causal = jnp.tril(jnp.ones((HG_CHUNK, HG_CHUNK), dtype=bool))

    def chunk_step(state, inp):
        qc, kc, vc, gc = inp
        bcum = jnp.cumsum(gc, axis=2)
        o_inter = jnp.einsum('bhtk,bhkv->bhtv', qc * jnp.exp(bcum), state)
        rel = jnp.where(causal[:, :, None], bcum[:, :, :, None, :] - bcum[:, :, None, :, :], -jnp.inf)
        attn = jnp.einsum('bhtk,bhsk,bhtsk->bhts', qc, kc, jnp.exp(rel))
        o_intra = jnp.einsum('bhts,bhsv->bhtv', attn, vc)
        b_last = bcum[:, :, -1:, :]
        new_state = (jnp.exp(b_last[:, :, 0, :])[..., None] * state +
                     jnp.einsum('bhsk,bhsv->bhkv', kc * jnp.exp(b_last - bcum), vc))
        return new_state, o_inter + o_intra

    state0 = jnp.zeros((b, HG_HEADS, HG_KEY_DIM, HG_VAL_DIM), f32)
    _, o = lax.scan(chunk_step, state0, (to_chunks(q), to_chunks(k), to_chunks(v), to_chunks(log_f)))
    return o.transpose(1, 0, 3, 2, 4).reshape(b, s, HG_HEADS, HG_VAL_DIM)


def hier_moe(h, w_grp, b_grp, w_rtr, b_rtr, w_gate, w_up, w_down):
    f32 = jnp.float32
    b, s, d = h.shape
    t = h.reshape(b * s, d)
    grp_logits = (t @ w_grp).astype(f32) + b_grp.astype(f32)
    grp_sel = jnp.argmax(grp_logits, axis=-1)
    grp_prob = jnp.take_along_axis(jax.nn.softmax(grp_logits, axis=-1), grp_sel[:, None], axis=1)
    exp_logits = ((t @ w_rtr).astype(f32) + b_rtr.astype(f32)).reshape(-1, N_GROUPS, EXPERTS_PER_GROUP)
    in_grp = jnp.take_along_axis(exp_logits, grp_sel[:, None, None], axis=1)[:, 0]
    top_logits, top_idx = lax.top_k(in_grp, TOP_K_IN_GROUP)
    top_w = jax.nn.softmax(top_logits, axis=-1) * grp_prob
    expert_ids = grp_sel[:, None] * EXPERTS_PER_GROUP + top_idx
    combine = jnp.sum(jax.nn.one_hot(expert_ids, N_EXPERTS, dtype=f32) * top_w[..., None], axis=1)
    out = jnp.zeros((b * s, d), f32)
    for e in range(N_EXPERTS):
        hid = jax.nn.silu(t @ w_gate[e]) * (t @ w_up[e])
        out = out + combine[:, e:e + 1] * (hid @ w_down[e]).astype(f32)
    return out.reshape(b, s, d).astype(h.dtype)


def setup_inputs(seed: int = 0) -> dict:
    key = jax.random.key(seed)
    ks = jax.random.split(key, 24)
    f32 = jnp.float32
    nrm = lambda k, shape, scale: jax.random.normal(k, shape, f32) * scale
    return {
        'x': nrm(ks[0], (BATCH, SEQ, D_MODEL), 1.0),
        'attn_norm': 1.0 + nrm(ks[1], (DEPTH, D_MODEL), 0.01),
        'w_in': nrm(ks[2], (DEPTH, D_MODEL, IN_COLS), D_MODEL ** -0.5),
        'w_cmp_k': nrm(ks[3], (DEPTH, CMP_LEN * NSA_HEAD_DIM, NSA_HEAD_DIM), (CMP_LEN * NSA_HEAD_DIM) ** -0.5),
        'w_cmp_v': nrm(ks[4], (DEPTH, CMP_LEN * NSA_HEAD_DIM, NSA_HEAD_DIM), (CMP_LEN * NSA_HEAD_DIM) ** -0.5),
        'cmp_pos': nrm(ks[5], (DEPTH, CMP_LEN, NSA_HEAD_DIM), 0.02),
        'hg_lb_logits': nrm(ks[6], (DEPTH + 1, HG_HEADS * HG_KEY_DIM), 0.5),
        'hg_norm': 1.0 + nrm(ks[7], (DEPTH, HG_VAL_DIM), 0.01),
        'w_br_nsa': nrm(ks[8], (DEPTH, NSA_WIDTH, D_MODEL), NSA_WIDTH ** -0.5),
        'w_br_hg': nrm(ks[9], (DEPTH, HG_WIDTH, D_MODEL), HG_WIDTH ** -0.5),
        'w_out': nrm(ks[10], (DEPTH, D_MODEL, D_MODEL), D_MODEL ** -0.5),
        'ffn_norm': 1.0 + nrm(ks[11], (DEPTH, D_MODEL), 0.01),
        'w_grp': nrm(ks[12], (DEPTH, D_MODEL, N_GROUPS), D_MODEL ** -0.5),
        'b_grp': nrm(ks[13], (DEPTH, N_GROUPS), 0.01),
        'w_rtr': nrm(ks[14], (DEPTH, D_MODEL, N_EXPERTS), D_MODEL ** -0.5),
        'b_rtr': nrm(ks[15], (DEPTH, N_EXPERTS), 0.01),
        'w_gate': nrm(ks[16], (DEPTH, N_EXPERTS, D_MODEL, D_FF_EXPERT), D_MODEL ** -0.5),
        'w_up': nrm(ks[17], (DEPTH, N_EXPERTS, D_MODEL, D_FF_EXPERT), D_MODEL ** -0.5),
        'w_down': nrm(ks[18], (DEPTH, N_EXPERTS, D_FF_EXPERT, D_MODEL), D_FF_EXPERT ** -0.5),
        'final_norm': 1.0 + nrm(ks[19], (D_MODEL,), 0.01),
    }


def reference(x, attn_norm, w_in, w_cmp_k, w_cmp_v, cmp_pos, hg_lb_logits, hg_norm, w_br_nsa, w_br_hg,
              w_out, ffn_norm, w_grp, b_grp, w_rtr, b_rtr, w_gate, w_up, w_down, final_norm):
    b, s, _ = x.shape
    lower_bounds = jnp.cumsum(jax.nn.softmax(hg_lb_logits.astype(jnp.float32), axis=0), axis=0)
    split_points = np.cumsum(IN_SPLITS)[:-1].tolist()
    for layer in range(DEPTH):
        h = rmsnorm(x, attn_norm[layer])
        proj = h @ w_in[layer]
        nsa_q, nsa_kv, nsa_gate, hg_q, hg_f, hg_i, hg_g, merge_logits = jnp.split(proj, split_points, axis=-1)
        kv = nsa_kv.reshape(b, s, 6, NSA_KV_GROUPS, NSA_HEAD_DIM)
        o_nsa = nsa_mixer(nsa_q.reshape(b, s, NSA_HEADS, NSA_HEAD_DIM),
                          kv[:, :, 0], kv[:, :, 1], kv[:, :, 2], kv[:, :, 3], kv[:, :, 4], kv[:, :, 5],
                          nsa_gate.reshape(b, s, NSA_HEADS, 3),
                          w_cmp_k[layer], w_cmp_v[layer], cmp_pos[layer])
        lb = lower_bounds[layer].reshape(HG_HEADS, HG_KEY_DIM)
        o_hg = hgrn2_mixer(hg_q, hg_f, hg_i, lb)
        o_hg = rmsnorm(o_hg, hg_norm[layer]) * jax.nn.silu(
            hg_g.astype(jnp.float32).reshape(b, s, HG_HEADS, HG_VAL_DIM))
        o_hg = o_hg.reshape(b, s, HG_WIDTH).astype(x.dtype)
        gate_nsa, gate_hg = jnp.split(jax.nn.sigmoid(merge_logits), 2, axis=-1)
        mixed = (gate_nsa * (o_nsa.astype(x.dtype) @ w_br_nsa[layer]) +
                 gate_hg * (o_hg @ w_br_hg[layer]))
        x = x + mixed @ w_out[layer]
        x = x + hier_moe(rmsnorm(x, ffn_norm[layer]), w_grp[layer], b_grp[layer], w_rtr[layer], b_rtr[layer],
                         w_gate[layer], w_up[layer], w_down[layer])
    return rmsnorm(x, final_norm)
```

```python
import numpy as np
import ml_dtypes
import concourse.bass as bass
import concourse.mybir as mybir
from concourse.bass_utils import run_bass_kernel_spmd
from contextlib import ExitStack

F32 = mybir.dt.float32
BF16 = mybir.dt.bfloat16
AF = mybir.ActivationFunctionType
ALU = mybir.AluOpType
AX = mybir.AxisListType


class Res:
    __slots__ = ("name", "w", "rs", "dsem", "dcnt", "excl")

    def __init__(self, name, excl=False):
        self.name = name
        self.excl = excl
        self.w = None
        self.rs = {}
        self.dsem = None
        self.dcnt = 0


class _Proxy:
    def __init__(self):
        self.calls = []

    def __getattr__(self, name):
        def rec(*a, **kw):
            self.calls.append((name, a, kw))
        return rec


def _bind(f):
    p = _Proxy()
    f(p)
    assert len(p.calls) == 1, "one engine instruction per callable"
    name, a, kw = p.calls[0]
    return lambda eng: getattr(eng, name)(*a, **kw)


class KH:
    ENG = ("pe", "dve", "act", "pool", "sp")

    def __init__(self, nc, es):
        self.nc = nc
        self.es = es
        self.sem = {}
        self.cnt = {}
        for e in self.ENG:
            self.sem[e] = es.enter_context(nc.semaphore("s_" + e))
            self.cnt[e] = 0
        self.rec = {e: [] for e in self.ENG}
        self.seen = {e: {} for e in self.ENG}
        self.nsem = len(self.ENG)
        self.semobj = dict(self.sem)

    def res(self, name, excl=False):
        return Res(name, excl)

    def _dma_sem(self, r):
        if r.dsem is None:
            r.dsem = "d_" + r.name + "_%d" % self.nsem
            self.semobj[r.dsem] = self.es.enter_context(self.nc.semaphore(r.dsem))
            self.nsem += 1
        return r.dsem

    def _deps(self, e, reads, writes):
        deps = {}
        for r in reads:
            if r.w is not None:
                k, v = r.w
                deps[k] = max(deps.get(k, 0), v)
        for w in writes:
            if w.w is not None:
                k, v = w.w
                deps[k] = max(deps.get(k, 0), v)
            for k, v in w.rs.items():
                deps[k] = max(deps.get(k, 0), v)
        seen = self.seen[e]
        for k, v in deps.items():
            if seen.get(k, 0) >= v:
                continue
            seen[k] = v
            self.rec[e].append(("w", k, v))

    def op(self, e, fns, reads=(), writes=()):
        if callable(fns):
            fns = [fns]
        self.opn = getattr(self, "opn", 0) + 1
        if self.opn > getattr(self, "oplim", 10 ** 9):
            return
        ex = [r for r in reads if r.excl]
        if ex:
            reads = [r for r in reads if not r.excl]
            writes = list(writes) + [r for r in ex if r not in writes]
        self._deps(e, reads, writes)
        self.cnt[e] += 1
        v = self.cnt[e]
        fns = [_bind(f) for f in fns]
        for f in fns[:-1]:
            self.rec[e].append(("i", f, None, 0))
        self.rec[e].append(("i", fns[-1], e, 1))
        self.seen[e][e] = max(self.seen[e].get(e, 0), 0)
        for r in reads:
            r.rs[e] = v
        for w in writes:
            w.w = (e, v)
            w.rs = {}

    def dma(self, q, out, in_, reads=(), writes=(), key=None, **kw):
        self._deps(q, reads, writes)
        kr = key or (writes[0] if writes else reads[0])
        sk = self._dma_sem(kr)
        kr.dcnt += 16
        v = kr.dcnt
        self.rec[q].append(("i", lambda eng: eng.dma_start(out=out, in_=in_, **kw), sk, 16))
        for r in reads:
            r.rs[sk] = v
        for w in writes:
            w.w = (sk, v)
            w.rs = {}

    def wait_res(self, e, rs):
        self._deps(e, rs, ())

    def simulate(self):
        if not hasattr(self, "simval"):
            self.simval = {}
        val = self.simval
        ptr = {e: 0 for e in self.ENG}
        prog = True
        while prog:
            prog = False
            for e in self.ENG:
                items = self.rec[e]
                while ptr[e] < len(items):
                    it = items[ptr[e]]
                    if it[0] == "w":
                        if val.get(it[1], 0) >= it[2]:
                            ptr[e] += 1
                            prog = True
                        else:
                            break
                    else:
                        if it[2] is not None:
                            val[it[2]] = val.get(it[2], 0) + it[3]
                        ptr[e] += 1
                        prog = True
        for e in self.ENG:
            if ptr[e] < len(self.rec[e]):
                it = self.rec[e][ptr[e]]
                raise RuntimeError("DEADLOCK: engine %s stuck at item %d/%d waiting %s >= %s (have %s)" % (
                    e, ptr[e], len(self.rec[e]), it[1], it[2], val.get(it[1], 0)))

    def flush(self, name=None):
        nc = self.nc
        rec = self.rec
        semobj = self.semobj
        self.simulate()
        import os
        if os.environ.get("KH_DEBUG"):
            print("KH flush: ops so far", getattr(self, "opn", 0), {e: len(v) for e, v in self.rec.items()}, "nsem", self.nsem, flush=True)

        def play(eng, items):
            for it in items:
                if it[0] == "w":
                    eng.wait_ge(semobj[it[1]], it[2])
                else:
                    ins = it[1](eng)
                    if it[2] is not None:
                        ins.then_inc(semobj[it[2]], it[3])

        with nc.Block() as block:
            if rec["sp"]:
                @block.sync
                def _(eng):
                    play(eng, rec["sp"])
            if rec["pe"]:
                @block.tensor
                def _(eng):
                    play(eng, rec["pe"])
            if rec["dve"]:
                @block.vector
                def _(eng):
                    play(eng, rec["dve"])
            if rec["act"]:
                @block.scalar
                def _(eng):
                    play(eng, rec["act"])
            if rec["pool"]:
                @block.gpsimd
                def _(eng):
                    play(eng, rec["pool"])
        self.rec = {e: [] for e in self.ENG}

NEG = -30000.0
NT = 16
NTOK = NT * 128
SEQ = 8192
D = 1024
NE = 32
DFF = 512
EPS = 1e-6


class Arena:
    def __init__(self, big, nbytes):
        self.big = big
        self.n = nbytes
        self.off = 0

    def mark(self):
        return self.off

    def release(self, m):
        import os
        if os.environ.get("KH_DEBUG"):
            print("arena release: peak", getattr(self, "peak", 0), "->", m, "of", self.n, flush=True)
        self.peak = m
        self.off = m

    def al(self, shape, dt):
        esz = 4 if dt == F32 else 2
        per = int(np.prod(shape[1:])) * esz
        self.off = (self.off + 63) // 64 * 64
        o = self.off
        assert o + per <= self.n, ("arena overflow", o, per, self.n)
        self.off = o + per
        self.peak = max(getattr(self, "peak", 0), self.off)
        v = self.big[0:shape[0], o // 2:(o + per) // 2]
        if dt == F32:
            v = v.bitcast(F32)
        if len(shape) == 3:
            v = v.rearrange("p (a b) -> p a b", a=shape[1])
        elif len(shape) == 4:
            v = v.rearrange("p (a b c) -> p a b c", a=shape[1], b=shape[2])
        return v


class Ctx:
    pass


def rms_rstd(k, c, src, src_res, scr, scr_res, n_feat, tag):
    i = c.rs_i % 8
    c.rs_i += 1
    ssq, std, rstd = c.ssq[:, i:i + 1], c.std[:, i:i + 1], c.rstd[:, i:i + 1]
    R = c.rs_res[i]
    k.op("act", lambda e: e.activation(out=scr, in_=src, func=AF.Square, accum_out=ssq),
         reads=[src_res], writes=[scr_res, R])
    k.op("act", lambda e: e.activation(out=std, in_=ssq, func=AF.Sqrt, scale=1.0 / n_feat, bias=c.epsb[:, 0:1]),
         reads=[R, c.R_const], writes=[R])
    k.op("dve", lambda e: e.reciprocal(out=rstd, in_=std), reads=[R], writes=[R])
    return rstd, R


def phase_moe(k, c, ar, X1, R_X1, hfT, R_hfT):
    nc = c.nc
    Dr = c.D
    m0 = ar.mark()
    hf32 = [ar.al([128, D], F32) for _ in range(2)]
    R_hf32 = [k.res("hf32_%d" % i) for i in range(2)]
    scr = ar.al([128, D], F32)
    R_scr = k.res("moe_scr")
    hT32 = [ar.al([128, 8, 128], F32) for _ in range(2)]
    R_hT32 = [k.res("hT32_%d" % i) for i in range(2)]
    wr32 = ar.al([128, 8, 36], F32)
    R_wr = k.res("wr32")
    brt = ar.al([128, 36], F32)
    gft = ar.al([128, D], F32)
    R_gft = k.res("gft")
    comb = ar.al([128, NT, NE], F32)
    R_comb = k.res("comb")
    sm = ar.al([128, 128], F32)
    R_sm = k.res("moe_sm")
    k.dma("sp", wr32, Dr["w_r"].rearrange("(kt p) n -> p kt n", p=128), writes=[R_wr])
    k.dma("sp", brt, Dr["b_r"].partition_broadcast(128), writes=[R_wr])
    k.dma("sp", gft, Dr["g_ffn"].partition_broadcast(128), writes=[R_gft])
    pT = [c.pwide(0), c.pwide(2)]
    R_pT = [[c.R_ps[0], c.R_ps[1]], [c.R_ps[2], c.R_ps[3]]]
    pL = c.psum[4]
    R_pL = c.R_ps[4]
    for t in range(NT):
        b = t % 2
        xs = X1[:, t, :]
        rstd, R_r = rms_rstd(k, c, xs, R_X1[t], scr, R_scr, D, "moe")
        k.op("dve", lambda e, b=b, xs=xs, rstd=rstd: e.scalar_tensor_tensor(
            out=hf32[b], in0=xs, scalar=rstd, in1=gft, op0=ALU.mult, op1=ALU.mult),
            reads=[R_X1[t], R_r, R_gft], writes=[R_hf32[b]])
        p2 = pT[b]
        k.op("pe", [(lambda e, i=i, b=b, p2=p2: e.transpose(out=p2[:, i * 128:(i + 1) * 128],
                                                            in_=hf32[b][:, i * 128:(i + 1) * 128], identity=c.identf))
                    for i in range(8)], reads=[R_hf32[b], c.R_const], writes=R_pT[b])
        k.op("act", lambda e, b=b, p2=p2: e.activation(out=hT32[b].rearrange("p a b -> p (a b)"), in_=p2, func=AF.Copy),
             reads=R_pT[b], writes=[R_hT32[b]])
        k.op("dve", lambda e, b=b, p2=p2, t=t: e.tensor_copy(
            out=hfT[:, :, t * 128:(t + 1) * 128], in_=p2.rearrange("p (a b) -> p a b", a=8)),
            reads=R_pT[b], writes=[R_hfT[t]])
        lg = pL[:, 0:36]
        k.op("pe", [(lambda e, i=i, b=b: e.matmul(lg, lhsT=hT32[b][:, i, :], rhs=wr32[:, i, :], start=(i == 0), stop=(i == 7)))
                    for i in range(8)], reads=[R_hT32[b], R_wr], writes=[R_pL])
        lgs = sm[:, 0:36]
        gmax, gsum, gp, pen = sm[:, 36:37], sm[:, 37:38], sm[:, 38:39], sm[:, 40:44]
        gex, goh = sm[:, 44:48], sm[:, 48:52]
        elm = sm[:, 52:84]
        m8 = sm[:, 84:92]
        dd, ee, w1, w2 = sm[:, 92:93], sm[:, 93:94], sm[:, 94:95], sm[:, 95:96]
        oh = sm[:, 96:128]
        ct = comb[:, t, :]
        RW = dict(reads=[R_sm], writes=[R_sm])
        k.op("dve", lambda e: e.tensor_tensor(out=lgs, in0=lg, in1=brt, op=ALU.add), reads=[R_pL, R_wr, R_sm], writes=[R_sm])
        k.op("dve", lambda e: e.reduce_max(out=gmax, in_=lgs[:, 0:4], axis=AX.X), **RW)
        k.op("dve", lambda e: e.tensor_scalar(out=gex, in0=lgs[:, 0:4], scalar1=gmax, scalar2=None, op0=ALU.subtract), **RW)
        k.op("act", lambda e: e.activation(out=gex, in_=gex, func=AF.Exp, accum_out=gsum), **RW)
        k.op("dve", lambda e: e.reciprocal(out=gp, in_=gsum), **RW)
        k.op("dve", lambda e: e.tensor_scalar(out=pen, in0=lgs[:, 0:4], scalar1=gmax, scalar2=-1e30, op0=ALU.is_lt, op1=ALU.mult), **RW)
        k.op("dve", lambda e: e.tensor_tensor(out=elm.rearrange("p (g x) -> p g x", g=4),
                                              in0=lgs[:, 4:36].rearrange("p (g x) -> p g x", g=4),
                                              in1=pen.unsqueeze(2).broadcast_to([128, 4, 8]), op=ALU.add), **RW)
        k.op("dve", lambda e: e.max(out=m8, in_=elm), **RW)
        k.op("dve", lambda e: e.tensor_tensor(out=dd, in0=m8[:, 1:2], in1=m8[:, 0:1], op=ALU.subtract), **RW)
        k.op("act", lambda e: e.activation(out=ee, in_=dd, func=AF.Exp), **RW)
        k.op("dve", lambda e: e.tensor_scalar(out=ee, in0=ee, scalar1=1.0, scalar2=None, op0=ALU.add), **RW)
        k.op("dve", lambda e: e.reciprocal(out=w1, in_=ee), **RW)
        k.op("dve", lambda e: e.tensor_scalar(out=w2, in0=w1, scalar1=-1.0, scalar2=1.0, op0=ALU.mult, op1=ALU.add), **RW)
        k.op("dve", lambda e: e.tensor_tensor(out=w1, in0=w1, in1=gp, op=ALU.mult), **RW)
        k.op("dve", lambda e: e.tensor_tensor(out=w2, in0=w2, in1=gp, op=ALU.mult), **RW)
        k.op("dve", lambda e: e.tensor_scalar(out=oh, in0=elm, scalar1=m8[:, 0:1], scalar2=w1, op0=ALU.is_equal, op1=ALU.mult), **RW)
        k.op("dve", lambda e, ct=ct: e.tensor_scalar(out=ct, in0=elm, scalar1=m8[:, 1:2], scalar2=w2, op0=ALU.is_equal, op1=ALU.mult),
             reads=[R_sm], writes=[R_comb])
        k.op("dve", lambda e, ct=ct: e.tensor_tensor(out=ct, in0=ct, in1=oh, op=ALU.add), reads=[R_sm, R_comb], writes=[R_comb])

    if c.n_exp == 0:
        ar.release(m0)
        return
    NWB = 2
    wg = [ar.al([128, 8, DFF], BF16) for _ in range(NWB)]
    wu = [ar.al([128, 8, DFF], BF16) for _ in range(NWB)]
    wd = [ar.al([128, 4, D], BF16) for _ in range(NWB)]
    R_wg = [k.res("wg%d" % i) for i in range(NWB)]
    R_wu = [k.res("wu%d" % i) for i in range(NWB)]
    R_wd = [k.res("wd%d" % i) for i in range(NWB)]
    hid = [ar.al([128, 4, 512], BF16) for _ in range(2)]
    R_hid = [k.res("hid%d" % i) for i in range(2)]
    sg = [ar.al([128, 512], F32) for _ in range(2)]
    R_sg = [k.res("sg%d" % i) for i in range(2)]
    n_exp = c.n_exp

    def load_w(e):
        b = e % NWB
        k.dma("pool", wg[b], Dr["w_gate"][e].rearrange("(kt p) n -> p kt n", p=128), writes=[R_wg[b]])
        k.dma("pool", wu[b], Dr["w_up"][e].rearrange("(kt p) n -> p kt n", p=128), writes=[R_wu[b]])
        k.dma("pool", wd[b], Dr["w_down"][e].rearrange("(kt p) n -> p kt n", p=128), writes=[R_wd[b]])

    units = [(e, g) for e in range(n_exp) for g in range(NT // 4)]
    pgu = [(c.psum[0], c.psum[1]), (c.psum[2], c.psum[3])]
    R_pgu = [(c.R_ps[0], c.R_ps[1]), (c.R_ps[2], c.R_ps[3])]
    pdn = [c.psum[4], c.psum[5], c.psum[6], c.psum[7]]
    R_pdn = [c.R_ps[4], c.R_ps[5], c.R_ps[6], c.R_ps[7]]
    st = dict(gu=0, dn=0)

    def gate_up(u):
        e, g = units[u]
        b = e % NWB
        hb = u % 2
        tok = slice(g * 512, (g + 1) * 512)
        for ff in range(4):
            pb = st["gu"] % 2
            st["gu"] += 1
            pg, pu = pgu[pb]
            k.op("pe", [(lambda en, i=i, pg=pg, b=b, ff=ff: en.matmul(pg, lhsT=wg[b][:, i, ff * 128:(ff + 1) * 128], rhs=hfT[:, i, tok],
                                                                      start=(i == 0), stop=(i == 7))) for i in range(8)],
                 reads=[R_wg[b]] + R_hfT[4 * g:4 * g + 4], writes=[R_pgu[pb][0]])
            k.op("pe", [(lambda en, i=i, pu=pu, b=b, ff=ff: en.matmul(pu, lhsT=wu[b][:, i, ff * 128:(ff + 1) * 128], rhs=hfT[:, i, tok],
                                                                      start=(i == 0), stop=(i == 7))) for i in range(8)],
                 reads=[R_wu[b]] + R_hfT[4 * g:4 * g + 4], writes=[R_pgu[pb][1]])
            k.op("act", lambda en, pg=pg, pb=pb: en.activation(out=sg[pb], in_=pg, func=AF.Silu),
                 reads=[R_pgu[pb][0]], writes=[R_sg[pb]])
            k.op("dve", lambda en, pu=pu, pb=pb, hb=hb, ff=ff: en.tensor_tensor(out=hid[hb][:, ff, :], in0=pu, in1=sg[pb], op=ALU.mult),
                 reads=[R_pgu[pb][1], R_sg[pb]], writes=[R_hid[hb]])

    def down(u):
        e, g = units[u]
        b = e % NWB
        hb = u % 2
        for tt in range(4):
            t = 4 * g + tt
            for hf in range(2):
                pb = st["dn"] % 4
                st["dn"] += 1
                po = pdn[pb]
                k.op("pe", [(lambda en, i=i, po=po, b=b, hb=hb, tt=tt, hf=hf: en.matmul(
                    po, lhsT=hid[hb][:, i, tt * 128:(tt + 1) * 128], rhs=wd[b][:, i, hf * 512:(hf + 1) * 512],
                    start=(i == 0), stop=(i == 3))) for i in range(4)],
                    reads=[R_hid[hb], R_wd[b]], writes=[R_pdn[pb]])
                xs = X1[:, t, hf * 512:(hf + 1) * 512]
                k.op("dve", lambda en, po=po, xs=xs, t=t, e=e: en.scalar_tensor_tensor(
                    out=xs, in0=po, scalar=comb[:, t, e:e + 1], in1=xs, op0=ALU.mult, op1=ALU.add),
                    reads=[R_pdn[pb], R_comb, R_X1[t]], writes=[R_X1[t]])

    load_w(0)
    for u in range(len(units)):
        e, g = units[u]
        gate_up(u)
        if u >= 1:
            down(u - 1)
        if g == 0 and e + 1 < n_exp:
            load_w(e + 1)
    down(len(units) - 1)
    ar.release(m0)


def phase_final(k, c, ar, X1, R_X1):
    Dr = c.D
    m0 = ar.mark()
    gft = ar.al([128, D], F32)
    R_g = k.res("gfin")
    scr = ar.al([128, D], F32)
    R_scr = k.res("fin_scr")
    ob = [ar.al([128, D], F32) for _ in range(2)]
    R_ob = [k.res("ob%d" % i) for i in range(2)]
    k.dma("sp", gft, Dr["g_fin"].partition_broadcast(128), writes=[R_g])
    for t in range(NT):
        b = t % 2
        xs = X1[:, t, :]
        rstd, R_r = rms_rstd(k, c, xs, R_X1[t], scr, R_scr, D, "fin")
        k.op("dve", lambda e, b=b, xs=xs, rstd=rstd: e.scalar_tensor_tensor(
            out=ob[b], in0=xs, scalar=rstd, in1=gft, op0=ALU.mult, op1=ALU.mult),
            reads=[R_X1[t], R_r, R_g], writes=[R_ob[b]])
        k.dma("sp", Dr["y"][t * 128:(t + 1) * 128, :], ob[b], reads=[R_ob[b]])
    for b in range(2):
        for sk, v in list(R_ob[b].rs.items()):
            if sk.startswith("d_"):
                k.rec["sp"].append(("w", sk, v))
    ar.release(m0)


Q0, KV0, GT0, HQ0, HF0, HI0, HG0, MG0 = 0, 512, 1280, 1304, 1816, 2328, 2840, 3352


def _partner(d):
    return d + 8 if d < 8 else (d - 8 if d < 16 else d)


def _rope_tables(pos):
    pos = np.asarray(pos, dtype=np.float32)
    inv = (np.float32(500000.0) ** (-np.arange(8, dtype=np.float32) / np.float32(8))).astype(np.float32)
    ang = (pos[None, :] * inv[:, None]).astype(np.float32)
    cs, sn = np.cos(ang).astype(np.float32), np.sin(ang).astype(np.float32)
    C = np.ones((64, len(pos)), np.float32)
    S = np.zeros((64, len(pos)), np.float32)
    C[0:8], C[8:16] = cs, cs
    S[0:8], S[8:16] = -sn, sn
    return C, S


def attn_input_specs():
    return [
        ("g_attn", (D,), F32), ("g_hg4", (512,), F32),
        ("w1f", (D, 1280), F32), ("w1t", (D, 768), F32),
        ("w2f", (D, 2048), F32), ("w2t", (D, 1536), F32),
        ("wck", (2048, 64), F32), ("wckp", (2048, 64), F32), ("wcv", (2048, 64), F32),
        ("posT", (128, 32), F32), ("lbl", (2, 512), F32),
        ("w_mg", (D, 2048), F32), ("w_brn", (512, D), F32), ("w_brh", (512, D), F32), ("w_out", (D, D), F32),
        ("CK", (128, SEQ), F32), ("SK", (128, SEQ), F32), ("CKc", (128, 512), F32), ("SKc", (128, 512), F32),
        ("CQ", (128, NTOK), F32), ("SQ", (128, NTOK), F32),
        ("ovl", (128, 4, 128), F32),
        ("CB", (128, NT, 128), F32), ("CM", (128, 4, 128), F32), ("WMT", (128, 8, 128), F32),
        ("VAL", (128, NT, 128), F32), ("ADDC", (128, NT, 128), F32),
        ("tri", (128, 128), F32), ("I4", (128, 512), F32), ("onehot", (128, 4), F32),
    ]


_TAB_CACHE = {}


def _const_tables(cp):
    if cp in _TAB_CACHE:
        return _TAB_CACHE[cp]
    m = {}
    C, S = _rope_tables(np.arange(SEQ))
    m["CK"], m["SK"] = np.concatenate([C, C], 0), np.concatenate([S, S], 0)
    C, S = _rope_tables(np.maximum(16 * (np.arange(512) - 1), 0))
    m["CKc"], m["SKc"] = np.concatenate([C, C], 0), np.concatenate([S, S], 0)
    tpos = (128 * (4 * np.arange(NT)[:, None] + cp) + np.arange(128)[None, :])
    C, S = _rope_tables(tpos.reshape(-1))
    m["CQ"] = np.concatenate([C, C], 0) * np.float32(0.125)
    m["SQ"] = np.concatenate([S, S], 0) * np.float32(0.125)
    n = np.arange(512) - 1
    cs, ce = 16 * n, 16 * n + 31
    ss = 64 * np.arange(128)
    ov = ((cs[:, None] < ss[None, :] + 64) & (ce[:, None] >= ss[None, :]) & (n[:, None] >= 0)).astype(np.float32)
    m["ovl"] = np.ascontiguousarray(ov.reshape(4, 128, 128).transpose(1, 0, 2))
    mt = (np.arange(NT) // 4)
    mm = mt[:, None] * 128 + np.arange(128)[None, :]
    nn = mm - 1
    okc = (nn[:, None, :] >= 0) & (16 * nn[:, None, :] + 31 <= tpos[:, :, None])
    m["CB"] = np.ascontiguousarray(np.where(okc, 0.0, NEG).astype(np.float32).transpose(1, 0, 2))
    blk = np.arange(128)
    jq = tpos // 64
    force = (blk[None, None, :] == jq[:, :, None]) | (blk[None, None, :] == 0)
    valid = (64 * blk[None, None, :] <= tpos[:, :, None])
    m["VAL"] = np.ascontiguousarray((valid & ~force).astype(np.float32).transpose(1, 0, 2))
    m["ADDC"] = np.ascontiguousarray(np.where(force, 1e4, np.where(valid, 0.0, -1.0)).astype(np.float32).transpose(1, 0, 2))
    t = np.arange(128)[:, None]
    p = np.arange(128)[None, :]
    caus = np.where(p <= t, 0.0, NEG).astype(np.float32)
    anti = np.where(p > t, 0.0, NEG).astype(np.float32)
    cm = np.zeros((128, 4, 128), np.float32)
    for r in range(4):
        cm[:, r, :] = 0.0 if r < cp else (caus if r == cp else NEG)
    m["CM"] = cm
    wm = np.zeros((128, 8, 128), np.float32)
    for r in range(8):
        dk = cp + 4 - r
        wm[:, r, :] = NEG if (dk < 0 or dk > 4) else (caus if dk == 0 else (anti if dk == 4 else 0.0))
    m["WMT"] = wm
    m["tri"] = (np.arange(128)[:, None] <= np.arange(128)[None, :]).astype(np.float32)
    m["I4"] = np.tile(np.eye(128, dtype=np.float32), (1, 4))
    oh = np.zeros((128, 4), np.float32)
    oh[:, cp] = 1.0
    m["onehot"] = oh
    _TAB_CACHE[cp] = m
    return m


def attn_host_inputs(inp, b, cp):
    m = dict(_const_tables(cp))
    w = inp["w_in"][0]
    pp = np.array([g * 64 + _partner(d) for g in range(2) for d in range(64)])
    kv = lambda s: KV0 + s * 128 + np.arange(128)
    hfc = HF0 + np.arange(512)
    m["w1f"] = np.ascontiguousarray(np.concatenate(
        [w[:, kv(0)], w[:, kv(1)], w[:, kv(2)], w[:, kv(2)[pp]], w[:, kv(4)], w[:, kv(4)[pp]], w[:, hfc]], axis=1))
    m["w1t"] = np.ascontiguousarray(np.concatenate([w[:, kv(3)], w[:, kv(5)], w[:, HI0:HI0 + 512]], axis=1))
    qcols, qpcols = [], []
    for a in range(4):
        for h in (a, 4 + a):
            qcols += [Q0 + h * 64 + d for d in range(64)]
            qpcols += [Q0 + h * 64 + _partner(d) for d in range(64)]
    m["w2f"] = np.ascontiguousarray(np.concatenate(
        [w[:, qcols], w[:, qpcols], w[:, HQ0:HQ0 + 512], w[:, hfc]], axis=1))
    gpad = np.concatenate([w[:, GT0:GT0 + 24], w[:, GT0:GT0 + 24][:, :0].repeat(1, 1)], axis=1)
    w2t = np.zeros((D, 1536), np.float32)
    w2t[:, 0:512] = w[:, HI0:HI0 + 512]
    w2t[:, 512:1024] = w[:, HG0:HG0 + 512]
    w2t[:, 1024:1048] = w[:, GT0:GT0 + 24]
    m["w2t"] = w2t
    pc = np.array([_partner(d) for d in range(64)])
    m["wck"] = np.ascontiguousarray(inp["w_cmp_k"][0])
    m["wckp"] = np.ascontiguousarray(inp["w_cmp_k"][0][:, pc])
    m["wcv"] = np.ascontiguousarray(inp["w_cmp_v"][0])
    pT = np.ascontiguousarray(inp["cmp_pos"][0].T)
    m["posT"] = np.concatenate([pT, pT], 0)
    m["lbl"] = np.ascontiguousarray(inp["hg_lb_logits"])
    m["g_attn"] = np.ascontiguousarray(inp["attn_norm"][0])
    m["g_hg4"] = np.ascontiguousarray(np.tile(inp["hg_norm"][0], 4))
    m["w_mg"] = np.ascontiguousarray(w[:, MG0:MG0 + 2048])
    m["w_brn"] = np.ascontiguousarray(inp["w_br_nsa"][0])
    m["w_brh"] = np.ascontiguousarray(inp["w_br_hg"][0])
    m["w_out"] = np.ascontiguousarray(inp["w_out"][0])
    return m


def norm_transpose_group(k, c, W, src_dram, row0, hT, R_hT):
    for tt in range(4):
        b = tt % 2
        k.dma("sp", W.xt[b], src_dram[row0 + tt * 128: row0 + (tt + 1) * 128, :], writes=[W.R_xt[b]])
        rstd, R_r = rms_rstd(k, c, W.xt[b], W.R_xt[b], W.scr, W.R_scr, D, "an")
        k.op("dve", lambda e, b=b, rstd=rstd: e.scalar_tensor_tensor(
            out=W.hb[b], in0=W.xt[b], scalar=rstd, in1=W.gA, op0=ALU.mult, op1=ALU.mult),
            reads=[W.R_xt[b], R_r, W.R_gA], writes=[W.R_hb[b]])
        pb = c.psum[b].bitcast(BF16)
        k.op("pe", [(lambda e, i=i, b=b, pb=pb: e.transpose(out=pb[:, i * 128:(i + 1) * 128],
                                                            in_=W.hb[b][:, i * 128:(i + 1) * 128], identity=c.identb))
                    for i in range(8)], reads=[W.R_hb[b], c.R_const], writes=[c.R_ps[b]])
        k.op("act", lambda e, pb=pb, tt=tt: e.activation(out=hT[:, :, tt * 128:(tt + 1) * 128],
                                                       in_=pb.rearrange("p (a b) -> p a b", a=8), func=AF.Copy),
             reads=[c.R_ps[b]], writes=[R_hT])


def f_front(k, c, W, fl_ps, R_fl, hd):
    u, a, bq, lk, L, RF = W.sets[hd % 2]
    k.op("act", lambda e: e.activation(out=u, in_=fl_ps, func=AF.Exp, scale=-1.0), reads=[R_fl], writes=[RF])
    k.op("act", lambda e: e.activation(out=a, in_=u, func=AF.Ln, scale=c.lbv[:, hd:hd + 1], bias=c.one_col[:, 0:1]),
         reads=[RF, c.R_const], writes=[RF])
    k.op("act", lambda e: e.activation(out=bq, in_=u, func=AF.Ln, bias=c.one_col[:, 0:1]), reads=[RF, c.R_const], writes=[RF])
    k.op("dve", lambda e: e.scalar_tensor_tensor(out=lk, in0=fl_ps, scalar=-1.0, in1=bq, op0=ALU.mult, op1=ALU.subtract),
         reads=[R_fl, RF], writes=[RF])
    for tt in range(4):
        sl = slice(tt * 128, (tt + 1) * 128)
        k.op("dve", lambda e, sl=sl: e.tensor_tensor_scan(out=L[:, sl], data0=a[:, sl], data1=bq[:, sl], initial=0.0,
                                                          op0=ALU.add, op1=ALU.subtract), reads=[RF], writes=[RF])
    k.op("pool", lambda e: e.tensor_tensor(out=lk, in0=lk, in1=L, op=ALU.subtract), reads=[RF], writes=[RF])


def f_back(k, c, W, hd):
    u, a, bq, lk, L, RF = W.sets[hd % 2]
    Lr = L.rearrange("p (t x) -> p t x", t=4)
    rcol, ecol = Lr[:, :, 63], Lr[:, :, 127]
    k.op("dve", lambda e: e.tensor_scalar(out=W.rb[:, hd, :], in0=rcol, scalar1=c.l1mlb[:, hd:hd + 1], scalar2=None, op0=ALU.add),
         reads=[RF, c.R_const], writes=[W.R_cols])
    k.op("dve", lambda e: e.tensor_scalar(out=W.negr[:, hd, :], in0=rcol, scalar1=-1.0, scalar2=None, op0=ALU.mult),
         reads=[RF], writes=[W.R_cols])
    k.op("dve", lambda e: e.tensor_tensor(out=W.dl[:, hd, :], in0=ecol, in1=rcol, op=ALU.subtract), reads=[RF], writes=[W.R_cols])
    k.op("act", lambda e: e.activation(out=W.c1[:, hd, :], in_=ecol, func=AF.Exp), reads=[RF], writes=[W.R_cols])
    k.op("act", lambda e: e.activation(out=W.c2[:, hd, :], in_=W.dl[:, hd, :], func=AF.Exp), reads=[W.R_cols], writes=[W.R_cols])
    k.op("act", lambda e: e.activation(out=W.er[:, hd, :], in_=rcol, func=AF.Exp), reads=[RF], writes=[W.R_cols])
    for tt in range(4):
        sl = slice(tt * 128, (tt + 1) * 128)
        k.op("act", lambda e, sl=sl, tt=tt: e.activation(out=W.kT[:, hd, sl], in_=lk[:, sl], func=AF.Exp, bias=W.rb[:, hd, tt:tt + 1]),
             reads=[RF, W.R_cols], writes=[W.R_kT])


def setup_lb(k, c, ar):
    Dr = c.D
    c.lbv = ar.al([128, 4], F32)
    c.l1mlb = ar.al([128, 4], F32)
    c.one_col = ar.al([128, 1], F32)
    c.ones128 = ar.al([128, 128], F32)
    l0 = ar.al([128, 4], F32)
    l1 = ar.al([128, 4], F32)
    R = c.R_const
    k.dma("sp", l0, Dr["lbl"][0].rearrange("(h p) -> p h", p=128), writes=[R], allow_slow_non_contiguous=True)
    k.dma("sp", l1, Dr["lbl"][1].rearrange("(h p) -> p h", p=128), writes=[R], allow_slow_non_contiguous=True)
    k.op("dve", lambda e: e.memset(c.one_col, 1.0), writes=[R])
    k.op("dve", lambda e: e.memset(c.ones128, 1.0), writes=[R])
    k.op("dve", lambda e: e.tensor_tensor(out=l1, in0=l1, in1=l0, op=ALU.subtract), reads=[R], writes=[R])
    k.op("act", lambda e: e.activation(out=l0, in_=l1, func=AF.Exp), reads=[R], writes=[R])
    k.op("dve", lambda e: e.tensor_scalar(out=l0, in0=l0, scalar1=1.0, scalar2=None, op0=ALU.add), reads=[R], writes=[R])
    k.op("dve", lambda e: e.reciprocal(out=c.lbv, in_=l0), reads=[R], writes=[R])
    k.op("act", lambda e: e.activation(out=l0, in_=l0, func=AF.Ln), reads=[R], writes=[R])
    k.op("dve", lambda e: e.tensor_tensor(out=c.l1mlb, in0=l1, in1=l0, op=ALU.subtract), reads=[R], writes=[R])


class WS:
    pass


def alloc_hg_ws(k, ar, W):
    W.sets = []
    for si in range(2):
        blk = ar.al([128, 5, 512], F32)
        W.sets.append(tuple(blk[:, i, :] for i in range(5)) + (k.res("fchain%d" % si),))
        if si == 0:
            W.ab = blk[:, 1:3, :].rearrange("p a b -> p (a b)")
    W.u, W.a, W.bq, W.lk, W.L, W.R_f = W.sets[0]
    W.rb, W.negr, W.dl, W.c1, W.c2, W.er = [ar.al([128, 4, 4], F32) for _ in range(6)]
    W.R_cols = k.res("fcols")
    W.kT = ar.al([128, 4, 512], BF16)
    W.R_kT = k.res("kT")


def alloc_x_ws(k, c, ar, W, region, scr=None, R_scr=None):
    if region is not None:
        W.xt = [region[:, 0, :].bitcast(F32), region[:, 1, :].bitcast(F32)]
        W.hb = [region[:, 2, 0:1024], region[:, 2, 1024:2048]]
        W.scr = region[:, 3, :].bitcast(F32)
        W.R_scr = k.res("xscr")
    else:
        W.xt = [ar.al([128, D], F32) for _ in range(2)]
        W.hb = [ar.al([128, D], BF16) for _ in range(2)]
        W.scr, W.R_scr = scr, R_scr
    W.R_xt = [k.res("xt0"), k.res("xt1")]
    W.R_hb = [k.res("hb0"), k.res("hb1")]
    W.gA = ar.al([128, D], F32)
    W.R_gA = k.res("gA")
    k.dma("sp", W.gA, c.D["g_attn"].partition_broadcast(128), writes=[W.R_gA])


def phase_p1(k, c, ar, St):
    Dr = c.D
    m0 = ar.mark()
    W = WS()
    alloc_x_ws(k, c, ar, W, c.oT_hg)
    w1f, w1t = c.R32[:, :, 0:1280], c.R32[:, :, 1280:2048]
    R_w1 = k.res("w1")
    k.dma("pool", w1f, Dr["w1f"].rearrange("(kt p) n -> p kt n", p=128), writes=[R_w1])
    k.dma("pool", w1t, Dr["w1t"].rearrange("(kt p) n -> p kt n", p=128), writes=[R_w1])
    hT = ar.al([128, 8, 512], BF16)
    R_hT = k.res("hT")
    CKg, SKg = ar.al([128, 512], F32), ar.al([128, 512], F32)
    R_rt = k.res("ropetab")
    alloc_hg_ws(k, ar, W)
    t1, t2, R_t12 = W.u, W.a, W.R_f
    vtok = ar.al([128, 4, 512], BF16)
    R_vtok = k.res("vtok")
    ktok = ar.al([128, 4, 128], BF16)
    R_ktok = k.res("ktok")
    Sst = ar.al([128, 4, 128], F32)
    snapacc = ar.al([128, 4, 128], F32)
    R_S, R_snapacc = k.res("S"), k.res("snapacc")
    WC = [ar.al([128, 32, 64], BF16) for _ in range(3)]
    R_WC = k.res("WC")
    posT = ar.al([128, 32], BF16)
    cb = ar.al([128, 4], F32)
    xin = [[ar.al([128, 528], BF16) for _ in range(2)] for _ in range(2)]
    R_xin = [[k.res("xin%d%d" % (a, b)) for b in range(2)] for a in range(2)]
    CKc, SKc = ar.al([128, 32], F32), ar.al([128, 32], F32)
    R_ckc = k.res("ckc")
    VCf = ar.al([128, 512], F32)
    R_VCf = k.res("VCf")
    ctmp = ar.al([128, 4, 32], F32)
    R_ctmp = k.res("ctmp")
    for xi, nm in enumerate(("wck", "wckp", "wcv")):
        for g in range(2):
            k.dma("pool", WC[xi][64 * g:64 * g + 64], Dr[nm].rearrange("(l d) e -> d l e", d=64), writes=[R_WC])
    k.dma("pool", posT, Dr["posT"], writes=[R_WC])
    k.op("dve", lambda e: e.memset(Sst, 0.0), writes=[R_S])
    k.op("dve", lambda e: e.memset(St.VsA[:, :, :, 64:65], 1.0), writes=[St.R_VsA])
    k.op("dve", lambda e: e.memset(St.VwA[:, :, :, 64:65], 1.0), writes=[St.R_VwA])
    for a in range(2):
        k.op("dve", lambda e, a=a: e.memset(xin[a][0][:, 0:16], 0.0), writes=[R_xin[a][0]])
    p6 = c.psum[6]
    fns = []
    for xi in range(3):
        for g in range(2):
            for l in range(32):
                fns.append(lambda e, xi=xi, g=g, l=l: e.matmul(p6[64 * g:64 * g + 64, xi:xi + 1], lhsT=WC[xi][64 * g:64 * g + 64, l, :],
                                                               rhs=posT[64 * g:64 * g + 64, l:l + 1], start=(l == 0), stop=(l == 31)))
    k.op("pe", fns, reads=[R_WC], writes=[c.R_ps[6]])
    k.op("dve", lambda e: e.tensor_copy(out=cb[:, 0:3], in_=p6[:, 0:3]), reads=[c.R_ps[6]], writes=[R_WC])

    NG = c.n_groups
    for G in range(NG):
        norm_transpose_group(k, c, W, Dr["xb"], G * 512, hT, R_hT)
        k.dma("sp", CKg, Dr["CK"][:, G * 512:(G + 1) * 512], writes=[R_rt])
        k.dma("sp", SKg, Dr["SK"][:, G * 512:(G + 1) * 512], writes=[R_rt])
        xb_ = G % 2

        def fm(ft, bank):
            k.op("pe", [(lambda e, i=i: e.matmul(c.psum[bank], lhsT=w1f[:, i, ft * 128:(ft + 1) * 128], rhs=hT[:, i, :],
                                                 start=(i == 0), stop=(i == 7))) for i in range(8)],
                 reads=[R_w1, R_hT], writes=[c.R_ps[bank]])
        for a in range(2):
            fm(a, 2 + a)
            k.op("act", lambda e, a=a: e.activation(out=xin[a][xb_][:, 16:528], in_=c.psum[2 + a], func=AF.Copy),
                 reads=[c.R_ps[2 + a]], writes=[R_xin[a][xb_]])
            k.op("pool", lambda e, a=a: e.tensor_copy(out=xin[a][1 - xb_][:, 0:16], in_=xin[a][xb_][:, 512:528]),
                 reads=[R_xin[a][xb_]], writes=[R_xin[a][1 - xb_]])
        for which, dst, R_dst in ((0, St.KTs, St.R_KTs), (1, St.KTw, St.R_KTw)):
            fm(2 + 2 * which, 2)
            fm(3 + 2 * which, 3)
            k.op("dve", lambda e: e.tensor_tensor(out=t1, in0=c.psum[2], in1=CKg, op=ALU.mult), reads=[c.R_ps[2], R_rt], writes=[R_t12])
            k.op("dve", lambda e: e.tensor_tensor(out=t2, in0=c.psum[3], in1=SKg, op=ALU.mult), reads=[c.R_ps[3], R_rt, R_t12], writes=[R_t12])
            k.op("pool", lambda e, dst=dst: e.tensor_tensor(out=dst[:, G * 512:(G + 1) * 512], in0=t1, in1=t2, op=ALU.add),
                 reads=[R_t12], writes=[R_dst])
        for tt in range(4):
            tile_ = 4 * G + tt
            lt = hT
            k.op("pe", [(lambda e, i=i, tt=tt: e.matmul(c.psum[4][:, 0:256], lhsT=hT[:, i, tt * 128:(tt + 1) * 128], rhs=w1t[:, i, 0:256],
                                                        start=(i == 0), stop=(i == 7))) for i in range(8)],
                 reads=[R_w1, R_hT], writes=[c.R_ps[4]])
            k.op("pe", [(lambda e, i=i, tt=tt: e.matmul(c.psum[5], lhsT=hT[:, i, tt * 128:(tt + 1) * 128], rhs=w1t[:, i, 256:768],
                                                        start=(i == 0), stop=(i == 7))) for i in range(8)],
                 reads=[R_w1, R_hT], writes=[c.R_ps[5]])
            k.op("act", lambda e, tile_=tile_: e.activation(out=St.VsA[:, tile_, :, 0:64],
                                                            in_=c.psum[4][:, 0:128].rearrange("p (g d) -> p g d", g=2), func=AF.Copy),
                 reads=[c.R_ps[4]], writes=[St.R_VsA])
            k.op("act", lambda e, tile_=tile_: e.activation(out=St.VwA[:, tile_, :, 0:64],
                                                            in_=c.psum[4][:, 128:256].rearrange("p (g d) -> p g d", g=2), func=AF.Copy),
                 reads=[c.R_ps[4]], writes=[St.R_VwA])
            k.op("dve", lambda e, tt=tt: e.tensor_copy(out=vtok[:, tt, :], in_=c.psum[5]), reads=[c.R_ps[5]], writes=[R_vtok])
        fns = []
        for xi in range(3):
            src = xin[0][xb_] if xi < 2 else xin[1][xb_]
            for g in range(2):
                for l in range(32):
                    fns.append(lambda e, xi=xi, g=g, l=l, src=src: e.matmul(
                        p6[64 * g:64 * g + 64, 32 * xi:32 * xi + 32], lhsT=WC[xi][64 * g:64 * g + 64, l, :],
                        rhs=src[64 * g:64 * g + 64, l:l + 497:16], start=(l == 0), stop=(l == 31)))
        k.op("pe", fns, reads=[R_WC, R_xin[0][xb_], R_xin[1][xb_]], writes=[c.R_ps[6]])
        ms = slice(32 * G, 32 * G + 32)
        k.dma("sp", CKc, Dr["CKc"][:, ms], writes=[R_ckc])
        k.dma("sp", SKc, Dr["SKc"][:, ms], writes=[R_ckc])
        k.op("dve", lambda e: e.tensor_scalar(out=ctmp[:, 0, :], in0=p6[:, 0:32], scalar1=cb[:, 0:1], scalar2=None, op0=ALU.add),
             reads=[c.R_ps[6], R_WC], writes=[R_ctmp])
        k.op("dve", lambda e: e.tensor_scalar(out=ctmp[:, 1, :], in0=p6[:, 32:64], scalar1=cb[:, 1:2], scalar2=None, op0=ALU.add),
             reads=[c.R_ps[6], R_WC], writes=[R_ctmp])
        k.op("dve", lambda e, ms=ms: e.tensor_scalar(out=VCf[:, ms], in0=p6[:, 64:96], scalar1=cb[:, 2:3], scalar2=None, op0=ALU.add),
             reads=[c.R_ps[6], R_WC], writes=[R_VCf])
        k.op("pool", lambda e, ms=ms: e.tensor_tensor(out=ctmp[:, 0, :], in0=ctmp[:, 0, :], in1=CKc, op=ALU.mult),
             reads=[R_ctmp, R_ckc], writes=[R_ctmp])
        k.op("pool", lambda e, ms=ms: e.tensor_tensor(out=ctmp[:, 1, :], in0=ctmp[:, 1, :], in1=SKc, op=ALU.mult),
             reads=[R_ctmp, R_ckc], writes=[R_ctmp])
        k.op("pool", lambda e, ms=ms: e.tensor_tensor(out=St.KC[:, ms], in0=ctmp[:, 0, :], in1=ctmp[:, 1, :], op=ALU.add),
             reads=[R_ctmp], writes=[St.R_KC])
        def front(hd):
            bank = 2 + hd % 2
            fm(6 + hd, bank)
            f_front(k, c, W, c.psum[bank], c.R_ps[bank], hd)
        front(0)
        front(1)
        f_back(k, c, W, 0)
        front(2)
        f_back(k, c, W, 1)
        front(3)
        f_back(k, c, W, 2)
        f_back(k, c, W, 3)
        p6b = c.psum[6].bitcast(BF16)
        for tt in range(4):
            sl = slice(tt * 128, (tt + 1) * 128)
            k.op("pe", [(lambda e, hd=hd, sl=sl: e.transpose(out=p6b[:, hd * 128:(hd + 1) * 128], in_=W.kT[:, hd, sl], identity=c.identb))
                        for hd in range(4)], reads=[W.R_kT, c.R_const], writes=[c.R_ps[6]])
            k.op("act", lambda e: e.activation(out=ktok, in_=p6b[:, 0:512].rearrange("p (h x) -> p h x", h=4), func=AF.Copy),
                 reads=[c.R_ps[6]], writes=[R_ktok])
            k.op("pe", [(lambda e, hd=hd, tt=tt: e.matmul(c.psum[7][:, hd * 128:(hd + 1) * 128], lhsT=ktok[:, hd, :],
                                                          rhs=vtok[:, tt, hd * 128:(hd + 1) * 128], start=True, stop=True))
                        for hd in range(4)], reads=[R_ktok, R_vtok], writes=[c.R_ps[7]])
            Sf, Af = Sst.rearrange("p h x -> p (h x)"), snapacc.rearrange("p h x -> p (h x)")
            if tt == 0:
                k.op("dve", lambda e: e.tensor_scalar(out=Af, in0=Sf, scalar1=c.onehot[:, 0:1], scalar2=None, op0=ALU.mult),
                     reads=[R_S, c.R_const], writes=[R_snapacc])
            else:
                k.op("dve", lambda e, tt=tt: e.scalar_tensor_tensor(out=Af, in0=Sf, scalar=c.onehot[:, tt:tt + 1], in1=Af,
                                                                    op0=ALU.mult, op1=ALU.add),
                     reads=[R_S, c.R_const, R_snapacc], writes=[R_snapacc])
            for hd in range(4):
                k.op("dve", lambda e, hd=hd, tt=tt: e.tensor_scalar(out=Sst[:, hd, :], in0=Sst[:, hd, :], scalar1=W.c1[:, hd, tt:tt + 1],
                                                                    scalar2=None, op0=ALU.mult),
                     reads=[R_S, W.R_cols], writes=[R_S])
                k.op("dve", lambda e, hd=hd, tt=tt: e.scalar_tensor_tensor(
                    out=Sst[:, hd, :], in0=c.psum[7][:, hd * 128:(hd + 1) * 128], scalar=W.c2[:, hd, tt:tt + 1], in1=Sst[:, hd, :],
                    op0=ALU.mult, op1=ALU.add), reads=[c.R_ps[7], R_S, W.R_cols], writes=[R_S])
        k.op("act", lambda e: e.activation(out=St.SNAP[:, G, :, :], in_=snapacc, func=AF.Copy), reads=[R_snapacc], writes=[St.R_SNAP])
    k.op("dve", lambda e: e.memset(St.VCA[:, :, :, 64:65], 1.0), writes=[St.R_VCA])
    for g in range(2):
        k.dma("pool", St.VCA[:, :, g, 65:193], Dr["ovl"], writes=[St.R_VCA])
    pw = c.psum[6]
    k.op("pe", [(lambda e, mt=mt: e.transpose(out=pw[:, mt * 128:(mt + 1) * 128], in_=VCf[:, mt * 128:(mt + 1) * 128], identity=c.identf))
                for mt in range(4)], reads=[R_VCf, c.R_const], writes=[c.R_ps[6]])
    for mt in range(4):
        k.op("act", lambda e, mt=mt: e.activation(out=St.VCA[:, mt, :, 0:64],
                                                  in_=pw[:, mt * 128:(mt + 1) * 128].rearrange("p (g d) -> p g d", g=2), func=AF.Copy),
             reads=[c.R_ps[6]], writes=[St.R_VCA])
    k.op("dve", lambda e: e.memset(St.VCA[0:1, 0, :, :], 0.0), writes=[St.R_VCA])
    ar.release(m0)


def phase_p2pre(k, c, ar, St):
    Dr = c.D
    m0 = ar.mark()
    W = WS()
    alloc_hg_ws(k, ar, W)
    alloc_x_ws(k, c, ar, W, None, scr=W.ab, R_scr=W.R_f)
    hT = ar.al([128, 8, 512], BF16)
    R_hT = k.res("hT2")
    wch = [c.R32f[:, 8192 + b * 4096: 8192 + (b + 1) * 4096].rearrange("p (a b) -> p a b", a=8) for b in range(2)]
    R_wch = [k.res("wch%d" % i) for i in range(2)]
    wgt = ar.al([128, 8, 32], BF16)
    R_wgt = k.res("wgt")
    wst = dict(n=0)
    CQg, SQg = ar.al([128, 512], F32), ar.al([128, 512], F32)
    R_rt = k.res("ropetabq")
    t1, t2, R_t12 = W.u, W.a, W.R_f
    qT = ar.al([128, 4, 512], BF16)
    R_qT = k.res("qTh")
    e1, R_e1 = W.u, W.R_f
    vtok = ar.al([128, 512], BF16)
    R_vtok = k.res("vtok2")
    sgt = ar.al([128, 512], F32)
    R_sgt = k.res("sgt")
    AT = ar.al([128, 4, 128], BF16)
    R_AT = k.res("AT")
    Sp = ar.al([128, 4, 128], BF16)
    R_Sp = k.res("Sp")
    gnt = ar.al([128, 512], F32)
    R_gnt = k.res("gnt")
    o1, o2, R_o = W.bq, W.a, W.R_f
    yb = ar.al([128, 512], BF16)
    R_yb = k.res("yb")
    hs = ar.al([128, 16], F32)
    R_hs = k.res("hs")
    k.dma("pool", wgt, Dr["w2t"][:, 1024:1056].rearrange("(kt p) n -> p kt n", p=128), writes=[R_wgt])
    k.dma("sp", gnt, Dr["g_hg4"].partition_broadcast(128), writes=[R_gnt])

    def wload(src, c0, n=512):
        b = wst["n"] % 2
        wst["n"] += 1
        k.dma("pool", wch[b][:, :, 0:n], src[:, c0:c0 + n].rearrange("(kt p) n -> p kt n", p=128), writes=[R_wch[b]])
        return wch[b], R_wch[b]

    for go in range(NT // 4):
        tok = slice(go * 512, (go + 1) * 512)
        norm_transpose_group(k, c, W, Dr["xo"], go * 512, hT, R_hT)
        k.dma("sp", CQg, Dr["CQ"][:, tok], writes=[R_rt])
        k.dma("sp", SQg, Dr["SQ"][:, tok], writes=[R_rt])

        def fm(wt, R_wt, j, bank):
            k.op("pe", [(lambda e, i=i: e.matmul(c.psum[bank], lhsT=wt[:, i, j * 128:(j + 1) * 128], rhs=hT[:, i, :],
                                                 start=(i == 0), stop=(i == 7))) for i in range(8)],
                 reads=[R_wt, R_hT], writes=[c.R_ps[bank]])
        wq, R_wq = wload(Dr["w2f"], 0)
        wqp, R_wqp = wload(Dr["w2f"], 512)
        for a in range(4):
            fm(wq, R_wq, a, 2)
            fm(wqp, R_wqp, a, 3)
            k.op("dve", lambda e: e.tensor_tensor(out=t1, in0=c.psum[2], in1=CQg, op=ALU.mult), reads=[c.R_ps[2], R_rt], writes=[R_t12])
            k.op("dve", lambda e: e.tensor_tensor(out=t2, in0=c.psum[3], in1=SQg, op=ALU.mult), reads=[c.R_ps[3], R_rt, R_t12], writes=[R_t12])
            k.op("pool", lambda e, a=a: e.tensor_tensor(out=c.QT[:, 4 * go:4 * go + 4, a, :], in0=t1.rearrange("p (i t) -> p i t", i=4),
                                                        in1=t2.rearrange("p (i t) -> p i t", i=4), op=ALU.add), reads=[R_t12], writes=[c.R_QT])
        whq, R_whq = wload(Dr["w2f"], 1024)
        whf, R_whf = wload(Dr["w2f"], 1536)
        def front(hd):
            bank = 2 + hd % 2
            fm(whf, R_whf, hd, bank)
            f_front(k, c, W, c.psum[bank], c.R_ps[bank], hd)

        def back(hd):
            f_back(k, c, W, hd)
            su, sa, sbq, slk, sL, sRF = W.sets[hd % 2]
            fm(whq, R_whq, hd, 6)
            for tt in range(4):
                sl = slice(tt * 128, (tt + 1) * 128)
                k.op("act", lambda e, sl=sl, tt=tt: e.activation(out=su[:, sl], in_=sL[:, sl], func=AF.Exp, bias=W.negr[:, hd, tt:tt + 1]),
                     reads=[sRF, W.R_cols], writes=[sRF])
            k.op("dve", lambda e: e.tensor_tensor(out=qT[:, hd, :], in0=c.psum[6], in1=su, op=ALU.mult),
                 reads=[c.R_ps[6], sRF], writes=[R_qT])
        front(0)
        front(1)
        back(0)
        front(2)
        back(1)
        front(3)
        back(2)
        back(3)
        whi, R_whi = wload(Dr["w2t"], 0)
        whg, R_whg = wload(Dr["w2t"], 512)
        for tt in range(4):
            i_own = 4 * go + tt
            sl = slice(tt * 128, (tt + 1) * 128)
            for (wt, R_wt, n, bank) in ((whi, R_whi, 512, 4), (whg, R_whg, 512, 5), (wgt, R_wgt, 32, 6)):
                k.op("pe", [(lambda e, i=i, wt=wt, n=n, bank=bank: e.matmul(c.psum[bank][:, 0:n], lhsT=hT[:, i, sl], rhs=wt[:, i, 0:n],
                                                                            start=(i == 0), stop=(i == 7))) for i in range(8)],
                     reads=[R_wt, R_hT], writes=[c.R_ps[bank]])
            k.op("dve", lambda e: e.tensor_copy(out=vtok, in_=c.psum[4]), reads=[c.R_ps[4]], writes=[R_vtok])
            k.op("act", lambda e: e.activation(out=sgt, in_=c.psum[5], func=AF.Silu), reads=[c.R_ps[5]], writes=[R_sgt])
            k.op("act", lambda e, i_own=i_own: e.activation(out=c.gsig[:, i_own, :], in_=c.psum[6][:, 0:24], func=AF.Sigmoid),
                 reads=[c.R_ps[6]], writes=[c.R_gsig])
            k.op("pe", [(lambda e, hd=hd: e.matmul(c.psum[7][:, hd * 128:(hd + 1) * 128], lhsT=W.kT[:, hd, sl], rhs=qT[:, hd, sl],
                                                   start=True, stop=True)) for hd in range(4)],
                 reads=[W.R_kT, R_qT], writes=[c.R_ps[7]])
            k.op("dve", lambda e: e.tensor_scalar(out=W.lk, in0=c.psum[7], scalar1=1e30, scalar2=-1e30, op0=ALU.min, op1=ALU.max),
                 reads=[c.R_ps[7], W.R_f], writes=[W.R_f])
            k.op("dve", lambda e: e.tensor_tensor(out=AT, in0=W.lk.rearrange("p (h x) -> p h x", h=4),
                                                  in1=c.tri.unsqueeze(1).broadcast_to([128, 4, 128]), op=ALU.mult),
                 reads=[W.R_f, c.R_const], writes=[R_AT])
            for hd in range(4):
                k.op("act", lambda e, hd=hd, tt=tt, i_own=i_own: e.activation(out=Sp[:, hd, :], in_=St.SNAP[:, i_own, hd, :], func=AF.Copy,
                                                                              scale=W.er[:, hd, tt:tt + 1]),
                     reads=[St.R_SNAP, W.R_cols], writes=[R_Sp])
            fns = []
            for hd in range(4):
                fns.append(lambda e, hd=hd, tt=tt: e.matmul(c.psum[4][:, hd * 128:(hd + 1) * 128], lhsT=AT[:, hd, :],
                                                            rhs=vtok[:, hd * 128:(hd + 1) * 128], start=True, stop=False))
                fns.append(lambda e, hd=hd: e.matmul(c.psum[4][:, hd * 128:(hd + 1) * 128], lhsT=qT[:, hd, sl],
                                                     rhs=Sp[:, hd, :], start=False, stop=True))
            k.op("pe", fns, reads=[R_AT, R_vtok, R_qT, R_Sp], writes=[c.R_ps[4]])
            for hd in range(4):
                k.op("act", lambda e, hd=hd: e.activation(out=o2[:, hd * 128:(hd + 1) * 128], in_=c.psum[4][:, hd * 128:(hd + 1) * 128],
                                                          func=AF.Square, accum_out=hs[:, hd:hd + 1]),
                     reads=[c.R_ps[4]], writes=[R_o, R_hs])
            k.op("act", lambda e: e.activation(out=hs[:, 4:8], in_=hs[:, 0:4], func=AF.Sqrt, scale=1.0 / 128, bias=c.epsb[:, 0:1]),
                 reads=[R_hs, c.R_const], writes=[R_hs])
            k.op("dve", lambda e: e.reciprocal(out=hs[:, 8:12], in_=hs[:, 4:8]), reads=[R_hs], writes=[R_hs])
            k.op("dve", lambda e: e.tensor_tensor(out=o1, in0=c.psum[4], in1=gnt, op=ALU.mult), reads=[c.R_ps[4], R_gnt, R_o], writes=[R_o])
            k.op("pool", lambda e, tt=tt: e.tensor_tensor(out=o1, in0=o1, in1=sgt, op=ALU.mult), reads=[R_o, R_sgt], writes=[R_o])
            k.op("dve", lambda e: e.tensor_tensor(out=yb.rearrange("p (h x) -> p h x", h=4), in0=o1.rearrange("p (h x) -> p h x", h=4),
                                                  in1=hs[:, 8:12].unsqueeze(2).broadcast_to([128, 4, 128]), op=ALU.mult),
                 reads=[R_o, R_hs], writes=[R_yb])
            p6b = c.psum[6].bitcast(BF16)
            k.op("pe", [(lambda e, hd=hd: e.transpose(out=p6b[:, hd * 128:(hd + 1) * 128], in_=yb[:, hd * 128:(hd + 1) * 128], identity=c.identb))
                        for hd in range(4)], reads=[R_yb, c.R_const], writes=[c.R_ps[6]])
            k.op("act", lambda e, i_own=i_own: e.activation(out=c.oT_hg[:, :, i_own * 128:(i_own + 1) * 128],
                                                            in_=p6b[:, 0:512].rearrange("p (h x) -> p h x", h=4), func=AF.Copy),
                 reads=[c.R_ps[6]], writes=[c.R_oThg])
    ar.release(m0)


def phase_nsa(k, c, ar, St):
    Dr = c.D
    m0 = ar.mark()
    TINY = 1e-30
    CBi = [ar.al([128, 128], BF16) for _ in range(2)]
    VALi = [ar.al([128, 128], F32) for _ in range(2)]
    ADDCi = [ar.al([128, 128], F32) for _ in range(2)]
    R_tab = [k.res("nsatab%d" % i) for i in range(2)]
    WMT = ar.al([128, 8, 128], BF16)
    CM = ar.al([128, 4, 128], BF16)
    R_cst = k.res("nsacst")
    k.dma("pool", WMT, Dr["WMT"], writes=[R_cst])
    k.dma("pool", CM, Dr["CM"], writes=[R_cst])
    PT = [ar.al([128, 512], BF16) for _ in range(3)]
    R_PT = [k.res("PT%d" % i) for i in range(3)]
    Uc = ar.al([128, 4, 193], F32)
    R_Uc = k.res("Uc")
    Os = ar.al([128, 4, 65], F32)
    Ow = ar.al([128, 4, 65], F32)
    R_Os, R_Ow = k.res("Os"), k.res("Ow")
    score, sc2, imp = ar.al([128, 128], F32), ar.al([128, 128], F32), ar.al([128, 128], F32)
    R_sel = k.res("sel")
    selb = ar.al([128, 128], BF16)
    R_selb = k.res("selb")
    bd = ar.al([128, 4, 128], BF16)
    R_bd = k.res("bd")
    selX = ar.al([128, 128, 64], BF16)
    R_selX = k.res("selX")
    cols = ar.al([128, 64], F32)
    R_cols = k.res("nsacols")
    acc, tmp = ar.al([128, 4, 64], F32), ar.al([128, 4, 64], F32)
    R_acc = k.res("nsaacc")
    onsa = ar.al([128, 2, 4, 64], BF16)
    R_onsa = k.res("onsa")
    st = dict(s=0, p=0)
    pO_s, pO_w = c.psum[3][:, 0:260], c.psum[4][:, 0:260]
    pU = [c.psum[5], c.psum[6]]

    pend = []

    def flush_pv(keep=0):
        while len(pend) > keep:
            pend.pop(0)()

    def unit(KT, R_KT, kt_slice, QTg, g, bias, Vaug, R_V, outs, R_outs):
        sb = st["s"] % 3
        st["s"] += 1
        pb = st["p"] % 3
        st["p"] += 1
        S = c.psum[sb]
        fns = [lambda e: e.matmul(S, lhsT=KT[64 * g:64 * g + 64, kt_slice], rhs=QTg, start=True, stop=(bias is None))]
        rd = [R_KT, c.R_QT]
        if bias is not None:
            bl, R_bl = bias
            fns.append(lambda e: e.matmul(S, lhsT=bl, rhs=c.I4, start=False, stop=True))
            rd += [R_bl, c.R_const]
        k.op("pe", fns, reads=rd, writes=[c.R_ps[sb]])
        k.op("act", lambda e: e.activation(out=PT[pb], in_=S, func=AF.Exp), reads=[c.R_ps[sb]], writes=[R_PT[pb]])

        def pv():
            k.op("pe", [(lambda e, a=a: e.matmul(outs[a], lhsT=PT[pb][:, a * 128:(a + 1) * 128], rhs=Vaug, start=False, stop=False,
                                                 skip_group_check=True)) for a in range(4)],
                 reads=[R_PT[pb], R_V], writes=R_outs)
        pend.append(pv)
        flush_pv(keep=2)

    for i in range(c.n_blocks):
        tb = i % 2
        k.dma("pool", CBi[tb], Dr["CB"][:, i, :], writes=[R_tab[tb]])
        k.dma("sp", VALi[tb], Dr["VAL"][:, i, :], writes=[R_tab[tb]])
        k.dma("sp", ADDCi[tb], Dr["ADDC"][:, i, :], writes=[R_tab[tb]])
        for g in range(2):
            QTg = c.QT[64 * g:64 * g + 64, i, :, :].rearrange("p a t -> p (a t)")
            nmt = i // 4 + 1
            k.op("dve", lambda e: e.memset(pU[0], 0.0), writes=[c.R_ps[5]])
            k.op("dve", lambda e: e.memset(pU[1], 0.0), writes=[c.R_ps[6]])
            outsU = [pU[a // 2][:, (a % 2) * 193:(a % 2) * 193 + 193] for a in range(4)]
            for mt in range(nmt):
                bias = (CBi[tb], R_tab[tb]) if mt == nmt - 1 else None
                unit(St.KC, St.R_KC, slice(mt * 128, (mt + 1) * 128), QTg, g, bias, St.VCA[:, mt, g, :], St.R_VCA, outsU, [c.R_ps[5], c.R_ps[6]])
            flush_pv()
            k.op("act", lambda e: e.activation(out=Uc[:, 0:2, :], in_=pU[0][:, 0:386].rearrange("p (a x) -> p a x", a=2), func=AF.Copy),
                 reads=[c.R_ps[5]], writes=[R_Uc])
            k.op("act", lambda e: e.activation(out=Uc[:, 2:4, :], in_=pU[1][:, 0:386].rearrange("p (a x) -> p a x", a=2), func=AF.Copy),
                 reads=[c.R_ps[6]], writes=[R_Uc])
            k.op("dve", lambda e: e.memset(c.psum[4], 0.0), writes=[c.R_ps[4]])
            outsW = [pO_w[:, a * 65:(a + 1) * 65] for a in range(4)]
            for r in range(8):
                kt = 4 * i - 4 + r
                if kt < 0:
                    continue
                unit(St.KTw, St.R_KTw, slice(kt * 128, (kt + 1) * 128), QTg, g, (WMT[:, r, :], R_cst), St.VwA[:, kt, g, :], St.R_VwA, outsW, [c.R_ps[4]])
            zc, rzc = cols[:, 0:4], cols[:, 4:8]
            k.op("dve", lambda e: e.tensor_scalar(out=zc, in0=Uc[:, :, 64], scalar1=TINY, scalar2=None, op0=ALU.max), reads=[R_Uc], writes=[R_cols])
            k.op("dve", lambda e: e.reciprocal(out=rzc, in_=zc), reads=[R_cols], writes=[R_cols])
            k.op("dve", lambda e: e.tensor_scalar(out=imp, in0=Uc[:, 0, 65:193], scalar1=rzc[:, 0:1], scalar2=None, op0=ALU.mult),
                 reads=[R_Uc, R_cols], writes=[R_sel])
            for a in range(1, 4):
                k.op("dve", lambda e, a=a: e.scalar_tensor_tensor(out=imp, in0=Uc[:, a, 65:193], scalar=rzc[:, a:a + 1], in1=imp,
                                                                  op0=ALU.mult, op1=ALU.add), reads=[R_Uc, R_cols, R_sel], writes=[R_sel])
            k.op("dve", lambda e: e.tensor_tensor(out=score, in0=imp, in1=VALi[tb], op=ALU.mult), reads=[R_sel, R_tab[tb]], writes=[R_sel])
            k.op("dve", lambda e: e.tensor_tensor(out=score, in0=score, in1=ADDCi[tb], op=ALU.add), reads=[R_sel, R_tab[tb]], writes=[R_sel])
            m8a, m8b = cols[:, 8:16], cols[:, 16:24]
            k.op("dve", lambda e: e.max(out=m8a, in_=score), reads=[R_sel], writes=[R_cols])
            k.op("dve", lambda e: e.match_replace(out=sc2, in_to_replace=m8a, in_values=score, imm_value=-1e9), reads=[R_sel, R_cols], writes=[R_sel])
            k.op("dve", lambda e: e.max(out=m8b, in_=sc2), reads=[R_sel], writes=[R_cols])
            k.op("dve", lambda e: e.tensor_scalar(out=selb, in0=score, scalar1=m8b[:, 7:8], scalar2=NEG, op0=ALU.is_lt, op1=ALU.mult),
                 reads=[R_sel, R_cols], writes=[R_selb])
            for r in range(4):
                kt = 4 * i + r
                k.op("dve", lambda e, r=r, kt=kt: e.tensor_tensor(
                    out=bd[:, r, :].rearrange("p (b x) -> p b x", b=2), in0=CM[:, r, :].rearrange("p (b x) -> p b x", b=2),
                    in1=selb[:, 2 * kt:2 * kt + 2].unsqueeze(2).broadcast_to([128, 2, 64]), op=ALU.add),
                    reads=[R_cst, R_selb], writes=[R_bd])
            if i > 0:
                nbk = 8 * i
                k.op("pool", lambda e, nbk=nbk: e.tensor_copy(out=selX[:, 0:nbk, :], in_=selb[:, 0:nbk].unsqueeze(2).broadcast_to([128, nbk, 64])),
                     reads=[R_selb], writes=[R_selX])
            k.op("dve", lambda e: e.memset(c.psum[3], 0.0), writes=[c.R_ps[3]])
            outsS = [pO_s[:, a * 65:(a + 1) * 65] for a in range(4)]
            for kt in range(4 * i + 4):
                if kt < 4 * i:
                    bl = selX[:, 2 * kt:2 * kt + 2, :].rearrange("p b x -> p (b x)")
                    bias = (bl, R_selX)
                else:
                    bias = (bd[:, kt - 4 * i, :], R_bd)
                unit(St.KTs, St.R_KTs, slice(kt * 128, (kt + 1) * 128), QTg, g, bias, St.VsA[:, kt, g, :], St.R_VsA, outsS, [c.R_ps[3]])
            flush_pv()
            k.op("act", lambda e: e.activation(out=Os, in_=pO_s.rearrange("p (a x) -> p a x", a=4), func=AF.Copy), reads=[c.R_ps[3]], writes=[R_Os])
            k.op("act", lambda e: e.activation(out=Ow, in_=pO_w.rearrange("p (a x) -> p a x", a=4), func=AF.Copy), reads=[c.R_ps[4]], writes=[R_Ow])
            gs = c.gsig[:, i, 12 * g:12 * g + 12].rearrange("p (a x) -> p a x", a=4)
            zs, zw, cfc, cfs, cfw = cols[:, 24:28], cols[:, 28:32], cols[:, 32:36], cols[:, 36:40], cols[:, 40:44]
            k.op("dve", lambda e: e.tensor_scalar(out=zs, in0=Os[:, :, 64], scalar1=TINY, scalar2=None, op0=ALU.max), reads=[R_Os], writes=[R_cols])
            k.op("dve", lambda e: e.tensor_scalar(out=zw, in0=Ow[:, :, 64], scalar1=TINY, scalar2=None, op0=ALU.max), reads=[R_Ow], writes=[R_cols])
            k.op("dve", lambda e: e.reciprocal(out=zs, in_=zs), reads=[R_cols], writes=[R_cols])
            k.op("dve", lambda e: e.reciprocal(out=zw, in_=zw), reads=[R_cols], writes=[R_cols])
            k.op("dve", lambda e: e.tensor_tensor(out=cfc, in0=rzc, in1=gs[:, :, 0], op=ALU.mult), reads=[R_cols, c.R_gsig], writes=[R_cols])
            k.op("dve", lambda e: e.tensor_tensor(out=cfs, in0=zs, in1=gs[:, :, 1], op=ALU.mult), reads=[R_cols, c.R_gsig], writes=[R_cols])
            k.op("dve", lambda e: e.tensor_tensor(out=cfw, in0=zw, in1=gs[:, :, 2], op=ALU.mult), reads=[R_cols, c.R_gsig], writes=[R_cols])
            bc = lambda col: col.unsqueeze(2).broadcast_to([128, 4, 64])
            k.op("dve", lambda e: e.tensor_tensor(out=acc, in0=Uc[:, :, 0:64], in1=bc(cfc), op=ALU.mult), reads=[R_Uc, R_cols], writes=[R_acc])
            k.op("dve", lambda e: e.tensor_tensor(out=tmp, in0=Os[:, :, 0:64], in1=bc(cfs), op=ALU.mult), reads=[R_Os, R_cols, R_acc], writes=[R_acc])
            k.op("pool", lambda e: e.tensor_tensor(out=acc, in0=acc, in1=tmp, op=ALU.add), reads=[R_acc], writes=[R_acc])
            k.op("dve", lambda e: e.tensor_tensor(out=tmp, in0=Ow[:, :, 0:64], in1=bc(cfw), op=ALU.mult), reads=[R_Ow, R_cols, R_acc], writes=[R_acc])
            k.op("pool", lambda e, g=g: e.tensor_tensor(out=onsa[:, g, :, :], in0=acc, in1=tmp, op=ALU.add), reads=[R_acc], writes=[R_onsa])
        p7b = c.psum[7].bitcast(BF16)
        of = onsa.rearrange("p g a d -> p (g a d)")
        k.op("pe", [(lambda e, j=j: e.transpose(out=p7b[:, j * 128:(j + 1) * 128], in_=of[:, j * 128:(j + 1) * 128], identity=c.identb))
                    for j in range(4)], reads=[R_onsa, c.R_const], writes=[c.R_ps[7]])
        k.op("act", lambda e, i=i: e.activation(out=c.oT_nsa[:, :, i * 128:(i + 1) * 128],
                                                in_=p7b[:, 0:512].rearrange("p (j x) -> p j x", j=4), func=AF.Copy),
             reads=[c.R_ps[7]], writes=[c.R_oTnsa])
    ar.release(m0)


def phase_p2c(k, c, ar, X1, R_X1):
    Dr = c.D
    m0 = ar.mark()
    W = WS()
    scr = ar.al([128, D], F32)
    alloc_x_ws(k, c, ar, W, None, scr=scr, R_scr=k.res("scr2c"))
    hT = ar.al([128, 8, 512], BF16)
    R_hT = k.res("hT3")
    wbn, wbh = ar.al([128, 4, D], BF16), ar.al([128, 4, D], BF16)
    R_wb = k.res("wbr")
    wch = [ar.al([128, 8, 512], BF16) for _ in range(2)]
    R_wch = [k.res("wchc%d" % i) for i in range(2)]
    mixT = ar.al([128, 8, 512], BF16)
    R_mixT = k.res("mixT")
    sg1, sg2, mx1 = ar.al([128, 512], F32), ar.al([128, 512], F32), ar.al([128, 512], F32)
    R_sg1, R_sg2, R_mx = k.res("sg1"), k.res("sg2"), k.res("mx1")
    k.dma("pool", wbn, Dr["w_brn"].rearrange("(kt p) n -> p kt n", p=128), writes=[R_wb])
    k.dma("pool", wbh, Dr["w_brh"].rearrange("(kt p) n -> p kt n", p=128), writes=[R_wb])
    for go in range(NT // 4):
        tok = slice(go * 512, (go + 1) * 512)
        for tt in range(4):
            t = 4 * go + tt
            k.dma("sp", X1[:, t, :], Dr["xo"][t * 128:(t + 1) * 128, :], writes=[R_X1[t]])
        norm_transpose_group(k, c, W, Dr["xo"], go * 512, hT, R_hT)
        for hf in range(2):
            k.dma("pool", wch[0], Dr["w_mg"][:, hf * 512:(hf + 1) * 512].rearrange("(kt p) n -> p kt n", p=128), writes=[R_wch[0]])
            k.dma("pool", wch[1], Dr["w_mg"][:, 1024 + hf * 512:1024 + (hf + 1) * 512].rearrange("(kt p) n -> p kt n", p=128), writes=[R_wch[1]])
            for f4 in range(4):
                ft = hf * 4 + f4
                fs = slice(f4 * 128, (f4 + 1) * 128)
                gs_ = slice(ft * 128, (ft + 1) * 128)
                k.op("pe", [(lambda e, i=i: e.matmul(c.psum[2], lhsT=wch[0][:, i, fs], rhs=hT[:, i, :], start=(i == 0), stop=(i == 7)))
                            for i in range(8)], reads=[R_wch[0], R_hT], writes=[c.R_ps[2]])
                k.op("pe", [(lambda e, i=i: e.matmul(c.psum[3], lhsT=wch[1][:, i, fs], rhs=hT[:, i, :], start=(i == 0), stop=(i == 7)))
                            for i in range(8)], reads=[R_wch[1], R_hT], writes=[c.R_ps[3]])
                k.op("pe", [(lambda e, i=i: e.matmul(c.psum[4], lhsT=wbn[:, i, gs_], rhs=c.oT_nsa[:, i, tok], start=(i == 0), stop=(i == 3)))
                            for i in range(4)], reads=[R_wb, c.R_oTnsa], writes=[c.R_ps[4]])
                k.op("pe", [(lambda e, i=i: e.matmul(c.psum[5], lhsT=wbh[:, i, gs_], rhs=c.oT_hg[:, i, tok], start=(i == 0), stop=(i == 3)))
                            for i in range(4)], reads=[R_wb, c.R_oThg], writes=[c.R_ps[5]])
                k.op("act", lambda e: e.activation(out=sg1, in_=c.psum[2], func=AF.Sigmoid), reads=[c.R_ps[2]], writes=[R_sg1])
                k.op("act", lambda e: e.activation(out=sg2, in_=c.psum[3], func=AF.Sigmoid), reads=[c.R_ps[3]], writes=[R_sg2])
                k.op("dve", lambda e: e.tensor_tensor(out=mx1, in0=c.psum[4], in1=sg1, op=ALU.mult), reads=[c.R_ps[4], R_sg1], writes=[R_mx])
                k.op("dve", lambda e: e.tensor_tensor(out=sg2, in0=c.psum[5], in1=sg2, op=ALU.mult), reads=[c.R_ps[5], R_sg2], writes=[R_sg2])
                k.op("pool", lambda e, ft=ft: e.tensor_tensor(out=mixT[:, ft, :], in0=mx1, in1=sg2, op=ALU.add),
                     reads=[R_mx, R_sg2], writes=[R_mixT])
        for hf in range(2):
            k.dma("pool", wch[hf], Dr["w_out"][:, hf * 512:(hf + 1) * 512].rearrange("(kt p) n -> p kt n", p=128), writes=[R_wch[hf]])
        for tt in range(4):
            t = 4 * go + tt
            for hf in range(2):
                bank = 6 + hf
                k.op("pe", [(lambda e, i=i: e.matmul(c.psum[bank], lhsT=mixT[:, i, tt * 128:(tt + 1) * 128], rhs=wch[hf][:, i, :],
                                                     start=(i == 0), stop=(i == 7))) for i in range(8)],
                     reads=[R_mixT, R_wch[hf]], writes=[c.R_ps[bank]])
                xs = X1[:, t, hf * 512:(hf + 1) * 512]
                k.op("dve", lambda e, xs=xs, bank=bank: e.tensor_tensor(out=xs, in0=c.psum[bank], in1=xs, op=ALU.add),
                     reads=[c.R_ps[bank], R_X1[t]], writes=[R_X1[t]])
    ar.release(m0)


def phase_attn(k, c, ar, stage):
    St = WS()
    St.KTs, St.KTw = ar.al([128, SEQ], BF16), ar.al([128, SEQ], BF16)
    St.VsA, St.VwA = ar.al([128, 64, 2, 65], BF16), ar.al([128, 64, 2, 65], BF16)
    St.KC = ar.al([128, 512], BF16)
    St.VCA = ar.al([128, 4, 2, 193], BF16)
    St.SNAP = ar.al([128, NT, 4, 128], BF16)
    for n in ("KTs", "KTw", "VsA", "VwA", "KC", "VCA", "SNAP"):
        setattr(St, "R_" + n, k.res(n))
    phase_p1(k, c, ar, St)
    for n in ("KTs", "KTw", "VsA", "VwA", "KC", "VCA", "SNAP"):
        c.dump(n, getattr(St, n), [getattr(St, "R_" + n)])
    k.flush()
    phase_p2pre(k, c, ar, St)
    c.dump("QT", c.QT, [c.R_QT])
    c.dump("gsig", c.gsig, [c.R_gsig])
    c.dump("oT_hg", c.oT_hg, [c.R_oThg])
    k.flush()
    phase_nsa(k, c, ar, St)
    c.dump("oT_nsa", c.oT_nsa, [c.R_oTnsa])
    return St


def build(stage="full", n_exp=NE):
    nc = bass.Bass("TRN2", target_bir_lowering=False)
    Dr = {}

    def din(name, shape, dt=F32):
        Dr[name] = nc.dram_tensor(name, list(shape), dt, kind="ExternalInput").ap()

    for name, shape, dt in input_specs(max(n_exp, 1), stage):
        din(name, shape, dt)
    Dr["y"] = nc.dram_tensor("y", [NTOK, D], F32, kind="ExternalOutput").ap()
    with ExitStack() as es:
        ARENA_BYTES = 207 * 1024
        big = es.enter_context(nc.sbuf_tensor("arena", [128, ARENA_BYTES // 2], BF16))
        pst = es.enter_context(nc.psum_tensor("ps", [128, 4096], F32))
        ar = Arena(big, ARENA_BYTES)
        k = KH(nc, es)
        k.oplim = _NC_CACHE.get("oplim", 10 ** 9)
        c = Ctx()
        c.nc, c.D, c.n_exp = nc, Dr, n_exp
        dumps = []

        def dump(name, ap, rs):
            if not _NC_CACHE.get("dbg"):
                return
            dt_ = ap.dtype
            dr = nc.dram_tensor("dbg_" + name, list(ap.shape), dt_, kind="ExternalOutput").ap()
            r = k.res("dbg_" + name)
            k.dma("sp", dr, ap, reads=list(rs), key=r)
            dumps.append(r)
        c.dump = dump
        c.psum = [pst[:, i * 512:(i + 1) * 512] for i in range(8)]
        c.pwide = lambda i: pst[:, i * 512:(i + 2) * 512]
        c.R_ps = [k.res("psb%d" % i, excl=True) for i in range(8)]
        c.R_const = k.res("const")
        c.identf = ar.al([128, 128], F32)
        c.identb = ar.al([128, 128], BF16)
        c.epsb = ar.al([128, 1], F32)
        c.ssq = ar.al([128, 8], F32)
        c.std = ar.al([128, 8], F32)
        c.rstd = ar.al([128, 8], F32)
        c.rs_res = [k.res("rs%d" % i) for i in range(8)]
        c.rs_i = 0
        k.dma("sp", c.identf, Dr["identf"], writes=[c.R_const])
        k.dma("sp", c.identb, Dr["identb"], writes=[c.R_const])
        k.op("dve", lambda e: e.memset(c.epsb, EPS), writes=[c.R_const])
        c.n_groups = _NC_CACHE.get("n_groups", 16)
        c.n_blocks = _NC_CACHE.get("n_blocks", NT)
        c.R32 = ar.al([128, 8, NTOK], BF16)
        c.R32f = c.R32.rearrange("p a b -> p (a b)")
        c.QT = c.R32f[:, 0:8192].rearrange("p (i a t) -> p i a t", i=NT, a=4)
        c.oT_nsa = c.R32[:, 4:8, :]
        c.oT_hg = ar.al([128, 4, NTOK], BF16)
        c.gsig = ar.al([128, NT, 24], F32)
        c.R_QT, c.R_oTnsa, c.R_oThg, c.R_gsig = k.res("QT"), k.res("oTnsa"), k.res("oThg"), k.res("gsig")
        hfT = c.R32
        R_hfT = [k.res("hfT_%d" % t) for t in range(NT)]
        R_X1 = [k.res("x1_%d" % t) for t in range(NT)]
        if stage != "moe_only":
            c.I4 = ar.al([128, 512], BF16)
            c.tri = ar.al([128, 128], BF16)
            c.onehot = ar.al([128, 4], F32)
            k.dma("pool", c.I4, Dr["I4"], writes=[c.R_const])
            k.dma("pool", c.tri, Dr["tri"], writes=[c.R_const])
            k.dma("sp", c.onehot, Dr["onehot"], writes=[c.R_const])
            setup_lb(k, c, ar)
            M1 = ar.mark()
            phase_attn(k, c, ar, stage)
            for r in dumps:
                k.rec["sp"].append(("w", r.dsem, r.dcnt))
            k.flush()
            ar.release(M1)
        X1 = ar.al([128, NT, D], F32)
        if stage == "moe_only":
            for t in range(NT):
                k.dma("sp", X1[:, t, :], Dr["xo"][t * 128:(t + 1) * 128, :], writes=[R_X1[t]])
        else:
            phase_p2c(k, c, ar, X1, R_X1)
            c.dump("X1", X1, R_X1)
            for r in dumps:
                if r.name == "dbg_X1":
                    k.rec["sp"].append(("w", r.dsem, r.dcnt))
        k.flush()
        if n_exp >= 0:
            phase_moe(k, c, ar, X1, R_X1, hfT, R_hfT)
            k.flush()
        phase_final(k, c, ar, X1, R_X1)
        k.flush()
    return nc


def input_specs(ne=NE, stage="full"):
    return [
        ("xb", (SEQ, D), F32), ("xo", (NTOK, D), F32),
        ("g_ffn", (D,), F32), ("g_fin", (D,), F32),
        ("w_r", (D, 36), F32), ("b_r", (36,), F32),
        ("w_gate", (ne, D, DFF), F32), ("w_up", (ne, D, DFF), F32), ("w_down", (ne, DFF, D), F32),
        ("identf", (128, 128), F32), ("identb", (128, 128), BF16),
    ] + (attn_input_specs() if stage != "moe_only" else [])


_NC_CACHE = {}


def host_inputs(inp, core, ne=NE):
    b, cp = core // 4, core % 4
    x = np.asarray(inp["x"], dtype=np.float32)
    m = {}
    m["xb"] = np.ascontiguousarray(x[b])
    m["xo"] = np.ascontiguousarray(x[b].reshape(NT, 4, 128, D)[:, cp].reshape(NTOK, D))
    m["g_ffn"] = np.ascontiguousarray(inp["ffn_norm"][0])
    m["g_fin"] = np.ascontiguousarray(inp["final_norm"])
    m["w_r"] = np.ascontiguousarray(np.concatenate([inp["w_grp"][0], inp["w_rtr"][0]], axis=1))
    m["b_r"] = np.ascontiguousarray(np.concatenate([inp["b_grp"][0], inp["b_rtr"][0]], axis=0))
    m["w_gate"] = np.ascontiguousarray(inp["w_gate"][0, :ne])
    m["w_up"] = np.ascontiguousarray(inp["w_up"][0, :ne])
    m["w_down"] = np.ascontiguousarray(inp["w_down"][0, :ne])
    m["identf"] = np.eye(128, dtype=np.float32)
    m["identb"] = np.eye(128, dtype=np.float32).astype(ml_dtypes.bfloat16)
    if _NC_CACHE.get("stage", "full") != "moe_only":
        m.update(attn_host_inputs(inp, b, cp))
    return m


def kernel(**inp):
    inp = {k_: np.asarray(v) for k_, v in inp.items()}
    stage = _NC_CACHE.get("stage", "full")
    key = ("nc", stage)
    if key not in _NC_CACHE:
        _NC_CACHE[key] = build(stage, _NC_CACHE.get("n_exp", NE))
    nc = _NC_CACHE[key]
    shared = None
    in_maps = []
    for core in range(8):
        m = host_inputs(inp, core, max(_NC_CACHE.get("n_exp", NE), 1))
        if shared is None:
            shared = m
        else:
            for kk in ("w_gate", "w_up", "w_down"):
                m[kk] = shared[kk]
        in_maps.append(m)
    res = run_bass_kernel_spmd(nc, in_maps, core_ids=list(range(8)))
    _NC_CACHE["last_results"] = res.results
    out = np.zeros((2, SEQ // 128, 128, D), dtype=np.float32)
    for core in range(8):
        b, cp = core // 4, core % 4
        y = np.asarray(res.results[core]["y"]).reshape(NT, 128, D)
        out[b, cp::4] = y
    return out.reshape(2, SEQ, D)
```

```python
import numpy as np
import ml_dtypes
import concourse.bass as bass
import concourse.mybir as mybir
from concourse.bass_utils import run_bass_kernel_spmd
from contextlib import ExitStack

F32 = mybir.dt.float32
BF16 = mybir.dt.bfloat16
AF = mybir.ActivationFunctionType
ALU = mybir.AluOpType
AX = mybir.AxisListType


class Res:
    __slots__ = ("name", "w", "rs", "dsem", "dcnt", "excl")

    def __init__(self, name, excl=False):
        self.name = name
        self.excl = excl
        self.w = None
        self.rs = {}
        self.dsem = None
        self.dcnt = 0


class _Proxy:
    def __init__(self):
        self.calls = []

    def __getattr__(self, name):
        def rec(*a, **kw):
            self.calls.append((name, a, kw))
        return rec


def _bind(f):
    p = _Proxy()
    f(p)
    assert len(p.calls) == 1, "one engine instruction per callable"
    name, a, kw = p.calls[0]
    return lambda eng: getattr(eng, name)(*a, **kw)


class KH:
    ENG = ("pe", "dve", "act", "pool", "sp")

    def __init__(self, nc, es):
        self.nc = nc
        self.es = es
        self.sem = {}
        self.cnt = {}
        for e in self.ENG:
            self.sem[e] = es.enter_context(nc.semaphore("s_" + e))
            self.cnt[e] = 0
        self.rec = {e: [] for e in self.ENG}
        self.seen = {e: {} for e in self.ENG}
        self.nsem = len(self.ENG)
        self.semobj = dict(self.sem)

    def res(self, name, excl=False):
        return Res(name, excl)

    def _dma_sem(self, r):
        if r.dsem is None:
            r.dsem = "d_" + r.name + "_%d" % self.nsem
            self.semobj[r.dsem] = self.es.enter_context(self.nc.semaphore(r.dsem))
            self.nsem += 1
        return r.dsem

    def _deps(self, e, reads, writes):
        deps = {}
        for r in reads:
            if r.w is not None:
                k, v = r.w
                deps[k] = max(deps.get(k, 0), v)
        for w in writes:
            if w.w is not None:
                k, v = w.w
                deps[k] = max(deps.get(k, 0), v)
            for k, v in w.rs.items():
                deps[k] = max(deps.get(k, 0), v)
        seen = self.seen[e]
        for k, v in deps.items():
            if seen.get(k, 0) >= v:
                continue
            seen[k] = v
            self.rec[e].append(("w", k, v))

    def op(self, e, fns, reads=(), writes=()):
        if callable(fns):
            fns = [fns]
        self.opn = getattr(self, "opn", 0) + 1
        if self.opn > getattr(self, "oplim", 10 ** 9):
            return
        ex = [r for r in reads if r.excl]
        if ex:
            reads = [r for r in reads if not r.excl]
            writes = list(writes) + [r for r in ex if r not in writes]
        self._deps(e, reads, writes)
        self.cnt[e] += 1
        v = self.cnt[e]
        fns = [_bind(f) for f in fns]
        for f in fns[:-1]:
            self.rec[e].append(("i", f, None, 0))
        self.rec[e].append(("i", fns[-1], e, 1))
        self.seen[e][e] = max(self.seen[e].get(e, 0), 0)
        for r in reads:
            r.rs[e] = v
        for w in writes:
            w.w = (e, v)
            w.rs = {}

    def dma(self, q, out, in_, reads=(), writes=(), key=None, **kw):
        self._deps(q, reads, writes)
        kr = key or (writes[0] if writes else reads[0])
        sk = self._dma_sem(kr)
        kr.dcnt += 16
        v = kr.dcnt
        self.rec[q].append(("i", lambda eng: eng.dma_start(out=out, in_=in_, **kw), sk, 16))
        for r in reads:
            r.rs[sk] = v
        for w in writes:
            w.w = (sk, v)
            w.rs = {}

    def wait_res(self, e, rs):
        self._deps(e, rs, ())

    def simulate(self):
        if not hasattr(self, "simval"):
            self.simval = {}
        val = self.simval
        ptr = {e: 0 for e in self.ENG}
        prog = True
        while prog:
            prog = False
            for e in self.ENG:
                items = self.rec[e]
                while ptr[e] < len(items):
                    it = items[ptr[e]]
                    if it[0] == "w":
                        if val.get(it[1], 0) >= it[2]:
                            ptr[e] += 1
                            prog = True
                        else:
                            break
                    else:
                        if it[2] is not None:
                            val[it[2]] = val.get(it[2], 0) + it[3]
                        ptr[e] += 1
                        prog = True
        for e in self.ENG:
            if ptr[e] < len(self.rec[e]):
                it = self.rec[e][ptr[e]]
                raise RuntimeError("DEADLOCK: engine %s stuck at item %d/%d waiting %s >= %s (have %s)" % (
                    e, ptr[e], len(self.rec[e]), it[1], it[2], val.get(it[1], 0)))

    def flush(self, name=None):
        nc = self.nc
        rec = self.rec
        semobj = self.semobj
        self.simulate()
        import os
        if os.environ.get("KH_DEBUG"):
            print("KH flush: ops so far", getattr(self, "opn", 0), {e: len(v) for e, v in self.rec.items()}, "nsem", self.nsem, flush=True)

        def play(eng, items):
            for it in items:
                if it[0] == "w":
                    eng.wait_ge(semobj[it[1]], it[2])
                else:
                    ins = it[1](eng)
                    if it[2] is not None:
                        ins.then_inc(semobj[it[2]], it[3])

        with nc.Block() as block:
            if rec["sp"]:
                @block.sync
                def _(eng):
                    play(eng, rec["sp"])
            if rec["pe"]:
                @block.tensor
                def _(eng):
                    play(eng, rec["pe"])
            if rec["dve"]:
                @block.vector
                def _(eng):
                    play(eng, rec["dve"])
            if rec["act"]:
                @block.scalar
                def _(eng):
                    play(eng, rec["act"])
            if rec["pool"]:
                @block.gpsimd
                def _(eng):
                    play(eng, rec["pool"])
        self.rec = {e: [] for e in self.ENG}

NEG = -30000.0
NT = 16
NTOK = NT * 128
SEQ = 8192
D = 1024
NE = 32
DFF = 512
EPS = 1e-6


class Arena:
    def __init__(self, big, nbytes):
        self.big = big
        self.n = nbytes
        self.off = 0

    def mark(self):
        return self.off

    def release(self, m):
        import os
        if os.environ.get("KH_DEBUG"):
            print("arena release: peak", getattr(self, "peak", 0), "->", m, "of", self.n, flush=True)
        self.peak = m
        self.off = m

    def al(self, shape, dt):
        esz = 4 if dt == F32 else 2
        per = int(np.prod(shape[1:])) * esz
        self.off = (self.off + 63) // 64 * 64
        o = self.off
        assert o + per <= self.n, ("arena overflow", o, per, self.n)
        self.off = o + per
        self.peak = max(getattr(self, "peak", 0), self.off)
        v = self.big[0:shape[0], o // 2:(o + per) // 2]
        if dt == F32:
            v = v.bitcast(F32)
        if len(shape) == 3:
            v = v.rearrange("p (a b) -> p a b", a=shape[1])
        elif len(shape) == 4:
            v = v.rearrange("p (a b c) -> p a b c", a=shape[1], b=shape[2])
        return v


class Ctx:
    pass


def rms_rstd(k, c, src, src_res, scr, scr_res, n_feat, tag):
    i = c.rs_i % 8
    c.rs_i += 1
    ssq, std, rstd = c.ssq[:, i:i + 1], c.std[:, i:i + 1], c.rstd[:, i:i + 1]
    R = c.rs_res[i]
    k.op("act", lambda e: e.activation(out=scr, in_=src, func=AF.Square, accum_out=ssq),
         reads=[src_res], writes=[scr_res, R])
    k.op("act", lambda e: e.activation(out=std, in_=ssq, func=AF.Sqrt, scale=1.0 / n_feat, bias=c.epsb[:, 0:1]),
         reads=[R, c.R_const], writes=[R])
    k.op("dve", lambda e: e.reciprocal(out=rstd, in_=std), reads=[R], writes=[R])
    return rstd, R


def phase_moe(k, c, ar, X1, R_X1, hfT, R_hfT):
    nc = c.nc
    Dr = c.D
    m0 = ar.mark()
    hf32 = [ar.al([128, D], F32) for _ in range(2)]
    R_hf32 = [k.res("hf32_%d" % i) for i in range(2)]
    scr = ar.al([128, D], F32)
    R_scr = k.res("moe_scr")
    hT32 = [ar.al([128, 8, 128], F32) for _ in range(2)]
    R_hT32 = [k.res("hT32_%d" % i) for i in range(2)]
    wr32 = ar.al([128, 8, 36], F32)
    R_wr = k.res("wr32")
    brt = ar.al([128, 36], F32)
    gft = ar.al([128, D], F32)
    R_gft = k.res("gft")
    comb = ar.al([128, NT, NE], F32)
    R_comb = k.res("comb")
    sm = ar.al([128, 128], F32)
    R_sm = k.res("moe_sm")
    k.dma("sp", wr32, Dr["w_r"].rearrange("(kt p) n -> p kt n", p=128), writes=[R_wr])
    k.dma("sp", brt, Dr["b_r"].partition_broadcast(128), writes=[R_wr])
    k.dma("sp", gft, Dr["g_ffn"].partition_broadcast(128), writes=[R_gft])
    pT = [c.pwide(0), c.pwide(2)]
    R_pT = [[c.R_ps[0], c.R_ps[1]], [c.R_ps[2], c.R_ps[3]]]
    pL = c.psum[4]
    R_pL = c.R_ps[4]
    for t in range(NT):
        b = t % 2
        xs = X1[:, t, :]
        rstd, R_r = rms_rstd(k, c, xs, R_X1[t], scr, R_scr, D, "moe")
        k.op("dve", lambda e, b=b, xs=xs, rstd=rstd: e.scalar_tensor_tensor(
            out=hf32[b], in0=xs, scalar=rstd, in1=gft, op0=ALU.mult, op1=ALU.mult),
            reads=[R_X1[t], R_r, R_gft], writes=[R_hf32[b]])
        p2 = pT[b]
        k.op("pe", [(lambda e, i=i, b=b, p2=p2: e.transpose(out=p2[:, i * 128:(i + 1) * 128],
                                                            in_=hf32[b][:, i * 128:(i + 1) * 128], identity=c.identf))
                    for i in range(8)], reads=[R_hf32[b], c.R_const], writes=R_pT[b])
        k.op("act", lambda e, b=b, p2=p2: e.activation(out=hT32[b].rearrange("p a b -> p (a b)"), in_=p2, func=AF.Copy),
             reads=R_pT[b], writes=[R_hT32[b]])
        k.op("dve", lambda e, b=b, p2=p2, t=t: e.tensor_copy(
            out=hfT[:, :, t * 128:(t + 1) * 128], in_=p2.rearrange("p (a b) -> p a b", a=8)),
            reads=R_pT[b], writes=[R_hfT[t]])
        lg = pL[:, 0:36]
        k.op("pe", [(lambda e, i=i, b=b: e.matmul(lg, lhsT=hT32[b][:, i, :], rhs=wr32[:, i, :], start=(i == 0), stop=(i == 7)))
                    for i in range(8)], reads=[R_hT32[b], R_wr], writes=[R_pL])
        lgs = sm[:, 0:36]
        gmax, gsum, gp, pen = sm[:, 36:37], sm[:, 37:38], sm[:, 38:39], sm[:, 40:44]
        gex, goh = sm[:, 44:48], sm[:, 48:52]
        elm = sm[:, 52:84]
        m8 = sm[:, 84:92]
        dd, ee, w1, w2 = sm[:, 92:93], sm[:, 93:94], sm[:, 94:95], sm[:, 95:96]
        oh = sm[:, 96:128]
        ct = comb[:, t, :]
        RW = dict(reads=[R_sm], writes=[R_sm])
        k.op("dve", lambda e: e.tensor_tensor(out=lgs, in0=lg, in1=brt, op=ALU.add), reads=[R_pL, R_wr, R_sm], writes=[R_sm])
        k.op("dve", lambda e: e.reduce_max(out=gmax, in_=lgs[:, 0:4], axis=AX.X), **RW)
        k.op("dve", lambda e: e.tensor_scalar(out=gex, in0=lgs[:, 0:4], scalar1=gmax, scalar2=None, op0=ALU.subtract), **RW)
        k.op("act", lambda e: e.activation(out=gex, in_=gex, func=AF.Exp, accum_out=gsum), **RW)
        k.op("dve", lambda e: e.reciprocal(out=gp, in_=gsum), **RW)
        k.op("dve", lambda e: e.tensor_scalar(out=pen, in0=lgs[:, 0:4], scalar1=gmax, scalar2=-1e30, op0=ALU.is_lt, op1=ALU.mult), **RW)
        k.op("dve", lambda e: e.tensor_tensor(out=elm.rearrange("p (g x) -> p g x", g=4),
                                              in0=lgs[:, 4:36].rearrange("p (g x) -> p g x", g=4),
                                              in1=pen.unsqueeze(2).broadcast_to([128, 4, 8]), op=ALU.add), **RW)
        k.op("dve", lambda e: e.max(out=m8, in_=elm), **RW)
        k.op("dve", lambda e: e.tensor_tensor(out=dd, in0=m8[:, 1:2], in1=m8[:, 0:1], op=ALU.subtract), **RW)
        k.op("act", lambda e: e.activation(out=ee, in_=dd, func=AF.Exp), **RW)
        k.op("dve", lambda e: e.tensor_scalar(out=ee, in0=ee, scalar1=1.0, scalar2=None, op0=ALU.add), **RW)
        k.op("dve", lambda e: e.reciprocal(out=w1, in_=ee), **RW)
        k.op("dve", lambda e: e.tensor_scalar(out=w2, in0=w1, scalar1=-1.0, scalar2=1.0, op0=ALU.mult, op1=ALU.add), **RW)
        k.op("dve", lambda e: e.tensor_tensor(out=w1, in0=w1, in1=gp, op=ALU.mult), **RW)
        k.op("dve", lambda e: e.tensor_tensor(out=w2, in0=w2, in1=gp, op=ALU.mult), **RW)
        k.op("dve", lambda e: e.tensor_scalar(out=oh, in0=elm, scalar1=m8[:, 0:1], scalar2=w1, op0=ALU.is_equal, op1=ALU.mult), **RW)
        k.op("dve", lambda e, ct=ct: e.tensor_scalar(out=ct, in0=elm, scalar1=m8[:, 1:2], scalar2=w2, op0=ALU.is_equal, op1=ALU.mult),
             reads=[R_sm], writes=[R_comb])
        k.op("dve", lambda e, ct=ct: e.tensor_tensor(out=ct, in0=ct, in1=oh, op=ALU.add), reads=[R_sm, R_comb], writes=[R_comb])

    if c.n_exp == 0:
        ar.release(m0)
        return
    NWB = 2
    wg = [ar.al([128, 8, DFF], BF16) for _ in range(NWB)]
    wu = [ar.al([128, 8, DFF], BF16) for _ in range(NWB)]
    wd = [ar.al([128, 4, D], BF16) for _ in range(NWB)]
    R_wg = [k.res("wg%d" % i) for i in range(NWB)]
    R_wu = [k.res("wu%d" % i) for i in range(NWB)]
    R_wd = [k.res("wd%d" % i) for i in range(NWB)]
    hid = [ar.al([128, 4, 512], BF16) for _ in range(2)]
    R_hid = [k.res("hid%d" % i) for i in range(2)]
    sg = [ar.al([128, 512], F32) for _ in range(2)]
    R_sg = [k.res("sg%d" % i) for i in range(2)]
    n_exp = c.n_exp

    def load_w(e):
        b = e % NWB
        k.dma("pool", wg[b], Dr["w_gate"][e].rearrange("(kt p) n -> p kt n", p=128), writes=[R_wg[b]])
        k.dma("pool", wu[b], Dr["w_up"][e].rearrange("(kt p) n -> p kt n", p=128), writes=[R_wu[b]])
        k.dma("pool", wd[b], Dr["w_down"][e].rearrange("(kt p) n -> p kt n", p=128), writes=[R_wd[b]])

    units = [(e, g) for e in range(n_exp) for g in range(NT // 4)]
    pgu = [(c.psum[0], c.psum[1]), (c.psum[2], c.psum[3])]
    R_pgu = [(c.R_ps[0], c.R_ps[1]), (c.R_ps[2], c.R_ps[3])]
    pdn = [c.psum[4], c.psum[5], c.psum[6], c.psum[7]]
    R_pdn = [c.R_ps[4], c.R_ps[5], c.R_ps[6], c.R_ps[7]]
    st = dict(gu=0, dn=0)

    def gate_up(u):
        e, g = units[u]
        b = e % NWB
        hb = u % 2
        tok = slice(g * 512, (g + 1) * 512)
        for ff in range(4):
            pb = st["gu"] % 2
            st["gu"] += 1
            pg, pu = pgu[pb]
            k.op("pe", [(lambda en, i=i, pg=pg, b=b, ff=ff: en.matmul(pg, lhsT=wg[b][:, i, ff * 128:(ff + 1) * 128], rhs=hfT[:, i, tok],
                                                                      start=(i == 0), stop=(i == 7))) for i in range(8)],
                 reads=[R_wg[b]] + R_hfT[4 * g:4 * g + 4], writes=[R_pgu[pb][0]])
            k.op("pe", [(lambda en, i=i, pu=pu, b=b, ff=ff: en.matmul(pu, lhsT=wu[b][:, i, ff * 128:(ff + 1) * 128], rhs=hfT[:, i, tok],
                                                                      start=(i == 0), stop=(i == 7))) for i in range(8)],
                 reads=[R_wu[b]] + R_hfT[4 * g:4 * g + 4], writes=[R_pgu[pb][1]])
            k.op("act", lambda en, pg=pg, pb=pb: en.activation(out=sg[pb], in_=pg, func=AF.Silu),
                 reads=[R_pgu[pb][0]], writes=[R_sg[pb]])
            k.op("dve", lambda en, pu=pu, pb=pb, hb=hb, ff=ff: en.tensor_tensor(out=hid[hb][:, ff, :], in0=pu, in1=sg[pb], op=ALU.mult),
                 reads=[R_pgu[pb][1], R_sg[pb]], writes=[R_hid[hb]])

    def down(u):
        e, g = units[u]
        b = e % NWB
        hb = u % 2
        for tt in range(4):
            t = 4 * g + tt
            for hf in range(2):
                pb = st["dn"] % 4
                st["dn"] += 1
                po = pdn[pb]
                k.op("pe", [(lambda en, i=i, po=po, b=b, hb=hb, tt=tt, hf=hf: en.matmul(
                    po, lhsT=hid[hb][:, i, tt * 128:(tt + 1) * 128], rhs=wd[b][:, i, hf * 512:(hf + 1) * 512],
                    start=(i == 0), stop=(i == 3))) for i in range(4)],
                    reads=[R_hid[hb], R_wd[b]], writes=[R_pdn[pb]])
                xs = X1[:, t, hf * 512:(hf + 1) * 512]
                k.op("dve", lambda en, po=po, xs=xs, t=t, e=e: en.scalar_tensor_tensor(
                    out=xs, in0=po, scalar=comb[:, t, e:e + 1], in1=xs, op0=ALU.mult, op1=ALU.add),
                    reads=[R_pdn[pb], R_comb, R_X1[t]], writes=[R_X1[t]])

    load_w(0)
    for u in range(len(units)):
        e, g = units[u]
        gate_up(u)
        if u >= 1:
            down(u - 1)
        if g == 0 and e + 1 < n_exp:
            load_w(e + 1)
    down(len(units) - 1)
    ar.release(m0)


def phase_final(k, c, ar, X1, R_X1):
    Dr = c.D
    m0 = ar.mark()
    gft = ar.al([128, D], F32)
    R_g = k.res("gfin")
    scr = ar.al([128, D], F32)
    R_scr = k.res("fin_scr")
    ob = [ar.al([128, D], F32) for _ in range(2)]
    R_ob = [k.res("ob%d" % i) for i in range(2)]
    k.dma("sp", gft, Dr["g_fin"].partition_broadcast(128), writes=[R_g])
    for t in range(NT):
        b = t % 2
        xs = X1[:, t, :]
        rstd, R_r = rms_rstd(k, c, xs, R_X1[t], scr, R_scr, D, "fin")
        k.op("dve", lambda e, b=b, xs=xs, rstd=rstd: e.scalar_tensor_tensor(
            out=ob[b], in0=xs, scalar=rstd, in1=gft, op0=ALU.mult, op1=ALU.mult),
            reads=[R_X1[t], R_r, R_g], writes=[R_ob[b]])
        k.dma("sp", Dr["y"][t * 128:(t + 1) * 128, :], ob[b], reads=[R_ob[b]])
    for b in range(2):
        for sk, v in list(R_ob[b].rs.items()):
            if sk.startswith("d_"):
                k.rec["sp"].append(("w", sk, v))
    ar.release(m0)


Q0, KV0, GT0, HQ0, HF0, HI0, HG0, MG0 = 0, 512, 1280, 1304, 1816, 2328, 2840, 3352


def _partner(d):
    return d + 8 if d < 8 else (d - 8 if d < 16 else d)


def _rope_tables(pos):
    pos = np.asarray(pos, dtype=np.float32)
    inv = (np.float32(500000.0) ** (-np.arange(8, dtype=np.float32) / np.float32(8))).astype(np.float32)
    ang = (pos[None, :] * inv[:, None]).astype(np.float32)
    cs, sn = np.cos(ang).astype(np.float32), np.sin(ang).astype(np.float32)
    C = np.ones((64, len(pos)), np.float32)
    S = np.zeros((64, len(pos)), np.float32)
    C[0:8], C[8:16] = cs, cs
    S[0:8], S[8:16] = -sn, sn
    return C, S


def attn_input_specs():
    return [
        ("g_attn", (D,), F32), ("g_hg4", (512,), F32),
        ("w1f", (D, 1280), F32), ("w1t", (D, 768), F32),
        ("w2f", (D, 2048), F32), ("w2t", (D, 1536), F32),
        ("wck", (2048, 64), F32), ("wckp", (2048, 64), F32), ("wcv", (2048, 64), F32),
        ("posT", (128, 32), F32), ("lbl", (2, 512), F32),
        ("w_mg", (D, 2048), F32), ("w_brn", (512, D), F32), ("w_brh", (512, D), F32), ("w_out", (D, D), F32),
        ("CK", (128, SEQ), F32), ("SK", (128, SEQ), F32), ("CKc", (128, 512), F32), ("SKc", (128, 512), F32),
        ("CQ", (128, NTOK), F32), ("SQ", (128, NTOK), F32),
        ("ovl", (128, 4, 128), F32),
        ("CB", (128, NT, 128), F32), ("CM", (128, 4, 128), F32), ("WMT", (128, 8, 128), F32),
        ("VAL", (128, NT, 128), F32), ("ADDC", (128, NT, 128), F32),
        ("tri", (128, 128), F32), ("I4", (128, 512), F32), ("onehot", (128, 4), F32),
    ]


_TAB_CACHE = {}


def _const_tables(cp):
    if cp in _TAB_CACHE:
        return _TAB_CACHE[cp]
    m = {}
    C, S = _rope_tables(np.arange(SEQ))
    m["CK"], m["SK"] = np.concatenate([C, C], 0), np.concatenate([S, S], 0)
    C, S = _rope_tables(np.maximum(16 * (np.arange(512) - 1), 0))
    m["CKc"], m["SKc"] = np.concatenate([C, C], 0), np.concatenate([S, S], 0)
    tpos = (128 * (4 * np.arange(NT)[:, None] + cp) + np.arange(128)[None, :])
    C, S = _rope_tables(tpos.reshape(-1))
    m["CQ"] = np.concatenate([C, C], 0) * np.float32(0.125)
    m["SQ"] = np.concatenate([S, S], 0) * np.float32(0.125)
    n = np.arange(512) - 1
    cs, ce = 16 * n, 16 * n + 31
    ss = 64 * np.arange(128)
    ov = ((cs[:, None] < ss[None, :] + 64) & (ce[:, None] >= ss[None, :]) & (n[:, None] >= 0)).astype(np.float32)
    m["ovl"] = np.ascontiguousarray(ov.reshape(4, 128, 128).transpose(1, 0, 2))
    mt = (np.arange(NT) // 4)
    mm = mt[:, None] * 128 + np.arange(128)[None, :]
    nn = mm - 1
    okc = (nn[:, None, :] >= 0) & (16 * nn[:, None, :] + 31 <= tpos[:, :, None])
    m["CB"] = np.ascontiguousarray(np.where(okc, 0.0, NEG).astype(np.float32).transpose(1, 0, 2))
    blk = np.arange(128)
    jq = tpos // 64
    force = (blk[None, None, :] == jq[:, :, None]) | (blk[None, None, :] == 0)
    valid = (64 * blk[None, None, :] <= tpos[:, :, None])
    m["VAL"] = np.ascontiguousarray((valid & ~force).astype(np.float32).transpose(1, 0, 2))
    m["ADDC"] = np.ascontiguousarray(np.where(force, 1e4, np.where(valid, 0.0, -1.0)).astype(np.float32).transpose(1, 0, 2))
    t = np.arange(128)[:, None]
    p = np.arange(128)[None, :]
    caus = np.where(p <= t, 0.0, NEG).astype(np.float32)
    anti = np.where(p > t, 0.0, NEG).astype(np.float32)
    cm = np.zeros((128, 4, 128), np.float32)
    for r in range(4):
        cm[:, r, :] = 0.0 if r < cp else (caus if r == cp else NEG)
    m["CM"] = cm
    wm = np.zeros((128, 8, 128), np.float32)
    for r in range(8):
        dk = cp + 4 - r
        wm[:, r, :] = NEG if (dk < 0 or dk > 4) else (caus if dk == 0 else (anti if dk == 4 else 0.0))
    m["WMT"] = wm
    m["tri"] = (np.arange(128)[:, None] <= np.arange(128)[None, :]).astype(np.float32)
    m["I4"] = np.tile(np.eye(128, dtype=np.float32), (1, 4))
    oh = np.zeros((128, 4), np.float32)
    oh[:, cp] = 1.0
    m["onehot"] = oh
    _TAB_CACHE[cp] = m
    return m


def attn_host_inputs(inp, b, cp):
    m = dict(_const_tables(cp))
    w = inp["w_in"][0]
    pp = np.array([g * 64 + _partner(d) for g in range(2) for d in range(64)])
    kv = lambda s: KV0 + s * 128 + np.arange(128)
    hfc = HF0 + np.arange(512)
    m["w1f"] = np.ascontiguousarray(np.concatenate(
        [w[:, kv(0)], w[:, kv(1)], w[:, kv(2)], w[:, kv(2)[pp]], w[:, kv(4)], w[:, kv(4)[pp]], w[:, hfc]], axis=1))
    m["w1t"] = np.ascontiguousarray(np.concatenate([w[:, kv(3)], w[:, kv(5)], w[:, HI0:HI0 + 512]], axis=1))
    qcols, qpcols = [], []
    for a in range(4):
        for h in (a, 4 + a):
            qcols += [Q0 + h * 64 + d for d in range(64)]
            qpcols += [Q0 + h * 64 + _partner(d) for d in range(64)]
    m["w2f"] = np.ascontiguousarray(np.concatenate(
        [w[:, qcols], w[:, qpcols], w[:, HQ0:HQ0 + 512], w[:, hfc]], axis=1))
    gpad = np.concatenate([w[:, GT0:GT0 + 24], w[:, GT0:GT0 + 24][:, :0].repeat(1, 1)], axis=1)
    w2t = np.zeros((D, 1536), np.float32)
    w2t[:, 0:512] = w[:, HI0:HI0 + 512]
    w2t[:, 512:1024] = w[:, HG0:HG0 + 512]
    w2t[:, 1024:1048] = w[:, GT0:GT0 + 24]
    m["w2t"] = w2t
    pc = np.array([_partner(d) for d in range(64)])
    m["wck"] = np.ascontiguousarray(inp["w_cmp_k"][0])
    m["wckp"] = np.ascontiguousarray(inp["w_cmp_k"][0][:, pc])
    m["wcv"] = np.ascontiguousarray(inp["w_cmp_v"][0])
    pT = np.ascontiguousarray(inp["cmp_pos"][0].T)
    m["posT"] = np.concatenate([pT, pT], 0)
    m["lbl"] = np.ascontiguousarray(inp["hg_lb_logits"])
    m["g_attn"] = np.ascontiguousarray(inp["attn_norm"][0])
    m["g_hg4"] = np.ascontiguousarray(np.tile(inp["hg_norm"][0], 4))
    m["w_mg"] = np.ascontiguousarray(w[:, MG0:MG0 + 2048])
    m["w_brn"] = np.ascontiguousarray(inp["w_br_nsa"][0])
    m["w_brh"] = np.ascontiguousarray(inp["w_br_hg"][0])
    m["w_out"] = np.ascontiguousarray(inp["w_out"][0])
    return m


def norm_transpose_group(k, c, W, src_dram, row0, hT, R_hT):
    for tt in range(4):
        b = tt % 2
        k.dma("sp", W.xt[b], src_dram[row0 + tt * 128: row0 + (tt + 1) * 128, :], writes=[W.R_xt[b]])
        rstd, R_r = rms_rstd(k, c, W.xt[b], W.R_xt[b], W.scr, W.R_scr, D, "an")
        k.op("dve", lambda e, b=b, rstd=rstd: e.scalar_tensor_tensor(
            out=W.hb[b], in0=W.xt[b], scalar=rstd, in1=W.gA, op0=ALU.mult, op1=ALU.mult),
            reads=[W.R_xt[b], R_r, W.R_gA], writes=[W.R_hb[b]])
        pb = c.psum[b].bitcast(BF16)
        k.op("pe", [(lambda e, i=i, b=b, pb=pb: e.transpose(out=pb[:, i * 128:(i + 1) * 128],
                                                            in_=W.hb[b][:, i * 128:(i + 1) * 128], identity=c.identb))
                    for i in range(8)], reads=[W.R_hb[b], c.R_const], writes=[c.R_ps[b]])
        k.op("act", lambda e, pb=pb, tt=tt: e.activation(out=hT[:, :, tt * 128:(tt + 1) * 128],
                                                       in_=pb.rearrange("p (a b) -> p a b", a=8), func=AF.Copy),
             reads=[c.R_ps[b]], writes=[R_hT])


def f_front(k, c, W, fl_ps, R_fl, hd):
    u, a, bq, lk, L, RF = W.sets[hd % 2]
    k.op("act", lambda e: e.activation(out=u, in_=fl_ps, func=AF.Exp, scale=-1.0), reads=[R_fl], writes=[RF])
    k.op("act", lambda e: e.activation(out=a, in_=u, func=AF.Ln, scale=c.lbv[:, hd:hd + 1], bias=c.one_col[:, 0:1]),
         reads=[RF, c.R_const], writes=[RF])
    k.op("act", lambda e: e.activation(out=bq, in_=u, func=AF.Ln, bias=c.one_col[:, 0:1]), reads=[RF, c.R_const], writes=[RF])
    k.op("dve", lambda e: e.scalar_tensor_tensor(out=lk, in0=fl_ps, scalar=-1.0, in1=bq, op0=ALU.mult, op1=ALU.subtract),
         reads=[R_fl, RF], writes=[RF])
    for tt in range(4):
        sl = slice(tt * 128, (tt + 1) * 128)
        k.op("dve", lambda e, sl=sl: e.tensor_tensor_scan(out=L[:, sl], data0=a[:, sl], data1=bq[:, sl], initial=0.0,
                                                          op0=ALU.add, op1=ALU.subtract), reads=[RF], writes=[RF])
    k.op("pool", lambda e: e.tensor_tensor(out=lk, in0=lk, in1=L, op=ALU.subtract), reads=[RF], writes=[RF])


def f_back(k, c, W, hd, H=None):
    u, a, bq, lk, L, RF = W.sets[hd % 2]
    W_, W = W, (H if H is not None else W)
    Lr = L.rearrange("p (t x) -> p t x", t=4)
    rcol, ecol = Lr[:, :, 63], Lr[:, :, 127]
    k.op("dve", lambda e: e.tensor_scalar(out=W.rb[:, hd, :], in0=rcol, scalar1=c.l1mlb[:, hd:hd + 1], scalar2=None, op0=ALU.add),
         reads=[RF, c.R_const], writes=[W.R_cols])
    k.op("dve", lambda e: e.tensor_scalar(out=W.negr[:, hd, :], in0=rcol, scalar1=-1.0, scalar2=None, op0=ALU.mult),
         reads=[RF], writes=[W.R_cols])
    k.op("dve", lambda e: e.tensor_tensor(out=W.dl[:, hd, :], in0=ecol, in1=rcol, op=ALU.subtract), reads=[RF], writes=[W.R_cols])
    k.op("act", lambda e: e.activation(out=W.c1[:, hd, :], in_=ecol, func=AF.Exp), reads=[RF], writes=[W.R_cols])
    k.op("act", lambda e: e.activation(out=W.c2[:, hd, :], in_=W.dl[:, hd, :], func=AF.Exp), reads=[W.R_cols], writes=[W.R_cols])
    k.op("act", lambda e: e.activation(out=W.er[:, hd, :], in_=rcol, func=AF.Exp), reads=[RF], writes=[W.R_cols])
    for tt in range(4):
        sl = slice(tt * 128, (tt + 1) * 128)
        k.op("act", lambda e, sl=sl, tt=tt: e.activation(out=W.kT[:, hd, sl], in_=lk[:, sl], func=AF.Exp, bias=W.rb[:, hd, tt:tt + 1]),
             reads=[RF, W.R_cols], writes=[W.R_kT])


def setup_lb(k, c, ar):
    Dr = c.D
    c.lbv = ar.al([128, 4], F32)
    c.l1mlb = ar.al([128, 4], F32)
    c.one_col = ar.al([128, 1], F32)
    c.ones128 = ar.al([128, 128], F32)
    l0 = ar.al([128, 4], F32)
    l1 = ar.al([128, 4], F32)
    R = c.R_const
    k.dma("sp", l0, Dr["lbl"][0].rearrange("(h p) -> p h", p=128), writes=[R], allow_slow_non_contiguous=True)
    k.dma("sp", l1, Dr["lbl"][1].rearrange("(h p) -> p h", p=128), writes=[R], allow_slow_non_contiguous=True)
    k.op("dve", lambda e: e.memset(c.one_col, 1.0), writes=[R])
    k.op("dve", lambda e: e.memset(c.ones128, 1.0), writes=[R])
    k.op("dve", lambda e: e.tensor_tensor(out=l1, in0=l1, in1=l0, op=ALU.subtract), reads=[R], writes=[R])
    k.op("act", lambda e: e.activation(out=l0, in_=l1, func=AF.Exp), reads=[R], writes=[R])
    k.op("dve", lambda e: e.tensor_scalar(out=l0, in0=l0, scalar1=1.0, scalar2=None, op0=ALU.add), reads=[R], writes=[R])
    k.op("dve", lambda e: e.reciprocal(out=c.lbv, in_=l0), reads=[R], writes=[R])
    k.op("act", lambda e: e.activation(out=l0, in_=l0, func=AF.Ln), reads=[R], writes=[R])
    k.op("dve", lambda e: e.tensor_tensor(out=c.l1mlb, in0=l1, in1=l0, op=ALU.subtract), reads=[R], writes=[R])


class WS:
    pass


def alloc_hg_ws(k, ar, W, nsets=1):
    W.sets = []
    for si in range(nsets):
        blk = ar.al([128, 5, 512], F32)
        W.sets.append(tuple(blk[:, i, :] for i in range(5)) + (k.res("fchain%d" % si),))
        if si == 0:
            W.ab = blk[:, 1:3, :].rearrange("p a b -> p (a b)")
    if nsets == 1:
        W.sets.append(W.sets[0])
    W.u, W.a, W.bq, W.lk, W.L, W.R_f = W.sets[0]
    alloc_hslot(k, ar, W, "0")


def alloc_hslot(k, ar, H, tag):
    H.rb, H.negr, H.dl, H.c1, H.c2, H.er = [ar.al([128, 4, 4], F32) for _ in range(6)]
    H.R_cols = k.res("fcols" + tag)
    H.kT = ar.al([128, 4, 512], BF16)
    H.R_kT = k.res("kT" + tag)


def alloc_x_ws(k, c, ar, W, region, scr=None, R_scr=None):
    if region is not None:
        W.xt = [region[:, 0, :].bitcast(F32), region[:, 1, :].bitcast(F32)]
        W.hb = [region[:, 2, 0:1024], region[:, 2, 1024:2048]]
        W.scr = region[:, 3, :].bitcast(F32)
        W.R_scr = k.res("xscr")
    else:
        W.xt = [ar.al([128, D], F32) for _ in range(2)]
        W.hb = [ar.al([128, D], BF16) for _ in range(2)]
        W.scr, W.R_scr = scr, R_scr
    W.R_xt = [k.res("xt0"), k.res("xt1")]
    W.R_hb = [k.res("hb0"), k.res("hb1")]
    W.gA = ar.al([128, D], F32)
    W.R_gA = k.res("gA")
    k.dma("sp", W.gA, c.D["g_attn"].partition_broadcast(128), writes=[W.R_gA])


def phase_p1(k, c, ar, St):
    Dr = c.D
    m0 = ar.mark()
    W = WS()
    alloc_x_ws(k, c, ar, W, c.oT_hg)
    w1f, w1t = c.R32[:, :, 0:1280], c.R32[:, :, 1280:2048]
    R_w1 = k.res("w1")
    k.dma("pool", w1f, Dr["w1f"].rearrange("(kt p) n -> p kt n", p=128), writes=[R_w1])
    k.dma("pool", w1t, Dr["w1t"].rearrange("(kt p) n -> p kt n", p=128), writes=[R_w1])
    hT = ar.al([128, 8, 512], BF16)
    R_hT = k.res("hT")
    CKg, SKg = ar.al([128, 512], F32), ar.al([128, 512], F32)
    R_rt = k.res("ropetab")
    alloc_hg_ws(k, ar, W)
    t1, t2, R_t12 = W.u, W.a, W.R_f
    vtok = ar.al([128, 4, 512], BF16)
    R_vtok = k.res("vtok")
    ktok = ar.al([128, 4, 128], BF16)
    R_ktok = k.res("ktok")
    Sst = ar.al([128, 4, 128], F32)
    snapacc = ar.al([128, 4, 128], F32)
    R_S, R_snapacc = k.res("S"), k.res("snapacc")
    WC = [ar.al([128, 32, 64], BF16) for _ in range(3)]
    R_WC = k.res("WC")
    posT = ar.al([128, 32], BF16)
    cb = ar.al([128, 4], F32)
    xin = [[ar.al([128, 528], BF16) for _ in range(2)] for _ in range(2)]
    R_xin = [[k.res("xin%d%d" % (a, b)) for b in range(2)] for a in range(2)]
    CKc, SKc = ar.al([128, 32], F32), ar.al([128, 32], F32)
    R_ckc = k.res("ckc")
    VCf = ar.al([128, 512], F32)
    R_VCf = k.res("VCf")
    ctmp = ar.al([128, 4, 32], F32)
    R_ctmp = k.res("ctmp")
    for xi, nm in enumerate(("wck", "wckp", "wcv")):
        for g in range(2):
            k.dma("pool", WC[xi][64 * g:64 * g + 64], Dr[nm].rearrange("(l d) e -> d l e", d=64), writes=[R_WC])
    k.dma("pool", posT, Dr["posT"], writes=[R_WC])
    k.op("dve", lambda e: e.memset(Sst, 0.0), writes=[R_S])
    k.op("dve", lambda e: e.memset(St.VsA[:, :, :, 64:65], 1.0), writes=[St.R_VsA])
    k.op("dve", lambda e: e.memset(St.VwA[:, :, :, 64:65], 1.0), writes=[St.R_VwA])
    for a in range(2):
        k.op("dve", lambda e, a=a: e.memset(xin[a][0][:, 0:16], 0.0), writes=[R_xin[a][0]])
    p6 = c.psum[6]
    fns = []
    for xi in range(3):
        for g in range(2):
            for l in range(32):
                fns.append(lambda e, xi=xi, g=g, l=l: e.matmul(p6[64 * g:64 * g + 64, xi:xi + 1], lhsT=WC[xi][64 * g:64 * g + 64, l, :],
                                                               rhs=posT[64 * g:64 * g + 64, l:l + 1], start=(l == 0), stop=(l == 31)))
    k.op("pe", fns, reads=[R_WC], writes=[c.R_ps[6]])
    k.op("dve", lambda e: e.tensor_copy(out=cb[:, 0:3], in_=p6[:, 0:3]), reads=[c.R_ps[6]], writes=[R_WC])

    NG = c.n_groups
    H2 = WS()
    alloc_hslot(k, ar, H2, "1")
    Hs = [W, H2]
    vtok2 = ar.al([128, 4, 512], BF16)
    vtoks, R_vtoks = [vtok, vtok2], [R_vtok, k.res("vtokB")]
    p6b = c.psum[6].bitcast(BF16)

    def fm(ft, bank):
        k.op("pe", [(lambda e, i=i: e.matmul(c.psum[bank], lhsT=w1f[:, i, ft * 128:(ft + 1) * 128], rhs=hT[:, i, :],
                                             start=(i == 0), stop=(i == 7))) for i in range(8)],
             reads=[R_w1, R_hT], writes=[c.R_ps[bank]])

    def A_x(G):
        norm_transpose_group(k, c, W, Dr["xb"], G * 512, hT, R_hT)
        k.dma("sp", CKg, Dr["CK"][:, G * 512:(G + 1) * 512], writes=[R_rt])
        k.dma("sp", SKg, Dr["SK"][:, G * 512:(G + 1) * 512], writes=[R_rt])

    def A_kv(G):
        xb_ = G % 2
        for a in range(2):
            fm(a, 2 + a)
            k.op("act", lambda e, a=a: e.activation(out=xin[a][xb_][:, 16:528], in_=c.psum[2 + a], func=AF.Copy),
                 reads=[c.R_ps[2 + a]], writes=[R_xin[a][xb_]])
            k.op("pool", lambda e, a=a: e.tensor_copy(out=xin[a][1 - xb_][:, 0:16], in_=xin[a][xb_][:, 512:528]),
                 reads=[R_xin[a][xb_]], writes=[R_xin[a][1 - xb_]])
        for which, dst, R_dst in ((0, St.KTs, St.R_KTs), (1, St.KTw, St.R_KTw)):
            fm(2 + 2 * which, 2)
            fm(3 + 2 * which, 3)
            k.op("dve", lambda e: e.tensor_tensor(out=t1, in0=c.psum[2], in1=CKg, op=ALU.mult), reads=[c.R_ps[2], R_rt], writes=[R_t12])
            k.op("dve", lambda e: e.tensor_tensor(out=t2, in0=c.psum[3], in1=SKg, op=ALU.mult), reads=[c.R_ps[3], R_rt, R_t12], writes=[R_t12])
            k.op("pool", lambda e, dst=dst: e.tensor_tensor(out=dst[:, G * 512:(G + 1) * 512], in0=t1, in1=t2, op=ALU.add),
                 reads=[R_t12], writes=[R_dst])

    def A_tok(G):
        vt, R_vt = vtoks[G % 2], R_vtoks[G % 2]
        for tt in range(4):
            tile_ = 4 * G + tt
            k.op("pe", [(lambda e, i=i, tt=tt: e.matmul(c.psum[4][:, 0:256], lhsT=hT[:, i, tt * 128:(tt + 1) * 128], rhs=w1t[:, i, 0:256],
                                                        start=(i == 0), stop=(i == 7))) for i in range(8)],
                 reads=[R_w1, R_hT], writes=[c.R_ps[4]])
            k.op("pe", [(lambda e, i=i, tt=tt: e.matmul(c.psum[5], lhsT=hT[:, i, tt * 128:(tt + 1) * 128], rhs=w1t[:, i, 256:768],
                                                        start=(i == 0), stop=(i == 7))) for i in range(8)],
                 reads=[R_w1, R_hT], writes=[c.R_ps[5]])
            k.op("act", lambda e, tile_=tile_: e.activation(out=St.VsA[:, tile_, :, 0:64],
                                                            in_=c.psum[4][:, 0:128].rearrange("p (g d) -> p g d", g=2), func=AF.Copy),
                 reads=[c.R_ps[4]], writes=[St.R_VsA])
            k.op("act", lambda e, tile_=tile_: e.activation(out=St.VwA[:, tile_, :, 0:64],
                                                            in_=c.psum[4][:, 128:256].rearrange("p (g d) -> p g d", g=2), func=AF.Copy),
                 reads=[c.R_ps[4]], writes=[St.R_VwA])
            k.op("dve", lambda e, tt=tt: e.tensor_copy(out=vt[:, tt, :], in_=c.psum[5]), reads=[c.R_ps[5]], writes=[R_vt])

    def A_conv(G):
        xb_ = G % 2
        fns = []
        for xi in range(3):
            src = xin[0][xb_] if xi < 2 else xin[1][xb_]
            for g in range(2):
                for l in range(32):
                    fns.append(lambda e, xi=xi, g=g, l=l, src=src: e.matmul(
                        p6[64 * g:64 * g + 64, 32 * xi:32 * xi + 32], lhsT=WC[xi][64 * g:64 * g + 64, l, :],
                        rhs=src[64 * g:64 * g + 64, l:l + 497:16], start=(l == 0), stop=(l == 31)))
        k.op("pe", fns, reads=[R_WC, R_xin[0][xb_], R_xin[1][xb_]], writes=[c.R_ps[6]])
        ms = slice(32 * G, 32 * G + 32)
        k.dma("sp", CKc, Dr["CKc"][:, ms], writes=[R_ckc])
        k.dma("sp", SKc, Dr["SKc"][:, ms], writes=[R_ckc])
        k.op("dve", lambda e: e.tensor_scalar(out=ctmp[:, 0, :], in0=p6[:, 0:32], scalar1=cb[:, 0:1], scalar2=None, op0=ALU.add),
             reads=[c.R_ps[6], R_WC], writes=[R_ctmp])
        k.op("dve", lambda e: e.tensor_scalar(out=ctmp[:, 1, :], in0=p6[:, 32:64], scalar1=cb[:, 1:2], scalar2=None, op0=ALU.add),
             reads=[c.R_ps[6], R_WC], writes=[R_ctmp])
        k.op("dve", lambda e: e.tensor_scalar(out=VCf[:, ms], in0=p6[:, 64:96], scalar1=cb[:, 2:3], scalar2=None, op0=ALU.add),
             reads=[c.R_ps[6], R_WC], writes=[R_VCf])
        k.op("pool", lambda e: e.tensor_tensor(out=ctmp[:, 0, :], in0=ctmp[:, 0, :], in1=CKc, op=ALU.mult),
             reads=[R_ctmp, R_ckc], writes=[R_ctmp])
        k.op("pool", lambda e: e.tensor_tensor(out=ctmp[:, 1, :], in0=ctmp[:, 1, :], in1=SKc, op=ALU.mult),
             reads=[R_ctmp, R_ckc], writes=[R_ctmp])
        k.op("pool", lambda e: e.tensor_tensor(out=St.KC[:, ms], in0=ctmp[:, 0, :], in1=ctmp[:, 1, :], op=ALU.add),
             reads=[R_ctmp], writes=[St.R_KC])

    def A_f(G):
        H = Hs[G % 2]
        for hd in range(4):
            bank = 2 + hd % 2
            fm(6 + hd, bank)
            f_front(k, c, W, c.psum[bank], c.R_ps[bank], hd)
            f_back(k, c, W, hd, H)

    def B_step(G, tt):
        H = Hs[G % 2]
        vt, R_vt = vtoks[G % 2], R_vtoks[G % 2]
        sl = slice(tt * 128, (tt + 1) * 128)
        k.op("pe", [(lambda e, hd=hd: e.transpose(out=p6b[:, hd * 128:(hd + 1) * 128], in_=H.kT[:, hd, sl], identity=c.identb))
                    for hd in range(4)], reads=[H.R_kT, c.R_const], writes=[c.R_ps[6]])
        k.op("act", lambda e: e.activation(out=ktok, in_=p6b[:, 0:512].rearrange("p (h x) -> p h x", h=4), func=AF.Copy),
             reads=[c.R_ps[6]], writes=[R_ktok])
        k.op("pe", [(lambda e, hd=hd: e.matmul(c.psum[7][:, hd * 128:(hd + 1) * 128], lhsT=ktok[:, hd, :],
                                               rhs=vt[:, tt, hd * 128:(hd + 1) * 128], start=True, stop=True))
                    for hd in range(4)], reads=[R_ktok, R_vt], writes=[c.R_ps[7]])
        Sf, Af = Sst.rearrange("p h x -> p (h x)"), snapacc.rearrange("p h x -> p (h x)")
        if tt == 0:
            k.op("dve", lambda e: e.tensor_scalar(out=Af, in0=Sf, scalar1=c.onehot[:, 0:1], scalar2=None, op0=ALU.mult),
                 reads=[R_S, c.R_const], writes=[R_snapacc])
        else:
            k.op("dve", lambda e: e.scalar_tensor_tensor(out=Af, in0=Sf, scalar=c.onehot[:, tt:tt + 1], in1=Af,
                                                         op0=ALU.mult, op1=ALU.add),
                 reads=[R_S, c.R_const, R_snapacc], writes=[R_snapacc])
        for hd in range(4):
            k.op("dve", lambda e, hd=hd: e.tensor_scalar(out=Sst[:, hd, :], in0=Sst[:, hd, :], scalar1=H.c1[:, hd, tt:tt + 1],
                                                         scalar2=None, op0=ALU.mult),
                 reads=[R_S, H.R_cols], writes=[R_S])
            k.op("dve", lambda e, hd=hd: e.scalar_tensor_tensor(
                out=Sst[:, hd, :], in0=c.psum[7][:, hd * 128:(hd + 1) * 128], scalar=H.c2[:, hd, tt:tt + 1], in1=Sst[:, hd, :],
                op0=ALU.mult, op1=ALU.add), reads=[c.R_ps[7], R_S, H.R_cols], writes=[R_S])
        if tt == 3:
            k.op("act", lambda e: e.activation(out=St.SNAP[:, G, :, :], in_=snapacc, func=AF.Copy), reads=[R_snapacc], writes=[St.R_SNAP])

    for G in range(NG + 1):
        parts = [A_x, A_kv, A_tok, A_conv, A_f]
        for pi, part in enumerate(parts):
            if G < NG:
                part(G)
            if G >= 1 and pi < 4:
                B_step(G - 1, pi)
    k.op("dve", lambda e: e.memset(St.VCA[:, :, :, 64:65], 1.0), writes=[St.R_VCA])
    for g in range(2):
        k.dma("pool", St.VCA[:, :, g, 65:193], Dr["ovl"], writes=[St.R_VCA])
    pw = c.psum[6]
    k.op("pe", [(lambda e, mt=mt: e.transpose(out=pw[:, mt * 128:(mt + 1) * 128], in_=VCf[:, mt * 128:(mt + 1) * 128], identity=c.identf))
                for mt in range(4)], reads=[R_VCf, c.R_const], writes=[c.R_ps[6]])
    for mt in range(4):
        k.op("act", lambda e, mt=mt: e.activation(out=St.VCA[:, mt, :, 0:64],
                                                  in_=pw[:, mt * 128:(mt + 1) * 128].rearrange("p (g d) -> p g d", g=2), func=AF.Copy),
             reads=[c.R_ps[6]], writes=[St.R_VCA])
    k.op("dve", lambda e: e.memset(St.VCA[0:1, 0, :, :], 0.0), writes=[St.R_VCA])
    ar.release(m0)


def phase_p2pre(k, c, ar, St):
    Dr = c.D
    m0 = ar.mark()
    W = WS()
    alloc_hg_ws(k, ar, W, nsets=2)
    alloc_x_ws(k, c, ar, W, None, scr=W.ab, R_scr=W.R_f)
    hT = ar.al([128, 8, 512], BF16)
    R_hT = k.res("hT2")
    wch = [c.R32f[:, 8192 + b * 4096: 8192 + (b + 1) * 4096].rearrange("p (a b) -> p a b", a=8) for b in range(2)]
    R_wch = [k.res("wch%d" % i) for i in range(2)]
    wgt = ar.al([128, 8, 32], BF16)
    R_wgt = k.res("wgt")
    wst = dict(n=0)
    CQg, SQg = ar.al([128, 512], F32), ar.al([128, 512], F32)
    R_rt = k.res("ropetabq")
    t1, t2, R_t12 = W.u, W.a, W.R_f
    qT = ar.al([128, 4, 512], BF16)
    R_qT = k.res("qTh")
    e1, R_e1 = W.u, W.R_f
    vtok = ar.al([128, 512], BF16)
    R_vtok = k.res("vtok2")
    sgt = ar.al([128, 512], F32)
    R_sgt = k.res("sgt")
    AT = ar.al([128, 4, 128], BF16)
    R_AT = k.res("AT")
    Sp = ar.al([128, 4, 128], BF16)
    R_Sp = k.res("Sp")
    gnt = ar.al([128, 512], F32)
    R_gnt = k.res("gnt")
    o1, o2, R_o = W.bq, W.a, W.R_f
    yb = ar.al([128, 512], BF16)
    R_yb = k.res("yb")
    hs = ar.al([128, 16], F32)
    R_hs = k.res("hs")
    k.dma("pool", wgt, Dr["w2t"][:, 1024:1056].rearrange("(kt p) n -> p kt n", p=128), writes=[R_wgt])
    k.dma("sp", gnt, Dr["g_hg4"].partition_broadcast(128), writes=[R_gnt])

    def wload(src, c0, n=512):
        b = wst["n"] % 2
        wst["n"] += 1
        k.dma("pool", wch[b][:, :, 0:n], src[:, c0:c0 + n].rearrange("(kt p) n -> p kt n", p=128), writes=[R_wch[b]])
        return wch[b], R_wch[b]

    for go in range(NT // 4):
        tok = slice(go * 512, (go + 1) * 512)
        norm_transpose_group(k, c, W, Dr["xo"], go * 512, hT, R_hT)
        k.dma("sp", CQg, Dr["CQ"][:, tok], writes=[R_rt])
        k.dma("sp", SQg, Dr["SQ"][:, tok], writes=[R_rt])

        def fm(wt, R_wt, j, bank):
            k.op("pe", [(lambda e, i=i: e.matmul(c.psum[bank], lhsT=wt[:, i, j * 128:(j + 1) * 128], rhs=hT[:, i, :],
                                                 start=(i == 0), stop=(i == 7))) for i in range(8)],
                 reads=[R_wt, R_hT], writes=[c.R_ps[bank]])
        wq, R_wq = wload(Dr["w2f"], 0)
        wqp, R_wqp = wload(Dr["w2f"], 512)
        for a in range(4):
            fm(wq, R_wq, a, 2)
            fm(wqp, R_wqp, a, 3)
            k.op("dve", lambda e: e.tensor_tensor(out=t1, in0=c.psum[2], in1=CQg, op=ALU.mult), reads=[c.R_ps[2], R_rt], writes=[R_t12])
            k.op("dve", lambda e: e.tensor_tensor(out=t2, in0=c.psum[3], in1=SQg, op=ALU.mult), reads=[c.R_ps[3], R_rt, R_t12], writes=[R_t12])
            k.op("pool", lambda e, a=a: e.tensor_tensor(out=c.QT[:, 4 * go:4 * go + 4, a, :], in0=t1.rearrange("p (i t) -> p i t", i=4),
                                                        in1=t2.rearrange("p (i t) -> p i t", i=4), op=ALU.add), reads=[R_t12], writes=[c.R_QT])
        whq, R_whq = wload(Dr["w2f"], 1024)
        whf, R_whf = wload(Dr["w2f"], 1536)
        def front(hd):
            bank = 2 + hd % 2
            fm(whf, R_whf, hd, bank)
            f_front(k, c, W, c.psum[bank], c.R_ps[bank], hd)

        def back(hd):
            f_back(k, c, W, hd)
            su, sa, sbq, slk, sL, sRF = W.sets[hd % 2]
            fm(whq, R_whq, hd, 6)
            for tt in range(4):
                sl = slice(tt * 128, (tt + 1) * 128)
                k.op("act", lambda e, sl=sl, tt=tt: e.activation(out=su[:, sl], in_=sL[:, sl], func=AF.Exp, bias=W.negr[:, hd, tt:tt + 1]),
                     reads=[sRF, W.R_cols], writes=[sRF])
            k.op("dve", lambda e: e.tensor_tensor(out=qT[:, hd, :], in0=c.psum[6], in1=su, op=ALU.mult),
                 reads=[c.R_ps[6], sRF], writes=[R_qT])
        front(0)
        front(1)
        back(0)
        front(2)
        back(1)
        front(3)
        back(2)
        back(3)
        whi, R_whi = wload(Dr["w2t"], 0)
        whg, R_whg = wload(Dr["w2t"], 512)
        for tt in range(4):
            i_own = 4 * go + tt
            sl = slice(tt * 128, (tt + 1) * 128)
            for (wt, R_wt, n, bank) in ((whi, R_whi, 512, 4), (whg, R_whg, 512, 5), (wgt, R_wgt, 32, 6)):
                k.op("pe", [(lambda e, i=i, wt=wt, n=n, bank=bank: e.matmul(c.psum[bank][:, 0:n], lhsT=hT[:, i, sl], rhs=wt[:, i, 0:n],
                                                                            start=(i == 0), stop=(i == 7))) for i in range(8)],
                     reads=[R_wt, R_hT], writes=[c.R_ps[bank]])
            k.op("dve", lambda e: e.tensor_copy(out=vtok, in_=c.psum[4]), reads=[c.R_ps[4]], writes=[R_vtok])
            k.op("act", lambda e: e.activation(out=sgt, in_=c.psum[5], func=AF.Silu), reads=[c.R_ps[5]], writes=[R_sgt])
            k.op("act", lambda e, i_own=i_own: e.activation(out=c.gsig[:, i_own, :], in_=c.psum[6][:, 0:24], func=AF.Sigmoid),
                 reads=[c.R_ps[6]], writes=[c.R_gsig])
            k.op("pe", [(lambda e, hd=hd: e.matmul(c.psum[7][:, hd * 128:(hd + 1) * 128], lhsT=W.kT[:, hd, sl], rhs=qT[:, hd, sl],
                                                   start=True, stop=True)) for hd in range(4)],
                 reads=[W.R_kT, R_qT], writes=[c.R_ps[7]])
            k.op("dve", lambda e: e.tensor_scalar(out=W.lk, in0=c.psum[7], scalar1=1e30, scalar2=-1e30, op0=ALU.min, op1=ALU.max),
                 reads=[c.R_ps[7], W.R_f], writes=[W.R_f])
            k.op("dve", lambda e: e.tensor_tensor(out=AT, in0=W.lk.rearrange("p (h x) -> p h x", h=4),
                                                  in1=c.tri.unsqueeze(1).broadcast_to([128, 4, 128]), op=ALU.mult),
                 reads=[W.R_f, c.R_const], writes=[R_AT])
            for hd in range(4):
                k.op("act", lambda e, hd=hd, tt=tt, i_own=i_own: e.activation(out=Sp[:, hd, :], in_=St.SNAP[:, i_own, hd, :], func=AF.Copy,
                                                                              scale=W.er[:, hd, tt:tt + 1]),
                     reads=[St.R_SNAP, W.R_cols], writes=[R_Sp])
            fns = []
            for hd in range(4):
                fns.append(lambda e, hd=hd, tt=tt: e.matmul(c.psum[4][:, hd * 128:(hd + 1) * 128], lhsT=AT[:, hd, :],
                                                            rhs=vtok[:, hd * 128:(hd + 1) * 128], start=True, stop=False))
                fns.append(lambda e, hd=hd: e.matmul(c.psum[4][:, hd * 128:(hd + 1) * 128], lhsT=qT[:, hd, sl],
                                                     rhs=Sp[:, hd, :], start=False, stop=True))
            k.op("pe", fns, reads=[R_AT, R_vtok, R_qT, R_Sp], writes=[c.R_ps[4]])
            for hd in range(4):
                k.op("act", lambda e, hd=hd: e.activation(out=o2[:, hd * 128:(hd + 1) * 128], in_=c.psum[4][:, hd * 128:(hd + 1) * 128],
                                                          func=AF.Square, accum_out=hs[:, hd:hd + 1]),
                     reads=[c.R_ps[4]], writes=[R_o, R_hs])
            k.op("act", lambda e: e.activation(out=hs[:, 4:8], in_=hs[:, 0:4], func=AF.Sqrt, scale=1.0 / 128, bias=c.epsb[:, 0:1]),
                 reads=[R_hs, c.R_const], writes=[R_hs])
            k.op("dve", lambda e: e.reciprocal(out=hs[:, 8:12], in_=hs[:, 4:8]), reads=[R_hs], writes=[R_hs])
            k.op("dve", lambda e: e.tensor_tensor(out=o1, in0=c.psum[4], in1=gnt, op=ALU.mult), reads=[c.R_ps[4], R_gnt, R_o], writes=[R_o])
            k.op("pool", lambda e, tt=tt: e.tensor_tensor(out=o1, in0=o1, in1=sgt, op=ALU.mult), reads=[R_o, R_sgt], writes=[R_o])
            k.op("dve", lambda e: e.tensor_tensor(out=yb.rearrange("p (h x) -> p h x", h=4), in0=o1.rearrange("p (h x) -> p h x", h=4),
                                                  in1=hs[:, 8:12].unsqueeze(2).broadcast_to([128, 4, 128]), op=ALU.mult),
                 reads=[R_o, R_hs], writes=[R_yb])
            p6b = c.psum[6].bitcast(BF16)
            k.op("pe", [(lambda e, hd=hd: e.transpose(out=p6b[:, hd * 128:(hd + 1) * 128], in_=yb[:, hd * 128:(hd + 1) * 128], identity=c.identb))
                        for hd in range(4)], reads=[R_yb, c.R_const], writes=[c.R_ps[6]])
            k.op("act", lambda e, i_own=i_own: e.activation(out=c.oT_hg[:, :, i_own * 128:(i_own + 1) * 128],
                                                            in_=p6b[:, 0:512].rearrange("p (h x) -> p h x", h=4), func=AF.Copy),
                 reads=[c.R_ps[6]], writes=[c.R_oThg])
    ar.release(m0)


def phase_nsa(k, c, ar, St):
    Dr = c.D
    m0 = ar.mark()
    TINY = 1e-30
    CBi = [ar.al([128, 128], BF16) for _ in range(2)]
    VALi = [ar.al([128, 128], F32) for _ in range(2)]
    ADDCi = [ar.al([128, 128], F32) for _ in range(2)]
    R_tab = [k.res("nsatab%d" % i) for i in range(2)]
    WMT = ar.al([128, 8, 128], BF16)
    CM = ar.al([128, 4, 128], BF16)
    R_cst = k.res("nsacst")
    k.dma("pool", WMT, Dr["WMT"], writes=[R_cst])
    k.dma("pool", CM, Dr["CM"], writes=[R_cst])
    PT = [ar.al([128, 512], BF16) for _ in range(3)]
    R_PT = [k.res("PT%d" % i) for i in range(3)]
    Uc = ar.al([128, 4, 193], F32)
    R_Uc = k.res("Uc")
    Os = ar.al([128, 4, 65], F32)
    Ow = ar.al([128, 4, 65], F32)
    R_Os, R_Ow = k.res("Os"), k.res("Ow")
    score, sc2, imp = ar.al([128, 128], F32), ar.al([128, 128], F32), ar.al([128, 128], F32)
    R_sel = k.res("sel")
    selb = ar.al([128, 128], BF16)
    R_selb = k.res("selb")
    bd = ar.al([128, 4, 128], BF16)
    R_bd = k.res("bd")
    selX = ar.al([128, 128, 64], BF16)
    R_selX = k.res("selX")
    cols = ar.al([128, 64], F32)
    R_cols = k.res("nsacols")
    acc, tmp = ar.al([128, 4, 64], F32), ar.al([128, 4, 64], F32)
    R_acc = k.res("nsaacc")
    onsa = ar.al([128, 2, 4, 64], BF16)
    R_onsa = k.res("onsa")
    st = dict(s=0, p=0)
    pO_s, pO_w = c.psum[3][:, 0:260], c.psum[4][:, 0:260]
    pU = [c.psum[5], c.psum[6]]

    pend = []

    def flush_pv(keep=0):
        while len(pend) > keep:
            pend.pop(0)()

    def unit(KT, R_KT, kt_slice, QTg, g, bias, Vaug, R_V, outs, R_outs):
        sb = st["s"] % 3
        st["s"] += 1
        pb = st["p"] % 3
        st["p"] += 1
        S = c.psum[sb]
        fns = [lambda e: e.matmul(S, lhsT=KT[64 * g:64 * g + 64, kt_slice], rhs=QTg, start=True, stop=(bias is None))]
        rd = [R_KT, c.R_QT]
        if bias is not None:
            bl, R_bl = bias
            fns.append(lambda e: e.matmul(S, lhsT=bl, rhs=c.I4, start=False, stop=True))
            rd += [R_bl, c.R_const]
        k.op("pe", fns, reads=rd, writes=[c.R_ps[sb]])
        k.op("act", lambda e: e.activation(out=PT[pb], in_=S, func=AF.Exp), reads=[c.R_ps[sb]], writes=[R_PT[pb]])

        def pv():
            k.op("pe", [(lambda e, a=a: e.matmul(outs[a], lhsT=PT[pb][:, a * 128:(a + 1) * 128], rhs=Vaug, start=False, stop=False,
                                                 skip_group_check=True)) for a in range(4)],
                 reads=[R_PT[pb], R_V], writes=R_outs)
        pend.append(pv)
        flush_pv(keep=2)

    for i in range(c.n_blocks):
        tb = i % 2
        k.dma("pool", CBi[tb], Dr["CB"][:, i, :], writes=[R_tab[tb]])
        k.dma("sp", VALi[tb], Dr["VAL"][:, i, :], writes=[R_tab[tb]])
        k.dma("sp", ADDCi[tb], Dr["ADDC"][:, i, :], writes=[R_tab[tb]])
        for g in range(2):
            QTg = c.QT[64 * g:64 * g + 64, i, :, :].rearrange("p a t -> p (a t)")
            nmt = i // 4 + 1
            k.op("dve", lambda e: e.memset(pU[0], 0.0), writes=[c.R_ps[5]])
            k.op("dve", lambda e: e.memset(pU[1], 0.0), writes=[c.R_ps[6]])
            outsU = [pU[a // 2][:, (a % 2) * 193:(a % 2) * 193 + 193] for a in range(4)]
            for mt in range(nmt):
                bias = (CBi[tb], R_tab[tb]) if mt == nmt - 1 else None
                unit(St.KC, St.R_KC, slice(mt * 128, (mt + 1) * 128), QTg, g, bias, St.VCA[:, mt, g, :], St.R_VCA, outsU, [c.R_ps[5], c.R_ps[6]])
            flush_pv()
            k.op("act", lambda e: e.activation(out=Uc[:, 0:2, :], in_=pU[0][:, 0:386].rearrange("p (a x) -> p a x", a=2), func=AF.Copy),
                 reads=[c.R_ps[5]], writes=[R_Uc])
            k.op("act", lambda e: e.activation(out=Uc[:, 2:4, :], in_=pU[1][:, 0:386].rearrange("p (a x) -> p a x", a=2), func=AF.Copy),
                 reads=[c.R_ps[6]], writes=[R_Uc])
            k.op("dve", lambda e: e.memset(c.psum[4], 0.0), writes=[c.R_ps[4]])
            outsW = [pO_w[:, a * 65:(a + 1) * 65] for a in range(4)]
            for r in range(8):
                kt = 4 * i - 4 + r
                if kt < 0:
                    continue
                unit(St.KTw, St.R_KTw, slice(kt * 128, (kt + 1) * 128), QTg, g, (WMT[:, r, :], R_cst), St.VwA[:, kt, g, :], St.R_VwA, outsW, [c.R_ps[4]])
            zc, rzc = cols[:, 0:4], cols[:, 4:8]
            k.op("dve", lambda e: e.tensor_scalar(out=zc, in0=Uc[:, :, 64], scalar1=TINY, scalar2=None, op0=ALU.max), reads=[R_Uc], writes=[R_cols])
            k.op("dve", lambda e: e.reciprocal(out=rzc, in_=zc), reads=[R_cols], writes=[R_cols])
            k.op("dve", lambda e: e.tensor_scalar(out=imp, in0=Uc[:, 0, 65:193], scalar1=rzc[:, 0:1], scalar2=None, op0=ALU.mult),
                 reads=[R_Uc, R_cols], writes=[R_sel])
            for a in range(1, 4):
                k.op("dve", lambda e, a=a: e.scalar_tensor_tensor(out=imp, in0=Uc[:, a, 65:193], scalar=rzc[:, a:a + 1], in1=imp,
                                                                  op0=ALU.mult, op1=ALU.add), reads=[R_Uc, R_cols, R_sel], writes=[R_sel])
            k.op("dve", lambda e: e.tensor_tensor(out=score, in0=imp, in1=VALi[tb], op=ALU.mult), reads=[R_sel, R_tab[tb]], writes=[R_sel])
            k.op("dve", lambda e: e.tensor_tensor(out=score, in0=score, in1=ADDCi[tb], op=ALU.add), reads=[R_sel, R_tab[tb]], writes=[R_sel])
            m8a, m8b = cols[:, 8:16], cols[:, 16:24]
            k.op("dve", lambda e: e.max(out=m8a, in_=score), reads=[R_sel], writes=[R_cols])
            k.op("dve", lambda e: e.match_replace(out=sc2, in_to_replace=m8a, in_values=score, imm_value=-1e9), reads=[R_sel, R_cols], writes=[R_sel])
            k.op("dve", lambda e: e.max(out=m8b, in_=sc2), reads=[R_sel], writes=[R_cols])
            k.op("dve", lambda e: e.tensor_scalar(out=selb, in0=score, scalar1=m8b[:, 7:8], scalar2=NEG, op0=ALU.is_lt, op1=ALU.mult),
                 reads=[R_sel, R_cols], writes=[R_selb])
            for r in range(4):
                kt = 4 * i + r
                k.op("dve", lambda e, r=r, kt=kt: e.tensor_tensor(
                    out=bd[:, r, :].rearrange("p (b x) -> p b x", b=2), in0=CM[:, r, :].rearrange("p (b x) -> p b x", b=2),
                    in1=selb[:, 2 * kt:2 * kt + 2].unsqueeze(2).broadcast_to([128, 2, 64]), op=ALU.add),
                    reads=[R_cst, R_selb], writes=[R_bd])
            if i > 0:
                nbk = 8 * i
                k.op("pool", lambda e, nbk=nbk: e.tensor_copy(out=selX[:, 0:nbk, :], in_=selb[:, 0:nbk].unsqueeze(2).broadcast_to([128, nbk, 64])),
                     reads=[R_selb], writes=[R_selX])
            k.op("dve", lambda e: e.memset(c.psum[3], 0.0), writes=[c.R_ps[3]])
            outsS = [pO_s[:, a * 65:(a + 1) * 65] for a in range(4)]
            for kt in range(4 * i + 4):
                if kt < 4 * i:
                    bl = selX[:, 2 * kt:2 * kt + 2, :].rearrange("p b x -> p (b x)")
                    bias = (bl, R_selX)
                else:
                    bias = (bd[:, kt - 4 * i, :], R_bd)
                unit(St.KTs, St.R_KTs, slice(kt * 128, (kt + 1) * 128), QTg, g, bias, St.VsA[:, kt, g, :], St.R_VsA, outsS, [c.R_ps[3]])
            flush_pv()
            k.op("act", lambda e: e.activation(out=Os, in_=pO_s.rearrange("p (a x) -> p a x", a=4), func=AF.Copy), reads=[c.R_ps[3]], writes=[R_Os])
            k.op("act", lambda e: e.activation(out=Ow, in_=pO_w.rearrange("p (a x) -> p a x", a=4), func=AF.Copy), reads=[c.R_ps[4]], writes=[R_Ow])
            gs = c.gsig[:, i, 12 * g:12 * g + 12].rearrange("p (a x) -> p a x", a=4)
            zs, zw, cfc, cfs, cfw = cols[:, 24:28], cols[:, 28:32], cols[:, 32:36], cols[:, 36:40], cols[:, 40:44]
            k.op("dve", lambda e: e.tensor_scalar(out=zs, in0=Os[:, :, 64], scalar1=TINY, scalar2=None, op0=ALU.max), reads=[R_Os], writes=[R_cols])
            k.op("dve", lambda e: e.tensor_scalar(out=zw, in0=Ow[:, :, 64], scalar1=TINY, scalar2=None, op0=ALU.max), reads=[R_Ow], writes=[R_cols])
            k.op("dve", lambda e: e.reciprocal(out=zs, in_=zs), reads=[R_cols], writes=[R_cols])
            k.op("dve", lambda e: e.reciprocal(out=zw, in_=zw), reads=[R_cols], writes=[R_cols])
            k.op("dve", lambda e: e.tensor_tensor(out=cfc, in0=rzc, in1=gs[:, :, 0], op=ALU.mult), reads=[R_cols, c.R_gsig], writes=[R_cols])
            k.op("dve", lambda e: e.tensor_tensor(out=cfs, in0=zs, in1=gs[:, :, 1], op=ALU.mult), reads=[R_cols, c.R_gsig], writes=[R_cols])
            k.op("dve", lambda e: e.tensor_tensor(out=cfw, in0=zw, in1=gs[:, :, 2], op=ALU.mult), reads=[R_cols, c.R_gsig], writes=[R_cols])
            bc = lambda col: col.unsqueeze(2).broadcast_to([128, 4, 64])
            k.op("dve", lambda e: e.tensor_tensor(out=acc, in0=Uc[:, :, 0:64], in1=bc(cfc), op=ALU.mult), reads=[R_Uc, R_cols], writes=[R_acc])
            k.op("dve", lambda e: e.tensor_tensor(out=tmp, in0=Os[:, :, 0:64], in1=bc(cfs), op=ALU.mult), reads=[R_Os, R_cols, R_acc], writes=[R_acc])
            k.op("pool", lambda e: e.tensor_tensor(out=acc, in0=acc, in1=tmp, op=ALU.add), reads=[R_acc], writes=[R_acc])
            k.op("dve", lambda e: e.tensor_tensor(out=tmp, in0=Ow[:, :, 0:64], in1=bc(cfw), op=ALU.mult), reads=[R_Ow, R_cols, R_acc], writes=[R_acc])
            k.op("pool", lambda e, g=g: e.tensor_tensor(out=onsa[:, g, :, :], in0=acc, in1=tmp, op=ALU.add), reads=[R_acc], writes=[R_onsa])
        p7b = c.psum[7].bitcast(BF16)
        of = onsa.rearrange("p g a d -> p (g a d)")
        k.op("pe", [(lambda e, j=j: e.transpose(out=p7b[:, j * 128:(j + 1) * 128], in_=of[:, j * 128:(j + 1) * 128], identity=c.identb))
                    for j in range(4)], reads=[R_onsa, c.R_const], writes=[c.R_ps[7]])
        k.op("act", lambda e, i=i: e.activation(out=c.oT_nsa[:, :, i * 128:(i + 1) * 128],
                                                in_=p7b[:, 0:512].rearrange("p (j x) -> p j x", j=4), func=AF.Copy),
             reads=[c.R_ps[7]], writes=[c.R_oTnsa])
    ar.release(m0)


def phase_p2c(k, c, ar, X1, R_X1):
    Dr = c.D
    m0 = ar.mark()
    W = WS()
    scr = ar.al([128, D], F32)
    alloc_x_ws(k, c, ar, W, None, scr=scr, R_scr=k.res("scr2c"))
    hT = ar.al([128, 8, 512], BF16)
    R_hT = k.res("hT3")
    wbn, wbh = ar.al([128, 4, D], BF16), ar.al([128, 4, D], BF16)
    R_wb = k.res("wbr")
    wch = [ar.al([128, 8, 512], BF16) for _ in range(2)]
    R_wch = [k.res("wchc%d" % i) for i in range(2)]
    mixT = ar.al([128, 8, 512], BF16)
    R_mixT = k.res("mixT")
    sg1, sg2, mx1 = ar.al([128, 512], F32), ar.al([128, 512], F32), ar.al([128, 512], F32)
    R_sg1, R_sg2, R_mx = k.res("sg1"), k.res("sg2"), k.res("mx1")
    k.dma("pool", wbn, Dr["w_brn"].rearrange("(kt p) n -> p kt n", p=128), writes=[R_wb])
    k.dma("pool", wbh, Dr["w_brh"].rearrange("(kt p) n -> p kt n", p=128), writes=[R_wb])
    for go in range(NT // 4):
        tok = slice(go * 512, (go + 1) * 512)
        for tt in range(4):
            t = 4 * go + tt
            k.dma("sp", X1[:, t, :], Dr["xo"][t * 128:(t + 1) * 128, :], writes=[R_X1[t]])
        norm_transpose_group(k, c, W, Dr["xo"], go * 512, hT, R_hT)
        for hf in range(2):
            k.dma("pool", wch[0], Dr["w_mg"][:, hf * 512:(hf + 1) * 512].rearrange("(kt p) n -> p kt n", p=128), writes=[R_wch[0]])
            k.dma("pool", wch[1], Dr["w_mg"][:, 1024 + hf * 512:1024 + (hf + 1) * 512].rearrange("(kt p) n -> p kt n", p=128), writes=[R_wch[1]])
            for f4 in range(4):
                ft = hf * 4 + f4
                fs = slice(f4 * 128, (f4 + 1) * 128)
                gs_ = slice(ft * 128, (ft + 1) * 128)
                k.op("pe", [(lambda e, i=i: e.matmul(c.psum[2], lhsT=wch[0][:, i, fs], rhs=hT[:, i, :], start=(i == 0), stop=(i == 7)))
                            for i in range(8)], reads=[R_wch[0], R_hT], writes=[c.R_ps[2]])
                k.op("pe", [(lambda e, i=i: e.matmul(c.psum[3], lhsT=wch[1][:, i, fs], rhs=hT[:, i, :], start=(i == 0), stop=(i == 7)))
                            for i in range(8)], reads=[R_wch[1], R_hT], writes=[c.R_ps[3]])
                k.op("pe", [(lambda e, i=i: e.matmul(c.psum[4], lhsT=wbn[:, i, gs_], rhs=c.oT_nsa[:, i, tok], start=(i == 0), stop=(i == 3)))
                            for i in range(4)], reads=[R_wb, c.R_oTnsa], writes=[c.R_ps[4]])
                k.op("pe", [(lambda e, i=i: e.matmul(c.psum[5], lhsT=wbh[:, i, gs_], rhs=c.oT_hg[:, i, tok], start=(i == 0), stop=(i == 3)))
                            for i in range(4)], reads=[R_wb, c.R_oThg], writes=[c.R_ps[5]])
                k.op("act", lambda e: e.activation(out=sg1, in_=c.psum[2], func=AF.Sigmoid), reads=[c.R_ps[2]], writes=[R_sg1])
                k.op("act", lambda e: e.activation(out=sg2, in_=c.psum[3], func=AF.Sigmoid), reads=[c.R_ps[3]], writes=[R_sg2])
                k.op("dve", lambda e: e.tensor_tensor(out=mx1, in0=c.psum[4], in1=sg1, op=ALU.mult), reads=[c.R_ps[4], R_sg1], writes=[R_mx])
                k.op("dve", lambda e: e.tensor_tensor(out=sg2, in0=c.psum[5], in1=sg2, op=ALU.mult), reads=[c.R_ps[5], R_sg2], writes=[R_sg2])
                k.op("pool", lambda e, ft=ft: e.tensor_tensor(out=mixT[:, ft, :], in0=mx1, in1=sg2, op=ALU.add),
                     reads=[R_mx, R_sg2], writes=[R_mixT])
        for hf in range(2):
            k.dma("pool", wch[hf], Dr["w_out"][:, hf * 512:(hf + 1) * 512].rearrange("(kt p) n -> p kt n", p=128), writes=[R_wch[hf]])
        for tt in range(4):
            t = 4 * go + tt
            for hf in range(2):
                bank = 6 + hf
                k.op("pe", [(lambda e, i=i: e.matmul(c.psum[bank], lhsT=mixT[:, i, tt * 128:(tt + 1) * 128], rhs=wch[hf][:, i, :],
                                                     start=(i == 0), stop=(i == 7))) for i in range(8)],
                     reads=[R_mixT, R_wch[hf]], writes=[c.R_ps[bank]])
                xs = X1[:, t, hf * 512:(hf + 1) * 512]
                k.op("dve", lambda e, xs=xs, bank=bank: e.tensor_tensor(out=xs, in0=c.psum[bank], in1=xs, op=ALU.add),
                     reads=[c.R_ps[bank], R_X1[t]], writes=[R_X1[t]])
    ar.release(m0)


def phase_attn(k, c, ar, stage):
    St = WS()
    St.KTs, St.KTw = ar.al([128, SEQ], BF16), ar.al([128, SEQ], BF16)
    St.VsA, St.VwA = ar.al([128, 64, 2, 65], BF16), ar.al([128, 64, 2, 65], BF16)
    St.KC = ar.al([128, 512], BF16)
    St.VCA = ar.al([128, 4, 2, 193], BF16)
    St.SNAP = ar.al([128, NT, 4, 128], BF16)
    for n in ("KTs", "KTw", "VsA", "VwA", "KC", "VCA", "SNAP"):
        setattr(St, "R_" + n, k.res(n))
    phase_p1(k, c, ar, St)
    for n in ("KTs", "KTw", "VsA", "VwA", "KC", "VCA", "SNAP"):
        c.dump(n, getattr(St, n), [getattr(St, "R_" + n)])
    k.flush()
    phase_p2pre(k, c, ar, St)
    c.dump("QT", c.QT, [c.R_QT])
    c.dump("gsig", c.gsig, [c.R_gsig])
    c.dump("oT_hg", c.oT_hg, [c.R_oThg])
    k.flush()
    phase_nsa(k, c, ar, St)
    c.dump("oT_nsa", c.oT_nsa, [c.R_oTnsa])
    return St


def build(stage="full", n_exp=NE):
    nc = bass.Bass("TRN2", target_bir_lowering=False)
    Dr = {}

    def din(name, shape, dt=F32):
        Dr[name] = nc.dram_tensor(name, list(shape), dt, kind="ExternalInput").ap()

    for name, shape, dt in input_specs(max(n_exp, 1), stage):
        din(name, shape, dt)
    Dr["y"] = nc.dram_tensor("y", [NTOK, D], F32, kind="ExternalOutput").ap()
    with ExitStack() as es:
        ARENA_BYTES = 207 * 1024
        big = es.enter_context(nc.sbuf_tensor("arena", [128, ARENA_BYTES // 2], BF16))
        pst = es.enter_context(nc.psum_tensor("ps", [128, 4096], F32))
        ar = Arena(big, ARENA_BYTES)
        k = KH(nc, es)
        k.oplim = _NC_CACHE.get("oplim", 10 ** 9)
        c = Ctx()
        c.nc, c.D, c.n_exp = nc, Dr, n_exp
        dumps = []

        def dump(name, ap, rs):
            if not _NC_CACHE.get("dbg"):
                return
            dt_ = ap.dtype
            dr = nc.dram_tensor("dbg_" + name, list(ap.shape), dt_, kind="ExternalOutput").ap()
            r = k.res("dbg_" + name)
            k.dma("sp", dr, ap, reads=list(rs), key=r)
            dumps.append(r)
        c.dump = dump
        c.psum = [pst[:, i * 512:(i + 1) * 512] for i in range(8)]
        c.pwide = lambda i: pst[:, i * 512:(i + 2) * 512]
        c.R_ps = [k.res("psb%d" % i, excl=True) for i in range(8)]
        c.R_const = k.res("const")
        c.identf = ar.al([128, 128], F32)
        c.identb = ar.al([128, 128], BF16)
        c.epsb = ar.al([128, 1], F32)
        c.ssq = ar.al([128, 8], F32)
        c.std = ar.al([128, 8], F32)
        c.rstd = ar.al([128, 8], F32)
        c.rs_res = [k.res("rs%d" % i) for i in range(8)]
        c.rs_i = 0
        k.dma("sp", c.identf, Dr["identf"], writes=[c.R_const])
        k.dma("sp", c.identb, Dr["identb"], writes=[c.R_const])
        k.op("dve", lambda e: e.memset(c.epsb, EPS), writes=[c.R_const])
        c.n_groups = _NC_CACHE.get("n_groups", 16)
        c.n_blocks = _NC_CACHE.get("n_blocks", NT)
        c.R32 = ar.al([128, 8, NTOK], BF16)
        c.R32f = c.R32.rearrange("p a b -> p (a b)")
        c.QT = c.R32f[:, 0:8192].rearrange("p (i a t) -> p i a t", i=NT, a=4)
        c.oT_nsa = c.R32[:, 4:8, :]
        c.oT_hg = ar.al([128, 4, NTOK], BF16)
        c.gsig = ar.al([128, NT, 24], F32)
        c.R_QT, c.R_oTnsa, c.R_oThg, c.R_gsig = k.res("QT"), k.res("oTnsa"), k.res("oThg"), k.res("gsig")
        hfT = c.R32
        R_hfT = [k.res("hfT_%d" % t) for t in range(NT)]
        R_X1 = [k.res("x1_%d" % t) for t in range(NT)]
        if stage != "moe_only":
            c.I4 = ar.al([128, 512], BF16)
            c.tri = ar.al([128, 128], BF16)
            c.onehot = ar.al([128, 4], F32)
            k.dma("pool", c.I4, Dr["I4"], writes=[c.R_const])
            k.dma("pool", c.tri, Dr["tri"], writes=[c.R_const])
            k.dma("sp", c.onehot, Dr["onehot"], writes=[c.R_const])
            setup_lb(k, c, ar)
            M1 = ar.mark()
            phase_attn(k, c, ar, stage)
            for r in dumps:
                k.rec["sp"].append(("w", r.dsem, r.dcnt))
            k.flush()
            ar.release(M1)
        X1 = ar.al([128, NT, D], F32)
        if stage == "moe_only":
            for t in range(NT):
                k.dma("sp", X1[:, t, :], Dr["xo"][t * 128:(t + 1) * 128, :], writes=[R_X1[t]])
        else:
            phase_p2c(k, c, ar, X1, R_X1)
            c.dump("X1", X1, R_X1)
            for r in dumps:
                if r.name == "dbg_X1":
                    k.rec["sp"].append(("w", r.dsem, r.dcnt))
        k.flush()
        if n_exp >= 0:
            phase_moe(k, c, ar, X1, R_X1, hfT, R_hfT)
            k.flush()
        phase_final(k, c, ar, X1, R_X1)
        k.flush()
    return nc


def input_specs(ne=NE, stage="full"):
    return [
        ("xb", (SEQ, D), F32), ("xo", (NTOK, D), F32),
        ("g_ffn", (D,), F32), ("g_fin", (D,), F32),
        ("w_r", (D, 36), F32), ("b_r", (36,), F32),
        ("w_gate", (ne, D, DFF), F32), ("w_up", (ne, D, DFF), F32), ("w_down", (ne, DFF, D), F32),
        ("identf", (128, 128), F32), ("identb", (128, 128), BF16),
    ] + (attn_input_specs() if stage != "moe_only" else [])


_NC_CACHE = {}


def host_inputs(inp, core, ne=NE):
    b, cp = core // 4, core % 4
    x = np.asarray(inp["x"], dtype=np.float32)
    m = {}
    m["xb"] = np.ascontiguousarray(x[b])
    m["xo"] = np.ascontiguousarray(x[b].reshape(NT, 4, 128, D)[:, cp].reshape(NTOK, D))
    m["g_ffn"] = np.ascontiguousarray(inp["ffn_norm"][0])
    m["g_fin"] = np.ascontiguousarray(inp["final_norm"])
    m["w_r"] = np.ascontiguousarray(np.concatenate([inp["w_grp"][0], inp["w_rtr"][0]], axis=1))
    m["b_r"] = np.ascontiguousarray(np.concatenate([inp["b_grp"][0], inp["b_rtr"][0]], axis=0))
    m["w_gate"] = np.ascontiguousarray(inp["w_gate"][0, :ne])
    m["w_up"] = np.ascontiguousarray(inp["w_up"][0, :ne])
    m["w_down"] = np.ascontiguousarray(inp["w_down"][0, :ne])
    m["identf"] = np.eye(128, dtype=np.float32)
    m["identb"] = np.eye(128, dtype=np.float32).astype(ml_dtypes.bfloat16)
    if _NC_CACHE.get("stage", "full") != "moe_only":
        m.update(attn_host_inputs(inp, b, cp))
    return m


def kernel(**inp):
    inp = {k_: np.asarray(v) for k_, v in inp.items()}
    stage = _NC_CACHE.get("stage", "full")
    key = ("nc", stage)
    if key not in _NC_CACHE:
        _NC_CACHE[key] = build(stage, _NC_CACHE.get("n_exp", NE))
    nc = _NC_CACHE[key]
    shared = None
    in_maps = []
    for core in range(8):
        m = host_inputs(inp, core, max(_NC_CACHE.get("n_exp", NE), 1))
        if shared is None:
            shared = m
        else:
            for kk in ("w_gate", "w_up", "w_down"):
                m[kk] = shared[kk]
        in_maps.append(m)
    res = run_bass_kernel_spmd(nc, in_maps, core_ids=list(range(8)))
    _NC_CACHE["last_results"] = res.results
    out = np.zeros((2, SEQ // 128, 128, D), dtype=np.float32)
    for core in range(8):
        b, cp = core // 4, core % 4
        y = np.asarray(res.results[core]["y"]).reshape(NT, 128, D)
        out[b, cp::4] = y
    return out.reshape(2, SEQ, D)
```

```python
import numpy as np
import ml_dtypes
import concourse.bass as bass
import concourse.mybir as mybir
from concourse.bass_utils import run_bass_kernel_spmd
from contextlib import ExitStack

F32 = mybir.dt.float32
BF16 = mybir.dt.bfloat16
AF = mybir.ActivationFunctionType
ALU = mybir.AluOpType
AX = mybir.AxisListType


class Res:
    __slots__ = ("name", "w", "rs", "dsem", "dcnt", "excl")

    def __init__(self, name, excl=False):
        self.name = name
        self.excl = excl
        self.w = None
        self.rs = {}
        self.dsem = None
        self.dcnt = 0


class _Proxy:
    def __init__(self):
        self.calls = []

    def __getattr__(self, name):
        def rec(*a, **kw):
            self.calls.append((name, a, kw))
        return rec


def _bind(f):
    p = _Proxy()
    f(p)
    assert len(p.calls) == 1, "one engine instruction per callable"
    name, a, kw = p.calls[0]
    return lambda eng: getattr(eng, name)(*a, **kw)


class KH:
    ENG = ("pe", "dve", "act", "pool", "sp")

    def __init__(self, nc, es):
        self.nc = nc
        self.es = es
        self.sem = {}
        self.cnt = {}
        for e in self.ENG:
            self.sem[e] = es.enter_context(nc.semaphore("s_" + e))
            self.cnt[e] = 0
        self.rec = {e: [] for e in self.ENG}
        self.seen = {e: {} for e in self.ENG}
        self.nsem = len(self.ENG)
        self.semobj = dict(self.sem)

    def res(self, name, excl=False):
        return Res(name, excl)

    def _dma_sem(self, r):
        if r.dsem is None:
            r.dsem = "d_" + r.name + "_%d" % self.nsem
            self.semobj[r.dsem] = self.es.enter_context(self.nc.semaphore(r.dsem))
            self.nsem += 1
        return r.dsem

    def _deps(self, e, reads, writes):
        deps = {}
        for r in reads:
            if r.w is not None:
                k, v = r.w
                deps[k] = max(deps.get(k, 0), v)
        for w in writes:
            if w.w is not None:
                k, v = w.w
                deps[k] = max(deps.get(k, 0), v)
            for k, v in w.rs.items():
                deps[k] = max(deps.get(k, 0), v)
        seen = self.seen[e]
        for k, v in deps.items():
            if seen.get(k, 0) >= v:
                continue
            seen[k] = v
            self.rec[e].append(("w", k, v))

    def op(self, e, fns, reads=(), writes=()):
        if callable(fns):
            fns = [fns]
        self.opn = getattr(self, "opn", 0) + 1
        if self.opn > getattr(self, "oplim", 10 ** 9):
            return
        ex = [r for r in reads if r.excl]
        if ex:
            reads = [r for r in reads if not r.excl]
            writes = list(writes) + [r for r in ex if r not in writes]
        self._deps(e, reads, writes)
        self.cnt[e] += 1
        v = self.cnt[e]
        fns = [_bind(f) for f in fns]
        for f in fns[:-1]:
            self.rec[e].append(("i", f, None, 0))
        self.rec[e].append(("i", fns[-1], e, 1))
        self.seen[e][e] = max(self.seen[e].get(e, 0), 0)
        for r in reads:
            r.rs[e] = v
        for w in writes:
            w.w = (e, v)
            w.rs = {}

    def dma(self, q, out, in_, reads=(), writes=(), key=None, **kw):
        self._deps(q, reads, writes)
        kr = key or (writes[0] if writes else reads[0])
        sk = self._dma_sem(kr)
        kr.dcnt += 16
        v = kr.dcnt
        self.rec[q].append(("i", lambda eng: eng.dma_start(out=out, in_=in_, **kw), sk, 16))
        for r in reads:
            r.rs[sk] = v
        for w in writes:
            w.w = (sk, v)
            w.rs = {}

    def wait_res(self, e, rs):
        self._deps(e, rs, ())

    def simulate(self):
        if not hasattr(self, "simval"):
            self.simval = {}
        val = self.simval
        ptr = {e: 0 for e in self.ENG}
        prog = True
        while prog:
            prog = False
            for e in self.ENG:
                items = self.rec[e]
                while ptr[e] < len(items):
                    it = items[ptr[e]]
                    if it[0] == "w":
                        if val.get(it[1], 0) >= it[2]:
                            ptr[e] += 1
                            prog = True
                        else:
                            break
                    else:
                        if it[2] is not None:
                            val[it[2]] = val.get(it[2], 0) + it[3]
                        ptr[e] += 1
                        prog = True
        for e in self.ENG:
            if ptr[e] < len(self.rec[e]):
                it = self.rec[e][ptr[e]]
                raise RuntimeError("DEADLOCK: engine %s stuck at item %d/%d waiting %s >= %s (have %s)" % (
                    e, ptr[e], len(self.rec[e]), it[1], it[2], val.get(it[1], 0)))

    def flush(self, name=None):
        nc = self.nc
        rec = self.rec
        semobj = self.semobj
        self.simulate()
        import os
        if os.environ.get("KH_DEBUG"):
            print("KH flush: ops so far", getattr(self, "opn", 0), {e: len(v) for e, v in self.rec.items()}, "nsem", self.nsem, flush=True)

        def play(eng, items):
            for it in items:
                if it[0] == "w":
                    eng.wait_ge(semobj[it[1]], it[2])
                else:
                    ins = it[1](eng)
                    if it[2] is not None:
                        ins.then_inc(semobj[it[2]], it[3])

        with nc.Block() as block:
            if rec["sp"]:
                @block.sync
                def _(eng):
                    play(eng, rec["sp"])
            if rec["pe"]:
                @block.tensor
                def _(eng):
                    play(eng, rec["pe"])
            if rec["dve"]:
                @block.vector
                def _(eng):
                    play(eng, rec["dve"])
            if rec["act"]:
                @block.scalar
                def _(eng):
                    play(eng, rec["act"])
            if rec["pool"]:
                @block.gpsimd
                def _(eng):
                    play(eng, rec["pool"])
        self.rec = {e: [] for e in self.ENG}

NEG = -30000.0
NT = 16
NTOK = NT * 128
SEQ = 8192
D = 1024
NE = 32
DFF = 512
EPS = 1e-6


class Arena:
    def __init__(self, big, nbytes):
        self.big = big
        self.n = nbytes
        self.off = 0

    def mark(self):
        return self.off

    def release(self, m):
        import os
        if os.environ.get("KH_DEBUG"):
            print("arena release: peak", getattr(self, "peak", 0), "->", m, "of", self.n, flush=True)
        self.peak = m
        self.off = m

    def al(self, shape, dt):
        esz = 4 if dt == F32 else 2
        per = int(np.prod(shape[1:])) * esz
        self.off = (self.off + 63) // 64 * 64
        o = self.off
        assert o + per <= self.n, ("arena overflow", o, per, self.n)
        self.off = o + per
        self.peak = max(getattr(self, "peak", 0), self.off)
        v = self.big[0:shape[0], o // 2:(o + per) // 2]
        if dt == F32:
            v = v.bitcast(F32)
        if len(shape) == 3:
            v = v.rearrange("p (a b) -> p a b", a=shape[1])
        elif len(shape) == 4:
            v = v.rearrange("p (a b c) -> p a b c", a=shape[1], b=shape[2])
        return v


class Ctx:
    pass


def rms_rstd(k, c, src, src_res, scr, scr_res, n_feat, tag):
    i = c.rs_i % 8
    c.rs_i += 1
    ssq, std, rstd = c.ssq[:, i:i + 1], c.std[:, i:i + 1], c.rstd[:, i:i + 1]
    R = c.rs_res[i]
    k.op("act", lambda e: e.activation(out=scr, in_=src, func=AF.Square, accum_out=ssq),
         reads=[src_res], writes=[scr_res, R])
    k.op("act", lambda e: e.activation(out=std, in_=ssq, func=AF.Sqrt, scale=1.0 / n_feat, bias=c.epsb[:, 0:1]),
         reads=[R, c.R_const], writes=[R])
    k.op("dve", lambda e: e.reciprocal(out=rstd, in_=std), reads=[R], writes=[R])
    return rstd, R


def phase_moe(k, c, ar, X1, R_X1, hfT, R_hfT):
    nc = c.nc
    Dr = c.D
    m0 = ar.mark()
    hf32 = [ar.al([128, D], F32) for _ in range(2)]
    R_hf32 = [k.res("hf32_%d" % i) for i in range(2)]
    scr = ar.al([128, D], F32)
    R_scr = k.res("moe_scr")
    hT32 = [ar.al([128, 8, 128], F32) for _ in range(2)]
    R_hT32 = [k.res("hT32_%d" % i) for i in range(2)]
    wr32 = ar.al([128, 8, 36], F32)
    R_wr = k.res("wr32")
    brt = ar.al([128, 36], F32)
    gft = ar.al([128, D], F32)
    R_gft = k.res("gft")
    comb = ar.al([128, NT, NE], F32)
    R_comb = k.res("comb")
    sm = ar.al([128, 128], F32)
    R_sm = k.res("moe_sm")
    k.dma("sp", wr32, Dr["w_r"].rearrange("(kt p) n -> p kt n", p=128), writes=[R_wr])
    k.dma("sp", brt, Dr["b_r"].partition_broadcast(128), writes=[R_wr])
    k.dma("sp", gft, Dr["g_ffn"].partition_broadcast(128), writes=[R_gft])
    pT = [c.pwide(0), c.pwide(2)]
    R_pT = [[c.R_ps[0], c.R_ps[1]], [c.R_ps[2], c.R_ps[3]]]
    pL = c.psum[4]
    R_pL = c.R_ps[4]
    for t in range(NT):
        b = t % 2
        xs = X1[:, t, :]
        rstd, R_r = rms_rstd(k, c, xs, R_X1[t], scr, R_scr, D, "moe")
        k.op("dve", lambda e, b=b, xs=xs, rstd=rstd: e.scalar_tensor_tensor(
            out=hf32[b], in0=xs, scalar=rstd, in1=gft, op0=ALU.mult, op1=ALU.mult),
            reads=[R_X1[t], R_r, R_gft], writes=[R_hf32[b]])
        p2 = pT[b]
        k.op("pe", [(lambda e, i=i, b=b, p2=p2: e.transpose(out=p2[:, i * 128:(i + 1) * 128],
                                                            in_=hf32[b][:, i * 128:(i + 1) * 128], identity=c.identf))
                    for i in range(8)], reads=[R_hf32[b], c.R_const], writes=R_pT[b])
        k.op("act", lambda e, b=b, p2=p2: e.activation(out=hT32[b].rearrange("p a b -> p (a b)"), in_=p2, func=AF.Copy),
             reads=R_pT[b], writes=[R_hT32[b]])
        k.op("dve", lambda e, b=b, p2=p2, t=t: e.tensor_copy(
            out=hfT[:, :, t * 128:(t + 1) * 128], in_=p2.rearrange("p (a b) -> p a b", a=8)),
            reads=R_pT[b], writes=[R_hfT[t]])
        lg = pL[:, 0:36]
        k.op("pe", [(lambda e, i=i, b=b: e.matmul(lg, lhsT=hT32[b][:, i, :], rhs=wr32[:, i, :], start=(i == 0), stop=(i == 7)))
                    for i in range(8)], reads=[R_hT32[b], R_wr], writes=[R_pL])
        lgs = sm[:, 0:36]
        gmax, gsum, gp, pen = sm[:, 36:37], sm[:, 37:38], sm[:, 38:39], sm[:, 40:44]
        gex, goh = sm[:, 44:48], sm[:, 48:52]
        elm = sm[:, 52:84]
        m8 = sm[:, 84:92]
        dd, ee, w1, w2 = sm[:, 92:93], sm[:, 93:94], sm[:, 94:95], sm[:, 95:96]
        oh = sm[:, 96:128]
        ct = comb[:, t, :]
        RW = dict(reads=[R_sm], writes=[R_sm])
        k.op("dve", lambda e: e.tensor_tensor(out=lgs, in0=lg, in1=brt, op=ALU.add), reads=[R_pL, R_wr, R_sm], writes=[R_sm])
        k.op("dve", lambda e: e.reduce_max(out=gmax, in_=lgs[:, 0:4], axis=AX.X), **RW)
        k.op("dve", lambda e: e.tensor_scalar(out=gex, in0=lgs[:, 0:4], scalar1=gmax, scalar2=None, op0=ALU.subtract), **RW)
        k.op("act", lambda e: e.activation(out=gex, in_=gex, func=AF.Exp, accum_out=gsum), **RW)
        k.op("dve", lambda e: e.reciprocal(out=gp, in_=gsum), **RW)
        k.op("dve", lambda e: e.tensor_scalar(out=pen, in0=lgs[:, 0:4], scalar1=gmax, scalar2=-1e30, op0=ALU.is_lt, op1=ALU.mult), **RW)
        k.op("dve", lambda e: e.tensor_tensor(out=elm.rearrange("p (g x) -> p g x", g=4),
                                              in0=lgs[:, 4:36].rearrange("p (g x) -> p g x", g=4),
                                              in1=pen.unsqueeze(2).broadcast_to([128, 4, 8]), op=ALU.add), **RW)
        k.op("dve", lambda e: e.max(out=m8, in_=elm), **RW)
        k.op("dve", lambda e: e.tensor_tensor(out=dd, in0=m8[:, 1:2], in1=m8[:, 0:1], op=ALU.subtract), **RW)
        k.op("act", lambda e: e.activation(out=ee, in_=dd, func=AF.Exp), **RW)
        k.op("dve", lambda e: e.tensor_scalar(out=ee, in0=ee, scalar1=1.0, scalar2=None, op0=ALU.add), **RW)
        k.op("dve", lambda e: e.reciprocal(out=w1, in_=ee), **RW)
        k.op("dve", lambda e: e.tensor_scalar(out=w2, in0=w1, scalar1=-1.0, scalar2=1.0, op0=ALU.mult, op1=ALU.add), **RW)
        k.op("dve", lambda e: e.tensor_tensor(out=w1, in0=w1, in1=gp, op=ALU.mult), **RW)
        k.op("dve", lambda e: e.tensor_tensor(out=w2, in0=w2, in1=gp, op=ALU.mult), **RW)
        k.op("dve", lambda e: e.tensor_scalar(out=oh, in0=elm, scalar1=m8[:, 0:1], scalar2=w1, op0=ALU.is_equal, op1=ALU.mult), **RW)
        k.op("dve", lambda e, ct=ct: e.tensor_scalar(out=ct, in0=elm, scalar1=m8[:, 1:2], scalar2=w2, op0=ALU.is_equal, op1=ALU.mult),
             reads=[R_sm], writes=[R_comb])
        k.op("dve", lambda e, ct=ct: e.tensor_tensor(out=ct, in0=ct, in1=oh, op=ALU.add), reads=[R_sm, R_comb], writes=[R_comb])

    if c.n_exp == 0:
        ar.release(m0)
        return
    NWB = 2
    wg = [ar.al([128, 8, DFF], BF16) for _ in range(NWB)]
    wu = [ar.al([128, 8, DFF], BF16) for _ in range(NWB)]
    wd = [ar.al([128, 4, D], BF16) for _ in range(NWB)]
    R_wg = [k.res("wg%d" % i) for i in range(NWB)]
    R_wu = [k.res("wu%d" % i) for i in range(NWB)]
    R_wd = [k.res("wd%d" % i) for i in range(NWB)]
    hid = [ar.al([128, 4, 512], BF16) for _ in range(2)]
    R_hid = [k.res("hid%d" % i) for i in range(2)]
    sg = [ar.al([128, 512], F32) for _ in range(2)]
    R_sg = [k.res("sg%d" % i) for i in range(2)]
    n_exp = c.n_exp

    def load_w(e):
        b = e % NWB
        k.dma("pool", wg[b], Dr["w_gate"][e].rearrange("(kt p) n -> p kt n", p=128), writes=[R_wg[b]])
        k.dma("pool", wu[b], Dr["w_up"][e].rearrange("(kt p) n -> p kt n", p=128), writes=[R_wu[b]])
        k.dma("pool", wd[b], Dr["w_down"][e].rearrange("(kt p) n -> p kt n", p=128), writes=[R_wd[b]])

    units = [(e, g) for e in range(n_exp) for g in range(NT // 4)]
    pgu = [(c.psum[0], c.psum[1]), (c.psum[2], c.psum[3])]
    R_pgu = [(c.R_ps[0], c.R_ps[1]), (c.R_ps[2], c.R_ps[3])]
    pdn = [c.psum[4], c.psum[5], c.psum[6], c.psum[7]]
    R_pdn = [c.R_ps[4], c.R_ps[5], c.R_ps[6], c.R_ps[7]]
    st = dict(gu=0, dn=0)

    def gate_up(u):
        e, g = units[u]
        b = e % NWB
        hb = u % 2
        tok = slice(g * 512, (g + 1) * 512)
        for ff in range(4):
            pb = st["gu"] % 2
            st["gu"] += 1
            pg, pu = pgu[pb]
            k.op("pe", [(lambda en, i=i, pg=pg, b=b, ff=ff: en.matmul(pg, lhsT=wg[b][:, i, ff * 128:(ff + 1) * 128], rhs=hfT[:, i, tok],
                                                                      start=(i == 0), stop=(i == 7))) for i in range(8)],
                 reads=[R_wg[b]] + R_hfT[4 * g:4 * g + 4], writes=[R_pgu[pb][0]])
            k.op("pe", [(lambda en, i=i, pu=pu, b=b, ff=ff: en.matmul(pu, lhsT=wu[b][:, i, ff * 128:(ff + 1) * 128], rhs=hfT[:, i, tok],
                                                                      start=(i == 0), stop=(i == 7))) for i in range(8)],
                 reads=[R_wu[b]] + R_hfT[4 * g:4 * g + 4], writes=[R_pgu[pb][1]])
            k.op("act", lambda en, pg=pg, pb=pb: en.activation(out=sg[pb], in_=pg, func=AF.Silu),
                 reads=[R_pgu[pb][0]], writes=[R_sg[pb]])
            k.op("dve", lambda en, pu=pu, pb=pb, hb=hb, ff=ff: en.tensor_tensor(out=hid[hb][:, ff, :], in0=pu, in1=sg[pb], op=ALU.mult),
                 reads=[R_pgu[pb][1], R_sg[pb]], writes=[R_hid[hb]])

    def down(u):
        e, g = units[u]
        b = e % NWB
        hb = u % 2
        for tt in range(4):
            t = 4 * g + tt
            for hf in range(2):
                pb = st["dn"] % 4
                st["dn"] += 1
                po = pdn[pb]
                k.op("pe", [(lambda en, i=i, po=po, b=b, hb=hb, tt=tt, hf=hf: en.matmul(
                    po, lhsT=hid[hb][:, i, tt * 128:(tt + 1) * 128], rhs=wd[b][:, i, hf * 512:(hf + 1) * 512],
                    start=(i == 0), stop=(i == 3))) for i in range(4)],
                    reads=[R_hid[hb], R_wd[b]], writes=[R_pdn[pb]])
                xs = X1[:, t, hf * 512:(hf + 1) * 512]
                k.op("dve", lambda en, po=po, xs=xs, t=t, e=e: en.scalar_tensor_tensor(
                    out=xs, in0=po, scalar=comb[:, t, e:e + 1], in1=xs, op0=ALU.mult, op1=ALU.add),
                    reads=[R_pdn[pb], R_comb, R_X1[t]], writes=[R_X1[t]])

    load_w(0)
    for u in range(len(units)):
        e, g = units[u]
        gate_up(u)
        if u >= 1:
            down(u - 1)
        if g == 0 and e + 1 < n_exp:
            load_w(e + 1)
    down(len(units) - 1)
    ar.release(m0)


def phase_final(k, c, ar, X1, R_X1):
    Dr = c.D
    m0 = ar.mark()
    gft = ar.al([128, D], F32)
    R_g = k.res("gfin")
    scr = ar.al([128, D], F32)
    R_scr = k.res("fin_scr")
    ob = [ar.al([128, D], F32) for _ in range(2)]
    R_ob = [k.res("ob%d" % i) for i in range(2)]
    k.dma("sp", gft, Dr["g_fin"].partition_broadcast(128), writes=[R_g])
    for t in range(NT):
        b = t % 2
        xs = X1[:, t, :]
        rstd, R_r = rms_rstd(k, c, xs, R_X1[t], scr, R_scr, D, "fin")
        k.op("dve", lambda e, b=b, xs=xs, rstd=rstd: e.scalar_tensor_tensor(
            out=ob[b], in0=xs, scalar=rstd, in1=gft, op0=ALU.mult, op1=ALU.mult),
            reads=[R_X1[t], R_r, R_g], writes=[R_ob[b]])
        k.dma("sp", Dr["y"][t * 128:(t + 1) * 128, :], ob[b], reads=[R_ob[b]])
    for b in range(2):
        for sk, v in list(R_ob[b].rs.items()):
            if sk.startswith("d_"):
                k.rec["sp"].append(("w", sk, v))
    ar.release(m0)


Q0, KV0, GT0, HQ0, HF0, HI0, HG0, MG0 = 0, 512, 1280, 1304, 1816, 2328, 2840, 3352


def _partner(d):
    return d + 8 if d < 8 else (d - 8 if d < 16 else d)


def _rope_tables(pos):
    pos = np.asarray(pos, dtype=np.float32)
    inv = (np.float32(500000.0) ** (-np.arange(8, dtype=np.float32) / np.float32(8))).astype(np.float32)
    ang = (pos[None, :] * inv[:, None]).astype(np.float32)
    cs, sn = np.cos(ang).astype(np.float32), np.sin(ang).astype(np.float32)
    C = np.ones((64, len(pos)), np.float32)
    S = np.zeros((64, len(pos)), np.float32)
    C[0:8], C[8:16] = cs, cs
    S[0:8], S[8:16] = -sn, sn
    return C, S


def attn_input_specs():
    return [
        ("g_attn", (D,), F32), ("g_hg4", (512,), F32),
        ("w1f", (D, 1280), F32), ("w1t", (D, 768), F32),
        ("w2f", (D, 2048), F32), ("w2t", (D, 1536), F32),
        ("wck", (64, 2048), F32), ("wckp", (64, 2048), F32), ("wcv", (64, 2048), F32),
        ("posT", (128, 32), F32), ("lbl", (2, 512), F32),
        ("w_mg", (D, 2048), F32), ("w_brn", (512, D), F32), ("w_brh", (512, D), F32), ("w_out", (D, D), F32),
        ("CK", (128, SEQ), F32), ("SK", (128, SEQ), F32), ("CKc", (128, 512), F32), ("SKc", (128, 512), F32),
        ("CQ", (128, NTOK), F32), ("SQ", (128, NTOK), F32),
        ("ovl", (128, 4, 128), F32),
        ("CB", (128, NT, 128), F32), ("CM", (128, 4, 128), F32), ("WMT", (128, 8, 128), F32),
        ("VAL", (128, NT, 128), F32), ("ADDC", (128, NT, 128), F32),
        ("tri", (128, 128), F32), ("I4", (128, 512), F32), ("onehot", (128, 4), F32),
    ]


_TAB_CACHE = {}


def _const_tables(cp):
    if cp in _TAB_CACHE:
        return _TAB_CACHE[cp]
    m = {}
    C, S = _rope_tables(np.arange(SEQ))
    m["CK"], m["SK"] = np.concatenate([C, C], 0), np.concatenate([S, S], 0)
    C, S = _rope_tables(np.maximum(16 * (np.arange(512) - 1), 0))
    m["CKc"], m["SKc"] = np.concatenate([C, C], 0), np.concatenate([S, S], 0)
    tpos = (128 * (4 * np.arange(NT)[:, None] + cp) + np.arange(128)[None, :])
    C, S = _rope_tables(tpos.reshape(-1))
    m["CQ"] = np.concatenate([C, C], 0) * np.float32(0.125)
    m["SQ"] = np.concatenate([S, S], 0) * np.float32(0.125)
    n = np.arange(512) - 1
    cs, ce = 16 * n, 16 * n + 31
    ss = 64 * np.arange(128)
    ov = ((cs[:, None] < ss[None, :] + 64) & (ce[:, None] >= ss[None, :]) & (n[:, None] >= 0)).astype(np.float32)
    m["ovl"] = np.ascontiguousarray(ov.reshape(4, 128, 128).transpose(1, 0, 2))
    mt = (np.arange(NT) // 4)
    mm = mt[:, None] * 128 + np.arange(128)[None, :]
    nn = mm - 1
    okc = (nn[:, None, :] >= 0) & (16 * nn[:, None, :] + 31 <= tpos[:, :, None])
    m["CB"] = np.ascontiguousarray(np.where(okc, 0.0, NEG).astype(np.float32).transpose(1, 0, 2))
    blk = np.arange(128)
    jq = tpos // 64
    force = (blk[None, None, :] == jq[:, :, None]) | (blk[None, None, :] == 0)
    valid = (64 * blk[None, None, :] <= tpos[:, :, None])
    m["VAL"] = np.ascontiguousarray((valid & ~force).astype(np.float32).transpose(1, 0, 2))
    m["ADDC"] = np.ascontiguousarray(np.where(force, 1e4, np.where(valid, 0.0, -1.0)).astype(np.float32).transpose(1, 0, 2))
    t = np.arange(128)[:, None]
    p = np.arange(128)[None, :]
    caus = np.where(p <= t, 0.0, NEG).astype(np.float32)
    anti = np.where(p > t, 0.0, NEG).astype(np.float32)
    cm = np.zeros((128, 4, 128), np.float32)
    for r in range(4):
        cm[:, r, :] = 0.0 if r < cp else (caus if r == cp else NEG)
    m["CM"] = cm
    wm = np.zeros((128, 8, 128), np.float32)
    for r in range(8):
        dk = cp + 4 - r
        wm[:, r, :] = NEG if (dk < 0 or dk > 4) else (caus if dk == 0 else (anti if dk == 4 else 0.0))
    m["WMT"] = wm
    m["tri"] = (np.arange(128)[:, None] <= np.arange(128)[None, :]).astype(np.float32)
    m["I4"] = np.tile(np.eye(128, dtype=np.float32), (1, 4))
    oh = np.zeros((128, 4), np.float32)
    oh[:, cp] = 1.0
    m["onehot"] = oh
    _TAB_CACHE[cp] = m
    return m


def attn_host_inputs(inp, b, cp):
    m = dict(_const_tables(cp))
    w = inp["w_in"][0]
    pp = np.array([g * 64 + _partner(d) for g in range(2) for d in range(64)])
    kv = lambda s: KV0 + s * 128 + np.arange(128)
    hfc = HF0 + np.arange(512)
    m["w1f"] = np.ascontiguousarray(np.concatenate(
        [w[:, kv(0)], w[:, kv(1)], w[:, kv(2)], w[:, kv(2)[pp]], w[:, kv(4)], w[:, kv(4)[pp]], w[:, hfc]], axis=1))
    m["w1t"] = np.ascontiguousarray(np.concatenate([w[:, kv(3)], w[:, kv(5)], w[:, HI0:HI0 + 512]], axis=1))
    qcols, qpcols = [], []
    for a in range(4):
        for h in (a, 4 + a):
            qcols += [Q0 + h * 64 + d for d in range(64)]
            qpcols += [Q0 + h * 64 + _partner(d) for d in range(64)]
    m["w2f"] = np.ascontiguousarray(np.concatenate(
        [w[:, qcols], w[:, qpcols], w[:, HQ0:HQ0 + 512], w[:, hfc]], axis=1))
    gpad = np.concatenate([w[:, GT0:GT0 + 24], w[:, GT0:GT0 + 24][:, :0].repeat(1, 1)], axis=1)
    w2t = np.zeros((D, 1536), np.float32)
    w2t[:, 0:512] = w[:, HI0:HI0 + 512]
    w2t[:, 512:1024] = w[:, HG0:HG0 + 512]
    w2t[:, 1024:1048] = w[:, GT0:GT0 + 24]
    m["w2t"] = w2t
    pc = np.array([_partner(d) for d in range(64)])
    dle = lambda w_: np.ascontiguousarray(w_.reshape(32, 64, 64).transpose(1, 0, 2).reshape(64, 2048))
    m["wck"] = dle(inp["w_cmp_k"][0])
    m["wckp"] = dle(inp["w_cmp_k"][0][:, pc])
    m["wcv"] = dle(inp["w_cmp_v"][0])
    pT = np.ascontiguousarray(inp["cmp_pos"][0].T)
    m["posT"] = np.concatenate([pT, pT], 0)
    m["lbl"] = np.ascontiguousarray(inp["hg_lb_logits"])
    m["g_attn"] = np.ascontiguousarray(inp["attn_norm"][0])
    m["g_hg4"] = np.ascontiguousarray(np.tile(inp["hg_norm"][0], 4))
    m["w_mg"] = np.ascontiguousarray(w[:, MG0:MG0 + 2048])
    m["w_brn"] = np.ascontiguousarray(inp["w_br_nsa"][0])
    m["w_brh"] = np.ascontiguousarray(inp["w_br_hg"][0])
    m["w_out"] = np.ascontiguousarray(inp["w_out"][0])
    return m


def norm_transpose_group(k, c, W, src_dram, row0, hT, R_hT):
    def s1(tt):
        b = tt % 2
        k.dma("sp", W.xt[b], src_dram[row0 + tt * 128: row0 + (tt + 1) * 128, :], writes=[W.R_xt[b]])
        rstd, R_r = rms_rstd(k, c, W.xt[b], W.R_xt[b], W.scr, W.R_scr, D, "an")
        k.op("dve", lambda e: e.scalar_tensor_tensor(
            out=W.hb[b], in0=W.xt[b], scalar=rstd, in1=W.gA, op0=ALU.mult, op1=ALU.mult),
            reads=[W.R_xt[b], R_r, W.R_gA], writes=[W.R_hb[b]])
        pb = c.psum[b].bitcast(BF16)
        k.op("pe", [(lambda e, i=i: e.transpose(out=pb[:, i * 128:(i + 1) * 128],
                                                in_=W.hb[b][:, i * 128:(i + 1) * 128], identity=c.identb))
                    for i in range(8)], reads=[W.R_hb[b], c.R_const], writes=[c.R_ps[b]])

    def s2(tt):
        b = tt % 2
        pb = c.psum[b].bitcast(BF16)
        k.op("act", lambda e: e.activation(out=hT[:, :, tt * 128:(tt + 1) * 128],
                                           in_=pb.rearrange("p (a b) -> p a b", a=8), func=AF.Copy),
             reads=[c.R_ps[b]], writes=[R_hT])
    s1(0)
    s1(1)
    s2(0)
    s1(2)
    s2(1)
    s1(3)
    s2(2)
    s2(3)


def f_front(k, c, W, fl_ps, R_fl, hd):
    u, a, bq, lk, L, RF = W.sets[hd % 2]
    k.op("act", lambda e: e.activation(out=u, in_=fl_ps, func=AF.Exp, scale=-1.0), reads=[R_fl], writes=[RF])
    k.op("act", lambda e: e.activation(out=a, in_=u, func=AF.Ln, scale=c.lbv[:, hd:hd + 1], bias=c.one_col[:, 0:1]),
         reads=[RF, c.R_const], writes=[RF])
    k.op("act", lambda e: e.activation(out=bq, in_=u, func=AF.Ln, bias=c.one_col[:, 0:1]), reads=[RF, c.R_const], writes=[RF])
    k.op("dve", lambda e: e.scalar_tensor_tensor(out=lk, in0=fl_ps, scalar=-1.0, in1=bq, op0=ALU.mult, op1=ALU.subtract),
         reads=[R_fl, RF], writes=[RF])
    for tt in range(4):
        sl = slice(tt * 128, (tt + 1) * 128)
        k.op("dve", lambda e, sl=sl: e.tensor_tensor_scan(out=L[:, sl], data0=a[:, sl], data1=bq[:, sl], initial=0.0,
                                                          op0=ALU.add, op1=ALU.subtract), reads=[RF], writes=[RF])
    k.op("pool", lambda e: e.tensor_tensor(out=lk, in0=lk, in1=L, op=ALU.subtract), reads=[RF], writes=[RF])


def f_back(k, c, W, hd, H=None):
    u, a, bq, lk, L, RF = W.sets[hd % 2]
    W_, W = W, (H if H is not None else W)
    Lr = L.rearrange("p (t x) -> p t x", t=4)
    rcol, ecol = Lr[:, :, 63], Lr[:, :, 127]
    k.op("dve", lambda e: e.tensor_scalar(out=W.rb[:, hd, :], in0=rcol, scalar1=c.l1mlb[:, hd:hd + 1], scalar2=None, op0=ALU.add),
         reads=[RF, c.R_const], writes=[W.R_cols])
    k.op("dve", lambda e: e.tensor_scalar(out=W.negr[:, hd, :], in0=rcol, scalar1=-1.0, scalar2=None, op0=ALU.mult),
         reads=[RF], writes=[W.R_cols])
    k.op("dve", lambda e: e.tensor_tensor(out=W.dl[:, hd, :], in0=ecol, in1=rcol, op=ALU.subtract), reads=[RF], writes=[W.R_cols])
    k.op("act", lambda e: e.activation(out=W.c1[:, hd, :], in_=ecol, func=AF.Exp), reads=[RF], writes=[W.R_cols])
    k.op("act", lambda e: e.activation(out=W.c2[:, hd, :], in_=W.dl[:, hd, :], func=AF.Exp), reads=[W.R_cols], writes=[W.R_cols])
    k.op("act", lambda e: e.activation(out=W.er[:, hd, :], in_=rcol, func=AF.Exp), reads=[RF], writes=[W.R_cols])
    for tt in range(4):
        sl = slice(tt * 128, (tt + 1) * 128)
        k.op("act", lambda e, sl=sl, tt=tt: e.activation(out=W.kT[:, hd, sl], in_=lk[:, sl], func=AF.Exp, bias=W.rb[:, hd, tt:tt + 1]),
             reads=[RF, W.R_cols], writes=[W.R_kT])


def setup_lb(k, c, ar):
    Dr = c.D
    c.lbv = ar.al([128, 4], F32)
    c.l1mlb = ar.al([128, 4], F32)
    c.one_col = ar.al([128, 1], F32)
    c.ones128 = ar.al([128, 128], F32)
    l0 = ar.al([128, 4], F32)
    l1 = ar.al([128, 4], F32)
    R = c.R_const
    k.dma("sp", l0, Dr["lbl"][0].rearrange("(h p) -> p h", p=128), writes=[R], allow_slow_non_contiguous=True)
    k.dma("sp", l1, Dr["lbl"][1].rearrange("(h p) -> p h", p=128), writes=[R], allow_slow_non_contiguous=True)
    k.op("dve", lambda e: e.memset(c.one_col, 1.0), writes=[R])
    k.op("dve", lambda e: e.memset(c.ones128, 1.0), writes=[R])
    k.op("dve", lambda e: e.tensor_tensor(out=l1, in0=l1, in1=l0, op=ALU.subtract), reads=[R], writes=[R])
    k.op("act", lambda e: e.activation(out=l0, in_=l1, func=AF.Exp), reads=[R], writes=[R])
    k.op("dve", lambda e: e.tensor_scalar(out=l0, in0=l0, scalar1=1.0, scalar2=None, op0=ALU.add), reads=[R], writes=[R])
    k.op("dve", lambda e: e.reciprocal(out=c.lbv, in_=l0), reads=[R], writes=[R])
    k.op("act", lambda e: e.activation(out=l0, in_=l0, func=AF.Ln), reads=[R], writes=[R])
    k.op("dve", lambda e: e.tensor_tensor(out=c.l1mlb, in0=l1, in1=l0, op=ALU.subtract), reads=[R], writes=[R])


class WS:
    pass


def alloc_hg_ws(k, ar, W, nsets=1):
    W.sets = []
    for si in range(nsets):
        blk = ar.al([128, 5, 512], F32)
        W.sets.append(tuple(blk[:, i, :] for i in range(5)) + (k.res("fchain%d" % si),))
        if si == 0:
            W.ab = blk[:, 1:3, :].rearrange("p a b -> p (a b)")
    if nsets == 1:
        W.sets.append(W.sets[0])
    W.u, W.a, W.bq, W.lk, W.L, W.R_f = W.sets[0]
    alloc_hslot(k, ar, W, "0")


def alloc_hslot(k, ar, H, tag):
    H.rb, H.negr, H.dl, H.c1, H.c2, H.er = [ar.al([128, 4, 4], F32) for _ in range(6)]
    H.R_cols = k.res("fcols" + tag)
    H.kT = ar.al([128, 4, 512], BF16)
    H.R_kT = k.res("kT" + tag)


def alloc_x_ws(k, c, ar, W, region, scr=None, R_scr=None):
    if region is not None:
        W.xt = [region[:, 0, :].bitcast(F32), region[:, 1, :].bitcast(F32)]
        W.hb = [region[:, 2, 0:1024], region[:, 2, 1024:2048]]
        W.scr = region[:, 3, :].bitcast(F32)
        W.R_scr = k.res("xscr")
    else:
        W.xt = [ar.al([128, D], F32) for _ in range(2)]
        W.hb = [ar.al([128, D], BF16) for _ in range(2)]
        W.scr, W.R_scr = scr, R_scr
    W.R_xt = [k.res("xt0"), k.res("xt1")]
    W.R_hb = [k.res("hb0"), k.res("hb1")]
    W.gA = ar.al([128, D], F32)
    W.R_gA = k.res("gA")
    k.dma("sp", W.gA, c.D["g_attn"].partition_broadcast(128), writes=[W.R_gA])


def phase_p1(k, c, ar, St):
    Dr = c.D
    m0 = ar.mark()
    W = WS()
    alloc_x_ws(k, c, ar, W, c.oT_hg)
    w1f, w1t = c.R32[:, :, 0:1280], c.R32[:, :, 1280:2048]
    R_w1 = k.res("w1")
    k.dma("pool", w1f, Dr["w1f"].rearrange("(kt p) n -> p kt n", p=128), writes=[R_w1])
    k.dma("pool", w1t, Dr["w1t"].rearrange("(kt p) n -> p kt n", p=128), writes=[R_w1])
    hT = ar.al([128, 8, 512], BF16)
    R_hT = k.res("hT")
    CKg, SKg = ar.al([128, 512], F32), ar.al([128, 512], F32)
    R_rt = k.res("ropetab")
    alloc_hg_ws(k, ar, W, nsets=2)
    t1, t2, R_t12 = W.u, W.a, W.R_f
    vtok = ar.al([128, 4, 512], BF16)
    R_vtok = k.res("vtok")
    ktok = ar.al([128, 4, 128], BF16)
    R_ktok = k.res("ktok")
    Sst = ar.al([128, 4, 128], F32)
    snapacc = ar.al([128, 4, 128], F32)
    R_S, R_snapacc = k.res("S"), k.res("snapacc")
    WC = [ar.al([128, 32, 64], BF16) for _ in range(3)]
    R_WC = k.res("WC")
    posT = ar.al([128, 32], BF16)
    cb = ar.al([128, 4], F32)
    xin = [[ar.al([128, 528], BF16) for _ in range(2)] for _ in range(2)]
    R_xin = [[k.res("xin%d%d" % (a, b)) for b in range(2)] for a in range(2)]
    CKc, SKc = ar.al([128, 32], F32), ar.al([128, 32], F32)
    R_ckc = k.res("ckc")
    VCf = ar.al([128, 512], F32)
    R_VCf = k.res("VCf")
    ctmp = ar.al([128, 4, 32], F32)
    R_ctmp = k.res("ctmp")
    for xi, nm in enumerate(("wck", "wckp", "wcv")):
        for g in range(2):
            k.dma("pool", WC[xi][64 * g:64 * g + 64].rearrange("p l e -> p (l e)"), Dr[nm], writes=[R_WC])
    k.dma("pool", posT, Dr["posT"], writes=[R_WC])
    k.op("dve", lambda e: e.memset(Sst, 0.0), writes=[R_S])
    k.op("dve", lambda e: e.memset(St.VsA[:, :, :, 64:65], 1.0), writes=[St.R_VsA])
    k.op("dve", lambda e: e.memset(St.VwA[:, :, :, 64:65], 1.0), writes=[St.R_VwA])
    for a in range(2):
        k.op("dve", lambda e, a=a: e.memset(xin[a][0][:, 0:16], 0.0), writes=[R_xin[a][0]])
    p6 = c.psum[6]
    fns = []
    for xi in range(3):
        for g in range(2):
            for l in range(32):
                fns.append(lambda e, xi=xi, g=g, l=l: e.matmul(p6[64 * g:64 * g + 64, xi:xi + 1], lhsT=WC[xi][64 * g:64 * g + 64, l, :],
                                                               rhs=posT[64 * g:64 * g + 64, l:l + 1], start=(l == 0), stop=(l == 31)))
    k.op("pe", fns, reads=[R_WC], writes=[c.R_ps[6]])
    k.op("dve", lambda e: e.tensor_copy(out=cb[:, 0:3], in_=p6[:, 0:3]), reads=[c.R_ps[6]], writes=[R_WC])

    NG = c.n_groups
    Hs = [W, W]
    vtoks, R_vtoks = [vtok, vtok], [R_vtok, R_vtok]
    p6b = c.psum[6].bitcast(BF16)

    def fm(ft, bank):
        k.op("pe", [(lambda e, i=i: e.matmul(c.psum[bank], lhsT=w1f[:, i, ft * 128:(ft + 1) * 128], rhs=hT[:, i, :],
                                             start=(i == 0), stop=(i == 7))) for i in range(8)],
             reads=[R_w1, R_hT], writes=[c.R_ps[bank]])

    def A_x(G):
        norm_transpose_group(k, c, W, Dr["xb"], G * 512, hT, R_hT)
        k.dma("sp", CKg, Dr["CK"][:, G * 512:(G + 1) * 512], writes=[R_rt])
        k.dma("sp", SKg, Dr["SK"][:, G * 512:(G + 1) * 512], writes=[R_rt])

    def A_kv(G):
        xb_ = G % 2
        for a in range(2):
            fm(a, 2 + a)
            k.op("act", lambda e, a=a: e.activation(out=xin[a][xb_][:, 16:528], in_=c.psum[2 + a], func=AF.Copy),
                 reads=[c.R_ps[2 + a]], writes=[R_xin[a][xb_]])
            k.op("pool", lambda e, a=a: e.tensor_copy(out=xin[a][1 - xb_][:, 0:16], in_=xin[a][xb_][:, 512:528]),
                 reads=[R_xin[a][xb_]], writes=[R_xin[a][1 - xb_]])
        for which, dst, R_dst in ((0, St.KTs, St.R_KTs), (1, St.KTw, St.R_KTw)):
            fm(2 + 2 * which, 2)
            fm(3 + 2 * which, 3)
            k.op("dve", lambda e: e.tensor_tensor(out=t1, in0=c.psum[2], in1=CKg, op=ALU.mult), reads=[c.R_ps[2], R_rt], writes=[R_t12])
            k.op("dve", lambda e: e.tensor_tensor(out=t2, in0=c.psum[3], in1=SKg, op=ALU.mult), reads=[c.R_ps[3], R_rt, R_t12], writes=[R_t12])
            k.op("pool", lambda e, dst=dst: e.tensor_tensor(out=dst[:, G * 512:(G + 1) * 512], in0=t1, in1=t2, op=ALU.add),
                 reads=[R_t12], writes=[R_dst])

    def A_tok(G):
        vt, R_vt = vtoks[G % 2], R_vtoks[G % 2]
        for tt in range(4):
            tile_ = 4 * G + tt
            k.op("pe", [(lambda e, i=i, tt=tt: e.matmul(c.psum[4][:, 0:256], lhsT=hT[:, i, tt * 128:(tt + 1) * 128], rhs=w1t[:, i, 0:256],
                                                        start=(i == 0), stop=(i == 7))) for i in range(8)],
                 reads=[R_w1, R_hT], writes=[c.R_ps[4]])
            k.op("pe", [(lambda e, i=i, tt=tt: e.matmul(c.psum[5], lhsT=hT[:, i, tt * 128:(tt + 1) * 128], rhs=w1t[:, i, 256:768],
                                                        start=(i == 0), stop=(i == 7))) for i in range(8)],
                 reads=[R_w1, R_hT], writes=[c.R_ps[5]])
            k.op("act", lambda e, tile_=tile_: e.activation(out=St.VsA[:, tile_, :, 0:64],
                                                            in_=c.psum[4][:, 0:128].rearrange("p (g d) -> p g d", g=2), func=AF.Copy),
                 reads=[c.R_ps[4]], writes=[St.R_VsA])
            k.op("act", lambda e, tile_=tile_: e.activation(out=St.VwA[:, tile_, :, 0:64],
                                                            in_=c.psum[4][:, 128:256].rearrange("p (g d) -> p g d", g=2), func=AF.Copy),
                 reads=[c.R_ps[4]], writes=[St.R_VwA])
            k.op("dve", lambda e, tt=tt: e.tensor_copy(out=vt[:, tt, :], in_=c.psum[5]), reads=[c.R_ps[5]], writes=[R_vt])

    def A_conv(G):
        xb_ = G % 2
        fns = []
        for xi in range(3):
            src = xin[0][xb_] if xi < 2 else xin[1][xb_]
            for g in range(2):
                for l in range(32):
                    fns.append(lambda e, xi=xi, g=g, l=l, src=src: e.matmul(
                        p6[64 * g:64 * g + 64, 32 * xi:32 * xi + 32], lhsT=WC[xi][64 * g:64 * g + 64, l, :],
                        rhs=src[64 * g:64 * g + 64, l:l + 497:16], start=(l == 0), stop=(l == 31)))
        k.op("pe", fns, reads=[R_WC, R_xin[0][xb_], R_xin[1][xb_]], writes=[c.R_ps[6]])
        ms = slice(32 * G, 32 * G + 32)
        k.dma("sp", CKc, Dr["CKc"][:, ms], writes=[R_ckc])
        k.dma("sp", SKc, Dr["SKc"][:, ms], writes=[R_ckc])
        k.op("dve", lambda e: e.tensor_scalar(out=ctmp[:, 0, :], in0=p6[:, 0:32], scalar1=cb[:, 0:1], scalar2=None, op0=ALU.add),
             reads=[c.R_ps[6], R_WC], writes=[R_ctmp])
        k.op("dve", lambda e: e.tensor_scalar(out=ctmp[:, 1, :], in0=p6[:, 32:64], scalar1=cb[:, 1:2], scalar2=None, op0=ALU.add),
             reads=[c.R_ps[6], R_WC], writes=[R_ctmp])
        k.op("dve", lambda e: e.tensor_scalar(out=VCf[:, ms], in0=p6[:, 64:96], scalar1=cb[:, 2:3], scalar2=None, op0=ALU.add),
             reads=[c.R_ps[6], R_WC], writes=[R_VCf])
        k.op("pool", lambda e: e.tensor_tensor(out=ctmp[:, 0, :], in0=ctmp[:, 0, :], in1=CKc, op=ALU.mult),
             reads=[R_ctmp, R_ckc], writes=[R_ctmp])
        k.op("pool", lambda e: e.tensor_tensor(out=ctmp[:, 1, :], in0=ctmp[:, 1, :], in1=SKc, op=ALU.mult),
             reads=[R_ctmp, R_ckc], writes=[R_ctmp])
        k.op("pool", lambda e: e.tensor_tensor(out=St.KC[:, ms], in0=ctmp[:, 0, :], in1=ctmp[:, 1, :], op=ALU.add),
             reads=[R_ctmp], writes=[St.R_KC])

    def A_f(G):
        H = Hs[G % 2]

        def front(hd):
            bank = 2 + hd % 2
            fm(6 + hd, bank)
            f_front(k, c, W, c.psum[bank], c.R_ps[bank], hd)
        front(0)
        front(1)
        f_back(k, c, W, 0, H)
        front(2)
        f_back(k, c, W, 1, H)
        front(3)
        f_back(k, c, W, 2, H)
        f_back(k, c, W, 3, H)

    def B_step(G, tt):
        H = Hs[G % 2]
        vt, R_vt = vtoks[G % 2], R_vtoks[G % 2]
        sl = slice(tt * 128, (tt + 1) * 128)
        k.op("pe", [(lambda e, hd=hd: e.transpose(out=p6b[:, hd * 128:(hd + 1) * 128], in_=H.kT[:, hd, sl], identity=c.identb))
                    for hd in range(4)], reads=[H.R_kT, c.R_const], writes=[c.R_ps[6]])
        k.op("act", lambda e: e.activation(out=ktok, in_=p6b[:, 0:512].rearrange("p (h x) -> p h x", h=4), func=AF.Copy),
             reads=[c.R_ps[6]], writes=[R_ktok])
        k.op("pe", [(lambda e, hd=hd: e.matmul(c.psum[7][:, hd * 128:(hd + 1) * 128], lhsT=ktok[:, hd, :],
                                               rhs=vt[:, tt, hd * 128:(hd + 1) * 128], start=True, stop=True))
                    for hd in range(4)], reads=[R_ktok, R_vt], writes=[c.R_ps[7]])
        Sf, Af = Sst.rearrange("p h x -> p (h x)"), snapacc.rearrange("p h x -> p (h x)")
        if tt == 0:
            k.op("dve", lambda e: e.tensor_scalar(out=Af, in0=Sf, scalar1=c.onehot[:, 0:1], scalar2=None, op0=ALU.mult),
                 reads=[R_S, c.R_const], writes=[R_snapacc])
        else:
            k.op("dve", lambda e: e.scalar_tensor_tensor(out=Af, in0=Sf, scalar=c.onehot[:, tt:tt + 1], in1=Af,
                                                         op0=ALU.mult, op1=ALU.add),
                 reads=[R_S, c.R_const, R_snapacc], writes=[R_snapacc])
        for hd in range(4):
            k.op("dve", lambda e, hd=hd: e.tensor_scalar(out=Sst[:, hd, :], in0=Sst[:, hd, :], scalar1=H.c1[:, hd, tt:tt + 1],
                                                         scalar2=None, op0=ALU.mult),
                 reads=[R_S, H.R_cols], writes=[R_S])
            k.op("dve", lambda e, hd=hd: e.scalar_tensor_tensor(
                out=Sst[:, hd, :], in0=c.psum[7][:, hd * 128:(hd + 1) * 128], scalar=H.c2[:, hd, tt:tt + 1], in1=Sst[:, hd, :],
                op0=ALU.mult, op1=ALU.add), reads=[c.R_ps[7], R_S, H.R_cols], writes=[R_S])
        if tt == 3:
            k.op("act", lambda e: e.activation(out=St.SNAP[:, G, :, :], in_=snapacc, func=AF.Copy), reads=[R_snapacc], writes=[St.R_SNAP])

    for G in range(NG + 1):
        if G < NG:
            A_x(G)
        if G >= 1:
            B_step(G - 1, 0)
            B_step(G - 1, 1)
        if G < NG:
            A_kv(G)
        if G >= 1:
            B_step(G - 1, 2)
            B_step(G - 1, 3)
        if G < NG:
            A_tok(G)
            A_conv(G)
            A_f(G)
    k.op("dve", lambda e: e.memset(St.VCA[:, :, :, 64:65], 1.0), writes=[St.R_VCA])
    for g in range(2):
        k.dma("pool", St.VCA[:, :, g, 65:193], Dr["ovl"], writes=[St.R_VCA])
    pw = c.psum[6]
    k.op("pe", [(lambda e, mt=mt: e.transpose(out=pw[:, mt * 128:(mt + 1) * 128], in_=VCf[:, mt * 128:(mt + 1) * 128], identity=c.identf))
                for mt in range(4)], reads=[R_VCf, c.R_const], writes=[c.R_ps[6]])
    for mt in range(4):
        k.op("act", lambda e, mt=mt: e.activation(out=St.VCA[:, mt, :, 0:64],
                                                  in_=pw[:, mt * 128:(mt + 1) * 128].rearrange("p (g d) -> p g d", g=2), func=AF.Copy),
             reads=[c.R_ps[6]], writes=[St.R_VCA])
    k.op("dve", lambda e: e.memset(St.VCA[0:1, 0, :, :], 0.0), writes=[St.R_VCA])
    ar.release(m0)


def phase_p2pre(k, c, ar, St):
    Dr = c.D
    m0 = ar.mark()
    W = WS()
    alloc_hg_ws(k, ar, W, nsets=2)
    alloc_x_ws(k, c, ar, W, None, scr=W.ab, R_scr=W.R_f)
    hT = ar.al([128, 8, 512], BF16)
    R_hT = k.res("hT2")
    wch = [c.R32f[:, 8192 + b * 4096: 8192 + (b + 1) * 4096].rearrange("p (a b) -> p a b", a=8) for b in range(2)]
    R_wch = [k.res("wch%d" % i) for i in range(2)]
    wgt = ar.al([128, 8, 32], BF16)
    R_wgt = k.res("wgt")
    wst = dict(n=0)
    CQg, SQg = ar.al([128, 512], F32), ar.al([128, 512], F32)
    R_rt = k.res("ropetabq")
    t1, t2, R_t12 = W.u, W.a, W.R_f
    qT = ar.al([128, 4, 512], BF16)
    R_qT = k.res("qTh")
    e1, R_e1 = W.u, W.R_f
    vtok = ar.al([128, 512], BF16)
    R_vtok = k.res("vtok2")
    sgt = ar.al([128, 512], F32)
    R_sgt = k.res("sgt")
    AT = ar.al([128, 4, 128], BF16)
    R_AT = k.res("AT")
    Sp = ar.al([128, 4, 128], BF16)
    R_Sp = k.res("Sp")
    gnt = ar.al([128, 512], F32)
    R_gnt = k.res("gnt")
    o1, o2, R_o = W.bq, W.a, W.R_f
    yb = ar.al([128, 512], BF16)
    R_yb = k.res("yb")
    hs = ar.al([128, 16], F32)
    R_hs = k.res("hs")
    k.dma("pool", wgt, Dr["w2t"][:, 1024:1056].rearrange("(kt p) n -> p kt n", p=128), writes=[R_wgt])
    k.dma("sp", gnt, Dr["g_hg4"].partition_broadcast(128), writes=[R_gnt])

    def wload(src, c0, n=512):
        b = wst["n"] % 2
        wst["n"] += 1
        k.dma("pool", wch[b][:, :, 0:n], src[:, c0:c0 + n].rearrange("(kt p) n -> p kt n", p=128), writes=[R_wch[b]])
        return wch[b], R_wch[b]

    for go in range(NT // 4):
        tok = slice(go * 512, (go + 1) * 512)
        norm_transpose_group(k, c, W, Dr["xo"], go * 512, hT, R_hT)
        k.dma("sp", CQg, Dr["CQ"][:, tok], writes=[R_rt])
        k.dma("sp", SQg, Dr["SQ"][:, tok], writes=[R_rt])

        def fm(wt, R_wt, j, bank):
            k.op("pe", [(lambda e, i=i: e.matmul(c.psum[bank], lhsT=wt[:, i, j * 128:(j + 1) * 128], rhs=hT[:, i, :],
                                                 start=(i == 0), stop=(i == 7))) for i in range(8)],
                 reads=[R_wt, R_hT], writes=[c.R_ps[bank]])
        wq, R_wq = wload(Dr["w2f"], 0)
        wqp, R_wqp = wload(Dr["w2f"], 512)
        for a in range(4):
            fm(wq, R_wq, a, 2)
            fm(wqp, R_wqp, a, 3)
            k.op("dve", lambda e: e.tensor_tensor(out=t1, in0=c.psum[2], in1=CQg, op=ALU.mult), reads=[c.R_ps[2], R_rt], writes=[R_t12])
            k.op("dve", lambda e: e.tensor_tensor(out=t2, in0=c.psum[3], in1=SQg, op=ALU.mult), reads=[c.R_ps[3], R_rt, R_t12], writes=[R_t12])
            k.op("pool", lambda e, a=a: e.tensor_tensor(out=c.QT[:, 4 * go:4 * go + 4, a, :], in0=t1.rearrange("p (i t) -> p i t", i=4),
                                                        in1=t2.rearrange("p (i t) -> p i t", i=4), op=ALU.add), reads=[R_t12], writes=[c.R_QT])
        whq, R_whq = wload(Dr["w2f"], 1024)
        whf, R_whf = wload(Dr["w2f"], 1536)
        def front(hd):
            bank = 2 + hd % 2
            fm(whf, R_whf, hd, bank)
            f_front(k, c, W, c.psum[bank], c.R_ps[bank], hd)

        def back(hd):
            f_back(k, c, W, hd)
            su, sa, sbq, slk, sL, sRF = W.sets[hd % 2]
            fm(whq, R_whq, hd, 6)
            for tt in range(4):
                sl = slice(tt * 128, (tt + 1) * 128)
                k.op("act", lambda e, sl=sl, tt=tt: e.activation(out=su[:, sl], in_=sL[:, sl], func=AF.Exp, bias=W.negr[:, hd, tt:tt + 1]),
                     reads=[sRF, W.R_cols], writes=[sRF])
            k.op("dve", lambda e: e.tensor_tensor(out=qT[:, hd, :], in0=c.psum[6], in1=su, op=ALU.mult),
                 reads=[c.R_ps[6], sRF], writes=[R_qT])
        front(0)
        front(1)
        back(0)
        front(2)
        back(1)
        front(3)
        back(2)
        back(3)
        whi, R_whi = wload(Dr["w2t"], 0)
        whg, R_whg = wload(Dr["w2t"], 512)
        for tt in range(4):
            i_own = 4 * go + tt
            sl = slice(tt * 128, (tt + 1) * 128)
            for (wt, R_wt, n, bank) in ((whi, R_whi, 512, 4), (whg, R_whg, 512, 5), (wgt, R_wgt, 32, 6)):
                k.op("pe", [(lambda e, i=i, wt=wt, n=n, bank=bank: e.matmul(c.psum[bank][:, 0:n], lhsT=hT[:, i, sl], rhs=wt[:, i, 0:n],
                                                                            start=(i == 0), stop=(i == 7))) for i in range(8)],
                     reads=[R_wt, R_hT], writes=[c.R_ps[bank]])
            k.op("dve", lambda e: e.tensor_copy(out=vtok, in_=c.psum[4]), reads=[c.R_ps[4]], writes=[R_vtok])
            k.op("act", lambda e: e.activation(out=sgt, in_=c.psum[5], func=AF.Silu), reads=[c.R_ps[5]], writes=[R_sgt])
            k.op("act", lambda e, i_own=i_own: e.activation(out=c.gsig[:, i_own, :], in_=c.psum[6][:, 0:24], func=AF.Sigmoid),
                 reads=[c.R_ps[6]], writes=[c.R_gsig])
            k.op("pe", [(lambda e, hd=hd: e.matmul(c.psum[7][:, hd * 128:(hd + 1) * 128], lhsT=W.kT[:, hd, sl], rhs=qT[:, hd, sl],
                                                   start=True, stop=True)) for hd in range(4)],
                 reads=[W.R_kT, R_qT], writes=[c.R_ps[7]])
            k.op("dve", lambda e: e.tensor_scalar(out=W.lk, in0=c.psum[7], scalar1=1e30, scalar2=-1e30, op0=ALU.min, op1=ALU.max),
                 reads=[c.R_ps[7], W.R_f], writes=[W.R_f])
            k.op("dve", lambda e: e.tensor_tensor(out=AT, in0=W.lk.rearrange("p (h x) -> p h x", h=4),
                                                  in1=c.tri.unsqueeze(1).broadcast_to([128, 4, 128]), op=ALU.mult),
                 reads=[W.R_f, c.R_const], writes=[R_AT])
            for hd in range(4):
                k.op("act", lambda e, hd=hd, tt=tt, i_own=i_own: e.activation(out=Sp[:, hd, :], in_=St.SNAP[:, i_own, hd, :], func=AF.Copy,
                                                                              scale=W.er[:, hd, tt:tt + 1]),
                     reads=[St.R_SNAP, W.R_cols], writes=[R_Sp])
            fns = []
            for hd in range(4):
                fns.append(lambda e, hd=hd, tt=tt: e.matmul(c.psum[4][:, hd * 128:(hd + 1) * 128], lhsT=AT[:, hd, :],
                                                            rhs=vtok[:, hd * 128:(hd + 1) * 128], start=True, stop=False))
                fns.append(lambda e, hd=hd: e.matmul(c.psum[4][:, hd * 128:(hd + 1) * 128], lhsT=qT[:, hd, sl],
                                                     rhs=Sp[:, hd, :], start=False, stop=True))
            k.op("pe", fns, reads=[R_AT, R_vtok, R_qT, R_Sp], writes=[c.R_ps[4]])
            for hd in range(4):
                k.op("act", lambda e, hd=hd: e.activation(out=o2[:, hd * 128:(hd + 1) * 128], in_=c.psum[4][:, hd * 128:(hd + 1) * 128],
                                                          func=AF.Square, accum_out=hs[:, hd:hd + 1]),
                     reads=[c.R_ps[4]], writes=[R_o, R_hs])
            k.op("act", lambda e: e.activation(out=hs[:, 4:8], in_=hs[:, 0:4], func=AF.Sqrt, scale=1.0 / 128, bias=c.epsb[:, 0:1]),
                 reads=[R_hs, c.R_const], writes=[R_hs])
            k.op("dve", lambda e: e.reciprocal(out=hs[:, 8:12], in_=hs[:, 4:8]), reads=[R_hs], writes=[R_hs])
            k.op("dve", lambda e: e.tensor_tensor(out=o1, in0=c.psum[4], in1=gnt, op=ALU.mult), reads=[c.R_ps[4], R_gnt, R_o], writes=[R_o])
            k.op("pool", lambda e, tt=tt: e.tensor_tensor(out=o1, in0=o1, in1=sgt, op=ALU.mult), reads=[R_o, R_sgt], writes=[R_o])
            k.op("dve", lambda e: e.tensor_tensor(out=yb.rearrange("p (h x) -> p h x", h=4), in0=o1.rearrange("p (h x) -> p h x", h=4),
                                                  in1=hs[:, 8:12].unsqueeze(2).broadcast_to([128, 4, 128]), op=ALU.mult),
                 reads=[R_o, R_hs], writes=[R_yb])
            p6b = c.psum[6].bitcast(BF16)
            k.op("pe", [(lambda e, hd=hd: e.transpose(out=p6b[:, hd * 128:(hd + 1) * 128], in_=yb[:, hd * 128:(hd + 1) * 128], identity=c.identb))
                        for hd in range(4)], reads=[R_yb, c.R_const], writes=[c.R_ps[6]])
            k.op("act", lambda e, i_own=i_own: e.activation(out=c.oT_hg[:, :, i_own * 128:(i_own + 1) * 128],
                                                            in_=p6b[:, 0:512].rearrange("p (h x) -> p h x", h=4), func=AF.Copy),
                 reads=[c.R_ps[6]], writes=[c.R_oThg])
    ar.release(m0)


def phase_nsa(k, c, ar, St):
    Dr = c.D
    m0 = ar.mark()
    TINY = 1e-30
    CBi = [ar.al([128, 128], BF16) for _ in range(2)]
    VALi = [ar.al([128, 128], F32) for _ in range(2)]
    ADDCi = [ar.al([128, 128], F32) for _ in range(2)]
    R_tab = [k.res("nsatab%d" % i) for i in range(2)]
    WMT = ar.al([128, 8, 128], BF16)
    CM = ar.al([128, 4, 128], BF16)
    R_cst = k.res("nsacst")
    k.dma("pool", WMT, Dr["WMT"], writes=[R_cst])
    k.dma("pool", CM, Dr["CM"], writes=[R_cst])
    PT = [ar.al([128, 512], BF16) for _ in range(3)]
    R_PT = [k.res("PT%d" % i) for i in range(3)]
    Uc = ar.al([128, 4, 193], F32)
    R_Uc = k.res("Uc")
    Os = ar.al([128, 4, 65], F32)
    Ow = ar.al([128, 4, 65], F32)
    R_Os, R_Ow = k.res("Os"), k.res("Ow")
    score, sc2, imp = ar.al([128, 128], F32), ar.al([128, 128], F32), ar.al([128, 128], F32)
    R_sel = k.res("sel")
    selb = ar.al([128, 128], BF16)
    R_selb = k.res("selb")
    bd = ar.al([128, 4, 128], BF16)
    R_bd = k.res("bd")
    selX = ar.al([128, 128, 64], BF16)
    R_selX = k.res("selX")
    cols = ar.al([128, 64], F32)
    R_cols = k.res("nsacols")
    acc, tmp = ar.al([128, 4, 64], F32), ar.al([128, 4, 64], F32)
    R_acc = k.res("nsaacc")
    onsa = ar.al([128, 2, 4, 64], BF16)
    R_onsa = k.res("onsa")
    st = dict(s=0, p=0)
    pO_s, pO_w = c.psum[3][:, 0:260], c.psum[4][:, 0:260]
    pU = [c.psum[5], c.psum[6]]

    pend = []

    def flush_pv(keep=0):
        while len(pend) > keep:
            pend.pop(0)()

    def unit(KT, R_KT, kt_slice, QTg, g, bias, Vaug, R_V, outs, R_outs):
        sb = st["s"] % 3
        st["s"] += 1
        pb = st["p"] % 3
        st["p"] += 1
        S = c.psum[sb]
        fns = [lambda e: e.matmul(S, lhsT=KT[64 * g:64 * g + 64, kt_slice], rhs=QTg, start=True, stop=(bias is None))]
        rd = [R_KT, c.R_QT]
        if bias is not None:
            bl, R_bl = bias
            fns.append(lambda e: e.matmul(S, lhsT=bl, rhs=c.I4, start=False, stop=True))
            rd += [R_bl, c.R_const]
        k.op("pe", fns, reads=rd, writes=[c.R_ps[sb]])
        k.op("act", lambda e: e.activation(out=PT[pb], in_=S, func=AF.Exp), reads=[c.R_ps[sb]], writes=[R_PT[pb]])

        def pv():
            k.op("pe", [(lambda e, a=a: e.matmul(outs[a], lhsT=PT[pb][:, a * 128:(a + 1) * 128], rhs=Vaug, start=False, stop=False,
                                                 skip_group_check=True)) for a in range(4)],
                 reads=[R_PT[pb], R_V], writes=R_outs)
        pend.append(pv)
        flush_pv(keep=2)

    for i in range(c.n_blocks):
        tb = i % 2
        k.dma("pool", CBi[tb], Dr["CB"][:, i, :], writes=[R_tab[tb]])
        k.dma("sp", VALi[tb], Dr["VAL"][:, i, :], writes=[R_tab[tb]])
        k.dma("sp", ADDCi[tb], Dr["ADDC"][:, i, :], writes=[R_tab[tb]])
        for g in range(2):
            QTg = c.QT[64 * g:64 * g + 64, i, :, :].rearrange("p a t -> p (a t)")
            nmt = i // 4 + 1
            k.op("dve", lambda e: e.memset(pU[0], 0.0), writes=[c.R_ps[5]])
            k.op("dve", lambda e: e.memset(pU[1], 0.0), writes=[c.R_ps[6]])
            outsU = [pU[a // 2][:, (a % 2) * 193:(a % 2) * 193 + 193] for a in range(4)]
            for mt in range(nmt):
                bias = (CBi[tb], R_tab[tb]) if mt == nmt - 1 else None
                unit(St.KC, St.R_KC, slice(mt * 128, (mt + 1) * 128), QTg, g, bias, St.VCA[:, mt, g, :], St.R_VCA, outsU, [c.R_ps[5], c.R_ps[6]])
            flush_pv()
            k.op("act", lambda e: e.activation(out=Uc[:, 0:2, :], in_=pU[0][:, 0:386].rearrange("p (a x) -> p a x", a=2), func=AF.Copy),
                 reads=[c.R_ps[5]], writes=[R_Uc])
            k.op("act", lambda e: e.activation(out=Uc[:, 2:4, :], in_=pU[1][:, 0:386].rearrange("p (a x) -> p a x", a=2), func=AF.Copy),
                 reads=[c.R_ps[6]], writes=[R_Uc])
            k.op("dve", lambda e: e.memset(c.psum[4], 0.0), writes=[c.R_ps[4]])
            outsW = [pO_w[:, a * 65:(a + 1) * 65] for a in range(4)]
            for r in range(8):
                kt = 4 * i - 4 + r
                if kt < 0:
                    continue
                unit(St.KTw, St.R_KTw, slice(kt * 128, (kt + 1) * 128), QTg, g, (WMT[:, r, :], R_cst), St.VwA[:, kt, g, :], St.R_VwA, outsW, [c.R_ps[4]])
            zc, rzc = cols[:, 0:4], cols[:, 4:8]
            k.op("dve", lambda e: e.tensor_scalar(out=zc, in0=Uc[:, :, 64], scalar1=TINY, scalar2=None, op0=ALU.max), reads=[R_Uc], writes=[R_cols])
            k.op("dve", lambda e: e.reciprocal(out=rzc, in_=zc), reads=[R_cols], writes=[R_cols])
            k.op("dve", lambda e: e.tensor_scalar(out=imp, in0=Uc[:, 0, 65:193], scalar1=rzc[:, 0:1], scalar2=None, op0=ALU.mult),
                 reads=[R_Uc, R_cols], writes=[R_sel])
            for a in range(1, 4):
                k.op("dve", lambda e, a=a: e.scalar_tensor_tensor(out=imp, in0=Uc[:, a, 65:193], scalar=rzc[:, a:a + 1], in1=imp,
                                                                  op0=ALU.mult, op1=ALU.add), reads=[R_Uc, R_cols, R_sel], writes=[R_sel])
            k.op("dve", lambda e: e.tensor_tensor(out=score, in0=imp, in1=VALi[tb], op=ALU.mult), reads=[R_sel, R_tab[tb]], writes=[R_sel])
            k.op("dve", lambda e: e.tensor_tensor(out=score, in0=score, in1=ADDCi[tb], op=ALU.add), reads=[R_sel, R_tab[tb]], writes=[R_sel])
            m8a, m8b = cols[:, 8:16], cols[:, 16:24]
            k.op("dve", lambda e: e.max(out=m8a, in_=score), reads=[R_sel], writes=[R_cols])
            k.op("dve", lambda e: e.match_replace(out=sc2, in_to_replace=m8a, in_values=score, imm_value=-1e9), reads=[R_sel, R_cols], writes=[R_sel])
            k.op("dve", lambda e: e.max(out=m8b, in_=sc2), reads=[R_sel], writes=[R_cols])
            k.op("dve", lambda e: e.tensor_scalar(out=selb, in0=score, scalar1=m8b[:, 7:8], scalar2=NEG, op0=ALU.is_lt, op1=ALU.mult),
                 reads=[R_sel, R_cols], writes=[R_selb])
            for r in range(4):
                kt = 4 * i + r
                k.op("dve", lambda e, r=r, kt=kt: e.tensor_tensor(
                    out=bd[:, r, :].rearrange("p (b x) -> p b x", b=2), in0=CM[:, r, :].rearrange("p (b x) -> p b x", b=2),
                    in1=selb[:, 2 * kt:2 * kt + 2].unsqueeze(2).broadcast_to([128, 2, 64]), op=ALU.add),
                    reads=[R_cst, R_selb], writes=[R_bd])
            if i > 0:
                nbk = 8 * i
                k.op("pool", lambda e, nbk=nbk: e.tensor_copy(out=selX[:, 0:nbk, :], in_=selb[:, 0:nbk].unsqueeze(2).broadcast_to([128, nbk, 64])),
                     reads=[R_selb], writes=[R_selX])
            k.op("dve", lambda e: e.memset(c.psum[3], 0.0), writes=[c.R_ps[3]])
            outsS = [pO_s[:, a * 65:(a + 1) * 65] for a in range(4)]
            for kt in range(4 * i + 4):
                if kt < 4 * i:
                    bl = selX[:, 2 * kt:2 * kt + 2, :].rearrange("p b x -> p (b x)")
                    bias = (bl, R_selX)
                else:
                    bias = (bd[:, kt - 4 * i, :], R_bd)
                unit(St.KTs, St.R_KTs, slice(kt * 128, (kt + 1) * 128), QTg, g, bias, St.VsA[:, kt, g, :], St.R_VsA, outsS, [c.R_ps[3]])
            flush_pv()
            k.op("act", lambda e: e.activation(out=Os, in_=pO_s.rearrange("p (a x) -> p a x", a=4), func=AF.Copy), reads=[c.R_ps[3]], writes=[R_Os])
            k.op("act", lambda e: e.activation(out=Ow, in_=pO_w.rearrange("p (a x) -> p a x", a=4), func=AF.Copy), reads=[c.R_ps[4]], writes=[R_Ow])
            gs = c.gsig[:, i, 12 * g:12 * g + 12].rearrange("p (a x) -> p a x", a=4)
            zs, zw, cfc, cfs, cfw = cols[:, 24:28], cols[:, 28:32], cols[:, 32:36], cols[:, 36:40], cols[:, 40:44]
            k.op("dve", lambda e: e.tensor_scalar(out=zs, in0=Os[:, :, 64], scalar1=TINY, scalar2=None, op0=ALU.max), reads=[R_Os], writes=[R_cols])
            k.op("dve", lambda e: e.tensor_scalar(out=zw, in0=Ow[:, :, 64], scalar1=TINY, scalar2=None, op0=ALU.max), reads=[R_Ow], writes=[R_cols])
            k.op("dve", lambda e: e.reciprocal(out=zs, in_=zs), reads=[R_cols], writes=[R_cols])
            k.op("dve", lambda e: e.reciprocal(out=zw, in_=zw), reads=[R_cols], writes=[R_cols])
            k.op("dve", lambda e: e.tensor_tensor(out=cfc, in0=rzc, in1=gs[:, :, 0], op=ALU.mult), reads=[R_cols, c.R_gsig], writes=[R_cols])
            k.op("dve", lambda e: e.tensor_tensor(out=cfs, in0=zs, in1=gs[:, :, 1], op=ALU.mult), reads=[R_cols, c.R_gsig], writes=[R_cols])
            k.op("dve", lambda e: e.tensor_tensor(out=cfw, in0=zw, in1=gs[:, :, 2], op=ALU.mult), reads=[R_cols, c.R_gsig], writes=[R_cols])
            bc = lambda col: col.unsqueeze(2).broadcast_to([128, 4, 64])
            k.op("dve", lambda e: e.tensor_tensor(out=acc, in0=Uc[:, :, 0:64], in1=bc(cfc), op=ALU.mult), reads=[R_Uc, R_cols], writes=[R_acc])
            k.op("dve", lambda e: e.tensor_tensor(out=tmp, in0=Os[:, :, 0:64], in1=bc(cfs), op=ALU.mult), reads=[R_Os, R_cols, R_acc], writes=[R_acc])
            k.op("pool", lambda e: e.tensor_tensor(out=acc, in0=acc, in1=tmp, op=ALU.add), reads=[R_acc], writes=[R_acc])
            k.op("dve", lambda e: e.tensor_tensor(out=tmp, in0=Ow[:, :, 0:64], in1=bc(cfw), op=ALU.mult), reads=[R_Ow, R_cols, R_acc], writes=[R_acc])
            k.op("pool", lambda e, g=g: e.tensor_tensor(out=onsa[:, g, :, :], in0=acc, in1=tmp, op=ALU.add), reads=[R_acc], writes=[R_onsa])
        p7b = c.psum[7].bitcast(BF16)
        of = onsa.rearrange("p g a d -> p (g a d)")
        k.op("pe", [(lambda e, j=j: e.transpose(out=p7b[:, j * 128:(j + 1) * 128], in_=of[:, j * 128:(j + 1) * 128], identity=c.identb))
                    for j in range(4)], reads=[R_onsa, c.R_const], writes=[c.R_ps[7]])
        k.op("act", lambda e, i=i: e.activation(out=c.oT_nsa[:, :, i * 128:(i + 1) * 128],
                                                in_=p7b[:, 0:512].rearrange("p (j x) -> p j x", j=4), func=AF.Copy),
             reads=[c.R_ps[7]], writes=[c.R_oTnsa])
    ar.release(m0)


def phase_p2c(k, c, ar, X1, R_X1):
    Dr = c.D
    m0 = ar.mark()
    W = WS()
    scr = ar.al([128, D], F32)
    alloc_x_ws(k, c, ar, W, None, scr=scr, R_scr=k.res("scr2c"))
    hT = ar.al([128, 8, 512], BF16)
    R_hT = k.res("hT3")
    wbn, wbh = ar.al([128, 4, D], BF16), ar.al([128, 4, D], BF16)
    R_wb = k.res("wbr")
    wch = [ar.al([128, 8, 512], BF16) for _ in range(2)]
    R_wch = [k.res("wchc%d" % i) for i in range(2)]
    mixT = ar.al([128, 8, 512], BF16)
    R_mixT = k.res("mixT")
    sg1, sg2, mx1 = ar.al([128, 512], F32), ar.al([128, 512], F32), ar.al([128, 512], F32)
    R_sg1, R_sg2, R_mx = k.res("sg1"), k.res("sg2"), k.res("mx1")
    k.dma("pool", wbn, Dr["w_brn"].rearrange("(kt p) n -> p kt n", p=128), writes=[R_wb])
    k.dma("pool", wbh, Dr["w_brh"].rearrange("(kt p) n -> p kt n", p=128), writes=[R_wb])
    for go in range(NT // 4):
        tok = slice(go * 512, (go + 1) * 512)
        for tt in range(4):
            t = 4 * go + tt
            k.dma("sp", X1[:, t, :], Dr["xo"][t * 128:(t + 1) * 128, :], writes=[R_X1[t]])
        norm_transpose_group(k, c, W, Dr["xo"], go * 512, hT, R_hT)
        for hf in range(2):
            k.dma("pool", wch[0], Dr["w_mg"][:, hf * 512:(hf + 1) * 512].rearrange("(kt p) n -> p kt n", p=128), writes=[R_wch[0]])
            k.dma("pool", wch[1], Dr["w_mg"][:, 1024 + hf * 512:1024 + (hf + 1) * 512].rearrange("(kt p) n -> p kt n", p=128), writes=[R_wch[1]])
            for f4 in range(4):
                ft = hf * 4 + f4
                fs = slice(f4 * 128, (f4 + 1) * 128)
                gs_ = slice(ft * 128, (ft + 1) * 128)
                k.op("pe", [(lambda e, i=i: e.matmul(c.psum[2], lhsT=wch[0][:, i, fs], rhs=hT[:, i, :], start=(i == 0), stop=(i == 7)))
                            for i in range(8)], reads=[R_wch[0], R_hT], writes=[c.R_ps[2]])
                k.op("pe", [(lambda e, i=i: e.matmul(c.psum[3], lhsT=wch[1][:, i, fs], rhs=hT[:, i, :], start=(i == 0), stop=(i == 7)))
                            for i in range(8)], reads=[R_wch[1], R_hT], writes=[c.R_ps[3]])
                k.op("pe", [(lambda e, i=i: e.matmul(c.psum[4], lhsT=wbn[:, i, gs_], rhs=c.oT_nsa[:, i, tok], start=(i == 0), stop=(i == 3)))
                            for i in range(4)], reads=[R_wb, c.R_oTnsa], writes=[c.R_ps[4]])
                k.op("pe", [(lambda e, i=i: e.matmul(c.psum[5], lhsT=wbh[:, i, gs_], rhs=c.oT_hg[:, i, tok], start=(i == 0), stop=(i == 3)))
                            for i in range(4)], reads=[R_wb, c.R_oThg], writes=[c.R_ps[5]])
                k.op("act", lambda e: e.activation(out=sg1, in_=c.psum[2], func=AF.Sigmoid), reads=[c.R_ps[2]], writes=[R_sg1])
                k.op("act", lambda e: e.activation(out=sg2, in_=c.psum[3], func=AF.Sigmoid), reads=[c.R_ps[3]], writes=[R_sg2])
                k.op("dve", lambda e: e.tensor_tensor(out=mx1, in0=c.psum[4], in1=sg1, op=ALU.mult), reads=[c.R_ps[4], R_sg1], writes=[R_mx])
                k.op("dve", lambda e: e.tensor_tensor(out=sg2, in0=c.psum[5], in1=sg2, op=ALU.mult), reads=[c.R_ps[5], R_sg2], writes=[R_sg2])
                k.op("pool", lambda e, ft=ft: e.tensor_tensor(out=mixT[:, ft, :], in0=mx1, in1=sg2, op=ALU.add),
                     reads=[R_mx, R_sg2], writes=[R_mixT])
        for hf in range(2):
            k.dma("pool", wch[hf], Dr["w_out"][:, hf * 512:(hf + 1) * 512].rearrange("(kt p) n -> p kt n", p=128), writes=[R_wch[hf]])
        for tt in range(4):
            t = 4 * go + tt
            for hf in range(2):
                bank = 6 + hf
                k.op("pe", [(lambda e, i=i: e.matmul(c.psum[bank], lhsT=mixT[:, i, tt * 128:(tt + 1) * 128], rhs=wch[hf][:, i, :],
                                                     start=(i == 0), stop=(i == 7))) for i in range(8)],
                     reads=[R_mixT, R_wch[hf]], writes=[c.R_ps[bank]])
                xs = X1[:, t, hf * 512:(hf + 1) * 512]
                k.op("dve", lambda e, xs=xs, bank=bank: e.tensor_tensor(out=xs, in0=c.psum[bank], in1=xs, op=ALU.add),
                     reads=[c.R_ps[bank], R_X1[t]], writes=[R_X1[t]])
    ar.release(m0)


def phase_attn(k, c, ar, stage):
    St = WS()
    St.KTs, St.KTw = ar.al([128, SEQ], BF16), ar.al([128, SEQ], BF16)
    St.VsA, St.VwA = ar.al([128, 64, 2, 65], BF16), ar.al([128, 64, 2, 65], BF16)
    St.KC = ar.al([128, 512], BF16)
    St.VCA = ar.al([128, 4, 2, 193], BF16)
    St.SNAP = ar.al([128, NT, 4, 128], BF16)
    for n in ("KTs", "KTw", "VsA", "VwA", "KC", "VCA", "SNAP"):
        setattr(St, "R_" + n, k.res(n))
    phase_p1(k, c, ar, St)
    for n in ("KTs", "KTw", "VsA", "VwA", "KC", "VCA", "SNAP"):
        c.dump(n, getattr(St, n), [getattr(St, "R_" + n)])
    k.flush()
    phase_p2pre(k, c, ar, St)
    c.dump("QT", c.QT, [c.R_QT])
    c.dump("gsig", c.gsig, [c.R_gsig])
    c.dump("oT_hg", c.oT_hg, [c.R_oThg])
    k.flush()
    phase_nsa(k, c, ar, St)
    c.dump("oT_nsa", c.oT_nsa, [c.R_oTnsa])
    return St


def build(stage="full", n_exp=NE):
    nc = bass.Bass("TRN2", target_bir_lowering=False)
    Dr = {}

    def din(name, shape, dt=F32):
        Dr[name] = nc.dram_tensor(name, list(shape), dt, kind="ExternalInput").ap()

    for name, shape, dt in input_specs(max(n_exp, 1), stage):
        din(name, shape, dt)
    Dr["y"] = nc.dram_tensor("y", [NTOK, D], F32, kind="ExternalOutput").ap()
    with ExitStack() as es:
        ARENA_BYTES = 207 * 1024
        big = es.enter_context(nc.sbuf_tensor("arena", [128, ARENA_BYTES // 2], BF16))
        pst = es.enter_context(nc.psum_tensor("ps", [128, 4096], F32))
        ar = Arena(big, ARENA_BYTES)
        k = KH(nc, es)
        k.oplim = _NC_CACHE.get("oplim", 10 ** 9)
        c = Ctx()
        c.nc, c.D, c.n_exp = nc, Dr, n_exp
        dumps = []

        def dump(name, ap, rs):
            if not _NC_CACHE.get("dbg"):
                return
            dt_ = ap.dtype
            dr = nc.dram_tensor("dbg_" + name, list(ap.shape), dt_, kind="ExternalOutput").ap()
            r = k.res("dbg_" + name)
            k.dma("sp", dr, ap, reads=list(rs), key=r)
            dumps.append(r)
        c.dump = dump
        c.psum = [pst[:, i * 512:(i + 1) * 512] for i in range(8)]
        c.pwide = lambda i: pst[:, i * 512:(i + 2) * 512]
        c.R_ps = [k.res("psb%d" % i, excl=True) for i in range(8)]
        c.R_const = k.res("const")
        c.identf = ar.al([128, 128], F32)
        c.identb = ar.al([128, 128], BF16)
        c.epsb = ar.al([128, 1], F32)
        c.ssq = ar.al([128, 8], F32)
        c.std = ar.al([128, 8], F32)
        c.rstd = ar.al([128, 8], F32)
        c.rs_res = [k.res("rs%d" % i) for i in range(8)]
        c.rs_i = 0
        k.dma("sp", c.identf, Dr["identf"], writes=[c.R_const])
        k.dma("sp", c.identb, Dr["identb"], writes=[c.R_const])
        k.op("dve", lambda e: e.memset(c.epsb, EPS), writes=[c.R_const])
        c.n_groups = _NC_CACHE.get("n_groups", 16)
        c.n_blocks = _NC_CACHE.get("n_blocks", NT)
        c.R32 = ar.al([128, 8, NTOK], BF16)
        c.R32f = c.R32.rearrange("p a b -> p (a b)")
        c.QT = c.R32f[:, 0:8192].rearrange("p (i a t) -> p i a t", i=NT, a=4)
        c.oT_nsa = c.R32[:, 4:8, :]
        c.oT_hg = ar.al([128, 4, NTOK], BF16)
        c.gsig = ar.al([128, NT, 24], F32)
        c.R_QT, c.R_oTnsa, c.R_oThg, c.R_gsig = k.res("QT"), k.res("oTnsa"), k.res("oThg"), k.res("gsig")
        hfT = c.R32
        R_hfT = [k.res("hfT_%d" % t) for t in range(NT)]
        R_X1 = [k.res("x1_%d" % t) for t in range(NT)]
        if stage != "moe_only":
            c.I4 = ar.al([128, 512], BF16)
            c.tri = ar.al([128, 128], BF16)
            c.onehot = ar.al([128, 4], F32)
            k.dma("pool", c.I4, Dr["I4"], writes=[c.R_const])
            k.dma("pool", c.tri, Dr["tri"], writes=[c.R_const])
            k.dma("sp", c.onehot, Dr["onehot"], writes=[c.R_const])
            setup_lb(k, c, ar)
            M1 = ar.mark()
            phase_attn(k, c, ar, stage)
            for r in dumps:
                k.rec["sp"].append(("w", r.dsem, r.dcnt))
            k.flush()
            ar.release(M1)
        X1 = ar.al([128, NT, D], F32)
        if stage == "moe_only":
            for t in range(NT):
                k.dma("sp", X1[:, t, :], Dr["xo"][t * 128:(t + 1) * 128, :], writes=[R_X1[t]])
        else:
            phase_p2c(k, c, ar, X1, R_X1)
            c.dump("X1", X1, R_X1)
            for r in dumps:
                if r.name == "dbg_X1":
                    k.rec["sp"].append(("w", r.dsem, r.dcnt))
        k.flush()
        if n_exp >= 0:
            phase_moe(k, c, ar, X1, R_X1, hfT, R_hfT)
            k.flush()
        phase_final(k, c, ar, X1, R_X1)
        k.flush()
    return nc


def input_specs(ne=NE, stage="full"):
    return [
        ("xb", (SEQ, D), F32), ("xo", (NTOK, D), F32),
        ("g_ffn", (D,), F32), ("g_fin", (D,), F32),
        ("w_r", (D, 36), F32), ("b_r", (36,), F32),
        ("w_gate", (ne, D, DFF), F32), ("w_up", (ne, D, DFF), F32), ("w_down", (ne, DFF, D), F32),
        ("identf", (128, 128), F32), ("identb", (128, 128), BF16),
    ] + (attn_input_specs() if stage != "moe_only" else [])


_NC_CACHE = {}


def host_inputs(inp, core, ne=NE):
    b, cp = core // 4, core % 4
    x = np.asarray(inp["x"], dtype=np.float32)
    m = {}
    m["xb"] = np.ascontiguousarray(x[b])
    m["xo"] = np.ascontiguousarray(x[b].reshape(NT, 4, 128, D)[:, cp].reshape(NTOK, D))
    m["g_ffn"] = np.ascontiguousarray(inp["ffn_norm"][0])
    m["g_fin"] = np.ascontiguousarray(inp["final_norm"])
    m["w_r"] = np.ascontiguousarray(np.concatenate([inp["w_grp"][0], inp["w_rtr"][0]], axis=1))
    m["b_r"] = np.ascontiguousarray(np.concatenate([inp["b_grp"][0], inp["b_rtr"][0]], axis=0))
    m["w_gate"] = np.ascontiguousarray(inp["w_gate"][0, :ne])
    m["w_up"] = np.ascontiguousarray(inp["w_up"][0, :ne])
    m["w_down"] = np.ascontiguousarray(inp["w_down"][0, :ne])
    m["identf"] = np.eye(128, dtype=np.float32)
    m["identb"] = np.eye(128, dtype=np.float32).astype(ml_dtypes.bfloat16)
    if _NC_CACHE.get("stage", "full") != "moe_only":
        m.update(attn_host_inputs(inp, b, cp))
    return m


def kernel(**inp):
    inp = {k_: np.asarray(v) for k_, v in inp.items()}
    stage = _NC_CACHE.get("stage", "full")
    key = ("nc", stage)
    if key not in _NC_CACHE:
        _NC_CACHE[key] = build(stage, _NC_CACHE.get("n_exp", NE))
    nc = _NC_CACHE[key]
    shared = None
    in_maps = []
    for core in range(8):
        m = host_inputs(inp, core, max(_NC_CACHE.get("n_exp", NE), 1))
        if shared is None:
            shared = m
        else:
            for kk in ("w_gate", "w_up", "w_down"):
                m[kk] = shared[kk]
        in_maps.append(m)
    res = run_bass_kernel_spmd(nc, in_maps, core_ids=list(range(8)))
    _NC_CACHE["last_results"] = res.results
    out = np.zeros((2, SEQ // 128, 128, D), dtype=np.float32)
    for core in range(8):
        b, cp = core // 4, core % 4
        y = np.asarray(res.results[core]["y"]).reshape(NT, 128, D)
        out[b, cp::4] = y
    return out.reshape(2, SEQ, D)
```

```python
import numpy as np
import ml_dtypes
import concourse.bass as bass
import concourse.mybir as mybir
from concourse.bass_utils import run_bass_kernel_spmd
from contextlib import ExitStack

F32 = mybir.dt.float32
BF16 = mybir.dt.bfloat16
AF = mybir.ActivationFunctionType
ALU = mybir.AluOpType
AX = mybir.AxisListType


class Res:
    __slots__ = ("name", "w", "rs", "dsem", "dcnt", "excl")

    def __init__(self, name, excl=False):
        self.name = name
        self.excl = excl
        self.w = None
        self.rs = {}
        self.dsem = None
        self.dcnt = 0


class _Proxy:
    def __init__(self):
        self.calls = []

    def __getattr__(self, name):
        def rec(*a, **kw):
            self.calls.append((name, a, kw))
        return rec


def _bind(f):
    p = _Proxy()
    f(p)
    assert len(p.calls) == 1, "one engine instruction per callable"
    name, a, kw = p.calls[0]
    return lambda eng: getattr(eng, name)(*a, **kw)


class KH:
    ENG = ("pe", "dve", "act", "pool", "sp")

    def __init__(self, nc, es):
        self.nc = nc
        self.es = es
        self.sem = {}
        self.cnt = {}
        for e in self.ENG:
            self.sem[e] = es.enter_context(nc.semaphore("s_" + e))
            self.cnt[e] = 0
        self.rec = {e: [] for e in self.ENG}
        self.seen = {e: {} for e in self.ENG}
        self.nsem = len(self.ENG)
        self.semobj = dict(self.sem)

    def res(self, name, excl=False):
        return Res(name, excl)

    def _dma_sem(self, r):
        if r.dsem is None:
            r.dsem = "d_" + r.name + "_%d" % self.nsem
            self.semobj[r.dsem] = self.es.enter_context(self.nc.semaphore(r.dsem))
            self.nsem += 1
        return r.dsem

    def _deps(self, e, reads, writes):
        deps = {}
        for r in reads:
            if r.w is not None:
                k, v = r.w
                deps[k] = max(deps.get(k, 0), v)
        for w in writes:
            if w.w is not None:
                k, v = w.w
                deps[k] = max(deps.get(k, 0), v)
            for k, v in w.rs.items():
                deps[k] = max(deps.get(k, 0), v)
        seen = self.seen[e]
        for k, v in deps.items():
            if seen.get(k, 0) >= v:
                continue
            seen[k] = v
            self.rec[e].append(("w", k, v))

    def op(self, e, fns, reads=(), writes=()):
        if callable(fns):
            fns = [fns]
        self.opn = getattr(self, "opn", 0) + 1
        if self.opn > getattr(self, "oplim", 10 ** 9):
            return
        ex = [r for r in reads if r.excl]
        if ex:
            reads = [r for r in reads if not r.excl]
            writes = list(writes) + [r for r in ex if r not in writes]
        self._deps(e, reads, writes)
        self.cnt[e] += 1
        v = self.cnt[e]
        fns = [_bind(f) for f in fns]
        for f in fns[:-1]:
            self.rec[e].append(("i", f, None, 0))
        self.rec[e].append(("i", fns[-1], e, 1))
        self.seen[e][e] = max(self.seen[e].get(e, 0), 0)
        for r in reads:
            r.rs[e] = v
        for w in writes:
            w.w = (e, v)
            w.rs = {}

    def dma(self, q, out, in_, reads=(), writes=(), key=None, **kw):
        self._deps(q, reads, writes)
        kr = key or (writes[0] if writes else reads[0])
        sk = self._dma_sem(kr)
        kr.dcnt += 16
        v = kr.dcnt
        self.rec[q].append(("i", lambda eng: eng.dma_start(out=out, in_=in_, **kw), sk, 16))
        for r in reads:
            r.rs[sk] = v
        for w in writes:
            w.w = (sk, v)
            w.rs = {}

    def wait_res(self, e, rs):
        self._deps(e, rs, ())

    def simulate(self):
        if not hasattr(self, "simval"):
            self.simval = {}
        val = self.simval
        ptr = {e: 0 for e in self.ENG}
        prog = True
        while prog:
            prog = False
            for e in self.ENG:
                items = self.rec[e]
                while ptr[e] < len(items):
                    it = items[ptr[e]]
                    if it[0] == "w":
                        if val.get(it[1], 0) >= it[2]:
                            ptr[e] += 1
                            prog = True
                        else:
                            break
                    else:
                        if it[2] is not None:
                            val[it[2]] = val.get(it[2], 0) + it[3]
                        ptr[e] += 1
                        prog = True
        for e in self.ENG:
            if ptr[e] < len(self.rec[e]):
                it = self.rec[e][ptr[e]]
                raise RuntimeError("DEADLOCK: engine %s stuck at item %d/%d waiting %s >= %s (have %s)" % (
                    e, ptr[e], len(self.rec[e]), it[1], it[2], val.get(it[1], 0)))

    def flush(self, name=None):
        nc = self.nc
        rec = self.rec
        semobj = self.semobj
        self.simulate()
        import os
        if os.environ.get("KH_DEBUG"):
            print("KH flush: ops so far", getattr(self, "opn", 0), {e: len(v) for e, v in self.rec.items()}, "nsem", self.nsem, flush=True)

        def play(eng, items):
            for it in items:
                if it[0] == "w":
                    eng.wait_ge(semobj[it[1]], it[2])
                else:
                    ins = it[1](eng)
                    if it[2] is not None:
                        ins.then_inc(semobj[it[2]], it[3])

        with nc.Block() as block:
            if rec["sp"]:
                @block.sync
                def _(eng):
                    play(eng, rec["sp"])
            if rec["pe"]:
                @block.tensor
                def _(eng):
                    play(eng, rec["pe"])
            if rec["dve"]:
                @block.vector
                def _(eng):
                    play(eng, rec["dve"])
            if rec["act"]:
                @block.scalar
                def _(eng):
                    play(eng, rec["act"])
            if rec["pool"]:
                @block.gpsimd
                def _(eng):
                    play(eng, rec["pool"])
        self.rec = {e: [] for e in self.ENG}

NEG = -30000.0
NT = 16
NTOK = NT * 128
SEQ = 8192
D = 1024
NE = 32
DFF = 512
EPS = 1e-6


class Arena:
    def __init__(self, big, nbytes):
        self.big = big
        self.n = nbytes
        self.off = 0

    def mark(self):
        return self.off

    def release(self, m):
        import os
        if os.environ.get("KH_DEBUG"):
            print("arena release: peak", getattr(self, "peak", 0), "->", m, "of", self.n, flush=True)
        self.peak = m
        self.off = m

    def al(self, shape, dt):
        esz = 4 if dt == F32 else 2
        per = int(np.prod(shape[1:])) * esz
        self.off = (self.off + 63) // 64 * 64
        o = self.off
        assert o + per <= self.n, ("arena overflow", o, per, self.n)
        self.off = o + per
        self.peak = max(getattr(self, "peak", 0), self.off)
        v = self.big[0:shape[0], o // 2:(o + per) // 2]
        if dt == F32:
            v = v.bitcast(F32)
        if len(shape) == 3:
            v = v.rearrange("p (a b) -> p a b", a=shape[1])
        elif len(shape) == 4:
            v = v.rearrange("p (a b c) -> p a b c", a=shape[1], b=shape[2])
        return v


class Ctx:
    pass


def rms_rstd(k, c, src, src_res, scr, scr_res, n_feat, tag):
    i = c.rs_i % 8
    c.rs_i += 1
    ssq, std, rstd = c.ssq[:, i:i + 1], c.std[:, i:i + 1], c.rstd[:, i:i + 1]
    R = c.rs_res[i]
    k.op("act", lambda e: e.activation(out=scr, in_=src, func=AF.Square, accum_out=ssq),
         reads=[src_res], writes=[scr_res, R])
    k.op("act", lambda e: e.activation(out=std, in_=ssq, func=AF.Sqrt, scale=1.0 / n_feat, bias=c.epsb[:, 0:1]),
         reads=[R, c.R_const], writes=[R])
    k.op("dve", lambda e: e.reciprocal(out=rstd, in_=std), reads=[R], writes=[R])
    return rstd, R


def phase_moe(k, c, ar, X1, R_X1, hfT, R_hfT):
    nc = c.nc
    Dr = c.D
    m0 = ar.mark()
    hf32 = [ar.al([128, D], F32) for _ in range(2)]
    R_hf32 = [k.res("hf32_%d" % i) for i in range(2)]
    scr = ar.al([128, D], F32)
    R_scr = k.res("moe_scr")
    hT32 = [ar.al([128, 8, 128], F32) for _ in range(2)]
    R_hT32 = [k.res("hT32_%d" % i) for i in range(2)]
    wr32 = ar.al([128, 8, 36], F32)
    R_wr = k.res("wr32")
    brt = ar.al([128, 36], F32)
    gft = ar.al([128, D], F32)
    R_gft = k.res("gft")
    comb = ar.al([128, NT, NE], F32)
    R_comb = k.res("comb")
    sm = ar.al([128, 128], F32)
    R_sm = k.res("moe_sm")
    k.dma("sp", wr32, Dr["w_r"].rearrange("(kt p) n -> p kt n", p=128), writes=[R_wr])
    k.dma("sp", brt, Dr["b_r"].partition_broadcast(128), writes=[R_wr])
    k.dma("sp", gft, Dr["g_ffn"].partition_broadcast(128), writes=[R_gft])
    pT = [c.pwide(0), c.pwide(2)]
    R_pT = [[c.R_ps[0], c.R_ps[1]], [c.R_ps[2], c.R_ps[3]]]
    pL = c.psum[4]
    R_pL = c.R_ps[4]
    for t in range(NT):
        b = t % 2
        xs = X1[:, t, :]
        rstd, R_r = rms_rstd(k, c, xs, R_X1[t], scr, R_scr, D, "moe")
        k.op("dve", lambda e, b=b, xs=xs, rstd=rstd: e.scalar_tensor_tensor(
            out=hf32[b], in0=xs, scalar=rstd, in1=gft, op0=ALU.mult, op1=ALU.mult),
            reads=[R_X1[t], R_r, R_gft], writes=[R_hf32[b]])
        p2 = pT[b]
        k.op("pe", [(lambda e, i=i, b=b, p2=p2: e.transpose(out=p2[:, i * 128:(i + 1) * 128],
                                                            in_=hf32[b][:, i * 128:(i + 1) * 128], identity=c.identf))
                    for i in range(8)], reads=[R_hf32[b], c.R_const], writes=R_pT[b])
        k.op("act", lambda e, b=b, p2=p2: e.activation(out=hT32[b].rearrange("p a b -> p (a b)"), in_=p2, func=AF.Copy),
             reads=R_pT[b], writes=[R_hT32[b]])
        k.op("dve", lambda e, b=b, p2=p2, t=t: e.tensor_copy(
            out=hfT[:, :, t * 128:(t + 1) * 128], in_=p2.rearrange("p (a b) -> p a b", a=8)),
            reads=R_pT[b], writes=[R_hfT[t]])
        lg = pL[:, 0:36]
        k.op("pe", [(lambda e, i=i, b=b: e.matmul(lg, lhsT=hT32[b][:, i, :], rhs=wr32[:, i, :], start=(i == 0), stop=(i == 7)))
                    for i in range(8)], reads=[R_hT32[b], R_wr], writes=[R_pL])
        lgs = sm[:, 0:36]
        gmax, gsum, gp, pen = sm[:, 36:37], sm[:, 37:38], sm[:, 38:39], sm[:, 40:44]
        gex, goh = sm[:, 44:48], sm[:, 48:52]
        elm = sm[:, 52:84]
        m8 = sm[:, 84:92]
        dd, ee, w1, w2 = sm[:, 92:93], sm[:, 93:94], sm[:, 94:95], sm[:, 95:96]
        oh = sm[:, 96:128]
        ct = comb[:, t, :]
        RW = dict(reads=[R_sm], writes=[R_sm])
        k.op("dve", lambda e: e.tensor_tensor(out=lgs, in0=lg, in1=brt, op=ALU.add), reads=[R_pL, R_wr, R_sm], writes=[R_sm])
        k.op("dve", lambda e: e.reduce_max(out=gmax, in_=lgs[:, 0:4], axis=AX.X), **RW)
        k.op("dve", lambda e: e.tensor_scalar(out=gex, in0=lgs[:, 0:4], scalar1=gmax, scalar2=None, op0=ALU.subtract), **RW)
        k.op("act", lambda e: e.activation(out=gex, in_=gex, func=AF.Exp, accum_out=gsum), **RW)
        k.op("dve", lambda e: e.reciprocal(out=gp, in_=gsum), **RW)
        k.op("dve", lambda e: e.tensor_scalar(out=pen, in0=lgs[:, 0:4], scalar1=gmax, scalar2=-1e30, op0=ALU.is_lt, op1=ALU.mult), **RW)
        k.op("dve", lambda e: e.tensor_tensor(out=elm.rearrange("p (g x) -> p g x", g=4),
                                              in0=lgs[:, 4:36].rearrange("p (g x) -> p g x", g=4),
                                              in1=pen.unsqueeze(2).broadcast_to([128, 4, 8]), op=ALU.add), **RW)
        k.op("dve", lambda e: e.max(out=m8, in_=elm), **RW)
        k.op("dve", lambda e: e.tensor_tensor(out=dd, in0=m8[:, 1:2], in1=m8[:, 0:1], op=ALU.subtract), **RW)
        k.op("act", lambda e: e.activation(out=ee, in_=dd, func=AF.Exp), **RW)
        k.op("dve", lambda e: e.tensor_scalar(out=ee, in0=ee, scalar1=1.0, scalar2=None, op0=ALU.add), **RW)
        k.op("dve", lambda e: e.reciprocal(out=w1, in_=ee), **RW)
        k.op("dve", lambda e: e.tensor_scalar(out=w2, in0=w1, scalar1=-1.0, scalar2=1.0, op0=ALU.mult, op1=ALU.add), **RW)
        k.op("dve", lambda e: e.tensor_tensor(out=w1, in0=w1, in1=gp, op=ALU.mult), **RW)
        k.op("dve", lambda e: e.tensor_tensor(out=w2, in0=w2, in1=gp, op=ALU.mult), **RW)
        k.op("dve", lambda e: e.tensor_scalar(out=oh, in0=elm, scalar1=m8[:, 0:1], scalar2=w1, op0=ALU.is_equal, op1=ALU.mult), **RW)
        k.op("dve", lambda e, ct=ct: e.tensor_scalar(out=ct, in0=elm, scalar1=m8[:, 1:2], scalar2=w2, op0=ALU.is_equal, op1=ALU.mult),
             reads=[R_sm], writes=[R_comb])
        k.op("dve", lambda e, ct=ct: e.tensor_tensor(out=ct, in0=ct, in1=oh, op=ALU.add), reads=[R_sm, R_comb], writes=[R_comb])

    if c.n_exp == 0:
        ar.release(m0)
        return
    NWB = 2
    wg = [ar.al([128, 8, DFF], BF16) for _ in range(NWB)]
    wu = [ar.al([128, 8, DFF], BF16) for _ in range(NWB)]
    wd = [ar.al([128, 4, D], BF16) for _ in range(NWB)]
    R_wg = [k.res("wg%d" % i) for i in range(NWB)]
    R_wu = [k.res("wu%d" % i) for i in range(NWB)]
    R_wd = [k.res("wd%d" % i) for i in range(NWB)]
    hid = [ar.al([128, 4, 512], BF16) for _ in range(2)]
    R_hid = [k.res("hid%d" % i) for i in range(2)]
    sg = [ar.al([128, 512], F32) for _ in range(2)]
    R_sg = [k.res("sg%d" % i) for i in range(2)]
    n_exp = c.n_exp

    def load_w(e):
        b = e % NWB
        k.dma("pool", wg[b], Dr["w_gate"][e].rearrange("(kt p) n -> p kt n", p=128), writes=[R_wg[b]])
        k.dma("pool", wu[b], Dr["w_up"][e].rearrange("(kt p) n -> p kt n", p=128), writes=[R_wu[b]])
        k.dma("pool", wd[b], Dr["w_down"][e].rearrange("(kt p) n -> p kt n", p=128), writes=[R_wd[b]])

    units = [(e, g) for e in range(n_exp) for g in range(NT // 4)]
    pgu = [(c.psum[0], c.psum[1]), (c.psum[2], c.psum[3])]
    R_pgu = [(c.R_ps[0], c.R_ps[1]), (c.R_ps[2], c.R_ps[3])]
    pdn = [c.psum[4], c.psum[5], c.psum[6], c.psum[7]]
    R_pdn = [c.R_ps[4], c.R_ps[5], c.R_ps[6], c.R_ps[7]]
    st = dict(gu=0, dn=0)

    def gate_up(u):
        e, g = units[u]
        b = e % NWB
        hb = u % 2
        tok = slice(g * 512, (g + 1) * 512)
        for ff in range(4):
            pb = st["gu"] % 2
            st["gu"] += 1
            pg, pu = pgu[pb]
            k.op("pe", [(lambda en, i=i, pg=pg, b=b, ff=ff: en.matmul(pg, lhsT=wg[b][:, i, ff * 128:(ff + 1) * 128], rhs=hfT[:, i, tok],
                                                                      start=(i == 0), stop=(i == 7))) for i in range(8)],
                 reads=[R_wg[b]] + R_hfT[4 * g:4 * g + 4], writes=[R_pgu[pb][0]])
            k.op("pe", [(lambda en, i=i, pu=pu, b=b, ff=ff: en.matmul(pu, lhsT=wu[b][:, i, ff * 128:(ff + 1) * 128], rhs=hfT[:, i, tok],
                                                                      start=(i == 0), stop=(i == 7))) for i in range(8)],
                 reads=[R_wu[b]] + R_hfT[4 * g:4 * g + 4], writes=[R_pgu[pb][1]])
            k.op("act", lambda en, pg=pg, pb=pb: en.activation(out=sg[pb], in_=pg, func=AF.Silu),
                 reads=[R_pgu[pb][0]], writes=[R_sg[pb]])
            k.op("dve", lambda en, pu=pu, pb=pb, hb=hb, ff=ff: en.tensor_tensor(out=hid[hb][:, ff, :], in0=pu, in1=sg[pb], op=ALU.mult),
                 reads=[R_pgu[pb][1], R_sg[pb]], writes=[R_hid[hb]])

    def down(u):
        e, g = units[u]
        b = e % NWB
        hb = u % 2
        for tt in range(4):
            t = 4 * g + tt
            for hf in range(2):
                pb = st["dn"] % 4
                st["dn"] += 1
                po = pdn[pb]
                k.op("pe", [(lambda en, i=i, po=po, b=b, hb=hb, tt=tt, hf=hf: en.matmul(
                    po, lhsT=hid[hb][:, i, tt * 128:(tt + 1) * 128], rhs=wd[b][:, i, hf * 512:(hf + 1) * 512],
                    start=(i == 0), stop=(i == 3))) for i in range(4)],
                    reads=[R_hid[hb], R_wd[b]], writes=[R_pdn[pb]])
                xs = X1[:, t, hf * 512:(hf + 1) * 512]
                k.op("dve", lambda en, po=po, xs=xs, t=t, e=e: en.scalar_tensor_tensor(
                    out=xs, in0=po, scalar=comb[:, t, e:e + 1], in1=xs, op0=ALU.mult, op1=ALU.add),
                    reads=[R_pdn[pb], R_comb, R_X1[t]], writes=[R_X1[t]])

    load_w(0)
    for u in range(len(units)):
        e, g = units[u]
        gate_up(u)
        if u >= 1:
            down(u - 1)
        if g == 0 and e + 1 < n_exp:
            load_w(e + 1)
    down(len(units) - 1)
    ar.release(m0)


def phase_final(k, c, ar, X1, R_X1):
    Dr = c.D
    m0 = ar.mark()
    gft = ar.al([128, D], F32)
    R_g = k.res("gfin")
    scr = ar.al([128, D], F32)
    R_scr = k.res("fin_scr")
    ob = [ar.al([128, D], F32) for _ in range(2)]
    R_ob = [k.res("ob%d" % i) for i in range(2)]
    k.dma("sp", gft, Dr["g_fin"].partition_broadcast(128), writes=[R_g])
    for t in range(NT):
        b = t % 2
        xs = X1[:, t, :]
        rstd, R_r = rms_rstd(k, c, xs, R_X1[t], scr, R_scr, D, "fin")
        k.op("dve", lambda e, b=b, xs=xs, rstd=rstd: e.scalar_tensor_tensor(
            out=ob[b], in0=xs, scalar=rstd, in1=gft, op0=ALU.mult, op1=ALU.mult),
            reads=[R_X1[t], R_r, R_g], writes=[R_ob[b]])
        k.dma("sp", Dr["y"][t * 128:(t + 1) * 128, :], ob[b], reads=[R_ob[b]])
    for b in range(2):
        for sk, v in list(R_ob[b].rs.items()):
            if sk.startswith("d_"):
                k.rec["sp"].append(("w", sk, v))
    ar.release(m0)


Q0, KV0, GT0, HQ0, HF0, HI0, HG0, MG0 = 0, 512, 1280, 1304, 1816, 2328, 2840, 3352


def _partner(d):
    return d + 8 if d < 8 else (d - 8 if d < 16 else d)


def _rope_tables(pos):
    pos = np.asarray(pos, dtype=np.float32)
    inv = (np.float32(500000.0) ** (-np.arange(8, dtype=np.float32) / np.float32(8))).astype(np.float32)
    ang = (pos[None, :] * inv[:, None]).astype(np.float32)
    cs, sn = np.cos(ang).astype(np.float32), np.sin(ang).astype(np.float32)
    C = np.ones((64, len(pos)), np.float32)
    S = np.zeros((64, len(pos)), np.float32)
    C[0:8], C[8:16] = cs, cs
    S[0:8], S[8:16] = -sn, sn
    return C, S


def attn_input_specs():
    return [
        ("g_attn", (D,), F32), ("g_hg4", (512,), F32),
        ("w1f", (D, 1280), F32), ("w1t", (D, 768), F32),
        ("w2f", (D, 2048), F32), ("w2t", (D, 1536), F32),
        ("wck", (64, 2048), F32), ("wckp", (64, 2048), F32), ("wcv", (64, 2048), F32),
        ("posT", (128, 32), F32), ("lbl", (2, 512), F32),
        ("w_mg", (D, 2048), F32), ("w_brn", (512, D), F32), ("w_brh", (512, D), F32), ("w_out", (D, D), F32),
        ("CK", (128, SEQ), F32), ("SK", (128, SEQ), F32), ("CKc", (128, 512), F32), ("SKc", (128, 512), F32),
        ("CQ", (128, NTOK), F32), ("SQ", (128, NTOK), F32),
        ("ovl", (128, 4, 128), F32),
        ("CB", (128, NT, 128), F32), ("CM", (128, 4, 128), F32), ("WMT", (128, 8, 128), F32),
        ("VAL", (128, NT, 128), F32), ("ADDC", (128, NT, 128), F32),
        ("tri", (128, 128), F32), ("I4", (128, 512), F32), ("onehot", (128, 4), F32),
    ]


_TAB_CACHE = {}


def _const_tables(cp):
    if cp in _TAB_CACHE:
        return _TAB_CACHE[cp]
    m = {}
    C, S = _rope_tables(np.arange(SEQ))
    m["CK"], m["SK"] = np.concatenate([C, C], 0), np.concatenate([S, S], 0)
    C, S = _rope_tables(np.maximum(16 * (np.arange(512) - 1), 0))
    m["CKc"], m["SKc"] = np.concatenate([C, C], 0), np.concatenate([S, S], 0)
    tpos = (128 * (4 * np.arange(NT)[:, None] + cp) + np.arange(128)[None, :])
    C, S = _rope_tables(tpos.reshape(-1))
    m["CQ"] = np.concatenate([C, C], 0) * np.float32(0.125)
    m["SQ"] = np.concatenate([S, S], 0) * np.float32(0.125)
    n = np.arange(512) - 1
    cs, ce = 16 * n, 16 * n + 31
    ss = 64 * np.arange(128)
    ov = ((cs[:, None] < ss[None, :] + 64) & (ce[:, None] >= ss[None, :]) & (n[:, None] >= 0)).astype(np.float32)
    m["ovl"] = np.ascontiguousarray(ov.reshape(4, 128, 128).transpose(1, 0, 2))
    mt = (np.arange(NT) // 4)
    mm = mt[:, None] * 128 + np.arange(128)[None, :]
    nn = mm - 1
    okc = (nn[:, None, :] >= 0) & (16 * nn[:, None, :] + 31 <= tpos[:, :, None])
    m["CB"] = np.ascontiguousarray(np.where(okc, 0.0, NEG).astype(np.float32).transpose(1, 0, 2))
    blk = np.arange(128)
    jq = tpos // 64
    force = (blk[None, None, :] == jq[:, :, None]) | (blk[None, None, :] == 0)
    valid = (64 * blk[None, None, :] <= tpos[:, :, None])
    m["VAL"] = np.ascontiguousarray((valid & ~force).astype(np.float32).transpose(1, 0, 2))
    m["ADDC"] = np.ascontiguousarray(np.where(force, 1e4, np.where(valid, 0.0, -1.0)).astype(np.float32).transpose(1, 0, 2))
    t = np.arange(128)[:, None]
    p = np.arange(128)[None, :]
    caus = np.where(p <= t, 0.0, NEG).astype(np.float32)
    anti = np.where(p > t, 0.0, NEG).astype(np.float32)
    cm = np.zeros((128, 4, 128), np.float32)
    for r in range(4):
        cm[:, r, :] = 0.0 if r < cp else (caus if r == cp else NEG)
    m["CM"] = cm
    wm = np.zeros((128, 8, 128), np.float32)
    for r in range(8):
        dk = cp + 4 - r
        wm[:, r, :] = NEG if (dk < 0 or dk > 4) else (caus if dk == 0 else (anti if dk == 4 else 0.0))
    m["WMT"] = wm
    m["tri"] = (np.arange(128)[:, None] <= np.arange(128)[None, :]).astype(np.float32)
    m["I4"] = np.tile(np.eye(128, dtype=np.float32), (1, 4))
    oh = np.zeros((128, 4), np.float32)
    oh[:, cp] = 1.0
    m["onehot"] = oh
    _TAB_CACHE[cp] = m
    return m


def attn_host_inputs(inp, b, cp):
    m = dict(_const_tables(cp))
    w = inp["w_in"][0]
    pp = np.array([g * 64 + _partner(d) for g in range(2) for d in range(64)])
    kv = lambda s: KV0 + s * 128 + np.arange(128)
    hfc = HF0 + np.arange(512)
    m["w1f"] = np.ascontiguousarray(np.concatenate(
        [w[:, kv(0)], w[:, kv(1)], w[:, kv(2)], w[:, kv(2)[pp]], w[:, kv(4)], w[:, kv(4)[pp]], w[:, hfc]], axis=1))
    m["w1t"] = np.ascontiguousarray(np.concatenate([w[:, kv(3)], w[:, kv(5)], w[:, HI0:HI0 + 512]], axis=1))
    qcols, qpcols = [], []
    for a in range(4):
        for h in (a, 4 + a):
            qcols += [Q0 + h * 64 + d for d in range(64)]
            qpcols += [Q0 + h * 64 + _partner(d) for d in range(64)]
    m["w2f"] = np.ascontiguousarray(np.concatenate(
        [w[:, qcols], w[:, qpcols], w[:, HQ0:HQ0 + 512], w[:, hfc]], axis=1))
    gpad = np.concatenate([w[:, GT0:GT0 + 24], w[:, GT0:GT0 + 24][:, :0].repeat(1, 1)], axis=1)
    w2t = np.zeros((D, 1536), np.float32)
    w2t[:, 0:512] = w[:, HI0:HI0 + 512]
    w2t[:, 512:1024] = w[:, HG0:HG0 + 512]
    w2t[:, 1024:1048] = w[:, GT0:GT0 + 24]
    m["w2t"] = w2t
    pc = np.array([_partner(d) for d in range(64)])
    dle = lambda w_: np.ascontiguousarray(w_.reshape(32, 64, 64).transpose(1, 0, 2).reshape(64, 2048))
    m["wck"] = dle(inp["w_cmp_k"][0])
    m["wckp"] = dle(inp["w_cmp_k"][0][:, pc])
    m["wcv"] = dle(inp["w_cmp_v"][0])
    pT = np.ascontiguousarray(inp["cmp_pos"][0].T)
    m["posT"] = np.concatenate([pT, pT], 0)
    m["lbl"] = np.ascontiguousarray(inp["hg_lb_logits"])
    m["g_attn"] = np.ascontiguousarray(inp["attn_norm"][0])
    m["g_hg4"] = np.ascontiguousarray(np.tile(inp["hg_norm"][0], 4))
    m["w_mg"] = np.ascontiguousarray(w[:, MG0:MG0 + 2048])
    m["w_brn"] = np.ascontiguousarray(inp["w_br_nsa"][0])
    m["w_brh"] = np.ascontiguousarray(inp["w_br_hg"][0])
    m["w_out"] = np.ascontiguousarray(inp["w_out"][0])
    return m


def norm_transpose_group(k, c, W, src_dram, row0, hT, R_hT):
    def s1(tt):
        b = tt % 2
        k.dma("sp", W.xt[b], src_dram[row0 + tt * 128: row0 + (tt + 1) * 128, :], writes=[W.R_xt[b]])
        rstd, R_r = rms_rstd(k, c, W.xt[b], W.R_xt[b], W.scr, W.R_scr, D, "an")
        k.op("dve", lambda e: e.scalar_tensor_tensor(
            out=W.hb[b], in0=W.xt[b], scalar=rstd, in1=W.gA, op0=ALU.mult, op1=ALU.mult),
            reads=[W.R_xt[b], R_r, W.R_gA], writes=[W.R_hb[b]])
        pb = c.psum[b].bitcast(BF16)
        k.op("pe", [(lambda e, i=i: e.transpose(out=pb[:, i * 128:(i + 1) * 128],
                                                in_=W.hb[b][:, i * 128:(i + 1) * 128], identity=c.identb))
                    for i in range(8)], reads=[W.R_hb[b], c.R_const], writes=[c.R_ps[b]])

    def s2(tt):
        b = tt % 2
        pb = c.psum[b].bitcast(BF16)
        k.op("act", lambda e: e.activation(out=hT[:, :, tt * 128:(tt + 1) * 128],
                                           in_=pb.rearrange("p (a b) -> p a b", a=8), func=AF.Copy),
             reads=[c.R_ps[b]], writes=[R_hT])
    s1(0)
    s1(1)
    s2(0)
    s1(2)
    s2(1)
    s1(3)
    s2(2)
    s2(3)


def f_front(k, c, W, fl_ps, R_fl, hd):
    u, a, bq, lk, L, RF = W.sets[hd % 2]
    k.op("act", lambda e: e.activation(out=u, in_=fl_ps, func=AF.Exp, scale=-1.0), reads=[R_fl], writes=[RF])
    k.op("act", lambda e: e.activation(out=a, in_=u, func=AF.Ln, scale=c.lbv[:, hd:hd + 1], bias=c.one_col[:, 0:1]),
         reads=[RF, c.R_const], writes=[RF])
    k.op("act", lambda e: e.activation(out=bq, in_=u, func=AF.Ln, bias=c.one_col[:, 0:1]), reads=[RF, c.R_const], writes=[RF])
    k.op("dve", lambda e: e.scalar_tensor_tensor(out=lk, in0=fl_ps, scalar=-1.0, in1=bq, op0=ALU.mult, op1=ALU.subtract),
         reads=[R_fl, RF], writes=[RF])
    for tt in range(4):
        sl = slice(tt * 128, (tt + 1) * 128)
        k.op("dve", lambda e, sl=sl: e.tensor_tensor_scan(out=L[:, sl], data0=a[:, sl], data1=bq[:, sl], initial=0.0,
                                                          op0=ALU.add, op1=ALU.subtract), reads=[RF], writes=[RF])
    k.op("pool", lambda e: e.tensor_tensor(out=lk, in0=lk, in1=L, op=ALU.subtract), reads=[RF], writes=[RF])


def f_back(k, c, W, hd, H=None):
    u, a, bq, lk, L, RF = W.sets[hd % 2]
    W_, W = W, (H if H is not None else W)
    Lr = L.rearrange("p (t x) -> p t x", t=4)
    rcol, ecol = Lr[:, :, 63], Lr[:, :, 127]
    k.op("dve", lambda e: e.tensor_scalar(out=W.rb[:, hd, :], in0=rcol, scalar1=c.l1mlb[:, hd:hd + 1], scalar2=None, op0=ALU.add),
         reads=[RF, c.R_const], writes=[W.R_cols])
    k.op("dve", lambda e: e.tensor_scalar(out=W.negr[:, hd, :], in0=rcol, scalar1=-1.0, scalar2=None, op0=ALU.mult),
         reads=[RF], writes=[W.R_cols])
    k.op("dve", lambda e: e.tensor_tensor(out=W.dl[:, hd, :], in0=ecol, in1=rcol, op=ALU.subtract), reads=[RF], writes=[W.R_cols])
    k.op("act", lambda e: e.activation(out=W.c1[:, hd, :], in_=ecol, func=AF.Exp), reads=[RF], writes=[W.R_cols])
    k.op("act", lambda e: e.activation(out=W.c2[:, hd, :], in_=W.dl[:, hd, :], func=AF.Exp), reads=[W.R_cols], writes=[W.R_cols])
    k.op("act", lambda e: e.activation(out=W.er[:, hd, :], in_=rcol, func=AF.Exp), reads=[RF], writes=[W.R_cols])
    for tt in range(4):
        sl = slice(tt * 128, (tt + 1) * 128)
        k.op("act", lambda e, sl=sl, tt=tt: e.activation(out=W.kT[:, hd, sl], in_=lk[:, sl], func=AF.Exp, bias=W.rb[:, hd, tt:tt + 1]),
             reads=[RF, W.R_cols], writes=[W.R_kT])


def setup_lb(k, c, ar):
    Dr = c.D
    c.lbv = ar.al([128, 4], F32)
    c.l1mlb = ar.al([128, 4], F32)
    c.one_col = ar.al([128, 1], F32)
    c.ones128 = ar.al([128, 128], F32)
    l0 = ar.al([128, 4], F32)
    l1 = ar.al([128, 4], F32)
    R = c.R_const
    k.dma("sp", l0, Dr["lbl"][0].rearrange("(h p) -> p h", p=128), writes=[R], allow_slow_non_contiguous=True)
    k.dma("sp", l1, Dr["lbl"][1].rearrange("(h p) -> p h", p=128), writes=[R], allow_slow_non_contiguous=True)
    k.op("dve", lambda e: e.memset(c.one_col, 1.0), writes=[R])
    k.op("dve", lambda e: e.memset(c.ones128, 1.0), writes=[R])
    k.op("dve", lambda e: e.tensor_tensor(out=l1, in0=l1, in1=l0, op=ALU.subtract), reads=[R], writes=[R])
    k.op("act", lambda e: e.activation(out=l0, in_=l1, func=AF.Exp), reads=[R], writes=[R])
    k.op("dve", lambda e: e.tensor_scalar(out=l0, in0=l0, scalar1=1.0, scalar2=None, op0=ALU.add), reads=[R], writes=[R])
    k.op("dve", lambda e: e.reciprocal(out=c.lbv, in_=l0), reads=[R], writes=[R])
    k.op("act", lambda e: e.activation(out=l0, in_=l0, func=AF.Ln), reads=[R], writes=[R])
    k.op("dve", lambda e: e.tensor_tensor(out=c.l1mlb, in0=l1, in1=l0, op=ALU.subtract), reads=[R], writes=[R])


class WS:
    pass


def alloc_hg_ws(k, ar, W, nsets=1):
    W.sets = []
    for si in range(nsets):
        blk = ar.al([128, 5, 512], F32)
        W.sets.append(tuple(blk[:, i, :] for i in range(5)) + (k.res("fchain%d" % si),))
        if si == 0:
            W.ab = blk[:, 1:3, :].rearrange("p a b -> p (a b)")
    if nsets == 1:
        W.sets.append(W.sets[0])
    W.u, W.a, W.bq, W.lk, W.L, W.R_f = W.sets[0]
    alloc_hslot(k, ar, W, "0")


def alloc_hslot(k, ar, H, tag):
    H.rb, H.negr, H.dl, H.c1, H.c2, H.er = [ar.al([128, 4, 4], F32) for _ in range(6)]
    H.R_cols = k.res("fcols" + tag)
    H.kT = ar.al([128, 4, 512], BF16)
    H.R_kT = k.res("kT" + tag)


def alloc_x_ws(k, c, ar, W, region, scr=None, R_scr=None):
    if region is not None:
        W.xt = [region[:, 0, :].bitcast(F32), region[:, 1, :].bitcast(F32)]
        W.hb = [region[:, 2, 0:1024], region[:, 2, 1024:2048]]
        W.scr = region[:, 3, :].bitcast(F32)
        W.R_scr = k.res("xscr")
    else:
        W.xt = [ar.al([128, D], F32) for _ in range(2)]
        W.hb = [ar.al([128, D], BF16) for _ in range(2)]
        W.scr, W.R_scr = scr, R_scr
    W.R_xt = [k.res("xt0"), k.res("xt1")]
    W.R_hb = [k.res("hb0"), k.res("hb1")]
    W.gA = ar.al([128, D], F32)
    W.R_gA = k.res("gA")
    k.dma("sp", W.gA, c.D["g_attn"].partition_broadcast(128), writes=[W.R_gA])


def phase_p1(k, c, ar, St):
    Dr = c.D
    m0 = ar.mark()
    W = WS()
    alloc_x_ws(k, c, ar, W, c.oT_hg)
    w1f, w1t = c.R32[:, :, 0:1280], c.R32[:, :, 1280:2048]
    R_w1 = k.res("w1")
    k.dma("pool", w1f, Dr["w1f"].rearrange("(kt p) n -> p kt n", p=128), writes=[R_w1])
    k.dma("pool", w1t, Dr["w1t"].rearrange("(kt p) n -> p kt n", p=128), writes=[R_w1])
    hT = ar.al([128, 8, 512], BF16)
    R_hT = k.res("hT")
    CKg, SKg = ar.al([128, 512], F32), ar.al([128, 512], F32)
    R_rt = k.res("ropetab")
    alloc_hg_ws(k, ar, W, nsets=2)
    t1, t2, R_t12 = W.u, W.a, W.R_f
    vtok = ar.al([128, 4, 512], BF16)
    R_vtok = k.res("vtok")
    ktok = ar.al([128, 4, 128], BF16)
    R_ktok = k.res("ktok")
    Sst = ar.al([128, 4, 128], F32)
    snapacc = ar.al([128, 4, 128], F32)
    R_S, R_snapacc = k.res("S"), k.res("snapacc")
    WC = [ar.al([128, 32, 64], BF16) for _ in range(3)]
    R_WC = k.res("WC")
    posT = ar.al([128, 32], BF16)
    cb = ar.al([128, 4], F32)
    xin = [[ar.al([128, 528], BF16) for _ in range(2)] for _ in range(2)]
    R_xin = [[k.res("xin%d%d" % (a, b)) for b in range(2)] for a in range(2)]
    CKc, SKc = ar.al([128, 32], F32), ar.al([128, 32], F32)
    R_ckc = k.res("ckc")
    VCf = ar.al([128, 512], F32)
    R_VCf = k.res("VCf")
    ctmp = ar.al([128, 4, 32], F32)
    R_ctmp = k.res("ctmp")
    for xi, nm in enumerate(("wck", "wckp", "wcv")):
        for g in range(2):
            k.dma("pool", WC[xi][64 * g:64 * g + 64].rearrange("p l e -> p (l e)"), Dr[nm], writes=[R_WC])
    k.dma("pool", posT, Dr["posT"], writes=[R_WC])
    k.op("dve", lambda e: e.memset(Sst, 0.0), writes=[R_S])
    k.op("dve", lambda e: e.memset(St.VsA[:, :, :, 64:65], 1.0), writes=[St.R_VsA])
    k.op("dve", lambda e: e.memset(St.VwA[:, :, :, 64:65], 1.0), writes=[St.R_VwA])
    for a in range(2):
        k.op("dve", lambda e, a=a: e.memset(xin[a][0][:, 0:16], 0.0), writes=[R_xin[a][0]])
    p6 = c.psum[6]
    fns = []
    for xi in range(3):
        for g in range(2):
            for l in range(32):
                fns.append(lambda e, xi=xi, g=g, l=l: e.matmul(p6[64 * g:64 * g + 64, xi:xi + 1], lhsT=WC[xi][64 * g:64 * g + 64, l, :],
                                                               rhs=posT[64 * g:64 * g + 64, l:l + 1], start=(l == 0), stop=(l == 31)))
    k.op("pe", fns, reads=[R_WC], writes=[c.R_ps[6]])
    k.op("dve", lambda e: e.tensor_copy(out=cb[:, 0:3], in_=p6[:, 0:3]), reads=[c.R_ps[6]], writes=[R_WC])

    NG = c.n_groups
    Hs = [W, W]
    vtoks, R_vtoks = [vtok, vtok], [R_vtok, R_vtok]
    p6b = c.psum[6].bitcast(BF16)

    def fm(ft, bank):
        k.op("pe", [(lambda e, i=i: e.matmul(c.psum[bank], lhsT=w1f[:, i, ft * 128:(ft + 1) * 128], rhs=hT[:, i, :],
                                             start=(i == 0), stop=(i == 7))) for i in range(8)],
             reads=[R_w1, R_hT], writes=[c.R_ps[bank]])

    def A_x(G):
        norm_transpose_group(k, c, W, Dr["xb"], G * 512, hT, R_hT)
        k.dma("sp", CKg, Dr["CK"][:, G * 512:(G + 1) * 512], writes=[R_rt])
        k.dma("sp", SKg, Dr["SK"][:, G * 512:(G + 1) * 512], writes=[R_rt])

    def A_kv(G):
        xb_ = G % 2
        for a in range(2):
            fm(a, 2 + a)
            k.op("act", lambda e, a=a: e.activation(out=xin[a][xb_][:, 16:528], in_=c.psum[2 + a], func=AF.Copy),
                 reads=[c.R_ps[2 + a]], writes=[R_xin[a][xb_]])
            k.op("pool", lambda e, a=a: e.tensor_copy(out=xin[a][1 - xb_][:, 0:16], in_=xin[a][xb_][:, 512:528]),
                 reads=[R_xin[a][xb_]], writes=[R_xin[a][1 - xb_]])
        for which, dst, R_dst in ((0, St.KTs, St.R_KTs), (1, St.KTw, St.R_KTw)):
            fm(2 + 2 * which, 2)
            fm(3 + 2 * which, 3)
            k.op("dve", lambda e: e.tensor_tensor(out=t1, in0=c.psum[2], in1=CKg, op=ALU.mult), reads=[c.R_ps[2], R_rt], writes=[R_t12])
            k.op("dve", lambda e: e.tensor_tensor(out=t2, in0=c.psum[3], in1=SKg, op=ALU.mult), reads=[c.R_ps[3], R_rt, R_t12], writes=[R_t12])
            k.op("pool", lambda e, dst=dst: e.tensor_tensor(out=dst[:, G * 512:(G + 1) * 512], in0=t1, in1=t2, op=ALU.add),
                 reads=[R_t12], writes=[R_dst])

    def A_tok(G):
        vt, R_vt = vtoks[G % 2], R_vtoks[G % 2]
        for tt in range(4):
            tile_ = 4 * G + tt
            k.op("pe", [(lambda e, i=i, tt=tt: e.matmul(c.psum[4][:, 0:256], lhsT=hT[:, i, tt * 128:(tt + 1) * 128], rhs=w1t[:, i, 0:256],
                                                        start=(i == 0), stop=(i == 7))) for i in range(8)],
                 reads=[R_w1, R_hT], writes=[c.R_ps[4]])
            k.op("pe", [(lambda e, i=i, tt=tt: e.matmul(c.psum[5], lhsT=hT[:, i, tt * 128:(tt + 1) * 128], rhs=w1t[:, i, 256:768],
                                                        start=(i == 0), stop=(i == 7))) for i in range(8)],
                 reads=[R_w1, R_hT], writes=[c.R_ps[5]])
            k.op("act", lambda e, tile_=tile_: e.activation(out=St.VsA[:, tile_, :, 0:64],
                                                            in_=c.psum[4][:, 0:128].rearrange("p (g d) -> p g d", g=2), func=AF.Copy),
                 reads=[c.R_ps[4]], writes=[St.R_VsA])
            k.op("act", lambda e, tile_=tile_: e.activation(out=St.VwA[:, tile_, :, 0:64],
                                                            in_=c.psum[4][:, 128:256].rearrange("p (g d) -> p g d", g=2), func=AF.Copy),
                 reads=[c.R_ps[4]], writes=[St.R_VwA])
            k.op("dve", lambda e, tt=tt: e.tensor_copy(out=vt[:, tt, :], in_=c.psum[5]), reads=[c.R_ps[5]], writes=[R_vt])

    def A_conv(G):
        xb_ = G % 2
        fns = []
        for xi in range(3):
            src = xin[0][xb_] if xi < 2 else xin[1][xb_]
            for g in range(2):
                for l in range(32):
                    fns.append(lambda e, xi=xi, g=g, l=l, src=src: e.matmul(
                        p6[64 * g:64 * g + 64, 32 * xi:32 * xi + 32], lhsT=WC[xi][64 * g:64 * g + 64, l, :],
                        rhs=src[64 * g:64 * g + 64, l:l + 497:16], start=(l == 0), stop=(l == 31)))
        k.op("pe", fns, reads=[R_WC, R_xin[0][xb_], R_xin[1][xb_]], writes=[c.R_ps[6]])
        ms = slice(32 * G, 32 * G + 32)
        k.dma("sp", CKc, Dr["CKc"][:, ms], writes=[R_ckc])
        k.dma("sp", SKc, Dr["SKc"][:, ms], writes=[R_ckc])
        k.op("dve", lambda e: e.tensor_scalar(out=ctmp[:, 0, :], in0=p6[:, 0:32], scalar1=cb[:, 0:1], scalar2=None, op0=ALU.add),
             reads=[c.R_ps[6], R_WC], writes=[R_ctmp])
        k.op("dve", lambda e: e.tensor_scalar(out=ctmp[:, 1, :], in0=p6[:, 32:64], scalar1=cb[:, 1:2], scalar2=None, op0=ALU.add),
             reads=[c.R_ps[6], R_WC], writes=[R_ctmp])
        k.op("dve", lambda e: e.tensor_scalar(out=VCf[:, ms], in0=p6[:, 64:96], scalar1=cb[:, 2:3], scalar2=None, op0=ALU.add),
             reads=[c.R_ps[6], R_WC], writes=[R_VCf])
        k.op("pool", lambda e: e.tensor_tensor(out=ctmp[:, 0, :], in0=ctmp[:, 0, :], in1=CKc, op=ALU.mult),
             reads=[R_ctmp, R_ckc], writes=[R_ctmp])
        k.op("pool", lambda e: e.tensor_tensor(out=ctmp[:, 1, :], in0=ctmp[:, 1, :], in1=SKc, op=ALU.mult),
             reads=[R_ctmp, R_ckc], writes=[R_ctmp])
        k.op("pool", lambda e: e.tensor_tensor(out=St.KC[:, ms], in0=ctmp[:, 0, :], in1=ctmp[:, 1, :], op=ALU.add),
             reads=[R_ctmp], writes=[St.R_KC])

    def A_f(G):
        H = Hs[G % 2]

        def front(hd):
            bank = 2 + hd % 2
            fm(6 + hd, bank)
            f_front(k, c, W, c.psum[bank], c.R_ps[bank], hd)
        front(0)
        front(1)
        f_back(k, c, W, 0, H)
        front(2)
        f_back(k, c, W, 1, H)
        front(3)
        f_back(k, c, W, 2, H)
        f_back(k, c, W, 3, H)

    def B_step(G, tt):
        H = Hs[G % 2]
        vt, R_vt = vtoks[G % 2], R_vtoks[G % 2]
        sl = slice(tt * 128, (tt + 1) * 128)
        k.op("pe", [(lambda e, hd=hd: e.transpose(out=p6b[:, hd * 128:(hd + 1) * 128], in_=H.kT[:, hd, sl], identity=c.identb))
                    for hd in range(4)], reads=[H.R_kT, c.R_const], writes=[c.R_ps[6]])
        k.op("act", lambda e: e.activation(out=ktok, in_=p6b[:, 0:512].rearrange("p (h x) -> p h x", h=4), func=AF.Copy),
             reads=[c.R_ps[6]], writes=[R_ktok])
        k.op("pe", [(lambda e, hd=hd: e.matmul(c.psum[7][:, hd * 128:(hd + 1) * 128], lhsT=ktok[:, hd, :],
                                               rhs=vt[:, tt, hd * 128:(hd + 1) * 128], start=True, stop=True))
                    for hd in range(4)], reads=[R_ktok, R_vt], writes=[c.R_ps[7]])
        Sf, Af = Sst.rearrange("p h x -> p (h x)"), snapacc.rearrange("p h x -> p (h x)")
        if tt == 0:
            k.op("dve", lambda e: e.tensor_scalar(out=Af, in0=Sf, scalar1=c.onehot[:, 0:1], scalar2=None, op0=ALU.mult),
                 reads=[R_S, c.R_const], writes=[R_snapacc])
        else:
            k.op("dve", lambda e: e.scalar_tensor_tensor(out=Af, in0=Sf, scalar=c.onehot[:, tt:tt + 1], in1=Af,
                                                         op0=ALU.mult, op1=ALU.add),
                 reads=[R_S, c.R_const, R_snapacc], writes=[R_snapacc])
        for hd in range(4):
            k.op("dve", lambda e, hd=hd: e.tensor_scalar(out=Sst[:, hd, :], in0=Sst[:, hd, :], scalar1=H.c1[:, hd, tt:tt + 1],
                                                         scalar2=None, op0=ALU.mult),
                 reads=[R_S, H.R_cols], writes=[R_S])
            k.op("dve", lambda e, hd=hd: e.scalar_tensor_tensor(
                out=Sst[:, hd, :], in0=c.psum[7][:, hd * 128:(hd + 1) * 128], scalar=H.c2[:, hd, tt:tt + 1], in1=Sst[:, hd, :],
                op0=ALU.mult, op1=ALU.add), reads=[c.R_ps[7], R_S, H.R_cols], writes=[R_S])
        if tt == 3:
            k.op("act", lambda e: e.activation(out=St.SNAP[:, G, :, :], in_=snapacc, func=AF.Copy), reads=[R_snapacc], writes=[St.R_SNAP])

    for G in range(NG + 1):
        if G < NG:
            A_x(G)
        if G >= 1:
            B_step(G - 1, 0)
            B_step(G - 1, 1)
        if G < NG:
            A_kv(G)
        if G >= 1:
            B_step(G - 1, 2)
            B_step(G - 1, 3)
        if G < NG:
            A_tok(G)
            A_conv(G)
            A_f(G)
    k.op("dve", lambda e: e.memset(St.VCA[:, :, :, 64:65], 1.0), writes=[St.R_VCA])
    for g in range(2):
        k.dma("pool", St.VCA[:, :, g, 65:193], Dr["ovl"], writes=[St.R_VCA])
    pw = c.psum[6]
    k.op("pe", [(lambda e, mt=mt: e.transpose(out=pw[:, mt * 128:(mt + 1) * 128], in_=VCf[:, mt * 128:(mt + 1) * 128], identity=c.identf))
                for mt in range(4)], reads=[R_VCf, c.R_const], writes=[c.R_ps[6]])
    for mt in range(4):
        k.op("act", lambda e, mt=mt: e.activation(out=St.VCA[:, mt, :, 0:64],
                                                  in_=pw[:, mt * 128:(mt + 1) * 128].rearrange("p (g d) -> p g d", g=2), func=AF.Copy),
             reads=[c.R_ps[6]], writes=[St.R_VCA])
    k.op("dve", lambda e: e.memset(St.VCA[0:1, 0, :, :], 0.0), writes=[St.R_VCA])
    ar.release(m0)


def phase_p2pre(k, c, ar, St):
    Dr = c.D
    m0 = ar.mark()
    W = WS()
    alloc_hg_ws(k, ar, W, nsets=2)
    alloc_x_ws(k, c, ar, W, None, scr=W.ab, R_scr=W.R_f)
    hT = ar.al([128, 8, 512], BF16)
    R_hT = k.res("hT2")
    wch = [c.R32f[:, 8192 + b * 4096: 8192 + (b + 1) * 4096].rearrange("p (a b) -> p a b", a=8) for b in range(2)]
    R_wch = [k.res("wch%d" % i) for i in range(2)]
    wgt = ar.al([128, 8, 32], BF16)
    R_wgt = k.res("wgt")
    wst = dict(n=0)
    CQg, SQg = ar.al([128, 512], F32), ar.al([128, 512], F32)
    R_rt = k.res("ropetabq")
    t1, t2, R_t12 = W.u, W.a, W.R_f
    qT = ar.al([128, 4, 512], BF16)
    R_qT = k.res("qTh")
    e1, R_e1 = W.u, W.R_f
    vtok = ar.al([128, 512], BF16)
    R_vtok = k.res("vtok2")
    sgt = ar.al([128, 512], F32)
    R_sgt = k.res("sgt")
    AT = ar.al([128, 4, 128], BF16)
    R_AT = k.res("AT")
    Sp = ar.al([128, 4, 128], BF16)
    R_Sp = k.res("Sp")
    gnt = ar.al([128, 512], F32)
    R_gnt = k.res("gnt")
    o1, o2, R_o = W.bq, W.a, W.R_f
    yb = ar.al([128, 512], BF16)
    R_yb = k.res("yb")
    hs = ar.al([128, 16], F32)
    R_hs = k.res("hs")
    k.dma("pool", wgt, Dr["w2t"][:, 1024:1056].rearrange("(kt p) n -> p kt n", p=128), writes=[R_wgt])
    k.dma("sp", gnt, Dr["g_hg4"].partition_broadcast(128), writes=[R_gnt])

    def wload(src, c0, n=512):
        b = wst["n"] % 2
        wst["n"] += 1
        k.dma("pool", wch[b][:, :, 0:n], src[:, c0:c0 + n].rearrange("(kt p) n -> p kt n", p=128), writes=[R_wch[b]])
        return wch[b], R_wch[b]

    for go in range(NT // 4):
        tok = slice(go * 512, (go + 1) * 512)
        norm_transpose_group(k, c, W, Dr["xo"], go * 512, hT, R_hT)
        k.dma("sp", CQg, Dr["CQ"][:, tok], writes=[R_rt])
        k.dma("sp", SQg, Dr["SQ"][:, tok], writes=[R_rt])

        def fm(wt, R_wt, j, bank):
            k.op("pe", [(lambda e, i=i: e.matmul(c.psum[bank], lhsT=wt[:, i, j * 128:(j + 1) * 128], rhs=hT[:, i, :],
                                                 start=(i == 0), stop=(i == 7))) for i in range(8)],
                 reads=[R_wt, R_hT], writes=[c.R_ps[bank]])
        wq, R_wq = wload(Dr["w2f"], 0)
        wqp, R_wqp = wload(Dr["w2f"], 512)
        for a in range(4):
            fm(wq, R_wq, a, 2)
            fm(wqp, R_wqp, a, 3)
            k.op("dve", lambda e: e.tensor_tensor(out=t1, in0=c.psum[2], in1=CQg, op=ALU.mult), reads=[c.R_ps[2], R_rt], writes=[R_t12])
            k.op("dve", lambda e: e.tensor_tensor(out=t2, in0=c.psum[3], in1=SQg, op=ALU.mult), reads=[c.R_ps[3], R_rt, R_t12], writes=[R_t12])
            k.op("pool", lambda e, a=a: e.tensor_tensor(out=c.QT[:, 4 * go:4 * go + 4, a, :], in0=t1.rearrange("p (i t) -> p i t", i=4),
                                                        in1=t2.rearrange("p (i t) -> p i t", i=4), op=ALU.add), reads=[R_t12], writes=[c.R_QT])
        whq, R_whq = wload(Dr["w2f"], 1024)
        whf, R_whf = wload(Dr["w2f"], 1536)
        def front(hd):
            bank = 2 + hd % 2
            fm(whf, R_whf, hd, bank)
            f_front(k, c, W, c.psum[bank], c.R_ps[bank], hd)

        def back(hd):
            f_back(k, c, W, hd)
            su, sa, sbq, slk, sL, sRF = W.sets[hd % 2]
            fm(whq, R_whq, hd, 6)
            for tt in range(4):
                sl = slice(tt * 128, (tt + 1) * 128)
                k.op("act", lambda e, sl=sl, tt=tt: e.activation(out=su[:, sl], in_=sL[:, sl], func=AF.Exp, bias=W.negr[:, hd, tt:tt + 1]),
                     reads=[sRF, W.R_cols], writes=[sRF])
            k.op("dve", lambda e: e.tensor_tensor(out=qT[:, hd, :], in0=c.psum[6], in1=su, op=ALU.mult),
                 reads=[c.R_ps[6], sRF], writes=[R_qT])
        front(0)
        front(1)
        back(0)
        front(2)
        back(1)
        front(3)
        back(2)
        back(3)
        whi, R_whi = wload(Dr["w2t"], 0)
        whg, R_whg = wload(Dr["w2t"], 512)
        for tt in range(4):
            i_own = 4 * go + tt
            sl = slice(tt * 128, (tt + 1) * 128)
            for (wt, R_wt, n, bank) in ((whi, R_whi, 512, 4), (whg, R_whg, 512, 5), (wgt, R_wgt, 32, 6)):
                k.op("pe", [(lambda e, i=i, wt=wt, n=n, bank=bank: e.matmul(c.psum[bank][:, 0:n], lhsT=hT[:, i, sl], rhs=wt[:, i, 0:n],
                                                                            start=(i == 0), stop=(i == 7))) for i in range(8)],
                     reads=[R_wt, R_hT], writes=[c.R_ps[bank]])
            k.op("dve", lambda e: e.tensor_copy(out=vtok, in_=c.psum[4]), reads=[c.R_ps[4]], writes=[R_vtok])
            k.op("act", lambda e: e.activation(out=sgt, in_=c.psum[5], func=AF.Silu), reads=[c.R_ps[5]], writes=[R_sgt])
            k.op("act", lambda e, i_own=i_own: e.activation(out=c.gsig[:, i_own, :], in_=c.psum[6][:, 0:24], func=AF.Sigmoid),
                 reads=[c.R_ps[6]], writes=[c.R_gsig])
            k.op("pe", [(lambda e, hd=hd: e.matmul(c.psum[7][:, hd * 128:(hd + 1) * 128], lhsT=W.kT[:, hd, sl], rhs=qT[:, hd, sl],
                                                   start=True, stop=True)) for hd in range(4)],
                 reads=[W.R_kT, R_qT], writes=[c.R_ps[7]])
            k.op("dve", lambda e: e.tensor_scalar(out=W.lk, in0=c.psum[7], scalar1=1e30, scalar2=-1e30, op0=ALU.min, op1=ALU.max),
                 reads=[c.R_ps[7], W.R_f], writes=[W.R_f])
            k.op("dve", lambda e: e.tensor_tensor(out=AT, in0=W.lk.rearrange("p (h x) -> p h x", h=4),
                                                  in1=c.tri.unsqueeze(1).broadcast_to([128, 4, 128]), op=ALU.mult),
                 reads=[W.R_f, c.R_const], writes=[R_AT])
            for hd in range(4):
                k.op("act", lambda e, hd=hd, tt=tt, i_own=i_own: e.activation(out=Sp[:, hd, :], in_=St.SNAP[:, i_own, hd, :], func=AF.Copy,
                                                                              scale=W.er[:, hd, tt:tt + 1]),
                     reads=[St.R_SNAP, W.R_cols], writes=[R_Sp])
            fns = []
            for hd in range(4):
                fns.append(lambda e, hd=hd, tt=tt: e.matmul(c.psum[4][:, hd * 128:(hd + 1) * 128], lhsT=AT[:, hd, :],
                                                            rhs=vtok[:, hd * 128:(hd + 1) * 128], start=True, stop=False))
                fns.append(lambda e, hd=hd: e.matmul(c.psum[4][:, hd * 128:(hd + 1) * 128], lhsT=qT[:, hd, sl],
                                                     rhs=Sp[:, hd, :], start=False, stop=True))
            k.op("pe", fns, reads=[R_AT, R_vtok, R_qT, R_Sp], writes=[c.R_ps[4]])
            for hd in range(4):
                k.op("act", lambda e, hd=hd: e.activation(out=o2[:, hd * 128:(hd + 1) * 128], in_=c.psum[4][:, hd * 128:(hd + 1) * 128],
                                                          func=AF.Square, accum_out=hs[:, hd:hd + 1]),
                     reads=[c.R_ps[4]], writes=[R_o, R_hs])
            k.op("act", lambda e: e.activation(out=hs[:, 4:8], in_=hs[:, 0:4], func=AF.Sqrt, scale=1.0 / 128, bias=c.epsb[:, 0:1]),
                 reads=[R_hs, c.R_const], writes=[R_hs])
            k.op("dve", lambda e: e.reciprocal(out=hs[:, 8:12], in_=hs[:, 4:8]), reads=[R_hs], writes=[R_hs])
            k.op("dve", lambda e: e.tensor_tensor(out=o1, in0=c.psum[4], in1=gnt, op=ALU.mult), reads=[c.R_ps[4], R_gnt, R_o], writes=[R_o])
            k.op("pool", lambda e, tt=tt: e.tensor_tensor(out=o1, in0=o1, in1=sgt, op=ALU.mult), reads=[R_o, R_sgt], writes=[R_o])
            k.op("dve", lambda e: e.tensor_tensor(out=yb.rearrange("p (h x) -> p h x", h=4), in0=o1.rearrange("p (h x) -> p h x", h=4),
                                                  in1=hs[:, 8:12].unsqueeze(2).broadcast_to([128, 4, 128]), op=ALU.mult),
                 reads=[R_o, R_hs], writes=[R_yb])
            p6b = c.psum[6].bitcast(BF16)
            k.op("pe", [(lambda e, hd=hd: e.transpose(out=p6b[:, hd * 128:(hd + 1) * 128], in_=yb[:, hd * 128:(hd + 1) * 128], identity=c.identb))
                        for hd in range(4)], reads=[R_yb, c.R_const], writes=[c.R_ps[6]])
            k.op("act", lambda e, i_own=i_own: e.activation(out=c.oT_hg[:, :, i_own * 128:(i_own + 1) * 128],
                                                            in_=p6b[:, 0:512].rearrange("p (h x) -> p h x", h=4), func=AF.Copy),
                 reads=[c.R_ps[6]], writes=[c.R_oThg])
    ar.release(m0)


def phase_nsa(k, c, ar, St):
    Dr = c.D
    m0 = ar.mark()
    TINY = 1e-30
    CBi = [ar.al([128, 128], BF16) for _ in range(2)]
    VALi = [ar.al([128, 128], F32) for _ in range(2)]
    ADDCi = [ar.al([128, 128], F32) for _ in range(2)]
    R_tab = [k.res("nsatab%d" % i) for i in range(2)]
    WMT = ar.al([128, 8, 128], BF16)
    CM = ar.al([128, 4, 128], BF16)
    R_cst = k.res("nsacst")
    k.dma("pool", WMT, Dr["WMT"], writes=[R_cst])
    k.dma("pool", CM, Dr["CM"], writes=[R_cst])
    PT = [ar.al([128, 512], BF16) for _ in range(3)]
    R_PT = [k.res("PT%d" % i) for i in range(3)]
    Uc = ar.al([128, 4, 193], F32)
    R_Uc = k.res("Uc")
    Os = ar.al([128, 4, 65], F32)
    Ow = ar.al([128, 4, 65], F32)
    R_Os, R_Ow = k.res("Os"), k.res("Ow")
    score, sc2, imp = ar.al([128, 128], F32), ar.al([128, 128], F32), ar.al([128, 128], F32)
    R_sel = k.res("sel")
    selb = ar.al([128, 128], BF16)
    R_selb = k.res("selb")
    bd = ar.al([128, 4, 128], BF16)
    R_bd = k.res("bd")
    selX = ar.al([128, 128, 64], BF16)
    R_selX = k.res("selX")
    cols = ar.al([128, 64], F32)
    R_cols = k.res("nsacols")
    acc, tmp = ar.al([128, 4, 64], F32), ar.al([128, 4, 64], F32)
    R_acc = k.res("nsaacc")
    onsa = ar.al([128, 2, 4, 64], BF16)
    R_onsa = k.res("onsa")
    st = dict(s=0, p=0)
    pO_s, pO_w = c.psum[3][:, 0:260], c.psum[4][:, 0:260]
    pU = [c.psum[5], c.psum[6]]

    pend = []

    def flush_pv(keep=0):
        while len(pend) > keep:
            pend.pop(0)()

    def unit(KT, R_KT, kt_slice, QTg, g, bias, Vaug, R_V, outs, R_outs):
        sb = st["s"] % 3
        st["s"] += 1
        pb = st["p"] % 3
        st["p"] += 1
        S = c.psum[sb]
        fns = [lambda e: e.matmul(S, lhsT=KT[64 * g:64 * g + 64, kt_slice], rhs=QTg, start=True, stop=(bias is None))]
        rd = [R_KT, c.R_QT]
        if bias is not None:
            bl, R_bl = bias
            fns.append(lambda e: e.matmul(S, lhsT=bl, rhs=c.I4, start=False, stop=True))
            rd += [R_bl, c.R_const]
        k.op("pe", fns, reads=rd, writes=[c.R_ps[sb]])
        k.op("act", lambda e: e.activation(out=PT[pb], in_=S, func=AF.Exp), reads=[c.R_ps[sb]], writes=[R_PT[pb]])

        def pv():
            k.op("pe", [(lambda e, a=a: e.matmul(outs[a], lhsT=PT[pb][:, a * 128:(a + 1) * 128], rhs=Vaug, start=False, stop=False,
                                                 skip_group_check=True)) for a in range(4)],
                 reads=[R_PT[pb], R_V], writes=R_outs)
        pend.append(pv)
        flush_pv(keep=2)

    for i in range(c.n_blocks):
        tb = i % 2
        k.dma("pool", CBi[tb], Dr["CB"][:, i, :], writes=[R_tab[tb]])
        k.dma("sp", VALi[tb], Dr["VAL"][:, i, :], writes=[R_tab[tb]])
        k.dma("sp", ADDCi[tb], Dr["ADDC"][:, i, :], writes=[R_tab[tb]])
        for g in range(2):
            QTg = c.QT[64 * g:64 * g + 64, i, :, :].rearrange("p a t -> p (a t)")
            nmt = i // 4 + 1
            k.op("dve", lambda e: e.memset(pU[0], 0.0), writes=[c.R_ps[5]])
            k.op("dve", lambda e: e.memset(pU[1], 0.0), writes=[c.R_ps[6]])
            outsU = [pU[a // 2][:, (a % 2) * 193:(a % 2) * 193 + 193] for a in range(4)]
            for mt in range(nmt):
                bias = (CBi[tb], R_tab[tb]) if mt == nmt - 1 else None
                unit(St.KC, St.R_KC, slice(mt * 128, (mt + 1) * 128), QTg, g, bias, St.VCA[:, mt, g, :], St.R_VCA, outsU, [c.R_ps[5], c.R_ps[6]])
            flush_pv()
            k.op("act", lambda e: e.activation(out=Uc[:, 0:2, :], in_=pU[0][:, 0:386].rearrange("p (a x) -> p a x", a=2), func=AF.Copy),
                 reads=[c.R_ps[5]], writes=[R_Uc])
            k.op("act", lambda e: e.activation(out=Uc[:, 2:4, :], in_=pU[1][:, 0:386].rearrange("p (a x) -> p a x", a=2), func=AF.Copy),
                 reads=[c.R_ps[6]], writes=[R_Uc])
            k.op("dve", lambda e: e.memset(c.psum[4], 0.0), writes=[c.R_ps[4]])
            outsW = [pO_w[:, a * 65:(a + 1) * 65] for a in range(4)]
            for r in range(8):
                kt = 4 * i - 4 + r
                if kt < 0:
                    continue
                unit(St.KTw, St.R_KTw, slice(kt * 128, (kt + 1) * 128), QTg, g, (WMT[:, r, :], R_cst), St.VwA[:, kt, g, :], St.R_VwA, outsW, [c.R_ps[4]])
            zc, rzc = cols[:, 0:4], cols[:, 4:8]
            k.op("dve", lambda e: e.tensor_scalar(out=zc, in0=Uc[:, :, 64], scalar1=TINY, scalar2=None, op0=ALU.max), reads=[R_Uc], writes=[R_cols])
            k.op("dve", lambda e: e.reciprocal(out=rzc, in_=zc), reads=[R_cols], writes=[R_cols])
            k.op("dve", lambda e: e.tensor_scalar(out=imp, in0=Uc[:, 0, 65:193], scalar1=rzc[:, 0:1], scalar2=None, op0=ALU.mult),
                 reads=[R_Uc, R_cols], writes=[R_sel])
            for a in range(1, 4):
                k.op("dve", lambda e, a=a: e.scalar_tensor_tensor(out=imp, in0=Uc[:, a, 65:193], scalar=rzc[:, a:a + 1], in1=imp,
                                                                  op0=ALU.mult, op1=ALU.add), reads=[R_Uc, R_cols, R_sel], writes=[R_sel])
            k.op("dve", lambda e: e.tensor_tensor(out=score, in0=imp, in1=VALi[tb], op=ALU.mult), reads=[R_sel, R_tab[tb]], writes=[R_sel])
            k.op("dve", lambda e: e.tensor_tensor(out=score, in0=score, in1=ADDCi[tb], op=ALU.add), reads=[R_sel, R_tab[tb]], writes=[R_sel])
            m8a, m8b = cols[:, 8:16], cols[:, 16:24]
            k.op("dve", lambda e: e.max(out=m8a, in_=score), reads=[R_sel], writes=[R_cols])
            k.op("dve", lambda e: e.match_replace(out=sc2, in_to_replace=m8a, in_values=score, imm_value=-1e9), reads=[R_sel, R_cols], writes=[R_sel])
            k.op("dve", lambda e: e.max(out=m8b, in_=sc2), reads=[R_sel], writes=[R_cols])
            k.op("dve", lambda e: e.tensor_scalar(out=selb, in0=score, scalar1=m8b[:, 7:8], scalar2=NEG, op0=ALU.is_lt, op1=ALU.mult),
                 reads=[R_sel, R_cols], writes=[R_selb])
            for r in range(4):
                kt = 4 * i + r
                k.op("dve", lambda e, r=r, kt=kt: e.tensor_tensor(
                    out=bd[:, r, :].rearrange("p (b x) -> p b x", b=2), in0=CM[:, r, :].rearrange("p (b x) -> p b x", b=2),
                    in1=selb[:, 2 * kt:2 * kt + 2].unsqueeze(2).broadcast_to([128, 2, 64]), op=ALU.add),
                    reads=[R_cst, R_selb], writes=[R_bd])
            if i > 0:
                nbk = 8 * i
                k.op("pool", lambda e, nbk=nbk: e.tensor_copy(out=selX[:, 0:nbk, :], in_=selb[:, 0:nbk].unsqueeze(2).broadcast_to([128, nbk, 64])),
                     reads=[R_selb], writes=[R_selX])
            k.op("dve", lambda e: e.memset(c.psum[3], 0.0), writes=[c.R_ps[3]])
            outsS = [pO_s[:, a * 65:(a + 1) * 65] for a in range(4)]
            for kt in range(4 * i + 4):
                if kt < 4 * i:
                    bl = selX[:, 2 * kt:2 * kt + 2, :].rearrange("p b x -> p (b x)")
                    bias = (bl, R_selX)
                else:
                    bias = (bd[:, kt - 4 * i, :], R_bd)
                unit(St.KTs, St.R_KTs, slice(kt * 128, (kt + 1) * 128), QTg, g, bias, St.VsA[:, kt, g, :], St.R_VsA, outsS, [c.R_ps[3]])
            flush_pv()
            k.op("act", lambda e: e.activation(out=Os, in_=pO_s.rearrange("p (a x) -> p a x", a=4), func=AF.Copy), reads=[c.R_ps[3]], writes=[R_Os])
            k.op("act", lambda e: e.activation(out=Ow, in_=pO_w.rearrange("p (a x) -> p a x", a=4), func=AF.Copy), reads=[c.R_ps[4]], writes=[R_Ow])
            gs = c.gsig[:, i, 12 * g:12 * g + 12].rearrange("p (a x) -> p a x", a=4)
            zs, zw, cfc, cfs, cfw = cols[:, 24:28], cols[:, 28:32], cols[:, 32:36], cols[:, 36:40], cols[:, 40:44]
            k.op("dve", lambda e: e.tensor_scalar(out=zs, in0=Os[:, :, 64], scalar1=TINY, scalar2=None, op0=ALU.max), reads=[R_Os], writes=[R_cols])
            k.op("dve", lambda e: e.tensor_scalar(out=zw, in0=Ow[:, :, 64], scalar1=TINY, scalar2=None, op0=ALU.max), reads=[R_Ow], writes=[R_cols])
            k.op("dve", lambda e: e.reciprocal(out=zs, in_=zs), reads=[R_cols], writes=[R_cols])
            k.op("dve", lambda e: e.reciprocal(out=zw, in_=zw), reads=[R_cols], writes=[R_cols])
            k.op("dve", lambda e: e.tensor_tensor(out=cfc, in0=rzc, in1=gs[:, :, 0], op=ALU.mult), reads=[R_cols, c.R_gsig], writes=[R_cols])
            k.op("dve", lambda e: e.tensor_tensor(out=cfs, in0=zs, in1=gs[:, :, 1], op=ALU.mult), reads=[R_cols, c.R_gsig], writes=[R_cols])
            k.op("dve", lambda e: e.tensor_tensor(out=cfw, in0=zw, in1=gs[:, :, 2], op=ALU.mult), reads=[R_cols, c.R_gsig], writes=[R_cols])
            bc = lambda col: col.unsqueeze(2).broadcast_to([128, 4, 64])
            k.op("dve", lambda e: e.tensor_tensor(out=acc, in0=Uc[:, :, 0:64], in1=bc(cfc), op=ALU.mult), reads=[R_Uc, R_cols], writes=[R_acc])
            k.op("dve", lambda e: e.tensor_tensor(out=tmp, in0=Os[:, :, 0:64], in1=bc(cfs), op=ALU.mult), reads=[R_Os, R_cols, R_acc], writes=[R_acc])
            k.op("pool", lambda e: e.tensor_tensor(out=acc, in0=acc, in1=tmp, op=ALU.add), reads=[R_acc], writes=[R_acc])
            k.op("dve", lambda e: e.tensor_tensor(out=tmp, in0=Ow[:, :, 0:64], in1=bc(cfw), op=ALU.mult), reads=[R_Ow, R_cols, R_acc], writes=[R_acc])
            k.op("pool", lambda e, g=g: e.tensor_tensor(out=onsa[:, g, :, :], in0=acc, in1=tmp, op=ALU.add), reads=[R_acc], writes=[R_onsa])
        p7b = c.psum[7].bitcast(BF16)
        of = onsa.rearrange("p g a d -> p (g a d)")
        k.op("pe", [(lambda e, j=j: e.transpose(out=p7b[:, j * 128:(j + 1) * 128], in_=of[:, j * 128:(j + 1) * 128], identity=c.identb))
                    for j in range(4)], reads=[R_onsa, c.R_const], writes=[c.R_ps[7]])
        k.op("act", lambda e, i=i: e.activation(out=c.oT_nsa[:, :, i * 128:(i + 1) * 128],
                                                in_=p7b[:, 0:512].rearrange("p (j x) -> p j x", j=4), func=AF.Copy),
             reads=[c.R_ps[7]], writes=[c.R_oTnsa])
    ar.release(m0)


def phase_p2c(k, c, ar, X1, R_X1):
    Dr = c.D
    m0 = ar.mark()
    W = WS()
    scr = ar.al([128, D], F32)
    alloc_x_ws(k, c, ar, W, None, scr=scr, R_scr=k.res("scr2c"))
    hT = ar.al([128, 8, 512], BF16)
    R_hT = k.res("hT3")
    wbn, wbh = ar.al([128, 4, D], BF16), ar.al([128, 4, D], BF16)
    R_wb_ = k.res("wbr")
    wb = [ar.al([128, 8, 512], BF16) for _ in range(4)]
    R_wb = [k.res("wchc%d" % i) for i in range(4)]
    mixT = ar.al([128, 8, 512], BF16)
    R_mixT = k.res("mixT")
    sg1, sg2, mx1 = ar.al([128, 512], F32), ar.al([128, 512], F32), ar.al([128, 512], F32)
    R_sg1, R_sg2, R_mx = k.res("sg1"), k.res("sg2"), k.res("mx1")
    k.dma("pool", wbn, Dr["w_brn"].rearrange("(kt p) n -> p kt n", p=128), writes=[R_wb_])
    k.dma("pool", wbh, Dr["w_brh"].rearrange("(kt p) n -> p kt n", p=128), writes=[R_wb_])

    def wl(buf, src, c0):
        k.dma("pool", wb[buf], src[:, c0:c0 + 512].rearrange("(kt p) n -> p kt n", p=128), writes=[R_wb[buf]])

    NGo = NT // 4
    wl(0, Dr["w_mg"], 0)
    wl(1, Dr["w_mg"], 1024)
    for go in range(NGo):
        p = go % 2
        A = (2 * p, 2 * p + 1)
        B = (2 - 2 * p, 3 - 2 * p)
        tok = slice(go * 512, (go + 1) * 512)
        for tt in range(4):
            t = 4 * go + tt
            k.dma("sp", X1[:, t, :], Dr["xo"][t * 128:(t + 1) * 128, :], writes=[R_X1[t]])
        wl(B[0], Dr["w_mg"], 512)
        wl(B[1], Dr["w_mg"], 1024 + 512)
        norm_transpose_group(k, c, W, Dr["xo"], go * 512, hT, R_hT)
        for hf in range(2):
            w0, w1_ = (A if hf == 0 else B)
            for f4 in range(4):
                ft = hf * 4 + f4
                fs = slice(f4 * 128, (f4 + 1) * 128)
                gs_ = slice(ft * 128, (ft + 1) * 128)
                k.op("pe", [(lambda e, i=i: e.matmul(c.psum[2], lhsT=wb[w0][:, i, fs], rhs=hT[:, i, :], start=(i == 0), stop=(i == 7)))
                            for i in range(8)], reads=[R_wb[w0], R_hT], writes=[c.R_ps[2]])
                k.op("pe", [(lambda e, i=i: e.matmul(c.psum[3], lhsT=wb[w1_][:, i, fs], rhs=hT[:, i, :], start=(i == 0), stop=(i == 7)))
                            for i in range(8)], reads=[R_wb[w1_], R_hT], writes=[c.R_ps[3]])
                k.op("pe", [(lambda e, i=i: e.matmul(c.psum[4], lhsT=wbn[:, i, gs_], rhs=c.oT_nsa[:, i, tok], start=(i == 0), stop=(i == 3)))
                            for i in range(4)], reads=[R_wb_, c.R_oTnsa], writes=[c.R_ps[4]])
                k.op("pe", [(lambda e, i=i: e.matmul(c.psum[5], lhsT=wbh[:, i, gs_], rhs=c.oT_hg[:, i, tok], start=(i == 0), stop=(i == 3)))
                            for i in range(4)], reads=[R_wb_, c.R_oThg], writes=[c.R_ps[5]])
                k.op("act", lambda e: e.activation(out=sg1, in_=c.psum[2], func=AF.Sigmoid), reads=[c.R_ps[2]], writes=[R_sg1])
                k.op("act", lambda e: e.activation(out=sg2, in_=c.psum[3], func=AF.Sigmoid), reads=[c.R_ps[3]], writes=[R_sg2])
                k.op("dve", lambda e: e.tensor_tensor(out=mx1, in0=c.psum[4], in1=sg1, op=ALU.mult), reads=[c.R_ps[4], R_sg1], writes=[R_mx])
                k.op("dve", lambda e: e.tensor_tensor(out=sg2, in0=c.psum[5], in1=sg2, op=ALU.mult), reads=[c.R_ps[5], R_sg2], writes=[R_sg2])
                k.op("pool", lambda e, ft=ft: e.tensor_tensor(out=mixT[:, ft, :], in0=mx1, in1=sg2, op=ALU.add),
                     reads=[R_mx, R_sg2], writes=[R_mixT])
            if hf == 0:
                wl(A[0], Dr["w_out"], 0)
                wl(A[1], Dr["w_out"], 512)
            elif go + 1 < NGo:
                wl(B[0], Dr["w_mg"], 0)
                wl(B[1], Dr["w_mg"], 1024)
        for tt in range(4):
            t = 4 * go + tt
            for hf in range(2):
                bank = 6 + hf
                k.op("pe", [(lambda e, i=i: e.matmul(c.psum[bank], lhsT=mixT[:, i, tt * 128:(tt + 1) * 128], rhs=wb[A[hf]][:, i, :],
                                                     start=(i == 0), stop=(i == 7))) for i in range(8)],
                     reads=[R_mixT, R_wb[A[hf]]], writes=[c.R_ps[bank]])
                xs = X1[:, t, hf * 512:(hf + 1) * 512]
                k.op("dve", lambda e, xs=xs: e.tensor_tensor(out=xs, in0=c.psum[bank], in1=xs, op=ALU.add),
                     reads=[c.R_ps[bank], R_X1[t]], writes=[R_X1[t]])
    ar.release(m0)


def phase_attn(k, c, ar, stage):
    St = WS()
    St.KTs, St.KTw = ar.al([128, SEQ], BF16), ar.al([128, SEQ], BF16)
    St.VsA, St.VwA = ar.al([128, 64, 2, 65], BF16), ar.al([128, 64, 2, 65], BF16)
    St.KC = ar.al([128, 512], BF16)
    St.VCA = ar.al([128, 4, 2, 193], BF16)
    St.SNAP = ar.al([128, NT, 4, 128], BF16)
    for n in ("KTs", "KTw", "VsA", "VwA", "KC", "VCA", "SNAP"):
        setattr(St, "R_" + n, k.res(n))
    phase_p1(k, c, ar, St)
    for n in ("KTs", "KTw", "VsA", "VwA", "KC", "VCA", "SNAP"):
        c.dump(n, getattr(St, n), [getattr(St, "R_" + n)])
    k.flush()
    phase_p2pre(k, c, ar, St)
    c.dump("QT", c.QT, [c.R_QT])
    c.dump("gsig", c.gsig, [c.R_gsig])
    c.dump("oT_hg", c.oT_hg, [c.R_oThg])
    k.flush()
    phase_nsa(k, c, ar, St)
    c.dump("oT_nsa", c.oT_nsa, [c.R_oTnsa])
    return St


def build(stage="full", n_exp=NE):
    nc = bass.Bass("TRN2", target_bir_lowering=False)
    Dr = {}

    def din(name, shape, dt=F32):
        Dr[name] = nc.dram_tensor(name, list(shape), dt, kind="ExternalInput").ap()

    for name, shape, dt in input_specs(max(n_exp, 1), stage):
        din(name, shape, dt)
    Dr["y"] = nc.dram_tensor("y", [NTOK, D], F32, kind="ExternalOutput").ap()
    with ExitStack() as es:
        ARENA_BYTES = 207 * 1024
        big = es.enter_context(nc.sbuf_tensor("arena", [128, ARENA_BYTES // 2], BF16))
        pst = es.enter_context(nc.psum_tensor("ps", [128, 4096], F32))
        ar = Arena(big, ARENA_BYTES)
        k = KH(nc, es)
        k.oplim = _NC_CACHE.get("oplim", 10 ** 9)
        c = Ctx()
        c.nc, c.D, c.n_exp = nc, Dr, n_exp
        dumps = []

        def dump(name, ap, rs):
            if not _NC_CACHE.get("dbg"):
                return
            dt_ = ap.dtype
            dr = nc.dram_tensor("dbg_" + name, list(ap.shape), dt_, kind="ExternalOutput").ap()
            r = k.res("dbg_" + name)
            k.dma("sp", dr, ap, reads=list(rs), key=r)
            dumps.append(r)
        c.dump = dump
        c.psum = [pst[:, i * 512:(i + 1) * 512] for i in range(8)]
        c.pwide = lambda i: pst[:, i * 512:(i + 2) * 512]
        c.R_ps = [k.res("psb%d" % i, excl=True) for i in range(8)]
        c.R_const = k.res("const")
        c.identf = ar.al([128, 128], F32)
        c.identb = ar.al([128, 128], BF16)
        c.epsb = ar.al([128, 1], F32)
        c.ssq = ar.al([128, 8], F32)
        c.std = ar.al([128, 8], F32)
        c.rstd = ar.al([128, 8], F32)
        c.rs_res = [k.res("rs%d" % i) for i in range(8)]
        c.rs_i = 0
        k.dma("sp", c.identf, Dr["identf"], writes=[c.R_const])
        k.dma("sp", c.identb, Dr["identb"], writes=[c.R_const])
        k.op("dve", lambda e: e.memset(c.epsb, EPS), writes=[c.R_const])
        c.n_groups = _NC_CACHE.get("n_groups", 16)
        c.n_blocks = _NC_CACHE.get("n_blocks", NT)
        c.R32 = ar.al([128, 8, NTOK], BF16)
        c.R32f = c.R32.rearrange("p a b -> p (a b)")
        c.QT = c.R32f[:, 0:8192].rearrange("p (i a t) -> p i a t", i=NT, a=4)
        c.oT_nsa = c.R32[:, 4:8, :]
        c.oT_hg = ar.al([128, 4, NTOK], BF16)
        c.gsig = ar.al([128, NT, 24], F32)
        c.R_QT, c.R_oTnsa, c.R_oThg, c.R_gsig = k.res("QT"), k.res("oTnsa"), k.res("oThg"), k.res("gsig")
        hfT = c.R32
        R_hfT = [k.res("hfT_%d" % t) for t in range(NT)]
        R_X1 = [k.res("x1_%d" % t) for t in range(NT)]
        if stage != "moe_only":
            c.I4 = ar.al([128, 512], BF16)
            c.tri = ar.al([128, 128], BF16)
            c.onehot = ar.al([128, 4], F32)
            k.dma("pool", c.I4, Dr["I4"], writes=[c.R_const])
            k.dma("pool", c.tri, Dr["tri"], writes=[c.R_const])
            k.dma("sp", c.onehot, Dr["onehot"], writes=[c.R_const])
            setup_lb(k, c, ar)
            M1 = ar.mark()
            phase_attn(k, c, ar, stage)
            for r in dumps:
                k.rec["sp"].append(("w", r.dsem, r.dcnt))
            k.flush()
            ar.release(M1)
        X1 = ar.al([128, NT, D], F32)
        if stage == "moe_only":
            for t in range(NT):
                k.dma("sp", X1[:, t, :], Dr["xo"][t * 128:(t + 1) * 128, :], writes=[R_X1[t]])
        else:
            phase_p2c(k, c, ar, X1, R_X1)
            c.dump("X1", X1, R_X1)
            for r in dumps:
                if r.name == "dbg_X1":
                    k.rec["sp"].append(("w", r.dsem, r.dcnt))
        k.flush()
        if n_exp >= 0:
            phase_moe(k, c, ar, X1, R_X1, hfT, R_hfT)
            k.flush()
        phase_final(k, c, ar, X1, R_X1)
        k.flush()
    return nc


def input_specs(ne=NE, stage="full"):
    return [
        ("xb", (SEQ, D), F32), ("xo", (NTOK, D), F32),
        ("g_ffn", (D,), F32), ("g_fin", (D,), F32),
        ("w_r", (D, 36), F32), ("b_r", (36,), F32),
        ("w_gate", (ne, D, DFF), F32), ("w_up", (ne, D, DFF), F32), ("w_down", (ne, DFF, D), F32),
        ("identf", (128, 128), F32), ("identb", (128, 128), BF16),
    ] + (attn_input_specs() if stage != "moe_only" else [])


_NC_CACHE = {}


def host_inputs(inp, core, ne=NE):
    b, cp = core // 4, core % 4
    x = np.asarray(inp["x"], dtype=np.float32)
    m = {}
    m["xb"] = np.ascontiguousarray(x[b])
    m["xo"] = np.ascontiguousarray(x[b].reshape(NT, 4, 128, D)[:, cp].reshape(NTOK, D))
    m["g_ffn"] = np.ascontiguousarray(inp["ffn_norm"][0])
    m["g_fin"] = np.ascontiguousarray(inp["final_norm"])
    m["w_r"] = np.ascontiguousarray(np.concatenate([inp["w_grp"][0], inp["w_rtr"][0]], axis=1))
    m["b_r"] = np.ascontiguousarray(np.concatenate([inp["b_grp"][0], inp["b_rtr"][0]], axis=0))
    m["w_gate"] = np.ascontiguousarray(inp["w_gate"][0, :ne])
    m["w_up"] = np.ascontiguousarray(inp["w_up"][0, :ne])
    m["w_down"] = np.ascontiguousarray(inp["w_down"][0, :ne])
    m["identf"] = np.eye(128, dtype=np.float32)
    m["identb"] = np.eye(128, dtype=np.float32).astype(ml_dtypes.bfloat16)
    if _NC_CACHE.get("stage", "full") != "moe_only":
        m.update(attn_host_inputs(inp, b, cp))
    return m


def kernel(**inp):
    inp = {k_: np.asarray(v) for k_, v in inp.items()}
    stage = _NC_CACHE.get("stage", "full")
    key = ("nc", stage)
    if key not in _NC_CACHE:
        _NC_CACHE[key] = build(stage, _NC_CACHE.get("n_exp", NE))
    nc = _NC_CACHE[key]
    shared = None
    in_maps = []
    for core in range(8):
        m = host_inputs(inp, core, max(_NC_CACHE.get("n_exp", NE), 1))
        if shared is None:
            shared = m
        else:
            for kk in ("w_gate", "w_up", "w_down"):
                m[kk] = shared[kk]
        in_maps.append(m)
    res = run_bass_kernel_spmd(nc, in_maps, core_ids=list(range(8)))
    _NC_CACHE["last_results"] = res.results
    out = np.zeros((2, SEQ // 128, 128, D), dtype=np.float32)
    for core in range(8):
        b, cp = core // 4, core % 4
        y = np.asarray(res.results[core]["y"]).reshape(NT, 128, D)
        out[b, cp::4] = y
    return out.reshape(2, SEQ, D)
```

```python
import numpy as np
import ml_dtypes
import concourse.bass as bass
import concourse.mybir as mybir
from concourse.bass_utils import run_bass_kernel_spmd
from contextlib import ExitStack

F32 = mybir.dt.float32
BF16 = mybir.dt.bfloat16
AF = mybir.ActivationFunctionType
ALU = mybir.AluOpType
AX = mybir.AxisListType


class Res:
    __slots__ = ("name", "w", "rs", "dsem", "dcnt", "excl")

    def __init__(self, name, excl=False):
        self.name = name
        self.excl = excl
        self.w = None
        self.rs = {}
        self.dsem = None
        self.dcnt = 0


class _Proxy:
    def __init__(self):
        self.calls = []

    def __getattr__(self, name):
        def rec(*a, **kw):
            self.calls.append((name, a, kw))
        return rec


def _bind(f):
    p = _Proxy()
    f(p)
    assert len(p.calls) == 1, "one engine instruction per callable"
    name, a, kw = p.calls[0]
    return lambda eng: getattr(eng, name)(*a, **kw)


class KH:
    ENG = ("pe", "dve", "act", "pool", "sp")

    def __init__(self, nc, es):
        self.nc = nc
        self.es = es
        self.sem = {}
        self.cnt = {}
        for e in self.ENG:
            self.sem[e] = es.enter_context(nc.semaphore("s_" + e))
            self.cnt[e] = 0
        self.rec = {e: [] for e in self.ENG}
        self.seen = {e: {} for e in self.ENG}
        self.nsem = len(self.ENG)
        self.semobj = dict(self.sem)

    def res(self, name, excl=False):
        return Res(name, excl)

    def _dma_sem(self, r):
        if r.dsem is None:
            r.dsem = "d_" + r.name + "_%d" % self.nsem
            self.semobj[r.dsem] = self.es.enter_context(self.nc.semaphore(r.dsem))
            self.nsem += 1
        return r.dsem

    def _deps(self, e, reads, writes):
        deps = {}
        for r in reads:
            if r.w is not None:
                k, v = r.w
                deps[k] = max(deps.get(k, 0), v)
        for w in writes:
            if w.w is not None:
                k, v = w.w
                deps[k] = max(deps.get(k, 0), v)
            for k, v in w.rs.items():
                deps[k] = max(deps.get(k, 0), v)
        seen = self.seen[e]
        for k, v in deps.items():
            if seen.get(k, 0) >= v:
                continue
            seen[k] = v
            self.rec[e].append(("w", k, v))

    def op(self, e, fns, reads=(), writes=()):
        if callable(fns):
            fns = [fns]
        self.opn = getattr(self, "opn", 0) + 1
        if self.opn > getattr(self, "oplim", 10 ** 9):
            return
        ex = [r for r in reads if r.excl]
        if ex:
            reads = [r for r in reads if not r.excl]
            writes = list(writes) + [r for r in ex if r not in writes]
        self._deps(e, reads, writes)
        self.cnt[e] += 1
        v = self.cnt[e]
        fns = [_bind(f) for f in fns]
        for f in fns[:-1]:
            self.rec[e].append(("i", f, None, 0))
        self.rec[e].append(("i", fns[-1], e, 1))
        self.seen[e][e] = max(self.seen[e].get(e, 0), 0)
        for r in reads:
            r.rs[e] = v
        for w in writes:
            w.w = (e, v)
            w.rs = {}

    def dma(self, q, out, in_, reads=(), writes=(), key=None, **kw):
        self._deps(q, reads, writes)
        kr = key or (writes[0] if writes else reads[0])
        sk = self._dma_sem(kr)
        kr.dcnt += 16
        v = kr.dcnt
        self.rec[q].append(("i", lambda eng: eng.dma_start(out=out, in_=in_, **kw), sk, 16))
        for r in reads:
            r.rs[sk] = v
        for w in writes:
            w.w = (sk, v)
            w.rs = {}

    def wait_res(self, e, rs):
        self._deps(e, rs, ())

    def simulate(self):
        if not hasattr(self, "simval"):
            self.simval = {}
        val = self.simval
        ptr = {e: 0 for e in self.ENG}
        prog = True
        while prog:
            prog = False
            for e in self.ENG:
                items = self.rec[e]
                while ptr[e] < len(items):
                    it = items[ptr[e]]
                    if it[0] == "w":
                        if val.get(it[1], 0) >= it[2]:
                            ptr[e] += 1
                            prog = True
                        else:
                            break
                    else:
                        if it[2] is not None:
                            val[it[2]] = val.get(it[2], 0) + it[3]
                        ptr[e] += 1
                        prog = True
        for e in self.ENG:
            if ptr[e] < len(self.rec[e]):
                it = self.rec[e][ptr[e]]
                raise RuntimeError("DEADLOCK: engine %s stuck at item %d/%d waiting %s >= %s (have %s)" % (
                    e, ptr[e], len(self.rec[e]), it[1], it[2], val.get(it[1], 0)))

    def flush(self, name=None):
        nc = self.nc
        rec = self.rec
        semobj = self.semobj
        self.simulate()
        import os
        if os.environ.get("KH_DEBUG"):
            print("KH flush: ops so far", getattr(self, "opn", 0), {e: len(v) for e, v in self.rec.items()}, "nsem", self.nsem, flush=True)

        def play(eng, items):
            for it in items:
                if it[0] == "w":
                    eng.wait_ge(semobj[it[1]], it[2])
                else:
                    ins = it[1](eng)
                    if it[2] is not None:
                        ins.then_inc(semobj[it[2]], it[3])

        with nc.Block() as block:
            if rec["sp"]:
                @block.sync
                def _(eng):
                    play(eng, rec["sp"])
            if rec["pe"]:
                @block.tensor
                def _(eng):
                    play(eng, rec["pe"])
            if rec["dve"]:
                @block.vector
                def _(eng):
                    play(eng, rec["dve"])
            if rec["act"]:
                @block.scalar
                def _(eng):
                    play(eng, rec["act"])
            if rec["pool"]:
                @block.gpsimd
                def _(eng):
                    play(eng, rec["pool"])
        self.rec = {e: [] for e in self.ENG}

NEG = -30000.0
NT = 16
NTOK = NT * 128
SEQ = 8192
D = 1024
NE = 32
DFF = 512
EPS = 1e-6


class Arena:
    def __init__(self, big, nbytes):
        self.big = big
        self.n = nbytes
        self.off = 0

    def mark(self):
        return self.off

    def release(self, m):
        import os
        if os.environ.get("KH_DEBUG"):
            print("arena release: peak", getattr(self, "peak", 0), "->", m, "of", self.n, flush=True)
        self.peak = m
        self.off = m

    def al(self, shape, dt):
        esz = 4 if dt == F32 else 2
        per = int(np.prod(shape[1:])) * esz
        self.off = (self.off + 63) // 64 * 64
        o = self.off
        assert o + per <= self.n, ("arena overflow", o, per, self.n)
        self.off = o + per
        self.peak = max(getattr(self, "peak", 0), self.off)
        v = self.big[0:shape[0], o // 2:(o + per) // 2]
        if dt == F32:
            v = v.bitcast(F32)
        if len(shape) == 3:
            v = v.rearrange("p (a b) -> p a b", a=shape[1])
        elif len(shape) == 4:
            v = v.rearrange("p (a b c) -> p a b c", a=shape[1], b=shape[2])
        return v


class Ctx:
    pass


def rms_rstd(k, c, src, src_res, scr, scr_res, n_feat, tag):
    i = c.rs_i % 8
    c.rs_i += 1
    ssq, std, rstd = c.ssq[:, i:i + 1], c.std[:, i:i + 1], c.rstd[:, i:i + 1]
    R = c.rs_res[i]
    k.op("act", lambda e: e.activation(out=scr, in_=src, func=AF.Square, accum_out=ssq),
         reads=[src_res], writes=[scr_res, R])
    k.op("act", lambda e: e.activation(out=std, in_=ssq, func=AF.Sqrt, scale=1.0 / n_feat, bias=c.epsb[:, 0:1]),
         reads=[R, c.R_const], writes=[R])
    k.op("dve", lambda e: e.reciprocal(out=rstd, in_=std), reads=[R], writes=[R])
    return rstd, R


def phase_moe(k, c, ar, X1, R_X1, hfT, R_hfT):
    nc = c.nc
    Dr = c.D
    m0 = ar.mark()
    hf32 = [ar.al([128, D], F32) for _ in range(2)]
    R_hf32 = [k.res("hf32_%d" % i) for i in range(2)]
    scr = ar.al([128, D], F32)
    R_scr = k.res("moe_scr")
    hT32 = [ar.al([128, 8, 128], F32) for _ in range(2)]
    R_hT32 = [k.res("hT32_%d" % i) for i in range(2)]
    wr32 = ar.al([128, 8, 36], F32)
    R_wr = k.res("wr32")
    brt = ar.al([128, 36], F32)
    gft = ar.al([128, D], F32)
    R_gft = k.res("gft")
    comb = ar.al([128, NT, NE], F32)
    R_comb = k.res("comb")
    sm = ar.al([128, 128], F32)
    R_sm = k.res("moe_sm")
    k.dma("sp", wr32, Dr["w_r"].rearrange("(kt p) n -> p kt n", p=128), writes=[R_wr])
    k.dma("sp", brt, Dr["b_r"].partition_broadcast(128), writes=[R_wr])
    k.dma("sp", gft, Dr["g_ffn"].partition_broadcast(128), writes=[R_gft])
    pT = [c.pwide(0), c.pwide(2)]
    R_pT = [[c.R_ps[0], c.R_ps[1]], [c.R_ps[2], c.R_ps[3]]]
    pL = c.psum[4]
    R_pL = c.R_ps[4]
    for t in range(NT):
        b = t % 2
        xs = X1[:, t, :]
        rstd, R_r = rms_rstd(k, c, xs, R_X1[t], scr, R_scr, D, "moe")
        k.op("dve", lambda e, b=b, xs=xs, rstd=rstd: e.scalar_tensor_tensor(
            out=hf32[b], in0=xs, scalar=rstd, in1=gft, op0=ALU.mult, op1=ALU.mult),
            reads=[R_X1[t], R_r, R_gft], writes=[R_hf32[b]])
        p2 = pT[b]
        k.op("pe", [(lambda e, i=i, b=b, p2=p2: e.transpose(out=p2[:, i * 128:(i + 1) * 128],
                                                            in_=hf32[b][:, i * 128:(i + 1) * 128], identity=c.identf))
                    for i in range(8)], reads=[R_hf32[b], c.R_const], writes=R_pT[b])
        k.op("act", lambda e, b=b, p2=p2: e.activation(out=hT32[b].rearrange("p a b -> p (a b)"), in_=p2, func=AF.Copy),
             reads=R_pT[b], writes=[R_hT32[b]])
        k.op("dve", lambda e, b=b, p2=p2, t=t: e.tensor_copy(
            out=hfT[:, :, t * 128:(t + 1) * 128], in_=p2.rearrange("p (a b) -> p a b", a=8)),
            reads=R_pT[b], writes=[R_hfT[t]])
        lg = pL[:, 0:36]
        k.op("pe", [(lambda e, i=i, b=b: e.matmul(lg, lhsT=hT32[b][:, i, :], rhs=wr32[:, i, :], start=(i == 0), stop=(i == 7)))
                    for i in range(8)], reads=[R_hT32[b], R_wr], writes=[R_pL])
        lgs = sm[:, 0:36]
        gmax, gsum, gp, pen = sm[:, 36:37], sm[:, 37:38], sm[:, 38:39], sm[:, 40:44]
        gex, goh = sm[:, 44:48], sm[:, 48:52]
        elm = sm[:, 52:84]
        m8 = sm[:, 84:92]
        dd, ee, w1, w2 = sm[:, 92:93], sm[:, 93:94], sm[:, 94:95], sm[:, 95:96]
        oh = sm[:, 96:128]
        ct = comb[:, t, :]
        RW = dict(reads=[R_sm], writes=[R_sm])
        k.op("dve", lambda e: e.tensor_tensor(out=lgs, in0=lg, in1=brt, op=ALU.add), reads=[R_pL, R_wr, R_sm], writes=[R_sm])
        k.op("dve", lambda e: e.reduce_max(out=gmax, in_=lgs[:, 0:4], axis=AX.X), **RW)
        k.op("dve", lambda e: e.tensor_scalar(out=gex, in0=lgs[:, 0:4], scalar1=gmax, scalar2=None, op0=ALU.subtract), **RW)
        k.op("act", lambda e: e.activation(out=gex, in_=gex, func=AF.Exp, accum_out=gsum), **RW)
        k.op("dve", lambda e: e.reciprocal(out=gp, in_=gsum), **RW)
        k.op("dve", lambda e: e.tensor_scalar(out=pen, in0=lgs[:, 0:4], scalar1=gmax, scalar2=-1e30, op0=ALU.is_lt, op1=ALU.mult), **RW)
        k.op("dve", lambda e: e.tensor_tensor(out=elm.rearrange("p (g x) -> p g x", g=4),
                                              in0=lgs[:, 4:36].rearrange("p (g x) -> p g x", g=4),
                                              in1=pen.unsqueeze(2).broadcast_to([128, 4, 8]), op=ALU.add), **RW)
        k.op("dve", lambda e: e.max(out=m8, in_=elm), **RW)
        k.op("dve", lambda e: e.tensor_tensor(out=dd, in0=m8[:, 1:2], in1=m8[:, 0:1], op=ALU.subtract), **RW)
        k.op("act", lambda e: e.activation(out=ee, in_=dd, func=AF.Exp), **RW)
        k.op("dve", lambda e: e.tensor_scalar(out=ee, in0=ee, scalar1=1.0, scalar2=None, op0=ALU.add), **RW)
        k.op("dve", lambda e: e.reciprocal(out=w1, in_=ee), **RW)
        k.op("dve", lambda e: e.tensor_scalar(out=w2, in0=w1, scalar1=-1.0, scalar2=1.0, op0=ALU.mult, op1=ALU.add), **RW)
        k.op("dve", lambda e: e.tensor_tensor(out=w1, in0=w1, in1=gp, op=ALU.mult), **RW)
        k.op("dve", lambda e: e.tensor_tensor(out=w2, in0=w2, in1=gp, op=ALU.mult), **RW)
        k.op("dve", lambda e: e.tensor_scalar(out=oh, in0=elm, scalar1=m8[:, 0:1], scalar2=w1, op0=ALU.is_equal, op1=ALU.mult), **RW)
        k.op("dve", lambda e, ct=ct: e.tensor_scalar(out=ct, in0=elm, scalar1=m8[:, 1:2], scalar2=w2, op0=ALU.is_equal, op1=ALU.mult),
             reads=[R_sm], writes=[R_comb])
        k.op("dve", lambda e, ct=ct: e.tensor_tensor(out=ct, in0=ct, in1=oh, op=ALU.add), reads=[R_sm, R_comb], writes=[R_comb])

    if c.n_exp == 0:
        ar.release(m0)
        return
    NWB = 2
    wg = [ar.al([128, 8, DFF], BF16) for _ in range(NWB)]
    wu = [ar.al([128, 8, DFF], BF16) for _ in range(NWB)]
    wd = [ar.al([128, 4, D], BF16) for _ in range(NWB)]
    R_wg = [k.res("wg%d" % i) for i in range(NWB)]
    R_wu = [k.res("wu%d" % i) for i in range(NWB)]
    R_wd = [k.res("wd%d" % i) for i in range(NWB)]
    hid = [ar.al([128, 4, 512], BF16) for _ in range(2)]
    R_hid = [k.res("hid%d" % i) for i in range(2)]
    sg = [ar.al([128, 512], F32) for _ in range(2)]
    R_sg = [k.res("sg%d" % i) for i in range(2)]
    n_exp = c.n_exp

    def load_w(e):
        b = e % NWB
        k.dma("pool", wg[b], Dr["w_gate"][e].rearrange("(kt p) n -> p kt n", p=128), writes=[R_wg[b]])
        k.dma("pool", wu[b], Dr["w_up"][e].rearrange("(kt p) n -> p kt n", p=128), writes=[R_wu[b]])
        k.dma("pool", wd[b], Dr["w_down"][e].rearrange("(kt p) n -> p kt n", p=128), writes=[R_wd[b]])

    units = [(e, g) for e in range(n_exp) for g in range(NT // 4)]
    pgu = [(c.psum[0], c.psum[1]), (c.psum[2], c.psum[3])]
    R_pgu = [(c.R_ps[0], c.R_ps[1]), (c.R_ps[2], c.R_ps[3])]
    pdn = [c.psum[4], c.psum[5], c.psum[6], c.psum[7]]
    R_pdn = [c.R_ps[4], c.R_ps[5], c.R_ps[6], c.R_ps[7]]
    st = dict(gu=0, dn=0)

    def gate_up(u):
        e, g = units[u]
        b = e % NWB
        hb = u % 2
        tok = slice(g * 512, (g + 1) * 512)
        for ff in range(4):
            pb = st["gu"] % 2
            st["gu"] += 1
            pg, pu = pgu[pb]
            k.op("pe", [(lambda en, i=i, pg=pg, b=b, ff=ff: en.matmul(pg, lhsT=wg[b][:, i, ff * 128:(ff + 1) * 128], rhs=hfT[:, i, tok],
                                                                      start=(i == 0), stop=(i == 7))) for i in range(8)],
                 reads=[R_wg[b]] + R_hfT[4 * g:4 * g + 4], writes=[R_pgu[pb][0]])
            k.op("pe", [(lambda en, i=i, pu=pu, b=b, ff=ff: en.matmul(pu, lhsT=wu[b][:, i, ff * 128:(ff + 1) * 128], rhs=hfT[:, i, tok],
                                                                      start=(i == 0), stop=(i == 7))) for i in range(8)],
                 reads=[R_wu[b]] + R_hfT[4 * g:4 * g + 4], writes=[R_pgu[pb][1]])
            k.op("act", lambda en, pg=pg, pb=pb: en.activation(out=sg[pb], in_=pg, func=AF.Silu),
                 reads=[R_pgu[pb][0]], writes=[R_sg[pb]])
            k.op("dve", lambda en, pu=pu, pb=pb, hb=hb, ff=ff: en.tensor_tensor(out=hid[hb][:, ff, :], in0=pu, in1=sg[pb], op=ALU.mult),
                 reads=[R_pgu[pb][1], R_sg[pb]], writes=[R_hid[hb]])

    def down(u):
        e, g = units[u]
        b = e % NWB
        hb = u % 2
        for tt in range(4):
            t = 4 * g + tt
            for hf in range(2):
                pb = st["dn"] % 4
                st["dn"] += 1
                po = pdn[pb]
                k.op("pe", [(lambda en, i=i, po=po, b=b, hb=hb, tt=tt, hf=hf: en.matmul(
                    po, lhsT=hid[hb][:, i, tt * 128:(tt + 1) * 128], rhs=wd[b][:, i, hf * 512:(hf + 1) * 512],
                    start=(i == 0), stop=(i == 3))) for i in range(4)],
                    reads=[R_hid[hb], R_wd[b]], writes=[R_pdn[pb]])
                xs = X1[:, t, hf * 512:(hf + 1) * 512]
                k.op("dve", lambda en, po=po, xs=xs, t=t, e=e: en.scalar_tensor_tensor(
                    out=xs, in0=po, scalar=comb[:, t, e:e + 1], in1=xs, op0=ALU.mult, op1=ALU.add),
                    reads=[R_pdn[pb], R_comb, R_X1[t]], writes=[R_X1[t]])

    load_w(0)
    for u in range(len(units)):
        e, g = units[u]
        gate_up(u)
        if u >= 1:
            down(u - 1)
        if g == 0 and e + 1 < n_exp:
            load_w(e + 1)
    down(len(units) - 1)
    ar.release(m0)


def phase_final(k, c, ar, X1, R_X1):
    Dr = c.D
    m0 = ar.mark()
    gft = ar.al([128, D], F32)
    R_g = k.res("gfin")
    scr = ar.al([128, D], F32)
    R_scr = k.res("fin_scr")
    ob = [ar.al([128, D], F32) for _ in range(2)]
    R_ob = [k.res("ob%d" % i) for i in range(2)]
    k.dma("sp", gft, Dr["g_fin"].partition_broadcast(128), writes=[R_g])
    for t in range(NT):
        b = t % 2
        xs = X1[:, t, :]
        rstd, R_r = rms_rstd(k, c, xs, R_X1[t], scr, R_scr, D, "fin")
        k.op("dve", lambda e, b=b, xs=xs, rstd=rstd: e.scalar_tensor_tensor(
            out=ob[b], in0=xs, scalar=rstd, in1=gft, op0=ALU.mult, op1=ALU.mult),
            reads=[R_X1[t], R_r, R_g], writes=[R_ob[b]])
        k.dma("sp", Dr["y"][t * 128:(t + 1) * 128, :], ob[b], reads=[R_ob[b]])
    for b in range(2):
        for sk, v in list(R_ob[b].rs.items()):
            if sk.startswith("d_"):
                k.rec["sp"].append(("w", sk, v))
    ar.release(m0)


Q0, KV0, GT0, HQ0, HF0, HI0, HG0, MG0 = 0, 512, 1280, 1304, 1816, 2328, 2840, 3352


def _partner(d):
    return d + 8 if d < 8 else (d - 8 if d < 16 else d)


def _rope_tables(pos):
    pos = np.asarray(pos, dtype=np.float32)
    inv = (np.float32(500000.0) ** (-np.arange(8, dtype=np.float32) / np.float32(8))).astype(np.float32)
    ang = (pos[None, :] * inv[:, None]).astype(np.float32)
    cs, sn = np.cos(ang).astype(np.float32), np.sin(ang).astype(np.float32)
    C = np.ones((64, len(pos)), np.float32)
    S = np.zeros((64, len(pos)), np.float32)
    C[0:8], C[8:16] = cs, cs
    S[0:8], S[8:16] = -sn, sn
    return C, S


def attn_input_specs():
    return [
        ("g_attn", (D,), F32), ("g_hg4", (512,), F32),
        ("w1f", (D, 1280), F32), ("w1t", (D, 768), F32),
        ("w2f", (D, 2048), F32), ("w2t", (D, 1536), F32),
        ("wck", (64, 2048), F32), ("wckp", (64, 2048), F32), ("wcv", (64, 2048), F32),
        ("posT", (128, 32), F32), ("lbl", (2, 512), F32),
        ("w_mg", (D, 2048), F32), ("w_brn", (512, D), F32), ("w_brh", (512, D), F32), ("w_out", (D, D), F32),
        ("CK", (128, SEQ), F32), ("SK", (128, SEQ), F32), ("CKc", (128, 512), F32), ("SKc", (128, 512), F32),
        ("CQ", (128, NTOK), F32), ("SQ", (128, NTOK), F32),
        ("ovl", (128, 4, 128), F32),
        ("CB", (128, NT, 128), F32), ("CM", (128, 4, 128), F32), ("WMT", (128, 8, 128), F32),
        ("VAL", (128, NT, 128), F32), ("ADDC", (128, NT, 128), F32),
        ("tri", (128, 128), F32), ("I4", (128, 512), F32), ("onehot", (128, 4), F32),
    ]


_TAB_CACHE = {}


def _const_tables(cp):
    if cp in _TAB_CACHE:
        return _TAB_CACHE[cp]
    m = {}
    C, S = _rope_tables(np.arange(SEQ))
    m["CK"], m["SK"] = np.concatenate([C, C], 0), np.concatenate([S, S], 0)
    C, S = _rope_tables(np.maximum(16 * (np.arange(512) - 1), 0))
    m["CKc"], m["SKc"] = np.concatenate([C, C], 0), np.concatenate([S, S], 0)
    tpos = (128 * (4 * np.arange(NT)[:, None] + cp) + np.arange(128)[None, :])
    C, S = _rope_tables(tpos.reshape(-1))
    m["CQ"] = np.concatenate([C, C], 0) * np.float32(0.125)
    m["SQ"] = np.concatenate([S, S], 0) * np.float32(0.125)
    n = np.arange(512) - 1
    cs, ce = 16 * n, 16 * n + 31
    ss = 64 * np.arange(128)
    ov = ((cs[:, None] < ss[None, :] + 64) & (ce[:, None] >= ss[None, :]) & (n[:, None] >= 0)).astype(np.float32)
    m["ovl"] = np.ascontiguousarray(ov.reshape(4, 128, 128).transpose(1, 0, 2))
    mt = (np.arange(NT) // 4)
    mm = mt[:, None] * 128 + np.arange(128)[None, :]
    nn = mm - 1
    okc = (nn[:, None, :] >= 0) & (16 * nn[:, None, :] + 31 <= tpos[:, :, None])
    m["CB"] = np.ascontiguousarray(np.where(okc, 0.0, NEG).astype(np.float32).transpose(1, 0, 2))
    blk = np.arange(128)
    jq = tpos // 64
    force = (blk[None, None, :] == jq[:, :, None]) | (blk[None, None, :] == 0)
    valid = (64 * blk[None, None, :] <= tpos[:, :, None])
    m["VAL"] = np.ascontiguousarray((valid & ~force).astype(np.float32).transpose(1, 0, 2))
    m["ADDC"] = np.ascontiguousarray(np.where(force, 1e4, np.where(valid, 0.0, -1.0)).astype(np.float32).transpose(1, 0, 2))
    t = np.arange(128)[:, None]
    p = np.arange(128)[None, :]
    caus = np.where(p <= t, 0.0, NEG).astype(np.float32)
    anti = np.where(p > t, 0.0, NEG).astype(np.float32)
    cm = np.zeros((128, 4, 128), np.float32)
    for r in range(4):
        cm[:, r, :] = 0.0 if r < cp else (caus if r == cp else NEG)
    m["CM"] = cm
    wm = np.zeros((128, 8, 128), np.float32)
    for r in range(8):
        dk = cp + 4 - r
        wm[:, r, :] = NEG if (dk < 0 or dk > 4) else (caus if dk == 0 else (anti if dk == 4 else 0.0))
    m["WMT"] = wm
    m["tri"] = (np.arange(128)[:, None] <= np.arange(128)[None, :]).astype(np.float32)
    m["I4"] = np.tile(np.eye(128, dtype=np.float32), (1, 4))
    oh = np.zeros((128, 4), np.float32)
    oh[:, cp] = 1.0
    m["onehot"] = oh
    _TAB_CACHE[cp] = m
    return m


def attn_host_inputs(inp, b, cp):
    m = dict(_const_tables(cp))
    w = inp["w_in"][0]
    pp = np.array([g * 64 + _partner(d) for g in range(2) for d in range(64)])
    kv = lambda s: KV0 + s * 128 + np.arange(128)
    hfc = HF0 + np.arange(512)
    m["w1f"] = np.ascontiguousarray(np.concatenate(
        [w[:, kv(0)], w[:, kv(1)], w[:, kv(2)], w[:, kv(2)[pp]], w[:, kv(4)], w[:, kv(4)[pp]], w[:, hfc]], axis=1))
    m["w1t"] = np.ascontiguousarray(np.concatenate([w[:, kv(3)], w[:, kv(5)], w[:, HI0:HI0 + 512]], axis=1))
    qcols, qpcols = [], []
    for a in range(4):
        for h in (a, 4 + a):
            qcols += [Q0 + h * 64 + d for d in range(64)]
            qpcols += [Q0 + h * 64 + _partner(d) for d in range(64)]
    m["w2f"] = np.ascontiguousarray(np.concatenate(
        [w[:, qcols], w[:, qpcols], w[:, HQ0:HQ0 + 512], w[:, hfc]], axis=1))
    gpad = np.concatenate([w[:, GT0:GT0 + 24], w[:, GT0:GT0 + 24][:, :0].repeat(1, 1)], axis=1)
    w2t = np.zeros((D, 1536), np.float32)
    w2t[:, 0:512] = w[:, HI0:HI0 + 512]
    w2t[:, 512:1024] = w[:, HG0:HG0 + 512]
    w2t[:, 1024:1048] = w[:, GT0:GT0 + 24]
    m["w2t"] = w2t
    pc = np.array([_partner(d) for d in range(64)])
    dle = lambda w_: np.ascontiguousarray(w_.reshape(32, 64, 64).transpose(1, 0, 2).reshape(64, 2048))
    m["wck"] = dle(inp["w_cmp_k"][0])
    m["wckp"] = dle(inp["w_cmp_k"][0][:, pc])
    m["wcv"] = dle(inp["w_cmp_v"][0])
    pT = np.ascontiguousarray(inp["cmp_pos"][0].T)
    m["posT"] = np.concatenate([pT, pT], 0)
    m["lbl"] = np.ascontiguousarray(inp["hg_lb_logits"])
    m["g_attn"] = np.ascontiguousarray(inp["attn_norm"][0])
    m["g_hg4"] = np.ascontiguousarray(np.tile(inp["hg_norm"][0], 4))
    m["w_mg"] = np.ascontiguousarray(w[:, MG0:MG0 + 2048])
    m["w_brn"] = np.ascontiguousarray(inp["w_br_nsa"][0])
    m["w_brh"] = np.ascontiguousarray(inp["w_br_hg"][0])
    m["w_out"] = np.ascontiguousarray(inp["w_out"][0])
    return m


def norm_transpose_group(k, c, W, src_dram, row0, hT, R_hT):
    def s1(tt):
        b = tt % 2
        k.dma("sp", W.xt[b], src_dram[row0 + tt * 128: row0 + (tt + 1) * 128, :], writes=[W.R_xt[b]])
        rstd, R_r = rms_rstd(k, c, W.xt[b], W.R_xt[b], W.scr, W.R_scr, D, "an")
        k.op("dve", lambda e: e.scalar_tensor_tensor(
            out=W.hb[b], in0=W.xt[b], scalar=rstd, in1=W.gA, op0=ALU.mult, op1=ALU.mult),
            reads=[W.R_xt[b], R_r, W.R_gA], writes=[W.R_hb[b]])
        pb = c.psum[b].bitcast(BF16)
        k.op("pe", [(lambda e, i=i: e.transpose(out=pb[:, i * 128:(i + 1) * 128],
                                                in_=W.hb[b][:, i * 128:(i + 1) * 128], identity=c.identb))
                    for i in range(8)], reads=[W.R_hb[b], c.R_const], writes=[c.R_ps[b]])

    def s2(tt):
        b = tt % 2
        pb = c.psum[b].bitcast(BF16)
        k.op("act", lambda e: e.activation(out=hT[:, :, tt * 128:(tt + 1) * 128],
                                           in_=pb.rearrange("p (a b) -> p a b", a=8), func=AF.Copy),
             reads=[c.R_ps[b]], writes=[R_hT])
    s1(0)
    s1(1)
    s2(0)
    s1(2)
    s2(1)
    s1(3)
    s2(2)
    s2(3)


def f_front(k, c, W, fl_ps, R_fl, hd):
    u, a, bq, lk, L, RF = W.sets[hd % 2]
    k.op("act", lambda e: e.activation(out=u, in_=fl_ps, func=AF.Exp, scale=-1.0), reads=[R_fl], writes=[RF])
    k.op("act", lambda e: e.activation(out=a, in_=u, func=AF.Ln, scale=c.lbv[:, hd:hd + 1], bias=c.one_col[:, 0:1]),
         reads=[RF, c.R_const], writes=[RF])
    k.op("act", lambda e: e.activation(out=bq, in_=u, func=AF.Ln, bias=c.one_col[:, 0:1]), reads=[RF, c.R_const], writes=[RF])
    k.op("dve", lambda e: e.scalar_tensor_tensor(out=lk, in0=fl_ps, scalar=-1.0, in1=bq, op0=ALU.mult, op1=ALU.subtract),
         reads=[R_fl, RF], writes=[RF])
    for tt in range(4):
        sl = slice(tt * 128, (tt + 1) * 128)
        k.op("dve", lambda e, sl=sl: e.tensor_tensor_scan(out=L[:, sl], data0=a[:, sl], data1=bq[:, sl], initial=0.0,
                                                          op0=ALU.add, op1=ALU.subtract), reads=[RF], writes=[RF])
    k.op("pool", lambda e: e.tensor_tensor(out=lk, in0=lk, in1=L, op=ALU.subtract), reads=[RF], writes=[RF])


def f_back(k, c, W, hd, H=None):
    u, a, bq, lk, L, RF = W.sets[hd % 2]
    W_, W = W, (H if H is not None else W)
    Lr = L.rearrange("p (t x) -> p t x", t=4)
    rcol, ecol = Lr[:, :, 63], Lr[:, :, 127]
    k.op("dve", lambda e: e.tensor_scalar(out=W.rb[:, hd, :], in0=rcol, scalar1=c.l1mlb[:, hd:hd + 1], scalar2=None, op0=ALU.add),
         reads=[RF, c.R_const], writes=[W.R_cols])
    k.op("dve", lambda e: e.tensor_scalar(out=W.negr[:, hd, :], in0=rcol, scalar1=-1.0, scalar2=None, op0=ALU.mult),
         reads=[RF], writes=[W.R_cols])
    k.op("dve", lambda e: e.tensor_tensor(out=W.dl[:, hd, :], in0=ecol, in1=rcol, op=ALU.subtract), reads=[RF], writes=[W.R_cols])
    k.op("act", lambda e: e.activation(out=W.c1[:, hd, :], in_=ecol, func=AF.Exp), reads=[RF], writes=[W.R_cols])
    k.op("act", lambda e: e.activation(out=W.c2[:, hd, :], in_=W.dl[:, hd, :], func=AF.Exp), reads=[W.R_cols], writes=[W.R_cols])
    k.op("act", lambda e: e.activation(out=W.er[:, hd, :], in_=rcol, func=AF.Exp), reads=[RF], writes=[W.R_cols])
    for tt in range(4):
        sl = slice(tt * 128, (tt + 1) * 128)
        k.op("act", lambda e, sl=sl, tt=tt: e.activation(out=W.kT[:, hd, sl], in_=lk[:, sl], func=AF.Exp, bias=W.rb[:, hd, tt:tt + 1]),
             reads=[RF, W.R_cols], writes=[W.R_kT])


def setup_lb(k, c, ar):
    Dr = c.D
    c.lbv = ar.al([128, 4], F32)
    c.l1mlb = ar.al([128, 4], F32)
    c.one_col = ar.al([128, 1], F32)
    c.ones128 = ar.al([128, 128], F32)
    l0 = ar.al([128, 4], F32)
    l1 = ar.al([128, 4], F32)
    R = c.R_const
    k.dma("sp", l0, Dr["lbl"][0].rearrange("(h p) -> p h", p=128), writes=[R], allow_slow_non_contiguous=True)
    k.dma("sp", l1, Dr["lbl"][1].rearrange("(h p) -> p h", p=128), writes=[R], allow_slow_non_contiguous=True)
    k.op("dve", lambda e: e.memset(c.one_col, 1.0), writes=[R])
    k.op("dve", lambda e: e.memset(c.ones128, 1.0), writes=[R])
    k.op("dve", lambda e: e.tensor_tensor(out=l1, in0=l1, in1=l0, op=ALU.subtract), reads=[R], writes=[R])
    k.op("act", lambda e: e.activation(out=l0, in_=l1, func=AF.Exp), reads=[R], writes=[R])
    k.op("dve", lambda e: e.tensor_scalar(out=l0, in0=l0, scalar1=1.0, scalar2=None, op0=ALU.add), reads=[R], writes=[R])
    k.op("dve", lambda e: e.reciprocal(out=c.lbv, in_=l0), reads=[R], writes=[R])
    k.op("act", lambda e: e.activation(out=l0, in_=l0, func=AF.Ln), reads=[R], writes=[R])
    k.op("dve", lambda e: e.tensor_tensor(out=c.l1mlb, in0=l1, in1=l0, op=ALU.subtract), reads=[R], writes=[R])


class WS:
    pass


def alloc_hg_ws(k, ar, W, nsets=1):
    W.sets = []
    for si in range(nsets):
        blk = ar.al([128, 5, 512], F32)
        W.sets.append(tuple(blk[:, i, :] for i in range(5)) + (k.res("fchain%d" % si),))
        if si == 0:
            W.ab = blk[:, 1:3, :].rearrange("p a b -> p (a b)")
    if nsets == 1:
        W.sets.append(W.sets[0])
    W.u, W.a, W.bq, W.lk, W.L, W.R_f = W.sets[0]
    alloc_hslot(k, ar, W, "0")


def alloc_hslot(k, ar, H, tag):
    H.rb, H.negr, H.dl, H.c1, H.c2, H.er = [ar.al([128, 4, 4], F32) for _ in range(6)]
    H.R_cols = k.res("fcols" + tag)
    H.kT = ar.al([128, 4, 512], BF16)
    H.R_kT = k.res("kT" + tag)


def alloc_x_ws(k, c, ar, W, region, scr=None, R_scr=None):
    if region is not None:
        W.xt = [region[:, 0, :].bitcast(F32), region[:, 1, :].bitcast(F32)]
        W.hb = [region[:, 2, 0:1024], region[:, 2, 1024:2048]]
        W.scr = region[:, 3, :].bitcast(F32)
        W.R_scr = k.res("xscr")
    else:
        W.xt = [ar.al([128, D], F32) for _ in range(2)]
        W.hb = [ar.al([128, D], BF16) for _ in range(2)]
        W.scr, W.R_scr = scr, R_scr
    W.R_xt = [k.res("xt0"), k.res("xt1")]
    W.R_hb = [k.res("hb0"), k.res("hb1")]
    W.gA = ar.al([128, D], F32)
    W.R_gA = k.res("gA")
    k.dma("sp", W.gA, c.D["g_attn"].partition_broadcast(128), writes=[W.R_gA])


def phase_p1(k, c, ar, St):
    Dr = c.D
    m0 = ar.mark()
    W = WS()
    alloc_x_ws(k, c, ar, W, c.oT_hg)
    w1f, w1t = c.R32[:, :, 0:1280], c.R32[:, :, 1280:2048]
    R_w1 = k.res("w1")
    k.dma("pool", w1f, Dr["w1f"].rearrange("(kt p) n -> p kt n", p=128), writes=[R_w1])
    k.dma("pool", w1t, Dr["w1t"].rearrange("(kt p) n -> p kt n", p=128), writes=[R_w1])
    hT = ar.al([128, 8, 512], BF16)
    R_hT = k.res("hT")
    CKg, SKg = ar.al([128, 512], F32), ar.al([128, 512], F32)
    R_rt = k.res("ropetab")
    alloc_hg_ws(k, ar, W, nsets=2)
    t1, t2, R_t12 = W.u, W.a, W.R_f
    vtok = ar.al([128, 4, 512], BF16)
    R_vtok = k.res("vtok")
    ktok = ar.al([128, 4, 128], BF16)
    R_ktok = k.res("ktok")
    Sst = ar.al([128, 4, 128], F32)
    snapacc = ar.al([128, 4, 128], F32)
    R_S, R_snapacc = k.res("S"), k.res("snapacc")
    WC = [ar.al([128, 32, 64], BF16) for _ in range(3)]
    R_WC = k.res("WC")
    posT = ar.al([128, 32], BF16)
    cb = ar.al([128, 4], F32)
    xin = [[ar.al([128, 528], BF16) for _ in range(2)] for _ in range(2)]
    R_xin = [[k.res("xin%d%d" % (a, b)) for b in range(2)] for a in range(2)]
    CKc, SKc = ar.al([128, 32], F32), ar.al([128, 32], F32)
    R_ckc = k.res("ckc")
    VCf = ar.al([128, 512], F32)
    R_VCf = k.res("VCf")
    ctmp = ar.al([128, 4, 32], F32)
    R_ctmp = k.res("ctmp")
    for xi, nm in enumerate(("wck", "wckp", "wcv")):
        for g in range(2):
            k.dma("pool", WC[xi][64 * g:64 * g + 64].rearrange("p l e -> p (l e)"), Dr[nm], writes=[R_WC])
    k.dma("pool", posT, Dr["posT"], writes=[R_WC])
    k.op("dve", lambda e: e.memset(Sst, 0.0), writes=[R_S])
    k.op("dve", lambda e: e.memset(St.VsA[:, :, :, 64:65], 1.0), writes=[St.R_VsA])
    k.op("dve", lambda e: e.memset(St.VwA[:, :, :, 64:65], 1.0), writes=[St.R_VwA])
    for a in range(2):
        k.op("dve", lambda e, a=a: e.memset(xin[a][0][:, 0:16], 0.0), writes=[R_xin[a][0]])
    p6 = c.psum[6]
    fns = []
    for xi in range(3):
        for g in range(2):
            for l in range(32):
                fns.append(lambda e, xi=xi, g=g, l=l: e.matmul(p6[64 * g:64 * g + 64, xi:xi + 1], lhsT=WC[xi][64 * g:64 * g + 64, l, :],
                                                               rhs=posT[64 * g:64 * g + 64, l:l + 1], start=(l == 0), stop=(l == 31)))
    k.op("pe", fns, reads=[R_WC], writes=[c.R_ps[6]])
    k.op("dve", lambda e: e.tensor_copy(out=cb[:, 0:3], in_=p6[:, 0:3]), reads=[c.R_ps[6]], writes=[R_WC])

    NG = c.n_groups
    Hs = [W, W]
    vtoks, R_vtoks = [vtok, vtok], [R_vtok, R_vtok]
    p6b = c.psum[6].bitcast(BF16)

    def fm(ft, bank):
        k.op("pe", [(lambda e, i=i: e.matmul(c.psum[bank], lhsT=w1f[:, i, ft * 128:(ft + 1) * 128], rhs=hT[:, i, :],
                                             start=(i == 0), stop=(i == 7))) for i in range(8)],
             reads=[R_w1, R_hT], writes=[c.R_ps[bank]])

    def A_x(G):
        norm_transpose_group(k, c, W, Dr["xb"], G * 512, hT, R_hT)
        k.dma("sp", CKg, Dr["CK"][:, G * 512:(G + 1) * 512], writes=[R_rt])
        k.dma("sp", SKg, Dr["SK"][:, G * 512:(G + 1) * 512], writes=[R_rt])

    def A_kv(G):
        xb_ = G % 2
        for a in range(2):
            fm(a, 2 + a)
            k.op("act", lambda e, a=a: e.activation(out=xin[a][xb_][:, 16:528], in_=c.psum[2 + a], func=AF.Copy),
                 reads=[c.R_ps[2 + a]], writes=[R_xin[a][xb_]])
            k.op("pool", lambda e, a=a: e.tensor_copy(out=xin[a][1 - xb_][:, 0:16], in_=xin[a][xb_][:, 512:528]),
                 reads=[R_xin[a][xb_]], writes=[R_xin[a][1 - xb_]])
        for which, dst, R_dst in ((0, St.KTs, St.R_KTs), (1, St.KTw, St.R_KTw)):
            fm(2 + 2 * which, 2)
            fm(3 + 2 * which, 3)
            k.op("dve", lambda e: e.tensor_tensor(out=t1, in0=c.psum[2], in1=CKg, op=ALU.mult), reads=[c.R_ps[2], R_rt], writes=[R_t12])
            k.op("dve", lambda e: e.tensor_tensor(out=t2, in0=c.psum[3], in1=SKg, op=ALU.mult), reads=[c.R_ps[3], R_rt, R_t12], writes=[R_t12])
            k.op("pool", lambda e, dst=dst: e.tensor_tensor(out=dst[:, G * 512:(G + 1) * 512], in0=t1, in1=t2, op=ALU.add),
                 reads=[R_t12], writes=[R_dst])

    def A_tok(G):
        vt, R_vt = vtoks[G % 2], R_vtoks[G % 2]
        for tt in range(4):
            tile_ = 4 * G + tt
            k.op("pe", [(lambda e, i=i, tt=tt: e.matmul(c.psum[4][:, 0:256], lhsT=hT[:, i, tt * 128:(tt + 1) * 128], rhs=w1t[:, i, 0:256],
                                                        start=(i == 0), stop=(i == 7))) for i in range(8)],
                 reads=[R_w1, R_hT], writes=[c.R_ps[4]])
            k.op("pe", [(lambda e, i=i, tt=tt: e.matmul(c.psum[5], lhsT=hT[:, i, tt * 128:(tt + 1) * 128], rhs=w1t[:, i, 256:768],
                                                        start=(i == 0), stop=(i == 7))) for i in range(8)],
                 reads=[R_w1, R_hT], writes=[c.R_ps[5]])
            k.op("act", lambda e, tile_=tile_: e.activation(out=St.VsA[:, tile_, :, 0:64],
                                                            in_=c.psum[4][:, 0:128].rearrange("p (g d) -> p g d", g=2), func=AF.Copy),
                 reads=[c.R_ps[4]], writes=[St.R_VsA])
            k.op("act", lambda e, tile_=tile_: e.activation(out=St.VwA[:, tile_, :, 0:64],
                                                            in_=c.psum[4][:, 128:256].rearrange("p (g d) -> p g d", g=2), func=AF.Copy),
                 reads=[c.R_ps[4]], writes=[St.R_VwA])
            k.op("dve", lambda e, tt=tt: e.tensor_copy(out=vt[:, tt, :], in_=c.psum[5]), reads=[c.R_ps[5]], writes=[R_vt])

    def A_conv(G):
        xb_ = G % 2
        fns = []
        for xi in range(3):
            src = xin[0][xb_] if xi < 2 else xin[1][xb_]
            for g in range(2):
                for l in range(32):
                    fns.append(lambda e, xi=xi, g=g, l=l, src=src: e.matmul(
                        p6[64 * g:64 * g + 64, 32 * xi:32 * xi + 32], lhsT=WC[xi][64 * g:64 * g + 64, l, :],
                        rhs=src[64 * g:64 * g + 64, l:l + 497:16], start=(l == 0), stop=(l == 31)))
        k.op("pe", fns, reads=[R_WC, R_xin[0][xb_], R_xin[1][xb_]], writes=[c.R_ps[6]])
        ms = slice(32 * G, 32 * G + 32)
        k.dma("sp", CKc, Dr["CKc"][:, ms], writes=[R_ckc])
        k.dma("sp", SKc, Dr["SKc"][:, ms], writes=[R_ckc])
        k.op("dve", lambda e: e.tensor_scalar(out=ctmp[:, 0, :], in0=p6[:, 0:32], scalar1=cb[:, 0:1], scalar2=None, op0=ALU.add),
             reads=[c.R_ps[6], R_WC], writes=[R_ctmp])
        k.op("dve", lambda e: e.tensor_scalar(out=ctmp[:, 1, :], in0=p6[:, 32:64], scalar1=cb[:, 1:2], scalar2=None, op0=ALU.add),
             reads=[c.R_ps[6], R_WC], writes=[R_ctmp])
        k.op("dve", lambda e: e.tensor_scalar(out=VCf[:, ms], in0=p6[:, 64:96], scalar1=cb[:, 2:3], scalar2=None, op0=ALU.add),
             reads=[c.R_ps[6], R_WC], writes=[R_VCf])
        k.op("pool", lambda e: e.tensor_tensor(out=ctmp[:, 0, :], in0=ctmp[:, 0, :], in1=CKc, op=ALU.mult),
             reads=[R_ctmp, R_ckc], writes=[R_ctmp])
        k.op("pool", lambda e: e.tensor_tensor(out=ctmp[:, 1, :], in0=ctmp[:, 1, :], in1=SKc, op=ALU.mult),
             reads=[R_ctmp, R_ckc], writes=[R_ctmp])
        k.op("pool", lambda e: e.tensor_tensor(out=St.KC[:, ms], in0=ctmp[:, 0, :], in1=ctmp[:, 1, :], op=ALU.add),
             reads=[R_ctmp], writes=[St.R_KC])

    def A_f(G):
        H = Hs[G % 2]

        def front(hd):
            bank = 2 + hd % 2
            fm(6 + hd, bank)
            f_front(k, c, W, c.psum[bank], c.R_ps[bank], hd)
        front(0)
        front(1)
        f_back(k, c, W, 0, H)
        front(2)
        f_back(k, c, W, 1, H)
        front(3)
        f_back(k, c, W, 2, H)
        f_back(k, c, W, 3, H)

    def B_step(G, tt):
        H = Hs[G % 2]
        vt, R_vt = vtoks[G % 2], R_vtoks[G % 2]
        sl = slice(tt * 128, (tt + 1) * 128)
        k.op("pe", [(lambda e, hd=hd: e.transpose(out=p6b[:, hd * 128:(hd + 1) * 128], in_=H.kT[:, hd, sl], identity=c.identb))
                    for hd in range(4)], reads=[H.R_kT, c.R_const], writes=[c.R_ps[6]])
        k.op("act", lambda e: e.activation(out=ktok, in_=p6b[:, 0:512].rearrange("p (h x) -> p h x", h=4), func=AF.Copy),
             reads=[c.R_ps[6]], writes=[R_ktok])
        k.op("pe", [(lambda e, hd=hd: e.matmul(c.psum[7][:, hd * 128:(hd + 1) * 128], lhsT=ktok[:, hd, :],
                                               rhs=vt[:, tt, hd * 128:(hd + 1) * 128], start=True, stop=True))
                    for hd in range(4)], reads=[R_ktok, R_vt], writes=[c.R_ps[7]])
        Sf, Af = Sst.rearrange("p h x -> p (h x)"), snapacc.rearrange("p h x -> p (h x)")
        if tt == 0:
            k.op("dve", lambda e: e.tensor_scalar(out=Af, in0=Sf, scalar1=c.onehot[:, 0:1], scalar2=None, op0=ALU.mult),
                 reads=[R_S, c.R_const], writes=[R_snapacc])
        else:
            k.op("dve", lambda e: e.scalar_tensor_tensor(out=Af, in0=Sf, scalar=c.onehot[:, tt:tt + 1], in1=Af,
                                                         op0=ALU.mult, op1=ALU.add),
                 reads=[R_S, c.R_const, R_snapacc], writes=[R_snapacc])
        for hd in range(4):
            k.op("dve", lambda e, hd=hd: e.tensor_scalar(out=Sst[:, hd, :], in0=Sst[:, hd, :], scalar1=H.c1[:, hd, tt:tt + 1],
                                                         scalar2=None, op0=ALU.mult),
                 reads=[R_S, H.R_cols], writes=[R_S])
            k.op("dve", lambda e, hd=hd: e.scalar_tensor_tensor(
                out=Sst[:, hd, :], in0=c.psum[7][:, hd * 128:(hd + 1) * 128], scalar=H.c2[:, hd, tt:tt + 1], in1=Sst[:, hd, :],
                op0=ALU.mult, op1=ALU.add), reads=[c.R_ps[7], R_S, H.R_cols], writes=[R_S])
        if tt == 3:
            k.op("act", lambda e: e.activation(out=St.SNAP[:, G, :, :], in_=snapacc, func=AF.Copy), reads=[R_snapacc], writes=[St.R_SNAP])

    for G in range(NG + 1):
        if G < NG:
            A_x(G)
        if G >= 1:
            B_step(G - 1, 0)
            B_step(G - 1, 1)
        if G < NG:
            A_kv(G)
        if G >= 1:
            B_step(G - 1, 2)
            B_step(G - 1, 3)
        if G < NG:
            A_tok(G)
            A_conv(G)
            A_f(G)
    k.op("dve", lambda e: e.memset(St.VCA[:, :, :, 64:65], 1.0), writes=[St.R_VCA])
    for g in range(2):
        k.dma("pool", St.VCA[:, :, g, 65:193], Dr["ovl"], writes=[St.R_VCA])
    pw = c.psum[6]
    k.op("pe", [(lambda e, mt=mt: e.transpose(out=pw[:, mt * 128:(mt + 1) * 128], in_=VCf[:, mt * 128:(mt + 1) * 128], identity=c.identf))
                for mt in range(4)], reads=[R_VCf, c.R_const], writes=[c.R_ps[6]])
    for mt in range(4):
        k.op("act", lambda e, mt=mt: e.activation(out=St.VCA[:, mt, :, 0:64],
                                                  in_=pw[:, mt * 128:(mt + 1) * 128].rearrange("p (g d) -> p g d", g=2), func=AF.Copy),
             reads=[c.R_ps[6]], writes=[St.R_VCA])
    k.op("dve", lambda e: e.memset(St.VCA[0:1, 0, :, :], 0.0), writes=[St.R_VCA])
    ar.release(m0)


def phase_p2pre(k, c, ar, St):
    Dr = c.D
    m0 = ar.mark()
    W = WS()
    alloc_hg_ws(k, ar, W, nsets=2)
    alloc_x_ws(k, c, ar, W, None, scr=W.ab, R_scr=W.R_f)
    hT = ar.al([128, 8, 512], BF16)
    R_hT = k.res("hT2")
    wch = [c.R32f[:, 8192 + b * 4096: 8192 + (b + 1) * 4096].rearrange("p (a b) -> p a b", a=8) for b in range(2)]
    R_wch = [k.res("wch%d" % i) for i in range(2)]
    wgt = ar.al([128, 8, 32], BF16)
    R_wgt = k.res("wgt")
    wst = dict(n=0)
    CQg, SQg = ar.al([128, 512], F32), ar.al([128, 512], F32)
    R_rt = k.res("ropetabq")
    t1, t2, R_t12 = W.u, W.a, W.R_f
    qT = ar.al([128, 4, 512], BF16)
    R_qT = k.res("qTh")
    e1, R_e1 = W.u, W.R_f
    vtok = ar.al([128, 512], BF16)
    R_vtok = k.res("vtok2")
    sgt = ar.al([128, 512], F32)
    R_sgt = k.res("sgt")
    AT = ar.al([128, 4, 128], BF16)
    R_AT = k.res("AT")
    Sp = ar.al([128, 4, 128], BF16)
    R_Sp = k.res("Sp")
    gnt = ar.al([128, 512], F32)
    R_gnt = k.res("gnt")
    o1, o2, R_o = W.bq, W.a, W.R_f
    yb = ar.al([128, 512], BF16)
    R_yb = k.res("yb")
    hs = ar.al([128, 16], F32)
    R_hs = k.res("hs")
    k.dma("pool", wgt, Dr["w2t"][:, 1024:1056].rearrange("(kt p) n -> p kt n", p=128), writes=[R_wgt])
    k.dma("sp", gnt, Dr["g_hg4"].partition_broadcast(128), writes=[R_gnt])

    def wload(src, c0, n=512):
        b = wst["n"] % 2
        wst["n"] += 1
        k.dma("pool", wch[b][:, :, 0:n], src[:, c0:c0 + n].rearrange("(kt p) n -> p kt n", p=128), writes=[R_wch[b]])
        return wch[b], R_wch[b]

    for go in range(NT // 4):
        tok = slice(go * 512, (go + 1) * 512)
        norm_transpose_group(k, c, W, Dr["xo"], go * 512, hT, R_hT)
        k.dma("sp", CQg, Dr["CQ"][:, tok], writes=[R_rt])
        k.dma("sp", SQg, Dr["SQ"][:, tok], writes=[R_rt])

        def fm(wt, R_wt, j, bank):
            k.op("pe", [(lambda e, i=i: e.matmul(c.psum[bank], lhsT=wt[:, i, j * 128:(j + 1) * 128], rhs=hT[:, i, :],
                                                 start=(i == 0), stop=(i == 7))) for i in range(8)],
                 reads=[R_wt, R_hT], writes=[c.R_ps[bank]])
        wq, R_wq = wload(Dr["w2f"], 0)
        wqp, R_wqp = wload(Dr["w2f"], 512)
        for a in range(4):
            fm(wq, R_wq, a, 2)
            fm(wqp, R_wqp, a, 3)
            k.op("dve", lambda e: e.tensor_tensor(out=t1, in0=c.psum[2], in1=CQg, op=ALU.mult), reads=[c.R_ps[2], R_rt], writes=[R_t12])
            k.op("dve", lambda e: e.tensor_tensor(out=t2, in0=c.psum[3], in1=SQg, op=ALU.mult), reads=[c.R_ps[3], R_rt, R_t12], writes=[R_t12])
            k.op("pool", lambda e, a=a: e.tensor_tensor(out=c.QT[:, 4 * go:4 * go + 4, a, :], in0=t1.rearrange("p (i t) -> p i t", i=4),
                                                        in1=t2.rearrange("p (i t) -> p i t", i=4), op=ALU.add), reads=[R_t12], writes=[c.R_QT])
        whq, R_whq = wload(Dr["w2f"], 1024)
        whf, R_whf = wload(Dr["w2f"], 1536)
        def front(hd):
            bank = 2 + hd % 2
            fm(whf, R_whf, hd, bank)
            f_front(k, c, W, c.psum[bank], c.R_ps[bank], hd)

        def back(hd):
            f_back(k, c, W, hd)
            su, sa, sbq, slk, sL, sRF = W.sets[hd % 2]
            fm(whq, R_whq, hd, 6)
            for tt in range(4):
                sl = slice(tt * 128, (tt + 1) * 128)
                k.op("act", lambda e, sl=sl, tt=tt: e.activation(out=su[:, sl], in_=sL[:, sl], func=AF.Exp, bias=W.negr[:, hd, tt:tt + 1]),
                     reads=[sRF, W.R_cols], writes=[sRF])
            k.op("dve", lambda e: e.tensor_tensor(out=qT[:, hd, :], in0=c.psum[6], in1=su, op=ALU.mult),
                 reads=[c.R_ps[6], sRF], writes=[R_qT])
        front(0)
        front(1)
        back(0)
        front(2)
        back(1)
        front(3)
        back(2)
        back(3)
        whi, R_whi = wload(Dr["w2t"], 0)
        whg, R_whg = wload(Dr["w2t"], 512)
        for tt in range(4):
            i_own = 4 * go + tt
            sl = slice(tt * 128, (tt + 1) * 128)
            for (wt, R_wt, n, bank) in ((whi, R_whi, 512, 4), (whg, R_whg, 512, 5), (wgt, R_wgt, 32, 6)):
                k.op("pe", [(lambda e, i=i, wt=wt, n=n, bank=bank: e.matmul(c.psum[bank][:, 0:n], lhsT=hT[:, i, sl], rhs=wt[:, i, 0:n],
                                                                            start=(i == 0), stop=(i == 7))) for i in range(8)],
                     reads=[R_wt, R_hT], writes=[c.R_ps[bank]])
            k.op("dve", lambda e: e.tensor_copy(out=vtok, in_=c.psum[4]), reads=[c.R_ps[4]], writes=[R_vtok])
            k.op("act", lambda e: e.activation(out=sgt, in_=c.psum[5], func=AF.Silu), reads=[c.R_ps[5]], writes=[R_sgt])
            k.op("act", lambda e, i_own=i_own: e.activation(out=c.gsig[:, i_own, :], in_=c.psum[6][:, 0:24], func=AF.Sigmoid),
                 reads=[c.R_ps[6]], writes=[c.R_gsig])
            k.op("pe", [(lambda e, hd=hd: e.matmul(c.psum[7][:, hd * 128:(hd + 1) * 128], lhsT=W.kT[:, hd, sl], rhs=qT[:, hd, sl],
                                                   start=True, stop=True)) for hd in range(4)],
                 reads=[W.R_kT, R_qT], writes=[c.R_ps[7]])
            k.op("dve", lambda e: e.tensor_scalar(out=W.lk, in0=c.psum[7], scalar1=1e30, scalar2=-1e30, op0=ALU.min, op1=ALU.max),
                 reads=[c.R_ps[7], W.R_f], writes=[W.R_f])
            k.op("dve", lambda e: e.tensor_tensor(out=AT, in0=W.lk.rearrange("p (h x) -> p h x", h=4),
                                                  in1=c.tri.unsqueeze(1).broadcast_to([128, 4, 128]), op=ALU.mult),
                 reads=[W.R_f, c.R_constP], writes=[R_AT])
            for hd in range(4):
                k.op("act", lambda e, hd=hd, tt=tt, i_own=i_own: e.activation(out=Sp[:, hd, :], in_=St.SNAP[:, i_own, hd, :], func=AF.Copy,
                                                                              scale=W.er[:, hd, tt:tt + 1]),
                     reads=[St.R_SNAP, W.R_cols], writes=[R_Sp])
            fns = []
            for hd in range(4):
                fns.append(lambda e, hd=hd, tt=tt: e.matmul(c.psum[4][:, hd * 128:(hd + 1) * 128], lhsT=AT[:, hd, :],
                                                            rhs=vtok[:, hd * 128:(hd + 1) * 128], start=True, stop=False))
                fns.append(lambda e, hd=hd: e.matmul(c.psum[4][:, hd * 128:(hd + 1) * 128], lhsT=qT[:, hd, sl],
                                                     rhs=Sp[:, hd, :], start=False, stop=True))
            k.op("pe", fns, reads=[R_AT, R_vtok, R_qT, R_Sp], writes=[c.R_ps[4]])
            for hd in range(4):
                k.op("act", lambda e, hd=hd: e.activation(out=o2[:, hd * 128:(hd + 1) * 128], in_=c.psum[4][:, hd * 128:(hd + 1) * 128],
                                                          func=AF.Square, accum_out=hs[:, hd:hd + 1]),
                     reads=[c.R_ps[4]], writes=[R_o, R_hs])
            k.op("act", lambda e: e.activation(out=hs[:, 4:8], in_=hs[:, 0:4], func=AF.Sqrt, scale=1.0 / 128, bias=c.epsb[:, 0:1]),
                 reads=[R_hs, c.R_const], writes=[R_hs])
            k.op("dve", lambda e: e.reciprocal(out=hs[:, 8:12], in_=hs[:, 4:8]), reads=[R_hs], writes=[R_hs])
            k.op("dve", lambda e: e.tensor_tensor(out=o1, in0=c.psum[4], in1=gnt, op=ALU.mult), reads=[c.R_ps[4], R_gnt, R_o], writes=[R_o])
            k.op("pool", lambda e, tt=tt: e.tensor_tensor(out=o1, in0=o1, in1=sgt, op=ALU.mult), reads=[R_o, R_sgt], writes=[R_o])
            k.op("dve", lambda e: e.tensor_tensor(out=yb.rearrange("p (h x) -> p h x", h=4), in0=o1.rearrange("p (h x) -> p h x", h=4),
                                                  in1=hs[:, 8:12].unsqueeze(2).broadcast_to([128, 4, 128]), op=ALU.mult),
                 reads=[R_o, R_hs], writes=[R_yb])
            p6b = c.psum[6].bitcast(BF16)
            k.op("pe", [(lambda e, hd=hd: e.transpose(out=p6b[:, hd * 128:(hd + 1) * 128], in_=yb[:, hd * 128:(hd + 1) * 128], identity=c.identb))
                        for hd in range(4)], reads=[R_yb, c.R_const], writes=[c.R_ps[6]])
            k.op("act", lambda e, i_own=i_own: e.activation(out=c.oT_hg[:, :, i_own * 128:(i_own + 1) * 128],
                                                            in_=p6b[:, 0:512].rearrange("p (h x) -> p h x", h=4), func=AF.Copy),
                 reads=[c.R_ps[6]], writes=[c.R_oThg])
    ar.release(m0)


def phase_nsa(k, c, ar, St):
    Dr = c.D
    m0 = ar.mark()
    TINY = 1e-30
    CBi = [ar.al([128, 128], BF16) for _ in range(2)]
    VALi = [ar.al([128, 128], F32) for _ in range(2)]
    ADDCi = [ar.al([128, 128], F32) for _ in range(2)]
    R_tab = [k.res("nsatab%d" % i) for i in range(2)]
    R_tabP = [k.res("nsatabP%d" % i) for i in range(2)]
    WMT = ar.al([128, 8, 128], BF16)
    CM = ar.al([128, 4, 128], BF16)
    R_cst = k.res("nsacst")
    k.dma("pool", WMT, Dr["WMT"], writes=[R_cst])
    k.dma("pool", CM, Dr["CM"], writes=[R_cst])
    PT = [ar.al([128, 512], BF16) for _ in range(3)]
    R_PT = [k.res("PT%d" % i) for i in range(3)]
    Uc = ar.al([128, 4, 193], F32)
    R_Uc = k.res("Uc")
    Os = ar.al([128, 4, 65], F32)
    Ow = ar.al([128, 4, 65], F32)
    R_Os, R_Ow = k.res("Os"), k.res("Ow")
    score, sc2, imp = ar.al([128, 128], F32), ar.al([128, 128], F32), ar.al([128, 128], F32)
    R_sel = k.res("sel")
    selb = ar.al([128, 128], BF16)
    R_selb = k.res("selb")
    bd = ar.al([128, 4, 128], BF16)
    R_bd = k.res("bd")
    selX = ar.al([128, 128, 64], BF16)
    R_selX = k.res("selX")
    cols = ar.al([128, 64], F32)
    R_cols = k.res("nsacols")
    acc, tmp = ar.al([128, 4, 64], F32), ar.al([128, 4, 64], F32)
    R_acc = k.res("nsaacc")
    onsa = ar.al([128, 2, 4, 64], BF16)
    R_onsa = k.res("onsa")
    st = dict(s=0, p=0)
    pO_s, pO_w = c.psum[3][:, 0:260], c.psum[4][:, 0:260]
    pU = [c.psum[5], c.psum[6]]

    pend = []

    def flush_pv(keep=0):
        while len(pend) > keep:
            pend.pop(0)()

    def unit(KT, R_KT, kt_slice, QTg, g, bias, Vaug, R_V, outs, R_outs):
        sb = st["s"] % 3
        st["s"] += 1
        pb = st["p"] % 3
        st["p"] += 1
        S = c.psum[sb]
        fns = [lambda e: e.matmul(S, lhsT=KT[64 * g:64 * g + 64, kt_slice], rhs=QTg, start=True, stop=(bias is None))]
        rd = [R_KT, c.R_QT]
        if bias is not None:
            bl, R_bl = bias
            fns.append(lambda e: e.matmul(S, lhsT=bl, rhs=c.I4, start=False, stop=True))
            rd += [R_bl, c.R_constP]
        k.op("pe", fns, reads=rd, writes=[c.R_ps[sb]])
        k.op("act", lambda e: e.activation(out=PT[pb], in_=S, func=AF.Exp), reads=[c.R_ps[sb]], writes=[R_PT[pb]])

        def pv():
            k.op("pe", [(lambda e, a=a: e.matmul(outs[a], lhsT=PT[pb][:, a * 128:(a + 1) * 128], rhs=Vaug, start=False, stop=False,
                                                 skip_group_check=True)) for a in range(4)],
                 reads=[R_PT[pb], R_V], writes=R_outs)
        pend.append(pv)
        flush_pv(keep=2)

    for i in range(c.n_blocks):
        tb = i % 2
        k.dma("pool", CBi[tb], Dr["CB"][:, i, :], writes=[R_tabP[tb]])
        k.dma("sp", VALi[tb], Dr["VAL"][:, i, :], writes=[R_tab[tb]])
        k.dma("sp", ADDCi[tb], Dr["ADDC"][:, i, :], writes=[R_tab[tb]])
        for g in range(2):
            QTg = c.QT[64 * g:64 * g + 64, i, :, :].rearrange("p a t -> p (a t)")
            nmt = i // 4 + 1
            k.op("dve", lambda e: e.memset(pU[0], 0.0), writes=[c.R_ps[5]])
            k.op("dve", lambda e: e.memset(pU[1], 0.0), writes=[c.R_ps[6]])
            outsU = [pU[a // 2][:, (a % 2) * 193:(a % 2) * 193 + 193] for a in range(4)]
            for mt in range(nmt):
                bias = (CBi[tb], R_tabP[tb]) if mt == nmt - 1 else None
                unit(St.KC, St.R_KC, slice(mt * 128, (mt + 1) * 128), QTg, g, bias, St.VCA[:, mt, g, :], St.R_VCA, outsU, [c.R_ps[5], c.R_ps[6]])
            flush_pv()
            k.op("act", lambda e: e.activation(out=Uc[:, 0:2, :], in_=pU[0][:, 0:386].rearrange("p (a x) -> p a x", a=2), func=AF.Copy),
                 reads=[c.R_ps[5]], writes=[R_Uc])
            k.op("act", lambda e: e.activation(out=Uc[:, 2:4, :], in_=pU[1][:, 0:386].rearrange("p (a x) -> p a x", a=2), func=AF.Copy),
                 reads=[c.R_ps[6]], writes=[R_Uc])
            k.op("dve", lambda e: e.memset(c.psum[4], 0.0), writes=[c.R_ps[4]])
            outsW = [pO_w[:, a * 65:(a + 1) * 65] for a in range(4)]
            for r in range(8):
                kt = 4 * i - 4 + r
                if kt < 0:
                    continue
                unit(St.KTw, St.R_KTw, slice(kt * 128, (kt + 1) * 128), QTg, g, (WMT[:, r, :], R_cst), St.VwA[:, kt, g, :], St.R_VwA, outsW, [c.R_ps[4]])
            zc, rzc = cols[:, 0:4], cols[:, 4:8]
            k.op("dve", lambda e: e.tensor_scalar(out=zc, in0=Uc[:, :, 64], scalar1=TINY, scalar2=None, op0=ALU.max), reads=[R_Uc], writes=[R_cols])
            k.op("dve", lambda e: e.reciprocal(out=rzc, in_=zc), reads=[R_cols], writes=[R_cols])
            k.op("dve", lambda e: e.tensor_scalar(out=imp, in0=Uc[:, 0, 65:193], scalar1=rzc[:, 0:1], scalar2=None, op0=ALU.mult),
                 reads=[R_Uc, R_cols], writes=[R_sel])
            for a in range(1, 4):
                k.op("dve", lambda e, a=a: e.scalar_tensor_tensor(out=imp, in0=Uc[:, a, 65:193], scalar=rzc[:, a:a + 1], in1=imp,
                                                                  op0=ALU.mult, op1=ALU.add), reads=[R_Uc, R_cols, R_sel], writes=[R_sel])
            k.op("dve", lambda e: e.tensor_tensor(out=score, in0=imp, in1=VALi[tb], op=ALU.mult), reads=[R_sel, R_tab[tb]], writes=[R_sel])
            k.op("dve", lambda e: e.tensor_tensor(out=score, in0=score, in1=ADDCi[tb], op=ALU.add), reads=[R_sel, R_tab[tb]], writes=[R_sel])
            m8a, m8b = cols[:, 8:16], cols[:, 16:24]
            k.op("dve", lambda e: e.max(out=m8a, in_=score), reads=[R_sel], writes=[R_cols])
            k.op("dve", lambda e: e.match_replace(out=sc2, in_to_replace=m8a, in_values=score, imm_value=-1e9), reads=[R_sel, R_cols], writes=[R_sel])
            k.op("dve", lambda e: e.max(out=m8b, in_=sc2), reads=[R_sel], writes=[R_cols])
            k.op("dve", lambda e: e.tensor_scalar(out=selb, in0=score, scalar1=m8b[:, 7:8], scalar2=NEG, op0=ALU.is_lt, op1=ALU.mult),
                 reads=[R_sel, R_cols], writes=[R_selb])
            for r in range(4):
                kt = 4 * i + r
                k.op("dve", lambda e, r=r, kt=kt: e.tensor_tensor(
                    out=bd[:, r, :].rearrange("p (b x) -> p b x", b=2), in0=CM[:, r, :].rearrange("p (b x) -> p b x", b=2),
                    in1=selb[:, 2 * kt:2 * kt + 2].unsqueeze(2).broadcast_to([128, 2, 64]), op=ALU.add),
                    reads=[R_cst, R_selb], writes=[R_bd])
            if i > 0:
                nbk = 8 * i
                k.op("pool", lambda e, nbk=nbk: e.tensor_copy(out=selX[:, 0:nbk, :], in_=selb[:, 0:nbk].unsqueeze(2).broadcast_to([128, nbk, 64])),
                     reads=[R_selb], writes=[R_selX])
            k.op("dve", lambda e: e.memset(c.psum[3], 0.0), writes=[c.R_ps[3]])
            outsS = [pO_s[:, a * 65:(a + 1) * 65] for a in range(4)]
            for kt in range(4 * i + 4):
                if kt < 4 * i:
                    bl = selX[:, 2 * kt:2 * kt + 2, :].rearrange("p b x -> p (b x)")
                    bias = (bl, R_selX)
                else:
                    bias = (bd[:, kt - 4 * i, :], R_bd)
                unit(St.KTs, St.R_KTs, slice(kt * 128, (kt + 1) * 128), QTg, g, bias, St.VsA[:, kt, g, :], St.R_VsA, outsS, [c.R_ps[3]])
            flush_pv()
            k.op("act", lambda e: e.activation(out=Os, in_=pO_s.rearrange("p (a x) -> p a x", a=4), func=AF.Copy), reads=[c.R_ps[3]], writes=[R_Os])
            k.op("act", lambda e: e.activation(out=Ow, in_=pO_w.rearrange("p (a x) -> p a x", a=4), func=AF.Copy), reads=[c.R_ps[4]], writes=[R_Ow])
            gs = c.gsig[:, i, 12 * g:12 * g + 12].rearrange("p (a x) -> p a x", a=4)
            zs, zw, cfc, cfs, cfw = cols[:, 24:28], cols[:, 28:32], cols[:, 32:36], cols[:, 36:40], cols[:, 40:44]
            k.op("dve", lambda e: e.tensor_scalar(out=zs, in0=Os[:, :, 64], scalar1=TINY, scalar2=None, op0=ALU.max), reads=[R_Os], writes=[R_cols])
            k.op("dve", lambda e: e.tensor_scalar(out=zw, in0=Ow[:, :, 64], scalar1=TINY, scalar2=None, op0=ALU.max), reads=[R_Ow], writes=[R_cols])
            k.op("dve", lambda e: e.reciprocal(out=zs, in_=zs), reads=[R_cols], writes=[R_cols])
            k.op("dve", lambda e: e.reciprocal(out=zw, in_=zw), reads=[R_cols], writes=[R_cols])
            k.op("dve", lambda e: e.tensor_tensor(out=cfc, in0=rzc, in1=gs[:, :, 0], op=ALU.mult), reads=[R_cols, c.R_gsig], writes=[R_cols])
            k.op("dve", lambda e: e.tensor_tensor(out=cfs, in0=zs, in1=gs[:, :, 1], op=ALU.mult), reads=[R_cols, c.R_gsig], writes=[R_cols])
            k.op("dve", lambda e: e.tensor_tensor(out=cfw, in0=zw, in1=gs[:, :, 2], op=ALU.mult), reads=[R_cols, c.R_gsig], writes=[R_cols])
            bc = lambda col: col.unsqueeze(2).broadcast_to([128, 4, 64])
            k.op("dve", lambda e: e.tensor_tensor(out=acc, in0=Uc[:, :, 0:64], in1=bc(cfc), op=ALU.mult), reads=[R_Uc, R_cols], writes=[R_acc])
            k.op("dve", lambda e: e.tensor_tensor(out=tmp, in0=Os[:, :, 0:64], in1=bc(cfs), op=ALU.mult), reads=[R_Os, R_cols, R_acc], writes=[R_acc])
            k.op("pool", lambda e: e.tensor_tensor(out=acc, in0=acc, in1=tmp, op=ALU.add), reads=[R_acc], writes=[R_acc])
            k.op("dve", lambda e: e.tensor_tensor(out=tmp, in0=Ow[:, :, 0:64], in1=bc(cfw), op=ALU.mult), reads=[R_Ow, R_cols, R_acc], writes=[R_acc])
            k.op("pool", lambda e, g=g: e.tensor_tensor(out=onsa[:, g, :, :], in0=acc, in1=tmp, op=ALU.add), reads=[R_acc], writes=[R_onsa])
        p7b = c.psum[7].bitcast(BF16)
        of = onsa.rearrange("p g a d -> p (g a d)")
        k.op("pe", [(lambda e, j=j: e.transpose(out=p7b[:, j * 128:(j + 1) * 128], in_=of[:, j * 128:(j + 1) * 128], identity=c.identb))
                    for j in range(4)], reads=[R_onsa, c.R_const], writes=[c.R_ps[7]])
        k.op("act", lambda e, i=i: e.activation(out=c.oT_nsa[:, :, i * 128:(i + 1) * 128],
                                                in_=p7b[:, 0:512].rearrange("p (j x) -> p j x", j=4), func=AF.Copy),
             reads=[c.R_ps[7]], writes=[c.R_oTnsa])
    ar.release(m0)


def phase_p2c(k, c, ar, X1, R_X1):
    Dr = c.D
    m0 = ar.mark()
    W = WS()
    scr = ar.al([128, D], F32)
    alloc_x_ws(k, c, ar, W, None, scr=scr, R_scr=k.res("scr2c"))
    hT = ar.al([128, 8, 512], BF16)
    R_hT = k.res("hT3")
    wbn, wbh = ar.al([128, 4, D], BF16), ar.al([128, 4, D], BF16)
    R_wb_ = k.res("wbr")
    wb = [ar.al([128, 8, 512], BF16) for _ in range(4)]
    R_wb = [k.res("wchc%d" % i) for i in range(4)]
    mixT = ar.al([128, 8, 512], BF16)
    R_mixT = k.res("mixT")
    sg1, sg2, mx1 = ar.al([128, 512], F32), ar.al([128, 512], F32), ar.al([128, 512], F32)
    R_sg1, R_sg2, R_mx = k.res("sg1"), k.res("sg2"), k.res("mx1")
    k.dma("pool", wbn, Dr["w_brn"].rearrange("(kt p) n -> p kt n", p=128), writes=[R_wb_])
    k.dma("pool", wbh, Dr["w_brh"].rearrange("(kt p) n -> p kt n", p=128), writes=[R_wb_])

    def wl(buf, src, c0):
        k.dma("pool", wb[buf], src[:, c0:c0 + 512].rearrange("(kt p) n -> p kt n", p=128), writes=[R_wb[buf]])

    NGo = NT // 4
    wl(0, Dr["w_mg"], 0)
    wl(1, Dr["w_mg"], 1024)
    for go in range(NGo):
        p = go % 2
        A = (2 * p, 2 * p + 1)
        B = (2 - 2 * p, 3 - 2 * p)
        tok = slice(go * 512, (go + 1) * 512)
        for tt in range(4):
            t = 4 * go + tt
            k.dma("sp", X1[:, t, :], Dr["xo"][t * 128:(t + 1) * 128, :], writes=[R_X1[t]])
        wl(B[0], Dr["w_mg"], 512)
        wl(B[1], Dr["w_mg"], 1024 + 512)
        norm_transpose_group(k, c, W, Dr["xo"], go * 512, hT, R_hT)
        for hf in range(2):
            w0, w1_ = (A if hf == 0 else B)
            for f4 in range(4):
                ft = hf * 4 + f4
                fs = slice(f4 * 128, (f4 + 1) * 128)
                gs_ = slice(ft * 128, (ft + 1) * 128)
                k.op("pe", [(lambda e, i=i: e.matmul(c.psum[2], lhsT=wb[w0][:, i, fs], rhs=hT[:, i, :], start=(i == 0), stop=(i == 7)))
                            for i in range(8)], reads=[R_wb[w0], R_hT], writes=[c.R_ps[2]])
                k.op("pe", [(lambda e, i=i: e.matmul(c.psum[3], lhsT=wb[w1_][:, i, fs], rhs=hT[:, i, :], start=(i == 0), stop=(i == 7)))
                            for i in range(8)], reads=[R_wb[w1_], R_hT], writes=[c.R_ps[3]])
                k.op("pe", [(lambda e, i=i: e.matmul(c.psum[4], lhsT=wbn[:, i, gs_], rhs=c.oT_nsa[:, i, tok], start=(i == 0), stop=(i == 3)))
                            for i in range(4)], reads=[R_wb_, c.R_oTnsa], writes=[c.R_ps[4]])
                k.op("pe", [(lambda e, i=i: e.matmul(c.psum[5], lhsT=wbh[:, i, gs_], rhs=c.oT_hg[:, i, tok], start=(i == 0), stop=(i == 3)))
                            for i in range(4)], reads=[R_wb_, c.R_oThg], writes=[c.R_ps[5]])
                k.op("act", lambda e: e.activation(out=sg1, in_=c.psum[2], func=AF.Sigmoid), reads=[c.R_ps[2]], writes=[R_sg1])
                k.op("act", lambda e: e.activation(out=sg2, in_=c.psum[3], func=AF.Sigmoid), reads=[c.R_ps[3]], writes=[R_sg2])
                k.op("dve", lambda e: e.tensor_tensor(out=mx1, in0=c.psum[4], in1=sg1, op=ALU.mult), reads=[c.R_ps[4], R_sg1], writes=[R_mx])
                k.op("dve", lambda e: e.tensor_tensor(out=sg2, in0=c.psum[5], in1=sg2, op=ALU.mult), reads=[c.R_ps[5], R_sg2], writes=[R_sg2])
                k.op("pool", lambda e, ft=ft: e.tensor_tensor(out=mixT[:, ft, :], in0=mx1, in1=sg2, op=ALU.add),
                     reads=[R_mx, R_sg2], writes=[R_mixT])
            if hf == 0:
                wl(A[0], Dr["w_out"], 0)
                wl(A[1], Dr["w_out"], 512)
            elif go + 1 < NGo:
                wl(B[0], Dr["w_mg"], 0)
                wl(B[1], Dr["w_mg"], 1024)
        for tt in range(4):
            t = 4 * go + tt
            for hf in range(2):
                bank = 6 + hf
                k.op("pe", [(lambda e, i=i: e.matmul(c.psum[bank], lhsT=mixT[:, i, tt * 128:(tt + 1) * 128], rhs=wb[A[hf]][:, i, :],
                                                     start=(i == 0), stop=(i == 7))) for i in range(8)],
                     reads=[R_mixT, R_wb[A[hf]]], writes=[c.R_ps[bank]])
                xs = X1[:, t, hf * 512:(hf + 1) * 512]
                k.op("dve", lambda e, xs=xs: e.tensor_tensor(out=xs, in0=c.psum[bank], in1=xs, op=ALU.add),
                     reads=[c.R_ps[bank], R_X1[t]], writes=[R_X1[t]])
    ar.release(m0)


def phase_attn(k, c, ar, stage):
    St = WS()
    St.KTs, St.KTw = ar.al([128, SEQ], BF16), ar.al([128, SEQ], BF16)
    St.VsA, St.VwA = ar.al([128, 64, 2, 65], BF16), ar.al([128, 64, 2, 65], BF16)
    St.KC = ar.al([128, 512], BF16)
    St.VCA = ar.al([128, 4, 2, 193], BF16)
    St.SNAP = ar.al([128, NT, 4, 128], BF16)
    for n in ("KTs", "KTw", "VsA", "VwA", "KC", "VCA", "SNAP"):
        setattr(St, "R_" + n, k.res(n))
    phase_p1(k, c, ar, St)
    for n in ("KTs", "KTw", "VsA", "VwA", "KC", "VCA", "SNAP"):
        c.dump(n, getattr(St, n), [getattr(St, "R_" + n)])
    k.flush()
    phase_p2pre(k, c, ar, St)
    c.dump("QT", c.QT, [c.R_QT])
    c.dump("gsig", c.gsig, [c.R_gsig])
    c.dump("oT_hg", c.oT_hg, [c.R_oThg])
    k.flush()
    phase_nsa(k, c, ar, St)
    c.dump("oT_nsa", c.oT_nsa, [c.R_oTnsa])
    return St


def build(stage="full", n_exp=NE):
    nc = bass.Bass("TRN2", target_bir_lowering=False)
    Dr = {}

    def din(name, shape, dt=F32):
        Dr[name] = nc.dram_tensor(name, list(shape), dt, kind="ExternalInput").ap()

    for name, shape, dt in input_specs(max(n_exp, 1), stage):
        din(name, shape, dt)
    Dr["y"] = nc.dram_tensor("y", [NTOK, D], F32, kind="ExternalOutput").ap()
    with ExitStack() as es:
        ARENA_BYTES = 207 * 1024
        big = es.enter_context(nc.sbuf_tensor("arena", [128, ARENA_BYTES // 2], BF16))
        pst = es.enter_context(nc.psum_tensor("ps", [128, 4096], F32))
        ar = Arena(big, ARENA_BYTES)
        k = KH(nc, es)
        k.oplim = _NC_CACHE.get("oplim", 10 ** 9)
        c = Ctx()
        c.nc, c.D, c.n_exp = nc, Dr, n_exp
        dumps = []

        def dump(name, ap, rs):
            if not _NC_CACHE.get("dbg"):
                return
            dt_ = ap.dtype
            dr = nc.dram_tensor("dbg_" + name, list(ap.shape), dt_, kind="ExternalOutput").ap()
            r = k.res("dbg_" + name)
            k.dma("sp", dr, ap, reads=list(rs), key=r)
            dumps.append(r)
        c.dump = dump
        c.psum = [pst[:, i * 512:(i + 1) * 512] for i in range(8)]
        c.pwide = lambda i: pst[:, i * 512:(i + 2) * 512]
        c.R_ps = [k.res("psb%d" % i, excl=True) for i in range(8)]
        c.R_const = k.res("const")
        c.identf = ar.al([128, 128], F32)
        c.identb = ar.al([128, 128], BF16)
        c.epsb = ar.al([128, 1], F32)
        c.ssq = ar.al([128, 8], F32)
        c.std = ar.al([128, 8], F32)
        c.rstd = ar.al([128, 8], F32)
        c.rs_res = [k.res("rs%d" % i) for i in range(8)]
        c.rs_i = 0
        k.dma("sp", c.identf, Dr["identf"], writes=[c.R_const])
        k.dma("sp", c.identb, Dr["identb"], writes=[c.R_const])
        k.op("dve", lambda e: e.memset(c.epsb, EPS), writes=[c.R_const])
        c.n_groups = _NC_CACHE.get("n_groups", 16)
        c.n_blocks = _NC_CACHE.get("n_blocks", NT)
        c.R32 = ar.al([128, 8, NTOK], BF16)
        c.R32f = c.R32.rearrange("p a b -> p (a b)")
        c.QT = c.R32f[:, 0:8192].rearrange("p (i a t) -> p i a t", i=NT, a=4)
        c.oT_nsa = c.R32[:, 4:8, :]
        c.oT_hg = ar.al([128, 4, NTOK], BF16)
        c.gsig = ar.al([128, NT, 24], F32)
        c.R_QT, c.R_oTnsa, c.R_oThg, c.R_gsig = k.res("QT"), k.res("oTnsa"), k.res("oThg"), k.res("gsig")
        hfT = c.R32
        R_hfT = [k.res("hfT_%d" % t) for t in range(NT)]
        R_X1 = [k.res("x1_%d" % t) for t in range(NT)]
        if stage != "moe_only":
            c.I4 = ar.al([128, 512], BF16)
            c.tri = ar.al([128, 128], BF16)
            c.onehot = ar.al([128, 4], F32)
            c.R_constP = k.res("constP")
            k.dma("pool", c.I4, Dr["I4"], writes=[c.R_constP])
            k.dma("pool", c.tri, Dr["tri"], writes=[c.R_constP])
            k.dma("sp", c.onehot, Dr["onehot"], writes=[c.R_const])
            setup_lb(k, c, ar)
            M1 = ar.mark()
            phase_attn(k, c, ar, stage)
            for r in dumps:
                k.rec["sp"].append(("w", r.dsem, r.dcnt))
            k.flush()
            ar.release(M1)
        X1 = ar.al([128, NT, D], F32)
        if stage == "moe_only":
            for t in range(NT):
                k.dma("sp", X1[:, t, :], Dr["xo"][t * 128:(t + 1) * 128, :], writes=[R_X1[t]])
        else:
            phase_p2c(k, c, ar, X1, R_X1)
            c.dump("X1", X1, R_X1)
            for r in dumps:
                if r.name == "dbg_X1":
                    k.rec["sp"].append(("w", r.dsem, r.dcnt))
        k.flush()
        if n_exp >= 0:
            phase_moe(k, c, ar, X1, R_X1, hfT, R_hfT)
            k.flush()
        phase_final(k, c, ar, X1, R_X1)
        k.flush()
    return nc


def input_specs(ne=NE, stage="full"):
    return [
        ("xb", (SEQ, D), F32), ("xo", (NTOK, D), F32),
        ("g_ffn", (D,), F32), ("g_fin", (D,), F32),
        ("w_r", (D, 36), F32), ("b_r", (36,), F32),
        ("w_gate", (ne, D, DFF), F32), ("w_up", (ne, D, DFF), F32), ("w_down", (ne, DFF, D), F32),
        ("identf", (128, 128), F32), ("identb", (128, 128), BF16),
    ] + (attn_input_specs() if stage != "moe_only" else [])


_NC_CACHE = {}


def host_inputs(inp, core, ne=NE):
    b, cp = core // 4, core % 4
    x = np.asarray(inp["x"], dtype=np.float32)
    m = {}
    m["xb"] = np.ascontiguousarray(x[b])
    m["xo"] = np.ascontiguousarray(x[b].reshape(NT, 4, 128, D)[:, cp].reshape(NTOK, D))
    m["g_ffn"] = np.ascontiguousarray(inp["ffn_norm"][0])
    m["g_fin"] = np.ascontiguousarray(inp["final_norm"])
    m["w_r"] = np.ascontiguousarray(np.concatenate([inp["w_grp"][0], inp["w_rtr"][0]], axis=1))
    m["b_r"] = np.ascontiguousarray(np.concatenate([inp["b_grp"][0], inp["b_rtr"][0]], axis=0))
    m["w_gate"] = np.ascontiguousarray(inp["w_gate"][0, :ne])
    m["w_up"] = np.ascontiguousarray(inp["w_up"][0, :ne])
    m["w_down"] = np.ascontiguousarray(inp["w_down"][0, :ne])
    m["identf"] = np.eye(128, dtype=np.float32)
    m["identb"] = np.eye(128, dtype=np.float32).astype(ml_dtypes.bfloat16)
    if _NC_CACHE.get("stage", "full") != "moe_only":
        m.update(attn_host_inputs(inp, b, cp))
    return m


def kernel(**inp):
    inp = {k_: np.asarray(v) for k_, v in inp.items()}
    stage = _NC_CACHE.get("stage", "full")
    key = ("nc", stage)
    if key not in _NC_CACHE:
        _NC_CACHE[key] = build(stage, _NC_CACHE.get("n_exp", NE))
    nc = _NC_CACHE[key]
    shared = None
    in_maps = []
    for core in range(8):
        m = host_inputs(inp, core, max(_NC_CACHE.get("n_exp", NE), 1))
        if shared is None:
            shared = m
        else:
            for kk in ("w_gate", "w_up", "w_down"):
                m[kk] = shared[kk]
        in_maps.append(m)
    res = run_bass_kernel_spmd(nc, in_maps, core_ids=list(range(8)))
    _NC_CACHE["last_results"] = res.results
    out = np.zeros((2, SEQ // 128, 128, D), dtype=np.float32)
    for core in range(8):
        b, cp = core // 4, core % 4
        y = np.asarray(res.results[core]["y"]).reshape(NT, 128, D)
        out[b, cp::4] = y
    return out.reshape(2, SEQ, D)
```

```python
import numpy as np
import ml_dtypes
import concourse.bass as bass
import concourse.mybir as mybir
from concourse.bass_utils import run_bass_kernel_spmd
from contextlib import ExitStack

F32 = mybir.dt.float32
BF16 = mybir.dt.bfloat16
AF = mybir.ActivationFunctionType
ALU = mybir.AluOpType
AX = mybir.AxisListType


class Res:
    __slots__ = ("name", "w", "rs", "dsem", "dcnt", "excl")

    def __init__(self, name, excl=False):
        self.name = name
        self.excl = excl
        self.w = None
        self.rs = {}
        self.dsem = None
        self.dcnt = 0


class _Proxy:
    def __init__(self):
        self.calls = []

    def __getattr__(self, name):
        def rec(*a, **kw):
            self.calls.append((name, a, kw))
        return rec


def _bind(f):
    p = _Proxy()
    f(p)
    assert len(p.calls) == 1, "one engine instruction per callable"
    name, a, kw = p.calls[0]
    return lambda eng: getattr(eng, name)(*a, **kw)


class KH:
    ENG = ("pe", "dve", "act", "pool", "sp")

    def __init__(self, nc, es):
        self.nc = nc
        self.es = es
        self.sem = {}
        self.cnt = {}
        for e in self.ENG:
            self.sem[e] = es.enter_context(nc.semaphore("s_" + e))
            self.cnt[e] = 0
        self.rec = {e: [] for e in self.ENG}
        self.seen = {e: {} for e in self.ENG}
        self.nsem = len(self.ENG)
        self.semobj = dict(self.sem)

    def res(self, name, excl=False):
        return Res(name, excl)

    def _dma_sem(self, r):
        if r.dsem is None:
            r.dsem = "d_" + r.name + "_%d" % self.nsem
            self.semobj[r.dsem] = self.es.enter_context(self.nc.semaphore(r.dsem))
            self.nsem += 1
        return r.dsem

    def _deps(self, e, reads, writes):
        deps = {}
        for r in reads:
            if r.w is not None:
                k, v = r.w
                deps[k] = max(deps.get(k, 0), v)
        for w in writes:
            if w.w is not None:
                k, v = w.w
                deps[k] = max(deps.get(k, 0), v)
            for k, v in w.rs.items():
                deps[k] = max(deps.get(k, 0), v)
        seen = self.seen[e]
        for k, v in deps.items():
            if seen.get(k, 0) >= v:
                continue
            seen[k] = v
            self.rec[e].append(("w", k, v))

    def op(self, e, fns, reads=(), writes=()):
        if callable(fns):
            fns = [fns]
        self.opn = getattr(self, "opn", 0) + 1
        if self.opn > getattr(self, "oplim", 10 ** 9):
            return
        ex = [r for r in reads if r.excl]
        if ex:
            reads = [r for r in reads if not r.excl]
            writes = list(writes) + [r for r in ex if r not in writes]
        self._deps(e, reads, writes)
        self.cnt[e] += 1
        v = self.cnt[e]
        fns = [_bind(f) for f in fns]
        for f in fns[:-1]:
            self.rec[e].append(("i", f, None, 0))
        self.rec[e].append(("i", fns[-1], e, 1))
        self.seen[e][e] = max(self.seen[e].get(e, 0), 0)
        for r in reads:
            r.rs[e] = v
        for w in writes:
            w.w = (e, v)
            w.rs = {}

    def dma(self, q, out, in_, reads=(), writes=(), key=None, **kw):
        self._deps(q, reads, writes)
        kr = key or (writes[0] if writes else reads[0])
        sk = self._dma_sem(kr)
        kr.dcnt += 16
        v = kr.dcnt
        self.rec[q].append(("i", lambda eng: eng.dma_start(out=out, in_=in_, **kw), sk, 16))
        for r in reads:
            r.rs[sk] = v
        for w in writes:
            w.w = (sk, v)
            w.rs = {}

    def wait_res(self, e, rs):
        self._deps(e, rs, ())

    def simulate(self):
        if not hasattr(self, "simval"):
            self.simval = {}
        val = self.simval
        ptr = {e: 0 for e in self.ENG}
        prog = True
        while prog:
            prog = False
            for e in self.ENG:
                items = self.rec[e]
                while ptr[e] < len(items):
                    it = items[ptr[e]]
                    if it[0] == "w":
                        if val.get(it[1], 0) >= it[2]:
                            ptr[e] += 1
                            prog = True
                        else:
                            break
                    else:
                        if it[2] is not None:
                            val[it[2]] = val.get(it[2], 0) + it[3]
                        ptr[e] += 1
                        prog = True
        for e in self.ENG:
            if ptr[e] < len(self.rec[e]):
                it = self.rec[e][ptr[e]]
                raise RuntimeError("DEADLOCK: engine %s stuck at item %d/%d waiting %s >= %s (have %s)" % (
                    e, ptr[e], len(self.rec[e]), it[1], it[2], val.get(it[1], 0)))

    def flush(self, name=None):
        nc = self.nc
        rec = self.rec
        semobj = self.semobj
        self.simulate()
        import os
        if os.environ.get("KH_DEBUG"):
            print("KH flush: ops so far", getattr(self, "opn", 0), {e: len(v) for e, v in self.rec.items()}, "nsem", self.nsem, flush=True)

        def play(eng, items):
            for it in items:
                if it[0] == "w":
                    eng.wait_ge(semobj[it[1]], it[2])
                else:
                    ins = it[1](eng)
                    if it[2] is not None:
                        ins.then_inc(semobj[it[2]], it[3])

        with nc.Block() as block:
            if rec["sp"]:
                @block.sync
                def _(eng):
                    play(eng, rec["sp"])
            if rec["pe"]:
                @block.tensor
                def _(eng):
                    play(eng, rec["pe"])
            if rec["dve"]:
                @block.vector
                def _(eng):
                    play(eng, rec["dve"])
            if rec["act"]:
                @block.scalar
                def _(eng):
                    play(eng, rec["act"])
            if rec["pool"]:
                @block.gpsimd
                def _(eng):
                    play(eng, rec["pool"])
        self.rec = {e: [] for e in self.ENG}

NEG = -30000.0
NT = 16
NTOK = NT * 128
SEQ = 8192
D = 1024
NE = 32
DFF = 512
EPS = 1e-6


class Arena:
    def __init__(self, big, nbytes):
        self.big = big
        self.n = nbytes
        self.off = 0

    def mark(self):
        return self.off

    def release(self, m):
        import os
        if os.environ.get("KH_DEBUG"):
            print("arena release: peak", getattr(self, "peak", 0), "->", m, "of", self.n, flush=True)
        self.peak = m
        self.off = m

    def al(self, shape, dt):
        esz = 4 if dt == F32 else 2
        per = int(np.prod(shape[1:])) * esz
        self.off = (self.off + 63) // 64 * 64
        o = self.off
        assert o + per <= self.n, ("arena overflow", o, per, self.n)
        self.off = o + per
        self.peak = max(getattr(self, "peak", 0), self.off)
        v = self.big[0:shape[0], o // 2:(o + per) // 2]
        if dt == F32:
            v = v.bitcast(F32)
        if len(shape) == 3:
            v = v.rearrange("p (a b) -> p a b", a=shape[1])
        elif len(shape) == 4:
            v = v.rearrange("p (a b c) -> p a b c", a=shape[1], b=shape[2])
        return v


class Ctx:
    pass


def rms_rstd(k, c, src, src_res, scr, scr_res, n_feat, tag):
    i = c.rs_i % 8
    c.rs_i += 1
    ssq, std, rstd = c.ssq[:, i:i + 1], c.std[:, i:i + 1], c.rstd[:, i:i + 1]
    R = c.rs_res[i]
    k.op("act", lambda e: e.activation(out=scr, in_=src, func=AF.Square, accum_out=ssq),
         reads=[src_res], writes=[scr_res, R])
    k.op("act", lambda e: e.activation(out=std, in_=ssq, func=AF.Sqrt, scale=1.0 / n_feat, bias=c.epsb[:, 0:1]),
         reads=[R, c.R_const], writes=[R])
    k.op("dve", lambda e: e.reciprocal(out=rstd, in_=std), reads=[R], writes=[R])
    return rstd, R


def phase_moe(k, c, ar, X1, R_X1, hfT, R_hfT):
    nc = c.nc
    Dr = c.D
    m0 = ar.mark()
    hf32 = [ar.al([128, D], F32) for _ in range(2)]
    R_hf32 = [k.res("hf32_%d" % i) for i in range(2)]
    scr = ar.al([128, D], F32)
    R_scr = k.res("moe_scr")
    hT32 = [ar.al([128, 8, 128], F32) for _ in range(2)]
    R_hT32 = [k.res("hT32_%d" % i) for i in range(2)]
    wr32 = ar.al([128, 8, 36], F32)
    R_wr = k.res("wr32")
    brt = ar.al([128, 36], F32)
    gft = ar.al([128, D], F32)
    R_gft = k.res("gft")
    comb = ar.al([128, NT, NE], F32)
    R_comb = k.res("comb")
    sm = ar.al([128, 128], F32)
    R_sm = k.res("moe_sm")
    k.dma("sp", wr32, Dr["w_r"].rearrange("(kt p) n -> p kt n", p=128), writes=[R_wr])
    k.dma("sp", brt, Dr["b_r"].partition_broadcast(128), writes=[R_wr])
    k.dma("sp", gft, Dr["g_ffn"].partition_broadcast(128), writes=[R_gft])
    pT = [c.pwide(0), c.pwide(2)]
    R_pT = [[c.R_ps[0], c.R_ps[1]], [c.R_ps[2], c.R_ps[3]]]
    pL = c.psum[4]
    R_pL = c.R_ps[4]
    for t in range(NT):
        b = t % 2
        xs = X1[:, t, :]
        rstd, R_r = rms_rstd(k, c, xs, R_X1[t], scr, R_scr, D, "moe")
        k.op("dve", lambda e, b=b, xs=xs, rstd=rstd: e.scalar_tensor_tensor(
            out=hf32[b], in0=xs, scalar=rstd, in1=gft, op0=ALU.mult, op1=ALU.mult),
            reads=[R_X1[t], R_r, R_gft], writes=[R_hf32[b]])
        p2 = pT[b]
        k.op("pe", [(lambda e, i=i, b=b, p2=p2: e.transpose(out=p2[:, i * 128:(i + 1) * 128],
                                                            in_=hf32[b][:, i * 128:(i + 1) * 128], identity=c.identf))
                    for i in range(8)], reads=[R_hf32[b], c.R_const], writes=R_pT[b])
        k.op("act", lambda e, b=b, p2=p2: e.activation(out=hT32[b].rearrange("p a b -> p (a b)"), in_=p2, func=AF.Copy),
             reads=R_pT[b], writes=[R_hT32[b]])
        k.op("dve", lambda e, b=b, p2=p2, t=t: e.tensor_copy(
            out=hfT[:, :, t * 128:(t + 1) * 128], in_=p2.rearrange("p (a b) -> p a b", a=8)),
            reads=R_pT[b], writes=[R_hfT[t]])
        lg = pL[:, 0:36]
        k.op("pe", [(lambda e, i=i, b=b: e.matmul(lg, lhsT=hT32[b][:, i, :], rhs=wr32[:, i, :], start=(i == 0), stop=(i == 7)))
                    for i in range(8)], reads=[R_hT32[b], R_wr], writes=[R_pL])
        lgs = sm[:, 0:36]
        gmax, gsum, gp, pen = sm[:, 36:37], sm[:, 37:38], sm[:, 38:39], sm[:, 40:44]
        gex, goh = sm[:, 44:48], sm[:, 48:52]
        elm = sm[:, 52:84]
        m8 = sm[:, 84:92]
        dd, ee, w1, w2 = sm[:, 92:93], sm[:, 93:94], sm[:, 94:95], sm[:, 95:96]
        oh = sm[:, 96:128]
        ct = comb[:, t, :]
        RW = dict(reads=[R_sm], writes=[R_sm])
        k.op("dve", lambda e: e.tensor_tensor(out=lgs, in0=lg, in1=brt, op=ALU.add), reads=[R_pL, R_wr, R_sm], writes=[R_sm])
        k.op("dve", lambda e: e.reduce_max(out=gmax, in_=lgs[:, 0:4], axis=AX.X), **RW)
        k.op("dve", lambda e: e.tensor_scalar(out=gex, in0=lgs[:, 0:4], scalar1=gmax, scalar2=None, op0=ALU.subtract), **RW)
        k.op("act", lambda e: e.activation(out=gex, in_=gex, func=AF.Exp, accum_out=gsum), **RW)
        k.op("dve", lambda e: e.reciprocal(out=gp, in_=gsum), **RW)
        k.op("dve", lambda e: e.tensor_scalar(out=pen, in0=lgs[:, 0:4], scalar1=gmax, scalar2=-1e30, op0=ALU.is_lt, op1=ALU.mult), **RW)
        k.op("dve", lambda e: e.tensor_tensor(out=elm.rearrange("p (g x) -> p g x", g=4),
                                              in0=lgs[:, 4:36].rearrange("p (g x) -> p g x", g=4),
                                              in1=pen.unsqueeze(2).broadcast_to([128, 4, 8]), op=ALU.add), **RW)
        k.op("dve", lambda e: e.max(out=m8, in_=elm), **RW)
        k.op("dve", lambda e: e.tensor_tensor(out=dd, in0=m8[:, 1:2], in1=m8[:, 0:1], op=ALU.subtract), **RW)
        k.op("act", lambda e: e.activation(out=ee, in_=dd, func=AF.Exp), **RW)
        k.op("dve", lambda e: e.tensor_scalar(out=ee, in0=ee, scalar1=1.0, scalar2=None, op0=ALU.add), **RW)
        k.op("dve", lambda e: e.reciprocal(out=w1, in_=ee), **RW)
        k.op("dve", lambda e: e.tensor_scalar(out=w2, in0=w1, scalar1=-1.0, scalar2=1.0, op0=ALU.mult, op1=ALU.add), **RW)
        k.op("dve", lambda e: e.tensor_tensor(out=w1, in0=w1, in1=gp, op=ALU.mult), **RW)
        k.op("dve", lambda e: e.tensor_tensor(out=w2, in0=w2, in1=gp, op=ALU.mult), **RW)
        k.op("dve", lambda e: e.tensor_scalar(out=oh, in0=elm, scalar1=m8[:, 0:1], scalar2=w1, op0=ALU.is_equal, op1=ALU.mult), **RW)
        k.op("dve", lambda e, ct=ct: e.tensor_scalar(out=ct, in0=elm, scalar1=m8[:, 1:2], scalar2=w2, op0=ALU.is_equal, op1=ALU.mult),
             reads=[R_sm], writes=[R_comb])
        k.op("dve", lambda e, ct=ct: e.tensor_tensor(out=ct, in0=ct, in1=oh, op=ALU.add), reads=[R_sm, R_comb], writes=[R_comb])

    if c.n_exp == 0:
        ar.release(m0)
        return
    NWB = 2
    wg = [ar.al([128, 8, DFF], BF16) for _ in range(NWB)]
    wu = [ar.al([128, 8, DFF], BF16) for _ in range(NWB)]
    wd = [ar.al([128, 4, D], BF16) for _ in range(NWB)]
    R_wg = [k.res("wg%d" % i) for i in range(NWB)]
    R_wu = [k.res("wu%d" % i) for i in range(NWB)]
    R_wd = [k.res("wd%d" % i) for i in range(NWB)]
    hid = [ar.al([128, 4, 512], BF16) for _ in range(2)]
    R_hid = [k.res("hid%d" % i) for i in range(2)]
    sg = [ar.al([128, 512], F32) for _ in range(2)]
    R_sg = [k.res("sg%d" % i) for i in range(2)]
    n_exp = c.n_exp

    def load_w(e):
        b = e % NWB
        k.dma("pool", wg[b], Dr["w_gate"][e].rearrange("(kt p) n -> p kt n", p=128), writes=[R_wg[b]])
        k.dma("pool", wu[b], Dr["w_up"][e].rearrange("(kt p) n -> p kt n", p=128), writes=[R_wu[b]])
        k.dma("pool", wd[b], Dr["w_down"][e].rearrange("(kt p) n -> p kt n", p=128), writes=[R_wd[b]])

    units = [(e, g) for e in range(n_exp) for g in range(NT // 4)]
    pgu = [(c.psum[0], c.psum[1]), (c.psum[2], c.psum[3])]
    R_pgu = [(c.R_ps[0], c.R_ps[1]), (c.R_ps[2], c.R_ps[3])]
    pdn = [c.psum[4], c.psum[5], c.psum[6], c.psum[7]]
    R_pdn = [c.R_ps[4], c.R_ps[5], c.R_ps[6], c.R_ps[7]]
    st = dict(gu=0, dn=0)

    def gate_up(u):
        e, g = units[u]
        b = e % NWB
        hb = u % 2
        tok = slice(g * 512, (g + 1) * 512)
        for ff in range(4):
            pb = st["gu"] % 2
            st["gu"] += 1
            pg, pu = pgu[pb]
            k.op("pe", [(lambda en, i=i, pg=pg, b=b, ff=ff: en.matmul(pg, lhsT=wg[b][:, i, ff * 128:(ff + 1) * 128], rhs=hfT[:, i, tok],
                                                                      start=(i == 0), stop=(i == 7))) for i in range(8)],
                 reads=[R_wg[b]] + R_hfT[4 * g:4 * g + 4], writes=[R_pgu[pb][0]])
            k.op("pe", [(lambda en, i=i, pu=pu, b=b, ff=ff: en.matmul(pu, lhsT=wu[b][:, i, ff * 128:(ff + 1) * 128], rhs=hfT[:, i, tok],
                                                                      start=(i == 0), stop=(i == 7))) for i in range(8)],
                 reads=[R_wu[b]] + R_hfT[4 * g:4 * g + 4], writes=[R_pgu[pb][1]])
            k.op("act", lambda en, pg=pg, pb=pb: en.activation(out=sg[pb], in_=pg, func=AF.Silu),
                 reads=[R_pgu[pb][0]], writes=[R_sg[pb]])
            k.op("dve", lambda en, pu=pu, pb=pb, hb=hb, ff=ff: en.tensor_tensor(out=hid[hb][:, ff, :], in0=pu, in1=sg[pb], op=ALU.mult),
                 reads=[R_pgu[pb][1], R_sg[pb]], writes=[R_hid[hb]])

    def down(u):
        e, g = units[u]
        b = e % NWB
        hb = u % 2
        for tt in range(4):
            t = 4 * g + tt
            for hf in range(2):
                pb = st["dn"] % 4
                st["dn"] += 1
                po = pdn[pb]
                k.op("pe", [(lambda en, i=i, po=po, b=b, hb=hb, tt=tt, hf=hf: en.matmul(
                    po, lhsT=hid[hb][:, i, tt * 128:(tt + 1) * 128], rhs=wd[b][:, i, hf * 512:(hf + 1) * 512],
                    start=(i == 0), stop=(i == 3))) for i in range(4)],
                    reads=[R_hid[hb], R_wd[b]], writes=[R_pdn[pb]])
                xs = X1[:, t, hf * 512:(hf + 1) * 512]
                k.op("dve", lambda en, po=po, xs=xs, t=t, e=e: en.scalar_tensor_tensor(
                    out=xs, in0=po, scalar=comb[:, t, e:e + 1], in1=xs, op0=ALU.mult, op1=ALU.add),
                    reads=[R_pdn[pb], R_comb, R_X1[t]], writes=[R_X1[t]])

    load_w(0)
    for u in range(len(units)):
        e, g = units[u]
        gate_up(u)
        if u >= 1:
            down(u - 1)
        if g == 0 and e + 1 < n_exp:
            load_w(e + 1)
    down(len(units) - 1)
    ar.release(m0)


def phase_final(k, c, ar, X1, R_X1):
    Dr = c.D
    m0 = ar.mark()
    gft = ar.al([128, D], F32)
    R_g = k.res("gfin")
    scr = ar.al([128, D], F32)
    R_scr = k.res("fin_scr")
    ob = [ar.al([128, D], F32) for _ in range(2)]
    R_ob = [k.res("ob%d" % i) for i in range(2)]
    k.dma("sp", gft, Dr["g_fin"].partition_broadcast(128), writes=[R_g])
    for t in range(NT):
        b = t % 2
        xs = X1[:, t, :]
        rstd, R_r = rms_rstd(k, c, xs, R_X1[t], scr, R_scr, D, "fin")
        k.op("dve", lambda e, b=b, xs=xs, rstd=rstd: e.scalar_tensor_tensor(
            out=ob[b], in0=xs, scalar=rstd, in1=gft, op0=ALU.mult, op1=ALU.mult),
            reads=[R_X1[t], R_r, R_g], writes=[R_ob[b]])
        k.dma("sp", Dr["y"][t * 128:(t + 1) * 128, :], ob[b], reads=[R_ob[b]])
    for b in range(2):
        for sk, v in list(R_ob[b].rs.items()):
            if sk.startswith("d_"):
                k.rec["sp"].append(("w", sk, v))
    ar.release(m0)


Q0, KV0, GT0, HQ0, HF0, HI0, HG0, MG0 = 0, 512, 1280, 1304, 1816, 2328, 2840, 3352


def _partner(d):
    return d + 8 if d < 8 else (d - 8 if d < 16 else d)


def _rope_tables(pos):
    pos = np.asarray(pos, dtype=np.float32)
    inv = (np.float32(500000.0) ** (-np.arange(8, dtype=np.float32) / np.float32(8))).astype(np.float32)
    ang = (pos[None, :] * inv[:, None]).astype(np.float32)
    cs, sn = np.cos(ang).astype(np.float32), np.sin(ang).astype(np.float32)
    C = np.ones((64, len(pos)), np.float32)
    S = np.zeros((64, len(pos)), np.float32)
    C[0:8], C[8:16] = cs, cs
    S[0:8], S[8:16] = -sn, sn
    return C, S


def attn_input_specs():
    return [
        ("g_attn", (D,), F32), ("g_hg4", (512,), F32),
        ("w1f", (D, 1280), F32), ("w1t", (D, 768), F32),
        ("w2f", (D, 2048), F32), ("w2t", (D, 1536), F32),
        ("wck", (64, 2048), F32), ("wckp", (64, 2048), F32), ("wcv", (64, 2048), F32),
        ("posT", (128, 32), F32), ("lbl", (2, 512), F32),
        ("w_mg", (D, 2048), F32), ("w_brn", (512, D), F32), ("w_brh", (512, D), F32), ("w_out", (D, D), F32),
        ("CK", (128, SEQ), F32), ("SK", (128, SEQ), F32), ("CKc", (128, 512), F32), ("SKc", (128, 512), F32),
        ("CQ", (128, NTOK), F32), ("SQ", (128, NTOK), F32),
        ("ovl", (128, 4, 128), F32),
        ("CB", (128, NT, 128), F32), ("CM", (128, 4, 128), F32), ("WMT", (128, 8, 128), F32),
        ("VAL", (128, NT, 128), F32), ("ADDC", (128, NT, 128), F32),
        ("tri", (128, 128), F32), ("I4", (128, 512), F32), ("onehot", (128, 4), F32),
    ]


_TAB_CACHE = {}


def _const_tables(cp):
    if cp in _TAB_CACHE:
        return _TAB_CACHE[cp]
    m = {}
    C, S = _rope_tables(np.arange(SEQ))
    m["CK"], m["SK"] = np.concatenate([C, C], 0), np.concatenate([S, S], 0)
    C, S = _rope_tables(np.maximum(16 * (np.arange(512) - 1), 0))
    m["CKc"], m["SKc"] = np.concatenate([C, C], 0), np.concatenate([S, S], 0)
    tpos = (128 * (4 * np.arange(NT)[:, None] + cp) + np.arange(128)[None, :])
    C, S = _rope_tables(tpos.reshape(-1))
    m["CQ"] = np.concatenate([C, C], 0) * np.float32(0.125)
    m["SQ"] = np.concatenate([S, S], 0) * np.float32(0.125)
    n = np.arange(512) - 1
    cs, ce = 16 * n, 16 * n + 31
    ss = 64 * np.arange(128)
    ov = ((cs[:, None] < ss[None, :] + 64) & (ce[:, None] >= ss[None, :]) & (n[:, None] >= 0)).astype(np.float32)
    m["ovl"] = np.ascontiguousarray(ov.reshape(4, 128, 128).transpose(1, 0, 2))
    mt = (np.arange(NT) // 4)
    mm = mt[:, None] * 128 + np.arange(128)[None, :]
    nn = mm - 1
    okc = (nn[:, None, :] >= 0) & (16 * nn[:, None, :] + 31 <= tpos[:, :, None])
    m["CB"] = np.ascontiguousarray(np.where(okc, 0.0, NEG).astype(np.float32).transpose(1, 0, 2))
    blk = np.arange(128)
    jq = tpos // 64
    force = (blk[None, None, :] == jq[:, :, None]) | (blk[None, None, :] == 0)
    valid = (64 * blk[None, None, :] <= tpos[:, :, None])
    m["VAL"] = np.ascontiguousarray((valid & ~force).astype(np.float32).transpose(1, 0, 2))
    m["ADDC"] = np.ascontiguousarray(np.where(force, 1e4, np.where(valid, 0.0, -1.0)).astype(np.float32).transpose(1, 0, 2))
    t = np.arange(128)[:, None]
    p = np.arange(128)[None, :]
    caus = np.where(p <= t, 0.0, NEG).astype(np.float32)
    anti = np.where(p > t, 0.0, NEG).astype(np.float32)
    cm = np.zeros((128, 4, 128), np.float32)
    for r in range(4):
        cm[:, r, :] = 0.0 if r < cp else (caus if r == cp else NEG)
    m["CM"] = cm
    wm = np.zeros((128, 8, 128), np.float32)
    for r in range(8):
        dk = cp + 4 - r
        wm[:, r, :] = NEG if (dk < 0 or dk > 4) else (caus if dk == 0 else (anti if dk == 4 else 0.0))
    m["WMT"] = wm
    m["tri"] = (np.arange(128)[:, None] <= np.arange(128)[None, :]).astype(np.float32)
    m["I4"] = np.tile(np.eye(128, dtype=np.float32), (1, 4))
    oh = np.zeros((128, 4), np.float32)
    oh[:, cp] = 1.0
    m["onehot"] = oh
    _TAB_CACHE[cp] = m
    return m


def attn_host_inputs(inp, b, cp):
    m = dict(_const_tables(cp))
    w = inp["w_in"][0]
    pp = np.array([g * 64 + _partner(d) for g in range(2) for d in range(64)])
    kv = lambda s: KV0 + s * 128 + np.arange(128)
    hfc = HF0 + np.arange(512)
    m["w1f"] = np.ascontiguousarray(np.concatenate(
        [w[:, kv(0)], w[:, kv(1)], w[:, kv(2)], w[:, kv(2)[pp]], w[:, kv(4)], w[:, kv(4)[pp]], w[:, hfc]], axis=1))
    m["w1t"] = np.ascontiguousarray(np.concatenate([w[:, kv(3)], w[:, kv(5)], w[:, HI0:HI0 + 512]], axis=1))
    qcols, qpcols = [], []
    for a in range(4):
        for h in (a, 4 + a):
            qcols += [Q0 + h * 64 + d for d in range(64)]
            qpcols += [Q0 + h * 64 + _partner(d) for d in range(64)]
    m["w2f"] = np.ascontiguousarray(np.concatenate(
        [w[:, qcols], w[:, qpcols], w[:, HQ0:HQ0 + 512], w[:, hfc]], axis=1))
    gpad = np.concatenate([w[:, GT0:GT0 + 24], w[:, GT0:GT0 + 24][:, :0].repeat(1, 1)], axis=1)
    w2t = np.zeros((D, 1536), np.float32)
    w2t[:, 0:512] = w[:, HI0:HI0 + 512]
    w2t[:, 512:1024] = w[:, HG0:HG0 + 512]
    w2t[:, 1024:1048] = w[:, GT0:GT0 + 24]
    m["w2t"] = w2t
    pc = np.array([_partner(d) for d in range(64)])
    dle = lambda w_: np.ascontiguousarray(w_.reshape(32, 64, 64).transpose(1, 0, 2).reshape(64, 2048))
    m["wck"] = dle(inp["w_cmp_k"][0])
    m["wckp"] = dle(inp["w_cmp_k"][0][:, pc])
    m["wcv"] = dle(inp["w_cmp_v"][0])
    pT = np.ascontiguousarray(inp["cmp_pos"][0].T)
    m["posT"] = np.concatenate([pT, pT], 0)
    m["lbl"] = np.ascontiguousarray(inp["hg_lb_logits"])
    m["g_attn"] = np.ascontiguousarray(inp["attn_norm"][0])
    m["g_hg4"] = np.ascontiguousarray(np.tile(inp["hg_norm"][0], 4))
    m["w_mg"] = np.ascontiguousarray(w[:, MG0:MG0 + 2048])
    m["w_brn"] = np.ascontiguousarray(inp["w_br_nsa"][0])
    m["w_brh"] = np.ascontiguousarray(inp["w_br_hg"][0])
    m["w_out"] = np.ascontiguousarray(inp["w_out"][0])
    return m


def norm_transpose_group(k, c, W, src_dram, row0, hT, R_hT):
    def s1(tt):
        b = tt % 2
        k.dma("sp", W.xt[b], src_dram[row0 + tt * 128: row0 + (tt + 1) * 128, :], writes=[W.R_xt[b]])
        rstd, R_r = rms_rstd(k, c, W.xt[b], W.R_xt[b], W.scr, W.R_scr, D, "an")
        k.op("dve", lambda e: e.scalar_tensor_tensor(
            out=W.hb[b], in0=W.xt[b], scalar=rstd, in1=W.gA, op0=ALU.mult, op1=ALU.mult),
            reads=[W.R_xt[b], R_r, W.R_gA], writes=[W.R_hb[b]])
        pb = c.psum[b].bitcast(BF16)
        k.op("pe", [(lambda e, i=i: e.transpose(out=pb[:, i * 128:(i + 1) * 128],
                                                in_=W.hb[b][:, i * 128:(i + 1) * 128], identity=c.identb))
                    for i in range(8)], reads=[W.R_hb[b], c.R_const], writes=[c.R_ps[b]])

    def s2(tt):
        b = tt % 2
        pb = c.psum[b].bitcast(BF16)
        k.op("act", lambda e: e.activation(out=hT[:, :, tt * 128:(tt + 1) * 128],
                                           in_=pb.rearrange("p (a b) -> p a b", a=8), func=AF.Copy),
             reads=[c.R_ps[b]], writes=[R_hT])
    s1(0)
    s1(1)
    s2(0)
    s1(2)
    s2(1)
    s1(3)
    s2(2)
    s2(3)


def f_front(k, c, W, fl_ps, R_fl, hd):
    u, a, bq, lk, L, RF = W.sets[hd % 2]
    k.op("act", lambda e: e.activation(out=u, in_=fl_ps, func=AF.Exp, scale=-1.0), reads=[R_fl], writes=[RF])
    k.op("act", lambda e: e.activation(out=a, in_=u, func=AF.Ln, scale=c.lbv[:, hd:hd + 1], bias=c.one_col[:, 0:1]),
         reads=[RF, c.R_const], writes=[RF])
    k.op("act", lambda e: e.activation(out=bq, in_=u, func=AF.Ln, bias=c.one_col[:, 0:1]), reads=[RF, c.R_const], writes=[RF])
    k.op("dve", lambda e: e.scalar_tensor_tensor(out=lk, in0=fl_ps, scalar=-1.0, in1=bq, op0=ALU.mult, op1=ALU.subtract),
         reads=[R_fl, RF], writes=[RF])
    for tt in range(4):
        sl = slice(tt * 128, (tt + 1) * 128)
        k.op("dve", lambda e, sl=sl: e.tensor_tensor_scan(out=L[:, sl], data0=a[:, sl], data1=bq[:, sl], initial=0.0,
                                                          op0=ALU.add, op1=ALU.subtract), reads=[RF], writes=[RF])
    k.op("pool", lambda e: e.tensor_tensor(out=lk, in0=lk, in1=L, op=ALU.subtract), reads=[RF], writes=[RF])


def f_back(k, c, W, hd, H=None):
    u, a, bq, lk, L, RF = W.sets[hd % 2]
    W_, W = W, (H if H is not None else W)
    Lr = L.rearrange("p (t x) -> p t x", t=4)
    rcol, ecol = Lr[:, :, 63], Lr[:, :, 127]
    k.op("dve", lambda e: e.tensor_scalar(out=W.rb[:, hd, :], in0=rcol, scalar1=c.l1mlb[:, hd:hd + 1], scalar2=None, op0=ALU.add),
         reads=[RF, c.R_const], writes=[W.R_cols])
    k.op("dve", lambda e: e.tensor_scalar(out=W.negr[:, hd, :], in0=rcol, scalar1=-1.0, scalar2=None, op0=ALU.mult),
         reads=[RF], writes=[W.R_cols])
    k.op("dve", lambda e: e.tensor_tensor(out=W.dl[:, hd, :], in0=ecol, in1=rcol, op=ALU.subtract), reads=[RF], writes=[W.R_cols])
    k.op("act", lambda e: e.activation(out=W.c1[:, hd, :], in_=ecol, func=AF.Exp), reads=[RF], writes=[W.R_cols])
    k.op("act", lambda e: e.activation(out=W.c2[:, hd, :], in_=W.dl[:, hd, :], func=AF.Exp), reads=[W.R_cols], writes=[W.R_cols])
    k.op("act", lambda e: e.activation(out=W.er[:, hd, :], in_=rcol, func=AF.Exp), reads=[RF], writes=[W.R_cols])
    for tt in range(4):
        sl = slice(tt * 128, (tt + 1) * 128)
        k.op("act", lambda e, sl=sl, tt=tt: e.activation(out=W.kT[:, hd, sl], in_=lk[:, sl], func=AF.Exp, bias=W.rb[:, hd, tt:tt + 1]),
             reads=[RF, W.R_cols], writes=[W.R_kT])


def setup_lb(k, c, ar):
    Dr = c.D
    c.lbv = ar.al([128, 4], F32)
    c.l1mlb = ar.al([128, 4], F32)
    c.one_col = ar.al([128, 1], F32)
    c.ones128 = ar.al([128, 128], F32)
    l0 = ar.al([128, 4], F32)
    l1 = ar.al([128, 4], F32)
    R = c.R_const
    k.dma("sp", l0, Dr["lbl"][0].rearrange("(h p) -> p h", p=128), writes=[R], allow_slow_non_contiguous=True)
    k.dma("sp", l1, Dr["lbl"][1].rearrange("(h p) -> p h", p=128), writes=[R], allow_slow_non_contiguous=True)
    k.op("dve", lambda e: e.memset(c.one_col, 1.0), writes=[R])
    k.op("dve", lambda e: e.memset(c.ones128, 1.0), writes=[R])
    k.op("dve", lambda e: e.tensor_tensor(out=l1, in0=l1, in1=l0, op=ALU.subtract), reads=[R], writes=[R])
    k.op("act", lambda e: e.activation(out=l0, in_=l1, func=AF.Exp), reads=[R], writes=[R])
    k.op("dve", lambda e: e.tensor_scalar(out=l0, in0=l0, scalar1=1.0, scalar2=None, op0=ALU.add), reads=[R], writes=[R])
    k.op("dve", lambda e: e.reciprocal(out=c.lbv, in_=l0), reads=[R], writes=[R])
    k.op("act", lambda e: e.activation(out=l0, in_=l0, func=AF.Ln), reads=[R], writes=[R])
    k.op("dve", lambda e: e.tensor_tensor(out=c.l1mlb, in0=l1, in1=l0, op=ALU.subtract), reads=[R], writes=[R])


class WS:
    pass


def alloc_hg_ws(k, ar, W, nsets=1):
    W.sets = []
    for si in range(nsets):
        blk = ar.al([128, 5, 512], F32)
        W.sets.append(tuple(blk[:, i, :] for i in range(5)) + (k.res("fchain%d" % si),))
        if si == 0:
            W.ab = blk[:, 1:3, :].rearrange("p a b -> p (a b)")
    if nsets == 1:
        W.sets.append(W.sets[0])
    W.u, W.a, W.bq, W.lk, W.L, W.R_f = W.sets[0]
    alloc_hslot(k, ar, W, "0")


def alloc_hslot(k, ar, H, tag):
    H.rb, H.negr, H.dl, H.c1, H.c2, H.er = [ar.al([128, 4, 4], F32) for _ in range(6)]
    H.R_cols = k.res("fcols" + tag)
    H.kT = ar.al([128, 4, 512], BF16)
    H.R_kT = k.res("kT" + tag)


def alloc_x_ws(k, c, ar, W, region, scr=None, R_scr=None):
    if region is not None:
        W.xt = [region[:, 0, :].bitcast(F32), region[:, 1, :].bitcast(F32)]
        W.hb = [region[:, 2, 0:1024], region[:, 2, 1024:2048]]
        W.scr = region[:, 3, :].bitcast(F32)
        W.R_scr = k.res("xscr")
    else:
        W.xt = [ar.al([128, D], F32) for _ in range(2)]
        W.hb = [ar.al([128, D], BF16) for _ in range(2)]
        W.scr, W.R_scr = scr, R_scr
    W.R_xt = [k.res("xt0"), k.res("xt1")]
    W.R_hb = [k.res("hb0"), k.res("hb1")]
    W.gA = ar.al([128, D], F32)
    W.R_gA = k.res("gA")
    k.dma("sp", W.gA, c.D["g_attn"].partition_broadcast(128), writes=[W.R_gA])


def phase_p1(k, c, ar, St):
    Dr = c.D
    m0 = ar.mark()
    W = WS()
    alloc_x_ws(k, c, ar, W, c.oT_hg)
    w1f, w1t = c.R32[:, :, 0:1280], c.R32[:, :, 1280:2048]
    R_w1 = k.res("w1")
    k.dma("pool", w1f, Dr["w1f"].rearrange("(kt p) n -> p kt n", p=128), writes=[R_w1])
    k.dma("pool", w1t, Dr["w1t"].rearrange("(kt p) n -> p kt n", p=128), writes=[R_w1])
    hT = ar.al([128, 8, 512], BF16)
    R_hT = k.res("hT")
    CKg, SKg = ar.al([128, 512], F32), ar.al([128, 512], F32)
    R_rt = k.res("ropetab")
    alloc_hg_ws(k, ar, W, nsets=2)
    t1, t2, R_t12 = W.u, W.a, W.R_f
    vtok = ar.al([128, 4, 512], BF16)
    R_vtok = k.res("vtok")
    ktok = ar.al([128, 4, 128], BF16)
    R_ktok = k.res("ktok")
    Sst = ar.al([128, 4, 128], F32)
    snapacc = ar.al([128, 4, 128], F32)
    R_S, R_snapacc = k.res("S"), k.res("snapacc")
    WC = [ar.al([128, 32, 64], BF16) for _ in range(3)]
    R_WC = k.res("WC")
    posT = ar.al([128, 32], BF16)
    cb = ar.al([128, 4], F32)
    xin = [[ar.al([128, 528], BF16) for _ in range(2)] for _ in range(2)]
    R_xin = [[k.res("xin%d%d" % (a, b)) for b in range(2)] for a in range(2)]
    CKc, SKc = ar.al([128, 32], F32), ar.al([128, 32], F32)
    R_ckc = k.res("ckc")
    VCf = ar.al([128, 512], F32)
    R_VCf = k.res("VCf")
    ctmp = ar.al([128, 4, 32], F32)
    R_ctmp = k.res("ctmp")
    for xi, nm in enumerate(("wck", "wckp", "wcv")):
        for g in range(2):
            k.dma("pool", WC[xi][64 * g:64 * g + 64].rearrange("p l e -> p (l e)"), Dr[nm], writes=[R_WC])
    k.dma("pool", posT, Dr["posT"], writes=[R_WC])
    k.op("dve", lambda e: e.memset(Sst, 0.0), writes=[R_S])
    k.op("dve", lambda e: e.memset(St.VsA[:, :, :, 64:65], 1.0), writes=[St.R_VsA])
    k.op("dve", lambda e: e.memset(St.VwA[:, :, :, 64:65], 1.0), writes=[St.R_VwA])
    for a in range(2):
        k.op("dve", lambda e, a=a: e.memset(xin[a][0][:, 0:16], 0.0), writes=[R_xin[a][0]])
    p6 = c.psum[6]
    fns = []
    for xi in range(3):
        for g in range(2):
            for l in range(32):
                fns.append(lambda e, xi=xi, g=g, l=l: e.matmul(p6[64 * g:64 * g + 64, xi:xi + 1], lhsT=WC[xi][64 * g:64 * g + 64, l, :],
                                                               rhs=posT[64 * g:64 * g + 64, l:l + 1], start=(l == 0), stop=(l == 31)))
    k.op("pe", fns, reads=[R_WC], writes=[c.R_ps[6]])
    k.op("dve", lambda e: e.tensor_copy(out=cb[:, 0:3], in_=p6[:, 0:3]), reads=[c.R_ps[6]], writes=[R_WC])

    NG = c.n_groups
    Hs = [W, W]
    vtoks, R_vtoks = [vtok, vtok], [R_vtok, R_vtok]
    p6b = c.psum[6].bitcast(BF16)

    def fm(ft, bank):
        k.op("pe", [(lambda e, i=i: e.matmul(c.psum[bank], lhsT=w1f[:, i, ft * 128:(ft + 1) * 128], rhs=hT[:, i, :],
                                             start=(i == 0), stop=(i == 7))) for i in range(8)],
             reads=[R_w1, R_hT], writes=[c.R_ps[bank]])

    def A_x(G):
        norm_transpose_group(k, c, W, Dr["xb"], G * 512, hT, R_hT)
        k.dma("sp", CKg, Dr["CK"][:, G * 512:(G + 1) * 512], writes=[R_rt])
        k.dma("sp", SKg, Dr["SK"][:, G * 512:(G + 1) * 512], writes=[R_rt])

    def A_kv(G):
        xb_ = G % 2
        for a in range(2):
            fm(a, 2 + a)
            k.op("act", lambda e, a=a: e.activation(out=xin[a][xb_][:, 16:528], in_=c.psum[2 + a], func=AF.Copy),
                 reads=[c.R_ps[2 + a]], writes=[R_xin[a][xb_]])
            k.op("pool", lambda e, a=a: e.tensor_copy(out=xin[a][1 - xb_][:, 0:16], in_=xin[a][xb_][:, 512:528]),
                 reads=[R_xin[a][xb_]], writes=[R_xin[a][1 - xb_]])
        for which, dst, R_dst in ((0, St.KTs, St.R_KTs), (1, St.KTw, St.R_KTw)):
            fm(2 + 2 * which, 2)
            fm(3 + 2 * which, 3)
            k.op("dve", lambda e: e.tensor_tensor(out=t1, in0=c.psum[2], in1=CKg, op=ALU.mult), reads=[c.R_ps[2], R_rt], writes=[R_t12])
            k.op("dve", lambda e: e.tensor_tensor(out=t2, in0=c.psum[3], in1=SKg, op=ALU.mult), reads=[c.R_ps[3], R_rt, R_t12], writes=[R_t12])
            k.op("pool", lambda e, dst=dst: e.tensor_tensor(out=dst[:, G * 512:(G + 1) * 512], in0=t1, in1=t2, op=ALU.add),
                 reads=[R_t12], writes=[R_dst])

    def A_tok(G):
        vt, R_vt = vtoks[G % 2], R_vtoks[G % 2]
        for tt in range(4):
            tile_ = 4 * G + tt
            k.op("pe", [(lambda e, i=i, tt=tt: e.matmul(c.psum[4][:, 0:256], lhsT=hT[:, i, tt * 128:(tt + 1) * 128], rhs=w1t[:, i, 0:256],
                                                        start=(i == 0), stop=(i == 7))) for i in range(8)],
                 reads=[R_w1, R_hT], writes=[c.R_ps[4]])
            k.op("pe", [(lambda e, i=i, tt=tt: e.matmul(c.psum[5], lhsT=hT[:, i, tt * 128:(tt + 1) * 128], rhs=w1t[:, i, 256:768],
                                                        start=(i == 0), stop=(i == 7))) for i in range(8)],
                 reads=[R_w1, R_hT], writes=[c.R_ps[5]])
            k.op("act", lambda e, tile_=tile_: e.activation(out=St.VsA[:, tile_, :, 0:64],
                                                            in_=c.psum[4][:, 0:128].rearrange("p (g d) -> p g d", g=2), func=AF.Copy),
                 reads=[c.R_ps[4]], writes=[St.R_VsA])
            k.op("act", lambda e, tile_=tile_: e.activation(out=St.VwA[:, tile_, :, 0:64],
                                                            in_=c.psum[4][:, 128:256].rearrange("p (g d) -> p g d", g=2), func=AF.Copy),
                 reads=[c.R_ps[4]], writes=[St.R_VwA])
            k.op("dve", lambda e, tt=tt: e.tensor_copy(out=vt[:, tt, :], in_=c.psum[5]), reads=[c.R_ps[5]], writes=[R_vt])

    def A_conv(G):
        xb_ = G % 2
        fns = []
        for xi in range(3):
            src = xin[0][xb_] if xi < 2 else xin[1][xb_]
            for g in range(2):
                for l in range(32):
                    fns.append(lambda e, xi=xi, g=g, l=l, src=src: e.matmul(
                        p6[64 * g:64 * g + 64, 32 * xi:32 * xi + 32], lhsT=WC[xi][64 * g:64 * g + 64, l, :],
                        rhs=src[64 * g:64 * g + 64, l:l + 497:16], start=(l == 0), stop=(l == 31)))
        k.op("pe", fns, reads=[R_WC, R_xin[0][xb_], R_xin[1][xb_]], writes=[c.R_ps[6]])
        ms = slice(32 * G, 32 * G + 32)
        k.dma("sp", CKc, Dr["CKc"][:, ms], writes=[R_ckc])
        k.dma("sp", SKc, Dr["SKc"][:, ms], writes=[R_ckc])
        k.op("dve", lambda e: e.tensor_scalar(out=ctmp[:, 0, :], in0=p6[:, 0:32], scalar1=cb[:, 0:1], scalar2=None, op0=ALU.add),
             reads=[c.R_ps[6], R_WC], writes=[R_ctmp])
        k.op("dve", lambda e: e.tensor_scalar(out=ctmp[:, 1, :], in0=p6[:, 32:64], scalar1=cb[:, 1:2], scalar2=None, op0=ALU.add),
             reads=[c.R_ps[6], R_WC], writes=[R_ctmp])
        k.op("dve", lambda e: e.tensor_scalar(out=VCf[:, ms], in0=p6[:, 64:96], scalar1=cb[:, 2:3], scalar2=None, op0=ALU.add),
             reads=[c.R_ps[6], R_WC], writes=[R_VCf])
        k.op("pool", lambda e: e.tensor_tensor(out=ctmp[:, 0, :], in0=ctmp[:, 0, :], in1=CKc, op=ALU.mult),
             reads=[R_ctmp, R_ckc], writes=[R_ctmp])
        k.op("pool", lambda e: e.tensor_tensor(out=ctmp[:, 1, :], in0=ctmp[:, 1, :], in1=SKc, op=ALU.mult),
             reads=[R_ctmp, R_ckc], writes=[R_ctmp])
        k.op("pool", lambda e: e.tensor_tensor(out=St.KC[:, ms], in0=ctmp[:, 0, :], in1=ctmp[:, 1, :], op=ALU.add),
             reads=[R_ctmp], writes=[St.R_KC])

    def A_f(G):
        H = Hs[G % 2]

        def front(hd):
            bank = 2 + hd % 2
            fm(6 + hd, bank)
            f_front(k, c, W, c.psum[bank], c.R_ps[bank], hd)
        front(0)
        front(1)
        f_back(k, c, W, 0, H)
        front(2)
        f_back(k, c, W, 1, H)
        front(3)
        f_back(k, c, W, 2, H)
        f_back(k, c, W, 3, H)

    def B_step(G, tt):
        H = Hs[G % 2]
        vt, R_vt = vtoks[G % 2], R_vtoks[G % 2]
        sl = slice(tt * 128, (tt + 1) * 128)
        k.op("pe", [(lambda e, hd=hd: e.transpose(out=p6b[:, hd * 128:(hd + 1) * 128], in_=H.kT[:, hd, sl], identity=c.identb))
                    for hd in range(4)], reads=[H.R_kT, c.R_const], writes=[c.R_ps[6]])
        k.op("act", lambda e: e.activation(out=ktok, in_=p6b[:, 0:512].rearrange("p (h x) -> p h x", h=4), func=AF.Copy),
             reads=[c.R_ps[6]], writes=[R_ktok])
        k.op("pe", [(lambda e, hd=hd: e.matmul(c.psum[7][:, hd * 128:(hd + 1) * 128], lhsT=ktok[:, hd, :],
                                               rhs=vt[:, tt, hd * 128:(hd + 1) * 128], start=True, stop=True))
                    for hd in range(4)], reads=[R_ktok, R_vt], writes=[c.R_ps[7]])
        Sf, Af = Sst.rearrange("p h x -> p (h x)"), snapacc.rearrange("p h x -> p (h x)")
        if tt == 0:
            k.op("dve", lambda e: e.tensor_scalar(out=Af, in0=Sf, scalar1=c.onehot[:, 0:1], scalar2=None, op0=ALU.mult),
                 reads=[R_S, c.R_const], writes=[R_snapacc])
        else:
            k.op("dve", lambda e: e.scalar_tensor_tensor(out=Af, in0=Sf, scalar=c.onehot[:, tt:tt + 1], in1=Af,
                                                         op0=ALU.mult, op1=ALU.add),
                 reads=[R_S, c.R_const, R_snapacc], writes=[R_snapacc])
        for hd in range(4):
            k.op("dve", lambda e, hd=hd: e.tensor_scalar(out=Sst[:, hd, :], in0=Sst[:, hd, :], scalar1=H.c1[:, hd, tt:tt + 1],
                                                         scalar2=None, op0=ALU.mult),
                 reads=[R_S, H.R_cols], writes=[R_S])
            k.op("dve", lambda e, hd=hd: e.scalar_tensor_tensor(
                out=Sst[:, hd, :], in0=c.psum[7][:, hd * 128:(hd + 1) * 128], scalar=H.c2[:, hd, tt:tt + 1], in1=Sst[:, hd, :],
                op0=ALU.mult, op1=ALU.add), reads=[c.R_ps[7], R_S, H.R_cols], writes=[R_S])
        if tt == 3:
            k.op("act", lambda e: e.activation(out=St.SNAP[:, G, :, :], in_=snapacc, func=AF.Copy), reads=[R_snapacc], writes=[St.R_SNAP])

    for G in range(NG + 1):
        if G < NG:
            A_x(G)
        if G >= 1:
            B_step(G - 1, 0)
            B_step(G - 1, 1)
        if G < NG:
            A_kv(G)
        if G >= 1:
            B_step(G - 1, 2)
            B_step(G - 1, 3)
        if G < NG:
            A_tok(G)
            A_conv(G)
            A_f(G)
    k.op("dve", lambda e: e.memset(St.VCA[:, :, :, 64:65], 1.0), writes=[St.R_VCA])
    for g in range(2):
        k.dma("pool", St.VCA[:, :, g, 65:193], Dr["ovl"], writes=[St.R_VCA])
    pw = c.psum[6]
    k.op("pe", [(lambda e, mt=mt: e.transpose(out=pw[:, mt * 128:(mt + 1) * 128], in_=VCf[:, mt * 128:(mt + 1) * 128], identity=c.identf))
                for mt in range(4)], reads=[R_VCf, c.R_const], writes=[c.R_ps[6]])
    for mt in range(4):
        k.op("act", lambda e, mt=mt: e.activation(out=St.VCA[:, mt, :, 0:64],
                                                  in_=pw[:, mt * 128:(mt + 1) * 128].rearrange("p (g d) -> p g d", g=2), func=AF.Copy),
             reads=[c.R_ps[6]], writes=[St.R_VCA])
    k.op("dve", lambda e: e.memset(St.VCA[0:1, 0, :, :], 0.0), writes=[St.R_VCA])
    ar.release(m0)


def phase_p2pre(k, c, ar, St):
    Dr = c.D
    m0 = ar.mark()
    W = WS()
    alloc_hg_ws(k, ar, W, nsets=2)
    alloc_x_ws(k, c, ar, W, None, scr=W.ab, R_scr=W.R_f)
    hT = ar.al([128, 8, 512], BF16)
    R_hT = k.res("hT2")
    wch = [c.R32f[:, 8192 + b * 4096: 8192 + (b + 1) * 4096].rearrange("p (a b) -> p a b", a=8) for b in range(2)]
    R_wch = [k.res("wch%d" % i) for i in range(2)]
    wgt = ar.al([128, 8, 32], BF16)
    R_wgt = k.res("wgt")
    wst = dict(n=0)
    CQg, SQg = ar.al([128, 512], F32), ar.al([128, 512], F32)
    R_rt = k.res("ropetabq")
    t1, t2, R_t12 = W.u, W.a, W.R_f
    qT = ar.al([128, 4, 512], BF16)
    R_qT = k.res("qTh")
    e1, R_e1 = W.u, W.R_f
    vtok = ar.al([128, 512], BF16)
    R_vtok = k.res("vtok2")
    sgt = ar.al([128, 512], F32)
    R_sgt = k.res("sgt")
    AT = ar.al([128, 4, 128], BF16)
    R_AT = k.res("AT")
    Sp = ar.al([128, 4, 128], BF16)
    R_Sp = k.res("Sp")
    gnt = ar.al([128, 512], F32)
    R_gnt = k.res("gnt")
    o1, o2, R_o = W.bq, W.a, W.R_f
    yb = ar.al([128, 512], BF16)
    R_yb = k.res("yb")
    hs = ar.al([128, 16], F32)
    R_hs = k.res("hs")
    k.dma("pool", wgt, Dr["w2t"][:, 1024:1056].rearrange("(kt p) n -> p kt n", p=128), writes=[R_wgt])
    k.dma("sp", gnt, Dr["g_hg4"].partition_broadcast(128), writes=[R_gnt])

    def wload(src, c0, n=512):
        b = wst["n"] % 2
        wst["n"] += 1
        k.dma("pool", wch[b][:, :, 0:n], src[:, c0:c0 + n].rearrange("(kt p) n -> p kt n", p=128), writes=[R_wch[b]])
        return wch[b], R_wch[b]

    for go in range(NT // 4):
        tok = slice(go * 512, (go + 1) * 512)
        norm_transpose_group(k, c, W, Dr["xo"], go * 512, hT, R_hT)
        k.dma("sp", CQg, Dr["CQ"][:, tok], writes=[R_rt])
        k.dma("sp", SQg, Dr["SQ"][:, tok], writes=[R_rt])

        def fm(wt, R_wt, j, bank):
            k.op("pe", [(lambda e, i=i: e.matmul(c.psum[bank], lhsT=wt[:, i, j * 128:(j + 1) * 128], rhs=hT[:, i, :],
                                                 start=(i == 0), stop=(i == 7))) for i in range(8)],
                 reads=[R_wt, R_hT], writes=[c.R_ps[bank]])
        wq, R_wq = wload(Dr["w2f"], 0)
        wqp, R_wqp = wload(Dr["w2f"], 512)
        for a in range(4):
            fm(wq, R_wq, a, 2)
            fm(wqp, R_wqp, a, 3)
            k.op("dve", lambda e: e.tensor_tensor(out=t1, in0=c.psum[2], in1=CQg, op=ALU.mult), reads=[c.R_ps[2], R_rt], writes=[R_t12])
            k.op("dve", lambda e: e.tensor_tensor(out=t2, in0=c.psum[3], in1=SQg, op=ALU.mult), reads=[c.R_ps[3], R_rt, R_t12], writes=[R_t12])
            k.op("pool", lambda e, a=a: e.tensor_tensor(out=c.QT[:, 4 * go:4 * go + 4, a, :], in0=t1.rearrange("p (i t) -> p i t", i=4),
                                                        in1=t2.rearrange("p (i t) -> p i t", i=4), op=ALU.add), reads=[R_t12], writes=[c.R_QT])
        whq, R_whq = wload(Dr["w2f"], 1024)
        whf, R_whf = wload(Dr["w2f"], 1536)
        def front(hd):
            bank = 2 + hd % 2
            fm(whf, R_whf, hd, bank)
            f_front(k, c, W, c.psum[bank], c.R_ps[bank], hd)

        def back(hd):
            f_back(k, c, W, hd)
            su, sa, sbq, slk, sL, sRF = W.sets[hd % 2]
            fm(whq, R_whq, hd, 6)
            for tt in range(4):
                sl = slice(tt * 128, (tt + 1) * 128)
                k.op("act", lambda e, sl=sl, tt=tt: e.activation(out=su[:, sl], in_=sL[:, sl], func=AF.Exp, bias=W.negr[:, hd, tt:tt + 1]),
                     reads=[sRF, W.R_cols], writes=[sRF])
            k.op("dve", lambda e: e.tensor_tensor(out=qT[:, hd, :], in0=c.psum[6], in1=su, op=ALU.mult),
                 reads=[c.R_ps[6], sRF], writes=[R_qT])
        front(0)
        front(1)
        back(0)
        front(2)
        back(1)
        front(3)
        back(2)
        back(3)
        whi, R_whi = wload(Dr["w2t"], 0)
        whg, R_whg = wload(Dr["w2t"], 512)
        for tt in range(4):
            i_own = 4 * go + tt
            sl = slice(tt * 128, (tt + 1) * 128)
            for (wt, R_wt, n, bank) in ((whi, R_whi, 512, 4), (whg, R_whg, 512, 5), (wgt, R_wgt, 32, 6)):
                k.op("pe", [(lambda e, i=i, wt=wt, n=n, bank=bank: e.matmul(c.psum[bank][:, 0:n], lhsT=hT[:, i, sl], rhs=wt[:, i, 0:n],
                                                                            start=(i == 0), stop=(i == 7))) for i in range(8)],
                     reads=[R_wt, R_hT], writes=[c.R_ps[bank]])
            k.op("dve", lambda e: e.tensor_copy(out=vtok, in_=c.psum[4]), reads=[c.R_ps[4]], writes=[R_vtok])
            k.op("act", lambda e: e.activation(out=sgt, in_=c.psum[5], func=AF.Silu), reads=[c.R_ps[5]], writes=[R_sgt])
            k.op("act", lambda e, i_own=i_own: e.activation(out=c.gsig[:, i_own, :], in_=c.psum[6][:, 0:24], func=AF.Sigmoid),
                 reads=[c.R_ps[6]], writes=[c.R_gsig])
            k.op("pe", [(lambda e, hd=hd: e.matmul(c.psum[7][:, hd * 128:(hd + 1) * 128], lhsT=W.kT[:, hd, sl], rhs=qT[:, hd, sl],
                                                   start=True, stop=True)) for hd in range(4)],
                 reads=[W.R_kT, R_qT], writes=[c.R_ps[7]])
            k.op("dve", lambda e: e.tensor_scalar(out=W.lk, in0=c.psum[7], scalar1=1e30, scalar2=-1e30, op0=ALU.min, op1=ALU.max),
                 reads=[c.R_ps[7], W.R_f], writes=[W.R_f])
            k.op("dve", lambda e: e.tensor_tensor(out=AT, in0=W.lk.rearrange("p (h x) -> p h x", h=4),
                                                  in1=c.tri.unsqueeze(1).broadcast_to([128, 4, 128]), op=ALU.mult),
                 reads=[W.R_f, c.R_constP], writes=[R_AT])
            for hd in range(4):
                k.op("act", lambda e, hd=hd, tt=tt, i_own=i_own: e.activation(out=Sp[:, hd, :], in_=St.SNAP[:, i_own, hd, :], func=AF.Copy,
                                                                              scale=W.er[:, hd, tt:tt + 1]),
                     reads=[St.R_SNAP, W.R_cols], writes=[R_Sp])
            fns = []
            for hd in range(4):
                fns.append(lambda e, hd=hd, tt=tt: e.matmul(c.psum[4][:, hd * 128:(hd + 1) * 128], lhsT=AT[:, hd, :],
                                                            rhs=vtok[:, hd * 128:(hd + 1) * 128], start=True, stop=False))
                fns.append(lambda e, hd=hd: e.matmul(c.psum[4][:, hd * 128:(hd + 1) * 128], lhsT=qT[:, hd, sl],
                                                     rhs=Sp[:, hd, :], start=False, stop=True))
            k.op("pe", fns, reads=[R_AT, R_vtok, R_qT, R_Sp], writes=[c.R_ps[4]])
            for hd in range(4):
                k.op("act", lambda e, hd=hd: e.activation(out=o2[:, hd * 128:(hd + 1) * 128], in_=c.psum[4][:, hd * 128:(hd + 1) * 128],
                                                          func=AF.Square, accum_out=hs[:, hd:hd + 1]),
                     reads=[c.R_ps[4]], writes=[R_o, R_hs])
            k.op("act", lambda e: e.activation(out=hs[:, 4:8], in_=hs[:, 0:4], func=AF.Sqrt, scale=1.0 / 128, bias=c.epsb[:, 0:1]),
                 reads=[R_hs, c.R_const], writes=[R_hs])
            k.op("dve", lambda e: e.reciprocal(out=hs[:, 8:12], in_=hs[:, 4:8]), reads=[R_hs], writes=[R_hs])
            k.op("dve", lambda e: e.tensor_tensor(out=o1, in0=c.psum[4], in1=gnt, op=ALU.mult), reads=[c.R_ps[4], R_gnt, R_o], writes=[R_o])
            k.op("pool", lambda e, tt=tt: e.tensor_tensor(out=o1, in0=o1, in1=sgt, op=ALU.mult), reads=[R_o, R_sgt], writes=[R_o])
            k.op("dve", lambda e: e.tensor_tensor(out=yb.rearrange("p (h x) -> p h x", h=4), in0=o1.rearrange("p (h x) -> p h x", h=4),
                                                  in1=hs[:, 8:12].unsqueeze(2).broadcast_to([128, 4, 128]), op=ALU.mult),
                 reads=[R_o, R_hs], writes=[R_yb])
            p6b = c.psum[6].bitcast(BF16)
            k.op("pe", [(lambda e, hd=hd: e.transpose(out=p6b[:, hd * 128:(hd + 1) * 128], in_=yb[:, hd * 128:(hd + 1) * 128], identity=c.identb))
                        for hd in range(4)], reads=[R_yb, c.R_const], writes=[c.R_ps[6]])
            k.op("act", lambda e, i_own=i_own: e.activation(out=c.oT_hg[:, :, i_own * 128:(i_own + 1) * 128],
                                                            in_=p6b[:, 0:512].rearrange("p (h x) -> p h x", h=4), func=AF.Copy),
                 reads=[c.R_ps[6]], writes=[c.R_oThg])
    ar.release(m0)


def phase_nsa(k, c, ar, St):
    Dr = c.D
    m0 = ar.mark()
    TINY = 1e-30
    CBi = [ar.al([128, 128], BF16) for _ in range(2)]
    VALi = [ar.al([128, 128], F32) for _ in range(2)]
    ADDCi = [ar.al([128, 128], F32) for _ in range(2)]
    R_tab = [k.res("nsatab%d" % i) for i in range(2)]
    R_tabP = [k.res("nsatabP%d" % i) for i in range(2)]
    WMT = ar.al([128, 8, 128], BF16)
    CM = ar.al([128, 4, 128], BF16)
    R_cst = k.res("nsacst")
    k.dma("pool", WMT, Dr["WMT"], writes=[R_cst])
    k.dma("pool", CM, Dr["CM"], writes=[R_cst])
    PT = [ar.al([128, 512], BF16) for _ in range(3)]
    R_PT = [k.res("PT%d" % i) for i in range(3)]
    Uc = ar.al([128, 4, 193], F32)
    R_Uc = k.res("Uc")
    Os = ar.al([128, 4, 65], F32)
    Ow = ar.al([128, 4, 65], F32)
    R_Os, R_Ow = k.res("Os"), k.res("Ow")
    score, sc2, imp = ar.al([128, 128], F32), ar.al([128, 128], F32), ar.al([128, 128], F32)
    R_sel = k.res("sel")
    selb = ar.al([128, 128], BF16)
    R_selb = k.res("selb")
    bd = ar.al([128, 4, 128], BF16)
    R_bd = k.res("bd")
    selX = ar.al([128, 128, 64], BF16)
    R_selX = [k.res("selX0"), k.res("selX1")]
    cols = ar.al([128, 64], F32)
    R_cols = k.res("nsacols")
    acc, tmp = ar.al([128, 4, 64], F32), ar.al([128, 4, 64], F32)
    R_acc = k.res("nsaacc")
    onsa = ar.al([128, 2, 4, 64], BF16)
    R_onsa = k.res("onsa")
    st = dict(s=0, p=0)
    pO_s, pO_w = c.psum[3][:, 0:260], c.psum[4][:, 0:260]
    pU = [c.psum[5], c.psum[6]]

    pend = []

    def flush_pv(keep=0):
        while len(pend) > keep:
            pend.pop(0)()

    def unit(KT, R_KT, kt_slice, QTg, g, bias, Vaug, R_V, outs, R_outs):
        sb = st["s"] % 3
        st["s"] += 1
        pb = st["p"] % 3
        st["p"] += 1
        S = c.psum[sb]
        fns = [lambda e: e.matmul(S, lhsT=KT[64 * g:64 * g + 64, kt_slice], rhs=QTg, start=True, stop=(bias is None))]
        rd = [R_KT, c.R_QT]
        if bias is not None:
            bl, R_bl = bias
            fns.append(lambda e: e.matmul(S, lhsT=bl, rhs=c.I4, start=False, stop=True))
            rd += [R_bl, c.R_constP]
        k.op("pe", fns, reads=rd, writes=[c.R_ps[sb]])
        k.op("act", lambda e: e.activation(out=PT[pb], in_=S, func=AF.Exp), reads=[c.R_ps[sb]], writes=[R_PT[pb]])

        def pv():
            k.op("pe", [(lambda e, a=a: e.matmul(outs[a], lhsT=PT[pb][:, a * 128:(a + 1) * 128], rhs=Vaug, start=False, stop=False,
                                                 skip_group_check=True)) for a in range(4)],
                 reads=[R_PT[pb], R_V], writes=R_outs)
        pend.append(pv)
        flush_pv(keep=2)

    for i in range(c.n_blocks):
        tb = i % 2
        k.dma("pool", CBi[tb], Dr["CB"][:, i, :], writes=[R_tabP[tb]])
        k.dma("sp", VALi[tb], Dr["VAL"][:, i, :], writes=[R_tab[tb]])
        k.dma("sp", ADDCi[tb], Dr["ADDC"][:, i, :], writes=[R_tab[tb]])
        for g in range(2):
            QTg = c.QT[64 * g:64 * g + 64, i, :, :].rearrange("p a t -> p (a t)")
            nmt = i // 4 + 1
            k.op("dve", lambda e: e.memset(pU[0], 0.0), writes=[c.R_ps[5]])
            k.op("dve", lambda e: e.memset(pU[1], 0.0), writes=[c.R_ps[6]])
            outsU = [pU[a // 2][:, (a % 2) * 193:(a % 2) * 193 + 193] for a in range(4)]
            for mt in range(nmt):
                bias = (CBi[tb], R_tabP[tb]) if mt == nmt - 1 else None
                unit(St.KC, St.R_KC, slice(mt * 128, (mt + 1) * 128), QTg, g, bias, St.VCA[:, mt, g, :], St.R_VCA, outsU, [c.R_ps[5], c.R_ps[6]])
            flush_pv()
            k.op("act", lambda e: e.activation(out=Uc[:, 0:2, :], in_=pU[0][:, 0:386].rearrange("p (a x) -> p a x", a=2), func=AF.Copy),
                 reads=[c.R_ps[5]], writes=[R_Uc])
            k.op("act", lambda e: e.activation(out=Uc[:, 2:4, :], in_=pU[1][:, 0:386].rearrange("p (a x) -> p a x", a=2), func=AF.Copy),
                 reads=[c.R_ps[6]], writes=[R_Uc])
            k.op("dve", lambda e: e.memset(c.psum[4], 0.0), writes=[c.R_ps[4]])
            outsW = [pO_w[:, a * 65:(a + 1) * 65] for a in range(4)]
            for r in range(8):
                kt = 4 * i - 4 + r
                if kt < 0:
                    continue
                unit(St.KTw, St.R_KTw, slice(kt * 128, (kt + 1) * 128), QTg, g, (WMT[:, r, :], R_cst), St.VwA[:, kt, g, :], St.R_VwA, outsW, [c.R_ps[4]])
            zc, rzc = cols[:, 0:4], cols[:, 4:8]
            k.op("dve", lambda e: e.tensor_scalar(out=zc, in0=Uc[:, :, 64], scalar1=TINY, scalar2=None, op0=ALU.max), reads=[R_Uc], writes=[R_cols])
            k.op("dve", lambda e: e.reciprocal(out=rzc, in_=zc), reads=[R_cols], writes=[R_cols])
            k.op("dve", lambda e: e.tensor_scalar(out=imp, in0=Uc[:, 0, 65:193], scalar1=rzc[:, 0:1], scalar2=None, op0=ALU.mult),
                 reads=[R_Uc, R_cols], writes=[R_sel])
            for a in range(1, 4):
                k.op("dve", lambda e, a=a: e.scalar_tensor_tensor(out=imp, in0=Uc[:, a, 65:193], scalar=rzc[:, a:a + 1], in1=imp,
                                                                  op0=ALU.mult, op1=ALU.add), reads=[R_Uc, R_cols, R_sel], writes=[R_sel])
            k.op("dve", lambda e: e.tensor_tensor(out=score, in0=imp, in1=VALi[tb], op=ALU.mult), reads=[R_sel, R_tab[tb]], writes=[R_sel])
            k.op("dve", lambda e: e.tensor_tensor(out=score, in0=score, in1=ADDCi[tb], op=ALU.add), reads=[R_sel, R_tab[tb]], writes=[R_sel])
            m8a, m8b = cols[:, 8:16], cols[:, 16:24]
            k.op("dve", lambda e: e.max(out=m8a, in_=score), reads=[R_sel], writes=[R_cols])
            k.op("dve", lambda e: e.match_replace(out=sc2, in_to_replace=m8a, in_values=score, imm_value=-1e9), reads=[R_sel, R_cols], writes=[R_sel])
            k.op("dve", lambda e: e.max(out=m8b, in_=sc2), reads=[R_sel], writes=[R_cols])
            k.op("dve", lambda e: e.tensor_scalar(out=selb, in0=score, scalar1=m8b[:, 7:8], scalar2=NEG, op0=ALU.is_lt, op1=ALU.mult),
                 reads=[R_sel, R_cols], writes=[R_selb])
            for r in range(4):
                kt = 4 * i + r
                k.op("dve", lambda e, r=r, kt=kt: e.tensor_tensor(
                    out=bd[:, r, :].rearrange("p (b x) -> p b x", b=2), in0=CM[:, r, :].rearrange("p (b x) -> p b x", b=2),
                    in1=selb[:, 2 * kt:2 * kt + 2].unsqueeze(2).broadcast_to([128, 2, 64]), op=ALU.add),
                    reads=[R_cst, R_selb], writes=[R_bd])
            if i > 0:
                for hx in range(2):
                    b0_, b1_ = 4 * i * hx, 4 * i * (hx + 1)
                    k.op("dve", lambda e, b0_=b0_, b1_=b1_: e.tensor_copy(
                        out=selX[:, b0_:b1_, :], in_=selb[:, b0_:b1_].unsqueeze(2).broadcast_to([128, b1_ - b0_, 64])),
                        reads=[R_selb], writes=[R_selX[hx]])
            k.op("dve", lambda e: e.memset(c.psum[3], 0.0), writes=[c.R_ps[3]])
            outsS = [pO_s[:, a * 65:(a + 1) * 65] for a in range(4)]
            for kt in list(range(4 * i, 4 * i + 4)) + list(range(4 * i)):
                if kt < 4 * i:
                    bl = selX[:, 2 * kt:2 * kt + 2, :].rearrange("p b x -> p (b x)")
                    bias = (bl, R_selX[0 if kt < 2 * i else 1])
                else:
                    bias = (bd[:, kt - 4 * i, :], R_bd)
                unit(St.KTs, St.R_KTs, slice(kt * 128, (kt + 1) * 128), QTg, g, bias, St.VsA[:, kt, g, :], St.R_VsA, outsS, [c.R_ps[3]])
            flush_pv()
            k.op("act", lambda e: e.activation(out=Os, in_=pO_s.rearrange("p (a x) -> p a x", a=4), func=AF.Copy), reads=[c.R_ps[3]], writes=[R_Os])
            k.op("act", lambda e: e.activation(out=Ow, in_=pO_w.rearrange("p (a x) -> p a x", a=4), func=AF.Copy), reads=[c.R_ps[4]], writes=[R_Ow])
            gs = c.gsig[:, i, 12 * g:12 * g + 12].rearrange("p (a x) -> p a x", a=4)
            zs, zw, cfc, cfs, cfw = cols[:, 24:28], cols[:, 28:32], cols[:, 32:36], cols[:, 36:40], cols[:, 40:44]
            k.op("dve", lambda e: e.tensor_scalar(out=zs, in0=Os[:, :, 64], scalar1=TINY, scalar2=None, op0=ALU.max), reads=[R_Os], writes=[R_cols])
            k.op("dve", lambda e: e.tensor_scalar(out=zw, in0=Ow[:, :, 64], scalar1=TINY, scalar2=None, op0=ALU.max), reads=[R_Ow], writes=[R_cols])
            k.op("dve", lambda e: e.reciprocal(out=zs, in_=zs), reads=[R_cols], writes=[R_cols])
            k.op("dve", lambda e: e.reciprocal(out=zw, in_=zw), reads=[R_cols], writes=[R_cols])
            k.op("dve", lambda e: e.tensor_tensor(out=cfc, in0=rzc, in1=gs[:, :, 0], op=ALU.mult), reads=[R_cols, c.R_gsig], writes=[R_cols])
            k.op("dve", lambda e: e.tensor_tensor(out=cfs, in0=zs, in1=gs[:, :, 1], op=ALU.mult), reads=[R_cols, c.R_gsig], writes=[R_cols])
            k.op("dve", lambda e: e.tensor_tensor(out=cfw, in0=zw, in1=gs[:, :, 2], op=ALU.mult), reads=[R_cols, c.R_gsig], writes=[R_cols])
            bc = lambda col: col.unsqueeze(2).broadcast_to([128, 4, 64])
            k.op("dve", lambda e: e.tensor_tensor(out=acc, in0=Uc[:, :, 0:64], in1=bc(cfc), op=ALU.mult), reads=[R_Uc, R_cols], writes=[R_acc])
            k.op("dve", lambda e: e.tensor_tensor(out=tmp, in0=Os[:, :, 0:64], in1=bc(cfs), op=ALU.mult), reads=[R_Os, R_cols, R_acc], writes=[R_acc])
            k.op("pool", lambda e: e.tensor_tensor(out=acc, in0=acc, in1=tmp, op=ALU.add), reads=[R_acc], writes=[R_acc])
            k.op("dve", lambda e: e.tensor_tensor(out=tmp, in0=Ow[:, :, 0:64], in1=bc(cfw), op=ALU.mult), reads=[R_Ow, R_cols, R_acc], writes=[R_acc])
            k.op("pool", lambda e, g=g: e.tensor_tensor(out=onsa[:, g, :, :], in0=acc, in1=tmp, op=ALU.add), reads=[R_acc], writes=[R_onsa])
        p7b = c.psum[7].bitcast(BF16)
        of = onsa.rearrange("p g a d -> p (g a d)")
        k.op("pe", [(lambda e, j=j: e.transpose(out=p7b[:, j * 128:(j + 1) * 128], in_=of[:, j * 128:(j + 1) * 128], identity=c.identb))
                    for j in range(4)], reads=[R_onsa, c.R_const], writes=[c.R_ps[7]])
        k.op("act", lambda e, i=i: e.activation(out=c.oT_nsa[:, :, i * 128:(i + 1) * 128],
                                                in_=p7b[:, 0:512].rearrange("p (j x) -> p j x", j=4), func=AF.Copy),
             reads=[c.R_ps[7]], writes=[c.R_oTnsa])
    ar.release(m0)


def phase_p2c(k, c, ar, X1, R_X1):
    Dr = c.D
    m0 = ar.mark()
    W = WS()
    scr = ar.al([128, D], F32)
    alloc_x_ws(k, c, ar, W, None, scr=scr, R_scr=k.res("scr2c"))
    hT = ar.al([128, 8, 512], BF16)
    R_hT = k.res("hT3")
    wbn, wbh = ar.al([128, 4, D], BF16), ar.al([128, 4, D], BF16)
    R_wb_ = k.res("wbr")
    wb = [ar.al([128, 8, 512], BF16) for _ in range(4)]
    R_wb = [k.res("wchc%d" % i) for i in range(4)]
    mixT = ar.al([128, 8, 512], BF16)
    R_mixT = k.res("mixT")
    sg1, sg2, mx1 = ar.al([128, 512], F32), ar.al([128, 512], F32), ar.al([128, 512], F32)
    R_sg1, R_sg2, R_mx = k.res("sg1"), k.res("sg2"), k.res("mx1")
    k.dma("pool", wbn, Dr["w_brn"].rearrange("(kt p) n -> p kt n", p=128), writes=[R_wb_])
    k.dma("pool", wbh, Dr["w_brh"].rearrange("(kt p) n -> p kt n", p=128), writes=[R_wb_])

    def wl(buf, src, c0):
        k.dma("pool", wb[buf], src[:, c0:c0 + 512].rearrange("(kt p) n -> p kt n", p=128), writes=[R_wb[buf]])

    NGo = NT // 4
    wl(0, Dr["w_mg"], 0)
    wl(1, Dr["w_mg"], 1024)
    for go in range(NGo):
        p = go % 2
        A = (2 * p, 2 * p + 1)
        B = (2 - 2 * p, 3 - 2 * p)
        tok = slice(go * 512, (go + 1) * 512)
        for tt in range(4):
            t = 4 * go + tt
            k.dma("sp", X1[:, t, :], Dr["xo"][t * 128:(t + 1) * 128, :], writes=[R_X1[t]])
        wl(B[0], Dr["w_mg"], 512)
        wl(B[1], Dr["w_mg"], 1024 + 512)
        norm_transpose_group(k, c, W, Dr["xo"], go * 512, hT, R_hT)
        for hf in range(2):
            w0, w1_ = (A if hf == 0 else B)
            for f4 in range(4):
                ft = hf * 4 + f4
                fs = slice(f4 * 128, (f4 + 1) * 128)
                gs_ = slice(ft * 128, (ft + 1) * 128)
                k.op("pe", [(lambda e, i=i: e.matmul(c.psum[2], lhsT=wb[w0][:, i, fs], rhs=hT[:, i, :], start=(i == 0), stop=(i == 7)))
                            for i in range(8)], reads=[R_wb[w0], R_hT], writes=[c.R_ps[2]])
                k.op("pe", [(lambda e, i=i: e.matmul(c.psum[3], lhsT=wb[w1_][:, i, fs], rhs=hT[:, i, :], start=(i == 0), stop=(i == 7)))
                            for i in range(8)], reads=[R_wb[w1_], R_hT], writes=[c.R_ps[3]])
                k.op("pe", [(lambda e, i=i: e.matmul(c.psum[4], lhsT=wbn[:, i, gs_], rhs=c.oT_nsa[:, i, tok], start=(i == 0), stop=(i == 3)))
                            for i in range(4)], reads=[R_wb_, c.R_oTnsa], writes=[c.R_ps[4]])
                k.op("pe", [(lambda e, i=i: e.matmul(c.psum[5], lhsT=wbh[:, i, gs_], rhs=c.oT_hg[:, i, tok], start=(i == 0), stop=(i == 3)))
                            for i in range(4)], reads=[R_wb_, c.R_oThg], writes=[c.R_ps[5]])
                k.op("act", lambda e: e.activation(out=sg1, in_=c.psum[2], func=AF.Sigmoid), reads=[c.R_ps[2]], writes=[R_sg1])
                k.op("act", lambda e: e.activation(out=sg2, in_=c.psum[3], func=AF.Sigmoid), reads=[c.R_ps[3]], writes=[R_sg2])
                k.op("dve", lambda e: e.tensor_tensor(out=mx1, in0=c.psum[4], in1=sg1, op=ALU.mult), reads=[c.R_ps[4], R_sg1], writes=[R_mx])
                k.op("dve", lambda e: e.tensor_tensor(out=sg2, in0=c.psum[5], in1=sg2, op=ALU.mult), reads=[c.R_ps[5], R_sg2], writes=[R_sg2])
                k.op("pool", lambda e, ft=ft: e.tensor_tensor(out=mixT[:, ft, :], in0=mx1, in1=sg2, op=ALU.add),
                     reads=[R_mx, R_sg2], writes=[R_mixT])
            if hf == 0:
                wl(A[0], Dr["w_out"], 0)
                wl(A[1], Dr["w_out"], 512)
            elif go + 1 < NGo:
                wl(B[0], Dr["w_mg"], 0)
                wl(B[1], Dr["w_mg"], 1024)
        for tt in range(4):
            t = 4 * go + tt
            for hf in range(2):
                bank = 6 + hf
                k.op("pe", [(lambda e, i=i: e.matmul(c.psum[bank], lhsT=mixT[:, i, tt * 128:(tt + 1) * 128], rhs=wb[A[hf]][:, i, :],
                                                     start=(i == 0), stop=(i == 7))) for i in range(8)],
                     reads=[R_mixT, R_wb[A[hf]]], writes=[c.R_ps[bank]])
                xs = X1[:, t, hf * 512:(hf + 1) * 512]
                k.op("dve", lambda e, xs=xs: e.tensor_tensor(out=xs, in0=c.psum[bank], in1=xs, op=ALU.add),
                     reads=[c.R_ps[bank], R_X1[t]], writes=[R_X1[t]])
    ar.release(m0)


def phase_attn(k, c, ar, stage):
    St = WS()
    St.KTs, St.KTw = ar.al([128, SEQ], BF16), ar.al([128, SEQ], BF16)
    St.VsA, St.VwA = ar.al([128, 64, 2, 65], BF16), ar.al([128, 64, 2, 65], BF16)
    St.KC = ar.al([128, 512], BF16)
    St.VCA = ar.al([128, 4, 2, 193], BF16)
    St.SNAP = ar.al([128, NT, 4, 128], BF16)
    for n in ("KTs", "KTw", "VsA", "VwA", "KC", "VCA", "SNAP"):
        setattr(St, "R_" + n, k.res(n))
    phase_p1(k, c, ar, St)
    for n in ("KTs", "KTw", "VsA", "VwA", "KC", "VCA", "SNAP"):
        c.dump(n, getattr(St, n), [getattr(St, "R_" + n)])
    k.flush()
    phase_p2pre(k, c, ar, St)
    c.dump("QT", c.QT, [c.R_QT])
    c.dump("gsig", c.gsig, [c.R_gsig])
    c.dump("oT_hg", c.oT_hg, [c.R_oThg])
    k.flush()
    phase_nsa(k, c, ar, St)
    c.dump("oT_nsa", c.oT_nsa, [c.R_oTnsa])
    return St


def build(stage="full", n_exp=NE):
    nc = bass.Bass("TRN2", target_bir_lowering=False)
    Dr = {}

    def din(name, shape, dt=F32):
        Dr[name] = nc.dram_tensor(name, list(shape), dt, kind="ExternalInput").ap()

    for name, shape, dt in input_specs(max(n_exp, 1), stage):
        din(name, shape, dt)
    Dr["y"] = nc.dram_tensor("y", [NTOK, D], F32, kind="ExternalOutput").ap()
    with ExitStack() as es:
        ARENA_BYTES = 207 * 1024
        big = es.enter_context(nc.sbuf_tensor("arena", [128, ARENA_BYTES // 2], BF16))
        pst = es.enter_context(nc.psum_tensor("ps", [128, 4096], F32))
        ar = Arena(big, ARENA_BYTES)
        k = KH(nc, es)
        k.oplim = _NC_CACHE.get("oplim", 10 ** 9)
        c = Ctx()
        c.nc, c.D, c.n_exp = nc, Dr, n_exp
        dumps = []

        def dump(name, ap, rs):
            if not _NC_CACHE.get("dbg"):
                return
            dt_ = ap.dtype
            dr = nc.dram_tensor("dbg_" + name, list(ap.shape), dt_, kind="ExternalOutput").ap()
            r = k.res("dbg_" + name)
            k.dma("sp", dr, ap, reads=list(rs), key=r)
            dumps.append(r)
        c.dump = dump
        c.psum = [pst[:, i * 512:(i + 1) * 512] for i in range(8)]
        c.pwide = lambda i: pst[:, i * 512:(i + 2) * 512]
        c.R_ps = [k.res("psb%d" % i, excl=True) for i in range(8)]
        c.R_const = k.res("const")
        c.identf = ar.al([128, 128], F32)
        c.identb = ar.al([128, 128], BF16)
        c.epsb = ar.al([128, 1], F32)
        c.ssq = ar.al([128, 8], F32)
        c.std = ar.al([128, 8], F32)
        c.rstd = ar.al([128, 8], F32)
        c.rs_res = [k.res("rs%d" % i) for i in range(8)]
        c.rs_i = 0
        k.dma("sp", c.identf, Dr["identf"], writes=[c.R_const])
        k.dma("sp", c.identb, Dr["identb"], writes=[c.R_const])
        k.op("dve", lambda e: e.memset(c.epsb, EPS), writes=[c.R_const])
        c.n_groups = _NC_CACHE.get("n_groups", 16)
        c.n_blocks = _NC_CACHE.get("n_blocks", NT)
        c.R32 = ar.al([128, 8, NTOK], BF16)
        c.R32f = c.R32.rearrange("p a b -> p (a b)")
        c.QT = c.R32f[:, 0:8192].rearrange("p (i a t) -> p i a t", i=NT, a=4)
        c.oT_nsa = c.R32[:, 4:8, :]
        c.oT_hg = ar.al([128, 4, NTOK], BF16)
        c.gsig = ar.al([128, NT, 24], F32)
        c.R_QT, c.R_oTnsa, c.R_oThg, c.R_gsig = k.res("QT"), k.res("oTnsa"), k.res("oThg"), k.res("gsig")
        hfT = c.R32
        R_hfT = [k.res("hfT_%d" % t) for t in range(NT)]
        R_X1 = [k.res("x1_%d" % t) for t in range(NT)]
        if stage != "moe_only":
            c.I4 = ar.al([128, 512], BF16)
            c.tri = ar.al([128, 128], BF16)
            c.onehot = ar.al([128, 4], F32)
            c.R_constP = k.res("constP")
            k.dma("pool", c.I4, Dr["I4"], writes=[c.R_constP])
            k.dma("pool", c.tri, Dr["tri"], writes=[c.R_constP])
            k.dma("sp", c.onehot, Dr["onehot"], writes=[c.R_const])
            setup_lb(k, c, ar)
            M1 = ar.mark()
            phase_attn(k, c, ar, stage)
            for r in dumps:
                k.rec["sp"].append(("w", r.dsem, r.dcnt))
            k.flush()
            ar.release(M1)
        X1 = ar.al([128, NT, D], F32)
        if stage == "moe_only":
            for t in range(NT):
                k.dma("sp", X1[:, t, :], Dr["xo"][t * 128:(t + 1) * 128, :], writes=[R_X1[t]])
        else:
            phase_p2c(k, c, ar, X1, R_X1)
            c.dump("X1", X1, R_X1)
            for r in dumps:
                if r.name == "dbg_X1":
                    k.rec["sp"].append(("w", r.dsem, r.dcnt))
        k.flush()
        if n_exp >= 0:
            phase_moe(k, c, ar, X1, R_X1, hfT, R_hfT)
            k.flush()
        phase_final(k, c, ar, X1, R_X1)
        k.flush()
    return nc


def input_specs(ne=NE, stage="full"):
    return [
        ("xb", (SEQ, D), F32), ("xo", (NTOK, D), F32),
        ("g_ffn", (D,), F32), ("g_fin", (D,), F32),
        ("w_r", (D, 36), F32), ("b_r", (36,), F32),
        ("w_gate", (ne, D, DFF), F32), ("w_up", (ne, D, DFF), F32), ("w_down", (ne, DFF, D), F32),
        ("identf", (128, 128), F32), ("identb", (128, 128), BF16),
    ] + (attn_input_specs() if stage != "moe_only" else [])


_NC_CACHE = {}


def host_inputs(inp, core, ne=NE):
    b, cp = core // 4, core % 4
    x = np.asarray(inp["x"], dtype=np.float32)
    m = {}
    m["xb"] = np.ascontiguousarray(x[b])
    m["xo"] = np.ascontiguousarray(x[b].reshape(NT, 4, 128, D)[:, cp].reshape(NTOK, D))
    m["g_ffn"] = np.ascontiguousarray(inp["ffn_norm"][0])
    m["g_fin"] = np.ascontiguousarray(inp["final_norm"])
    m["w_r"] = np.ascontiguousarray(np.concatenate([inp["w_grp"][0], inp["w_rtr"][0]], axis=1))
    m["b_r"] = np.ascontiguousarray(np.concatenate([inp["b_grp"][0], inp["b_rtr"][0]], axis=0))
    m["w_gate"] = np.ascontiguousarray(inp["w_gate"][0, :ne])
    m["w_up"] = np.ascontiguousarray(inp["w_up"][0, :ne])
    m["w_down"] = np.ascontiguousarray(inp["w_down"][0, :ne])
    m["identf"] = np.eye(128, dtype=np.float32)
    m["identb"] = np.eye(128, dtype=np.float32).astype(ml_dtypes.bfloat16)
    if _NC_CACHE.get("stage", "full") != "moe_only":
        m.update(attn_host_inputs(inp, b, cp))
    return m


def kernel(**inp):
    inp = {k_: np.asarray(v) for k_, v in inp.items()}
    stage = _NC_CACHE.get("stage", "full")
    key = ("nc", stage)
    if key not in _NC_CACHE:
        _NC_CACHE[key] = build(stage, _NC_CACHE.get("n_exp", NE))
    nc = _NC_CACHE[key]
    shared = None
    in_maps = []
    for core in range(8):
        m = host_inputs(inp, core, max(_NC_CACHE.get("n_exp", NE), 1))
        if shared is None:
            shared = m
        else:
            for kk in ("w_gate", "w_up", "w_down"):
                m[kk] = shared[kk]
        in_maps.append(m)
    res = run_bass_kernel_spmd(nc, in_maps, core_ids=list(range(8)))
    _NC_CACHE["last_results"] = res.results
    out = np.zeros((2, SEQ // 128, 128, D), dtype=np.float32)
    for core in range(8):
        b, cp = core // 4, core % 4
        y = np.asarray(res.results[core]["y"]).reshape(NT, 128, D)
        out[b, cp::4] = y
    return out.reshape(2, SEQ, D)
```

```python
import numpy as np
import ml_dtypes
import concourse.bass as bass
import concourse.mybir as mybir
from concourse.bass_utils import run_bass_kernel_spmd
from contextlib import ExitStack

F32 = mybir.dt.float32
BF16 = mybir.dt.bfloat16
AF = mybir.ActivationFunctionType
ALU = mybir.AluOpType
AX = mybir.AxisListType


class Res:
    __slots__ = ("name", "w", "rs", "dsem", "dcnt", "excl")

    def __init__(self, name, excl=False):
        self.name = name
        self.excl = excl
        self.w = None
        self.rs = {}
        self.dsem = None
        self.dcnt = 0


class _Proxy:
    def __init__(self):
        self.calls = []

    def __getattr__(self, name):
        def rec(*a, **kw):
            self.calls.append((name, a, kw))
        return rec


def _bind(f):
    p = _Proxy()
    f(p)
    assert len(p.calls) == 1, "one engine instruction per callable"
    name, a, kw = p.calls[0]
    return lambda eng: getattr(eng, name)(*a, **kw)


class KH:
    ENG = ("pe", "dve", "act", "pool", "sp")

    def __init__(self, nc, es):
        self.nc = nc
        self.es = es
        self.sem = {}
        self.cnt = {}
        for e in self.ENG:
            self.sem[e] = es.enter_context(nc.semaphore("s_" + e))
            self.cnt[e] = 0
        self.rec = {e: [] for e in self.ENG}
        self.seen = {e: {} for e in self.ENG}
        self.nsem = len(self.ENG)
        self.semobj = dict(self.sem)

    def res(self, name, excl=False):
        return Res(name, excl)

    def _dma_sem(self, r):
        if r.dsem is None:
            r.dsem = "d_" + r.name + "_%d" % self.nsem
            self.semobj[r.dsem] = self.es.enter_context(self.nc.semaphore(r.dsem))
            self.nsem += 1
        return r.dsem

    def _deps(self, e, reads, writes):
        deps = {}
        for r in reads:
            if r.w is not None:
                k, v = r.w
                deps[k] = max(deps.get(k, 0), v)
        for w in writes:
            if w.w is not None:
                k, v = w.w
                deps[k] = max(deps.get(k, 0), v)
            for k, v in w.rs.items():
                deps[k] = max(deps.get(k, 0), v)
        seen = self.seen[e]
        for k, v in deps.items():
            if seen.get(k, 0) >= v:
                continue
            seen[k] = v
            self.rec[e].append(("w", k, v))

    def op(self, e, fns, reads=(), writes=()):
        if callable(fns):
            fns = [fns]
        self.opn = getattr(self, "opn", 0) + 1
        if self.opn > getattr(self, "oplim", 10 ** 9):
            return
        ex = [r for r in reads if r.excl]
        if ex:
            reads = [r for r in reads if not r.excl]
            writes = list(writes) + [r for r in ex if r not in writes]
        self._deps(e, reads, writes)
        self.cnt[e] += 1
        v = self.cnt[e]
        fns = [_bind(f) for f in fns]
        for f in fns[:-1]:
            self.rec[e].append(("i", f, None, 0))
        self.rec[e].append(("i", fns[-1], e, 1))
        self.seen[e][e] = max(self.seen[e].get(e, 0), 0)
        for r in reads:
            r.rs[e] = v
        for w in writes:
            w.w = (e, v)
            w.rs = {}

    def dma(self, q, out, in_, reads=(), writes=(), key=None, **kw):
        self._deps(q, reads, writes)
        kr = key or (writes[0] if writes else reads[0])
        sk = self._dma_sem(kr)
        kr.dcnt += 16
        v = kr.dcnt
        self.rec[q].append(("i", lambda eng: eng.dma_start(out=out, in_=in_, **kw), sk, 16))
        for r in reads:
            r.rs[sk] = v
        for w in writes:
            w.w = (sk, v)
            w.rs = {}

    def wait_res(self, e, rs):
        self._deps(e, rs, ())

    def simulate(self):
        if not hasattr(self, "simval"):
            self.simval = {}
        val = self.simval
        ptr = {e: 0 for e in self.ENG}
        prog = True
        while prog:
            prog = False
            for e in self.ENG:
                items = self.rec[e]
                while ptr[e] < len(items):
                    it = items[ptr[e]]
                    if it[0] == "w":
                        if val.get(it[1], 0) >= it[2]:
                            ptr[e] += 1
                            prog = True
                        else:
                            break
                    else:
                        if it[2] is not None:
                            val[it[2]] = val.get(it[2], 0) + it[3]
                        ptr[e] += 1
                        prog = True
        for e in self.ENG:
            if ptr[e] < len(self.rec[e]):
                it = self.rec[e][ptr[e]]
                raise RuntimeError("DEADLOCK: engine %s stuck at item %d/%d waiting %s >= %s (have %s)" % (
                    e, ptr[e], len(self.rec[e]), it[1], it[2], val.get(it[1], 0)))

    def flush(self, name=None):
        nc = self.nc
        rec = self.rec
        semobj = self.semobj
        self.simulate()
        import os
        if os.environ.get("KH_DEBUG"):
            print("KH flush: ops so far", getattr(self, "opn", 0), {e: len(v) for e, v in self.rec.items()}, "nsem", self.nsem, flush=True)

        def play(eng, items):
            for it in items:
                if it[0] == "w":
                    eng.wait_ge(semobj[it[1]], it[2])
                else:
                    ins = it[1](eng)
                    if it[2] is not None:
                        ins.then_inc(semobj[it[2]], it[3])

        with nc.Block() as block:
            if rec["sp"]:
                @block.sync
                def _(eng):
                    play(eng, rec["sp"])
            if rec["pe"]:
                @block.tensor
                def _(eng):
                    play(eng, rec["pe"])
            if rec["dve"]:
                @block.vector
                def _(eng):
                    play(eng, rec["dve"])
            if rec["act"]:
                @block.scalar
                def _(eng):
                    play(eng, rec["act"])
            if rec["pool"]:
                @block.gpsimd
                def _(eng):
                    play(eng, rec["pool"])
        self.rec = {e: [] for e in self.ENG}

NEG = -30000.0
NT = 16
NTOK = NT * 128
SEQ = 8192
D = 1024
NE = 32
DFF = 512
EPS = 1e-6


class Arena:
    def __init__(self, big, nbytes):
        self.big = big
        self.n = nbytes
        self.off = 0

    def mark(self):
        return self.off

    def release(self, m):
        import os
        if os.environ.get("KH_DEBUG"):
            print("arena release: peak", getattr(self, "peak", 0), "->", m, "of", self.n, flush=True)
        self.peak = m
        self.off = m

    def al(self, shape, dt):
        esz = 4 if dt == F32 else 2
        per = int(np.prod(shape[1:])) * esz
        self.off = (self.off + 63) // 64 * 64
        o = self.off
        assert o + per <= self.n, ("arena overflow", o, per, self.n)
        self.off = o + per
        self.peak = max(getattr(self, "peak", 0), self.off)
        v = self.big[0:shape[0], o // 2:(o + per) // 2]
        if dt == F32:
            v = v.bitcast(F32)
        if len(shape) == 3:
            v = v.rearrange("p (a b) -> p a b", a=shape[1])
        elif len(shape) == 4:
            v = v.rearrange("p (a b c) -> p a b c", a=shape[1], b=shape[2])
        return v


class Ctx:
    pass


def rms_rstd(k, c, src, src_res, scr, scr_res, n_feat, tag):
    i = c.rs_i % 8
    c.rs_i += 1
    ssq, std, rstd = c.ssq[:, i:i + 1], c.std[:, i:i + 1], c.rstd[:, i:i + 1]
    R = c.rs_res[i]
    k.op("act", lambda e: e.activation(out=scr, in_=src, func=AF.Square, accum_out=ssq),
         reads=[src_res], writes=[scr_res, R])
    k.op("act", lambda e: e.activation(out=std, in_=ssq, func=AF.Sqrt, scale=1.0 / n_feat, bias=c.epsb[:, 0:1]),
         reads=[R, c.R_const], writes=[R])
    k.op("dve", lambda e: e.reciprocal(out=rstd, in_=std), reads=[R], writes=[R])
    return rstd, R


def phase_moe(k, c, ar, X1, R_X1, hfT, R_hfT):
    nc = c.nc
    Dr = c.D
    m0 = ar.mark()
    hf32 = [ar.al([128, D], F32) for _ in range(2)]
    R_hf32 = [k.res("hf32_%d" % i) for i in range(2)]
    scr = ar.al([128, D], F32)
    R_scr = k.res("moe_scr")
    hT32 = [ar.al([128, 8, 128], F32) for _ in range(2)]
    R_hT32 = [k.res("hT32_%d" % i) for i in range(2)]
    wr32 = ar.al([128, 8, 36], F32)
    R_wr = k.res("wr32")
    brt = ar.al([128, 36], F32)
    gft = ar.al([128, D], F32)
    R_gft = k.res("gft")
    comb = ar.al([128, NT, NE], F32)
    R_comb = k.res("comb")
    sm = ar.al([128, 128], F32)
    R_sm = k.res("moe_sm")
    k.dma("sp", wr32, Dr["w_r"].rearrange("(kt p) n -> p kt n", p=128), writes=[R_wr])
    k.dma("sp", brt, Dr["b_r"].partition_broadcast(128), writes=[R_wr])
    k.dma("sp", gft, Dr["g_ffn"].partition_broadcast(128), writes=[R_gft])
    pT = [c.pwide(0), c.pwide(2)]
    R_pT = [[c.R_ps[0], c.R_ps[1]], [c.R_ps[2], c.R_ps[3]]]
    pL = c.psum[4]
    R_pL = c.R_ps[4]
    for t in range(NT):
        b = t % 2
        xs = X1[:, t, :]
        rstd, R_r = rms_rstd(k, c, xs, R_X1[t], scr, R_scr, D, "moe")
        k.op("dve", lambda e, b=b, xs=xs, rstd=rstd: e.scalar_tensor_tensor(
            out=hf32[b], in0=xs, scalar=rstd, in1=gft, op0=ALU.mult, op1=ALU.mult),
            reads=[R_X1[t], R_r, R_gft], writes=[R_hf32[b]])
        p2 = pT[b]
        k.op("pe", [(lambda e, i=i, b=b, p2=p2: e.transpose(out=p2[:, i * 128:(i + 1) * 128],
                                                            in_=hf32[b][:, i * 128:(i + 1) * 128], identity=c.identf))
                    for i in range(8)], reads=[R_hf32[b], c.R_const], writes=R_pT[b])
        k.op("act", lambda e, b=b, p2=p2: e.activation(out=hT32[b].rearrange("p a b -> p (a b)"), in_=p2, func=AF.Copy),
             reads=R_pT[b], writes=[R_hT32[b]])
        k.op("dve", lambda e, b=b, p2=p2, t=t: e.tensor_copy(
            out=hfT[:, :, t * 128:(t + 1) * 128], in_=p2.rearrange("p (a b) -> p a b", a=8)),
            reads=R_pT[b], writes=[R_hfT[t]])
        lg = pL[:, 0:36]
        k.op("pe", [(lambda e, i=i, b=b: e.matmul(lg, lhsT=hT32[b][:, i, :], rhs=wr32[:, i, :], start=(i == 0), stop=(i == 7)))
                    for i in range(8)], reads=[R_hT32[b], R_wr], writes=[R_pL])
        lgs = sm[:, 0:36]
        gmax, gsum, gp, pen = sm[:, 36:37], sm[:, 37:38], sm[:, 38:39], sm[:, 40:44]
        gex, goh = sm[:, 44:48], sm[:, 48:52]
        elm = sm[:, 52:84]
        m8 = sm[:, 84:92]
        dd, ee, w1, w2 = sm[:, 92:93], sm[:, 93:94], sm[:, 94:95], sm[:, 95:96]
        oh = sm[:, 96:128]
        ct = comb[:, t, :]
        RW = dict(reads=[R_sm], writes=[R_sm])
        k.op("dve", lambda e: e.tensor_tensor(out=lgs, in0=lg, in1=brt, op=ALU.add), reads=[R_pL, R_wr, R_sm], writes=[R_sm])
        k.op("dve", lambda e: e.reduce_max(out=gmax, in_=lgs[:, 0:4], axis=AX.X), **RW)
        k.op("dve", lambda e: e.tensor_scalar(out=gex, in0=lgs[:, 0:4], scalar1=gmax, scalar2=None, op0=ALU.subtract), **RW)
        k.op("act", lambda e: e.activation(out=gex, in_=gex, func=AF.Exp, accum_out=gsum), **RW)
        k.op("dve", lambda e: e.reciprocal(out=gp, in_=gsum), **RW)
        k.op("dve", lambda e: e.tensor_scalar(out=pen, in0=lgs[:, 0:4], scalar1=gmax, scalar2=-1e30, op0=ALU.is_lt, op1=ALU.mult), **RW)
        k.op("dve", lambda e: e.tensor_tensor(out=elm.rearrange("p (g x) -> p g x", g=4),
                                              in0=lgs[:, 4:36].rearrange("p (g x) -> p g x", g=4),
                                              in1=pen.unsqueeze(2).broadcast_to([128, 4, 8]), op=ALU.add), **RW)
        k.op("dve", lambda e: e.max(out=m8, in_=elm), **RW)
        k.op("dve", lambda e: e.tensor_tensor(out=dd, in0=m8[:, 1:2], in1=m8[:, 0:1], op=ALU.subtract), **RW)
        k.op("act", lambda e: e.activation(out=ee, in_=dd, func=AF.Exp), **RW)
        k.op("dve", lambda e: e.tensor_scalar(out=ee, in0=ee, scalar1=1.0, scalar2=None, op0=ALU.add), **RW)
        k.op("dve", lambda e: e.reciprocal(out=w1, in_=ee), **RW)
        k.op("dve", lambda e: e.tensor_scalar(out=w2, in0=w1, scalar1=-1.0, scalar2=1.0, op0=ALU.mult, op1=ALU.add), **RW)
        k.op("dve", lambda e: e.tensor_tensor(out=w1, in0=w1, in1=gp, op=ALU.mult), **RW)
        k.op("dve", lambda e: e.tensor_tensor(out=w2, in0=w2, in1=gp, op=ALU.mult), **RW)
        k.op("dve", lambda e: e.tensor_scalar(out=oh, in0=elm, scalar1=m8[:, 0:1], scalar2=w1, op0=ALU.is_equal, op1=ALU.mult), **RW)
        k.op("dve", lambda e, ct=ct: e.tensor_scalar(out=ct, in0=elm, scalar1=m8[:, 1:2], scalar2=w2, op0=ALU.is_equal, op1=ALU.mult),
             reads=[R_sm], writes=[R_comb])
        k.op("dve", lambda e, ct=ct: e.tensor_tensor(out=ct, in0=ct, in1=oh, op=ALU.add), reads=[R_sm, R_comb], writes=[R_comb])

    if c.n_exp == 0:
        ar.release(m0)
        return
    NWB = 2
    wg = [ar.al([128, 8, DFF], BF16) for _ in range(NWB)]
    wu = [ar.al([128, 8, DFF], BF16) for _ in range(NWB)]
    wd = [ar.al([128, 4, D], BF16) for _ in range(NWB)]
    R_wg = [k.res("wg%d" % i) for i in range(NWB)]
    R_wu = [k.res("wu%d" % i) for i in range(NWB)]
    R_wd = [k.res("wd%d" % i) for i in range(NWB)]
    hid = [ar.al([128, 4, 512], BF16) for _ in range(2)]
    R_hid = [k.res("hid%d" % i) for i in range(2)]
    sg = [ar.al([128, 512], F32) for _ in range(2)]
    R_sg = [k.res("sg%d" % i) for i in range(2)]
    n_exp = c.n_exp

    def load_w(e):
        b = e % NWB
        k.dma("pool", wg[b], Dr["w_gate"][e].rearrange("(kt p) n -> p kt n", p=128), writes=[R_wg[b]])
        k.dma("pool", wu[b], Dr["w_up"][e].rearrange("(kt p) n -> p kt n", p=128), writes=[R_wu[b]])
        k.dma("pool", wd[b], Dr["w_down"][e].rearrange("(kt p) n -> p kt n", p=128), writes=[R_wd[b]])

    units = [(e, g) for e in range(n_exp) for g in range(NT // 4)]
    pgu = [(c.psum[0], c.psum[1]), (c.psum[2], c.psum[3])]
    R_pgu = [(c.R_ps[0], c.R_ps[1]), (c.R_ps[2], c.R_ps[3])]
    pdn = [c.psum[4], c.psum[5], c.psum[6], c.psum[7]]
    R_pdn = [c.R_ps[4], c.R_ps[5], c.R_ps[6], c.R_ps[7]]
    st = dict(gu=0, dn=0)

    def gate_up(u):
        e, g = units[u]
        b = e % NWB
        hb = u % 2
        tok = slice(g * 512, (g + 1) * 512)
        for ff in range(4):
            pb = st["gu"] % 2
            st["gu"] += 1
            pg, pu = pgu[pb]
            k.op("pe", [(lambda en, i=i, pg=pg, b=b, ff=ff: en.matmul(pg, lhsT=wg[b][:, i, ff * 128:(ff + 1) * 128], rhs=hfT[:, i, tok],
                                                                      start=(i == 0), stop=(i == 7))) for i in range(8)],
                 reads=[R_wg[b]] + R_hfT[4 * g:4 * g + 4], writes=[R_pgu[pb][0]])
            k.op("pe", [(lambda en, i=i, pu=pu, b=b, ff=ff: en.matmul(pu, lhsT=wu[b][:, i, ff * 128:(ff + 1) * 128], rhs=hfT[:, i, tok],
                                                                      start=(i == 0), stop=(i == 7))) for i in range(8)],
                 reads=[R_wu[b]] + R_hfT[4 * g:4 * g + 4], writes=[R_pgu[pb][1]])
            k.op("act", lambda en, pg=pg, pb=pb: en.activation(out=sg[pb], in_=pg, func=AF.Silu),
                 reads=[R_pgu[pb][0]], writes=[R_sg[pb]])
            k.op("dve", lambda en, pu=pu, pb=pb, hb=hb, ff=ff: en.tensor_tensor(out=hid[hb][:, ff, :], in0=pu, in1=sg[pb], op=ALU.mult),
                 reads=[R_pgu[pb][1], R_sg[pb]], writes=[R_hid[hb]])

    def down(u):
        e, g = units[u]
        b = e % NWB
        hb = u % 2
        for tt in range(4):
            t = 4 * g + tt
            for hf in range(2):
                pb = st["dn"] % 4
                st["dn"] += 1
                po = pdn[pb]
                k.op("pe", [(lambda en, i=i, po=po, b=b, hb=hb, tt=tt, hf=hf: en.matmul(
                    po, lhsT=hid[hb][:, i, tt * 128:(tt + 1) * 128], rhs=wd[b][:, i, hf * 512:(hf + 1) * 512],
                    start=(i == 0), stop=(i == 3))) for i in range(4)],
                    reads=[R_hid[hb], R_wd[b]], writes=[R_pdn[pb]])
                xs = X1[:, t, hf * 512:(hf + 1) * 512]
                k.op("dve", lambda en, po=po, xs=xs, t=t, e=e: en.scalar_tensor_tensor(
                    out=xs, in0=po, scalar=comb[:, t, e:e + 1], in1=xs, op0=ALU.mult, op1=ALU.add),
                    reads=[R_pdn[pb], R_comb, R_X1[t]], writes=[R_X1[t]])

    load_w(0)
    for u in range(len(units)):
        e, g = units[u]
        gate_up(u)
        if u >= 1:
            down(u - 1)
        if g == 0 and e + 1 < n_exp:
            load_w(e + 1)
    down(len(units) - 1)
    ar.release(m0)


def phase_final(k, c, ar, X1, R_X1):
    Dr = c.D
    m0 = ar.mark()
    gft = ar.al([128, D], F32)
    R_g = k.res("gfin")
    scr = ar.al([128, D], F32)
    R_scr = k.res("fin_scr")
    ob = [ar.al([128, D], F32) for _ in range(2)]
    R_ob = [k.res("ob%d" % i) for i in range(2)]
    k.dma("sp", gft, Dr["g_fin"].partition_broadcast(128), writes=[R_g])
    for t in range(NT):
        b = t % 2
        xs = X1[:, t, :]
        rstd, R_r = rms_rstd(k, c, xs, R_X1[t], scr, R_scr, D, "fin")
        k.op("dve", lambda e, b=b, xs=xs, rstd=rstd: e.scalar_tensor_tensor(
            out=ob[b], in0=xs, scalar=rstd, in1=gft, op0=ALU.mult, op1=ALU.mult),
            reads=[R_X1[t], R_r, R_g], writes=[R_ob[b]])
        k.dma("sp", Dr["y"][t * 128:(t + 1) * 128, :], ob[b], reads=[R_ob[b]])
    for b in range(2):
        for sk, v in list(R_ob[b].rs.items()):
            if sk.startswith("d_"):
                k.rec["sp"].append(("w", sk, v))
    ar.release(m0)


Q0, KV0, GT0, HQ0, HF0, HI0, HG0, MG0 = 0, 512, 1280, 1304, 1816, 2328, 2840, 3352


def _partner(d):
    return d + 8 if d < 8 else (d - 8 if d < 16 else d)


def _rope_tables(pos):
    pos = np.asarray(pos, dtype=np.float32)
    inv = (np.float32(500000.0) ** (-np.arange(8, dtype=np.float32) / np.float32(8))).astype(np.float32)
    ang = (pos[None, :] * inv[:, None]).astype(np.float32)
    cs, sn = np.cos(ang).astype(np.float32), np.sin(ang).astype(np.float32)
    C = np.ones((64, len(pos)), np.float32)
    S = np.zeros((64, len(pos)), np.float32)
    C[0:8], C[8:16] = cs, cs
    S[0:8], S[8:16] = -sn, sn
    return C, S


def attn_input_specs():
    return [
        ("g_attn", (D,), F32), ("g_hg4", (512,), F32),
        ("w1f", (D, 1280), F32), ("w1t", (D, 768), F32),
        ("w2f", (D, 2048), F32), ("w2t", (D, 1536), F32),
        ("wck", (64, 2048), F32), ("wckp", (64, 2048), F32), ("wcv", (64, 2048), F32),
        ("posT", (128, 32), F32), ("lbl", (2, 512), F32),
        ("w_mg", (D, 2048), F32), ("w_brn", (512, D), F32), ("w_brh", (512, D), F32), ("w_out", (D, D), F32),
        ("CK", (128, SEQ), F32), ("SK", (128, SEQ), F32), ("CKc", (128, 512), F32), ("SKc", (128, 512), F32),
        ("CQ", (128, NTOK), F32), ("SQ", (128, NTOK), F32),
        ("ovl", (128, 4, 128), F32),
        ("CB", (128, NT, 128), F32), ("CM", (128, 4, 128), F32), ("WMT", (128, 8, 128), F32),
        ("VAL", (128, NT, 128), F32), ("ADDC", (128, NT, 128), F32),
        ("tri", (128, 128), F32), ("I4", (128, 512), F32), ("onehot", (128, 4), F32),
    ]


_TAB_CACHE = {}


def _const_tables(cp):
    if cp in _TAB_CACHE:
        return _TAB_CACHE[cp]
    m = {}
    C, S = _rope_tables(np.arange(SEQ))
    m["CK"], m["SK"] = np.concatenate([C, C], 0), np.concatenate([S, S], 0)
    C, S = _rope_tables(np.maximum(16 * (np.arange(512) - 1), 0))
    m["CKc"], m["SKc"] = np.concatenate([C, C], 0), np.concatenate([S, S], 0)
    tpos = (128 * (4 * np.arange(NT)[:, None] + cp) + np.arange(128)[None, :])
    C, S = _rope_tables(tpos.reshape(-1))
    m["CQ"] = np.concatenate([C, C], 0) * np.float32(0.125)
    m["SQ"] = np.concatenate([S, S], 0) * np.float32(0.125)
    n = np.arange(512) - 1
    cs, ce = 16 * n, 16 * n + 31
    ss = 64 * np.arange(128)
    ov = ((cs[:, None] < ss[None, :] + 64) & (ce[:, None] >= ss[None, :]) & (n[:, None] >= 0)).astype(np.float32)
    m["ovl"] = np.ascontiguousarray(ov.reshape(4, 128, 128).transpose(1, 0, 2))
    mt = (np.arange(NT) // 4)
    mm = mt[:, None] * 128 + np.arange(128)[None, :]
    nn = mm - 1
    okc = (nn[:, None, :] >= 0) & (16 * nn[:, None, :] + 31 <= tpos[:, :, None])
    m["CB"] = np.ascontiguousarray(np.where(okc, 0.0, NEG).astype(np.float32).transpose(1, 0, 2))
    blk = np.arange(128)
    jq = tpos // 64
    force = (blk[None, None, :] == jq[:, :, None]) | (blk[None, None, :] == 0)
    valid = (64 * blk[None, None, :] <= tpos[:, :, None])
    m["VAL"] = np.ascontiguousarray((valid & ~force).astype(np.float32).transpose(1, 0, 2))
    m["ADDC"] = np.ascontiguousarray(np.where(force, 1e4, np.where(valid, 0.0, -1.0)).astype(np.float32).transpose(1, 0, 2))
    t = np.arange(128)[:, None]
    p = np.arange(128)[None, :]
    caus = np.where(p <= t, 0.0, NEG).astype(np.float32)
    anti = np.where(p > t, 0.0, NEG).astype(np.float32)
    cm = np.zeros((128, 4, 128), np.float32)
    for r in range(4):
        cm[:, r, :] = 0.0 if r < cp else (caus if r == cp else NEG)
    m["CM"] = cm
    wm = np.zeros((128, 8, 128), np.float32)
    for r in range(8):
        dk = cp + 4 - r
        wm[:, r, :] = NEG if (dk < 0 or dk > 4) else (caus if dk == 0 else (anti if dk == 4 else 0.0))
    m["WMT"] = wm
    m["tri"] = (np.arange(128)[:, None] <= np.arange(128)[None, :]).astype(np.float32)
    m["I4"] = np.tile(np.eye(128, dtype=np.float32), (1, 4))
    oh = np.zeros((128, 4), np.float32)
    oh[:, cp] = 1.0
    m["onehot"] = oh
    _TAB_CACHE[cp] = m
    return m


def attn_host_inputs(inp, b, cp):
    m = dict(_const_tables(cp))
    w = inp["w_in"][0]
    pp = np.array([g * 64 + _partner(d) for g in range(2) for d in range(64)])
    kv = lambda s: KV0 + s * 128 + np.arange(128)
    hfc = HF0 + np.arange(512)
    m["w1f"] = np.ascontiguousarray(np.concatenate(
        [w[:, kv(0)], w[:, kv(1)], w[:, kv(2)], w[:, kv(2)[pp]], w[:, kv(4)], w[:, kv(4)[pp]], w[:, hfc]], axis=1))
    m["w1t"] = np.ascontiguousarray(np.concatenate([w[:, kv(3)], w[:, kv(5)], w[:, HI0:HI0 + 512]], axis=1))
    qcols, qpcols = [], []
    for a in range(4):
        for h in (a, 4 + a):
            qcols += [Q0 + h * 64 + d for d in range(64)]
            qpcols += [Q0 + h * 64 + _partner(d) for d in range(64)]
    m["w2f"] = np.ascontiguousarray(np.concatenate(
        [w[:, qcols], w[:, qpcols], w[:, HQ0:HQ0 + 512], w[:, hfc]], axis=1))
    gpad = np.concatenate([w[:, GT0:GT0 + 24], w[:, GT0:GT0 + 24][:, :0].repeat(1, 1)], axis=1)
    w2t = np.zeros((D, 1536), np.float32)
    w2t[:, 0:512] = w[:, HI0:HI0 + 512]
    w2t[:, 512:1024] = w[:, HG0:HG0 + 512]
    w2t[:, 1024:1048] = w[:, GT0:GT0 + 24]
    m["w2t"] = w2t
    pc = np.array([_partner(d) for d in range(64)])
    dle = lambda w_: np.ascontiguousarray(w_.reshape(32, 64, 64).transpose(1, 0, 2).reshape(64, 2048))
    m["wck"] = dle(inp["w_cmp_k"][0])
    m["wckp"] = dle(inp["w_cmp_k"][0][:, pc])
    m["wcv"] = dle(inp["w_cmp_v"][0])
    pT = np.ascontiguousarray(inp["cmp_pos"][0].T)
    m["posT"] = np.concatenate([pT, pT], 0)
    m["lbl"] = np.ascontiguousarray(inp["hg_lb_logits"])
    m["g_attn"] = np.ascontiguousarray(inp["attn_norm"][0])
    m["g_hg4"] = np.ascontiguousarray(np.tile(inp["hg_norm"][0], 4))
    m["w_mg"] = np.ascontiguousarray(w[:, MG0:MG0 + 2048])
    m["w_brn"] = np.ascontiguousarray(inp["w_br_nsa"][0])
    m["w_brh"] = np.ascontiguousarray(inp["w_br_hg"][0])
    m["w_out"] = np.ascontiguousarray(inp["w_out"][0])
    return m


def norm_transpose_group(k, c, W, src_dram, row0, hT, R_hT):
    def s1(tt):
        b = tt % 2
        k.dma("sp", W.xt[b], src_dram[row0 + tt * 128: row0 + (tt + 1) * 128, :], writes=[W.R_xt[b]])
        rstd, R_r = rms_rstd(k, c, W.xt[b], W.R_xt[b], W.scr, W.R_scr, D, "an")
        k.op("dve", lambda e: e.scalar_tensor_tensor(
            out=W.hb[b], in0=W.xt[b], scalar=rstd, in1=W.gA, op0=ALU.mult, op1=ALU.mult),
            reads=[W.R_xt[b], R_r, W.R_gA], writes=[W.R_hb[b]])
        pb = c.psum[b].bitcast(BF16)
        k.op("pe", [(lambda e, i=i: e.transpose(out=pb[:, i * 128:(i + 1) * 128],
                                                in_=W.hb[b][:, i * 128:(i + 1) * 128], identity=c.identb))
                    for i in range(8)], reads=[W.R_hb[b], c.R_const], writes=[c.R_ps[b]])

    def s2(tt):
        b = tt % 2
        pb = c.psum[b].bitcast(BF16)
        k.op("act", lambda e: e.activation(out=hT[:, :, tt * 128:(tt + 1) * 128],
                                           in_=pb.rearrange("p (a b) -> p a b", a=8), func=AF.Copy),
             reads=[c.R_ps[b]], writes=[R_hT])
    s1(0)
    s1(1)
    s2(0)
    s1(2)
    s2(1)
    s1(3)
    s2(2)
    s2(3)


def f_front(k, c, W, fl_ps, R_fl, hd):
    u, a, bq, lk, L, RF = W.sets[hd % 2]
    k.op("act", lambda e: e.activation(out=u, in_=fl_ps, func=AF.Exp, scale=-1.0), reads=[R_fl], writes=[RF])
    k.op("act", lambda e: e.activation(out=a, in_=u, func=AF.Ln, scale=c.lbv[:, hd:hd + 1], bias=c.one_col[:, 0:1]),
         reads=[RF, c.R_const], writes=[RF])
    k.op("act", lambda e: e.activation(out=bq, in_=u, func=AF.Ln, bias=c.one_col[:, 0:1]), reads=[RF, c.R_const], writes=[RF])
    k.op("dve", lambda e: e.scalar_tensor_tensor(out=lk, in0=fl_ps, scalar=-1.0, in1=bq, op0=ALU.mult, op1=ALU.subtract),
         reads=[R_fl, RF], writes=[RF])
    for tt in range(4):
        sl = slice(tt * 128, (tt + 1) * 128)
        k.op("dve", lambda e, sl=sl: e.tensor_tensor_scan(out=L[:, sl], data0=a[:, sl], data1=bq[:, sl], initial=0.0,
                                                          op0=ALU.add, op1=ALU.subtract), reads=[RF], writes=[RF])
    k.op("pool", lambda e: e.tensor_tensor(out=lk, in0=lk, in1=L, op=ALU.subtract), reads=[RF], writes=[RF])


def f_back(k, c, W, hd, H=None):
    u, a, bq, lk, L, RF = W.sets[hd % 2]
    W_, W = W, (H if H is not None else W)
    Lr = L.rearrange("p (t x) -> p t x", t=4)
    rcol, ecol = Lr[:, :, 63], Lr[:, :, 127]
    k.op("dve", lambda e: e.tensor_scalar(out=W.rb[:, hd, :], in0=rcol, scalar1=c.l1mlb[:, hd:hd + 1], scalar2=None, op0=ALU.add),
         reads=[RF, c.R_const], writes=[W.R_cols])
    k.op("dve", lambda e: e.tensor_scalar(out=W.negr[:, hd, :], in0=rcol, scalar1=-1.0, scalar2=None, op0=ALU.mult),
         reads=[RF], writes=[W.R_cols])
    k.op("dve", lambda e: e.tensor_tensor(out=W.dl[:, hd, :], in0=ecol, in1=rcol, op=ALU.subtract), reads=[RF], writes=[W.R_cols])
    k.op("act", lambda e: e.activation(out=W.c1[:, hd, :], in_=ecol, func=AF.Exp), reads=[RF], writes=[W.R_cols])
    k.op("act", lambda e: e.activation(out=W.c2[:, hd, :], in_=W.dl[:, hd, :], func=AF.Exp), reads=[W.R_cols], writes=[W.R_cols])
    k.op("act", lambda e: e.activation(out=W.er[:, hd, :], in_=rcol, func=AF.Exp), reads=[RF], writes=[W.R_cols])
    for tt in range(4):
        sl = slice(tt * 128, (tt + 1) * 128)
        k.op("act", lambda e, sl=sl, tt=tt: e.activation(out=W.kT[:, hd, sl], in_=lk[:, sl], func=AF.Exp, bias=W.rb[:, hd, tt:tt + 1]),
             reads=[RF, W.R_cols], writes=[W.R_kT])


def setup_lb(k, c, ar):
    Dr = c.D
    c.lbv = ar.al([128, 4], F32)
    c.l1mlb = ar.al([128, 4], F32)
    c.one_col = ar.al([128, 1], F32)
    c.ones128 = ar.al([128, 128], F32)
    l0 = ar.al([128, 4], F32)
    l1 = ar.al([128, 4], F32)
    R = c.R_const
    k.dma("sp", l0, Dr["lbl"][0].rearrange("(h p) -> p h", p=128), writes=[R], allow_slow_non_contiguous=True)
    k.dma("sp", l1, Dr["lbl"][1].rearrange("(h p) -> p h", p=128), writes=[R], allow_slow_non_contiguous=True)
    k.op("dve", lambda e: e.memset(c.one_col, 1.0), writes=[R])
    k.op("dve", lambda e: e.memset(c.ones128, 1.0), writes=[R])
    k.op("dve", lambda e: e.tensor_tensor(out=l1, in0=l1, in1=l0, op=ALU.subtract), reads=[R], writes=[R])
    k.op("act", lambda e: e.activation(out=l0, in_=l1, func=AF.Exp), reads=[R], writes=[R])
    k.op("dve", lambda e: e.tensor_scalar(out=l0, in0=l0, scalar1=1.0, scalar2=None, op0=ALU.add), reads=[R], writes=[R])
    k.op("dve", lambda e: e.reciprocal(out=c.lbv, in_=l0), reads=[R], writes=[R])
    k.op("act", lambda e: e.activation(out=l0, in_=l0, func=AF.Ln), reads=[R], writes=[R])
    k.op("dve", lambda e: e.tensor_tensor(out=c.l1mlb, in0=l1, in1=l0, op=ALU.subtract), reads=[R], writes=[R])


class WS:
    pass


def alloc_hg_ws(k, ar, W, nsets=1):
    W.sets = []
    for si in range(nsets):
        blk = ar.al([128, 5, 512], F32)
        W.sets.append(tuple(blk[:, i, :] for i in range(5)) + (k.res("fchain%d" % si),))
        if si == 0:
            W.ab = blk[:, 1:3, :].rearrange("p a b -> p (a b)")
    if nsets == 1:
        W.sets.append(W.sets[0])
    W.u, W.a, W.bq, W.lk, W.L, W.R_f = W.sets[0]
    alloc_hslot(k, ar, W, "0")


def alloc_hslot(k, ar, H, tag):
    H.rb, H.negr, H.dl, H.c1, H.c2, H.er = [ar.al([128, 4, 4], F32) for _ in range(6)]
    H.R_cols = k.res("fcols" + tag)
    H.kT = ar.al([128, 4, 512], BF16)
    H.R_kT = k.res("kT" + tag)


def alloc_x_ws(k, c, ar, W, region, scr=None, R_scr=None):
    if region is not None:
        W.xt = [region[:, 0, :].bitcast(F32), region[:, 1, :].bitcast(F32)]
        W.hb = [region[:, 2, 0:1024], region[:, 2, 1024:2048]]
        W.scr = region[:, 3, :].bitcast(F32)
        W.R_scr = k.res("xscr")
    else:
        W.xt = [ar.al([128, D], F32) for _ in range(2)]
        W.hb = [ar.al([128, D], BF16) for _ in range(2)]
        W.scr, W.R_scr = scr, R_scr
    W.R_xt = [k.res("xt0"), k.res("xt1")]
    W.R_hb = [k.res("hb0"), k.res("hb1")]
    W.gA = ar.al([128, D], F32)
    W.R_gA = k.res("gA")
    k.dma("sp", W.gA, c.D["g_attn"].partition_broadcast(128), writes=[W.R_gA])


def phase_p1(k, c, ar, St):
    Dr = c.D
    m0 = ar.mark()
    W = WS()
    alloc_x_ws(k, c, ar, W, c.oT_hg)
    w1f, w1t = c.R32[:, :, 0:1280], c.R32[:, :, 1280:2048]
    R_w1 = k.res("w1")
    k.dma("pool", w1f, Dr["w1f"].rearrange("(kt p) n -> p kt n", p=128), writes=[R_w1])
    k.dma("pool", w1t, Dr["w1t"].rearrange("(kt p) n -> p kt n", p=128), writes=[R_w1])
    hT = ar.al([128, 8, 512], BF16)
    R_hT = k.res("hT")
    CKg, SKg = ar.al([128, 512], F32), ar.al([128, 512], F32)
    R_rt = k.res("ropetab")
    alloc_hg_ws(k, ar, W, nsets=2)
    t1, t2, R_t12 = W.u, W.a, W.R_f
    vtok = ar.al([128, 4, 512], BF16)
    R_vtok = k.res("vtok")
    ktok = ar.al([128, 4, 128], BF16)
    R_ktok = k.res("ktok")
    Sst = ar.al([128, 4, 128], F32)
    snapacc = ar.al([128, 4, 128], F32)
    R_S, R_snapacc = k.res("S"), k.res("snapacc")
    WC = [ar.al([128, 32, 64], BF16) for _ in range(3)]
    R_WC = k.res("WC")
    posT = ar.al([128, 32], BF16)
    cb = ar.al([128, 4], F32)
    xin = [[ar.al([128, 528], BF16) for _ in range(2)] for _ in range(2)]
    R_xin = [[k.res("xin%d%d" % (a, b)) for b in range(2)] for a in range(2)]
    CKc, SKc = ar.al([128, 32], F32), ar.al([128, 32], F32)
    R_ckc = k.res("ckc")
    VCf = ar.al([128, 512], F32)
    R_VCf = k.res("VCf")
    ctmp = ar.al([128, 4, 32], F32)
    R_ctmp = k.res("ctmp")
    for xi, nm in enumerate(("wck", "wckp", "wcv")):
        for g in range(2):
            k.dma("pool", WC[xi][64 * g:64 * g + 64].rearrange("p l e -> p (l e)"), Dr[nm], writes=[R_WC])
    k.dma("pool", posT, Dr["posT"], writes=[R_WC])
    k.op("dve", lambda e: e.memset(Sst, 0.0), writes=[R_S])
    k.op("dve", lambda e: e.memset(St.VsA[:, :, :, 64:65], 1.0), writes=[St.R_VsA])
    k.op("dve", lambda e: e.memset(St.VwA[:, :, :, 64:65], 1.0), writes=[St.R_VwA])
    for a in range(2):
        k.op("dve", lambda e, a=a: e.memset(xin[a][0][:, 0:16], 0.0), writes=[R_xin[a][0]])
    p6 = c.psum[6]
    fns = []
    for xi in range(3):
        for g in range(2):
            for l in range(32):
                fns.append(lambda e, xi=xi, g=g, l=l: e.matmul(p6[64 * g:64 * g + 64, xi:xi + 1], lhsT=WC[xi][64 * g:64 * g + 64, l, :],
                                                               rhs=posT[64 * g:64 * g + 64, l:l + 1], start=(l == 0), stop=(l == 31)))
    k.op("pe", fns, reads=[R_WC], writes=[c.R_ps[6]])
    k.op("dve", lambda e: e.tensor_copy(out=cb[:, 0:3], in_=p6[:, 0:3]), reads=[c.R_ps[6]], writes=[R_WC])

    NG = c.n_groups
    Hs = [W, W]
    vtoks, R_vtoks = [vtok, vtok], [R_vtok, R_vtok]
    p6b = c.psum[6].bitcast(BF16)

    def fm(ft, bank):
        k.op("pe", [(lambda e, i=i: e.matmul(c.psum[bank], lhsT=w1f[:, i, ft * 128:(ft + 1) * 128], rhs=hT[:, i, :],
                                             start=(i == 0), stop=(i == 7))) for i in range(8)],
             reads=[R_w1, R_hT], writes=[c.R_ps[bank]])

    def A_x(G):
        norm_transpose_group(k, c, W, Dr["xb"], G * 512, hT, R_hT)
        k.dma("sp", CKg, Dr["CK"][:, G * 512:(G + 1) * 512], writes=[R_rt])
        k.dma("sp", SKg, Dr["SK"][:, G * 512:(G + 1) * 512], writes=[R_rt])

    def A_kv(G):
        xb_ = G % 2
        for a in range(2):
            fm(a, 2 + a)
            k.op("act", lambda e, a=a: e.activation(out=xin[a][xb_][:, 16:528], in_=c.psum[2 + a], func=AF.Copy),
                 reads=[c.R_ps[2 + a]], writes=[R_xin[a][xb_]])
            k.op("pool", lambda e, a=a: e.tensor_copy(out=xin[a][1 - xb_][:, 0:16], in_=xin[a][xb_][:, 512:528]),
                 reads=[R_xin[a][xb_]], writes=[R_xin[a][1 - xb_]])
        for which, dst, R_dst in ((0, St.KTs, St.R_KTs), (1, St.KTw, St.R_KTw)):
            fm(2 + 2 * which, 2)
            fm(3 + 2 * which, 3)
            k.op("dve", lambda e: e.tensor_tensor(out=t1, in0=c.psum[2], in1=CKg, op=ALU.mult), reads=[c.R_ps[2], R_rt], writes=[R_t12])
            k.op("dve", lambda e: e.tensor_tensor(out=t2, in0=c.psum[3], in1=SKg, op=ALU.mult), reads=[c.R_ps[3], R_rt, R_t12], writes=[R_t12])
            k.op("pool", lambda e, dst=dst: e.tensor_tensor(out=dst[:, G * 512:(G + 1) * 512], in0=t1, in1=t2, op=ALU.add),
                 reads=[R_t12], writes=[R_dst])

    def A_tok(G):
        vt, R_vt = vtoks[G % 2], R_vtoks[G % 2]
        for tt in range(4):
            tile_ = 4 * G + tt
            k.op("pe", [(lambda e, i=i, tt=tt: e.matmul(c.psum[4][:, 0:256], lhsT=hT[:, i, tt * 128:(tt + 1) * 128], rhs=w1t[:, i, 0:256],
                                                        start=(i == 0), stop=(i == 7))) for i in range(8)],
                 reads=[R_w1, R_hT], writes=[c.R_ps[4]])
            k.op("pe", [(lambda e, i=i, tt=tt: e.matmul(c.psum[5], lhsT=hT[:, i, tt * 128:(tt + 1) * 128], rhs=w1t[:, i, 256:768],
                                                        start=(i == 0), stop=(i == 7))) for i in range(8)],
                 reads=[R_w1, R_hT], writes=[c.R_ps[5]])
            k.op("act", lambda e, tile_=tile_: e.activation(out=St.VsA[:, tile_, :, 0:64],
                                                            in_=c.psum[4][:, 0:128].rearrange("p (g d) -> p g d", g=2), func=AF.Copy),
                 reads=[c.R_ps[4]], writes=[St.R_VsA])
            k.op("act", lambda e, tile_=tile_: e.activation(out=St.VwA[:, tile_, :, 0:64],
                                                            in_=c.psum[4][:, 128:256].rearrange("p (g d) -> p g d", g=2), func=AF.Copy),
                 reads=[c.R_ps[4]], writes=[St.R_VwA])
            k.op("dve", lambda e, tt=tt: e.tensor_copy(out=vt[:, tt, :], in_=c.psum[5]), reads=[c.R_ps[5]], writes=[R_vt])

    def A_conv(G):
        xb_ = G % 2
        fns = []
        for xi in range(3):
            src = xin[0][xb_] if xi < 2 else xin[1][xb_]
            for l in range(32):
                for g in range(2):
                    fns.append(lambda e, xi=xi, g=g, l=l, src=src: e.matmul(
                        p6[64 * g:64 * g + 64, 32 * xi:32 * xi + 32], lhsT=WC[xi][64 * g:64 * g + 64, l, :],
                        rhs=src[64 * g:64 * g + 64, l:l + 497:16], start=(l == 0), stop=(l == 31)))
        k.op("pe", fns, reads=[R_WC, R_xin[0][xb_], R_xin[1][xb_]], writes=[c.R_ps[6]])
        ms = slice(32 * G, 32 * G + 32)
        k.dma("sp", CKc, Dr["CKc"][:, ms], writes=[R_ckc])
        k.dma("sp", SKc, Dr["SKc"][:, ms], writes=[R_ckc])
        k.op("dve", lambda e: e.tensor_scalar(out=ctmp[:, 0, :], in0=p6[:, 0:32], scalar1=cb[:, 0:1], scalar2=None, op0=ALU.add),
             reads=[c.R_ps[6], R_WC], writes=[R_ctmp])
        k.op("dve", lambda e: e.tensor_scalar(out=ctmp[:, 1, :], in0=p6[:, 32:64], scalar1=cb[:, 1:2], scalar2=None, op0=ALU.add),
             reads=[c.R_ps[6], R_WC], writes=[R_ctmp])
        k.op("dve", lambda e: e.tensor_scalar(out=VCf[:, ms], in0=p6[:, 64:96], scalar1=cb[:, 2:3], scalar2=None, op0=ALU.add),
             reads=[c.R_ps[6], R_WC], writes=[R_VCf])
        k.op("pool", lambda e: e.tensor_tensor(out=ctmp[:, 0, :], in0=ctmp[:, 0, :], in1=CKc, op=ALU.mult),
             reads=[R_ctmp, R_ckc], writes=[R_ctmp])
        k.op("pool", lambda e: e.tensor_tensor(out=ctmp[:, 1, :], in0=ctmp[:, 1, :], in1=SKc, op=ALU.mult),
             reads=[R_ctmp, R_ckc], writes=[R_ctmp])
        k.op("pool", lambda e: e.tensor_tensor(out=St.KC[:, ms], in0=ctmp[:, 0, :], in1=ctmp[:, 1, :], op=ALU.add),
             reads=[R_ctmp], writes=[St.R_KC])

    def A_f(G):
        H = Hs[G % 2]

        def front(hd):
            bank = 2 + hd % 2
            fm(6 + hd, bank)
            f_front(k, c, W, c.psum[bank], c.R_ps[bank], hd)
        front(0)
        front(1)
        f_back(k, c, W, 0, H)
        front(2)
        f_back(k, c, W, 1, H)
        front(3)
        f_back(k, c, W, 2, H)
        f_back(k, c, W, 3, H)

    def B_step(G, tt):
        H = Hs[G % 2]
        vt, R_vt = vtoks[G % 2], R_vtoks[G % 2]
        sl = slice(tt * 128, (tt + 1) * 128)
        k.op("pe", [(lambda e, hd=hd: e.transpose(out=p6b[:, hd * 128:(hd + 1) * 128], in_=H.kT[:, hd, sl], identity=c.identb))
                    for hd in range(4)], reads=[H.R_kT, c.R_const], writes=[c.R_ps[6]])
        k.op("act", lambda e: e.activation(out=ktok, in_=p6b[:, 0:512].rearrange("p (h x) -> p h x", h=4), func=AF.Copy),
             reads=[c.R_ps[6]], writes=[R_ktok])
        k.op("pe", [(lambda e, hd=hd: e.matmul(c.psum[7][:, hd * 128:(hd + 1) * 128], lhsT=ktok[:, hd, :],
                                               rhs=vt[:, tt, hd * 128:(hd + 1) * 128], start=True, stop=True))
                    for hd in range(4)], reads=[R_ktok, R_vt], writes=[c.R_ps[7]])
        Sf, Af = Sst.rearrange("p h x -> p (h x)"), snapacc.rearrange("p h x -> p (h x)")
        if tt == 0:
            k.op("dve", lambda e: e.tensor_scalar(out=Af, in0=Sf, scalar1=c.onehot[:, 0:1], scalar2=None, op0=ALU.mult),
                 reads=[R_S, c.R_const], writes=[R_snapacc])
        else:
            k.op("dve", lambda e: e.scalar_tensor_tensor(out=Af, in0=Sf, scalar=c.onehot[:, tt:tt + 1], in1=Af,
                                                         op0=ALU.mult, op1=ALU.add),
                 reads=[R_S, c.R_const, R_snapacc], writes=[R_snapacc])
        for hd in range(4):
            k.op("dve", lambda e, hd=hd: e.tensor_scalar(out=Sst[:, hd, :], in0=Sst[:, hd, :], scalar1=H.c1[:, hd, tt:tt + 1],
                                                         scalar2=None, op0=ALU.mult),
                 reads=[R_S, H.R_cols], writes=[R_S])
            k.op("dve", lambda e, hd=hd: e.scalar_tensor_tensor(
                out=Sst[:, hd, :], in0=c.psum[7][:, hd * 128:(hd + 1) * 128], scalar=H.c2[:, hd, tt:tt + 1], in1=Sst[:, hd, :],
                op0=ALU.mult, op1=ALU.add), reads=[c.R_ps[7], R_S, H.R_cols], writes=[R_S])
        if tt == 3:
            k.op("act", lambda e: e.activation(out=St.SNAP[:, G, :, :], in_=snapacc, func=AF.Copy), reads=[R_snapacc], writes=[St.R_SNAP])

    for G in range(NG + 1):
        if G < NG:
            A_x(G)
        if G >= 1:
            B_step(G - 1, 0)
            B_step(G - 1, 1)
        if G < NG:
            A_kv(G)
        if G >= 1:
            B_step(G - 1, 2)
            B_step(G - 1, 3)
        if G < NG:
            A_tok(G)
            A_conv(G)
            A_f(G)
    k.op("dve", lambda e: e.memset(St.VCA[:, :, :, 64:65], 1.0), writes=[St.R_VCA])
    for g in range(2):
        k.dma("pool", St.VCA[:, :, g, 65:193], Dr["ovl"], writes=[St.R_VCA])
    pw = c.psum[6]
    k.op("pe", [(lambda e, mt=mt: e.transpose(out=pw[:, mt * 128:(mt + 1) * 128], in_=VCf[:, mt * 128:(mt + 1) * 128], identity=c.identf))
                for mt in range(4)], reads=[R_VCf, c.R_const], writes=[c.R_ps[6]])
    for mt in range(4):
        k.op("act", lambda e, mt=mt: e.activation(out=St.VCA[:, mt, :, 0:64],
                                                  in_=pw[:, mt * 128:(mt + 1) * 128].rearrange("p (g d) -> p g d", g=2), func=AF.Copy),
             reads=[c.R_ps[6]], writes=[St.R_VCA])
    k.op("dve", lambda e: e.memset(St.VCA[0:1, 0, :, :], 0.0), writes=[St.R_VCA])
    ar.release(m0)


def phase_p2pre(k, c, ar, St):
    Dr = c.D
    m0 = ar.mark()
    W = WS()
    alloc_hg_ws(k, ar, W, nsets=2)
    alloc_x_ws(k, c, ar, W, None, scr=W.ab, R_scr=W.R_f)
    hT = ar.al([128, 8, 512], BF16)
    R_hT = k.res("hT2")
    wch = [c.R32f[:, 8192 + b * 4096: 8192 + (b + 1) * 4096].rearrange("p (a b) -> p a b", a=8) for b in range(2)]
    R_wch = [k.res("wch%d" % i) for i in range(2)]
    wgt = ar.al([128, 8, 32], BF16)
    R_wgt = k.res("wgt")
    wst = dict(n=0)
    CQg, SQg = ar.al([128, 512], F32), ar.al([128, 512], F32)
    R_rt = k.res("ropetabq")
    t1, t2, R_t12 = W.u, W.a, W.R_f
    qT = ar.al([128, 4, 512], BF16)
    R_qT = k.res("qTh")
    e1, R_e1 = W.u, W.R_f
    vtoks = [ar.al([128, 512], BF16) for _ in range(2)]
    R_vtoks = [k.res("vtok2_%d" % i) for i in range(2)]
    sgts = [ar.al([128, 512], F32) for _ in range(2)]
    R_sgts = [k.res("sgt%d" % i) for i in range(2)]
    AT = ar.al([128, 4, 128], BF16)
    R_AT = k.res("AT")
    Sp = ar.al([128, 4, 128], BF16)
    R_Sp = k.res("Sp")
    gnt = ar.al([128, 512], F32)
    R_gnt = k.res("gnt")
    o1, o2, R_o = W.bq, W.a, W.R_f
    yb = ar.al([128, 512], BF16)
    R_yb = k.res("yb")
    hs = ar.al([128, 16], F32)
    R_hs = k.res("hs")
    k.dma("pool", wgt, Dr["w2t"][:, 1024:1056].rearrange("(kt p) n -> p kt n", p=128), writes=[R_wgt])
    k.dma("sp", gnt, Dr["g_hg4"].partition_broadcast(128), writes=[R_gnt])

    def wload(src, c0, n=512):
        b = wst["n"] % 2
        wst["n"] += 1
        k.dma("pool", wch[b][:, :, 0:n], src[:, c0:c0 + n].rearrange("(kt p) n -> p kt n", p=128), writes=[R_wch[b]])
        return wch[b], R_wch[b]

    for go in range(NT // 4):
        tok = slice(go * 512, (go + 1) * 512)
        norm_transpose_group(k, c, W, Dr["xo"], go * 512, hT, R_hT)
        k.dma("sp", CQg, Dr["CQ"][:, tok], writes=[R_rt])
        k.dma("sp", SQg, Dr["SQ"][:, tok], writes=[R_rt])

        def fm(wt, R_wt, j, bank):
            k.op("pe", [(lambda e, i=i: e.matmul(c.psum[bank], lhsT=wt[:, i, j * 128:(j + 1) * 128], rhs=hT[:, i, :],
                                                 start=(i == 0), stop=(i == 7))) for i in range(8)],
                 reads=[R_wt, R_hT], writes=[c.R_ps[bank]])
        wq, R_wq = wload(Dr["w2f"], 0)
        wqp, R_wqp = wload(Dr["w2f"], 512)
        for a in range(4):
            fm(wq, R_wq, a, 2)
            fm(wqp, R_wqp, a, 3)
            k.op("dve", lambda e: e.tensor_tensor(out=t1, in0=c.psum[2], in1=CQg, op=ALU.mult), reads=[c.R_ps[2], R_rt], writes=[R_t12])
            k.op("dve", lambda e: e.tensor_tensor(out=t2, in0=c.psum[3], in1=SQg, op=ALU.mult), reads=[c.R_ps[3], R_rt, R_t12], writes=[R_t12])
            k.op("pool", lambda e, a=a: e.tensor_tensor(out=c.QT[:, 4 * go:4 * go + 4, a, :], in0=t1.rearrange("p (i t) -> p i t", i=4),
                                                        in1=t2.rearrange("p (i t) -> p i t", i=4), op=ALU.add), reads=[R_t12], writes=[c.R_QT])
        whq, R_whq = wload(Dr["w2f"], 1024)
        whf, R_whf = wload(Dr["w2f"], 1536)
        def front(hd):
            bank = 2 + hd % 2
            fm(whf, R_whf, hd, bank)
            f_front(k, c, W, c.psum[bank], c.R_ps[bank], hd)

        def back(hd):
            f_back(k, c, W, hd)
            su, sa, sbq, slk, sL, sRF = W.sets[hd % 2]
            fm(whq, R_whq, hd, 6)
            for tt in range(4):
                sl = slice(tt * 128, (tt + 1) * 128)
                k.op("act", lambda e, sl=sl, tt=tt: e.activation(out=su[:, sl], in_=sL[:, sl], func=AF.Exp, bias=W.negr[:, hd, tt:tt + 1]),
                     reads=[sRF, W.R_cols], writes=[sRF])
            k.op("dve", lambda e: e.tensor_tensor(out=qT[:, hd, :], in0=c.psum[6], in1=su, op=ALU.mult),
                 reads=[c.R_ps[6], sRF], writes=[R_qT])
        front(0)
        front(1)
        back(0)
        front(2)
        back(1)
        front(3)
        back(2)
        back(3)
        whi, R_whi = wload(Dr["w2t"], 0)
        whg, R_whg = wload(Dr["w2t"], 512)

        def s1(tt):
            i_own = 4 * go + tt
            pb = tt % 2
            sl = slice(tt * 128, (tt + 1) * 128)
            for (wt, R_wt, n, bank) in ((whi, R_whi, 512, pb), (whg, R_whg, 512, 2 + pb), (wgt, R_wgt, 32, 6)):
                k.op("pe", [(lambda e, i=i, wt=wt, n=n, bank=bank: e.matmul(c.psum[bank][:, 0:n], lhsT=hT[:, i, sl], rhs=wt[:, i, 0:n],
                                                                            start=(i == 0), stop=(i == 7))) for i in range(8)],
                     reads=[R_wt, R_hT], writes=[c.R_ps[bank]])
            k.op("dve", lambda e: e.tensor_copy(out=vtoks[pb], in_=c.psum[pb]), reads=[c.R_ps[pb]], writes=[R_vtoks[pb]])
            k.op("act", lambda e: e.activation(out=sgts[pb], in_=c.psum[2 + pb], func=AF.Silu), reads=[c.R_ps[2 + pb]], writes=[R_sgts[pb]])
            k.op("act", lambda e: e.activation(out=c.gsig[:, i_own, :], in_=c.psum[6][:, 0:24], func=AF.Sigmoid),
                 reads=[c.R_ps[6]], writes=[c.R_gsig])

        def s2(tt):
            i_own = 4 * go + tt
            pb = tt % 2
            vtok, R_vtok, sgt, R_sgt = vtoks[pb], R_vtoks[pb], sgts[pb], R_sgts[pb]
            sl = slice(tt * 128, (tt + 1) * 128)
            k.op("pe", [(lambda e, hd=hd: e.matmul(c.psum[7][:, hd * 128:(hd + 1) * 128], lhsT=W.kT[:, hd, sl], rhs=qT[:, hd, sl],
                                                   start=True, stop=True)) for hd in range(4)],
                 reads=[W.R_kT, R_qT], writes=[c.R_ps[7]])
            k.op("dve", lambda e: e.tensor_scalar(out=W.lk, in0=c.psum[7], scalar1=1e30, scalar2=-1e30, op0=ALU.min, op1=ALU.max),
                 reads=[c.R_ps[7], W.R_f], writes=[W.R_f])
            k.op("dve", lambda e: e.tensor_tensor(out=AT, in0=W.lk.rearrange("p (h x) -> p h x", h=4),
                                                  in1=c.tri.unsqueeze(1).broadcast_to([128, 4, 128]), op=ALU.mult),
                 reads=[W.R_f, c.R_constP], writes=[R_AT])
            for hd in range(4):
                k.op("act", lambda e, hd=hd: e.activation(out=Sp[:, hd, :], in_=St.SNAP[:, i_own, hd, :], func=AF.Copy,
                                                          scale=W.er[:, hd, tt:tt + 1]),
                     reads=[St.R_SNAP, W.R_cols], writes=[R_Sp])
            fns = []
            for hd in range(4):
                fns.append(lambda e, hd=hd: e.matmul(c.psum[4][:, hd * 128:(hd + 1) * 128], lhsT=AT[:, hd, :],
                                                     rhs=vtok[:, hd * 128:(hd + 1) * 128], start=True, stop=False))
                fns.append(lambda e, hd=hd: e.matmul(c.psum[4][:, hd * 128:(hd + 1) * 128], lhsT=qT[:, hd, sl],
                                                     rhs=Sp[:, hd, :], start=False, stop=True))
            k.op("pe", fns, reads=[R_AT, R_vtok, R_qT, R_Sp], writes=[c.R_ps[4]])
            for hd in range(4):
                k.op("act", lambda e, hd=hd: e.activation(out=o2[:, hd * 128:(hd + 1) * 128], in_=c.psum[4][:, hd * 128:(hd + 1) * 128],
                                                          func=AF.Square, accum_out=hs[:, hd:hd + 1]),
                     reads=[c.R_ps[4]], writes=[R_o, R_hs])
            k.op("act", lambda e: e.activation(out=hs[:, 4:8], in_=hs[:, 0:4], func=AF.Sqrt, scale=1.0 / 128, bias=c.epsb[:, 0:1]),
                 reads=[R_hs, c.R_const], writes=[R_hs])
            k.op("dve", lambda e: e.reciprocal(out=hs[:, 8:12], in_=hs[:, 4:8]), reads=[R_hs], writes=[R_hs])
            k.op("dve", lambda e: e.tensor_tensor(out=o1, in0=c.psum[4], in1=gnt, op=ALU.mult), reads=[c.R_ps[4], R_gnt, R_o], writes=[R_o])
            k.op("pool", lambda e: e.tensor_tensor(out=o1, in0=o1, in1=sgt, op=ALU.mult), reads=[R_o, R_sgt], writes=[R_o])
            k.op("dve", lambda e: e.tensor_tensor(out=yb.rearrange("p (h x) -> p h x", h=4), in0=o1.rearrange("p (h x) -> p h x", h=4),
                                                  in1=hs[:, 8:12].unsqueeze(2).broadcast_to([128, 4, 128]), op=ALU.mult),
                 reads=[R_o, R_hs], writes=[R_yb])
            p5b = c.psum[5].bitcast(BF16)
            k.op("pe", [(lambda e, hd=hd: e.transpose(out=p5b[:, hd * 128:(hd + 1) * 128], in_=yb[:, hd * 128:(hd + 1) * 128], identity=c.identb))
                        for hd in range(4)], reads=[R_yb, c.R_const], writes=[c.R_ps[5]])
            k.op("act", lambda e: e.activation(out=c.oT_hg[:, :, i_own * 128:(i_own + 1) * 128],
                                               in_=p5b[:, 0:512].rearrange("p (h x) -> p h x", h=4), func=AF.Copy),
                 reads=[c.R_ps[5]], writes=[c.R_oThg])
        s1(0)
        s1(1)
        s2(0)
        s1(2)
        s2(1)
        s1(3)
        s2(2)
        s2(3)
    ar.release(m0)


def phase_nsa(k, c, ar, St):
    Dr = c.D
    m0 = ar.mark()
    TINY = 1e-30
    CBi = [ar.al([128, 128], BF16) for _ in range(2)]
    VALi = [ar.al([128, 128], F32) for _ in range(2)]
    ADDCi = [ar.al([128, 128], F32) for _ in range(2)]
    R_tab = [k.res("nsatab%d" % i) for i in range(2)]
    R_tabP = [k.res("nsatabP%d" % i) for i in range(2)]
    WMT = ar.al([128, 8, 128], BF16)
    CM = ar.al([128, 4, 128], BF16)
    R_cst = k.res("nsacst")
    k.dma("pool", WMT, Dr["WMT"], writes=[R_cst])
    k.dma("pool", CM, Dr["CM"], writes=[R_cst])
    PT = [ar.al([128, 512], BF16) for _ in range(3)]
    R_PT = [k.res("PT%d" % i) for i in range(3)]
    Uc = ar.al([128, 4, 193], F32)
    R_Uc = k.res("Uc")
    Os = ar.al([128, 4, 65], F32)
    Ow = ar.al([128, 4, 65], F32)
    R_Os, R_Ow = k.res("Os"), k.res("Ow")
    score, sc2, imp = ar.al([128, 128], F32), ar.al([128, 128], F32), ar.al([128, 128], F32)
    R_sel = k.res("sel")
    selb = ar.al([128, 128], BF16)
    R_selb = k.res("selb")
    bd = ar.al([128, 4, 128], BF16)
    R_bd = k.res("bd")
    selX = ar.al([128, 128, 64], BF16)
    R_selX = [k.res("selX0"), k.res("selX1")]
    cols = ar.al([128, 64], F32)
    R_cols = k.res("nsacols")
    acc, tmp = ar.al([128, 4, 64], F32), ar.al([128, 4, 64], F32)
    R_acc = k.res("nsaacc")
    onsa = ar.al([128, 2, 4, 64], BF16)
    R_onsa = k.res("onsa")
    st = dict(s=0, p=0)
    pO_s, pO_w = c.psum[3][:, 0:260], c.psum[4][:, 0:260]
    pU = [c.psum[5], c.psum[6]]

    pend = []

    def flush_pv(keep=0):
        while len(pend) > keep:
            pend.pop(0)()

    def unit(KT, R_KT, kt_slice, QTg, g, bias, Vaug, R_V, outs, R_outs):
        sb = st["s"] % 3
        st["s"] += 1
        pb = st["p"] % 3
        st["p"] += 1
        S = c.psum[sb]
        fns = [lambda e: e.matmul(S, lhsT=KT[64 * g:64 * g + 64, kt_slice], rhs=QTg, start=True, stop=(bias is None))]
        rd = [R_KT, c.R_QT]
        if bias is not None:
            bl, R_bl = bias
            fns.append(lambda e: e.matmul(S, lhsT=bl, rhs=c.I4, start=False, stop=True))
            rd += [R_bl, c.R_constP]
        k.op("pe", fns, reads=rd, writes=[c.R_ps[sb]])
        k.op("act", lambda e: e.activation(out=PT[pb], in_=S, func=AF.Exp), reads=[c.R_ps[sb]], writes=[R_PT[pb]])

        def pv():
            k.op("pe", [(lambda e, a=a: e.matmul(outs[a], lhsT=PT[pb][:, a * 128:(a + 1) * 128], rhs=Vaug, start=False, stop=False,
                                                 skip_group_check=True)) for a in range(4)],
                 reads=[R_PT[pb], R_V], writes=R_outs)
        pend.append(pv)
        flush_pv(keep=2)

    for i in range(c.n_blocks):
        tb = i % 2
        k.dma("pool", CBi[tb], Dr["CB"][:, i, :], writes=[R_tabP[tb]])
        k.dma("sp", VALi[tb], Dr["VAL"][:, i, :], writes=[R_tab[tb]])
        k.dma("sp", ADDCi[tb], Dr["ADDC"][:, i, :], writes=[R_tab[tb]])
        for g in range(2):
            QTg = c.QT[64 * g:64 * g + 64, i, :, :].rearrange("p a t -> p (a t)")
            nmt = i // 4 + 1
            k.op("dve", lambda e: e.memset(pU[0], 0.0), writes=[c.R_ps[5]])
            k.op("dve", lambda e: e.memset(pU[1], 0.0), writes=[c.R_ps[6]])
            outsU = [pU[a // 2][:, (a % 2) * 193:(a % 2) * 193 + 193] for a in range(4)]
            for mt in range(nmt):
                bias = (CBi[tb], R_tabP[tb]) if mt == nmt - 1 else None
                unit(St.KC, St.R_KC, slice(mt * 128, (mt + 1) * 128), QTg, g, bias, St.VCA[:, mt, g, :], St.R_VCA, outsU, [c.R_ps[5], c.R_ps[6]])
            flush_pv()
            k.op("act", lambda e: e.activation(out=Uc[:, 0:2, :], in_=pU[0][:, 0:386].rearrange("p (a x) -> p a x", a=2), func=AF.Copy),
                 reads=[c.R_ps[5]], writes=[R_Uc])
            k.op("act", lambda e: e.activation(out=Uc[:, 2:4, :], in_=pU[1][:, 0:386].rearrange("p (a x) -> p a x", a=2), func=AF.Copy),
                 reads=[c.R_ps[6]], writes=[R_Uc])
            k.op("dve", lambda e: e.memset(c.psum[4], 0.0), writes=[c.R_ps[4]])
            outsW = [pO_w[:, a * 65:(a + 1) * 65] for a in range(4)]
            for r in range(8):
                kt = 4 * i - 4 + r
                if kt < 0:
                    continue
                unit(St.KTw, St.R_KTw, slice(kt * 128, (kt + 1) * 128), QTg, g, (WMT[:, r, :], R_cst), St.VwA[:, kt, g, :], St.R_VwA, outsW, [c.R_ps[4]])
            zc, rzc = cols[:, 0:4], cols[:, 4:8]
            k.op("dve", lambda e: e.tensor_scalar(out=zc, in0=Uc[:, :, 64], scalar1=TINY, scalar2=None, op0=ALU.max), reads=[R_Uc], writes=[R_cols])
            k.op("dve", lambda e: e.reciprocal(out=rzc, in_=zc), reads=[R_cols], writes=[R_cols])
            k.op("dve", lambda e: e.tensor_scalar(out=imp, in0=Uc[:, 0, 65:193], scalar1=rzc[:, 0:1], scalar2=None, op0=ALU.mult),
                 reads=[R_Uc, R_cols], writes=[R_sel])
            for a in range(1, 4):
                k.op("dve", lambda e, a=a: e.scalar_tensor_tensor(out=imp, in0=Uc[:, a, 65:193], scalar=rzc[:, a:a + 1], in1=imp,
                                                                  op0=ALU.mult, op1=ALU.add), reads=[R_Uc, R_cols, R_sel], writes=[R_sel])
            k.op("dve", lambda e: e.tensor_tensor(out=score, in0=imp, in1=VALi[tb], op=ALU.mult), reads=[R_sel, R_tab[tb]], writes=[R_sel])
            k.op("dve", lambda e: e.tensor_tensor(out=score, in0=score, in1=ADDCi[tb], op=ALU.add), reads=[R_sel, R_tab[tb]], writes=[R_sel])
            m8a, m8b = cols[:, 8:16], cols[:, 16:24]
            k.op("dve", lambda e: e.max(out=m8a, in_=score), reads=[R_sel], writes=[R_cols])
            k.op("dve", lambda e: e.match_replace(out=sc2, in_to_replace=m8a, in_values=score, imm_value=-1e9), reads=[R_sel, R_cols], writes=[R_sel])
            k.op("dve", lambda e: e.max(out=m8b, in_=sc2), reads=[R_sel], writes=[R_cols])
            k.op("dve", lambda e: e.tensor_scalar(out=selb, in0=score, scalar1=m8b[:, 7:8], scalar2=NEG, op0=ALU.is_lt, op1=ALU.mult),
                 reads=[R_sel, R_cols], writes=[R_selb])
            for r in range(4):
                kt = 4 * i + r
                k.op("dve", lambda e, r=r, kt=kt: e.tensor_tensor(
                    out=bd[:, r, :].rearrange("p (b x) -> p b x", b=2), in0=CM[:, r, :].rearrange("p (b x) -> p b x", b=2),
                    in1=selb[:, 2 * kt:2 * kt + 2].unsqueeze(2).broadcast_to([128, 2, 64]), op=ALU.add),
                    reads=[R_cst, R_selb], writes=[R_bd])
            if i > 0:
                for hx in range(2):
                    b0_, b1_ = 4 * i * hx, 4 * i * (hx + 1)
                    k.op("dve", lambda e, b0_=b0_, b1_=b1_: e.tensor_copy(
                        out=selX[:, b0_:b1_, :], in_=selb[:, b0_:b1_].unsqueeze(2).broadcast_to([128, b1_ - b0_, 64])),
                        reads=[R_selb], writes=[R_selX[hx]])
            k.op("dve", lambda e: e.memset(c.psum[3], 0.0), writes=[c.R_ps[3]])
            outsS = [pO_s[:, a * 65:(a + 1) * 65] for a in range(4)]
            for kt in list(range(4 * i, 4 * i + 4)) + list(range(4 * i)):
                if kt < 4 * i:
                    bl = selX[:, 2 * kt:2 * kt + 2, :].rearrange("p b x -> p (b x)")
                    bias = (bl, R_selX[0 if kt < 2 * i else 1])
                else:
                    bias = (bd[:, kt - 4 * i, :], R_bd)
                unit(St.KTs, St.R_KTs, slice(kt * 128, (kt + 1) * 128), QTg, g, bias, St.VsA[:, kt, g, :], St.R_VsA, outsS, [c.R_ps[3]])
            flush_pv()
            k.op("act", lambda e: e.activation(out=Os, in_=pO_s.rearrange("p (a x) -> p a x", a=4), func=AF.Copy), reads=[c.R_ps[3]], writes=[R_Os])
            k.op("act", lambda e: e.activation(out=Ow, in_=pO_w.rearrange("p (a x) -> p a x", a=4), func=AF.Copy), reads=[c.R_ps[4]], writes=[R_Ow])
            gs = c.gsig[:, i, 12 * g:12 * g + 12].rearrange("p (a x) -> p a x", a=4)
            zs, zw, cfc, cfs, cfw = cols[:, 24:28], cols[:, 28:32], cols[:, 32:36], cols[:, 36:40], cols[:, 40:44]
            k.op("dve", lambda e: e.tensor_scalar(out=zs, in0=Os[:, :, 64], scalar1=TINY, scalar2=None, op0=ALU.max), reads=[R_Os], writes=[R_cols])
            k.op("dve", lambda e: e.tensor_scalar(out=zw, in0=Ow[:, :, 64], scalar1=TINY, scalar2=None, op0=ALU.max), reads=[R_Ow], writes=[R_cols])
            k.op("dve", lambda e: e.reciprocal(out=zs, in_=zs), reads=[R_cols], writes=[R_cols])
            k.op("dve", lambda e: e.reciprocal(out=zw, in_=zw), reads=[R_cols], writes=[R_cols])
            k.op("dve", lambda e: e.tensor_tensor(out=cfc, in0=rzc, in1=gs[:, :, 0], op=ALU.mult), reads=[R_cols, c.R_gsig], writes=[R_cols])
            k.op("dve", lambda e: e.tensor_tensor(out=cfs, in0=zs, in1=gs[:, :, 1], op=ALU.mult), reads=[R_cols, c.R_gsig], writes=[R_cols])
            k.op("dve", lambda e: e.tensor_tensor(out=cfw, in0=zw, in1=gs[:, :, 2], op=ALU.mult), reads=[R_cols, c.R_gsig], writes=[R_cols])
            bc = lambda col: col.unsqueeze(2).broadcast_to([128, 4, 64])
            k.op("dve", lambda e: e.tensor_tensor(out=acc, in0=Uc[:, :, 0:64], in1=bc(cfc), op=ALU.mult), reads=[R_Uc, R_cols], writes=[R_acc])
            k.op("dve", lambda e: e.tensor_tensor(out=tmp, in0=Os[:, :, 0:64], in1=bc(cfs), op=ALU.mult), reads=[R_Os, R_cols, R_acc], writes=[R_acc])
            k.op("pool", lambda e: e.tensor_tensor(out=acc, in0=acc, in1=tmp, op=ALU.add), reads=[R_acc], writes=[R_acc])
            k.op("dve", lambda e: e.tensor_tensor(out=tmp, in0=Ow[:, :, 0:64], in1=bc(cfw), op=ALU.mult), reads=[R_Ow, R_cols, R_acc], writes=[R_acc])
            k.op("pool", lambda e, g=g: e.tensor_tensor(out=onsa[:, g, :, :], in0=acc, in1=tmp, op=ALU.add), reads=[R_acc], writes=[R_onsa])
        p7b = c.psum[7].bitcast(BF16)
        of = onsa.rearrange("p g a d -> p (g a d)")
        k.op("pe", [(lambda e, j=j: e.transpose(out=p7b[:, j * 128:(j + 1) * 128], in_=of[:, j * 128:(j + 1) * 128], identity=c.identb))
                    for j in range(4)], reads=[R_onsa, c.R_const], writes=[c.R_ps[7]])
        k.op("act", lambda e, i=i: e.activation(out=c.oT_nsa[:, :, i * 128:(i + 1) * 128],
                                                in_=p7b[:, 0:512].rearrange("p (j x) -> p j x", j=4), func=AF.Copy),
             reads=[c.R_ps[7]], writes=[c.R_oTnsa])
    ar.release(m0)


def phase_p2c(k, c, ar, X1, R_X1):
    Dr = c.D
    m0 = ar.mark()
    W = WS()
    scr = ar.al([128, D], F32)
    alloc_x_ws(k, c, ar, W, None, scr=scr, R_scr=k.res("scr2c"))
    hT = ar.al([128, 8, 512], BF16)
    R_hT = k.res("hT3")
    wbn, wbh = ar.al([128, 4, D], BF16), ar.al([128, 4, D], BF16)
    R_wb_ = k.res("wbr")
    wb = [ar.al([128, 8, 512], BF16) for _ in range(4)]
    R_wb = [k.res("wchc%d" % i) for i in range(4)]
    mixT = ar.al([128, 8, 512], BF16)
    R_mixT = k.res("mixT")
    sg1, sg2, mx1 = ar.al([128, 512], F32), ar.al([128, 512], F32), ar.al([128, 512], F32)
    R_sg1, R_sg2, R_mx = k.res("sg1"), k.res("sg2"), k.res("mx1")
    k.dma("pool", wbn, Dr["w_brn"].rearrange("(kt p) n -> p kt n", p=128), writes=[R_wb_])
    k.dma("pool", wbh, Dr["w_brh"].rearrange("(kt p) n -> p kt n", p=128), writes=[R_wb_])

    def wl(buf, src, c0):
        k.dma("pool", wb[buf], src[:, c0:c0 + 512].rearrange("(kt p) n -> p kt n", p=128), writes=[R_wb[buf]])

    NGo = NT // 4
    wl(0, Dr["w_mg"], 0)
    wl(1, Dr["w_mg"], 1024)
    for go in range(NGo):
        p = go % 2
        A = (2 * p, 2 * p + 1)
        B = (2 - 2 * p, 3 - 2 * p)
        tok = slice(go * 512, (go + 1) * 512)
        for tt in range(4):
            t = 4 * go + tt
            k.dma("sp", X1[:, t, :], Dr["xo"][t * 128:(t + 1) * 128, :], writes=[R_X1[t]])
        wl(B[0], Dr["w_mg"], 512)
        wl(B[1], Dr["w_mg"], 1024 + 512)
        norm_transpose_group(k, c, W, Dr["xo"], go * 512, hT, R_hT)
        for hf in range(2):
            w0, w1_ = (A if hf == 0 else B)
            for f4 in range(4):
                ft = hf * 4 + f4
                fs = slice(f4 * 128, (f4 + 1) * 128)
                gs_ = slice(ft * 128, (ft + 1) * 128)
                k.op("pe", [(lambda e, i=i: e.matmul(c.psum[2], lhsT=wb[w0][:, i, fs], rhs=hT[:, i, :], start=(i == 0), stop=(i == 7)))
                            for i in range(8)], reads=[R_wb[w0], R_hT], writes=[c.R_ps[2]])
                k.op("pe", [(lambda e, i=i: e.matmul(c.psum[3], lhsT=wb[w1_][:, i, fs], rhs=hT[:, i, :], start=(i == 0), stop=(i == 7)))
                            for i in range(8)], reads=[R_wb[w1_], R_hT], writes=[c.R_ps[3]])
                k.op("pe", [(lambda e, i=i: e.matmul(c.psum[4], lhsT=wbn[:, i, gs_], rhs=c.oT_nsa[:, i, tok], start=(i == 0), stop=(i == 3)))
                            for i in range(4)], reads=[R_wb_, c.R_oTnsa], writes=[c.R_ps[4]])
                k.op("pe", [(lambda e, i=i: e.matmul(c.psum[5], lhsT=wbh[:, i, gs_], rhs=c.oT_hg[:, i, tok], start=(i == 0), stop=(i == 3)))
                            for i in range(4)], reads=[R_wb_, c.R_oThg], writes=[c.R_ps[5]])
                k.op("act", lambda e: e.activation(out=sg1, in_=c.psum[2], func=AF.Sigmoid), reads=[c.R_ps[2]], writes=[R_sg1])
                k.op("act", lambda e: e.activation(out=sg2, in_=c.psum[3], func=AF.Sigmoid), reads=[c.R_ps[3]], writes=[R_sg2])
                k.op("dve", lambda e: e.tensor_tensor(out=mx1, in0=c.psum[4], in1=sg1, op=ALU.mult), reads=[c.R_ps[4], R_sg1], writes=[R_mx])
                k.op("dve", lambda e: e.tensor_tensor(out=sg2, in0=c.psum[5], in1=sg2, op=ALU.mult), reads=[c.R_ps[5], R_sg2], writes=[R_sg2])
                k.op("pool", lambda e, ft=ft: e.tensor_tensor(out=mixT[:, ft, :], in0=mx1, in1=sg2, op=ALU.add),
                     reads=[R_mx, R_sg2], writes=[R_mixT])
            if hf == 0:
                wl(A[0], Dr["w_out"], 0)
                wl(A[1], Dr["w_out"], 512)
            elif go + 1 < NGo:
                wl(B[0], Dr["w_mg"], 0)
                wl(B[1], Dr["w_mg"], 1024)
        for tt in range(4):
            t = 4 * go + tt
            for hf in range(2):
                bank = 6 + hf
                k.op("pe", [(lambda e, i=i: e.matmul(c.psum[bank], lhsT=mixT[:, i, tt * 128:(tt + 1) * 128], rhs=wb[A[hf]][:, i, :],
                                                     start=(i == 0), stop=(i == 7))) for i in range(8)],
                     reads=[R_mixT, R_wb[A[hf]]], writes=[c.R_ps[bank]])
                xs = X1[:, t, hf * 512:(hf + 1) * 512]
                k.op("dve", lambda e, xs=xs: e.tensor_tensor(out=xs, in0=c.psum[bank], in1=xs, op=ALU.add),
                     reads=[c.R_ps[bank], R_X1[t]], writes=[R_X1[t]])
    ar.release(m0)


def phase_attn(k, c, ar, stage):
    St = WS()
    St.KTs, St.KTw = ar.al([128, SEQ], BF16), ar.al([128, SEQ], BF16)
    St.VsA, St.VwA = ar.al([128, 64, 2, 65], BF16), ar.al([128, 64, 2, 65], BF16)
    St.KC = ar.al([128, 512], BF16)
    St.VCA = ar.al([128, 4, 2, 193], BF16)
    St.SNAP = ar.al([128, NT, 4, 128], BF16)
    for n in ("KTs", "KTw", "VsA", "VwA", "KC", "VCA", "SNAP"):
        setattr(St, "R_" + n, k.res(n))
    phase_p1(k, c, ar, St)
    for n in ("KTs", "KTw", "VsA", "VwA", "KC", "VCA", "SNAP"):
        c.dump(n, getattr(St, n), [getattr(St, "R_" + n)])
    k.flush()
    phase_p2pre(k, c, ar, St)
    c.dump("QT", c.QT, [c.R_QT])
    c.dump("gsig", c.gsig, [c.R_gsig])
    c.dump("oT_hg", c.oT_hg, [c.R_oThg])
    k.flush()
    phase_nsa(k, c, ar, St)
    c.dump("oT_nsa", c.oT_nsa, [c.R_oTnsa])
    return St


def build(stage="full", n_exp=NE):
    nc = bass.Bass("TRN2", target_bir_lowering=False)
    Dr = {}

    def din(name, shape, dt=F32):
        Dr[name] = nc.dram_tensor(name, list(shape), dt, kind="ExternalInput").ap()

    for name, shape, dt in input_specs(max(n_exp, 1), stage):
        din(name, shape, dt)
    Dr["y"] = nc.dram_tensor("y", [NTOK, D], F32, kind="ExternalOutput").ap()
    with ExitStack() as es:
        ARENA_BYTES = 207 * 1024
        big = es.enter_context(nc.sbuf_tensor("arena", [128, ARENA_BYTES // 2], BF16))
        pst = es.enter_context(nc.psum_tensor("ps", [128, 4096], F32))
        ar = Arena(big, ARENA_BYTES)
        k = KH(nc, es)
        k.oplim = _NC_CACHE.get("oplim", 10 ** 9)
        c = Ctx()
        c.nc, c.D, c.n_exp = nc, Dr, n_exp
        dumps = []

        def dump(name, ap, rs):
            if not _NC_CACHE.get("dbg"):
                return
            dt_ = ap.dtype
            dr = nc.dram_tensor("dbg_" + name, list(ap.shape), dt_, kind="ExternalOutput").ap()
            r = k.res("dbg_" + name)
            k.dma("sp", dr, ap, reads=list(rs), key=r)
            dumps.append(r)
        c.dump = dump
        c.psum = [pst[:, i * 512:(i + 1) * 512] for i in range(8)]
        c.pwide = lambda i: pst[:, i * 512:(i + 2) * 512]
        c.R_ps = [k.res("psb%d" % i, excl=True) for i in range(8)]
        c.R_const = k.res("const")
        c.identf = ar.al([128, 128], F32)
        c.identb = ar.al([128, 128], BF16)
        c.epsb = ar.al([128, 1], F32)
        c.ssq = ar.al([128, 8], F32)
        c.std = ar.al([128, 8], F32)
        c.rstd = ar.al([128, 8], F32)
        c.rs_res = [k.res("rs%d" % i) for i in range(8)]
        c.rs_i = 0
        k.dma("sp", c.identf, Dr["identf"], writes=[c.R_const])
        k.dma("sp", c.identb, Dr["identb"], writes=[c.R_const])
        k.op("dve", lambda e: e.memset(c.epsb, EPS), writes=[c.R_const])
        c.n_groups = _NC_CACHE.get("n_groups", 16)
        c.n_blocks = _NC_CACHE.get("n_blocks", NT)
        c.R32 = ar.al([128, 8, NTOK], BF16)
        c.R32f = c.R32.rearrange("p a b -> p (a b)")
        c.QT = c.R32f[:, 0:8192].rearrange("p (i a t) -> p i a t", i=NT, a=4)
        c.oT_nsa = c.R32[:, 4:8, :]
        c.oT_hg = ar.al([128, 4, NTOK], BF16)
        c.gsig = ar.al([128, NT, 24], F32)
        c.R_QT, c.R_oTnsa, c.R_oThg, c.R_gsig = k.res("QT"), k.res("oTnsa"), k.res("oThg"), k.res("gsig")
        hfT = c.R32
        R_hfT = [k.res("hfT_%d" % t) for t in range(NT)]
        R_X1 = [k.res("x1_%d" % t) for t in range(NT)]
        if stage != "moe_only":
            c.I4 = ar.al([128, 512], BF16)
            c.tri = ar.al([128, 128], BF16)
            c.onehot = ar.al([128, 4], F32)
            c.R_constP = k.res("constP")
            k.dma("pool", c.I4, Dr["I4"], writes=[c.R_constP])
            k.dma("pool", c.tri, Dr["tri"], writes=[c.R_constP])
            k.dma("sp", c.onehot, Dr["onehot"], writes=[c.R_const])
            setup_lb(k, c, ar)
            M1 = ar.mark()
            phase_attn(k, c, ar, stage)
            for r in dumps:
                k.rec["sp"].append(("w", r.dsem, r.dcnt))
            k.flush()
            ar.release(M1)
        X1 = ar.al([128, NT, D], F32)
        if stage == "moe_only":
            for t in range(NT):
                k.dma("sp", X1[:, t, :], Dr["xo"][t * 128:(t + 1) * 128, :], writes=[R_X1[t]])
        else:
            phase_p2c(k, c, ar, X1, R_X1)
            c.dump("X1", X1, R_X1)
            for r in dumps:
                if r.name == "dbg_X1":
                    k.rec["sp"].append(("w", r.dsem, r.dcnt))
        k.flush()
        if n_exp >= 0:
            phase_moe(k, c, ar, X1, R_X1, hfT, R_hfT)
            k.flush()
        phase_final(k, c, ar, X1, R_X1)
        k.flush()
    return nc


def input_specs(ne=NE, stage="full"):
    return [
        ("xb", (SEQ, D), F32), ("xo", (NTOK, D), F32),
        ("g_ffn", (D,), F32), ("g_fin", (D,), F32),
        ("w_r", (D, 36), F32), ("b_r", (36,), F32),
        ("w_gate", (ne, D, DFF), F32), ("w_up", (ne, D, DFF), F32), ("w_down", (ne, DFF, D), F32),
        ("identf", (128, 128), F32), ("identb", (128, 128), BF16),
    ] + (attn_input_specs() if stage != "moe_only" else [])


_NC_CACHE = {}


def host_inputs(inp, core, ne=NE):
    b, cp = core // 4, core % 4
    x = np.asarray(inp["x"], dtype=np.float32)
    m = {}
    m["xb"] = np.ascontiguousarray(x[b])
    m["xo"] = np.ascontiguousarray(x[b].reshape(NT, 4, 128, D)[:, cp].reshape(NTOK, D))
    m["g_ffn"] = np.ascontiguousarray(inp["ffn_norm"][0])
    m["g_fin"] = np.ascontiguousarray(inp["final_norm"])
    m["w_r"] = np.ascontiguousarray(np.concatenate([inp["w_grp"][0], inp["w_rtr"][0]], axis=1))
    m["b_r"] = np.ascontiguousarray(np.concatenate([inp["b_grp"][0], inp["b_rtr"][0]], axis=0))
    m["w_gate"] = np.ascontiguousarray(inp["w_gate"][0, :ne])
    m["w_up"] = np.ascontiguousarray(inp["w_up"][0, :ne])
    m["w_down"] = np.ascontiguousarray(inp["w_down"][0, :ne])
    m["identf"] = np.eye(128, dtype=np.float32)
    m["identb"] = np.eye(128, dtype=np.float32).astype(ml_dtypes.bfloat16)
    if _NC_CACHE.get("stage", "full") != "moe_only":
        m.update(attn_host_inputs(inp, b, cp))
    return m


def kernel(**inp):
    inp = {k_: np.asarray(v) for k_, v in inp.items()}
    stage = _NC_CACHE.get("stage", "full")
    key = ("nc", stage)
    if key not in _NC_CACHE:
        _NC_CACHE[key] = build(stage, _NC_CACHE.get("n_exp", NE))
    nc = _NC_CACHE[key]
    shared = None
    in_maps = []
    for core in range(8):
        m = host_inputs(inp, core, max(_NC_CACHE.get("n_exp", NE), 1))
        if shared is None:
            shared = m
        else:
            for kk in ("w_gate", "w_up", "w_down"):
                m[kk] = shared[kk]
        in_maps.append(m)
    res = run_bass_kernel_spmd(nc, in_maps, core_ids=list(range(8)))
    _NC_CACHE["last_results"] = res.results
    out = np.zeros((2, SEQ // 128, 128, D), dtype=np.float32)
    for core in range(8):
        b, cp = core // 4, core % 4
        y = np.asarray(res.results[core]["y"]).reshape(NT, 128, D)
        out[b, cp::4] = y
    return out.reshape(2, SEQ, D)
```

```python
import numpy as np
import ml_dtypes
import concourse.bass as bass
import concourse.mybir as mybir
from concourse.bass_utils import run_bass_kernel_spmd
from contextlib import ExitStack

F32 = mybir.dt.float32
BF16 = mybir.dt.bfloat16
AF = mybir.ActivationFunctionType
ALU = mybir.AluOpType
AX = mybir.AxisListType


class Res:
    __slots__ = ("name", "w", "rs", "dsem", "dcnt", "excl")

    def __init__(self, name, excl=False):
        self.name = name
        self.excl = excl
        self.w = None
        self.rs = {}
        self.dsem = None
        self.dcnt = 0


class _Proxy:
    def __init__(self):
        self.calls = []

    def __getattr__(self, name):
        def rec(*a, **kw):
            self.calls.append((name, a, kw))
        return rec


def _bind(f):
    p = _Proxy()
    f(p)
    assert len(p.calls) == 1, "one engine instruction per callable"
    name, a, kw = p.calls[0]
    return lambda eng: getattr(eng, name)(*a, **kw)


class KH:
    ENG = ("pe", "dve", "act", "pool", "sp")

    def __init__(self, nc, es):
        self.nc = nc
        self.es = es
        self.sem = {}
        self.cnt = {}
        for e in self.ENG:
            self.sem[e] = es.enter_context(nc.semaphore("s_" + e))
            self.cnt[e] = 0
        self.rec = {e: [] for e in self.ENG}
        self.seen = {e: {} for e in self.ENG}
        self.nsem = len(self.ENG)
        self.semobj = dict(self.sem)

    def res(self, name, excl=False):
        return Res(name, excl)

    def _dma_sem(self, r):
        if r.dsem is None:
            r.dsem = "d_" + r.name + "_%d" % self.nsem
            self.semobj[r.dsem] = self.es.enter_context(self.nc.semaphore(r.dsem))
            self.nsem += 1
        return r.dsem

    def _deps(self, e, reads, writes):
        deps = {}
        for r in reads:
            if r.w is not None:
                k, v = r.w
                deps[k] = max(deps.get(k, 0), v)
        for w in writes:
            if w.w is not None:
                k, v = w.w
                deps[k] = max(deps.get(k, 0), v)
            for k, v in w.rs.items():
                deps[k] = max(deps.get(k, 0), v)
        seen = self.seen[e]
        for k, v in deps.items():
            if seen.get(k, 0) >= v:
                continue
            seen[k] = v
            self.rec[e].append(("w", k, v))

    def op(self, e, fns, reads=(), writes=()):
        if callable(fns):
            fns = [fns]
        self.opn = getattr(self, "opn", 0) + 1
        if self.opn > getattr(self, "oplim", 10 ** 9):
            return
        ex = [r for r in reads if r.excl]
        if ex:
            reads = [r for r in reads if not r.excl]
            writes = list(writes) + [r for r in ex if r not in writes]
        self._deps(e, reads, writes)
        self.cnt[e] += 1
        v = self.cnt[e]
        fns = [_bind(f) for f in fns]
        for f in fns[:-1]:
            self.rec[e].append(("i", f, None, 0))
        self.rec[e].append(("i", fns[-1], e, 1))
        self.seen[e][e] = max(self.seen[e].get(e, 0), 0)
        for r in reads:
            r.rs[e] = v
        for w in writes:
            w.w = (e, v)
            w.rs = {}

    def dma(self, q, out, in_, reads=(), writes=(), key=None, **kw):
        self._deps(q, reads, writes)
        kr = key or (writes[0] if writes else reads[0])
        sk = self._dma_sem(kr)
        kr.dcnt += 16
        v = kr.dcnt
        self.rec[q].append(("i", lambda eng: eng.dma_start(out=out, in_=in_, **kw), sk, 16))
        for r in reads:
            r.rs[sk] = v
        for w in writes:
            w.w = (sk, v)
            w.rs = {}

    def wait_res(self, e, rs):
        self._deps(e, rs, ())

    def simulate(self):
        if not hasattr(self, "simval"):
            self.simval = {}
        val = self.simval
        ptr = {e: 0 for e in self.ENG}
        prog = True
        while prog:
            prog = False
            for e in self.ENG:
                items = self.rec[e]
                while ptr[e] < len(items):
                    it = items[ptr[e]]
                    if it[0] == "w":
                        if val.get(it[1], 0) >= it[2]:
                            ptr[e] += 1
                            prog = True
                        else:
                            break
                    else:
                        if it[2] is not None:
                            val[it[2]] = val.get(it[2], 0) + it[3]
                        ptr[e] += 1
                        prog = True
        for e in self.ENG:
            if ptr[e] < len(self.rec[e]):
                it = self.rec[e][ptr[e]]
                raise RuntimeError("DEADLOCK: engine %s stuck at item %d/%d waiting %s >= %s (have %s)" % (
                    e, ptr[e], len(self.rec[e]), it[1], it[2], val.get(it[1], 0)))

    def flush(self, name=None):
        nc = self.nc
        rec = self.rec
        semobj = self.semobj
        self.simulate()
        import os
        if os.environ.get("KH_DEBUG"):
            print("KH flush: ops so far", getattr(self, "opn", 0), {e: len(v) for e, v in self.rec.items()}, "nsem", self.nsem, flush=True)

        def play(eng, items):
            for it in items:
                if it[0] == "w":
                    eng.wait_ge(semobj[it[1]], it[2])
                else:
                    ins = it[1](eng)
                    if it[2] is not None:
                        ins.then_inc(semobj[it[2]], it[3])

        with nc.Block() as block:
            if rec["sp"]:
                @block.sync
                def _(eng):
                    play(eng, rec["sp"])
            if rec["pe"]:
                @block.tensor
                def _(eng):
                    play(eng, rec["pe"])
            if rec["dve"]:
                @block.vector
                def _(eng):
                    play(eng, rec["dve"])
            if rec["act"]:
                @block.scalar
                def _(eng):
                    play(eng, rec["act"])
            if rec["pool"]:
                @block.gpsimd
                def _(eng):
                    play(eng, rec["pool"])
        self.rec = {e: [] for e in self.ENG}

NEG = -30000.0
NT = 16
NTOK = NT * 128
SEQ = 8192
D = 1024
NE = 32
DFF = 512
EPS = 1e-6


class Arena:
    def __init__(self, big, nbytes):
        self.big = big
        self.n = nbytes
        self.off = 0

    def mark(self):
        return self.off

    def release(self, m):
        import os
        if os.environ.get("KH_DEBUG"):
            print("arena release: peak", getattr(self, "peak", 0), "->", m, "of", self.n, flush=True)
        self.peak = m
        self.off = m

    def al(self, shape, dt):
        esz = 4 if dt == F32 else 2
        per = int(np.prod(shape[1:])) * esz
        self.off = (self.off + 63) // 64 * 64
        o = self.off
        assert o + per <= self.n, ("arena overflow", o, per, self.n)
        self.off = o + per
        self.peak = max(getattr(self, "peak", 0), self.off)
        v = self.big[0:shape[0], o // 2:(o + per) // 2]
        if dt == F32:
            v = v.bitcast(F32)
        if len(shape) == 3:
            v = v.rearrange("p (a b) -> p a b", a=shape[1])
        elif len(shape) == 4:
            v = v.rearrange("p (a b c) -> p a b c", a=shape[1], b=shape[2])
        return v


class Ctx:
    pass


def rms_rstd(k, c, src, src_res, scr, scr_res, n_feat, tag):
    i = c.rs_i % 8
    c.rs_i += 1
    ssq, std, rstd = c.ssq[:, i:i + 1], c.std[:, i:i + 1], c.rstd[:, i:i + 1]
    R = c.rs_res[i]
    k.op("act", lambda e: e.activation(out=scr, in_=src, func=AF.Square, accum_out=ssq),
         reads=[src_res], writes=[scr_res, R])
    k.op("act", lambda e: e.activation(out=std, in_=ssq, func=AF.Sqrt, scale=1.0 / n_feat, bias=c.epsb[:, 0:1]),
         reads=[R, c.R_const], writes=[R])
    k.op("dve", lambda e: e.reciprocal(out=rstd, in_=std), reads=[R], writes=[R])
    return rstd, R


def phase_moe(k, c, ar, X1, R_X1, hfT, R_hfT):
    nc = c.nc
    Dr = c.D
    m0 = ar.mark()
    hf32 = [ar.al([128, D], F32) for _ in range(2)]
    R_hf32 = [k.res("hf32_%d" % i) for i in range(2)]
    scr = ar.al([128, D], F32)
    R_scr = k.res("moe_scr")
    hT32 = [ar.al([128, 8, 128], F32) for _ in range(2)]
    R_hT32 = [k.res("hT32_%d" % i) for i in range(2)]
    wr32 = ar.al([128, 8, 36], F32)
    R_wr = k.res("wr32")
    brt = ar.al([128, 36], F32)
    gft = ar.al([128, D], F32)
    R_gft = k.res("gft")
    comb = ar.al([128, NT, NE], F32)
    R_comb = k.res("comb")
    sm = ar.al([128, 128], F32)
    R_sm = k.res("moe_sm")
    k.dma("sp", wr32, Dr["w_r"].rearrange("(kt p) n -> p kt n", p=128), writes=[R_wr])
    k.dma("sp", brt, Dr["b_r"].partition_broadcast(128), writes=[R_wr])
    k.dma("sp", gft, Dr["g_ffn"].partition_broadcast(128), writes=[R_gft])
    pT = [c.pwide(0), c.pwide(2)]
    R_pT = [[c.R_ps[0], c.R_ps[1]], [c.R_ps[2], c.R_ps[3]]]
    pL = c.psum[4]
    R_pL = c.R_ps[4]
    for t in range(NT):
        b = t % 2
        xs = X1[:, t, :]
        rstd, R_r = rms_rstd(k, c, xs, R_X1[t], scr, R_scr, D, "moe")
        k.op("dve", lambda e, b=b, xs=xs, rstd=rstd: e.scalar_tensor_tensor(
            out=hf32[b], in0=xs, scalar=rstd, in1=gft, op0=ALU.mult, op1=ALU.mult),
            reads=[R_X1[t], R_r, R_gft], writes=[R_hf32[b]])
        p2 = pT[b]
        k.op("pe", [(lambda e, i=i, b=b, p2=p2: e.transpose(out=p2[:, i * 128:(i + 1) * 128],
                                                            in_=hf32[b][:, i * 128:(i + 1) * 128], identity=c.identf))
                    for i in range(8)], reads=[R_hf32[b], c.R_const], writes=R_pT[b])
        k.op("act", lambda e, b=b, p2=p2: e.activation(out=hT32[b].rearrange("p a b -> p (a b)"), in_=p2, func=AF.Copy),
             reads=R_pT[b], writes=[R_hT32[b]])
        k.op("dve", lambda e, b=b, p2=p2, t=t: e.tensor_copy(
            out=hfT[:, :, t * 128:(t + 1) * 128], in_=p2.rearrange("p (a b) -> p a b", a=8)),
            reads=R_pT[b], writes=[R_hfT[t]])
        lg = pL[:, 0:36]
        k.op("pe", [(lambda e, i=i, b=b: e.matmul(lg, lhsT=hT32[b][:, i, :], rhs=wr32[:, i, :], start=(i == 0), stop=(i == 7)))
                    for i in range(8)], reads=[R_hT32[b], R_wr], writes=[R_pL])
        lgs = sm[:, 0:36]
        gmax, gsum, gp, pen = sm[:, 36:37], sm[:, 37:38], sm[:, 38:39], sm[:, 40:44]
        gex, goh = sm[:, 44:48], sm[:, 48:52]
        elm = sm[:, 52:84]
        m8 = sm[:, 84:92]
        dd, ee, w1, w2 = sm[:, 92:93], sm[:, 93:94], sm[:, 94:95], sm[:, 95:96]
        oh = sm[:, 96:128]
        ct = comb[:, t, :]
        RW = dict(reads=[R_sm], writes=[R_sm])
        k.op("dve", lambda e: e.tensor_tensor(out=lgs, in0=lg, in1=brt, op=ALU.add), reads=[R_pL, R_wr, R_sm], writes=[R_sm])
        k.op("dve", lambda e: e.reduce_max(out=gmax, in_=lgs[:, 0:4], axis=AX.X), **RW)
        k.op("dve", lambda e: e.tensor_scalar(out=gex, in0=lgs[:, 0:4], scalar1=gmax, scalar2=None, op0=ALU.subtract), **RW)
        k.op("act", lambda e: e.activation(out=gex, in_=gex, func=AF.Exp, accum_out=gsum), **RW)
        k.op("dve", lambda e: e.reciprocal(out=gp, in_=gsum), **RW)
        k.op("dve", lambda e: e.tensor_scalar(out=pen, in0=lgs[:, 0:4], scalar1=gmax, scalar2=-1e30, op0=ALU.is_lt, op1=ALU.mult), **RW)
        k.op("dve", lambda e: e.tensor_tensor(out=elm.rearrange("p (g x) -> p g x", g=4),
                                              in0=lgs[:, 4:36].rearrange("p (g x) -> p g x", g=4),
                                              in1=pen.unsqueeze(2).broadcast_to([128, 4, 8]), op=ALU.add), **RW)
        k.op("dve", lambda e: e.max(out=m8, in_=elm), **RW)
        k.op("dve", lambda e: e.tensor_tensor(out=dd, in0=m8[:, 1:2], in1=m8[:, 0:1], op=ALU.subtract), **RW)
        k.op("act", lambda e: e.activation(out=ee, in_=dd, func=AF.Exp), **RW)
        k.op("dve", lambda e: e.tensor_scalar(out=ee, in0=ee, scalar1=1.0, scalar2=None, op0=ALU.add), **RW)
        k.op("dve", lambda e: e.reciprocal(out=w1, in_=ee), **RW)
        k.op("dve", lambda e: e.tensor_scalar(out=w2, in0=w1, scalar1=-1.0, scalar2=1.0, op0=ALU.mult, op1=ALU.add), **RW)
        k.op("dve", lambda e: e.tensor_tensor(out=w1, in0=w1, in1=gp, op=ALU.mult), **RW)
        k.op("dve", lambda e: e.tensor_tensor(out=w2, in0=w2, in1=gp, op=ALU.mult), **RW)
        k.op("dve", lambda e: e.tensor_scalar(out=oh, in0=elm, scalar1=m8[:, 0:1], scalar2=w1, op0=ALU.is_equal, op1=ALU.mult), **RW)
        k.op("dve", lambda e, ct=ct: e.tensor_scalar(out=ct, in0=elm, scalar1=m8[:, 1:2], scalar2=w2, op0=ALU.is_equal, op1=ALU.mult),
             reads=[R_sm], writes=[R_comb])
        k.op("dve", lambda e, ct=ct: e.tensor_tensor(out=ct, in0=ct, in1=oh, op=ALU.add), reads=[R_sm, R_comb], writes=[R_comb])

    if c.n_exp == 0:
        ar.release(m0)
        return
    NWB = 2
    wg = [ar.al([128, 8, DFF], BF16) for _ in range(NWB)]
    wu = [ar.al([128, 8, DFF], BF16) for _ in range(NWB)]
    wd = [ar.al([128, 4, D], BF16) for _ in range(NWB)]
    R_wg = [k.res("wg%d" % i) for i in range(NWB)]
    R_wu = [k.res("wu%d" % i) for i in range(NWB)]
    R_wd = [k.res("wd%d" % i) for i in range(NWB)]
    hid = [ar.al([128, 4, 512], BF16) for _ in range(2)]
    R_hid = [k.res("hid%d" % i) for i in range(2)]
    sg = [ar.al([128, 512], F32) for _ in range(2)]
    R_sg = [k.res("sg%d" % i) for i in range(2)]
    n_exp = c.n_exp

    def load_w(e):
        b = e % NWB
        k.dma("pool", wg[b], Dr["w_gate"][e].rearrange("(kt p) n -> p kt n", p=128), writes=[R_wg[b]])
        k.dma("pool", wu[b], Dr["w_up"][e].rearrange("(kt p) n -> p kt n", p=128), writes=[R_wu[b]])
        k.dma("pool", wd[b], Dr["w_down"][e].rearrange("(kt p) n -> p kt n", p=128), writes=[R_wd[b]])

    units = [(e, g) for e in range(n_exp) for g in range(NT // 4)]
    pgu = [(c.psum[0], c.psum[1]), (c.psum[2], c.psum[3])]
    R_pgu = [(c.R_ps[0], c.R_ps[1]), (c.R_ps[2], c.R_ps[3])]
    pdn = [c.psum[4], c.psum[5], c.psum[6], c.psum[7]]
    R_pdn = [c.R_ps[4], c.R_ps[5], c.R_ps[6], c.R_ps[7]]
    st = dict(gu=0, dn=0)

    def gate_up(u):
        e, g = units[u]
        b = e % NWB
        hb = u % 2
        tok = slice(g * 512, (g + 1) * 512)
        for ff in range(4):
            pb = st["gu"] % 2
            st["gu"] += 1
            pg, pu = pgu[pb]
            k.op("pe", [(lambda en, i=i, pg=pg, b=b, ff=ff: en.matmul(pg, lhsT=wg[b][:, i, ff * 128:(ff + 1) * 128], rhs=hfT[:, i, tok],
                                                                      start=(i == 0), stop=(i == 7))) for i in range(8)],
                 reads=[R_wg[b]] + R_hfT[4 * g:4 * g + 4], writes=[R_pgu[pb][0]])
            k.op("pe", [(lambda en, i=i, pu=pu, b=b, ff=ff: en.matmul(pu, lhsT=wu[b][:, i, ff * 128:(ff + 1) * 128], rhs=hfT[:, i, tok],
                                                                      start=(i == 0), stop=(i == 7))) for i in range(8)],
                 reads=[R_wu[b]] + R_hfT[4 * g:4 * g + 4], writes=[R_pgu[pb][1]])
            k.op("act", lambda en, pg=pg, pb=pb: en.activation(out=sg[pb], in_=pg, func=AF.Silu),
                 reads=[R_pgu[pb][0]], writes=[R_sg[pb]])
            k.op("dve", lambda en, pu=pu, pb=pb, hb=hb, ff=ff: en.tensor_tensor(out=hid[hb][:, ff, :], in0=pu, in1=sg[pb], op=ALU.mult),
                 reads=[R_pgu[pb][1], R_sg[pb]], writes=[R_hid[hb]])

    def down(u):
        e, g = units[u]
        b = e % NWB
        hb = u % 2
        for tt in range(4):
            t = 4 * g + tt
            for hf in range(2):
                pb = st["dn"] % 4
                st["dn"] += 1
                po = pdn[pb]
                k.op("pe", [(lambda en, i=i, po=po, b=b, hb=hb, tt=tt, hf=hf: en.matmul(
                    po, lhsT=hid[hb][:, i, tt * 128:(tt + 1) * 128], rhs=wd[b][:, i, hf * 512:(hf + 1) * 512],
                    start=(i == 0), stop=(i == 3))) for i in range(4)],
                    reads=[R_hid[hb], R_wd[b]], writes=[R_pdn[pb]])
                xs = X1[:, t, hf * 512:(hf + 1) * 512]
                k.op("dve", lambda en, po=po, xs=xs, t=t, e=e: en.scalar_tensor_tensor(
                    out=xs, in0=po, scalar=comb[:, t, e:e + 1], in1=xs, op0=ALU.mult, op1=ALU.add),
                    reads=[R_pdn[pb], R_comb, R_X1[t]], writes=[R_X1[t]])

    load_w(0)
    for u in range(len(units)):
        e, g = units[u]
        gate_up(u)
        if u >= 1:
            down(u - 1)
        if g == 0 and e + 1 < n_exp:
            load_w(e + 1)
    down(len(units) - 1)
    ar.release(m0)


def phase_final(k, c, ar, X1, R_X1):
    Dr = c.D
    m0 = ar.mark()
    gft = ar.al([128, D], F32)
    R_g = k.res("gfin")
    scr = ar.al([128, D], F32)
    R_scr = k.res("fin_scr")
    ob = [ar.al([128, D], F32) for _ in range(2)]
    R_ob = [k.res("ob%d" % i) for i in range(2)]
    k.dma("sp", gft, Dr["g_fin"].partition_broadcast(128), writes=[R_g])
    for t in range(NT):
        b = t % 2
        xs = X1[:, t, :]
        rstd, R_r = rms_rstd(k, c, xs, R_X1[t], scr, R_scr, D, "fin")
        k.op("dve", lambda e, b=b, xs=xs, rstd=rstd: e.scalar_tensor_tensor(
            out=ob[b], in0=xs, scalar=rstd, in1=gft, op0=ALU.mult, op1=ALU.mult),
            reads=[R_X1[t], R_r, R_g], writes=[R_ob[b]])
        k.dma("sp", Dr["y"][t * 128:(t + 1) * 128, :], ob[b], reads=[R_ob[b]])
    for b in range(2):
        for sk, v in list(R_ob[b].rs.items()):
            if sk.startswith("d_"):
                k.rec["sp"].append(("w", sk, v))
    ar.release(m0)


Q0, KV0, GT0, HQ0, HF0, HI0, HG0, MG0 = 0, 512, 1280, 1304, 1816, 2328, 2840, 3352


def _partner(d):
    return d + 8 if d < 8 else (d - 8 if d < 16 else d)


def _rope_tables(pos):
    pos = np.asarray(pos, dtype=np.float32)
    inv = (np.float32(500000.0) ** (-np.arange(8, dtype=np.float32) / np.float32(8))).astype(np.float32)
    ang = (pos[None, :] * inv[:, None]).astype(np.float32)
    cs, sn = np.cos(ang).astype(np.float32), np.sin(ang).astype(np.float32)
    C = np.ones((64, len(pos)), np.float32)
    S = np.zeros((64, len(pos)), np.float32)
    C[0:8], C[8:16] = cs, cs
    S[0:8], S[8:16] = -sn, sn
    return C, S


def attn_input_specs():
    return [
        ("g_attn", (D,), F32), ("g_hg4", (512,), F32),
        ("w1f", (D, 1280), F32), ("w1t", (D, 768), F32),
        ("w2f", (D, 2048), F32), ("w2t", (D, 1536), F32),
        ("wck", (64, 2048), F32), ("wckp", (64, 2048), F32), ("wcv", (64, 2048), F32),
        ("posT", (128, 32), F32), ("lbl", (2, 512), F32),
        ("w_mg", (D, 2048), F32), ("w_brn", (512, D), F32), ("w_brh", (512, D), F32), ("w_out", (D, D), F32),
        ("CK", (128, SEQ), F32), ("SK", (128, SEQ), F32), ("CKc", (128, 512), F32), ("SKc", (128, 512), F32),
        ("CQ", (128, NTOK), F32), ("SQ", (128, NTOK), F32),
        ("ovl", (128, 4, 128), F32),
        ("CB", (128, NT, 128), F32), ("CM", (128, 4, 128), F32), ("WMT", (128, 8, 128), F32),
        ("VAL", (128, NT, 128), F32), ("ADDC", (128, NT, 128), F32),
        ("tri", (128, 128), F32), ("I4", (128, 512), F32), ("onehot", (128, 4), F32),
    ]


_TAB_CACHE = {}


def _const_tables(cp):
    if cp in _TAB_CACHE:
        return _TAB_CACHE[cp]
    m = {}
    C, S = _rope_tables(np.arange(SEQ))
    m["CK"], m["SK"] = np.concatenate([C, C], 0), np.concatenate([S, S], 0)
    C, S = _rope_tables(np.maximum(16 * (np.arange(512) - 1), 0))
    m["CKc"], m["SKc"] = np.concatenate([C, C], 0), np.concatenate([S, S], 0)
    tpos = (128 * (4 * np.arange(NT)[:, None] + cp) + np.arange(128)[None, :])
    C, S = _rope_tables(tpos.reshape(-1))
    m["CQ"] = np.concatenate([C, C], 0) * np.float32(0.125)
    m["SQ"] = np.concatenate([S, S], 0) * np.float32(0.125)
    n = np.arange(512) - 1
    cs, ce = 16 * n, 16 * n + 31
    ss = 64 * np.arange(128)
    ov = ((cs[:, None] < ss[None, :] + 64) & (ce[:, None] >= ss[None, :]) & (n[:, None] >= 0)).astype(np.float32)
    m["ovl"] = np.ascontiguousarray(ov.reshape(4, 128, 128).transpose(1, 0, 2))
    mt = (np.arange(NT) // 4)
    mm = mt[:, None] * 128 + np.arange(128)[None, :]
    nn = mm - 1
    okc = (nn[:, None, :] >= 0) & (16 * nn[:, None, :] + 31 <= tpos[:, :, None])
    m["CB"] = np.ascontiguousarray(np.where(okc, 0.0, NEG).astype(np.float32).transpose(1, 0, 2))
    blk = np.arange(128)
    jq = tpos // 64
    force = (blk[None, None, :] == jq[:, :, None]) | (blk[None, None, :] == 0)
    valid = (64 * blk[None, None, :] <= tpos[:, :, None])
    m["VAL"] = np.ascontiguousarray((valid & ~force).astype(np.float32).transpose(1, 0, 2))
    m["ADDC"] = np.ascontiguousarray(np.where(force, 1e4, np.where(valid, 0.0, -1.0)).astype(np.float32).transpose(1, 0, 2))
    t = np.arange(128)[:, None]
    p = np.arange(128)[None, :]
    caus = np.where(p <= t, 0.0, NEG).astype(np.float32)
    anti = np.where(p > t, 0.0, NEG).astype(np.float32)
    cm = np.zeros((128, 4, 128), np.float32)
    for r in range(4):
        cm[:, r, :] = 0.0 if r < cp else (caus if r == cp else NEG)
    m["CM"] = cm
    wm = np.zeros((128, 8, 128), np.float32)
    for r in range(8):
        dk = cp + 4 - r
        wm[:, r, :] = NEG if (dk < 0 or dk > 4) else (caus if dk == 0 else (anti if dk == 4 else 0.0))
    m["WMT"] = wm
    m["tri"] = (np.arange(128)[:, None] <= np.arange(128)[None, :]).astype(np.float32)
    m["I4"] = np.tile(np.eye(128, dtype=np.float32), (1, 4))
    oh = np.zeros((128, 4), np.float32)
    oh[:, cp] = 1.0
    m["onehot"] = oh
    _TAB_CACHE[cp] = m
    return m


def attn_host_inputs(inp, b, cp):
    m = dict(_const_tables(cp))
    w = inp["w_in"][0]
    pp = np.array([g * 64 + _partner(d) for g in range(2) for d in range(64)])
    kv = lambda s: KV0 + s * 128 + np.arange(128)
    hfc = HF0 + np.arange(512)
    m["w1f"] = np.ascontiguousarray(np.concatenate(
        [w[:, kv(0)], w[:, kv(1)], w[:, kv(2)], w[:, kv(2)[pp]], w[:, kv(4)], w[:, kv(4)[pp]], w[:, hfc]], axis=1))
    m["w1t"] = np.ascontiguousarray(np.concatenate([w[:, kv(3)], w[:, kv(5)], w[:, HI0:HI0 + 512]], axis=1))
    qcols, qpcols = [], []
    for a in range(4):
        for h in (a, 4 + a):
            qcols += [Q0 + h * 64 + d for d in range(64)]
            qpcols += [Q0 + h * 64 + _partner(d) for d in range(64)]
    m["w2f"] = np.ascontiguousarray(np.concatenate(
        [w[:, qcols], w[:, qpcols], w[:, HQ0:HQ0 + 512], w[:, hfc]], axis=1))
    gpad = np.concatenate([w[:, GT0:GT0 + 24], w[:, GT0:GT0 + 24][:, :0].repeat(1, 1)], axis=1)
    w2t = np.zeros((D, 1536), np.float32)
    w2t[:, 0:512] = w[:, HI0:HI0 + 512]
    w2t[:, 512:1024] = w[:, HG0:HG0 + 512]
    w2t[:, 1024:1048] = w[:, GT0:GT0 + 24]
    m["w2t"] = w2t
    pc = np.array([_partner(d) for d in range(64)])
    dle = lambda w_: np.ascontiguousarray(w_.reshape(32, 64, 64).transpose(1, 0, 2).reshape(64, 2048))
    m["wck"] = dle(inp["w_cmp_k"][0])
    m["wckp"] = dle(inp["w_cmp_k"][0][:, pc])
    m["wcv"] = dle(inp["w_cmp_v"][0])
    pT = np.ascontiguousarray(inp["cmp_pos"][0].T)
    m["posT"] = np.concatenate([pT, pT], 0)
    m["lbl"] = np.ascontiguousarray(inp["hg_lb_logits"])
    m["g_attn"] = np.ascontiguousarray(inp["attn_norm"][0])
    m["g_hg4"] = np.ascontiguousarray(np.tile(inp["hg_norm"][0], 4))
    m["w_mg"] = np.ascontiguousarray(w[:, MG0:MG0 + 2048])
    m["w_brn"] = np.ascontiguousarray(inp["w_br_nsa"][0])
    m["w_brh"] = np.ascontiguousarray(inp["w_br_hg"][0])
    m["w_out"] = np.ascontiguousarray(inp["w_out"][0])
    return m


def norm_transpose_group(k, c, W, src_dram, row0, hT, R_hT):
    def s1(tt):
        b = tt % 2
        k.dma("sp", W.xt[b], src_dram[row0 + tt * 128: row0 + (tt + 1) * 128, :], writes=[W.R_xt[b]])
        rstd, R_r = rms_rstd(k, c, W.xt[b], W.R_xt[b], W.scr, W.R_scr, D, "an")
        k.op("dve", lambda e: e.scalar_tensor_tensor(
            out=W.hb[b], in0=W.xt[b], scalar=rstd, in1=W.gA, op0=ALU.mult, op1=ALU.mult),
            reads=[W.R_xt[b], R_r, W.R_gA], writes=[W.R_hb[b]])
        pb = c.psum[b].bitcast(BF16)
        k.op("pe", [(lambda e, i=i: e.transpose(out=pb[:, i * 128:(i + 1) * 128],
                                                in_=W.hb[b][:, i * 128:(i + 1) * 128], identity=c.identb))
                    for i in range(8)], reads=[W.R_hb[b], c.R_const], writes=[c.R_ps[b]])

    def s2(tt):
        b = tt % 2
        pb = c.psum[b].bitcast(BF16)
        k.op("act", lambda e: e.activation(out=hT[:, :, tt * 128:(tt + 1) * 128],
                                           in_=pb.rearrange("p (a b) -> p a b", a=8), func=AF.Copy),
             reads=[c.R_ps[b]], writes=[R_hT])
    s1(0)
    s1(1)
    s2(0)
    s1(2)
    s2(1)
    s1(3)
    s2(2)
    s2(3)


def f_front(k, c, W, fl_ps, R_fl, hd):
    u, a, bq, lk, L, RF = W.sets[hd % 2]
    k.op("act", lambda e: e.activation(out=u, in_=fl_ps, func=AF.Exp, scale=-1.0), reads=[R_fl], writes=[RF])
    k.op("act", lambda e: e.activation(out=a, in_=u, func=AF.Ln, scale=c.lbv[:, hd:hd + 1], bias=c.one_col[:, 0:1]),
         reads=[RF, c.R_const], writes=[RF])
    k.op("act", lambda e: e.activation(out=bq, in_=u, func=AF.Ln, bias=c.one_col[:, 0:1]), reads=[RF, c.R_const], writes=[RF])
    k.op("dve", lambda e: e.scalar_tensor_tensor(out=lk, in0=fl_ps, scalar=-1.0, in1=bq, op0=ALU.mult, op1=ALU.subtract),
         reads=[R_fl, RF], writes=[RF])
    for tt in range(4):
        sl = slice(tt * 128, (tt + 1) * 128)
        k.op("dve", lambda e, sl=sl: e.tensor_tensor_scan(out=L[:, sl], data0=a[:, sl], data1=bq[:, sl], initial=0.0,
                                                          op0=ALU.add, op1=ALU.subtract), reads=[RF], writes=[RF])
    k.op("pool", lambda e: e.tensor_tensor(out=lk, in0=lk, in1=L, op=ALU.subtract), reads=[RF], writes=[RF])


def f_back(k, c, W, hd, H=None):
    u, a, bq, lk, L, RF = W.sets[hd % 2]
    W_, W = W, (H if H is not None else W)
    Lr = L.rearrange("p (t x) -> p t x", t=4)
    rcol, ecol = Lr[:, :, 63], Lr[:, :, 127]
    k.op("dve", lambda e: e.tensor_scalar(out=W.rb[:, hd, :], in0=rcol, scalar1=c.l1mlb[:, hd:hd + 1], scalar2=None, op0=ALU.add),
         reads=[RF, c.R_const], writes=[W.R_cols])
    k.op("dve", lambda e: e.tensor_scalar(out=W.negr[:, hd, :], in0=rcol, scalar1=-1.0, scalar2=None, op0=ALU.mult),
         reads=[RF], writes=[W.R_cols])
    k.op("dve", lambda e: e.tensor_tensor(out=W.dl[:, hd, :], in0=ecol, in1=rcol, op=ALU.subtract), reads=[RF], writes=[W.R_cols])
    k.op("act", lambda e: e.activation(out=W.c1[:, hd, :], in_=ecol, func=AF.Exp), reads=[RF], writes=[W.R_cols])
    k.op("act", lambda e: e.activation(out=W.c2[:, hd, :], in_=W.dl[:, hd, :], func=AF.Exp), reads=[W.R_cols], writes=[W.R_cols])
    k.op("act", lambda e: e.activation(out=W.er[:, hd, :], in_=rcol, func=AF.Exp), reads=[RF], writes=[W.R_cols])
    for tt in range(4):
        sl = slice(tt * 128, (tt + 1) * 128)
        k.op("act", lambda e, sl=sl, tt=tt: e.activation(out=W.kT[:, hd, sl], in_=lk[:, sl], func=AF.Exp, bias=W.rb[:, hd, tt:tt + 1]),
             reads=[RF, W.R_cols], writes=[W.R_kT])


def setup_lb(k, c, ar):
    Dr = c.D
    c.lbv = ar.al([128, 4], F32)
    c.l1mlb = ar.al([128, 4], F32)
    c.one_col = ar.al([128, 1], F32)
    c.ones128 = ar.al([128, 128], F32)
    l0 = ar.al([128, 4], F32)
    l1 = ar.al([128, 4], F32)
    R = c.R_const
    k.dma("sp", l0, Dr["lbl"][0].rearrange("(h p) -> p h", p=128), writes=[R], allow_slow_non_contiguous=True)
    k.dma("sp", l1, Dr["lbl"][1].rearrange("(h p) -> p h", p=128), writes=[R], allow_slow_non_contiguous=True)
    k.op("dve", lambda e: e.memset(c.one_col, 1.0), writes=[R])
    k.op("dve", lambda e: e.memset(c.ones128, 1.0), writes=[R])
    k.op("dve", lambda e: e.tensor_tensor(out=l1, in0=l1, in1=l0, op=ALU.subtract), reads=[R], writes=[R])
    k.op("act", lambda e: e.activation(out=l0, in_=l1, func=AF.Exp), reads=[R], writes=[R])
    k.op("dve", lambda e: e.tensor_scalar(out=l0, in0=l0, scalar1=1.0, scalar2=None, op0=ALU.add), reads=[R], writes=[R])
    k.op("dve", lambda e: e.reciprocal(out=c.lbv, in_=l0), reads=[R], writes=[R])
    k.op("act", lambda e: e.activation(out=l0, in_=l0, func=AF.Ln), reads=[R], writes=[R])
    k.op("dve", lambda e: e.tensor_tensor(out=c.l1mlb, in0=l1, in1=l0, op=ALU.subtract), reads=[R], writes=[R])


class WS:
    pass


def alloc_hg_ws(k, ar, W, nsets=1):
    W.sets = []
    for si in range(nsets):
        blk = ar.al([128, 5, 512], F32)
        W.sets.append(tuple(blk[:, i, :] for i in range(5)) + (k.res("fchain%d" % si),))
        if si == 0:
            W.ab = blk[:, 1:3, :].rearrange("p a b -> p (a b)")
    if nsets == 1:
        W.sets.append(W.sets[0])
    W.u, W.a, W.bq, W.lk, W.L, W.R_f = W.sets[0]
    alloc_hslot(k, ar, W, "0")


def alloc_hslot(k, ar, H, tag):
    H.rb, H.negr, H.dl, H.c1, H.c2, H.er = [ar.al([128, 4, 4], F32) for _ in range(6)]
    H.R_cols = k.res("fcols" + tag)
    H.kT = ar.al([128, 4, 512], BF16)
    H.R_kT = k.res("kT" + tag)


def alloc_x_ws(k, c, ar, W, region, scr=None, R_scr=None):
    if region is not None:
        W.xt = [region[:, 0, :].bitcast(F32), region[:, 1, :].bitcast(F32)]
        W.hb = [region[:, 2, 0:1024], region[:, 2, 1024:2048]]
        W.scr = region[:, 3, :].bitcast(F32)
        W.R_scr = k.res("xscr")
    else:
        W.xt = [ar.al([128, D], F32) for _ in range(2)]
        W.hb = [ar.al([128, D], BF16) for _ in range(2)]
        W.scr, W.R_scr = scr, R_scr
    W.R_xt = [k.res("xt0"), k.res("xt1")]
    W.R_hb = [k.res("hb0"), k.res("hb1")]
    W.gA = ar.al([128, D], F32)
    W.R_gA = k.res("gA")
    k.dma("sp", W.gA, c.D["g_attn"].partition_broadcast(128), writes=[W.R_gA])


def phase_p1(k, c, ar, St):
    Dr = c.D
    m0 = ar.mark()
    W = WS()
    alloc_x_ws(k, c, ar, W, c.oT_hg)
    w1f, w1t = c.R32[:, :, 0:1280], c.R32[:, :, 1280:2048]
    R_w1 = k.res("w1")
    k.dma("pool", w1f, Dr["w1f"].rearrange("(kt p) n -> p kt n", p=128), writes=[R_w1])
    k.dma("pool", w1t, Dr["w1t"].rearrange("(kt p) n -> p kt n", p=128), writes=[R_w1])
    hT = ar.al([128, 8, 512], BF16)
    R_hT = k.res("hT")
    CKg, SKg = ar.al([128, 512], F32), ar.al([128, 512], F32)
    R_rt = k.res("ropetab")
    alloc_hg_ws(k, ar, W, nsets=2)
    t1, t2, R_t12 = W.u, W.a, W.R_f
    vtok = ar.al([128, 4, 512], BF16)
    R_vtok = k.res("vtok")
    ktok = ar.al([128, 4, 128], BF16)
    R_ktok = k.res("ktok")
    Sst = ar.al([128, 4, 128], F32)
    snapacc = ar.al([128, 4, 128], F32)
    R_S, R_snapacc = k.res("S"), k.res("snapacc")
    WC = [ar.al([128, 32, 64], BF16) for _ in range(3)]
    R_WC = k.res("WC")
    posT = ar.al([128, 32], BF16)
    cb = ar.al([128, 4], F32)
    xin = [[ar.al([128, 528], BF16) for _ in range(2)] for _ in range(2)]
    R_xin = [[k.res("xin%d%d" % (a, b)) for b in range(2)] for a in range(2)]
    CKc, SKc = ar.al([128, 32], F32), ar.al([128, 32], F32)
    R_ckc = k.res("ckc")
    VCf = ar.al([128, 512], F32)
    R_VCf = k.res("VCf")
    ctmp = ar.al([128, 4, 32], F32)
    R_ctmp = k.res("ctmp")
    for xi, nm in enumerate(("wck", "wckp", "wcv")):
        for g in range(2):
            k.dma("pool", WC[xi][64 * g:64 * g + 64].rearrange("p l e -> p (l e)"), Dr[nm], writes=[R_WC])
    k.dma("pool", posT, Dr["posT"], writes=[R_WC])
    k.op("dve", lambda e: e.memset(Sst, 0.0), writes=[R_S])
    k.op("dve", lambda e: e.memset(St.VsA[:, :, :, 64:65], 1.0), writes=[St.R_VsA])
    k.op("dve", lambda e: e.memset(St.VwA[:, :, :, 64:65], 1.0), writes=[St.R_VwA])
    for a in range(2):
        k.op("dve", lambda e, a=a: e.memset(xin[a][0][:, 0:16], 0.0), writes=[R_xin[a][0]])
    p6 = c.psum[6]
    fns = []
    for xi in range(3):
        for g in range(2):
            for l in range(32):
                fns.append(lambda e, xi=xi, g=g, l=l: e.matmul(p6[64 * g:64 * g + 64, xi:xi + 1], lhsT=WC[xi][64 * g:64 * g + 64, l, :],
                                                               rhs=posT[64 * g:64 * g + 64, l:l + 1], start=(l == 0), stop=(l == 31)))
    k.op("pe", fns, reads=[R_WC], writes=[c.R_ps[6]])
    k.op("dve", lambda e: e.tensor_copy(out=cb[:, 0:3], in_=p6[:, 0:3]), reads=[c.R_ps[6]], writes=[R_WC])

    NG = c.n_groups
    Hs = [W, W]
    vtoks, R_vtoks = [vtok, vtok], [R_vtok, R_vtok]
    p6b = c.psum[6].bitcast(BF16)

    def fm(ft, bank):
        k.op("pe", [(lambda e, i=i: e.matmul(c.psum[bank], lhsT=w1f[:, i, ft * 128:(ft + 1) * 128], rhs=hT[:, i, :],
                                             start=(i == 0), stop=(i == 7))) for i in range(8)],
             reads=[R_w1, R_hT], writes=[c.R_ps[bank]])

    def A_x(G):
        norm_transpose_group(k, c, W, Dr["xb"], G * 512, hT, R_hT)
        k.dma("sp", CKg, Dr["CK"][:, G * 512:(G + 1) * 512], writes=[R_rt])
        k.dma("sp", SKg, Dr["SK"][:, G * 512:(G + 1) * 512], writes=[R_rt])

    def A_kv(G):
        xb_ = G % 2
        for a in range(2):
            fm(a, 2 + a)
            k.op("act", lambda e, a=a: e.activation(out=xin[a][xb_][:, 16:528], in_=c.psum[2 + a], func=AF.Copy),
                 reads=[c.R_ps[2 + a]], writes=[R_xin[a][xb_]])
            k.op("pool", lambda e, a=a: e.tensor_copy(out=xin[a][1 - xb_][:, 0:16], in_=xin[a][xb_][:, 512:528]),
                 reads=[R_xin[a][xb_]], writes=[R_xin[a][1 - xb_]])
        for which, dst, R_dst in ((0, St.KTs, St.R_KTs), (1, St.KTw, St.R_KTw)):
            fm(2 + 2 * which, 2)
            fm(3 + 2 * which, 3)
            k.op("dve", lambda e: e.tensor_tensor(out=t1, in0=c.psum[2], in1=CKg, op=ALU.mult), reads=[c.R_ps[2], R_rt], writes=[R_t12])
            k.op("dve", lambda e: e.tensor_tensor(out=t2, in0=c.psum[3], in1=SKg, op=ALU.mult), reads=[c.R_ps[3], R_rt, R_t12], writes=[R_t12])
            k.op("pool", lambda e, dst=dst: e.tensor_tensor(out=dst[:, G * 512:(G + 1) * 512], in0=t1, in1=t2, op=ALU.add),
                 reads=[R_t12], writes=[R_dst])

    def A_tok(G):
        vt, R_vt = vtoks[G % 2], R_vtoks[G % 2]
        for tt in range(4):
            tile_ = 4 * G + tt
            k.op("pe", [(lambda e, i=i, tt=tt: e.matmul(c.psum[4][:, 0:256], lhsT=hT[:, i, tt * 128:(tt + 1) * 128], rhs=w1t[:, i, 0:256],
                                                        start=(i == 0), stop=(i == 7))) for i in range(8)],
                 reads=[R_w1, R_hT], writes=[c.R_ps[4]])
            k.op("pe", [(lambda e, i=i, tt=tt: e.matmul(c.psum[5], lhsT=hT[:, i, tt * 128:(tt + 1) * 128], rhs=w1t[:, i, 256:768],
                                                        start=(i == 0), stop=(i == 7))) for i in range(8)],
                 reads=[R_w1, R_hT], writes=[c.R_ps[5]])
            k.op("act", lambda e, tile_=tile_: e.activation(out=St.VsA[:, tile_, :, 0:64],
                                                            in_=c.psum[4][:, 0:128].rearrange("p (g d) -> p g d", g=2), func=AF.Copy),
                 reads=[c.R_ps[4]], writes=[St.R_VsA])
            k.op("act", lambda e, tile_=tile_: e.activation(out=St.VwA[:, tile_, :, 0:64],
                                                            in_=c.psum[4][:, 128:256].rearrange("p (g d) -> p g d", g=2), func=AF.Copy),
                 reads=[c.R_ps[4]], writes=[St.R_VwA])
            k.op("dve", lambda e, tt=tt: e.tensor_copy(out=vt[:, tt, :], in_=c.psum[5]), reads=[c.R_ps[5]], writes=[R_vt])

    def A_conv(G):
        xb_ = G % 2
        fns = []
        for xi in range(3):
            src = xin[0][xb_] if xi < 2 else xin[1][xb_]
            for l in range(32):
                for g in range(2):
                    fns.append(lambda e, xi=xi, g=g, l=l, src=src: e.matmul(
                        p6[64 * g:64 * g + 64, 32 * xi:32 * xi + 32], lhsT=WC[xi][64 * g:64 * g + 64, l, :],
                        rhs=src[64 * g:64 * g + 64, l:l + 497:16], start=(l == 0), stop=(l == 31)))
        k.op("pe", fns, reads=[R_WC, R_xin[0][xb_], R_xin[1][xb_]], writes=[c.R_ps[6]])
        ms = slice(32 * G, 32 * G + 32)
        k.dma("sp", CKc, Dr["CKc"][:, ms], writes=[R_ckc])
        k.dma("sp", SKc, Dr["SKc"][:, ms], writes=[R_ckc])
        k.op("dve", lambda e: e.tensor_scalar(out=ctmp[:, 0, :], in0=p6[:, 0:32], scalar1=cb[:, 0:1], scalar2=None, op0=ALU.add),
             reads=[c.R_ps[6], R_WC], writes=[R_ctmp])
        k.op("dve", lambda e: e.tensor_scalar(out=ctmp[:, 1, :], in0=p6[:, 32:64], scalar1=cb[:, 1:2], scalar2=None, op0=ALU.add),
             reads=[c.R_ps[6], R_WC], writes=[R_ctmp])
        k.op("dve", lambda e: e.tensor_scalar(out=VCf[:, ms], in0=p6[:, 64:96], scalar1=cb[:, 2:3], scalar2=None, op0=ALU.add),
             reads=[c.R_ps[6], R_WC], writes=[R_VCf])
        k.op("pool", lambda e: e.tensor_tensor(out=ctmp[:, 0, :], in0=ctmp[:, 0, :], in1=CKc, op=ALU.mult),
             reads=[R_ctmp, R_ckc], writes=[R_ctmp])
        k.op("pool", lambda e: e.tensor_tensor(out=ctmp[:, 1, :], in0=ctmp[:, 1, :], in1=SKc, op=ALU.mult),
             reads=[R_ctmp, R_ckc], writes=[R_ctmp])
        k.op("pool", lambda e: e.tensor_tensor(out=St.KC[:, ms], in0=ctmp[:, 0, :], in1=ctmp[:, 1, :], op=ALU.add),
             reads=[R_ctmp], writes=[St.R_KC])

    def front(hd):
        bank = 2 + hd % 2
        fm(6 + hd, bank)
        f_front(k, c, W, c.psum[bank], c.R_ps[bank], hd)

    def A_f(G):
        H = Hs[G % 2]
        front(0)
        front(1)
        A_tok(G)
        f_back(k, c, W, 0, H)
        front(2)
        A_conv(G)
        f_back(k, c, W, 1, H)
        front(3)
        f_back(k, c, W, 2, H)
        f_back(k, c, W, 3, H)

    def B_step(G, tt):
        H = Hs[G % 2]
        vt, R_vt = vtoks[G % 2], R_vtoks[G % 2]
        sl = slice(tt * 128, (tt + 1) * 128)
        k.op("pe", [(lambda e, hd=hd: e.transpose(out=p6b[:, hd * 128:(hd + 1) * 128], in_=H.kT[:, hd, sl], identity=c.identb))
                    for hd in range(4)], reads=[H.R_kT, c.R_const], writes=[c.R_ps[6]])
        k.op("act", lambda e: e.activation(out=ktok, in_=p6b[:, 0:512].rearrange("p (h x) -> p h x", h=4), func=AF.Copy),
             reads=[c.R_ps[6]], writes=[R_ktok])
        k.op("pe", [(lambda e, hd=hd: e.matmul(c.psum[7][:, hd * 128:(hd + 1) * 128], lhsT=ktok[:, hd, :],
                                               rhs=vt[:, tt, hd * 128:(hd + 1) * 128], start=True, stop=True))
                    for hd in range(4)], reads=[R_ktok, R_vt], writes=[c.R_ps[7]])
        Sf, Af = Sst.rearrange("p h x -> p (h x)"), snapacc.rearrange("p h x -> p (h x)")
        if tt == 0:
            k.op("dve", lambda e: e.tensor_scalar(out=Af, in0=Sf, scalar1=c.onehot[:, 0:1], scalar2=None, op0=ALU.mult),
                 reads=[R_S, c.R_const], writes=[R_snapacc])
        else:
            k.op("dve", lambda e: e.scalar_tensor_tensor(out=Af, in0=Sf, scalar=c.onehot[:, tt:tt + 1], in1=Af,
                                                         op0=ALU.mult, op1=ALU.add),
                 reads=[R_S, c.R_const, R_snapacc], writes=[R_snapacc])
        for hd in range(4):
            k.op("dve", lambda e, hd=hd: e.tensor_scalar(out=Sst[:, hd, :], in0=Sst[:, hd, :], scalar1=H.c1[:, hd, tt:tt + 1],
                                                         scalar2=None, op0=ALU.mult),
                 reads=[R_S, H.R_cols], writes=[R_S])
            k.op("dve", lambda e, hd=hd: e.scalar_tensor_tensor(
                out=Sst[:, hd, :], in0=c.psum[7][:, hd * 128:(hd + 1) * 128], scalar=H.c2[:, hd, tt:tt + 1], in1=Sst[:, hd, :],
                op0=ALU.mult, op1=ALU.add), reads=[c.R_ps[7], R_S, H.R_cols], writes=[R_S])
        if tt == 3:
            k.op("act", lambda e: e.activation(out=St.SNAP[:, G, :, :], in_=snapacc, func=AF.Copy), reads=[R_snapacc], writes=[St.R_SNAP])

    for G in range(NG + 1):
        if G < NG:
            A_x(G)
        if G >= 1:
            B_step(G - 1, 0)
            B_step(G - 1, 1)
        if G < NG:
            A_kv(G)
        if G >= 1:
            B_step(G - 1, 2)
            B_step(G - 1, 3)
        if G < NG:
            A_f(G)
    k.op("dve", lambda e: e.memset(St.VCA[:, :, :, 64:65], 1.0), writes=[St.R_VCA])
    for g in range(2):
        k.dma("pool", St.VCA[:, :, g, 65:193], Dr["ovl"], writes=[St.R_VCA])
    pw = c.psum[6]
    k.op("pe", [(lambda e, mt=mt: e.transpose(out=pw[:, mt * 128:(mt + 1) * 128], in_=VCf[:, mt * 128:(mt + 1) * 128], identity=c.identf))
                for mt in range(4)], reads=[R_VCf, c.R_const], writes=[c.R_ps[6]])
    for mt in range(4):
        k.op("act", lambda e, mt=mt: e.activation(out=St.VCA[:, mt, :, 0:64],
                                                  in_=pw[:, mt * 128:(mt + 1) * 128].rearrange("p (g d) -> p g d", g=2), func=AF.Copy),
             reads=[c.R_ps[6]], writes=[St.R_VCA])
    k.op("dve", lambda e: e.memset(St.VCA[0:1, 0, :, :], 0.0), writes=[St.R_VCA])
    ar.release(m0)


def phase_p2pre(k, c, ar, St):
    Dr = c.D
    m0 = ar.mark()
    W = WS()
    alloc_hg_ws(k, ar, W, nsets=2)
    alloc_x_ws(k, c, ar, W, None, scr=W.ab, R_scr=W.R_f)
    hT = ar.al([128, 8, 512], BF16)
    R_hT = k.res("hT2")
    wch = [c.R32f[:, 8192 + b * 4096: 8192 + (b + 1) * 4096].rearrange("p (a b) -> p a b", a=8) for b in range(2)]
    R_wch = [k.res("wch%d" % i) for i in range(2)]
    wgt = ar.al([128, 8, 32], BF16)
    R_wgt = k.res("wgt")
    wst = dict(n=0)
    CQg, SQg = ar.al([128, 512], F32), ar.al([128, 512], F32)
    R_rt = k.res("ropetabq")
    t1, t2, R_t12 = W.u, W.a, W.R_f
    qT = ar.al([128, 4, 512], BF16)
    R_qT = k.res("qTh")
    e1, R_e1 = W.u, W.R_f
    vtoks = [ar.al([128, 512], BF16) for _ in range(2)]
    R_vtoks = [k.res("vtok2_%d" % i) for i in range(2)]
    sgts = [ar.al([128, 512], F32) for _ in range(2)]
    R_sgts = [k.res("sgt%d" % i) for i in range(2)]
    AT = ar.al([128, 4, 128], BF16)
    R_AT = k.res("AT")
    Sp = ar.al([128, 4, 128], BF16)
    R_Sp = k.res("Sp")
    gnt = ar.al([128, 512], F32)
    R_gnt = k.res("gnt")
    o1, o2, R_o = W.bq, W.a, W.R_f
    yb = ar.al([128, 512], BF16)
    R_yb = k.res("yb")
    hs = ar.al([128, 16], F32)
    R_hs = k.res("hs")
    k.dma("pool", wgt, Dr["w2t"][:, 1024:1056].rearrange("(kt p) n -> p kt n", p=128), writes=[R_wgt])
    k.dma("sp", gnt, Dr["g_hg4"].partition_broadcast(128), writes=[R_gnt])

    def wload(src, c0, n=512):
        b = wst["n"] % 2
        wst["n"] += 1
        k.dma("pool", wch[b][:, :, 0:n], src[:, c0:c0 + n].rearrange("(kt p) n -> p kt n", p=128), writes=[R_wch[b]])
        return wch[b], R_wch[b]

    for go in range(NT // 4):
        tok = slice(go * 512, (go + 1) * 512)
        norm_transpose_group(k, c, W, Dr["xo"], go * 512, hT, R_hT)
        k.dma("sp", CQg, Dr["CQ"][:, tok], writes=[R_rt])
        k.dma("sp", SQg, Dr["SQ"][:, tok], writes=[R_rt])

        def fm(wt, R_wt, j, bank):
            k.op("pe", [(lambda e, i=i: e.matmul(c.psum[bank], lhsT=wt[:, i, j * 128:(j + 1) * 128], rhs=hT[:, i, :],
                                                 start=(i == 0), stop=(i == 7))) for i in range(8)],
                 reads=[R_wt, R_hT], writes=[c.R_ps[bank]])
        wq, R_wq = wload(Dr["w2f"], 0)
        wqp, R_wqp = wload(Dr["w2f"], 512)
        for a in range(4):
            fm(wq, R_wq, a, 2)
            fm(wqp, R_wqp, a, 3)
            k.op("dve", lambda e: e.tensor_tensor(out=t1, in0=c.psum[2], in1=CQg, op=ALU.mult), reads=[c.R_ps[2], R_rt], writes=[R_t12])
            k.op("dve", lambda e: e.tensor_tensor(out=t2, in0=c.psum[3], in1=SQg, op=ALU.mult), reads=[c.R_ps[3], R_rt, R_t12], writes=[R_t12])
            k.op("pool", lambda e, a=a: e.tensor_tensor(out=c.QT[:, 4 * go:4 * go + 4, a, :], in0=t1.rearrange("p (i t) -> p i t", i=4),
                                                        in1=t2.rearrange("p (i t) -> p i t", i=4), op=ALU.add), reads=[R_t12], writes=[c.R_QT])
        whq, R_whq = wload(Dr["w2f"], 1024)
        whf, R_whf = wload(Dr["w2f"], 1536)
        def front(hd):
            bank = 2 + hd % 2
            fm(whf, R_whf, hd, bank)
            f_front(k, c, W, c.psum[bank], c.R_ps[bank], hd)

        def back(hd):
            f_back(k, c, W, hd)
            su, sa, sbq, slk, sL, sRF = W.sets[hd % 2]
            fm(whq, R_whq, hd, 6)
            for tt in range(4):
                sl = slice(tt * 128, (tt + 1) * 128)
                k.op("act", lambda e, sl=sl, tt=tt: e.activation(out=su[:, sl], in_=sL[:, sl], func=AF.Exp, bias=W.negr[:, hd, tt:tt + 1]),
                     reads=[sRF, W.R_cols], writes=[sRF])
            k.op("dve", lambda e: e.tensor_tensor(out=qT[:, hd, :], in0=c.psum[6], in1=su, op=ALU.mult),
                 reads=[c.R_ps[6], sRF], writes=[R_qT])
        front(0)
        front(1)
        back(0)
        front(2)
        back(1)
        front(3)
        back(2)
        back(3)
        whi, R_whi = wload(Dr["w2t"], 0)
        whg, R_whg = wload(Dr["w2t"], 512)

        def s1(tt):
            i_own = 4 * go + tt
            pb = tt % 2
            sl = slice(tt * 128, (tt + 1) * 128)
            for (wt, R_wt, n, bank) in ((whi, R_whi, 512, pb), (whg, R_whg, 512, 2 + pb), (wgt, R_wgt, 32, 6)):
                k.op("pe", [(lambda e, i=i, wt=wt, n=n, bank=bank: e.matmul(c.psum[bank][:, 0:n], lhsT=hT[:, i, sl], rhs=wt[:, i, 0:n],
                                                                            start=(i == 0), stop=(i == 7))) for i in range(8)],
                     reads=[R_wt, R_hT], writes=[c.R_ps[bank]])
            k.op("dve", lambda e: e.tensor_copy(out=vtoks[pb], in_=c.psum[pb]), reads=[c.R_ps[pb]], writes=[R_vtoks[pb]])
            k.op("act", lambda e: e.activation(out=sgts[pb], in_=c.psum[2 + pb], func=AF.Silu), reads=[c.R_ps[2 + pb]], writes=[R_sgts[pb]])
            k.op("act", lambda e: e.activation(out=c.gsig[:, i_own, :], in_=c.psum[6][:, 0:24], func=AF.Sigmoid),
                 reads=[c.R_ps[6]], writes=[c.R_gsig])

        def s2(tt):
            i_own = 4 * go + tt
            pb = tt % 2
            vtok, R_vtok, sgt, R_sgt = vtoks[pb], R_vtoks[pb], sgts[pb], R_sgts[pb]
            sl = slice(tt * 128, (tt + 1) * 128)
            k.op("pe", [(lambda e, hd=hd: e.matmul(c.psum[7][:, hd * 128:(hd + 1) * 128], lhsT=W.kT[:, hd, sl], rhs=qT[:, hd, sl],
                                                   start=True, stop=True)) for hd in range(4)],
                 reads=[W.R_kT, R_qT], writes=[c.R_ps[7]])
            k.op("dve", lambda e: e.tensor_scalar(out=W.lk, in0=c.psum[7], scalar1=1e30, scalar2=-1e30, op0=ALU.min, op1=ALU.max),
                 reads=[c.R_ps[7], W.R_f], writes=[W.R_f])
            k.op("dve", lambda e: e.tensor_tensor(out=AT, in0=W.lk.rearrange("p (h x) -> p h x", h=4),
                                                  in1=c.tri.unsqueeze(1).broadcast_to([128, 4, 128]), op=ALU.mult),
                 reads=[W.R_f, c.R_constP], writes=[R_AT])
            for hd in range(4):
                k.op("act", lambda e, hd=hd: e.activation(out=Sp[:, hd, :], in_=St.SNAP[:, i_own, hd, :], func=AF.Copy,
                                                          scale=W.er[:, hd, tt:tt + 1]),
                     reads=[St.R_SNAP, W.R_cols], writes=[R_Sp])
            fns = []
            for hd in range(4):
                fns.append(lambda e, hd=hd: e.matmul(c.psum[4][:, hd * 128:(hd + 1) * 128], lhsT=AT[:, hd, :],
                                                     rhs=vtok[:, hd * 128:(hd + 1) * 128], start=True, stop=False))
                fns.append(lambda e, hd=hd: e.matmul(c.psum[4][:, hd * 128:(hd + 1) * 128], lhsT=qT[:, hd, sl],
                                                     rhs=Sp[:, hd, :], start=False, stop=True))
            k.op("pe", fns, reads=[R_AT, R_vtok, R_qT, R_Sp], writes=[c.R_ps[4]])
            for hd in range(4):
                k.op("act", lambda e, hd=hd: e.activation(out=o2[:, hd * 128:(hd + 1) * 128], in_=c.psum[4][:, hd * 128:(hd + 1) * 128],
                                                          func=AF.Square, accum_out=hs[:, hd:hd + 1]),
                     reads=[c.R_ps[4]], writes=[R_o, R_hs])
            k.op("act", lambda e: e.activation(out=hs[:, 4:8], in_=hs[:, 0:4], func=AF.Sqrt, scale=1.0 / 128, bias=c.epsb[:, 0:1]),
                 reads=[R_hs, c.R_const], writes=[R_hs])
            k.op("dve", lambda e: e.reciprocal(out=hs[:, 8:12], in_=hs[:, 4:8]), reads=[R_hs], writes=[R_hs])
            k.op("dve", lambda e: e.tensor_tensor(out=o1, in0=c.psum[4], in1=gnt, op=ALU.mult), reads=[c.R_ps[4], R_gnt, R_o], writes=[R_o])
            k.op("pool", lambda e: e.tensor_tensor(out=o1, in0=o1, in1=sgt, op=ALU.mult), reads=[R_o, R_sgt], writes=[R_o])
            k.op("dve", lambda e: e.tensor_tensor(out=yb.rearrange("p (h x) -> p h x", h=4), in0=o1.rearrange("p (h x) -> p h x", h=4),
                                                  in1=hs[:, 8:12].unsqueeze(2).broadcast_to([128, 4, 128]), op=ALU.mult),
                 reads=[R_o, R_hs], writes=[R_yb])
            p5b = c.psum[5].bitcast(BF16)
            k.op("pe", [(lambda e, hd=hd: e.transpose(out=p5b[:, hd * 128:(hd + 1) * 128], in_=yb[:, hd * 128:(hd + 1) * 128], identity=c.identb))
                        for hd in range(4)], reads=[R_yb, c.R_const], writes=[c.R_ps[5]])
            k.op("act", lambda e: e.activation(out=c.oT_hg[:, :, i_own * 128:(i_own + 1) * 128],
                                               in_=p5b[:, 0:512].rearrange("p (h x) -> p h x", h=4), func=AF.Copy),
                 reads=[c.R_ps[5]], writes=[c.R_oThg])
        s1(0)
        s1(1)
        s2(0)
        s1(2)
        s2(1)
        s1(3)
        s2(2)
        s2(3)
    ar.release(m0)


def phase_nsa(k, c, ar, St):
    Dr = c.D
    m0 = ar.mark()
    TINY = 1e-30
    CBi = [ar.al([128, 128], BF16) for _ in range(2)]
    VALi = [ar.al([128, 128], F32) for _ in range(2)]
    ADDCi = [ar.al([128, 128], F32) for _ in range(2)]
    R_tab = [k.res("nsatab%d" % i) for i in range(2)]
    R_tabP = [k.res("nsatabP%d" % i) for i in range(2)]
    WMT = ar.al([128, 8, 128], BF16)
    CM = ar.al([128, 4, 128], BF16)
    R_cst = k.res("nsacst")
    k.dma("pool", WMT, Dr["WMT"], writes=[R_cst])
    k.dma("pool", CM, Dr["CM"], writes=[R_cst])
    PT = [ar.al([128, 512], BF16) for _ in range(3)]
    R_PT = [k.res("PT%d" % i) for i in range(3)]
    Uc = ar.al([128, 4, 193], F32)
    R_Uc = k.res("Uc")
    Os = ar.al([128, 4, 65], F32)
    Ow = ar.al([128, 4, 65], F32)
    R_Os, R_Ow = k.res("Os"), k.res("Ow")
    score, sc2, imp = ar.al([128, 128], F32), ar.al([128, 128], F32), ar.al([128, 128], F32)
    R_sel = k.res("sel")
    selb = ar.al([128, 128], BF16)
    R_selb = k.res("selb")
    bd = ar.al([128, 4, 128], BF16)
    R_bd = k.res("bd")
    selX = ar.al([128, 128, 64], BF16)
    R_selX = [k.res("selX0"), k.res("selX1")]
    cols = ar.al([128, 64], F32)
    R_cols = k.res("nsacols")
    acc, tmp = ar.al([128, 4, 64], F32), ar.al([128, 4, 64], F32)
    R_acc = k.res("nsaacc")
    onsa = ar.al([128, 2, 4, 64], BF16)
    R_onsa = k.res("onsa")
    st = dict(s=0, p=0)
    pO_s, pO_w = c.psum[3][:, 0:260], c.psum[4][:, 0:260]
    pU = [c.psum[5], c.psum[6]]

    pend = []

    def flush_pv(keep=0):
        while len(pend) > keep:
            pend.pop(0)()

    def unit(KT, R_KT, kt_slice, QTg, g, bias, Vaug, R_V, outs, R_outs):
        sb = st["s"] % 3
        st["s"] += 1
        pb = st["p"] % 3
        st["p"] += 1
        S = c.psum[sb]
        fns = [lambda e: e.matmul(S, lhsT=KT[64 * g:64 * g + 64, kt_slice], rhs=QTg, start=True, stop=(bias is None))]
        rd = [R_KT, c.R_QT]
        if bias is not None:
            bl, R_bl = bias
            fns.append(lambda e: e.matmul(S, lhsT=bl, rhs=c.I4, start=False, stop=True))
            rd += [R_bl, c.R_constP]
        k.op("pe", fns, reads=rd, writes=[c.R_ps[sb]])
        k.op("act", lambda e: e.activation(out=PT[pb], in_=S, func=AF.Exp), reads=[c.R_ps[sb]], writes=[R_PT[pb]])

        def pv():
            k.op("pe", [(lambda e, a=a: e.matmul(outs[a], lhsT=PT[pb][:, a * 128:(a + 1) * 128], rhs=Vaug, start=False, stop=False,
                                                 skip_group_check=True)) for a in range(4)],
                 reads=[R_PT[pb], R_V], writes=R_outs)
        pend.append(pv)
        flush_pv(keep=2)

    for i in range(c.n_blocks):
        tb = i % 2
        k.dma("pool", CBi[tb], Dr["CB"][:, i, :], writes=[R_tabP[tb]])
        k.dma("sp", VALi[tb], Dr["VAL"][:, i, :], writes=[R_tab[tb]])
        k.dma("sp", ADDCi[tb], Dr["ADDC"][:, i, :], writes=[R_tab[tb]])
        for g in range(2):
            QTg = c.QT[64 * g:64 * g + 64, i, :, :].rearrange("p a t -> p (a t)")
            nmt = i // 4 + 1
            k.op("dve", lambda e: e.memset(pU[0], 0.0), writes=[c.R_ps[5]])
            k.op("dve", lambda e: e.memset(pU[1], 0.0), writes=[c.R_ps[6]])
            outsU = [pU[a // 2][:, (a % 2) * 193:(a % 2) * 193 + 193] for a in range(4)]
            for mt in range(nmt):
                bias = (CBi[tb], R_tabP[tb]) if mt == nmt - 1 else None
                unit(St.KC, St.R_KC, slice(mt * 128, (mt + 1) * 128), QTg, g, bias, St.VCA[:, mt, g, :], St.R_VCA, outsU, [c.R_ps[5], c.R_ps[6]])
            flush_pv()
            k.op("act", lambda e: e.activation(out=Uc[:, 0:2, :], in_=pU[0][:, 0:386].rearrange("p (a x) -> p a x", a=2), func=AF.Copy),
                 reads=[c.R_ps[5]], writes=[R_Uc])
            k.op("act", lambda e: e.activation(out=Uc[:, 2:4, :], in_=pU[1][:, 0:386].rearrange("p (a x) -> p a x", a=2), func=AF.Copy),
                 reads=[c.R_ps[6]], writes=[R_Uc])
            k.op("dve", lambda e: e.memset(c.psum[4], 0.0), writes=[c.R_ps[4]])
            outsW = [pO_w[:, a * 65:(a + 1) * 65] for a in range(4)]
            for r in range(8):
                kt = 4 * i - 4 + r
                if kt < 0:
                    continue
                unit(St.KTw, St.R_KTw, slice(kt * 128, (kt + 1) * 128), QTg, g, (WMT[:, r, :], R_cst), St.VwA[:, kt, g, :], St.R_VwA, outsW, [c.R_ps[4]])
            zc, rzc = cols[:, 0:4], cols[:, 4:8]
            k.op("dve", lambda e: e.tensor_scalar(out=zc, in0=Uc[:, :, 64], scalar1=TINY, scalar2=None, op0=ALU.max), reads=[R_Uc], writes=[R_cols])
            k.op("dve", lambda e: e.reciprocal(out=rzc, in_=zc), reads=[R_cols], writes=[R_cols])
            k.op("dve", lambda e: e.tensor_scalar(out=imp, in0=Uc[:, 0, 65:193], scalar1=rzc[:, 0:1], scalar2=None, op0=ALU.mult),
                 reads=[R_Uc, R_cols], writes=[R_sel])
            for a in range(1, 4):
                k.op("dve", lambda e, a=a: e.scalar_tensor_tensor(out=imp, in0=Uc[:, a, 65:193], scalar=rzc[:, a:a + 1], in1=imp,
                                                                  op0=ALU.mult, op1=ALU.add), reads=[R_Uc, R_cols, R_sel], writes=[R_sel])
            k.op("dve", lambda e: e.tensor_tensor(out=score, in0=imp, in1=VALi[tb], op=ALU.mult), reads=[R_sel, R_tab[tb]], writes=[R_sel])
            k.op("dve", lambda e: e.tensor_tensor(out=score, in0=score, in1=ADDCi[tb], op=ALU.add), reads=[R_sel, R_tab[tb]], writes=[R_sel])
            m8a, m8b = cols[:, 8:16], cols[:, 16:24]
            k.op("dve", lambda e: e.max(out=m8a, in_=score), reads=[R_sel], writes=[R_cols])
            k.op("dve", lambda e: e.match_replace(out=sc2, in_to_replace=m8a, in_values=score, imm_value=-1e9), reads=[R_sel, R_cols], writes=[R_sel])
            k.op("dve", lambda e: e.max(out=m8b, in_=sc2), reads=[R_sel], writes=[R_cols])
            k.op("dve", lambda e: e.tensor_scalar(out=selb, in0=score, scalar1=m8b[:, 7:8], scalar2=NEG, op0=ALU.is_lt, op1=ALU.mult),
                 reads=[R_sel, R_cols], writes=[R_selb])
            for r in range(4):
                kt = 4 * i + r
                k.op("dve", lambda e, r=r, kt=kt: e.tensor_tensor(
                    out=bd[:, r, :].rearrange("p (b x) -> p b x", b=2), in0=CM[:, r, :].rearrange("p (b x) -> p b x", b=2),
                    in1=selb[:, 2 * kt:2 * kt + 2].unsqueeze(2).broadcast_to([128, 2, 64]), op=ALU.add),
                    reads=[R_cst, R_selb], writes=[R_bd])
            if i > 0:
                for hx in range(2):
                    b0_, b1_ = 4 * i * hx, 4 * i * (hx + 1)
                    k.op("dve", lambda e, b0_=b0_, b1_=b1_: e.tensor_copy(
                        out=selX[:, b0_:b1_, :], in_=selb[:, b0_:b1_].unsqueeze(2).broadcast_to([128, b1_ - b0_, 64])),
                        reads=[R_selb], writes=[R_selX[hx]])
            k.op("dve", lambda e: e.memset(c.psum[3], 0.0), writes=[c.R_ps[3]])
            outsS = [pO_s[:, a * 65:(a + 1) * 65] for a in range(4)]
            for kt in list(range(4 * i, 4 * i + 4)) + list(range(4 * i)):
                if kt < 4 * i:
                    bl = selX[:, 2 * kt:2 * kt + 2, :].rearrange("p b x -> p (b x)")
                    bias = (bl, R_selX[0 if kt < 2 * i else 1])
                else:
                    bias = (bd[:, kt - 4 * i, :], R_bd)
                unit(St.KTs, St.R_KTs, slice(kt * 128, (kt + 1) * 128), QTg, g, bias, St.VsA[:, kt, g, :], St.R_VsA, outsS, [c.R_ps[3]])
            flush_pv()
            k.op("act", lambda e: e.activation(out=Os, in_=pO_s.rearrange("p (a x) -> p a x", a=4), func=AF.Copy), reads=[c.R_ps[3]], writes=[R_Os])
            k.op("act", lambda e: e.activation(out=Ow, in_=pO_w.rearrange("p (a x) -> p a x", a=4), func=AF.Copy), reads=[c.R_ps[4]], writes=[R_Ow])
            gs = c.gsig[:, i, 12 * g:12 * g + 12].rearrange("p (a x) -> p a x", a=4)
            zs, zw, cfc, cfs, cfw = cols[:, 24:28], cols[:, 28:32], cols[:, 32:36], cols[:, 36:40], cols[:, 40:44]
            k.op("dve", lambda e: e.tensor_scalar(out=zs, in0=Os[:, :, 64], scalar1=TINY, scalar2=None, op0=ALU.max), reads=[R_Os], writes=[R_cols])
            k.op("dve", lambda e: e.tensor_scalar(out=zw, in0=Ow[:, :, 64], scalar1=TINY, scalar2=None, op0=ALU.max), reads=[R_Ow], writes=[R_cols])
            k.op("dve", lambda e: e.reciprocal(out=zs, in_=zs), reads=[R_cols], writes=[R_cols])
            k.op("dve", lambda e: e.reciprocal(out=zw, in_=zw), reads=[R_cols], writes=[R_cols])
            k.op("dve", lambda e: e.tensor_tensor(out=cfc, in0=rzc, in1=gs[:, :, 0], op=ALU.mult), reads=[R_cols, c.R_gsig], writes=[R_cols])
            k.op("dve", lambda e: e.tensor_tensor(out=cfs, in0=zs, in1=gs[:, :, 1], op=ALU.mult), reads=[R_cols, c.R_gsig], writes=[R_cols])
            k.op("dve", lambda e: e.tensor_tensor(out=cfw, in0=zw, in1=gs[:, :, 2], op=ALU.mult), reads=[R_cols, c.R_gsig], writes=[R_cols])
            bc = lambda col: col.unsqueeze(2).broadcast_to([128, 4, 64])
            k.op("dve", lambda e: e.tensor_tensor(out=acc, in0=Uc[:, :, 0:64], in1=bc(cfc), op=ALU.mult), reads=[R_Uc, R_cols], writes=[R_acc])
            k.op("dve", lambda e: e.tensor_tensor(out=tmp, in0=Os[:, :, 0:64], in1=bc(cfs), op=ALU.mult), reads=[R_Os, R_cols, R_acc], writes=[R_acc])
            k.op("pool", lambda e: e.tensor_tensor(out=acc, in0=acc, in1=tmp, op=ALU.add), reads=[R_acc], writes=[R_acc])
            k.op("dve", lambda e: e.tensor_tensor(out=tmp, in0=Ow[:, :, 0:64], in1=bc(cfw), op=ALU.mult), reads=[R_Ow, R_cols, R_acc], writes=[R_acc])
            k.op("pool", lambda e, g=g: e.tensor_tensor(out=onsa[:, g, :, :], in0=acc, in1=tmp, op=ALU.add), reads=[R_acc], writes=[R_onsa])
        p7b = c.psum[7].bitcast(BF16)
        of = onsa.rearrange("p g a d -> p (g a d)")
        k.op("pe", [(lambda e, j=j: e.transpose(out=p7b[:, j * 128:(j + 1) * 128], in_=of[:, j * 128:(j + 1) * 128], identity=c.identb))
                    for j in range(4)], reads=[R_onsa, c.R_const], writes=[c.R_ps[7]])
        k.op("act", lambda e, i=i: e.activation(out=c.oT_nsa[:, :, i * 128:(i + 1) * 128],
                                                in_=p7b[:, 0:512].rearrange("p (j x) -> p j x", j=4), func=AF.Copy),
             reads=[c.R_ps[7]], writes=[c.R_oTnsa])
    ar.release(m0)


def phase_p2c(k, c, ar, X1, R_X1):
    Dr = c.D
    m0 = ar.mark()
    W = WS()
    scr = ar.al([128, D], F32)
    alloc_x_ws(k, c, ar, W, None, scr=scr, R_scr=k.res("scr2c"))
    hT = ar.al([128, 8, 512], BF16)
    R_hT = k.res("hT3")
    wbn, wbh = ar.al([128, 4, D], BF16), ar.al([128, 4, D], BF16)
    R_wb_ = k.res("wbr")
    wb = [ar.al([128, 8, 512], BF16) for _ in range(4)]
    R_wb = [k.res("wchc%d" % i) for i in range(4)]
    mixT = ar.al([128, 8, 512], BF16)
    R_mixT = k.res("mixT")
    sg1, sg2, mx1 = ar.al([128, 512], F32), ar.al([128, 512], F32), ar.al([128, 512], F32)
    R_sg1, R_sg2, R_mx = k.res("sg1"), k.res("sg2"), k.res("mx1")
    k.dma("pool", wbn, Dr["w_brn"].rearrange("(kt p) n -> p kt n", p=128), writes=[R_wb_])
    k.dma("pool", wbh, Dr["w_brh"].rearrange("(kt p) n -> p kt n", p=128), writes=[R_wb_])

    def wl(buf, src, c0):
        k.dma("pool", wb[buf], src[:, c0:c0 + 512].rearrange("(kt p) n -> p kt n", p=128), writes=[R_wb[buf]])

    NGo = NT // 4
    wl(0, Dr["w_mg"], 0)
    wl(1, Dr["w_mg"], 1024)
    for go in range(NGo):
        p = go % 2
        A = (2 * p, 2 * p + 1)
        B = (2 - 2 * p, 3 - 2 * p)
        tok = slice(go * 512, (go + 1) * 512)
        for tt in range(4):
            t = 4 * go + tt
            k.dma("sp", X1[:, t, :], Dr["xo"][t * 128:(t + 1) * 128, :], writes=[R_X1[t]])
        wl(B[0], Dr["w_mg"], 512)
        wl(B[1], Dr["w_mg"], 1024 + 512)
        norm_transpose_group(k, c, W, Dr["xo"], go * 512, hT, R_hT)
        for hf in range(2):
            w0, w1_ = (A if hf == 0 else B)
            for f4 in range(4):
                ft = hf * 4 + f4
                fs = slice(f4 * 128, (f4 + 1) * 128)
                gs_ = slice(ft * 128, (ft + 1) * 128)
                k.op("pe", [(lambda e, i=i: e.matmul(c.psum[2], lhsT=wb[w0][:, i, fs], rhs=hT[:, i, :], start=(i == 0), stop=(i == 7)))
                            for i in range(8)], reads=[R_wb[w0], R_hT], writes=[c.R_ps[2]])
                k.op("pe", [(lambda e, i=i: e.matmul(c.psum[3], lhsT=wb[w1_][:, i, fs], rhs=hT[:, i, :], start=(i == 0), stop=(i == 7)))
                            for i in range(8)], reads=[R_wb[w1_], R_hT], writes=[c.R_ps[3]])
                k.op("pe", [(lambda e, i=i: e.matmul(c.psum[4], lhsT=wbn[:, i, gs_], rhs=c.oT_nsa[:, i, tok], start=(i == 0), stop=(i == 3)))
                            for i in range(4)], reads=[R_wb_, c.R_oTnsa], writes=[c.R_ps[4]])
                k.op("pe", [(lambda e, i=i: e.matmul(c.psum[5], lhsT=wbh[:, i, gs_], rhs=c.oT_hg[:, i, tok], start=(i == 0), stop=(i == 3)))
                            for i in range(4)], reads=[R_wb_, c.R_oThg], writes=[c.R_ps[5]])
                k.op("act", lambda e: e.activation(out=sg1, in_=c.psum[2], func=AF.Sigmoid), reads=[c.R_ps[2]], writes=[R_sg1])
                k.op("act", lambda e: e.activation(out=sg2, in_=c.psum[3], func=AF.Sigmoid), reads=[c.R_ps[3]], writes=[R_sg2])
                k.op("dve", lambda e: e.tensor_tensor(out=mx1, in0=c.psum[4], in1=sg1, op=ALU.mult), reads=[c.R_ps[4], R_sg1], writes=[R_mx])
                k.op("dve", lambda e: e.tensor_tensor(out=sg2, in0=c.psum[5], in1=sg2, op=ALU.mult), reads=[c.R_ps[5], R_sg2], writes=[R_sg2])
                k.op("pool", lambda e, ft=ft: e.tensor_tensor(out=mixT[:, ft, :], in0=mx1, in1=sg2, op=ALU.add),
                     reads=[R_mx, R_sg2], writes=[R_mixT])
            if hf == 0:
                wl(A[0], Dr["w_out"], 0)
                wl(A[1], Dr["w_out"], 512)
            elif go + 1 < NGo:
                wl(B[0], Dr["w_mg"], 0)
                wl(B[1], Dr["w_mg"], 1024)
        for tt in range(4):
            t = 4 * go + tt
            for hf in range(2):
                bank = 6 + hf
                k.op("pe", [(lambda e, i=i: e.matmul(c.psum[bank], lhsT=mixT[:, i, tt * 128:(tt + 1) * 128], rhs=wb[A[hf]][:, i, :],
                                                     start=(i == 0), stop=(i == 7))) for i in range(8)],
                     reads=[R_mixT, R_wb[A[hf]]], writes=[c.R_ps[bank]])
                xs = X1[:, t, hf * 512:(hf + 1) * 512]
                k.op("dve", lambda e, xs=xs: e.tensor_tensor(out=xs, in0=c.psum[bank], in1=xs, op=ALU.add),
                     reads=[c.R_ps[bank], R_X1[t]], writes=[R_X1[t]])
    ar.release(m0)


def phase_attn(k, c, ar, stage):
    St = WS()
    St.KTs, St.KTw = ar.al([128, SEQ], BF16), ar.al([128, SEQ], BF16)
    St.VsA, St.VwA = ar.al([128, 64, 2, 65], BF16), ar.al([128, 64, 2, 65], BF16)
    St.KC = ar.al([128, 512], BF16)
    St.VCA = ar.al([128, 4, 2, 193], BF16)
    St.SNAP = ar.al([128, NT, 4, 128], BF16)
    for n in ("KTs", "KTw", "VsA", "VwA", "KC", "VCA", "SNAP"):
        setattr(St, "R_" + n, k.res(n))
    phase_p1(k, c, ar, St)
    for n in ("KTs", "KTw", "VsA", "VwA", "KC", "VCA", "SNAP"):
        c.dump(n, getattr(St, n), [getattr(St, "R_" + n)])
    k.flush()
    phase_p2pre(k, c, ar, St)
    c.dump("QT", c.QT, [c.R_QT])
    c.dump("gsig", c.gsig, [c.R_gsig])
    c.dump("oT_hg", c.oT_hg, [c.R_oThg])
    k.flush()
    phase_nsa(k, c, ar, St)
    c.dump("oT_nsa", c.oT_nsa, [c.R_oTnsa])
    return St


def build(stage="full", n_exp=NE):
    nc = bass.Bass("TRN2", target_bir_lowering=False)
    Dr = {}

    def din(name, shape, dt=F32):
        Dr[name] = nc.dram_tensor(name, list(shape), dt, kind="ExternalInput").ap()

    for name, shape, dt in input_specs(max(n_exp, 1), stage):
        din(name, shape, dt)
    Dr["y"] = nc.dram_tensor("y", [NTOK, D], F32, kind="ExternalOutput").ap()
    with ExitStack() as es:
        ARENA_BYTES = 207 * 1024
        big = es.enter_context(nc.sbuf_tensor("arena", [128, ARENA_BYTES // 2], BF16))
        pst = es.enter_context(nc.psum_tensor("ps", [128, 4096], F32))
        ar = Arena(big, ARENA_BYTES)
        k = KH(nc, es)
        k.oplim = _NC_CACHE.get("oplim", 10 ** 9)
        c = Ctx()
        c.nc, c.D, c.n_exp = nc, Dr, n_exp
        dumps = []

        def dump(name, ap, rs):
            if not _NC_CACHE.get("dbg"):
                return
            dt_ = ap.dtype
            dr = nc.dram_tensor("dbg_" + name, list(ap.shape), dt_, kind="ExternalOutput").ap()
            r = k.res("dbg_" + name)
            k.dma("sp", dr, ap, reads=list(rs), key=r)
            dumps.append(r)
        c.dump = dump
        c.psum = [pst[:, i * 512:(i + 1) * 512] for i in range(8)]
        c.pwide = lambda i: pst[:, i * 512:(i + 2) * 512]
        c.R_ps = [k.res("psb%d" % i, excl=True) for i in range(8)]
        c.R_const = k.res("const")
        c.identf = ar.al([128, 128], F32)
        c.identb = ar.al([128, 128], BF16)
        c.epsb = ar.al([128, 1], F32)
        c.ssq = ar.al([128, 8], F32)
        c.std = ar.al([128, 8], F32)
        c.rstd = ar.al([128, 8], F32)
        c.rs_res = [k.res("rs%d" % i) for i in range(8)]
        c.rs_i = 0
        k.dma("sp", c.identf, Dr["identf"], writes=[c.R_const])
        k.dma("sp", c.identb, Dr["identb"], writes=[c.R_const])
        k.op("dve", lambda e: e.memset(c.epsb, EPS), writes=[c.R_const])
        c.n_groups = _NC_CACHE.get("n_groups", 16)
        c.n_blocks = _NC_CACHE.get("n_blocks", NT)
        c.R32 = ar.al([128, 8, NTOK], BF16)
        c.R32f = c.R32.rearrange("p a b -> p (a b)")
        c.QT = c.R32f[:, 0:8192].rearrange("p (i a t) -> p i a t", i=NT, a=4)
        c.oT_nsa = c.R32[:, 4:8, :]
        c.oT_hg = ar.al([128, 4, NTOK], BF16)
        c.gsig = ar.al([128, NT, 24], F32)
        c.R_QT, c.R_oTnsa, c.R_oThg, c.R_gsig = k.res("QT"), k.res("oTnsa"), k.res("oThg"), k.res("gsig")
        hfT = c.R32
        R_hfT = [k.res("hfT_%d" % t) for t in range(NT)]
        R_X1 = [k.res("x1_%d" % t) for t in range(NT)]
        if stage != "moe_only":
            c.I4 = ar.al([128, 512], BF16)
            c.tri = ar.al([128, 128], BF16)
            c.onehot = ar.al([128, 4], F32)
            c.R_constP = k.res("constP")
            k.dma("pool", c.I4, Dr["I4"], writes=[c.R_constP])
            k.dma("pool", c.tri, Dr["tri"], writes=[c.R_constP])
            k.dma("sp", c.onehot, Dr["onehot"], writes=[c.R_const])
            setup_lb(k, c, ar)
            M1 = ar.mark()
            phase_attn(k, c, ar, stage)
            for r in dumps:
                k.rec["sp"].append(("w", r.dsem, r.dcnt))
            k.flush()
            ar.release(M1)
        X1 = ar.al([128, NT, D], F32)
        if stage == "moe_only":
            for t in range(NT):
                k.dma("sp", X1[:, t, :], Dr["xo"][t * 128:(t + 1) * 128, :], writes=[R_X1[t]])
        else:
            phase_p2c(k, c, ar, X1, R_X1)
            c.dump("X1", X1, R_X1)
            for r in dumps:
                if r.name == "dbg_X1":
                    k.rec["sp"].append(("w", r.dsem, r.dcnt))
        k.flush()
        if n_exp >= 0:
            phase_moe(k, c, ar, X1, R_X1, hfT, R_hfT)
            k.flush()
        phase_final(k, c, ar, X1, R_X1)
        k.flush()
    return nc


def input_specs(ne=NE, stage="full"):
    return [
        ("xb", (SEQ, D), F32), ("xo", (NTOK, D), F32),
        ("g_ffn", (D,), F32), ("g_fin", (D,), F32),
        ("w_r", (D, 36), F32), ("b_r", (36,), F32),
        ("w_gate", (ne, D, DFF), F32), ("w_up", (ne, D, DFF), F32), ("w_down", (ne, DFF, D), F32),
        ("identf", (128, 128), F32), ("identb", (128, 128), BF16),
    ] + (attn_input_specs() if stage != "moe_only" else [])


_NC_CACHE = {}


def host_inputs(inp, core, ne=NE):
    b, cp = core // 4, core % 4
    x = np.asarray(inp["x"], dtype=np.float32)
    m = {}
    m["xb"] = np.ascontiguousarray(x[b])
    m["xo"] = np.ascontiguousarray(x[b].reshape(NT, 4, 128, D)[:, cp].reshape(NTOK, D))
    m["g_ffn"] = np.ascontiguousarray(inp["ffn_norm"][0])
    m["g_fin"] = np.ascontiguousarray(inp["final_norm"])
    m["w_r"] = np.ascontiguousarray(np.concatenate([inp["w_grp"][0], inp["w_rtr"][0]], axis=1))
    m["b_r"] = np.ascontiguousarray(np.concatenate([inp["b_grp"][0], inp["b_rtr"][0]], axis=0))
    m["w_gate"] = np.ascontiguousarray(inp["w_gate"][0, :ne])
    m["w_up"] = np.ascontiguousarray(inp["w_up"][0, :ne])
    m["w_down"] = np.ascontiguousarray(inp["w_down"][0, :ne])
    m["identf"] = np.eye(128, dtype=np.float32)
    m["identb"] = np.eye(128, dtype=np.float32).astype(ml_dtypes.bfloat16)
    if _NC_CACHE.get("stage", "full") != "moe_only":
        m.update(attn_host_inputs(inp, b, cp))
    return m


def kernel(**inp):
    inp = {k_: np.asarray(v) for k_, v in inp.items()}
    stage = _NC_CACHE.get("stage", "full")
    key = ("nc", stage)
    if key not in _NC_CACHE:
        _NC_CACHE[key] = build(stage, _NC_CACHE.get("n_exp", NE))
    nc = _NC_CACHE[key]
    shared = None
    in_maps = []
    for core in range(8):
        m = host_inputs(inp, core, max(_NC_CACHE.get("n_exp", NE), 1))
        if shared is None:
            shared = m
        else:
            for kk in ("w_gate", "w_up", "w_down"):
                m[kk] = shared[kk]
        in_maps.append(m)
    res = run_bass_kernel_spmd(nc, in_maps, core_ids=list(range(8)))
    _NC_CACHE["last_results"] = res.results
    out = np.zeros((2, SEQ // 128, 128, D), dtype=np.float32)
    for core in range(8):
        b, cp = core // 4, core % 4
        y = np.asarray(res.results[core]["y"]).reshape(NT, 128, D)
        out[b, cp::4] = y
    return out.reshape(2, SEQ, D)
```

```python
import numpy as np
import ml_dtypes
import concourse.bass as bass
import concourse.mybir as mybir
from concourse.bass_utils import run_bass_kernel_spmd
from contextlib import ExitStack

F32 = mybir.dt.float32
BF16 = mybir.dt.bfloat16
AF = mybir.ActivationFunctionType
ALU = mybir.AluOpType
AX = mybir.AxisListType


class Res:
    __slots__ = ("name", "w", "rs", "dsem", "dcnt", "excl")

    def __init__(self, name, excl=False):
        self.name = name
        self.excl = excl
        self.w = None
        self.rs = {}
        self.dsem = None
        self.dcnt = 0


class _Proxy:
    def __init__(self):
        self.calls = []

    def __getattr__(self, name):
        def rec(*a, **kw):
            self.calls.append((name, a, kw))
        return rec


def _bind(f):
    p = _Proxy()
    f(p)
    assert len(p.calls) == 1, "one engine instruction per callable"
    name, a, kw = p.calls[0]
    return lambda eng: getattr(eng, name)(*a, **kw)


class KH:
    ENG = ("pe", "dve", "act", "pool", "sp")

    def __init__(self, nc, es):
        self.nc = nc
        self.es = es
        self.sem = {}
        self.cnt = {}
        for e in self.ENG:
            self.sem[e] = es.enter_context(nc.semaphore("s_" + e))
            self.cnt[e] = 0
        self.rec = {e: [] for e in self.ENG}
        self.seen = {e: {} for e in self.ENG}
        self.nsem = len(self.ENG)
        self.semobj = dict(self.sem)

    def res(self, name, excl=False):
        return Res(name, excl)

    def _dma_sem(self, r):
        if r.dsem is None:
            r.dsem = "d_" + r.name + "_%d" % self.nsem
            self.semobj[r.dsem] = self.es.enter_context(self.nc.semaphore(r.dsem))
            self.nsem += 1
        return r.dsem

    def _deps(self, e, reads, writes):
        deps = {}
        for r in reads:
            if r.w is not None:
                k, v = r.w
                deps[k] = max(deps.get(k, 0), v)
        for w in writes:
            if w.w is not None:
                k, v = w.w
                deps[k] = max(deps.get(k, 0), v)
            for k, v in w.rs.items():
                deps[k] = max(deps.get(k, 0), v)
        seen = self.seen[e]
        for k, v in deps.items():
            if seen.get(k, 0) >= v:
                continue
            seen[k] = v
            self.rec[e].append(("w", k, v))

    def op(self, e, fns, reads=(), writes=()):
        if callable(fns):
            fns = [fns]
        self.opn = getattr(self, "opn", 0) + 1
        if self.opn > getattr(self, "oplim", 10 ** 9):
            return
        ex = [r for r in reads if r.excl]
        if ex:
            reads = [r for r in reads if not r.excl]
            writes = list(writes) + [r for r in ex if r not in writes]
        self._deps(e, reads, writes)
        self.cnt[e] += 1
        v = self.cnt[e]
        fns = [_bind(f) for f in fns]
        for f in fns[:-1]:
            self.rec[e].append(("i", f, None, 0))
        self.rec[e].append(("i", fns[-1], e, 1))
        self.seen[e][e] = max(self.seen[e].get(e, 0), 0)
        for r in reads:
            r.rs[e] = v
        for w in writes:
            w.w = (e, v)
            w.rs = {}

    def dma(self, q, out, in_, reads=(), writes=(), key=None, **kw):
        self._deps(q, reads, writes)
        kr = key or (writes[0] if writes else reads[0])
        sk = self._dma_sem(kr)
        kr.dcnt += 16
        v = kr.dcnt
        self.rec[q].append(("i", lambda eng: eng.dma_start(out=out, in_=in_, **kw), sk, 16))
        for r in reads:
            r.rs[sk] = v
        for w in writes:
            w.w = (sk, v)
            w.rs = {}

    def wait_res(self, e, rs):
        self._deps(e, rs, ())

    def simulate(self):
        if not hasattr(self, "simval"):
            self.simval = {}
        val = self.simval
        ptr = {e: 0 for e in self.ENG}
        prog = True
        while prog:
            prog = False
            for e in self.ENG:
                items = self.rec[e]
                while ptr[e] < len(items):
                    it = items[ptr[e]]
                    if it[0] == "w":
                        if val.get(it[1], 0) >= it[2]:
                            ptr[e] += 1
                            prog = True
                        else:
                            break
                    else:
                        if it[2] is not None:
                            val[it[2]] = val.get(it[2], 0) + it[3]
                        ptr[e] += 1
                        prog = True
        for e in self.ENG:
            if ptr[e] < len(self.rec[e]):
                it = self.rec[e][ptr[e]]
                raise RuntimeError("DEADLOCK: engine %s stuck at item %d/%d waiting %s >= %s (have %s)" % (
                    e, ptr[e], len(self.rec[e]), it[1], it[2], val.get(it[1], 0)))

    def flush(self, name=None):
        nc = self.nc
        rec = self.rec
        semobj = self.semobj
        self.simulate()
        import os
        if os.environ.get("KH_DEBUG"):
            print("KH flush: ops so far", getattr(self, "opn", 0), {e: len(v) for e, v in self.rec.items()}, "nsem", self.nsem, flush=True)

        def play(eng, items):
            for it in items:
                if it[0] == "w":
                    eng.wait_ge(semobj[it[1]], it[2])
                else:
                    ins = it[1](eng)
                    if it[2] is not None:
                        ins.then_inc(semobj[it[2]], it[3])

        with nc.Block() as block:
            if rec["sp"]:
                @block.sync
                def _(eng):
                    play(eng, rec["sp"])
            if rec["pe"]:
                @block.tensor
                def _(eng):
                    play(eng, rec["pe"])
            if rec["dve"]:
                @block.vector
                def _(eng):
                    play(eng, rec["dve"])
            if rec["act"]:
                @block.scalar
                def _(eng):
                    play(eng, rec["act"])
            if rec["pool"]:
                @block.gpsimd
                def _(eng):
                    play(eng, rec["pool"])
        self.rec = {e: [] for e in self.ENG}

NEG = -30000.0
NT = 16
NTOK = NT * 128
SEQ = 8192
D = 1024
NE = 32
DFF = 512
EPS = 1e-6


class Arena:
    def __init__(self, big, nbytes):
        self.big = big
        self.n = nbytes
        self.off = 0

    def mark(self):
        return self.off

    def release(self, m):
        import os
        if os.environ.get("KH_DEBUG"):
            print("arena release: peak", getattr(self, "peak", 0), "->", m, "of", self.n, flush=True)
        self.peak = m
        self.off = m

    def al(self, shape, dt):
        esz = 4 if dt == F32 else 2
        per = int(np.prod(shape[1:])) * esz
        self.off = (self.off + 63) // 64 * 64
        o = self.off
        assert o + per <= self.n, ("arena overflow", o, per, self.n)
        self.off = o + per
        self.peak = max(getattr(self, "peak", 0), self.off)
        v = self.big[0:shape[0], o // 2:(o + per) // 2]
        if dt == F32:
            v = v.bitcast(F32)
        if len(shape) == 3:
            v = v.rearrange("p (a b) -> p a b", a=shape[1])
        elif len(shape) == 4:
            v = v.rearrange("p (a b c) -> p a b c", a=shape[1], b=shape[2])
        return v


class Ctx:
    pass


def rms_rstd(k, c, src, src_res, scr, scr_res, n_feat, tag):
    i = c.rs_i % 8
    c.rs_i += 1
    ssq, std, rstd = c.ssq[:, i:i + 1], c.std[:, i:i + 1], c.rstd[:, i:i + 1]
    R = c.rs_res[i]
    k.op("act", lambda e: e.activation(out=scr, in_=src, func=AF.Square, accum_out=ssq),
         reads=[src_res], writes=[scr_res, R])
    k.op("act", lambda e: e.activation(out=std, in_=ssq, func=AF.Sqrt, scale=1.0 / n_feat, bias=c.epsb[:, 0:1]),
         reads=[R, c.R_const], writes=[R])
    k.op("dve", lambda e: e.reciprocal(out=rstd, in_=std), reads=[R], writes=[R])
    return rstd, R


def phase_moe(k, c, ar, X1, R_X1, hfT, R_hfT):
    nc = c.nc
    Dr = c.D
    m0 = ar.mark()
    hf32 = [ar.al([128, D], F32) for _ in range(2)]
    R_hf32 = [k.res("hf32_%d" % i) for i in range(2)]
    scr = ar.al([128, D], F32)
    R_scr = k.res("moe_scr")
    hT32 = [ar.al([128, 8, 128], F32) for _ in range(2)]
    R_hT32 = [k.res("hT32_%d" % i) for i in range(2)]
    wr32 = ar.al([128, 8, 36], F32)
    R_wr = k.res("wr32")
    brt = ar.al([128, 36], F32)
    gft = ar.al([128, D], F32)
    R_gft = k.res("gft")
    comb = ar.al([128, NT, NE], F32)
    R_comb = k.res("comb")
    sm = ar.al([128, 128], F32)
    R_sm = k.res("moe_sm")
    k.dma("sp", wr32, Dr["w_r"].rearrange("(kt p) n -> p kt n", p=128), writes=[R_wr])
    k.dma("sp", brt, Dr["b_r"].partition_broadcast(128), writes=[R_wr])
    k.dma("sp", gft, Dr["g_ffn"].partition_broadcast(128), writes=[R_gft])
    pT = [c.pwide(0), c.pwide(2)]
    R_pT = [[c.R_ps[0], c.R_ps[1]], [c.R_ps[2], c.R_ps[3]]]
    pL = c.psum[4]
    R_pL = c.R_ps[4]
    for t in range(NT):
        b = t % 2
        xs = X1[:, t, :]
        rstd, R_r = rms_rstd(k, c, xs, R_X1[t], scr, R_scr, D, "moe")
        k.op("dve", lambda e, b=b, xs=xs, rstd=rstd: e.scalar_tensor_tensor(
            out=hf32[b], in0=xs, scalar=rstd, in1=gft, op0=ALU.mult, op1=ALU.mult),
            reads=[R_X1[t], R_r, R_gft], writes=[R_hf32[b]])
        p2 = pT[b]
        k.op("pe", [(lambda e, i=i, b=b, p2=p2: e.transpose(out=p2[:, i * 128:(i + 1) * 128],
                                                            in_=hf32[b][:, i * 128:(i + 1) * 128], identity=c.identf))
                    for i in range(8)], reads=[R_hf32[b], c.R_const], writes=R_pT[b])
        k.op("act", lambda e, b=b, p2=p2: e.activation(out=hT32[b].rearrange("p a b -> p (a b)"), in_=p2, func=AF.Copy),
             reads=R_pT[b], writes=[R_hT32[b]])
        k.op("dve", lambda e, b=b, p2=p2, t=t: e.tensor_copy(
            out=hfT[:, :, t * 128:(t + 1) * 128], in_=p2.rearrange("p (a b) -> p a b", a=8)),
            reads=R_pT[b], writes=[R_hfT[t]])
        lg = pL[:, 0:36]
        k.op("pe", [(lambda e, i=i, b=b: e.matmul(lg, lhsT=hT32[b][:, i, :], rhs=wr32[:, i, :], start=(i == 0), stop=(i == 7)))
                    for i in range(8)], reads=[R_hT32[b], R_wr], writes=[R_pL])
        lgs = sm[:, 0:36]
        gmax, gsum, gp, pen = sm[:, 36:37], sm[:, 37:38], sm[:, 38:39], sm[:, 40:44]
        gex, goh = sm[:, 44:48], sm[:, 48:52]
        elm = sm[:, 52:84]
        m8 = sm[:, 84:92]
        dd, ee, w1, w2 = sm[:, 92:93], sm[:, 93:94], sm[:, 94:95], sm[:, 95:96]
        oh = sm[:, 96:128]
        ct = comb[:, t, :]
        RW = dict(reads=[R_sm], writes=[R_sm])
        k.op("dve", lambda e: e.tensor_tensor(out=lgs, in0=lg, in1=brt, op=ALU.add), reads=[R_pL, R_wr, R_sm], writes=[R_sm])
        k.op("dve", lambda e: e.reduce_max(out=gmax, in_=lgs[:, 0:4], axis=AX.X), **RW)
        k.op("dve", lambda e: e.tensor_scalar(out=gex, in0=lgs[:, 0:4], scalar1=gmax, scalar2=None, op0=ALU.subtract), **RW)
        k.op("act", lambda e: e.activation(out=gex, in_=gex, func=AF.Exp, accum_out=gsum), **RW)
        k.op("dve", lambda e: e.reciprocal(out=gp, in_=gsum), **RW)
        k.op("dve", lambda e: e.tensor_scalar(out=pen, in0=lgs[:, 0:4], scalar1=gmax, scalar2=-1e30, op0=ALU.is_lt, op1=ALU.mult), **RW)
        k.op("dve", lambda e: e.tensor_tensor(out=elm.rearrange("p (g x) -> p g x", g=4),
                                              in0=lgs[:, 4:36].rearrange("p (g x) -> p g x", g=4),
                                              in1=pen.unsqueeze(2).broadcast_to([128, 4, 8]), op=ALU.add), **RW)
        k.op("dve", lambda e: e.max(out=m8, in_=elm), **RW)
        k.op("dve", lambda e: e.tensor_tensor(out=dd, in0=m8[:, 1:2], in1=m8[:, 0:1], op=ALU.subtract), **RW)
        k.op("act", lambda e: e.activation(out=ee, in_=dd, func=AF.Exp), **RW)
        k.op("dve", lambda e: e.tensor_scalar(out=ee, in0=ee, scalar1=1.0, scalar2=None, op0=ALU.add), **RW)
        k.op("dve", lambda e: e.reciprocal(out=w1, in_=ee), **RW)
        k.op("dve", lambda e: e.tensor_scalar(out=w2, in0=w1, scalar1=-1.0, scalar2=1.0, op0=ALU.mult, op1=ALU.add), **RW)
        k.op("dve", lambda e: e.tensor_tensor(out=w1, in0=w1, in1=gp, op=ALU.mult), **RW)
        k.op("dve", lambda e: e.tensor_tensor(out=w2, in0=w2, in1=gp, op=ALU.mult), **RW)
        k.op("dve", lambda e: e.tensor_scalar(out=oh, in0=elm, scalar1=m8[:, 0:1], scalar2=w1, op0=ALU.is_equal, op1=ALU.mult), **RW)
        k.op("dve", lambda e, ct=ct: e.tensor_scalar(out=ct, in0=elm, scalar1=m8[:, 1:2], scalar2=w2, op0=ALU.is_equal, op1=ALU.mult),
             reads=[R_sm], writes=[R_comb])
        k.op("dve", lambda e, ct=ct: e.tensor_tensor(out=ct, in0=ct, in1=oh, op=ALU.add), reads=[R_sm, R_comb], writes=[R_comb])

    if c.n_exp == 0:
        ar.release(m0)
        return
    NWB = 2
    wg = [ar.al([128, 8, DFF], BF16) for _ in range(NWB)]
    wu = [ar.al([128, 8, DFF], BF16) for _ in range(NWB)]
    wd = [ar.al([128, 4, D], BF16) for _ in range(NWB)]
    R_wg = [k.res("wg%d" % i) for i in range(NWB)]
    R_wu = [k.res("wu%d" % i) for i in range(NWB)]
    R_wd = [k.res("wd%d" % i) for i in range(NWB)]
    hid = [ar.al([128, 4, 512], BF16) for _ in range(2)]
    R_hid = [k.res("hid%d" % i) for i in range(2)]
    sg = [ar.al([128, 512], F32) for _ in range(2)]
    R_sg = [k.res("sg%d" % i) for i in range(2)]
    n_exp = c.n_exp

    def load_w(e):
        b = e % NWB
        k.dma("pool", wg[b], Dr["w_gate"][e].rearrange("(kt p) n -> p kt n", p=128), writes=[R_wg[b]])
        k.dma("pool", wu[b], Dr["w_up"][e].rearrange("(kt p) n -> p kt n", p=128), writes=[R_wu[b]])
        k.dma("pool", wd[b], Dr["w_down"][e].rearrange("(kt p) n -> p kt n", p=128), writes=[R_wd[b]])

    units = [(e, g) for e in range(n_exp) for g in range(NT // 4)]
    pgu = [(c.psum[0], c.psum[1]), (c.psum[2], c.psum[3])]
    R_pgu = [(c.R_ps[0], c.R_ps[1]), (c.R_ps[2], c.R_ps[3])]
    pdn = [c.psum[4], c.psum[5], c.psum[6], c.psum[7]]
    R_pdn = [c.R_ps[4], c.R_ps[5], c.R_ps[6], c.R_ps[7]]
    st = dict(gu=0, dn=0)

    def gate_up(u):
        e, g = units[u]
        b = e % NWB
        hb = u % 2
        tok = slice(g * 512, (g + 1) * 512)
        for ff in range(4):
            pb = st["gu"] % 2
            st["gu"] += 1
            pg, pu = pgu[pb]
            k.op("pe", [(lambda en, i=i, pg=pg, b=b, ff=ff: en.matmul(pg, lhsT=wg[b][:, i, ff * 128:(ff + 1) * 128], rhs=hfT[:, i, tok],
                                                                      start=(i == 0), stop=(i == 7))) for i in range(8)],
                 reads=[R_wg[b]] + R_hfT[4 * g:4 * g + 4], writes=[R_pgu[pb][0]])
            k.op("pe", [(lambda en, i=i, pu=pu, b=b, ff=ff: en.matmul(pu, lhsT=wu[b][:, i, ff * 128:(ff + 1) * 128], rhs=hfT[:, i, tok],
                                                                      start=(i == 0), stop=(i == 7))) for i in range(8)],
                 reads=[R_wu[b]] + R_hfT[4 * g:4 * g + 4], writes=[R_pgu[pb][1]])
            k.op("act", lambda en, pg=pg, pb=pb: en.activation(out=sg[pb], in_=pg, func=AF.Silu),
                 reads=[R_pgu[pb][0]], writes=[R_sg[pb]])
            k.op("dve", lambda en, pu=pu, pb=pb, hb=hb, ff=ff: en.tensor_tensor(out=hid[hb][:, ff, :], in0=pu, in1=sg[pb], op=ALU.mult),
                 reads=[R_pgu[pb][1], R_sg[pb]], writes=[R_hid[hb]])

    def down(u):
        e, g = units[u]
        b = e % NWB
        hb = u % 2
        for tt in range(4):
            t = 4 * g + tt
            for hf in range(2):
                pb = st["dn"] % 4
                st["dn"] += 1
                po = pdn[pb]
                k.op("pe", [(lambda en, i=i, po=po, b=b, hb=hb, tt=tt, hf=hf: en.matmul(
                    po, lhsT=hid[hb][:, i, tt * 128:(tt + 1) * 128], rhs=wd[b][:, i, hf * 512:(hf + 1) * 512],
                    start=(i == 0), stop=(i == 3))) for i in range(4)],
                    reads=[R_hid[hb], R_wd[b]], writes=[R_pdn[pb]])
                xs = X1[:, t, hf * 512:(hf + 1) * 512]
                k.op("dve", lambda en, po=po, xs=xs, t=t, e=e: en.scalar_tensor_tensor(
                    out=xs, in0=po, scalar=comb[:, t, e:e + 1], in1=xs, op0=ALU.mult, op1=ALU.add),
                    reads=[R_pdn[pb], R_comb, R_X1[t]], writes=[R_X1[t]])

    load_w(0)
    for u in range(len(units)):
        e, g = units[u]
        gate_up(u)
        if u >= 1:
            down(u - 1)
        if g == 0 and e + 1 < n_exp:
            load_w(e + 1)
    down(len(units) - 1)
    ar.release(m0)


def phase_final(k, c, ar, X1, R_X1):
    Dr = c.D
    m0 = ar.mark()
    gft = ar.al([128, D], F32)
    R_g = k.res("gfin")
    scr = ar.al([128, D], F32)
    R_scr = k.res("fin_scr")
    ob = [ar.al([128, D], F32) for _ in range(2)]
    R_ob = [k.res("ob%d" % i) for i in range(2)]
    k.dma("sp", gft, Dr["g_fin"].partition_broadcast(128), writes=[R_g])
    for t in range(NT):
        b = t % 2
        xs = X1[:, t, :]
        rstd, R_r = rms_rstd(k, c, xs, R_X1[t], scr, R_scr, D, "fin")
        k.op("dve", lambda e, b=b, xs=xs, rstd=rstd: e.scalar_tensor_tensor(
            out=ob[b], in0=xs, scalar=rstd, in1=gft, op0=ALU.mult, op1=ALU.mult),
            reads=[R_X1[t], R_r, R_g], writes=[R_ob[b]])
        k.dma("sp", Dr["y"][t * 128:(t + 1) * 128, :], ob[b], reads=[R_ob[b]])
    for b in range(2):
        for sk, v in list(R_ob[b].rs.items()):
            if sk.startswith("d_"):
                k.rec["sp"].append(("w", sk, v))
    ar.release(m0)


Q0, KV0, GT0, HQ0, HF0, HI0, HG0, MG0 = 0, 512, 1280, 1304, 1816, 2328, 2840, 3352


def _partner(d):
    return d + 8 if d < 8 else (d - 8 if d < 16 else d)


def _rope_tables(pos):
    pos = np.asarray(pos, dtype=np.float32)
    inv = (np.float32(500000.0) ** (-np.arange(8, dtype=np.float32) / np.float32(8))).astype(np.float32)
    ang = (pos[None, :] * inv[:, None]).astype(np.float32)
    cs, sn = np.cos(ang).astype(np.float32), np.sin(ang).astype(np.float32)
    C = np.ones((64, len(pos)), np.float32)
    S = np.zeros((64, len(pos)), np.float32)
    C[0:8], C[8:16] = cs, cs
    S[0:8], S[8:16] = -sn, sn
    return C, S


def attn_input_specs():
    return [
        ("g_attn", (D,), F32), ("g_hg4", (512,), F32),
        ("w1f", (D, 1280), F32), ("w1t", (D, 768), F32),
        ("w2f", (D, 2048), F32), ("w2t", (D, 1536), F32),
        ("wck", (64, 2048), F32), ("wckp", (64, 2048), F32), ("wcv", (64, 2048), F32),
        ("posT", (128, 32), F32), ("lbl", (2, 512), F32),
        ("w_mg", (D, 2048), F32), ("w_brn", (512, D), F32), ("w_brh", (512, D), F32), ("w_out", (D, D), F32),
        ("CK", (128, SEQ), F32), ("SK", (128, SEQ), F32), ("CKc", (128, 512), F32), ("SKc", (128, 512), F32),
        ("CQ", (128, NTOK), F32), ("SQ", (128, NTOK), F32),
        ("ovl", (128, 4, 128), F32),
        ("CB", (128, NT, 128), F32), ("CM", (128, 4, 128), F32), ("WMT", (128, 8, 128), F32),
        ("VAL", (128, NT, 128), F32), ("ADDC", (128, NT, 128), F32),
        ("tri", (128, 128), F32), ("I4", (128, 512), F32), ("onehot", (128, 4), F32),
    ]


_TAB_CACHE = {}


def _const_tables(cp):
    if cp in _TAB_CACHE:
        return _TAB_CACHE[cp]
    m = {}
    C, S = _rope_tables(np.arange(SEQ))
    m["CK"], m["SK"] = np.concatenate([C, C], 0), np.concatenate([S, S], 0)
    C, S = _rope_tables(np.maximum(16 * (np.arange(512) - 1), 0))
    m["CKc"], m["SKc"] = np.concatenate([C, C], 0), np.concatenate([S, S], 0)
    tpos = (128 * (4 * np.arange(NT)[:, None] + cp) + np.arange(128)[None, :])
    C, S = _rope_tables(tpos.reshape(-1))
    m["CQ"] = np.concatenate([C, C], 0) * np.float32(0.125)
    m["SQ"] = np.concatenate([S, S], 0) * np.float32(0.125)
    n = np.arange(512) - 1
    cs, ce = 16 * n, 16 * n + 31
    ss = 64 * np.arange(128)
    ov = ((cs[:, None] < ss[None, :] + 64) & (ce[:, None] >= ss[None, :]) & (n[:, None] >= 0)).astype(np.float32)
    m["ovl"] = np.ascontiguousarray(ov.reshape(4, 128, 128).transpose(1, 0, 2))
    mt = (np.arange(NT) // 4)
    mm = mt[:, None] * 128 + np.arange(128)[None, :]
    nn = mm - 1
    okc = (nn[:, None, :] >= 0) & (16 * nn[:, None, :] + 31 <= tpos[:, :, None])
    m["CB"] = np.ascontiguousarray(np.where(okc, 0.0, NEG).astype(np.float32).transpose(1, 0, 2))
    blk = np.arange(128)
    jq = tpos // 64
    force = (blk[None, None, :] == jq[:, :, None]) | (blk[None, None, :] == 0)
    valid = (64 * blk[None, None, :] <= tpos[:, :, None])
    m["VAL"] = np.ascontiguousarray((valid & ~force).astype(np.float32).transpose(1, 0, 2))
    m["ADDC"] = np.ascontiguousarray(np.where(force, 1e4, np.where(valid, 0.0, -1.0)).astype(np.float32).transpose(1, 0, 2))
    t = np.arange(128)[:, None]
    p = np.arange(128)[None, :]
    caus = np.where(p <= t, 0.0, NEG).astype(np.float32)
    anti = np.where(p > t, 0.0, NEG).astype(np.float32)
    cm = np.zeros((128, 4, 128), np.float32)
    for r in range(4):
        cm[:, r, :] = 0.0 if r < cp else (caus if r == cp else NEG)
    m["CM"] = cm
    wm = np.zeros((128, 8, 128), np.float32)
    for r in range(8):
        dk = cp + 4 - r
        wm[:, r, :] = NEG if (dk < 0 or dk > 4) else (caus if dk == 0 else (anti if dk == 4 else 0.0))
    m["WMT"] = wm
    m["tri"] = (np.arange(128)[:, None] <= np.arange(128)[None, :]).astype(np.float32)
    m["I4"] = np.tile(np.eye(128, dtype=np.float32), (1, 4))
    oh = np.zeros((128, 4), np.float32)
    oh[:, cp] = 1.0
    m["onehot"] = oh
    _TAB_CACHE[cp] = m
    return m


def attn_host_inputs(inp, b, cp):
    m = dict(_const_tables(cp))
    w = inp["w_in"][0]
    pp = np.array([g * 64 + _partner(d) for g in range(2) for d in range(64)])
    kv = lambda s: KV0 + s * 128 + np.arange(128)
    hfc = HF0 + np.arange(512)
    m["w1f"] = np.ascontiguousarray(np.concatenate(
        [w[:, kv(0)], w[:, kv(1)], w[:, kv(2)], w[:, kv(2)[pp]], w[:, kv(4)], w[:, kv(4)[pp]], w[:, hfc]], axis=1))
    m["w1t"] = np.ascontiguousarray(np.concatenate([w[:, kv(3)], w[:, kv(5)], w[:, HI0:HI0 + 512]], axis=1))
    qcols, qpcols = [], []
    for a in range(4):
        for h in (a, 4 + a):
            qcols += [Q0 + h * 64 + d for d in range(64)]
            qpcols += [Q0 + h * 64 + _partner(d) for d in range(64)]
    m["w2f"] = np.ascontiguousarray(np.concatenate(
        [w[:, qcols], w[:, qpcols], w[:, HQ0:HQ0 + 512], w[:, hfc]], axis=1))
    gpad = np.concatenate([w[:, GT0:GT0 + 24], w[:, GT0:GT0 + 24][:, :0].repeat(1, 1)], axis=1)
    w2t = np.zeros((D, 1536), np.float32)
    w2t[:, 0:512] = w[:, HI0:HI0 + 512]
    w2t[:, 512:1024] = w[:, HG0:HG0 + 512]
    w2t[:, 1024:1048] = w[:, GT0:GT0 + 24]
    m["w2t"] = w2t
    pc = np.array([_partner(d) for d in range(64)])
    dle = lambda w_: np.ascontiguousarray(w_.reshape(32, 64, 64).transpose(1, 0, 2).reshape(64, 2048))
    m["wck"] = dle(inp["w_cmp_k"][0])
    m["wckp"] = dle(inp["w_cmp_k"][0][:, pc])
    m["wcv"] = dle(inp["w_cmp_v"][0])
    pT = np.ascontiguousarray(inp["cmp_pos"][0].T)
    m["posT"] = np.concatenate([pT, pT], 0)
    m["lbl"] = np.ascontiguousarray(inp["hg_lb_logits"])
    m["g_attn"] = np.ascontiguousarray(inp["attn_norm"][0])
    m["g_hg4"] = np.ascontiguousarray(np.tile(inp["hg_norm"][0], 4))
    m["w_mg"] = np.ascontiguousarray(w[:, MG0:MG0 + 2048])
    m["w_brn"] = np.ascontiguousarray(inp["w_br_nsa"][0])
    m["w_brh"] = np.ascontiguousarray(inp["w_br_hg"][0])
    m["w_out"] = np.ascontiguousarray(inp["w_out"][0])
    return m


def norm_transpose_group(k, c, W, src_dram, row0, hT, R_hT):
    def s1(tt):
        b = tt % 2
        k.dma("sp", W.xt[b], src_dram[row0 + tt * 128: row0 + (tt + 1) * 128, :], writes=[W.R_xt[b]])
        rstd, R_r = rms_rstd(k, c, W.xt[b], W.R_xt[b], W.scr, W.R_scr, D, "an")
        k.op("dve", lambda e: e.scalar_tensor_tensor(
            out=W.hb[b], in0=W.xt[b], scalar=rstd, in1=W.gA, op0=ALU.mult, op1=ALU.mult),
            reads=[W.R_xt[b], R_r, W.R_gA], writes=[W.R_hb[b]])
        pb = c.psum[b].bitcast(BF16)
        k.op("pe", [(lambda e, i=i: e.transpose(out=pb[:, i * 128:(i + 1) * 128],
                                                in_=W.hb[b][:, i * 128:(i + 1) * 128], identity=c.identb))
                    for i in range(8)], reads=[W.R_hb[b], c.R_const], writes=[c.R_ps[b]])

    def s2(tt):
        b = tt % 2
        pb = c.psum[b].bitcast(BF16)
        k.op("act", lambda e: e.activation(out=hT[:, :, tt * 128:(tt + 1) * 128],
                                           in_=pb.rearrange("p (a b) -> p a b", a=8), func=AF.Copy),
             reads=[c.R_ps[b]], writes=[R_hT])
    s1(0)
    s1(1)
    s2(0)
    s1(2)
    s2(1)
    s1(3)
    s2(2)
    s2(3)


def f_front(k, c, W, fl_ps, R_fl, hd):
    u, a, bq, lk, L, RF = W.sets[hd % 2]
    k.op("act", lambda e: e.activation(out=u, in_=fl_ps, func=AF.Exp, scale=-1.0), reads=[R_fl], writes=[RF])
    k.op("act", lambda e: e.activation(out=a, in_=u, func=AF.Ln, scale=c.lbv[:, hd:hd + 1], bias=c.one_col[:, 0:1]),
         reads=[RF, c.R_const], writes=[RF])
    k.op("act", lambda e: e.activation(out=bq, in_=u, func=AF.Ln, bias=c.one_col[:, 0:1]), reads=[RF, c.R_const], writes=[RF])
    k.op("dve", lambda e: e.scalar_tensor_tensor(out=lk, in0=fl_ps, scalar=-1.0, in1=bq, op0=ALU.mult, op1=ALU.subtract),
         reads=[R_fl, RF], writes=[RF])
    for tt in range(4):
        sl = slice(tt * 128, (tt + 1) * 128)
        k.op("dve", lambda e, sl=sl: e.tensor_tensor_scan(out=L[:, sl], data0=a[:, sl], data1=bq[:, sl], initial=0.0,
                                                          op0=ALU.add, op1=ALU.subtract), reads=[RF], writes=[RF])
    k.op("pool", lambda e: e.tensor_tensor(out=lk, in0=lk, in1=L, op=ALU.subtract), reads=[RF], writes=[RF])


def f_back(k, c, W, hd, H=None):
    u, a, bq, lk, L, RF = W.sets[hd % 2]
    W_, W = W, (H if H is not None else W)
    Lr = L.rearrange("p (t x) -> p t x", t=4)
    rcol, ecol = Lr[:, :, 63], Lr[:, :, 127]
    k.op("dve", lambda e: e.tensor_scalar(out=W.rb[:, hd, :], in0=rcol, scalar1=c.l1mlb[:, hd:hd + 1], scalar2=None, op0=ALU.add),
         reads=[RF, c.R_const], writes=[W.R_cols])
    k.op("dve", lambda e: e.tensor_scalar(out=W.negr[:, hd, :], in0=rcol, scalar1=-1.0, scalar2=None, op0=ALU.mult),
         reads=[RF], writes=[W.R_cols])
    k.op("dve", lambda e: e.tensor_tensor(out=W.dl[:, hd, :], in0=ecol, in1=rcol, op=ALU.subtract), reads=[RF], writes=[W.R_cols])
    k.op("act", lambda e: e.activation(out=W.c1[:, hd, :], in_=ecol, func=AF.Exp), reads=[RF], writes=[W.R_cols])
    k.op("act", lambda e: e.activation(out=W.c2[:, hd, :], in_=W.dl[:, hd, :], func=AF.Exp), reads=[W.R_cols], writes=[W.R_cols])
    k.op("act", lambda e: e.activation(out=W.er[:, hd, :], in_=rcol, func=AF.Exp), reads=[RF], writes=[W.R_cols])
    for tt in range(4):
        sl = slice(tt * 128, (tt + 1) * 128)
        k.op("act", lambda e, sl=sl, tt=tt: e.activation(out=W.kT[:, hd, sl], in_=lk[:, sl], func=AF.Exp, bias=W.rb[:, hd, tt:tt + 1]),
             reads=[RF, W.R_cols], writes=[W.R_kT])


def setup_lb(k, c, ar):
    Dr = c.D
    c.lbv = ar.al([128, 4], F32)
    c.l1mlb = ar.al([128, 4], F32)
    c.one_col = ar.al([128, 1], F32)
    c.ones128 = ar.al([128, 128], F32)
    l0 = ar.al([128, 4], F32)
    l1 = ar.al([128, 4], F32)
    R = c.R_const
    k.dma("sp", l0, Dr["lbl"][0].rearrange("(h p) -> p h", p=128), writes=[R], allow_slow_non_contiguous=True)
    k.dma("sp", l1, Dr["lbl"][1].rearrange("(h p) -> p h", p=128), writes=[R], allow_slow_non_contiguous=True)
    k.op("dve", lambda e: e.memset(c.one_col, 1.0), writes=[R])
    k.op("dve", lambda e: e.memset(c.ones128, 1.0), writes=[R])
    k.op("dve", lambda e: e.tensor_tensor(out=l1, in0=l1, in1=l0, op=ALU.subtract), reads=[R], writes=[R])
    k.op("act", lambda e: e.activation(out=l0, in_=l1, func=AF.Exp), reads=[R], writes=[R])
    k.op("dve", lambda e: e.tensor_scalar(out=l0, in0=l0, scalar1=1.0, scalar2=None, op0=ALU.add), reads=[R], writes=[R])
    k.op("dve", lambda e: e.reciprocal(out=c.lbv, in_=l0), reads=[R], writes=[R])
    k.op("act", lambda e: e.activation(out=l0, in_=l0, func=AF.Ln), reads=[R], writes=[R])
    k.op("dve", lambda e: e.tensor_tensor(out=c.l1mlb, in0=l1, in1=l0, op=ALU.subtract), reads=[R], writes=[R])


class WS:
    pass


def alloc_hg_ws(k, ar, W, nsets=1):
    W.sets = []
    for si in range(nsets):
        blk = ar.al([128, 5, 512], F32)
        W.sets.append(tuple(blk[:, i, :] for i in range(5)) + (k.res("fchain%d" % si),))
        if si == 0:
            W.ab = blk[:, 1:3, :].rearrange("p a b -> p (a b)")
    if nsets == 1:
        W.sets.append(W.sets[0])
    W.u, W.a, W.bq, W.lk, W.L, W.R_f = W.sets[0]
    alloc_hslot(k, ar, W, "0")


def alloc_hslot(k, ar, H, tag):
    H.rb, H.negr, H.dl, H.c1, H.c2, H.er = [ar.al([128, 4, 4], F32) for _ in range(6)]
    H.R_cols = k.res("fcols" + tag)
    H.kT = ar.al([128, 4, 512], BF16)
    H.R_kT = k.res("kT" + tag)


def alloc_x_ws(k, c, ar, W, region, scr=None, R_scr=None):
    if region is not None:
        W.xt = [region[:, 0, :].bitcast(F32), region[:, 1, :].bitcast(F32)]
        W.hb = [region[:, 2, 0:1024], region[:, 2, 1024:2048]]
        W.scr = region[:, 3, :].bitcast(F32)
        W.R_scr = k.res("xscr")
    else:
        W.xt = [ar.al([128, D], F32) for _ in range(2)]
        W.hb = [ar.al([128, D], BF16) for _ in range(2)]
        W.scr, W.R_scr = scr, R_scr
    W.R_xt = [k.res("xt0"), k.res("xt1")]
    W.R_hb = [k.res("hb0"), k.res("hb1")]
    W.gA = ar.al([128, D], F32)
    W.R_gA = k.res("gA")
    k.dma("sp", W.gA, c.D["g_attn"].partition_broadcast(128), writes=[W.R_gA])


def phase_p1(k, c, ar, St):
    Dr = c.D
    m0 = ar.mark()
    W = WS()
    alloc_x_ws(k, c, ar, W, c.oT_hg)
    w1f, w1t = c.R32[:, :, 0:1280], c.R32[:, :, 1280:2048]
    R_w1 = k.res("w1")
    k.dma("pool", w1f, Dr["w1f"].rearrange("(kt p) n -> p kt n", p=128), writes=[R_w1])
    k.dma("pool", w1t, Dr["w1t"].rearrange("(kt p) n -> p kt n", p=128), writes=[R_w1])
    hT = ar.al([128, 8, 512], BF16)
    R_hT = k.res("hT")
    CKg, SKg = ar.al([128, 512], F32), ar.al([128, 512], F32)
    R_rt = k.res("ropetab")
    alloc_hg_ws(k, ar, W, nsets=2)
    t1, t2, R_t12 = W.u, W.a, W.R_f
    vtok = ar.al([128, 4, 512], BF16)
    R_vtok = k.res("vtok")
    ktok = ar.al([128, 4, 128], BF16)
    R_ktok = k.res("ktok")
    Sst = ar.al([128, 4, 128], F32)
    snapacc = ar.al([128, 4, 128], F32)
    R_S, R_snapacc = k.res("S"), k.res("snapacc")
    WC = [ar.al([128, 32, 64], BF16) for _ in range(3)]
    R_WC = k.res("WC")
    posT = ar.al([128, 32], BF16)
    cb = ar.al([128, 4], F32)
    xin = [[ar.al([128, 528], BF16) for _ in range(2)] for _ in range(2)]
    R_xin = [[k.res("xin%d%d" % (a, b)) for b in range(2)] for a in range(2)]
    CKc, SKc = ar.al([128, 32], F32), ar.al([128, 32], F32)
    R_ckc = k.res("ckc")
    VCf = ar.al([128, 512], F32)
    R_VCf = k.res("VCf")
    ctmp = ar.al([128, 4, 32], F32)
    R_ctmp = k.res("ctmp")
    for xi, nm in enumerate(("wck", "wckp", "wcv")):
        for g in range(2):
            k.dma("pool", WC[xi][64 * g:64 * g + 64].rearrange("p l e -> p (l e)"), Dr[nm], writes=[R_WC])
    k.dma("pool", posT, Dr["posT"], writes=[R_WC])
    k.op("dve", lambda e: e.memset(Sst, 0.0), writes=[R_S])
    k.op("dve", lambda e: e.memset(St.VsA[:, :, :, 64:65], 1.0), writes=[St.R_VsA])
    k.op("dve", lambda e: e.memset(St.VwA[:, :, :, 64:65], 1.0), writes=[St.R_VwA])
    for a in range(2):
        k.op("dve", lambda e, a=a: e.memset(xin[a][0][:, 0:16], 0.0), writes=[R_xin[a][0]])
    p6 = c.psum[6]
    fns = []
    for xi in range(3):
        for g in range(2):
            for l in range(32):
                fns.append(lambda e, xi=xi, g=g, l=l: e.matmul(p6[64 * g:64 * g + 64, xi:xi + 1], lhsT=WC[xi][64 * g:64 * g + 64, l, :],
                                                               rhs=posT[64 * g:64 * g + 64, l:l + 1], start=(l == 0), stop=(l == 31)))
    k.op("pe", fns, reads=[R_WC], writes=[c.R_ps[6]])
    k.op("dve", lambda e: e.tensor_copy(out=cb[:, 0:3], in_=p6[:, 0:3]), reads=[c.R_ps[6]], writes=[R_WC])

    NG = c.n_groups
    Hs = [W, W]
    vtoks, R_vtoks = [vtok, vtok], [R_vtok, R_vtok]
    p6b = c.psum[6].bitcast(BF16)

    def fm(ft, bank):
        k.op("pe", [(lambda e, i=i: e.matmul(c.psum[bank], lhsT=w1f[:, i, ft * 128:(ft + 1) * 128], rhs=hT[:, i, :],
                                             start=(i == 0), stop=(i == 7))) for i in range(8)],
             reads=[R_w1, R_hT], writes=[c.R_ps[bank]])

    def A_x(G):
        norm_transpose_group(k, c, W, Dr["xb"], G * 512, hT, R_hT)
        k.dma("sp", CKg, Dr["CK"][:, G * 512:(G + 1) * 512], writes=[R_rt])
        k.dma("sp", SKg, Dr["SK"][:, G * 512:(G + 1) * 512], writes=[R_rt])

    def A_kv(G):
        xb_ = G % 2
        for a in range(2):
            fm(a, 2 + a)
            k.op("act", lambda e, a=a: e.activation(out=xin[a][xb_][:, 16:528], in_=c.psum[2 + a], func=AF.Copy),
                 reads=[c.R_ps[2 + a]], writes=[R_xin[a][xb_]])
            k.op("pool", lambda e, a=a: e.tensor_copy(out=xin[a][1 - xb_][:, 0:16], in_=xin[a][xb_][:, 512:528]),
                 reads=[R_xin[a][xb_]], writes=[R_xin[a][1 - xb_]])
        for which, dst, R_dst in ((0, St.KTs, St.R_KTs), (1, St.KTw, St.R_KTw)):
            fm(2 + 2 * which, 2)
            fm(3 + 2 * which, 3)
            k.op("dve", lambda e: e.tensor_tensor(out=t1, in0=c.psum[2], in1=CKg, op=ALU.mult), reads=[c.R_ps[2], R_rt], writes=[R_t12])
            k.op("dve", lambda e: e.tensor_tensor(out=t2, in0=c.psum[3], in1=SKg, op=ALU.mult), reads=[c.R_ps[3], R_rt, R_t12], writes=[R_t12])
            k.op("pool", lambda e, dst=dst: e.tensor_tensor(out=dst[:, G * 512:(G + 1) * 512], in0=t1, in1=t2, op=ALU.add),
                 reads=[R_t12], writes=[R_dst])

    def A_tok(G):
        vt, R_vt = vtoks[G % 2], R_vtoks[G % 2]
        for tt in range(4):
            tile_ = 4 * G + tt
            k.op("pe", [(lambda e, i=i, tt=tt: e.matmul(c.psum[4][:, 0:256], lhsT=hT[:, i, tt * 128:(tt + 1) * 128], rhs=w1t[:, i, 0:256],
                                                        start=(i == 0), stop=(i == 7))) for i in range(8)],
                 reads=[R_w1, R_hT], writes=[c.R_ps[4]])
            k.op("pe", [(lambda e, i=i, tt=tt: e.matmul(c.psum[5], lhsT=hT[:, i, tt * 128:(tt + 1) * 128], rhs=w1t[:, i, 256:768],
                                                        start=(i == 0), stop=(i == 7))) for i in range(8)],
                 reads=[R_w1, R_hT], writes=[c.R_ps[5]])
            k.op("act", lambda e, tile_=tile_: e.activation(out=St.VsA[:, tile_, :, 0:64],
                                                            in_=c.psum[4][:, 0:128].rearrange("p (g d) -> p g d", g=2), func=AF.Copy),
                 reads=[c.R_ps[4]], writes=[St.R_VsA])
            k.op("act", lambda e, tile_=tile_: e.activation(out=St.VwA[:, tile_, :, 0:64],
                                                            in_=c.psum[4][:, 128:256].rearrange("p (g d) -> p g d", g=2), func=AF.Copy),
                 reads=[c.R_ps[4]], writes=[St.R_VwA])
            k.op("dve", lambda e, tt=tt: e.tensor_copy(out=vt[:, tt, :], in_=c.psum[5]), reads=[c.R_ps[5]], writes=[R_vt])

    def A_conv(G):
        xb_ = G % 2
        fns = []
        for xi in range(3):
            src = xin[0][xb_] if xi < 2 else xin[1][xb_]
            for l in range(32):
                for g in range(2):
                    fns.append(lambda e, xi=xi, g=g, l=l, src=src: e.matmul(
                        p6[64 * g:64 * g + 64, 32 * xi:32 * xi + 32], lhsT=WC[xi][64 * g:64 * g + 64, l, :],
                        rhs=src[64 * g:64 * g + 64, l:l + 497:16], start=(l == 0), stop=(l == 31)))
        k.op("pe", fns, reads=[R_WC, R_xin[0][xb_], R_xin[1][xb_]], writes=[c.R_ps[6]])
        ms = slice(32 * G, 32 * G + 32)
        k.dma("sp", CKc, Dr["CKc"][:, ms], writes=[R_ckc])
        k.dma("sp", SKc, Dr["SKc"][:, ms], writes=[R_ckc])
        k.op("dve", lambda e: e.tensor_scalar(out=ctmp[:, 0, :], in0=p6[:, 0:32], scalar1=cb[:, 0:1], scalar2=None, op0=ALU.add),
             reads=[c.R_ps[6], R_WC], writes=[R_ctmp])
        k.op("dve", lambda e: e.tensor_scalar(out=ctmp[:, 1, :], in0=p6[:, 32:64], scalar1=cb[:, 1:2], scalar2=None, op0=ALU.add),
             reads=[c.R_ps[6], R_WC], writes=[R_ctmp])
        k.op("dve", lambda e: e.tensor_scalar(out=VCf[:, ms], in0=p6[:, 64:96], scalar1=cb[:, 2:3], scalar2=None, op0=ALU.add),
             reads=[c.R_ps[6], R_WC], writes=[R_VCf])
        k.op("pool", lambda e: e.tensor_tensor(out=ctmp[:, 0, :], in0=ctmp[:, 0, :], in1=CKc, op=ALU.mult),
             reads=[R_ctmp, R_ckc], writes=[R_ctmp])
        k.op("pool", lambda e: e.tensor_tensor(out=ctmp[:, 1, :], in0=ctmp[:, 1, :], in1=SKc, op=ALU.mult),
             reads=[R_ctmp, R_ckc], writes=[R_ctmp])
        k.op("pool", lambda e: e.tensor_tensor(out=St.KC[:, ms], in0=ctmp[:, 0, :], in1=ctmp[:, 1, :], op=ALU.add),
             reads=[R_ctmp], writes=[St.R_KC])

    def front(hd):
        bank = 2 + hd % 2
        fm(6 + hd, bank)
        f_front(k, c, W, c.psum[bank], c.R_ps[bank], hd)

    def A_f(G):
        H = Hs[G % 2]
        front(0)
        front(1)
        A_tok(G)
        f_back(k, c, W, 0, H)
        front(2)
        A_conv(G)
        f_back(k, c, W, 1, H)
        front(3)
        f_back(k, c, W, 2, H)
        f_back(k, c, W, 3, H)

    def B_step(G, tt):
        H = Hs[G % 2]
        vt, R_vt = vtoks[G % 2], R_vtoks[G % 2]
        sl = slice(tt * 128, (tt + 1) * 128)
        k.op("pe", [(lambda e, hd=hd: e.transpose(out=p6b[:, hd * 128:(hd + 1) * 128], in_=H.kT[:, hd, sl], identity=c.identb))
                    for hd in range(4)], reads=[H.R_kT, c.R_const], writes=[c.R_ps[6]])
        k.op("act", lambda e: e.activation(out=ktok, in_=p6b[:, 0:512].rearrange("p (h x) -> p h x", h=4), func=AF.Copy),
             reads=[c.R_ps[6]], writes=[R_ktok])
        k.op("pe", [(lambda e, hd=hd: e.matmul(c.psum[7][:, hd * 128:(hd + 1) * 128], lhsT=ktok[:, hd, :],
                                               rhs=vt[:, tt, hd * 128:(hd + 1) * 128], start=True, stop=True))
                    for hd in range(4)], reads=[R_ktok, R_vt], writes=[c.R_ps[7]])
        Sf, Af = Sst.rearrange("p h x -> p (h x)"), snapacc.rearrange("p h x -> p (h x)")
        if tt == 0:
            k.op("dve", lambda e: e.tensor_scalar(out=Af, in0=Sf, scalar1=c.onehot[:, 0:1], scalar2=None, op0=ALU.mult),
                 reads=[R_S, c.R_const], writes=[R_snapacc])
        else:
            k.op("dve", lambda e: e.scalar_tensor_tensor(out=Af, in0=Sf, scalar=c.onehot[:, tt:tt + 1], in1=Af,
                                                         op0=ALU.mult, op1=ALU.add),
                 reads=[R_S, c.R_const, R_snapacc], writes=[R_snapacc])
        for hd in range(4):
            k.op("dve", lambda e, hd=hd: e.tensor_scalar(out=Sst[:, hd, :], in0=Sst[:, hd, :], scalar1=H.c1[:, hd, tt:tt + 1],
                                                         scalar2=None, op0=ALU.mult),
                 reads=[R_S, H.R_cols], writes=[R_S])
            k.op("dve", lambda e, hd=hd: e.scalar_tensor_tensor(
                out=Sst[:, hd, :], in0=c.psum[7][:, hd * 128:(hd + 1) * 128], scalar=H.c2[:, hd, tt:tt + 1], in1=Sst[:, hd, :],
                op0=ALU.mult, op1=ALU.add), reads=[c.R_ps[7], R_S, H.R_cols], writes=[R_S])
        if tt == 3:
            k.op("act", lambda e: e.activation(out=St.SNAP[:, G, :, :], in_=snapacc, func=AF.Copy), reads=[R_snapacc], writes=[St.R_SNAP])

    for G in range(NG + 1):
        if G < NG:
            A_x(G)
        if G >= 1:
            B_step(G - 1, 0)
            B_step(G - 1, 1)
        if G < NG:
            A_kv(G)
        if G >= 1:
            B_step(G - 1, 2)
            B_step(G - 1, 3)
        if G < NG:
            A_f(G)
    k.op("dve", lambda e: e.memset(St.VCA[:, :, :, 64:65], 1.0), writes=[St.R_VCA])
    for g in range(2):
        k.dma("pool", St.VCA[:, :, g, 65:193], Dr["ovl"], writes=[St.R_VCA])
    pw = c.psum[6]
    k.op("pe", [(lambda e, mt=mt: e.transpose(out=pw[:, mt * 128:(mt + 1) * 128], in_=VCf[:, mt * 128:(mt + 1) * 128], identity=c.identf))
                for mt in range(4)], reads=[R_VCf, c.R_const], writes=[c.R_ps[6]])
    for mt in range(4):
        k.op("act", lambda e, mt=mt: e.activation(out=St.VCA[:, mt, :, 0:64],
                                                  in_=pw[:, mt * 128:(mt + 1) * 128].rearrange("p (g d) -> p g d", g=2), func=AF.Copy),
             reads=[c.R_ps[6]], writes=[St.R_VCA])
    k.op("dve", lambda e: e.memset(St.VCA[0:1, 0, :, :], 0.0), writes=[St.R_VCA])
    ar.release(m0)


def phase_p2pre(k, c, ar, St):
    Dr = c.D
    m0 = ar.mark()
    W = WS()
    alloc_hg_ws(k, ar, W, nsets=2)
    alloc_x_ws(k, c, ar, W, None, scr=W.ab, R_scr=W.R_f)
    hT = ar.al([128, 8, 512], BF16)
    R_hT = k.res("hT2")
    wch = [c.R32f[:, 8192 + b * 4096: 8192 + (b + 1) * 4096].rearrange("p (a b) -> p a b", a=8) for b in range(2)]
    R_wch = [k.res("wch%d" % i) for i in range(2)]
    wgt = ar.al([128, 8, 32], BF16)
    R_wgt = k.res("wgt")
    wst = dict(n=0)
    CQg, SQg = ar.al([128, 512], F32), ar.al([128, 512], F32)
    R_rt = k.res("ropetabq")
    t1, t2, R_t12 = W.u, W.a, W.R_f
    qT = ar.al([128, 4, 512], BF16)
    R_qT = k.res("qTh")
    e1, R_e1 = W.u, W.R_f
    vtoks = [ar.al([128, 512], BF16) for _ in range(2)]
    R_vtoks = [k.res("vtok2_%d" % i) for i in range(2)]
    sgts = [ar.al([128, 512], F32) for _ in range(2)]
    R_sgts = [k.res("sgt%d" % i) for i in range(2)]
    AT = ar.al([128, 4, 128], BF16)
    R_AT = k.res("AT")
    Sp = ar.al([128, 4, 128], BF16)
    R_Sp = k.res("Sp")
    gnt = ar.al([128, 512], F32)
    R_gnt = k.res("gnt")
    o1, o2, R_o = W.bq, W.a, W.R_f
    yb = ar.al([128, 512], BF16)
    R_yb = k.res("yb")
    hs = ar.al([128, 16], F32)
    R_hs = k.res("hs")
    k.dma("pool", wgt, Dr["w2t"][:, 1024:1056].rearrange("(kt p) n -> p kt n", p=128), writes=[R_wgt])
    k.dma("sp", gnt, Dr["g_hg4"].partition_broadcast(128), writes=[R_gnt])

    def wload(src, c0, n=512):
        b = wst["n"] % 2
        wst["n"] += 1
        k.dma("pool", wch[b][:, :, 0:n], src[:, c0:c0 + n].rearrange("(kt p) n -> p kt n", p=128), writes=[R_wch[b]])
        return wch[b], R_wch[b]

    for go in range(NT // 4):
        tok = slice(go * 512, (go + 1) * 512)
        norm_transpose_group(k, c, W, Dr["xo"], go * 512, hT, R_hT)
        k.dma("sp", CQg, Dr["CQ"][:, tok], writes=[R_rt])
        k.dma("sp", SQg, Dr["SQ"][:, tok], writes=[R_rt])

        def fm(wt, R_wt, j, bank):
            k.op("pe", [(lambda e, i=i: e.matmul(c.psum[bank], lhsT=wt[:, i, j * 128:(j + 1) * 128], rhs=hT[:, i, :],
                                                 start=(i == 0), stop=(i == 7))) for i in range(8)],
                 reads=[R_wt, R_hT], writes=[c.R_ps[bank]])
        wq, R_wq = wload(Dr["w2f"], 0)
        wqp, R_wqp = wload(Dr["w2f"], 512)
        for a in range(4):
            fm(wq, R_wq, a, 2)
            fm(wqp, R_wqp, a, 3)
            k.op("dve", lambda e: e.tensor_tensor(out=t1, in0=c.psum[2], in1=CQg, op=ALU.mult), reads=[c.R_ps[2], R_rt], writes=[R_t12])
            k.op("dve", lambda e: e.tensor_tensor(out=t2, in0=c.psum[3], in1=SQg, op=ALU.mult), reads=[c.R_ps[3], R_rt, R_t12], writes=[R_t12])
            k.op("pool", lambda e, a=a: e.tensor_tensor(out=c.QT[:, 4 * go:4 * go + 4, a, :], in0=t1.rearrange("p (i t) -> p i t", i=4),
                                                        in1=t2.rearrange("p (i t) -> p i t", i=4), op=ALU.add), reads=[R_t12], writes=[c.R_QT])
        whq, R_whq = wload(Dr["w2f"], 1024)
        whf, R_whf = wload(Dr["w2f"], 1536)
        def front(hd):
            bank = 2 + hd % 2
            fm(whf, R_whf, hd, bank)
            f_front(k, c, W, c.psum[bank], c.R_ps[bank], hd)

        def back(hd):
            f_back(k, c, W, hd)
            su, sa, sbq, slk, sL, sRF = W.sets[hd % 2]
            fm(whq, R_whq, hd, 6)
            for tt in range(4):
                sl = slice(tt * 128, (tt + 1) * 128)
                k.op("act", lambda e, sl=sl, tt=tt: e.activation(out=su[:, sl], in_=sL[:, sl], func=AF.Exp, bias=W.negr[:, hd, tt:tt + 1]),
                     reads=[sRF, W.R_cols], writes=[sRF])
            k.op("dve", lambda e: e.tensor_tensor(out=qT[:, hd, :], in0=c.psum[6], in1=su, op=ALU.mult),
                 reads=[c.R_ps[6], sRF], writes=[R_qT])
        front(0)
        front(1)
        back(0)
        front(2)
        back(1)
        front(3)
        back(2)
        back(3)
        whi, R_whi = wload(Dr["w2t"], 0)
        whg, R_whg = wload(Dr["w2t"], 512)

        def s1(tt):
            i_own = 4 * go + tt
            pb = tt % 2
            sl = slice(tt * 128, (tt + 1) * 128)
            for (wt, R_wt, n, bank) in ((whi, R_whi, 512, pb), (whg, R_whg, 512, 2 + pb), (wgt, R_wgt, 32, 6)):
                k.op("pe", [(lambda e, i=i, wt=wt, n=n, bank=bank: e.matmul(c.psum[bank][:, 0:n], lhsT=hT[:, i, sl], rhs=wt[:, i, 0:n],
                                                                            start=(i == 0), stop=(i == 7))) for i in range(8)],
                     reads=[R_wt, R_hT], writes=[c.R_ps[bank]])
            k.op("dve", lambda e: e.tensor_copy(out=vtoks[pb], in_=c.psum[pb]), reads=[c.R_ps[pb]], writes=[R_vtoks[pb]])
            k.op("act", lambda e: e.activation(out=sgts[pb], in_=c.psum[2 + pb], func=AF.Silu), reads=[c.R_ps[2 + pb]], writes=[R_sgts[pb]])
            k.op("act", lambda e: e.activation(out=c.gsig[:, i_own, :], in_=c.psum[6][:, 0:24], func=AF.Sigmoid),
                 reads=[c.R_ps[6]], writes=[c.R_gsig])

        def s2(tt):
            i_own = 4 * go + tt
            pb = tt % 2
            vtok, R_vtok, sgt, R_sgt = vtoks[pb], R_vtoks[pb], sgts[pb], R_sgts[pb]
            sl = slice(tt * 128, (tt + 1) * 128)
            k.op("pe", [(lambda e, hd=hd: e.matmul(c.psum[7][:, hd * 128:(hd + 1) * 128], lhsT=W.kT[:, hd, sl], rhs=qT[:, hd, sl],
                                                   start=True, stop=True)) for hd in range(4)],
                 reads=[W.R_kT, R_qT], writes=[c.R_ps[7]])
            k.op("dve", lambda e: e.tensor_scalar(out=W.lk, in0=c.psum[7], scalar1=1e30, scalar2=-1e30, op0=ALU.min, op1=ALU.max),
                 reads=[c.R_ps[7], W.R_f], writes=[W.R_f])
            k.op("dve", lambda e: e.tensor_tensor(out=AT, in0=W.lk.rearrange("p (h x) -> p h x", h=4),
                                                  in1=c.tri.unsqueeze(1).broadcast_to([128, 4, 128]), op=ALU.mult),
                 reads=[W.R_f, c.R_constP], writes=[R_AT])
            for hd in range(4):
                k.op("act", lambda e, hd=hd: e.activation(out=Sp[:, hd, :], in_=St.SNAP[:, i_own, hd, :], func=AF.Copy,
                                                          scale=W.er[:, hd, tt:tt + 1]),
                     reads=[St.R_SNAP, W.R_cols], writes=[R_Sp])
            fns = []
            for hd in range(4):
                fns.append(lambda e, hd=hd: e.matmul(c.psum[4][:, hd * 128:(hd + 1) * 128], lhsT=AT[:, hd, :],
                                                     rhs=vtok[:, hd * 128:(hd + 1) * 128], start=True, stop=False))
                fns.append(lambda e, hd=hd: e.matmul(c.psum[4][:, hd * 128:(hd + 1) * 128], lhsT=qT[:, hd, sl],
                                                     rhs=Sp[:, hd, :], start=False, stop=True))
            k.op("pe", fns, reads=[R_AT, R_vtok, R_qT, R_Sp], writes=[c.R_ps[4]])
            for hd in range(4):
                k.op("act", lambda e, hd=hd: e.activation(out=o2[:, hd * 128:(hd + 1) * 128], in_=c.psum[4][:, hd * 128:(hd + 1) * 128],
                                                          func=AF.Square, accum_out=hs[:, hd:hd + 1]),
                     reads=[c.R_ps[4]], writes=[R_o, R_hs])
            k.op("act", lambda e: e.activation(out=hs[:, 4:8], in_=hs[:, 0:4], func=AF.Sqrt, scale=1.0 / 128, bias=c.epsb[:, 0:1]),
                 reads=[R_hs, c.R_const], writes=[R_hs])
            k.op("dve", lambda e: e.reciprocal(out=hs[:, 8:12], in_=hs[:, 4:8]), reads=[R_hs], writes=[R_hs])
            k.op("dve", lambda e: e.tensor_tensor(out=o1, in0=c.psum[4], in1=gnt, op=ALU.mult), reads=[c.R_ps[4], R_gnt, R_o], writes=[R_o])
            k.op("pool", lambda e: e.tensor_tensor(out=o1, in0=o1, in1=sgt, op=ALU.mult), reads=[R_o, R_sgt], writes=[R_o])
            k.op("dve", lambda e: e.tensor_tensor(out=yb.rearrange("p (h x) -> p h x", h=4), in0=o1.rearrange("p (h x) -> p h x", h=4),
                                                  in1=hs[:, 8:12].unsqueeze(2).broadcast_to([128, 4, 128]), op=ALU.mult),
                 reads=[R_o, R_hs], writes=[R_yb])
            p5b = c.psum[5].bitcast(BF16)
            k.op("pe", [(lambda e, hd=hd: e.transpose(out=p5b[:, hd * 128:(hd + 1) * 128], in_=yb[:, hd * 128:(hd + 1) * 128], identity=c.identb))
                        for hd in range(4)], reads=[R_yb, c.R_const], writes=[c.R_ps[5]])
            k.op("act", lambda e: e.activation(out=c.oT_hg[:, :, i_own * 128:(i_own + 1) * 128],
                                               in_=p5b[:, 0:512].rearrange("p (h x) -> p h x", h=4), func=AF.Copy),
                 reads=[c.R_ps[5]], writes=[c.R_oThg])
        s1(0)
        s1(1)
        s2(0)
        s1(2)
        s2(1)
        s1(3)
        s2(2)
        s2(3)
    ar.release(m0)


def phase_nsa(k, c, ar, St):
    Dr = c.D
    m0 = ar.mark()
    TINY = 1e-30
    CBi = [ar.al([128, 128], BF16) for _ in range(2)]
    VALi = [ar.al([128, 128], F32) for _ in range(2)]
    ADDCi = [ar.al([128, 128], F32) for _ in range(2)]
    R_tab = [k.res("nsatab%d" % i) for i in range(2)]
    R_tabP = [k.res("nsatabP%d" % i) for i in range(2)]
    WMT = ar.al([128, 8, 128], BF16)
    CM = ar.al([128, 4, 128], BF16)
    R_cst = k.res("nsacst")
    k.dma("pool", WMT, Dr["WMT"], writes=[R_cst])
    k.dma("pool", CM, Dr["CM"], writes=[R_cst])
    PT = [ar.al([128, 512], BF16) for _ in range(3)]
    R_PT = [k.res("PT%d" % i) for i in range(3)]
    Uc = ar.al([128, 4, 193], F32)
    R_Uc = k.res("Uc")
    Os = ar.al([128, 4, 65], F32)
    Ow = ar.al([128, 4, 65], F32)
    R_Os, R_Ow = k.res("Os"), k.res("Ow")
    score, sc2, imp = ar.al([128, 128], F32), ar.al([128, 128], F32), ar.al([128, 128], F32)
    R_sel = k.res("sel")
    selb = ar.al([128, 128], BF16)
    R_selb = k.res("selb")
    bd = ar.al([128, 4, 128], BF16)
    R_bd = k.res("bd")
    selX = ar.al([128, 128, 64], BF16)
    R_selX = [k.res("selX0"), k.res("selX1")]
    cols = ar.al([128, 64], F32)
    R_cols = k.res("nsacols")
    acc, tmp = ar.al([128, 4, 64], F32), ar.al([128, 4, 64], F32)
    R_acc = k.res("nsaacc")
    onsa = ar.al([128, 2, 4, 64], BF16)
    R_onsa = k.res("onsa")
    st = dict(s=0, p=0)
    pO_s, pO_w = c.psum[3][:, 0:260], c.psum[4][:, 0:260]
    pU = [c.psum[5], c.psum[6]]

    pend = []

    def flush_pv(keep=0):
        while len(pend) > keep:
            pend.pop(0)()

    def unit(KT, R_KT, kt_slice, QTg, g, bias, Vaug, R_V, outs, R_outs):
        sb = st["s"] % 3
        st["s"] += 1
        pb = st["p"] % 3
        st["p"] += 1
        S = c.psum[sb]
        fns = [lambda e: e.matmul(S, lhsT=KT[64 * g:64 * g + 64, kt_slice], rhs=QTg, start=True, stop=(bias is None))]
        rd = [R_KT, c.R_QT]
        if bias is not None:
            bl, R_bl = bias
            fns.append(lambda e: e.matmul(S, lhsT=bl, rhs=c.I4, start=False, stop=True))
            rd += [R_bl, c.R_constP]
        k.op("pe", fns, reads=rd, writes=[c.R_ps[sb]])
        k.op("act", lambda e: e.activation(out=PT[pb], in_=S, func=AF.Exp), reads=[c.R_ps[sb]], writes=[R_PT[pb]])

        def pv():
            k.op("pe", [(lambda e, a=a: e.matmul(outs[a], lhsT=PT[pb][:, a * 128:(a + 1) * 128], rhs=Vaug, start=False, stop=False,
                                                 skip_group_check=True)) for a in range(4)],
                 reads=[R_PT[pb], R_V], writes=R_outs)
        pend.append(pv)
        flush_pv(keep=2)

    for i in range(c.n_blocks):
        tb = i % 2
        k.dma("pool", CBi[tb], Dr["CB"][:, i, :], writes=[R_tabP[tb]])
        k.dma("sp", VALi[tb], Dr["VAL"][:, i, :], writes=[R_tab[tb]])
        k.dma("sp", ADDCi[tb], Dr["ADDC"][:, i, :], writes=[R_tab[tb]])
        for g in range(2):
            QTg = c.QT[64 * g:64 * g + 64, i, :, :].rearrange("p a t -> p (a t)")
            nmt = i // 4 + 1
            k.op("dve", lambda e: e.memset(pU[0], 0.0), writes=[c.R_ps[5]])
            k.op("dve", lambda e: e.memset(pU[1], 0.0), writes=[c.R_ps[6]])
            outsU = [pU[a // 2][:, (a % 2) * 193:(a % 2) * 193 + 193] for a in range(4)]
            for mt in range(nmt):
                bias = (CBi[tb], R_tabP[tb]) if mt == nmt - 1 else None
                unit(St.KC, St.R_KC, slice(mt * 128, (mt + 1) * 128), QTg, g, bias, St.VCA[:, mt, g, :], St.R_VCA, outsU, [c.R_ps[5], c.R_ps[6]])
            flush_pv()
            k.op("act", lambda e: e.activation(out=Uc[:, 0:2, :], in_=pU[0][:, 0:386].rearrange("p (a x) -> p a x", a=2), func=AF.Copy),
                 reads=[c.R_ps[5]], writes=[R_Uc])
            k.op("act", lambda e: e.activation(out=Uc[:, 2:4, :], in_=pU[1][:, 0:386].rearrange("p (a x) -> p a x", a=2), func=AF.Copy),
                 reads=[c.R_ps[6]], writes=[R_Uc])
            k.op("dve", lambda e: e.memset(c.psum[4], 0.0), writes=[c.R_ps[4]])
            outsW = [pO_w[:, a * 65:(a + 1) * 65] for a in range(4)]
            for r in range(8):
                kt = 4 * i - 4 + r
                if kt < 0:
                    continue
                unit(St.KTw, St.R_KTw, slice(kt * 128, (kt + 1) * 128), QTg, g, (WMT[:, r, :], R_cst), St.VwA[:, kt, g, :], St.R_VwA, outsW, [c.R_ps[4]])
            zc, rzc = cols[:, 0:4], cols[:, 4:8]
            k.op("dve", lambda e: e.tensor_scalar(out=zc, in0=Uc[:, :, 64], scalar1=TINY, scalar2=None, op0=ALU.max), reads=[R_Uc], writes=[R_cols])
            k.op("dve", lambda e: e.reciprocal(out=rzc, in_=zc), reads=[R_cols], writes=[R_cols])
            k.op("dve", lambda e: e.tensor_scalar(out=imp, in0=Uc[:, 0, 65:193], scalar1=rzc[:, 0:1], scalar2=None, op0=ALU.mult),
                 reads=[R_Uc, R_cols], writes=[R_sel])
            for a in range(1, 4):
                k.op("dve", lambda e, a=a: e.scalar_tensor_tensor(out=imp, in0=Uc[:, a, 65:193], scalar=rzc[:, a:a + 1], in1=imp,
                                                                  op0=ALU.mult, op1=ALU.add), reads=[R_Uc, R_cols, R_sel], writes=[R_sel])
            k.op("dve", lambda e: e.tensor_tensor(out=score, in0=imp, in1=VALi[tb], op=ALU.mult), reads=[R_sel, R_tab[tb]], writes=[R_sel])
            k.op("dve", lambda e: e.tensor_tensor(out=score, in0=score, in1=ADDCi[tb], op=ALU.add), reads=[R_sel, R_tab[tb]], writes=[R_sel])
            m8a, m8b = cols[:, 8:16], cols[:, 16:24]
            k.op("dve", lambda e: e.max(out=m8a, in_=score), reads=[R_sel], writes=[R_cols])
            k.op("dve", lambda e: e.match_replace(out=sc2, in_to_replace=m8a, in_values=score, imm_value=-1e9), reads=[R_sel, R_cols], writes=[R_sel])
            k.op("dve", lambda e: e.max(out=m8b, in_=sc2), reads=[R_sel], writes=[R_cols])
            k.op("dve", lambda e: e.tensor_scalar(out=selb, in0=score, scalar1=m8b[:, 7:8], scalar2=NEG, op0=ALU.is_lt, op1=ALU.mult),
                 reads=[R_sel, R_cols], writes=[R_selb])
            for r in range(4):
                kt = 4 * i + r
                k.op("dve", lambda e, r=r, kt=kt: e.tensor_tensor(
                    out=bd[:, r, :].rearrange("p (b x) -> p b x", b=2), in0=CM[:, r, :].rearrange("p (b x) -> p b x", b=2),
                    in1=selb[:, 2 * kt:2 * kt + 2].unsqueeze(2).broadcast_to([128, 2, 64]), op=ALU.add),
                    reads=[R_cst, R_selb], writes=[R_bd])
            if i > 0:
                for hx in range(2):
                    b0_, b1_ = 4 * i * hx, 4 * i * (hx + 1)
                    k.op("dve", lambda e, b0_=b0_, b1_=b1_: e.tensor_copy(
                        out=selX[:, b0_:b1_, :], in_=selb[:, b0_:b1_].unsqueeze(2).broadcast_to([128, b1_ - b0_, 64])),
                        reads=[R_selb], writes=[R_selX[hx]])
            k.op("dve", lambda e: e.memset(c.psum[3], 0.0), writes=[c.R_ps[3]])
            outsS = [pO_s[:, a * 65:(a + 1) * 65] for a in range(4)]
            for kt in list(range(4 * i, 4 * i + 4)) + list(range(4 * i)):
                if kt < 4 * i:
                    bl = selX[:, 2 * kt:2 * kt + 2, :].rearrange("p b x -> p (b x)")
                    bias = (bl, R_selX[0 if kt < 2 * i else 1])
                else:
                    bias = (bd[:, kt - 4 * i, :], R_bd)
                unit(St.KTs, St.R_KTs, slice(kt * 128, (kt + 1) * 128), QTg, g, bias, St.VsA[:, kt, g, :], St.R_VsA, outsS, [c.R_ps[3]])
            flush_pv()
            k.op("act", lambda e: e.activation(out=Os, in_=pO_s.rearrange("p (a x) -> p a x", a=4), func=AF.Copy), reads=[c.R_ps[3]], writes=[R_Os])
            k.op("act", lambda e: e.activation(out=Ow, in_=pO_w.rearrange("p (a x) -> p a x", a=4), func=AF.Copy), reads=[c.R_ps[4]], writes=[R_Ow])
            gs = c.gsig[:, i, 12 * g:12 * g + 12].rearrange("p (a x) -> p a x", a=4)
            zs, zw, cfc, cfs, cfw = cols[:, 24:28], cols[:, 28:32], cols[:, 32:36], cols[:, 36:40], cols[:, 40:44]
            k.op("dve", lambda e: e.tensor_scalar(out=zs, in0=Os[:, :, 64], scalar1=TINY, scalar2=None, op0=ALU.max), reads=[R_Os], writes=[R_cols])
            k.op("dve", lambda e: e.tensor_scalar(out=zw, in0=Ow[:, :, 64], scalar1=TINY, scalar2=None, op0=ALU.max), reads=[R_Ow], writes=[R_cols])
            k.op("dve", lambda e: e.reciprocal(out=zs, in_=zs), reads=[R_cols], writes=[R_cols])
            k.op("dve", lambda e: e.reciprocal(out=zw, in_=zw), reads=[R_cols], writes=[R_cols])
            k.op("dve", lambda e: e.tensor_tensor(out=cfc, in0=rzc, in1=gs[:, :, 0], op=ALU.mult), reads=[R_cols, c.R_gsig], writes=[R_cols])
            k.op("dve", lambda e: e.tensor_tensor(out=cfs, in0=zs, in1=gs[:, :, 1], op=ALU.mult), reads=[R_cols, c.R_gsig], writes=[R_cols])
            k.op("dve", lambda e: e.tensor_tensor(out=cfw, in0=zw, in1=gs[:, :, 2], op=ALU.mult), reads=[R_cols, c.R_gsig], writes=[R_cols])
            bc = lambda col: col.unsqueeze(2).broadcast_to([128, 4, 64])
            k.op("dve", lambda e: e.tensor_tensor(out=acc, in0=Uc[:, :, 0:64], in1=bc(cfc), op=ALU.mult), reads=[R_Uc, R_cols], writes=[R_acc])
            k.op("dve", lambda e: e.tensor_tensor(out=tmp, in0=Os[:, :, 0:64], in1=bc(cfs), op=ALU.mult), reads=[R_Os, R_cols, R_acc], writes=[R_acc])
            k.op("pool", lambda e: e.tensor_tensor(out=acc, in0=acc, in1=tmp, op=ALU.add), reads=[R_acc], writes=[R_acc])
            k.op("dve", lambda e: e.tensor_tensor(out=tmp, in0=Ow[:, :, 0:64], in1=bc(cfw), op=ALU.mult), reads=[R_Ow, R_cols, R_acc], writes=[R_acc])
            k.op("pool", lambda e, g=g: e.tensor_tensor(out=onsa[:, g, :, :], in0=acc, in1=tmp, op=ALU.add), reads=[R_acc], writes=[R_onsa])
        p7b = c.psum[7].bitcast(BF16)
        of = onsa.rearrange("p g a d -> p (g a d)")
        k.op("pe", [(lambda e, j=j: e.transpose(out=p7b[:, j * 128:(j + 1) * 128], in_=of[:, j * 128:(j + 1) * 128], identity=c.identb))
                    for j in range(4)], reads=[R_onsa, c.R_const], writes=[c.R_ps[7]])
        k.op("act", lambda e, i=i: e.activation(out=c.oT_nsa[:, :, i * 128:(i + 1) * 128],
                                                in_=p7b[:, 0:512].rearrange("p (j x) -> p j x", j=4), func=AF.Copy),
             reads=[c.R_ps[7]], writes=[c.R_oTnsa])
    ar.release(m0)


def phase_p2c(k, c, ar, X1, R_X1):
    Dr = c.D
    m0 = ar.mark()
    W = WS()
    scr = ar.al([128, D], F32)
    alloc_x_ws(k, c, ar, W, None, scr=scr, R_scr=k.res("scr2c"))
    hT = ar.al([128, 8, 512], BF16)
    R_hT = k.res("hT3")
    wbn, wbh = ar.al([128, 4, D], BF16), ar.al([128, 4, D], BF16)
    R_wb_ = k.res("wbr")
    wb = [ar.al([128, 8, 512], BF16) for _ in range(4)]
    R_wb = [k.res("wchc%d" % i) for i in range(4)]
    mixT = ar.al([128, 8, 512], BF16)
    R_mixT = k.res("mixT")
    sg1, sg2, mx1 = ar.al([128, 512], F32), ar.al([128, 512], F32), ar.al([128, 512], F32)
    R_sg1, R_sg2, R_mx = k.res("sg1"), k.res("sg2"), k.res("mx1")
    k.dma("pool", wbn, Dr["w_brn"].rearrange("(kt p) n -> p kt n", p=128), writes=[R_wb_])
    k.dma("pool", wbh, Dr["w_brh"].rearrange("(kt p) n -> p kt n", p=128), writes=[R_wb_])

    def wl(buf, src, c0):
        k.dma("pool", wb[buf], src[:, c0:c0 + 512].rearrange("(kt p) n -> p kt n", p=128), writes=[R_wb[buf]])

    NGo = NT // 4

    def xprep(g_):
        for tt in range(4):
            t = 4 * g_ + tt
            k.dma("sp", X1[:, t, :], Dr["xo"][t * 128:(t + 1) * 128, :], writes=[R_X1[t]])
        norm_transpose_group(k, c, W, Dr["xo"], g_ * 512, hT, R_hT)

    wl(0, Dr["w_mg"], 0)
    wl(1, Dr["w_mg"], 1024)
    for go in range(NGo):
        p = go % 2
        A = (2 * p, 2 * p + 1)
        B = (2 - 2 * p, 3 - 2 * p)
        tok = slice(go * 512, (go + 1) * 512)
        wl(B[0], Dr["w_mg"], 512)
        wl(B[1], Dr["w_mg"], 1024 + 512)
        if go == 0:
            xprep(0)
        for hf in range(2):
            w0, w1_ = (A if hf == 0 else B)
            for f4 in range(4):
                ft = hf * 4 + f4
                fs = slice(f4 * 128, (f4 + 1) * 128)
                gs_ = slice(ft * 128, (ft + 1) * 128)
                k.op("pe", [(lambda e, i=i: e.matmul(c.psum[2], lhsT=wb[w0][:, i, fs], rhs=hT[:, i, :], start=(i == 0), stop=(i == 7)))
                            for i in range(8)], reads=[R_wb[w0], R_hT], writes=[c.R_ps[2]])
                k.op("pe", [(lambda e, i=i: e.matmul(c.psum[3], lhsT=wb[w1_][:, i, fs], rhs=hT[:, i, :], start=(i == 0), stop=(i == 7)))
                            for i in range(8)], reads=[R_wb[w1_], R_hT], writes=[c.R_ps[3]])
                k.op("pe", [(lambda e, i=i: e.matmul(c.psum[4], lhsT=wbn[:, i, gs_], rhs=c.oT_nsa[:, i, tok], start=(i == 0), stop=(i == 3)))
                            for i in range(4)], reads=[R_wb_, c.R_oTnsa], writes=[c.R_ps[4]])
                k.op("pe", [(lambda e, i=i: e.matmul(c.psum[5], lhsT=wbh[:, i, gs_], rhs=c.oT_hg[:, i, tok], start=(i == 0), stop=(i == 3)))
                            for i in range(4)], reads=[R_wb_, c.R_oThg], writes=[c.R_ps[5]])
                k.op("act", lambda e: e.activation(out=sg1, in_=c.psum[2], func=AF.Sigmoid), reads=[c.R_ps[2]], writes=[R_sg1])
                k.op("act", lambda e: e.activation(out=sg2, in_=c.psum[3], func=AF.Sigmoid), reads=[c.R_ps[3]], writes=[R_sg2])
                k.op("dve", lambda e: e.tensor_tensor(out=mx1, in0=c.psum[4], in1=sg1, op=ALU.mult), reads=[c.R_ps[4], R_sg1], writes=[R_mx])
                k.op("dve", lambda e: e.tensor_tensor(out=sg2, in0=c.psum[5], in1=sg2, op=ALU.mult), reads=[c.R_ps[5], R_sg2], writes=[R_sg2])
                k.op("pool", lambda e, ft=ft: e.tensor_tensor(out=mixT[:, ft, :], in0=mx1, in1=sg2, op=ALU.add),
                     reads=[R_mx, R_sg2], writes=[R_mixT])
            if hf == 0:
                wl(A[0], Dr["w_out"], 0)
                wl(A[1], Dr["w_out"], 512)
            elif go + 1 < NGo:
                wl(B[0], Dr["w_mg"], 0)
                wl(B[1], Dr["w_mg"], 1024)
        if go + 1 < NGo:
            xprep(go + 1)
        for tt in range(4):
            t = 4 * go + tt
            for hf in range(2):
                bank = 6 + hf
                k.op("pe", [(lambda e, i=i: e.matmul(c.psum[bank], lhsT=mixT[:, i, tt * 128:(tt + 1) * 128], rhs=wb[A[hf]][:, i, :],
                                                     start=(i == 0), stop=(i == 7))) for i in range(8)],
                     reads=[R_mixT, R_wb[A[hf]]], writes=[c.R_ps[bank]])
                xs = X1[:, t, hf * 512:(hf + 1) * 512]
                k.op("dve", lambda e, xs=xs: e.tensor_tensor(out=xs, in0=c.psum[bank], in1=xs, op=ALU.add),
                     reads=[c.R_ps[bank], R_X1[t]], writes=[R_X1[t]])
    ar.release(m0)


def phase_attn(k, c, ar, stage):
    St = WS()
    St.KTs, St.KTw = ar.al([128, SEQ], BF16), ar.al([128, SEQ], BF16)
    St.VsA, St.VwA = ar.al([128, 64, 2, 65], BF16), ar.al([128, 64, 2, 65], BF16)
    St.KC = ar.al([128, 512], BF16)
    St.VCA = ar.al([128, 4, 2, 193], BF16)
    St.SNAP = ar.al([128, NT, 4, 128], BF16)
    for n in ("KTs", "KTw", "VsA", "VwA", "KC", "VCA", "SNAP"):
        setattr(St, "R_" + n, k.res(n))
    phase_p1(k, c, ar, St)
    for n in ("KTs", "KTw", "VsA", "VwA", "KC", "VCA", "SNAP"):
        c.dump(n, getattr(St, n), [getattr(St, "R_" + n)])
    k.flush()
    phase_p2pre(k, c, ar, St)
    c.dump("QT", c.QT, [c.R_QT])
    c.dump("gsig", c.gsig, [c.R_gsig])
    c.dump("oT_hg", c.oT_hg, [c.R_oThg])
    k.flush()
    phase_nsa(k, c, ar, St)
    c.dump("oT_nsa", c.oT_nsa, [c.R_oTnsa])
    return St


def build(stage="full", n_exp=NE):
    nc = bass.Bass("TRN2", target_bir_lowering=False)
    Dr = {}

    def din(name, shape, dt=F32):
        Dr[name] = nc.dram_tensor(name, list(shape), dt, kind="ExternalInput").ap()

    for name, shape, dt in input_specs(max(n_exp, 1), stage):
        din(name, shape, dt)
    Dr["y"] = nc.dram_tensor("y", [NTOK, D], F32, kind="ExternalOutput").ap()
    with ExitStack() as es:
        ARENA_BYTES = 207 * 1024
        big = es.enter_context(nc.sbuf_tensor("arena", [128, ARENA_BYTES // 2], BF16))
        pst = es.enter_context(nc.psum_tensor("ps", [128, 4096], F32))
        ar = Arena(big, ARENA_BYTES)
        k = KH(nc, es)
        k.oplim = _NC_CACHE.get("oplim", 10 ** 9)
        c = Ctx()
        c.nc, c.D, c.n_exp = nc, Dr, n_exp
        dumps = []

        def dump(name, ap, rs):
            if not _NC_CACHE.get("dbg"):
                return
            dt_ = ap.dtype
            dr = nc.dram_tensor("dbg_" + name, list(ap.shape), dt_, kind="ExternalOutput").ap()
            r = k.res("dbg_" + name)
            k.dma("sp", dr, ap, reads=list(rs), key=r)
            dumps.append(r)
        c.dump = dump
        c.psum = [pst[:, i * 512:(i + 1) * 512] for i in range(8)]
        c.pwide = lambda i: pst[:, i * 512:(i + 2) * 512]
        c.R_ps = [k.res("psb%d" % i, excl=True) for i in range(8)]
        c.R_const = k.res("const")
        c.identf = ar.al([128, 128], F32)
        c.identb = ar.al([128, 128], BF16)
        c.epsb = ar.al([128, 1], F32)
        c.ssq = ar.al([128, 8], F32)
        c.std = ar.al([128, 8], F32)
        c.rstd = ar.al([128, 8], F32)
        c.rs_res = [k.res("rs%d" % i) for i in range(8)]
        c.rs_i = 0
        k.dma("sp", c.identf, Dr["identf"], writes=[c.R_const])
        k.dma("sp", c.identb, Dr["identb"], writes=[c.R_const])
        k.op("dve", lambda e: e.memset(c.epsb, EPS), writes=[c.R_const])
        c.n_groups = _NC_CACHE.get("n_groups", 16)
        c.n_blocks = _NC_CACHE.get("n_blocks", NT)
        c.R32 = ar.al([128, 8, NTOK], BF16)
        c.R32f = c.R32.rearrange("p a b -> p (a b)")
        c.QT = c.R32f[:, 0:8192].rearrange("p (i a t) -> p i a t", i=NT, a=4)
        c.oT_nsa = c.R32[:, 4:8, :]
        c.oT_hg = ar.al([128, 4, NTOK], BF16)
        c.gsig = ar.al([128, NT, 24], F32)
        c.R_QT, c.R_oTnsa, c.R_oThg, c.R_gsig = k.res("QT"), k.res("oTnsa"), k.res("oThg"), k.res("gsig")
        hfT = c.R32
        R_hfT = [k.res("hfT_%d" % t) for t in range(NT)]
        R_X1 = [k.res("x1_%d" % t) for t in range(NT)]
        if stage != "moe_only":
            c.I4 = ar.al([128, 512], BF16)
            c.tri = ar.al([128, 128], BF16)
            c.onehot = ar.al([128, 4], F32)
            c.R_constP = k.res("constP")
            k.dma("pool", c.I4, Dr["I4"], writes=[c.R_constP])
            k.dma("pool", c.tri, Dr["tri"], writes=[c.R_constP])
            k.dma("sp", c.onehot, Dr["onehot"], writes=[c.R_const])
            setup_lb(k, c, ar)
            M1 = ar.mark()
            phase_attn(k, c, ar, stage)
            for r in dumps:
                k.rec["sp"].append(("w", r.dsem, r.dcnt))
            k.flush()
            ar.release(M1)
        X1 = ar.al([128, NT, D], F32)
        if stage == "moe_only":
            for t in range(NT):
                k.dma("sp", X1[:, t, :], Dr["xo"][t * 128:(t + 1) * 128, :], writes=[R_X1[t]])
        else:
            phase_p2c(k, c, ar, X1, R_X1)
            c.dump("X1", X1, R_X1)
            for r in dumps:
                if r.name == "dbg_X1":
                    k.rec["sp"].append(("w", r.dsem, r.dcnt))
        k.flush()
        if n_exp >= 0:
            phase_moe(k, c, ar, X1, R_X1, hfT, R_hfT)
            k.flush()
        phase_final(k, c, ar, X1, R_X1)
        k.flush()
    return nc


def input_specs(ne=NE, stage="full"):
    return [
        ("xb", (SEQ, D), F32), ("xo", (NTOK, D), F32),
        ("g_ffn", (D,), F32), ("g_fin", (D,), F32),
        ("w_r", (D, 36), F32), ("b_r", (36,), F32),
        ("w_gate", (ne, D, DFF), F32), ("w_up", (ne, D, DFF), F32), ("w_down", (ne, DFF, D), F32),
        ("identf", (128, 128), F32), ("identb", (128, 128), BF16),
    ] + (attn_input_specs() if stage != "moe_only" else [])


_NC_CACHE = {}


def host_inputs(inp, core, ne=NE):
    b, cp = core // 4, core % 4
    x = np.asarray(inp["x"], dtype=np.float32)
    m = {}
    m["xb"] = np.ascontiguousarray(x[b])
    m["xo"] = np.ascontiguousarray(x[b].reshape(NT, 4, 128, D)[:, cp].reshape(NTOK, D))
    m["g_ffn"] = np.ascontiguousarray(inp["ffn_norm"][0])
    m["g_fin"] = np.ascontiguousarray(inp["final_norm"])
    m["w_r"] = np.ascontiguousarray(np.concatenate([inp["w_grp"][0], inp["w_rtr"][0]], axis=1))
    m["b_r"] = np.ascontiguousarray(np.concatenate([inp["b_grp"][0], inp["b_rtr"][0]], axis=0))
    m["w_gate"] = np.ascontiguousarray(inp["w_gate"][0, :ne])
    m["w_up"] = np.ascontiguousarray(inp["w_up"][0, :ne])
    m["w_down"] = np.ascontiguousarray(inp["w_down"][0, :ne])
    m["identf"] = np.eye(128, dtype=np.float32)
    m["identb"] = np.eye(128, dtype=np.float32).astype(ml_dtypes.bfloat16)
    if _NC_CACHE.get("stage", "full") != "moe_only":
        m.update(attn_host_inputs(inp, b, cp))
    return m


def kernel(**inp):
    inp = {k_: np.asarray(v) for k_, v in inp.items()}
    stage = _NC_CACHE.get("stage", "full")
    key = ("nc", stage)
    if key not in _NC_CACHE:
        _NC_CACHE[key] = build(stage, _NC_CACHE.get("n_exp", NE))
    nc = _NC_CACHE[key]
    shared = None
    in_maps = []
    for core in range(8):
        m = host_inputs(inp, core, max(_NC_CACHE.get("n_exp", NE), 1))
        if shared is None:
            shared = m
        else:
            for kk in ("w_gate", "w_up", "w_down"):
                m[kk] = shared[kk]
        in_maps.append(m)
    res = run_bass_kernel_spmd(nc, in_maps, core_ids=list(range(8)))
    _NC_CACHE["last_results"] = res.results
    out = np.zeros((2, SEQ // 128, 128, D), dtype=np.float32)
    for core in range(8):
        b, cp = core // 4, core % 4
        y = np.asarray(res.results[core]["y"]).reshape(NT, 128, D)
        out[b, cp::4] = y
    return out.reshape(2, SEQ, D)
```
